# Optimizing a Trainium2 kernel written in Bass

```python
import math
import jax, jax.numpy as jnp
from jax import lax
import numpy as np


D_MODEL = 1024
BATCH = 32
SEQ = 2048
DEPTH = 1

CHUNK = 64
A_HEADS = 8
A_LATENT = 128
IDX_HEADS = 8
IDX_DIM = 64
TOPK_KEYS_MAX = 256
A_QBLOCK = 64
REL_BUCKETS = 32
REL_MAX_DIST = 128
SSM_D_INNER = 1024
SSM_HEADDIM = 64
SSM_HEADS = SSM_D_INNER // SSM_HEADDIM
SSM_GROUPS = 4
SSM_STATE = 128
SSM_CONV = 4
SSM_CONV_DIM = SSM_D_INNER + 2 * SSM_GROUPS * SSM_STATE
DT_MIN = 0.001
DT_MAX = 0.1
N_EXPERTS = 32
TOPK_EXPERTS = 4
D_FF_EXPERT = D_MODEL
SWIGLU_LIMIT = 7.0
SWIGLU_ALPHA = 1.702
MOE_BLOCK = 256
DEEPNORM_ALPHA = (2.0 * DEPTH) ** 0.25
DEEPNORM_BETA = (8.0 * DEPTH) ** -0.25
LN_EPS = 1e-5
IN_SPLITS = (A_HEADS * A_LATENT, A_LATENT, IDX_HEADS * IDX_DIM, IDX_DIM, IDX_HEADS,
             SSM_D_INNER, SSM_CONV_DIM, SSM_HEADS, D_MODEL, D_MODEL)
D_IN_PROJ = sum(IN_SPLITS)

kernel_name = "hybrid_dsa_ssd_moe_streaming_block"


def layer_norm(x, eps=LN_EPS):
    x32 = x.astype(jnp.float32)
    mu = jnp.mean(x32, -1, keepdims=True)
    var = jnp.mean(jnp.square(x32 - mu), -1, keepdims=True)
    return ((x32 - mu) * lax.rsqrt(var + eps)).astype(x.dtype)


def rms_norm(x, w, eps=LN_EPS):
    x32 = x.astype(jnp.float32)
    y = x32 * lax.rsqrt(jnp.mean(jnp.square(x32), -1, keepdims=True) + eps)
    return y.astype(x.dtype) * w


def t5_bucket(rel):
    half = REL_BUCKETS // 2
    max_exact = half // 2
    ret = (rel > 0).astype(jnp.int32) * half
    n = jnp.abs(rel)
    nf = jnp.maximum(n, 1).astype(jnp.float32)
    large = max_exact + (jnp.log(nf / max_exact) / math.log(REL_MAX_DIST / max_exact)
                         * (half - max_exact)).astype(jnp.int32)
    large = jnp.minimum(large, half - 1)
    return ret + jnp.where(n < max_exact, n, large)


def sparse_indexed_attention(q, kv, iq, ik, iw, rel_bias):
    bsz, seq = kv.shape[0], kv.shape[1]
    n_sel = min(TOPK_KEYS_MAX, seq // 4)
    n_blk = seq // A_QBLOCK
    key_chunk = jnp.arange(seq, dtype=jnp.int32) // CHUNK
    q_pos = jnp.arange(seq, dtype=jnp.int32).reshape(n_blk, A_QBLOCK)

    def blocks(a):
        return jnp.swapaxes(a.reshape(bsz, n_blk, A_QBLOCK, *a.shape[2:]), 0, 1)

    gather_keys = jax.vmap(lambda a, i: a[i])

    def one_block(args):
        qb, iqb, iwb, pos = args
        q_chunk = pos // CHUNK
        idx = jnp.einsum('bqhd,bsd->bqhs', iqb, ik) * IDX_DIM ** -0.5
        score = jnp.einsum('bqhs,bqh->bqs', jax.nn.relu(idx), iwb).astype(jnp.float32)
        admissible = key_chunk[None, :] <= q_chunk[:, None]
        score = jnp.where(admissible[None], score, -jnp.inf)
        _, sel = lax.top_k(score, n_sel)
        kv_sel = gather_keys(kv, sel)
        logits = jnp.einsum('bqhc,bqkc->bqhk', qb, kv_sel).astype(jnp.float32) * A_LATENT ** -0.5
        bias = rel_bias[t5_bucket(sel - pos[None, :, None])].astype(jnp.float32)
        logits = logits + jnp.transpose(bias, (0, 1, 3, 2))
        valid = (sel // CHUNK) <= q_chunk[None, :, None]
        logits = jnp.where(valid[:, :, None, :], logits, -jnp.inf)
        probs = jax.nn.softmax(logits, axis=-1).astype(kv.dtype)
        return jnp.einsum('bqhk,bqkc->bqhc', probs, kv_sel)

    out = lax.map(one_block, (blocks(q), blocks(iq), blocks(iw), q_pos))
    return jnp.swapaxes(out, 0, 1).reshape(bsz, seq, A_HEADS * A_LATENT)


def segsum(a):
    t = a.shape[-1]
    cs = jnp.cumsum(a, axis=-1)
    d = cs[..., :, None] - cs[..., None, :]
    return jnp.where(jnp.tril(jnp.ones((t, t), dtype=bool)), d, -jnp.inf)


def ssd_mixer(z, xbc, dt_raw, conv_w, conv_b, dt_bias, a_log, d_skip, norm_w):
    bsz, seq = xbc.shape[0], xbc.shape[1]
    n_ch = seq // CHUNK
    hpg = SSM_HEADS // SSM_GROUPS
    xbc = lax.conv_general_dilated(xbc, conv_w[:, None, :], window_strides=(1,),
                                   padding=((SSM_CONV - 1, 0),),
                                   dimension_numbers=('NWC', 'WIO', 'NWC'),
                                   feature_group_count=SSM_CONV_DIM) + conv_b
    xbc = jax.nn.silu(xbc)
    xs, bm, cm = jnp.split(xbc, [SSM_D_INNER, SSM_D_INNER + SSM_GROUPS * SSM_STATE], axis=-1)
    xs = xs.reshape(bsz, n_ch, CHUNK, SSM_GROUPS, hpg, SSM_HEADDIM)
    bm = bm.reshape(bsz, n_ch, CHUNK, SSM_GROUPS, SSM_STATE)
    cm = cm.reshape(bsz, n_ch, CHUNK, SSM_GROUPS, SSM_STATE)
    dt = jax.nn.softplus((dt_raw + dt_bias).astype(jnp.float32))
    a = -jnp.exp(a_log.astype(jnp.float32))
    dt = dt.reshape(bsz, n_ch, CHUNK, SSM_GROUPS, hpg)
    a_dt = jnp.transpose(dt * a.reshape(SSM_GROUPS, hpg), (0, 3, 4, 1, 2))
    xdt = xs * dt[..., None]
    a_cs = jnp.cumsum(a_dt, axis=-1)
    lmat = jnp.exp(segsum(a_dt))
    cb = jnp.einsum('bclgn,bcsgn->bcgls', cm, bm)
    y_diag = jnp.einsum('bcgls,bgrcls,bcsgrp->bclgrp', cb, lmat, xdt)
    decay_states = jnp.exp(a_cs[..., -1:] - a_cs)
    states = jnp.einsum('bclgn,bgrcl,bclgrp->bcgrpn', bm, decay_states, xdt)
    chunk_decay = jnp.exp(a_cs[..., -1])

    def step(h, inp):
        st, dec = inp
        return h * dec[..., None, None] + st, h

    h0 = jnp.zeros((bsz, SSM_GROUPS, hpg, SSM_HEADDIM, SSM_STATE), states.dtype)
    _, prev = lax.scan(step, h0, (jnp.swapaxes(states, 0, 1), jnp.moveaxis(chunk_decay, -1, 0)))
    prev = jnp.swapaxes(prev, 0, 1)
    y_off = jnp.einsum('bclgn,bcgrpn,bgrcl->bclgrp', cm, prev, jnp.exp(a_cs))
    y = y_diag + y_off + xs * d_skip.reshape(SSM_GROUPS, hpg)[:, :, None]
    y = y.reshape(bsz, seq, SSM_D_INNER) * jax.nn.silu(z)
    yg = y.reshape(bsz, seq, SSM_GROUPS, SSM_D_INNER // SSM_GROUPS).astype(jnp.float32)
    yg = yg * lax.rsqrt(jnp.mean(jnp.square(yg), -1, keepdims=True) + LN_EPS)
    return yg.reshape(bsz, seq, SSM_D_INNER).astype(z.dtype) * norm_w


def moe_ffn(u, w_router, b_router, w1, b1, w2, b2):
    bsz, seq, d = u.shape
    n_tok = bsz * seq
    n_asg = n_tok * TOPK_EXPERTS
    xt = u.reshape(n_tok, d)
    logits = (xt @ w_router + b_router).astype(jnp.float32)
    top_val, top_idx = lax.top_k(logits, TOPK_EXPERTS)
    gates = jax.nn.softmax(top_val, axis=-1).astype(u.dtype)
    e_flat = top_idx.reshape(n_asg)
    tok_flat = jnp.arange(n_asg, dtype=jnp.int32) // TOPK_EXPERTS
    g_flat = gates.reshape(n_asg)
    order = jnp.argsort(e_flat)
    e_sorted = e_flat[order]
    counts = jax.ops.segment_sum(jnp.ones((n_asg,), jnp.int32), e_flat, num_segments=N_EXPERTS)
    starts = jnp.cumsum(counts) - counts
    padded = (counts + MOE_BLOCK - 1) // MOE_BLOCK * MOE_BLOCK
    pends = jnp.cumsum(padded)
    pstarts = pends - padded
    dest = pstarts[e_sorted] + jnp.arange(n_asg, dtype=jnp.int32) - starts[e_sorted]
    n_blocks = n_asg // MOE_BLOCK + N_EXPERTS
    n_slots = n_blocks * MOE_BLOCK
    slot_tok = jnp.zeros((n_slots,), jnp.int32).at[dest].set(tok_flat[order])
    slot_gate = jnp.zeros((n_slots,), u.dtype).at[dest].set(g_flat[order])
    block_start = jnp.arange(n_blocks, dtype=jnp.int32) * MOE_BLOCK
    block_expert = jnp.minimum(jnp.sum(block_start[:, None] >= pends[None, :], axis=1), N_EXPERTS - 1)

    def expert_block(acc, inp):
        tok, g, e = inp
        h = xt[tok] @ w1[e] + b1[e]
        gate, up = h[:, :D_FF_EXPERT], h[:, D_FF_EXPERT:]
        gate = jnp.minimum(gate, SWIGLU_LIMIT)
        up = jnp.clip(up, -SWIGLU_LIMIT, SWIGLU_LIMIT)
        act = (up + 1.0) * gate * jax.nn.sigmoid(SWIGLU_ALPHA * gate)
        y = act @ w2[e] + b2[e]
        return acc.at[tok].add(y * g[:, None]), None

    acc, _ = lax.scan(expert_block, jnp.zeros((n_tok, d), xt.dtype),
                      (slot_tok.reshape(n_blocks, MOE_BLOCK), slot_gate.reshape(n_blocks, MOE_BLOCK),
                       block_expert))
    return acc.reshape(bsz, seq, d)


def setup_inputs(seed: int = 0) -> dict:
    key = jax.random.key(seed)
    ks = jax.random.split(key, 28)

    def nrm(k, shape, scale):
        return jax.random.normal(k, shape, jnp.float32) * scale

    u_dt = jax.random.uniform(ks[11], (DEPTH, SSM_HEADS), jnp.float32)
    dt0 = jnp.exp(u_dt * (math.log(DT_MAX) - math.log(DT_MIN)) + math.log(DT_MIN))
    return {
        'x': nrm(ks[0], (BATCH, SEQ, D_MODEL), 1.0),
        'c': nrm(ks[1], (BATCH, D_MODEL), 1.0),
        'w_mod': nrm(ks[2], (DEPTH, D_MODEL, 6 * D_MODEL), 0.5 * D_MODEL ** -0.5),
        'b_mod': nrm(ks[3], (DEPTH, 6 * D_MODEL), 0.02),
        'w_in': nrm(ks[4], (DEPTH, D_MODEL, D_IN_PROJ), D_MODEL ** -0.5),
        'kv_norm_w': 1.0 + nrm(ks[5], (DEPTH, A_LATENT), 0.1),
        'idx_k_norm_w': 1.0 + nrm(ks[6], (DEPTH, IDX_DIM), 0.1),
        'idx_k_norm_b': nrm(ks[7], (DEPTH, IDX_DIM), 0.02),
        'rel_bias': nrm(ks[8], (REL_BUCKETS, A_HEADS), 0.5),
        'conv_w': nrm(ks[9], (DEPTH, SSM_CONV, SSM_CONV_DIM), SSM_CONV ** -0.5),
        'conv_b': nrm(ks[10], (DEPTH, SSM_CONV_DIM), 0.02),
        'dt_bias': dt0 + jnp.log(-jnp.expm1(-dt0)),
        'a_log': jnp.log(jax.random.uniform(ks[12], (DEPTH, SSM_HEADS), jnp.float32, 1.0, 16.0)),
        'd_skip': 1.0 + nrm(ks[13], (DEPTH, SSM_HEADS), 0.1),
        'ssm_norm_w': 1.0 + nrm(ks[14], (DEPTH, SSM_D_INNER), 0.1),
        'w_proj_a': nrm(ks[15], (DEPTH, A_HEADS * A_LATENT, D_MODEL), (A_HEADS * A_LATENT) ** -0.5),
        'w_proj_b': nrm(ks[16], (DEPTH, SSM_D_INNER, D_MODEL), SSM_D_INNER ** -0.5),
        'w_out': nrm(ks[17], (DEPTH, D_MODEL, D_MODEL), DEEPNORM_BETA * D_MODEL ** -0.5),
        'ln1_g': 1.0 + nrm(ks[18], (DEPTH, D_MODEL), 0.1),
        'ln1_b': nrm(ks[19], (DEPTH, D_MODEL), 0.02),
        'w_router': nrm(ks[20], (DEPTH, D_MODEL, N_EXPERTS), D_MODEL ** -0.5),
        'b_router': nrm(ks[21], (DEPTH, N_EXPERTS), 0.01),
        'w1': nrm(ks[22], (DEPTH, N_EXPERTS, D_MODEL, 2 * D_FF_EXPERT), D_MODEL ** -0.5),
        'b1': nrm(ks[23], (DEPTH, N_EXPERTS, 2 * D_FF_EXPERT), 0.02),
        'w2': nrm(ks[24], (DEPTH, N_EXPERTS, D_FF_EXPERT, D_MODEL), DEEPNORM_BETA * D_FF_EXPERT ** -0.5),
        'b2': nrm(ks[25], (DEPTH, N_EXPERTS, D_MODEL), 0.02),
        'ln2_g': 1.0 + nrm(ks[26], (DEPTH, D_MODEL), 0.1),
        'ln2_b': nrm(ks[27], (DEPTH, D_MODEL), 0.02),
    }


def reference(x, c, w_mod, b_mod, w_in, kv_norm_w, idx_k_norm_w, idx_k_norm_b, rel_bias,
              conv_w, conv_b, dt_bias, a_log, d_skip, ssm_norm_w, w_proj_a, w_proj_b, w_out,
              ln1_g, ln1_b, w_router, b_router, w1, b1, w2, b2, ln2_g, ln2_b):
    bsz, seq, _ = x.shape
    split_at = np.cumsum(IN_SPLITS)[:-1].tolist()
    for l in range(DEPTH):
        mod = jax.nn.silu(c) @ w_mod[l] + b_mod[l]
        shift1, scale1, gate1, shift2, scale2, gate2 = [m[:, None, :] for m in jnp.split(mod, 6, axis=-1)]
        u = layer_norm(x) * (1.0 + scale1) + shift1
        proj = u @ w_in[l]
        q_a, kv_a, iq, ik, iw, z, xbc, dt_raw, g_a, g_b = jnp.split(proj, split_at, axis=-1)
        q_a = q_a.reshape(bsz, seq, A_HEADS, A_LATENT)
        kv_a = rms_norm(kv_a, kv_norm_w[l])
        iq = iq.reshape(bsz, seq, IDX_HEADS, IDX_DIM)
        ik = layer_norm(ik) * idx_k_norm_w[l] + idx_k_norm_b[l]
        iw = iw * IDX_HEADS ** -0.5
        o_a = sparse_indexed_attention(q_a, kv_a, iq, ik, iw, rel_bias)
        o_b = ssd_mixer(z, xbc, dt_raw, conv_w[l], conv_b[l], dt_bias[l], a_log[l],
                        d_skip[l], ssm_norm_w[l])
        merged = jax.nn.sigmoid(g_a) * (o_a @ w_proj_a[l]) + jax.nn.sigmoid(g_b) * (o_b @ w_proj_b[l])
        x = layer_norm(DEEPNORM_ALPHA * x + gate1 * (merged @ w_out[l])) * ln1_g[l] + ln1_b[l]
        u2 = layer_norm(x) * (1.0 + scale2) + shift2
        y = moe_ffn(u2, w_router[l], b_router[l], w1[l], b1[l], w2[l], b2[l])
        x = layer_norm(DEEPNORM_ALPHA * x + gate2 * y) * ln2_g[l] + ln2_b[l]
    return x
```

```python
import numpy as np
import concourse.bass as bass
import concourse.mybir as mybir
from concourse.bass_utils import run_bass_kernel_spmd

F32 = mybir.dt.float32
BF16 = mybir.dt.bfloat16
I32 = mybir.dt.int32
ALU = mybir.AluOpType
AF = mybir.ActivationFunctionType
AX = mybir.AxisListType

NCORES = 8
SEQ = 2048
D = 1024
NB = 4
NTOK = NB * SEQ
NT = NTOK // 128
TPS = SEQ // 128
DIN = 6872
C_Q, C_KV, C_IQ, C_IK, C_IW, C_Z, C_XBC, C_DT, C_GA, C_GB = 0, 1024, 1152, 1664, 1728, 1736, 2760, 4808, 4824, 5848
NE = 32
NBLK = NTOK * 4 // 512 + NE
ALPHA = 2.0 ** 0.25
EPS = 1e-5
NEG = -30000.0

ENGS = ("pe", "act", "dve", "pool", "sp")


class Res:
    __slots__ = ("name", "writers", "readers", "dsem", "dcount", "dram")

    def __init__(self, name):
        self.name = name
        self.dram = False
        self.writers = {}
        self.readers = {}
        self.dsem = None
        self.dcount = 0


class Sched:
    def __init__(self, nc):
        self.nc = nc
        self.eng = {"pe": nc.tensor, "act": nc.scalar, "dve": nc.vector,
                    "pool": nc.gpsimd, "sp": nc.sync}
        self.sem = {e: nc.alloc_semaphore("prog_" + e) for e in ENGS}
        self.cnt = {e: 0 for e in ENGS}
        self.waited = {e: {} for e in ENGS}
        self.all_res = []
        self.nwaits = 0
        self.nops = 0
        self.sempool = []

    def retire(self, rs):
        for r in rs:
            if r.dsem is not None:
                self.sempool.append((r.dsem, r.dcount))
                r.dsem = None
            if r in self.all_res:
                self.all_res.remove(r)

    def res(self, name):
        r = Res(name)
        self.all_res.append(r)
        return r

    def _need(self, eng, tok, deps):
        sem, val = tok
        k = sem.num
        if self.waited[eng].get(k, 0) >= val:
            return
        if k not in deps or deps[k][1] < val:
            deps[k] = (sem, val)

    def op(self, eng, fn, reads=(), writes=(), awrites=(), dma=False):
        deps = {}
        mykey = None if dma else eng
        for r in reads:
            for k, tok in r.writers.items():
                if k == mykey and eng == "pe":
                    continue
                self._need(eng, tok, deps)
        for r in writes:
            for k, tok in list(r.writers.items()) + list(r.readers.items()):
                if k == mykey:
                    continue
                self._need(eng, tok, deps)
        for r in awrites:
            for k, tok in r.readers.items():
                if k == mykey:
                    continue
                self._need(eng, tok, deps)
        e = self.eng[eng]
        for k, (sem, val) in deps.items():
            e.wait_ge(sem, val)
            self.waited[eng][k] = val
            self.nwaits += 1
        ins = fn(e)
        self.nops += 1
        if dma:
            dst = (list(writes) + list(awrites))[0]
            if dst.dram:
                sb = [r for r in reads if not r.dram]
                if sb:
                    dst = sb[0]
            if dst.dsem is None:
                if self.sempool:
                    dst.dsem, dst.dcount = self.sempool.pop()
                else:
                    dst.dsem = self.nc.alloc_semaphore("d_" + dst.name)
            dst.dcount += 16
            ins.then_inc(dst.dsem, 16)
            tok = (dst.dsem, dst.dcount)
            key = "dma%d" % dst.dsem.num
        else:
            self.cnt[eng] += 1
            ins.then_inc(self.sem[eng], 1)
            tok = (self.sem[eng], self.cnt[eng])
            key = eng
        for r in reads:
            r.readers[key] = tok
        for r in writes:
            r.writers = {key: tok}
            r.readers = {}
        for r in awrites:
            r.writers[key] = tok
        return ins

    def dma(self, eng, out, in_, reads=(), writes=(), awrites=(), **kw):
        return self.op(eng, lambda e: e.dma_start(out=out, in_=in_, **kw),
                       reads=reads, writes=writes, awrites=awrites, dma=True)

    def barrier(self):
        toks = {}
        for e in ENGS:
            if self.cnt[e]:
                toks[self.sem[e].num] = (self.sem[e], self.cnt[e])
        for r in self.all_res:
            if r.dsem is not None and r.dcount:
                toks[r.dsem.num] = (r.dsem, r.dcount)
        for e in ENGS:
            for k, (sem, val) in toks.items():
                if self.waited[e].get(k, 0) >= val:
                    continue
                self.eng[e].wait_ge(sem, val)
                self.waited[e][k] = val
                self.nwaits += 1
        for r in self.all_res:
            r.writers = {}
            r.readers = {}


class Ctx:
    def __init__(self, nc, S):
        self.nc = nc
        self.S = S
        self.stack = []

    def push(self):
        self.stack.append([])

    def pop(self):
        self.S.barrier()
        gs = self.stack.pop()
        self.S.retire([r for (_, r) in gs])
        for g, _ in reversed(gs):
            g.__exit__(None, None, None)

    def sb(self, name, shape, dt):
        g = self.nc.sbuf_tensor("s_" + name, list(shape), dt)
        t = g.__enter__()
        r = self.S.res(name)
        self.stack[-1].append((g, r))
        return t, r

    def ps(self, name, shape, dt=F32):
        g = self.nc.psum_tensor("p_" + name, list(shape), dt)
        t = g.__enter__()
        r = self.S.res(name)
        self.stack[-1].append((g, r))
        return t, r

    def ring(self, kind, name, n, shape, dt):
        f = self.sb if kind == "sb" else self.ps
        return [f("%s%d" % (name, i), shape, dt) for i in range(n)]


def build_program(debug=()):
    nc = bass.Bass("TRN2", target_bir_lowering=False)
    S = Sched(nc)
    C = Ctx(nc, S)
    dbg = set(debug)

    def din(name, shape, dt=F32):
        return nc.dram_tensor(name, list(shape), dt, kind="ExternalInput").ap()

    def scratch(name, shape, dt):
        kind = "ExternalOutput" if name in dbg else "Internal"
        r = S.res(name)
        r.dram = True
        return nc.dram_tensor(name, list(shape), dt, kind=kind).ap(), r

    x_d = din("x", [NTOK, D])
    cT_d = din("cT", [128, 8, NB])
    wmod_d = din("w_mod", [D, 6 * D])
    bmod_d = din("b_mod", [1, 6 * D])
    win_d = din("w_in", [D, DIN])
    ident_d = din("ident", [128, 128])
    kvw_d = din("kv_norm_w", [1, 128])
    ikw_d = din("idx_k_norm_w", [1, 64])
    ikb_d = din("idx_k_norm_b", [1, 64])
    dtb_d = din("dt_bias", [1, 16])
    convw_d = din("convw", [128, 16, 4])
    convb_d = din("convb", [128, 16])
    alog_d = din("a_log", [1, 16])
    dskip_d = din("d_skip", [1, 16])
    normw_d = din("ssm_norm_w", [1, D])
    triU_d = din("triU", [128, 128])
    SL_d = din("SL", [128, 128])
    negm4_d = din("negm4", [128, 512])
    tz_d = din("tz", [128, 2, 8, 128])
    cfar_d = din("cfar", [1, 8])
    wpa_d = din("w_proj_a", [D, D]); wpb_d = din("w_proj_b", [D, D]); wout_d = din("w_out", [D, D])
    ln1g_d = din("ln1_g", [1, D]); ln1b_d = din("ln1_b", [1, D]); ln2g_d = din("ln2_g", [1, D]); ln2b_d = din("ln2_b", [1, D])
    wr_d = din("w_router", [D, NE]); br_d = din("b_router", [1, NE])
    w1_d = din("w1", [NE * D, 2 * D]); w2_d = din("w2", [NE * D, D])
    b1r_d = din("b1r", [NE * 128, 16]); b2_d = din("b2", [NE, D])
    sut_d = din("sut", [128, 128]); thr16_d = din("thr16", [1, NE * 16]); bstart_d = din("bstart", [1, NBLK])
    kp_d = din("kp", [128, 8]); pcol_d = din("pcol", [128, 1]); sut32_d = din("sut32", [NE, NE])
    r_in = S.res("inputs")
    r_in.dram = True
    out_d = nc.dram_tensor("out", [NTOK, D], F32, kind="ExternalOutput").ap()
    r_out = S.res("out")
    r_out.dram = True

    mod_d, r_mod = scratch("mod_s", [NB, 6 * D], F32)
    qT_d, r_qT = scratch("qT_s", [8, 128, NTOK], BF16)
    iqT_d, r_iqT = scratch("iqT_s", [4, 128, NTOK], BF16)
    xbcT_d, r_xbcT = scratch("xbcT_s", [16, 128, NTOK], BF16)
    sgT_d, r_sgT = scratch("sgT_s", [16, 128, NTOK], BF16)
    kva_d, r_kva = scratch("kva_s", [NTOK, 136], BF16)
    kvT_d, r_kvT = scratch("kvT_s", [128, NTOK], BF16)
    ikT_d, r_ikT = scratch("ikT_s", [128, NTOK], BF16)
    iw_d, r_iw = scratch("iw_s", [NTOK, 8], F32)
    dt_d, r_dt = scratch("dt_s", [NTOK, 16], F32)
    zs_d, r_zs = scratch("zs_s", [NTOK, D], BF16)
    obT_d, r_obT = scratch("obT_s", [8, 128, NTOK], BF16)
    oaT_d, r_oaT = scratch("oaT_s", [8, 128, NTOK], BF16)
    mT_d, r_mTd = scratch("mT_s", [8, 128, NTOK], BF16)
    x1_d, r_x1 = scratch("x1_s", [NTOK, D], F32)
    u2_d, r_u2 = scratch("u2_s", [NTOK, D], BF16)
    xs_d, r_xsd = scratch("xsort_s", [NBLK * 512, D], BF16)
    ys_d, r_ysd = scratch("ysort_s", [NBLK * 512, D], BF16)

    w1b_d, r_w1bd = scratch("w1b_s", [NE * D, 2 * D], BF16)
    w2b_d, r_w2bd = scratch("w2b_s", [NE * D, D], BF16)

    C.push()
    ident_f, r_identf = C.sb("ident_f", [128, 128], F32)
    ident_b, r_identb = C.sb("ident_b", [128, 128], BF16)
    S.dma("sp", ident_f[:], ident_d, reads=[r_in], writes=[r_identf])
    S.op("dve", lambda e: e.tensor_copy(out=ident_b[:], in_=ident_f[:]), reads=[r_identf], writes=[r_identb])
    modT, r_modT = C.sb("modT", [128, 48, NB], F32)

    C.push()
    cT, r_cT = C.sb("cT", [128, 8, NB], F32)
    ones1, r_ones1 = C.sb("ones1", [1, NB], F32)
    bmod, r_bmod = C.sb("bmod", [1, 6 * D], F32)
    modrow, r_modrow = C.sb("modrow", [NB, 6 * D], F32)
    wm = C.ring("sb", "wm", 2, [128, 8, 512], F32)
    pmod = C.ring("ps", "pmod", 2, [NB, 512], F32)
    S.dma("sp", cT[:], cT_d, reads=[r_in], writes=[r_cT])
    S.dma("sp", bmod[:], bmod_d, reads=[r_in], writes=[r_bmod])
    S.op("act", lambda e: e.activation(out=cT[:], in_=cT[:], func=AF.Silu), reads=[r_cT], writes=[r_cT])
    S.op("dve", lambda e: e.memset(ones1[:], 1.0), writes=[r_ones1])
    for g in range(12):
        wt, r_wt = wm[g % 2]
        pt, r_pt = pmod[g % 2]
        S.dma("sp", wt[:], wmod_d[:, g * 512:(g + 1) * 512].rearrange("(k p) n -> p k n", p=128), reads=[r_in], writes=[r_wt])
        for k in range(8):
            S.op("pe", lambda e: e.matmul(pt[:], lhsT=cT[:, k, :], rhs=wt[:, k, :], start=(k == 0), stop=False),
                 reads=[r_cT, r_wt], writes=[r_pt] if k == 0 else (), awrites=() if k == 0 else [r_pt])
        S.op("pe", lambda e: e.matmul(pt[:], lhsT=ones1[:], rhs=bmod[:, g * 512:(g + 1) * 512], start=False, stop=True),
             reads=[r_ones1, r_bmod], awrites=[r_pt])
        S.op("act", lambda e: e.activation(out=modrow[:, g * 512:(g + 1) * 512], in_=pt[:], func=AF.Copy), reads=[r_pt], awrites=[r_modrow])
    S.dma("sp", mod_d, modrow[:], reads=[r_modrow], writes=[r_mod])
    pmt, r_pmt = pmod[0]
    pmt2, r_pmt2 = C.ps("pmodT", [128, 48 * NB], F32)
    for j in range(48):
        S.op("pe", lambda e: e.transpose(out=pmt2[:, j * NB:(j + 1) * NB], in_=modrow[:, j * 128:(j + 1) * 128], identity=ident_f[0:NB, 0:NB]),
             reads=[r_modrow, r_identf], writes=[r_pmt2] if j == 0 else (), awrites=() if j == 0 else [r_pmt2])
    S.op("act", lambda e: e.activation(out=modT[:].rearrange("p j b -> p (j b)"), in_=pmt2[:], func=AF.Copy), reads=[r_pmt2], writes=[r_modT])
    S.op("dve", lambda e: e.tensor_scalar_add(out=modT[:, 8:16, :], in0=modT[:, 8:16, :], scalar1=1.0), reads=[r_modT], awrites=[r_modT])
    S.op("dve", lambda e: e.tensor_scalar_add(out=modT[:, 32:40, :], in0=modT[:, 32:40, :], scalar1=1.0), reads=[r_modT], awrites=[r_modT])
    C.pop()
    if "stop0" in dbg:
        C.pop()
        return nc

    C.push()
    wI, r_wI = C.sb("wI", [128, 8, DIN], BF16)
    for i, (a, b_) in enumerate([(0, 1024), (1024, 1736), (1736, 2760), (2760, 3784), (3784, 4808), (4808, 5848), (5848, 6872)]):
        S.dma("pool", wI[:, :, a:b_], win_d[:, a:b_].rearrange("(k p) n -> p k n", p=128), reads=[r_in],
              writes=[r_wI] if i == 0 else (), awrites=() if i == 0 else [r_wI])
    kvw_bc, r_kvw = C.sb("kvw_bc", [128, 128], F32)
    ikw_bc, r_ikw = C.sb("ikw_bc", [128, 64], F32)
    ikb_bc, r_ikb = C.sb("ikb_bc", [128, 64], F32)
    dtb_bc, r_dtb = C.sb("dtb_bc", [128, 16], F32)
    S.dma("sp", kvw_bc[:], kvw_d.partition_broadcast(128), reads=[r_in], writes=[r_kvw])
    S.dma("sp", ikw_bc[:], ikw_d.partition_broadcast(128), reads=[r_in], writes=[r_ikw])
    S.dma("sp", ikb_bc[:], ikb_d.partition_broadcast(128), reads=[r_in], writes=[r_ikb])
    S.dma("sp", dtb_bc[:], dtb_d.partition_broadcast(128), reads=[r_in], writes=[r_dtb])

    xt = C.ring("sb", "xt", 2, [128, D], F32)
    xn = C.ring("sb", "xn", 2, [128, D], BF16)
    st = C.ring("sb", "st", 2, [128, 2, 6], F32)
    mv = C.ring("sb", "mv", 2, [128, 2], F32)
    rs = C.ring("sb", "rs", 2, [128, 1], F32)
    uT = C.ring("sb", "uT", 2, [128, 8, 512], BF16)
    ptr = C.ring("ps", "ptr", 1, [128, 8, 128], BF16)
    psm = C.ring("ps", "psm", 1, [128, 512], F32)
    pz = C.ring("ps", "pz", 2, [128, 512], F32)
    pf = C.ring("ps", "pf", 3, [128, 512], F32)
    pt2 = C.ring("ps", "pt2", 1, [128, 2, 128], BF16)
    kva = C.ring("sb", "kva", 2, [128, 136], BF16)
    ikn = C.ring("sb", "ikn", 2, [128, 128], BF16)
    sml = C.ring("sb", "sml", 2, [128, 64], F32)
    ikf = C.ring("sb", "ikf", 2, [128, 64], F32)
    iwt = C.ring("sb", "iwt", 2, [128, 8], F32)
    dtt = C.ring("sb", "dtt", 2, [128, 4, 16], F32)
    zst = C.ring("sb", "zst", 2, [128, D], BF16)
    tT = C.ring("sb", "tT", 2, [128, 2, 128], BF16)
    stg = C.ring("sb", "stg", 2, [128, 8, 512], BF16)
    for i in range(2):
        S.op("pool", lambda e: e.memset(kva[i][0][:, 128:136], 1.0), writes=[kva[i][1]])

    def ln_stats(src, r_src, i):
        st_t, r_st = st[i]
        mv_t, r_mv = mv[i]
        rs_t, r_rs = rs[i]
        for j in range(2):
            S.op("dve", lambda e: e.bn_stats(out=st_t[:, j, :], in_=src[:, j * 512:(j + 1) * 512]), reads=[r_src],
                 writes=[r_st] if j == 0 else (), awrites=() if j == 0 else [r_st])
        S.op("dve", lambda e: e.bn_aggr(out=mv_t[:], in_=st_t[:].rearrange("p a b -> p (a b)")), reads=[r_st], writes=[r_mv])
        S.op("act", lambda e: e.activation(out=rs_t[:], in_=mv_t[:, 1:2], func=AF.Sqrt, bias=EPS), reads=[r_mv], writes=[r_rs])
        S.op("dve", lambda e: e.reciprocal(out=rs_t[:], in_=rs_t[:]), reads=[r_rs], writes=[r_rs])
        return mv_t, r_mv, rs_t, r_rs

    tcount = 0
    for g in range(NTOK // 512):
        b = (g * 512) // SEQ
        u_t, r_u = uT[g % 2]
        for i4 in range(4):
            t = g * 4 + i4
            tok0 = t * 128
            ri = tcount % 2
            tcount += 1
            x_t, r_x = xt[ri]
            xn_t, r_xn = xn[ri]
            if t == 0:
                S.dma("sp", x_t[:], x_d[0:128, :], reads=[r_in], writes=[r_x])
            if t + 1 < NT:
                S.dma("sp", xt[(ri + 1) % 2][0][:], x_d[tok0 + 128:tok0 + 256, :], reads=[r_in], writes=[xt[(ri + 1) % 2][1]])
            mv_t, r_mv, rs_t, r_rs = ln_stats(x_t, r_x, ri)
            S.op("dve", lambda e: e.tensor_scalar(out=xn_t[:], in0=x_t[:], scalar1=mv_t[:, 0:1], scalar2=rs_t[:], op0=ALU.subtract, op1=ALU.mult),
                 reads=[r_x, r_mv, r_rs], writes=[r_xn])
            p_t, r_p = ptr[0]
            for k in range(8):
                S.op("pe", lambda e: e.transpose(out=p_t[:, k, :], in_=xn_t[:, k * 128:(k + 1) * 128], identity=ident_b[:]),
                     reads=[r_xn, r_identb], writes=[r_p] if k == 0 else (), awrites=() if k == 0 else [r_p])
            for k in range(8):
                S.op("act", lambda e: e.activation(out=u_t[:, k, i4 * 128:(i4 + 1) * 128], in_=p_t[:, k, :], func=AF.Identity,
                                                   scale=modT[:, 8 + k, b:b + 1], bias=modT[:, k, b:b + 1]),
                     reads=[r_p, r_modT], writes=[r_u] if (k == 0 and i4 == 0) else (), awrites=() if (k == 0 and i4 == 0) else [r_u])
            ps_t, r_ps = psm[0]
            for (c0, c1, o0) in [(C_KV, C_KV + 128, 0), (C_IK, C_IK + 72, 128), (C_DT, C_DT + 16, 200)]:
                for k in range(8):
                    S.op("pe", lambda e: e.matmul(ps_t[:, o0:o0 + (c1 - c0)], lhsT=u_t[:, k, i4 * 128:(i4 + 1) * 128], rhs=wI[:, k, c0:c1], start=(k == 0), stop=(k == 7)),
                         reads=[r_u, r_wI], writes=[r_ps] if (k == 0 and o0 == 0) else (), awrites=() if (k == 0 and o0 == 0) else [r_ps])
            zp = []
            for h in range(2):
                pz_t, r_pz = pz[h]
                zp.append((pz_t, r_pz))
                for k in range(8):
                    S.op("pe", lambda e: e.matmul(pz_t[:], lhsT=u_t[:, k, i4 * 128:(i4 + 1) * 128], rhs=wI[:, k, C_Z + h * 512:C_Z + (h + 1) * 512], start=(k == 0), stop=(k == 7)),
                         reads=[r_u, r_wI], writes=[r_pz] if k == 0 else (), awrites=() if k == 0 else [r_pz])
            sm_t, r_sm = sml[ri]
            kva_t, r_kva_t = kva[ri]
            ikf_t, r_ikf = ikf[ri]
            S.op("act", lambda e: e.activation(out=ikf_t[:, 0:64], in_=ps_t[:, 0:64], func=AF.Square, accum_out=sm_t[:, 0:1]), reads=[r_ps], writes=[r_ikf, r_sm])
            S.op("act", lambda e: e.activation(out=ikf_t[:, 0:64], in_=ps_t[:, 64:128], func=AF.Square, accum_out=sm_t[:, 1:2]), reads=[r_ps], writes=[r_ikf], awrites=[r_sm])
            S.op("dve", lambda e: e.tensor_tensor(out=sm_t[:, 0:1], in0=sm_t[:, 0:1], in1=sm_t[:, 1:2], op=ALU.add), reads=[r_sm], awrites=[r_sm])
            S.op("act", lambda e: e.activation(out=sm_t[:, 2:3], in_=sm_t[:, 0:1], func=AF.Sqrt, scale=1.0 / 128.0, bias=EPS), reads=[r_sm], awrites=[r_sm])
            S.op("dve", lambda e: e.reciprocal(out=sm_t[:, 3:4], in_=sm_t[:, 2:3]), reads=[r_sm], awrites=[r_sm])
            S.op("dve", lambda e: e.scalar_tensor_tensor(out=kva_t[:, 0:128], in0=ps_t[:, 0:128], scalar=sm_t[:, 3:4], in1=kvw_bc[:], op0=ALU.mult, op1=ALU.mult),
                 reads=[r_ps, r_sm, r_kvw], awrites=[r_kva_t])
            S.dma("sp", kva_d[tok0:tok0 + 128, :], kva_t[:], reads=[r_kva_t], awrites=[r_kva])
            ik_t, r_ik = ikn[ri]
            S.op("dve", lambda e: e.bn_stats(out=sm_t[:, 8:14], in_=ps_t[:, 128:192]), reads=[r_ps], awrites=[r_sm])
            S.op("dve", lambda e: e.bn_aggr(out=sm_t[:, 16:18], in_=sm_t[:, 8:14]), reads=[r_sm], awrites=[r_sm])
            S.op("act", lambda e: e.activation(out=sm_t[:, 18:19], in_=sm_t[:, 17:18], func=AF.Sqrt, bias=EPS), reads=[r_sm], awrites=[r_sm])
            S.op("dve", lambda e: e.reciprocal(out=sm_t[:, 19:20], in_=sm_t[:, 18:19]), reads=[r_sm], awrites=[r_sm])
            S.op("dve", lambda e: e.tensor_scalar(out=ikf_t[:], in0=ps_t[:, 128:192], scalar1=sm_t[:, 16:17], scalar2=sm_t[:, 19:20], op0=ALU.subtract, op1=ALU.mult),
                 reads=[r_ps, r_sm], writes=[r_ikf])
            S.op("dve", lambda e: e.tensor_tensor(out=ikf_t[:], in0=ikf_t[:], in1=ikw_bc[:], op=ALU.mult), reads=[r_ikf, r_ikw], writes=[r_ikf])
            S.op("dve", lambda e: e.tensor_tensor(out=ik_t[:, 0:64], in0=ikf_t[:], in1=ikb_bc[:], op=ALU.add), reads=[r_ikf, r_ikb], writes=[r_ik])
            S.op("dve", lambda e: e.tensor_copy(out=ik_t[:, 64:128], in_=ik_t[:, 0:64]), reads=[r_ik], awrites=[r_ik])
            iw_t, r_iwt = iwt[ri]
            S.op("act", lambda e: e.mul(out=iw_t[:], in_=ps_t[:, 192:200], mul=float(8 ** -0.5 * 64 ** -0.5)), reads=[r_ps], writes=[r_iwt])
            S.dma("sp", iw_d[tok0:tok0 + 128, :], iw_t[:], reads=[r_iwt], awrites=[r_iw])
            d_t, r_d = dtt[ri]
            S.op("dve", lambda e: e.tensor_tensor(out=d_t[:, 0, :], in0=ps_t[:, 200:216], in1=dtb_bc[:], op=ALU.add), reads=[r_ps, r_dtb], writes=[r_d])
            S.op("act", lambda e: e.activation(out=d_t[:, 1, :], in_=d_t[:, 0, :], func=AF.Abs), reads=[r_d], awrites=[r_d])
            S.op("act", lambda e: e.activation(out=d_t[:, 1, :], in_=d_t[:, 1, :], func=AF.Exp, scale=-1.0), reads=[r_d], awrites=[r_d])
            S.op("act", lambda e: e.activation(out=d_t[:, 1, :], in_=d_t[:, 1, :], func=AF.Ln, bias=1.0), reads=[r_d], awrites=[r_d])
            S.op("dve", lambda e: e.scalar_tensor_tensor(out=d_t[:, 2, :], in0=d_t[:, 0, :], scalar=0.0, in1=d_t[:, 1, :], op0=ALU.max, op1=ALU.add), reads=[r_d], awrites=[r_d])
            S.dma("sp", dt_d[tok0:tok0 + 128, :], d_t[:, 2, :], reads=[r_d], awrites=[r_dt])
            z_t, r_z = zst[ri]
            for h in range(2):
                S.op("act", lambda e: e.activation(out=z_t[:, h * 512:(h + 1) * 512], in_=zp[h][0][:], func=AF.Silu), reads=[zp[h][1]],
                     writes=[r_z] if h == 0 else (), awrites=() if h == 0 else [r_z])
            S.dma("sp", zs_d[tok0:tok0 + 128, :], z_t[:], reads=[r_z], awrites=[r_zs])
            p2, r_p2 = pt2[0]
            t_t, r_t = tT[ri]
            S.op("pe", lambda e: e.transpose(out=p2[:, 0, :], in_=kva_t[:, 0:128], identity=ident_b[:]), reads=[r_kva_t, r_identb], writes=[r_p2])
            S.op("pe", lambda e: e.transpose(out=p2[:, 1, :], in_=ik_t[:], identity=ident_b[:]), reads=[r_ik, r_identb], awrites=[r_p2])
            S.op("dve", lambda e: e.tensor_copy(out=t_t[:], in_=p2[:]), reads=[r_p2], writes=[r_t])
            S.dma("sp", kvT_d[:, tok0:tok0 + 128], t_t[:, 0, :], reads=[r_t], awrites=[r_kvT])
            S.dma("sp", ikT_d[:, tok0:tok0 + 128], t_t[:, 1, :], reads=[r_t], awrites=[r_ikT])
        g0 = g * 512
        fcount = 0
        for (c0, nch, dst, r_dst, ch0, func, scl) in [
                (C_Q, 8, qT_d, r_qT, 0, AF.Copy, float(128 ** -0.5)),
                (C_IQ, 4, iqT_d, r_iqT, 0, AF.Copy, 1.0),
                (C_XBC, 8, xbcT_d, r_xbcT, 0, AF.Copy, 1.0),
                (C_XBC + 1024, 8, xbcT_d, r_xbcT, 8, AF.Copy, 1.0),
                (C_GA, 8, sgT_d, r_sgT, 0, AF.Sigmoid, 1.0),
                (C_GB, 8, sgT_d, r_sgT, 8, AF.Sigmoid, 1.0)]:
            sg_t, r_sg = stg[fcount % 2]
            fcount += 1
            for j in range(nch):
                pf_t, r_pf = pf[j % 3]
                for k in range(8):
                    S.op("pe", lambda e: e.matmul(pf_t[:], lhsT=wI[:, k, c0 + j * 128:c0 + (j + 1) * 128], rhs=u_t[:, k, :], start=(k == 0), stop=(k == 7)),
                         reads=[r_u, r_wI], writes=[r_pf] if k == 0 else (), awrites=() if k == 0 else [r_pf])
                if func == AF.Copy and j % 2 == 1:
                    S.op("dve", lambda e: e.tensor_scalar_mul(out=sg_t[:, j, :], in0=pf_t[:], scalar1=scl), reads=[r_pf],
                         writes=[r_sg] if j == 0 else (), awrites=() if j == 0 else [r_sg])
                else:
                    S.op("act", lambda e: e.activation(out=sg_t[:, j, :], in_=pf_t[:], func=func, scale=scl), reads=[r_pf],
                         writes=[r_sg] if j == 0 else (), awrites=() if j == 0 else [r_sg])
            S.dma("sp", dst[ch0:ch0 + nch, :, g0:g0 + 512].rearrange("c p t -> p c t"), sg_t[:, 0:nch, :], reads=[r_sg], awrites=[r_dst])
    C.pop()
    if "stopA" in dbg:
        C.pop()
        return nc

    phase_B(nc, S, C, dbg, locals())
    if "stopB" in dbg:
        C.pop()
        return nc

    phase_C(nc, S, C, dbg, locals())
    if "stopC" in dbg:
        C.pop()
        return nc

    phase_DEF(nc, S, C, dbg, locals())
    C.pop()
    return nc


def phase_DEF(nc, S, C, dbg, L):
    g_ = lambda n: L[n]
    r_in = g_("r_in"); ident_b = g_("ident_b"); r_identb = g_("r_identb"); ident_f = g_("ident_f"); r_identf = g_("r_identf")
    modT = g_("modT"); r_modT = g_("r_modT"); mod_d = g_("mod_d"); r_mod = g_("r_mod")
    x_d = g_("x_d"); oaT_d, r_oaT = g_("oaT_d"), g_("r_oaT"); obT_d, r_obT = g_("obT_d"), g_("r_obT"); sgT_d, r_sgT = g_("sgT_d"), g_("r_sgT")
    x1_d, r_x1 = g_("x1_d"), g_("r_x1"); u2_d, r_u2 = g_("u2_d"), g_("r_u2"); xs_d, r_xsd = g_("xs_d"), g_("r_xsd"); ys_d, r_ysd = g_("ys_d"), g_("r_ysd")
    out_d, r_out = g_("out_d"), g_("r_out")
    b1r_d, b2_d = g_("b1r_d"), g_("b2_d")
    w1b_d, r_w1bd, w2b_d, r_w2bd = g_("w1b_d"), g_("r_w1bd"), g_("w2b_d"), g_("r_w2bd")

    C.push()
    idx4, r_idx4 = C.sb("idx4", [128, NT, 4], I32)
    g4, r_g4 = C.sb("g4", [128, NT, 4], F32)
    widx, r_widx = C.sb("widx", [128, NBLK, 8], I32)
    bidx, r_bidx = C.sb("bidx", [128, NBLK], I32)
    eidx, r_eidx = C.sb("eidx", [128, NBLK], I32)

    def ln_stats(src, r_src, st_t, r_st, mv_t, r_mv, rs_t, r_rs):
        for j in range(2):
            S.op("dve", lambda e: e.bn_stats(out=st_t[:, j, :], in_=src[:, j * 512:(j + 1) * 512]), reads=[r_src],
                 writes=[r_st] if j == 0 else (), awrites=() if j == 0 else [r_st])
        S.op("dve", lambda e: e.bn_aggr(out=mv_t[:], in_=st_t[:].rearrange("p a b -> p (a b)")), reads=[r_st], writes=[r_mv])
        S.op("act", lambda e: e.activation(out=rs_t[:], in_=mv_t[:, 1:2], func=AF.Sqrt, bias=EPS), reads=[r_mv], writes=[r_rs])
        S.op("dve", lambda e: e.reciprocal(out=rs_t[:], in_=rs_t[:]), reads=[r_rs], writes=[r_rs])

    mT_d, r_mTd = g_("mT_d"), g_("r_mTd")
    C.push()
    wpa, r_wpa = C.sb("wpa", [128, 8, D], BF16)
    wpb, r_wpb = C.sb("wpb", [128, 8, D], BF16)
    S.dma("pool", wpa[:], g_("wpa_d").rearrange("(k p) n -> p k n", p=128), reads=[r_in], writes=[r_wpa])
    S.dma("pool", wpb[:], g_("wpb_d").rearrange("(k p) n -> p k n", p=128), reads=[r_in], writes=[r_wpb])
    oaTr = C.ring("sb", "oaTd", 2, [128, 8, 512], BF16)
    obTr = C.ring("sb", "obTd", 2, [128, 8, 512], BF16)
    sgTr = C.ring("sb", "sgTd", 2, [128, 16, 512], BF16)
    mTr = C.ring("sb", "mT", 2, [128, 8, 512], BF16)
    ta = C.ring("sb", "ta", 3, [128, 512], F32)
    tb = C.ring("sb", "tb", 3, [128, 512], F32)
    pab = C.ring("ps", "pab", 4, [128, 512], F32)
    pbb = C.ring("ps", "pbb", 4, [128, 512], F32)

    def loadD1(g):
        g0 = g * 512
        S.dma("sp", oaTr[g % 2][0][:], oaT_d[:, :, g0:g0 + 512].rearrange("c p t -> p c t"), reads=[r_oaT], writes=[oaTr[g % 2][1]])
        S.dma("sp", obTr[g % 2][0][:], obT_d[:, :, g0:g0 + 512].rearrange("c p t -> p c t"), reads=[r_obT], writes=[obTr[g % 2][1]])
        S.dma("sp", sgTr[g % 2][0][:], sgT_d[:, :, g0:g0 + 512].rearrange("c p t -> p c t"), reads=[r_sgT], writes=[sgTr[g % 2][1]])

    NG = NTOK // 512
    loadD1(0)
    cn = 0
    for g in range(NG):
        g0 = g * 512
        if g + 1 < NG:
            loadD1(g + 1)
        oaT, r_oaTs = oaTr[g % 2]; obT, r_obTs = obTr[g % 2]; sgT, r_sgTs = sgTr[g % 2]; mT, r_mT = mTr[g % 2]
        for n in range(8):
            pa, r_pa = pab[cn % 4]
            pb, r_pb = pbb[cn % 4]
            ta_t, r_ta = ta[cn % 3]
            tb_t, r_tb = tb[cn % 3]
            cn += 1
            for k in range(8):
                S.op("pe", lambda e: e.matmul(pa[:], lhsT=wpa[:, k, n * 128:(n + 1) * 128], rhs=oaT[:, k, :], start=(k == 0), stop=(k == 7)),
                     reads=[r_wpa, r_oaTs], writes=[r_pa] if k == 0 else (), awrites=() if k == 0 else [r_pa])
            for k in range(8):
                S.op("pe", lambda e: e.matmul(pb[:], lhsT=wpb[:, k, n * 128:(n + 1) * 128], rhs=obT[:, k, :], start=(k == 0), stop=(k == 7)),
                     reads=[r_wpb, r_obTs], writes=[r_pb] if k == 0 else (), awrites=() if k == 0 else [r_pb])
            S.op("dve", lambda e: e.tensor_tensor(out=ta_t[:], in0=pa[:], in1=sgT[:, n, :], op=ALU.mult), reads=[r_pa, r_sgTs], writes=[r_ta])
            S.op("dve", lambda e: e.tensor_tensor(out=tb_t[:], in0=pb[:], in1=sgT[:, 8 + n, :], op=ALU.mult), reads=[r_pb, r_sgTs], writes=[r_tb])
            S.op("pool", lambda e: e.tensor_tensor(out=mT[:, n, :], in0=ta_t[:], in1=tb_t[:], op=ALU.add), reads=[r_ta, r_tb],
                 writes=[r_mT] if n == 0 else (), awrites=() if n == 0 else [r_mT])
        S.dma("sp", mT_d[:, :, g0:g0 + 512].rearrange("c p t -> p c t"), mT[:], reads=[r_mT], awrites=[r_mTd])
    C.pop()

    C.push()
    wout, r_wout = C.sb("wout", [128, 8, D], BF16)
    S.dma("pool", wout[:], g_("wout_d").rearrange("(k p) n -> p k n", p=128), reads=[r_in], writes=[r_wout])
    wr, r_wr = C.sb("wr", [128, 8, NE], F32)
    S.dma("sp", wr[:], g_("wr_d").rearrange("(k p) n -> p k n", p=128), reads=[r_in], writes=[r_wr])
    br_bc, r_br = C.sb("br_bc", [128, NE], F32)
    S.dma("sp", br_bc[:], g_("br_d").partition_broadcast(128), reads=[r_in], writes=[r_br])
    ln1g, r_ln1g = C.sb("ln1g", [128, D], F32)
    ln1b, r_ln1b = C.sb("ln1b", [128, D], F32)
    S.dma("sp", ln1g[:], g_("ln1g_d").partition_broadcast(128), reads=[r_in], writes=[r_ln1g])
    S.dma("sp", ln1b[:], g_("ln1b_d").partition_broadcast(128), reads=[r_in], writes=[r_ln1b])
    gater = C.ring("sb", "gate1", 2, [128, D], F32)
    sc2r = C.ring("sb", "sc2", 2, [128, D], F32)
    sh2r = C.ring("sb", "sh2", 2, [128, D], F32)
    sutf, r_sutf = C.sb("sutf", [128, 128], F32)
    sutb, r_sutb = C.sb("sutb", [128, 128], BF16)
    onesb, r_onesb = C.sb("onesb", [128, 128], BF16)
    S.dma("sp", sutf[:], g_("sut_d"), reads=[r_in], writes=[r_sutf])
    S.op("dve", lambda e: e.tensor_copy(out=sutb[:], in_=sutf[:]), reads=[r_sutf], writes=[r_sutb])
    S.op("dve", lambda e: e.memset(onesb[:], 1.0), writes=[r_onesb])
    maskall, r_maskall = C.sb("maskall", [128, NT, NE], F32)
    gall, r_gall = C.sb("gall", [128, NT, NE], F32)
    posall, r_posall = C.sb("posall", [128, NT, NE], F32)
    rrun, r_rrun = C.sb("rrun", [128, NE], F32)
    S.op("dve", lambda e: e.memset(rrun[:], 0.0), writes=[r_rrun])
    mtr = C.ring("sb", "mtl", 3, [128, 8, 128], BF16)
    xt = C.ring("sb", "xtd", 3, [128, D], F32)
    r1_r = C.ring("sb", "r1", 2, [128, D], F32)
    x1t_r = C.ring("sb", "x1t", 2, [128, D], F32)
    u2t_r = C.ring("sb", "u2t", 3, [128, D], F32)
    u2b = C.ring("sb", "u2b", 2, [128, D], BF16)
    u2T_r = C.ring("sb", "u2T", 2, [128, 8, 128], F32)
    st_r = C.ring("sb", "stD", 4, [128, 2, 6], F32)
    mv_r = C.ring("sb", "mvD", 4, [128, 2], F32)
    rs_r = C.ring("sb", "rsD", 4, [128, 1], F32)
    lg_r = C.ring("sb", "lg", 2, [128, NE], F32)
    m8_r = C.ring("sb", "m8D", 2, [128, 8], F32)
    sml_r = C.ring("sb", "smlD", 2, [128, 8], F32)
    ex_r = C.ring("sb", "exD", 2, [128, NE], F32)
    maskb_r = C.ring("sb", "maskb", 2, [128, NE], BF16)
    prs = C.ring("ps", "prs", 4, [128, 512], F32)
    ptT = C.ring("ps", "ptT", 2, [128, 512], F32)
    psl = C.ring("ps", "psl", 2, [128, 512], F32)

    def loadD2(t):
        tok0 = t * 128
        S.dma("sp", xt[t % 3][0][:], x_d[tok0:tok0 + 128, :], reads=[r_in], writes=[xt[t % 3][1]])
        S.dma("sp", mtr[t % 3][0][:], mT_d[:, :, tok0:tok0 + 128].rearrange("c p t -> p c t"), reads=[r_mTd], writes=[mtr[t % 3][1]])
        if t % TPS == 0:
            b = t // TPS
            gate1, r_gate1 = gater[b % 2]; sc2, r_sc2 = sc2r[b % 2]; sh2, r_sh2 = sh2r[b % 2]
            S.dma("sp", gate1[:], mod_d[b:b + 1, 2 * D:3 * D].partition_broadcast(128), reads=[r_mod], writes=[r_gate1])
            S.dma("sp", sh2[:], mod_d[b:b + 1, 3 * D:4 * D].partition_broadcast(128), reads=[r_mod], writes=[r_sh2])
            S.dma("sp", sc2[:], mod_d[b:b + 1, 4 * D:5 * D].partition_broadcast(128), reads=[r_mod], writes=[r_sc2])
            S.op("pool", lambda e: e.tensor_scalar_add(out=sc2[:], in0=sc2[:], scalar1=1.0), reads=[r_sc2], writes=[r_sc2])

    def stage1(t):
        tok0 = t * 128
        b = t // TPS
        gate1, r_gate1 = gater[b % 2]; sc2, r_sc2 = sc2r[b % 2]; sh2, r_sh2 = sh2r[b % 2]
        x_t, r_x = xt[t % 3]
        mt_t, r_mt = mtr[t % 3]
        r1, r_r1 = r1_r[t % 2]; x1t, r_x1t = x1t_r[t % 2]; u2t, r_u2t = u2t_r[t % 3]
        st, r_st = st_r[(2 * t) % 4]; mv, r_mv = mv_r[(2 * t) % 4]; rs, r_rs = rs_r[(2 * t) % 4]
        st2, r_st2 = st_r[(2 * t + 1) % 4]; mv2, r_mv2 = mv_r[(2 * t + 1) % 4]; rs2, r_rs2 = rs_r[(2 * t + 1) % 4]
        for hf in range(2):
            pr_t, r_pr = prs[(2 * t + hf) % 4]
            for n in range(8):
                S.op("pe", lambda e: e.matmul(pr_t[:], lhsT=mt_t[:, n, :], rhs=wout[:, n, hf * 512:(hf + 1) * 512], start=(n == 0), stop=(n == 7)),
                     reads=[r_mt, r_wout], writes=[r_pr] if n == 0 else (), awrites=() if n == 0 else [r_pr])
            S.op("dve", lambda e: e.tensor_tensor(out=r1[:, hf * 512:(hf + 1) * 512], in0=pr_t[:], in1=gate1[:, hf * 512:(hf + 1) * 512], op=ALU.mult),
                 reads=[r_pr, r_gate1], writes=[r_r1] if hf == 0 else (), awrites=() if hf == 0 else [r_r1])
        S.op("dve", lambda e: e.scalar_tensor_tensor(out=r1[:], in0=x_t[:], scalar=float(ALPHA), in1=r1[:], op0=ALU.mult, op1=ALU.add), reads=[r_x, r_r1], writes=[r_r1])
        ln_stats(r1, r_r1, st, r_st, mv, r_mv, rs, r_rs)
        S.op("dve", lambda e: e.tensor_scalar(out=x1t[:], in0=r1[:], scalar1=mv[:, 0:1], scalar2=rs[:], op0=ALU.subtract, op1=ALU.mult), reads=[r_r1, r_mv, r_rs], writes=[r_x1t])
        S.op("pool", lambda e: e.tensor_tensor(out=x1t[:], in0=x1t[:], in1=ln1g[:], op=ALU.mult), reads=[r_x1t, r_ln1g], writes=[r_x1t])
        S.op("pool", lambda e: e.tensor_tensor(out=x1t[:], in0=x1t[:], in1=ln1b[:], op=ALU.add), reads=[r_x1t, r_ln1b], writes=[r_x1t])
        S.dma("sp", x1_d[tok0:tok0 + 128, :], x1t[:], reads=[r_x1t], awrites=[r_x1])
        ln_stats(x1t, r_x1t, st2, r_st2, mv2, r_mv2, rs2, r_rs2)
        S.op("dve", lambda e: e.tensor_scalar(out=u2t[:], in0=x1t[:], scalar1=mv2[:, 0:1], scalar2=rs2[:], op0=ALU.subtract, op1=ALU.mult), reads=[r_x1t, r_mv2, r_rs2], writes=[r_u2t])
        S.op("pool", lambda e: e.tensor_tensor(out=u2t[:], in0=u2t[:], in1=sc2[:], op=ALU.mult), reads=[r_u2t, r_sc2], writes=[r_u2t])
        S.op("pool", lambda e: e.tensor_tensor(out=u2t[:], in0=u2t[:], in1=sh2[:], op=ALU.add), reads=[r_u2t, r_sh2], writes=[r_u2t])
        ub, r_ub = u2b[t % 2]
        S.op("act", lambda e: e.activation(out=ub[:], in_=u2t[:], func=AF.Copy), reads=[r_u2t], writes=[r_ub])
        S.dma("sp", u2_d[tok0:tok0 + 128, :], ub[:], reads=[r_ub], awrites=[r_u2])

    def stage2(t):
        u2t, r_u2t = u2t_r[t % 3]
        u2T, r_u2T = u2T_r[t % 2]
        lg, r_lg = lg_r[t % 2]; m8, r_m8 = m8_r[t % 2]; sml, r_sml = sml_r[t % 2]; ex, r_ex = ex_r[t % 2]; maskb, r_maskb = maskb_r[t % 2]
        for hf in range(2):
            pT, r_pT = (ptT[t % 2] if hf == 0 else psl[t % 2])
            for k4 in range(4):
                k = hf * 4 + k4
                S.op("pe", lambda e: e.transpose(out=pT[:, k4 * 128:(k4 + 1) * 128], in_=u2t[:, k * 128:(k + 1) * 128], identity=ident_f[:]),
                     reads=[r_u2t, r_identf], writes=[r_pT] if k4 == 0 else (), awrites=() if k4 == 0 else [r_pT])
            S.op("act", lambda e: e.activation(out=u2T[:, hf * 4:hf * 4 + 4, :].rearrange("p k t -> p (k t)"), in_=pT[:], func=AF.Copy), reads=[r_pT],
                 writes=[r_u2T] if hf == 0 else (), awrites=() if hf == 0 else [r_u2T])
        pl_, r_pl = psl[t % 2]
        for k in range(8):
            S.op("pe", lambda e: e.matmul(pl_[:, 0:NE], lhsT=u2T[:, k, :], rhs=wr[:, k, :], start=(k == 0), stop=(k == 7)),
                 reads=[r_u2T, r_wr], writes=[r_pl] if k == 0 else (), awrites=() if k == 0 else [r_pl])
        S.op("dve", lambda e: e.tensor_tensor(out=lg[:], in0=pl_[:, 0:NE], in1=br_bc[:], op=ALU.add), reads=[r_pl, r_br], writes=[r_lg])
        S.op("dve", lambda e: e.max(out=m8[:], in_=lg[:]), reads=[r_lg], writes=[r_m8])
        S.op("dve", lambda e: e.tensor_scalar(out=maskall[:, t, :], in0=lg[:], scalar1=m8[:, 3:4], scalar2=None, op0=ALU.is_ge), reads=[r_lg, r_m8], awrites=[r_maskall])
        S.op("dve", lambda e: e.tensor_scalar_mul(out=sml[:, 0:1], in0=m8[:, 0:1], scalar1=-1.0), reads=[r_m8], writes=[r_sml])
        S.op("act", lambda e: e.activation(out=ex[:], in_=lg[:], func=AF.Exp, bias=sml[:, 0:1]), reads=[r_lg, r_sml], writes=[r_ex])
        S.op("dve", lambda e: e.tensor_tensor(out=ex[:], in0=ex[:], in1=maskall[:, t, :], op=ALU.mult), reads=[r_ex, r_maskall], writes=[r_ex])
        S.op("dve", lambda e: e.reduce_sum(out=sml[:, 1:2], in_=ex[:], axis=AX.X), reads=[r_ex], awrites=[r_sml])
        S.op("dve", lambda e: e.reciprocal(out=sml[:, 2:3], in_=sml[:, 1:2]), reads=[r_sml], awrites=[r_sml])
        S.op("dve", lambda e: e.tensor_scalar(out=gall[:, t, :], in0=ex[:], scalar1=sml[:, 2:3], scalar2=None, op0=ALU.mult), reads=[r_ex, r_sml], awrites=[r_gall])
        S.op("dve", lambda e: e.tensor_copy(out=maskb[:], in_=maskall[:, t, :]), reads=[r_maskall], writes=[r_maskb])
        S.op("pe", lambda e: e.matmul(pl_[:, 64:64 + NE], lhsT=sutb[:], rhs=maskb[:], start=True, stop=True), reads=[r_sutb, r_maskb, r_lg], awrites=[r_pl])
        S.op("pe", lambda e: e.matmul(pl_[:, 128:128 + NE], lhsT=onesb[:], rhs=maskb[:], start=True, stop=True), reads=[r_onesb, r_maskb], awrites=[r_pl])
        S.op("dve", lambda e: e.tensor_tensor(out=posall[:, t, :], in0=pl_[:, 64:64 + NE], in1=rrun[:], op=ALU.add), reads=[r_pl, r_rrun], awrites=[r_posall])
        S.op("dve", lambda e: e.tensor_tensor(out=rrun[:], in0=pl_[:, 128:128 + NE], in1=rrun[:], op=ALU.add), reads=[r_pl, r_rrun], writes=[r_rrun])

    loadD2(0)
    loadD2(1)
    stage1(0)
    for t in range(NT):
        if t + 2 < NT:
            loadD2(t + 2)
        if t + 1 < NT:
            stage1(t + 1)
        stage2(t)

    thr16, r_thr16 = C.sb("thr16", [128, NE, 16], F32)
    bstart, r_bstart = C.sb("bstart", [128, NBLK], F32)
    kp, r_kp = C.sb("kp", [128, 8], F32)
    pcol, r_pcol = C.sb("pcol", [128, 1], F32)
    sut32, r_sut32 = C.sb("sut32", [NE, NE], F32)
    S.dma("sp", thr16[:].rearrange("p e m -> p (e m)"), g_("thr16_d").partition_broadcast(128), reads=[r_in], writes=[r_thr16])
    S.dma("sp", bstart[:], g_("bstart_d").partition_broadcast(128), reads=[r_in], writes=[r_bstart])
    S.dma("sp", kp[:], g_("kp_d"), reads=[r_in], writes=[r_kp])
    S.dma("sp", pcol[:], g_("pcol_d"), reads=[r_in], writes=[r_pcol])
    S.dma("sp", sut32[:], g_("sut32_d"), reads=[r_in], writes=[r_sut32])
    big, r_big = C.sb("bigD", [128, NBLK * NE], F32)
    nbk, r_nbk = C.sb("nbk", [128, NE], F32)
    padT, r_padT = C.sb("padT", [NE, 128], F32)
    pstart, r_pstart = C.sb("pstart", [128, NE], F32)
    pend, r_pend = C.sb("pend", [128, NE], F32)
    bexp, r_bexp = C.sb("bexp", [128, NBLK], F32)
    wf, r_wf = C.sb("wf", [128, NBLK, 8], F32)
    S.op("dve", lambda e: e.tensor_tensor(out=big[:, 0:NE * 16].rearrange("p (e m) -> p e m", m=16), in0=rrun[:].unsqueeze(2).to_broadcast([128, NE, 16]), in1=thr16[:], op=ALU.is_gt),
         reads=[r_rrun, r_thr16], writes=[r_big])
    S.op("dve", lambda e: e.tensor_reduce(out=nbk[:], in_=big[:, 0:NE * 16].rearrange("p (e m) -> p e m", m=16), axis=AX.X, op=ALU.add), reads=[r_big], writes=[r_nbk])
    S.op("dve", lambda e: e.tensor_scalar_mul(out=nbk[:], in0=nbk[:], scalar1=512.0), reads=[r_nbk], writes=[r_nbk])
    pq, r_pq = psl[0]
    S.op("pe", lambda e: e.transpose(out=pq[0:NE, 0:128], in_=nbk[:], identity=ident_f[:]), reads=[r_nbk, r_identf], writes=[r_pq])
    S.op("act", lambda e: e.activation(out=padT[:], in_=pq[0:NE, 0:128], func=AF.Copy), reads=[r_pq], writes=[r_padT])
    S.op("pe", lambda e: e.matmul(pq[:, 256:256 + NE], lhsT=padT[:], rhs=sut32[:], start=True, stop=True), reads=[r_padT, r_sut32], awrites=[r_pq])
    S.op("act", lambda e: e.activation(out=pstart[:], in_=pq[:, 256:256 + NE], func=AF.Copy), reads=[r_pq], writes=[r_pstart])
    S.op("dve", lambda e: e.tensor_tensor(out=pend[:], in0=pstart[:], in1=nbk[:], op=ALU.add), reads=[r_pstart, r_nbk], writes=[r_pend])
    S.op("dve", lambda e: e.tensor_tensor(out=big[:].rearrange("p (i e) -> p i e", e=NE), in0=bstart[:].unsqueeze(2).to_broadcast([128, NBLK, NE]),
                                         in1=pend[:].unsqueeze(1).to_broadcast([128, NBLK, NE]), op=ALU.is_ge), reads=[r_bstart, r_pend], writes=[r_big])
    S.op("dve", lambda e: e.tensor_reduce(out=bexp[:], in_=big[:].rearrange("p (i e) -> p i e", e=NE), axis=AX.X, op=ALU.add), reads=[r_big], writes=[r_bexp])
    S.op("dve", lambda e: e.tensor_scalar_min(out=bexp[:], in0=bexp[:], scalar1=float(NE - 1)), reads=[r_bexp], writes=[r_bexp])
    S.op("dve", lambda e: e.tensor_copy(out=eidx[:], in_=bexp[:]), reads=[r_bexp], writes=[r_eidx])
    S.op("dve", lambda e: e.tensor_scalar(out=bidx[:], in0=bexp[:], scalar1=128.0, scalar2=pcol[:, 0:1], op0=ALU.mult, op1=ALU.add), reads=[r_bexp, r_pcol], writes=[r_bidx])
    S.op("dve", lambda e: e.tensor_scalar_mul(out=wf[:], in0=bexp[:].unsqueeze(2).to_broadcast([128, NBLK, 8]), scalar1=float(D)), reads=[r_bexp], writes=[r_wf])
    S.op("dve", lambda e: e.tensor_tensor(out=widx[:], in0=wf[:], in1=kp[:].unsqueeze(1).to_broadcast([128, NBLK, 8]), op=ALU.add), reads=[r_wf, r_kp], writes=[r_widx])
    sl_, r_sl = C.sb("slD", [128, NE], F32)
    eqt, r_eqt = C.sb("eqt", [128, NE], F32)
    m8b, r_m8b = C.sb("m8b", [128, 8], F32)
    S.op("dve", lambda e: e.tensor_scalar_add(out=pstart[:], in0=pstart[:], scalar1=1.0), reads=[r_pstart], writes=[r_pstart])
    for t in range(NT):
        tok0 = t * 128
        ub, r_ub = u2b[t % 2]
        S.dma("sp", ub[:], u2_d[tok0:tok0 + 128, :], reads=[r_u2], writes=[r_ub])
        S.op("dve", lambda e: e.tensor_tensor(out=sl_[:], in0=posall[:, t, :], in1=pstart[:], op=ALU.add), reads=[r_posall, r_pstart], writes=[r_sl])
        S.op("dve", lambda e: e.tensor_tensor(out=sl_[:], in0=sl_[:], in1=maskall[:, t, :], op=ALU.mult), reads=[r_sl, r_maskall], writes=[r_sl])
        S.op("dve", lambda e: e.max(out=m8b[:], in_=sl_[:]), reads=[r_sl], writes=[r_m8b])
        for j in range(4):
            S.op("dve", lambda e: e.scalar_tensor_tensor(out=eqt[:], in0=sl_[:], scalar=m8b[:, j:j + 1], in1=gall[:, t, :], op0=ALU.is_equal, op1=ALU.mult, accum_out=g4[:, t, j:j + 1]),
                 reads=[r_sl, r_m8b, r_gall], writes=[r_eqt], awrites=[r_g4])
        S.op("dve", lambda e: e.tensor_scalar_add(out=idx4[:, t, :], in0=m8b[:, 0:4], scalar1=-1.0), reads=[r_m8b], awrites=[r_idx4])
        for j in range(4):
            S.op("pool", lambda e: e.indirect_dma_start(out=xs_d[:, :], out_offset=bass.IndirectOffsetOnAxis(ap=idx4[:, t, j:j + 1], axis=0), in_=ub[:], in_offset=None),
                 reads=[r_ub, r_idx4], awrites=[r_xsd], dma=True)
    C.pop()
    if "stopD" in dbg:
        C.pop()
        return

    C.push()
    w1b = C.ring("sb", "w1b", 2, [128, 8, 2 * D], BF16)
    w2b = C.ring("sb", "w2b", 2, [128, 8, D], BF16)
    b1t = C.ring("sb", "b1t", 2, [128, 16], F32)
    b2t = C.ring("sb", "b2t", 2, [2, D], F32)
    ones1f, r_ones1f = C.sb("ones1f", [1, 128], BF16)
    S.op("dve", lambda e: e.memset(ones1f[:], 1.0), writes=[r_ones1f])
    b2b = C.ring("sb", "b2b", 2, [1, D], BF16)
    xr = C.ring("sb", "xr", 8, [128, D], BF16)
    XT, r_XT = C.sb("XT", [128, 8, 512], BF16)
    hg = C.ring("sb", "hg", 2, [128, 512], F32)
    hu = C.ring("sb", "hu", 2, [128, 512], F32)
    sgm = C.ring("sb", "sgm", 2, [128, 512], F32)
    actT, r_actT = C.sb("actT", [128, 8, 512], BF16)
    ysb = C.ring("sb", "ysb", 2, [128, D], BF16)
    pxt = C.ring("ps", "pxt", 2, [128, 512], F32)
    pg = C.ring("ps", "pg", 2, [128, 512], F32)
    pu = C.ring("ps", "pu", 2, [128, 512], F32)
    py = C.ring("ps", "py", 2, [128, 512], F32)

    def load_weights(i, slot):
        w1t, r_w1 = w1b[slot]
        w2t, r_w2 = w2b[slot]
        for k in range(8):
            S.op("pool", lambda e: e.indirect_dma_start(out=w1t[:, k, :], out_offset=None, in_=w1b_d[:, :], in_offset=bass.IndirectOffsetOnAxis(ap=widx[:, i, k:k + 1], axis=0)),
                 reads=[r_w1bd, r_widx], writes=[r_w1] if k == 0 else (), awrites=() if k == 0 else [r_w1], dma=True)
        for k in range(8):
            S.op("pool", lambda e: e.indirect_dma_start(out=w2t[:, k, :], out_offset=None, in_=w2b_d[:, :], in_offset=bass.IndirectOffsetOnAxis(ap=widx[:, i, k:k + 1], axis=0)),
                 reads=[r_w2bd, r_widx], writes=[r_w2] if k == 0 else (), awrites=() if k == 0 else [r_w2], dma=True)
        S.op("pool", lambda e: e.indirect_dma_start(out=b1t[slot][0][:], out_offset=None, in_=b1r_d[:, :], in_offset=bass.IndirectOffsetOnAxis(ap=bidx[:, i:i + 1], axis=0)),
             reads=[r_in, r_bidx], writes=[b1t[slot][1]], dma=True)
        S.op("pool", lambda e: e.indirect_dma_start(out=b2t[slot][0][:], out_offset=None, in_=b2_d[:, :], in_offset=bass.IndirectOffsetOnAxis(ap=eidx[0:2, i:i + 1], axis=0)),
             reads=[r_in, r_eidx], writes=[b2t[slot][1]], dma=True)

    def load_x(i):
        for s4 in range(4):
            x_t, r_x = xr[(i % 2) * 4 + s4]
            row0 = i * 512 + s4 * 128
            S.dma("sp", x_t[:], xs_d[row0:row0 + 128, :], reads=[r_xsd], writes=[r_x])

    nblk_run = NBLK if "nblk" not in L else L["nblk"]
    load_weights(0, 0)
    load_x(0)
    xc = 0
    for i in range(nblk_run):
        slot = i % 2
        if i + 1 < nblk_run:
            load_weights(i + 1, (i + 1) % 2)
            load_x(i + 1)
        w1t, r_w1 = w1b[slot]
        w2t, r_w2 = w2b[slot]
        b1_t, r_b1 = b1t[slot]
        b2f_t, r_b2f = b2t[slot]
        b2_t, r_b2 = b2b[slot]
        S.op("pool", lambda e: e.tensor_copy(out=b2_t[:], in_=b2f_t[0:1, :]), reads=[r_b2f], writes=[r_b2])
        for s4 in range(4):
            x_t, r_x = xr[(i % 2) * 4 + s4]
            p_t, r_p = pxt[xc % 2]
            xc += 1
            p_b = p_t[:].bitcast(BF16)
            for k in range(8):
                S.op("pe", lambda e: e.transpose(out=p_b[:, k * 128:(k + 1) * 128], in_=x_t[:, k * 128:(k + 1) * 128], identity=ident_b[:]),
                     reads=[r_x, r_identb], writes=[r_p] if k == 0 else (), awrites=() if k == 0 else [r_p])
            S.op("act", lambda e: e.activation(out=XT[:, :, s4 * 128:(s4 + 1) * 128], in_=p_b.rearrange("p (k t) -> p k t", k=8), func=AF.Copy), reads=[r_p],
                 writes=[r_XT] if s4 == 0 else (), awrites=() if s4 == 0 else [r_XT])
        for fc in range(8):
            pg_t, r_pg = pg[fc % 2]
            pu_t, r_pu = pu[fc % 2]
            for k in range(8):
                S.op("pe", lambda e: e.matmul(pg_t[:], lhsT=w1t[:, k, fc * 128:(fc + 1) * 128], rhs=XT[:, k, :], start=(k == 0), stop=(k == 7)),
                     reads=[r_w1, r_XT], writes=[r_pg] if k == 0 else (), awrites=() if k == 0 else [r_pg])
            for k in range(8):
                S.op("pe", lambda e: e.matmul(pu_t[:], lhsT=w1t[:, k, D + fc * 128:D + (fc + 1) * 128], rhs=XT[:, k, :], start=(k == 0), stop=(k == 7)),
                     reads=[r_w1, r_XT], writes=[r_pu] if k == 0 else (), awrites=() if k == 0 else [r_pu])
            hg_t, r_hg = hg[fc % 2]
            hu_t, r_hu = hu[fc % 2]
            sg_t, r_sgm = sgm[fc % 2]
            S.op("act", lambda e: e.activation(out=hg_t[:], in_=pg_t[:], func=AF.Identity, bias=b1_t[:, fc:fc + 1]), reads=[r_pg, r_b1], writes=[r_hg])
            S.op("act", lambda e: e.activation(out=hu_t[:], in_=pu_t[:], func=AF.Identity, bias=b1_t[:, 8 + fc:9 + fc]), reads=[r_pu, r_b1], writes=[r_hu])
            S.op("dve", lambda e: e.tensor_scalar_min(out=hg_t[:], in0=hg_t[:], scalar1=7.0), reads=[r_hg], writes=[r_hg])
            S.op("act", lambda e: e.activation(out=sg_t[:], in_=hg_t[:], func=AF.Sigmoid, scale=1.702), reads=[r_hg], writes=[r_sgm])
            S.op("pool", lambda e: e.tensor_scalar(out=hu_t[:], in0=hu_t[:], scalar1=7.0, scalar2=-7.0, op0=ALU.min, op1=ALU.max), reads=[r_hu], writes=[r_hu])
            S.op("dve", lambda e: e.scalar_tensor_tensor(out=hu_t[:], in0=hu_t[:], scalar=1.0, in1=hg_t[:], op0=ALU.add, op1=ALU.mult), reads=[r_hu, r_hg], writes=[r_hu])
            S.op("dve", lambda e: e.tensor_tensor(out=actT[:, fc, :], in0=hu_t[:], in1=sg_t[:], op=ALU.mult), reads=[r_hu, r_sgm],
                 writes=[r_actT] if fc == 0 else (), awrites=() if fc == 0 else [r_actT])
        for s4 in range(4):
            y_t, r_y = ysb[s4 % 2]
            for hf in range(2):
                py_t, r_py = py[hf]
                for fc in range(8):
                    S.op("pe", lambda e: e.matmul(py_t[:], lhsT=actT[:, fc, s4 * 128:(s4 + 1) * 128], rhs=w2t[:, fc, hf * 512:(hf + 1) * 512], start=(fc == 0), stop=False),
                         reads=[r_actT, r_w2], writes=[r_py] if fc == 0 else (), awrites=() if fc == 0 else [r_py])
                S.op("pe", lambda e: e.matmul(py_t[:], lhsT=ones1f[:], rhs=b2_t[0:1, hf * 512:(hf + 1) * 512], start=False, stop=True), reads=[r_ones1f, r_b2], awrites=[r_py])
                S.op("act", lambda e: e.activation(out=y_t[:, hf * 512:(hf + 1) * 512], in_=py_t[:], func=AF.Copy), reads=[r_py],
                     writes=[r_y] if hf == 0 else (), awrites=() if hf == 0 else [r_y])
            row0 = i * 512 + s4 * 128
            S.dma("act", ys_d[row0:row0 + 128, :], y_t[:], reads=[r_y], awrites=[r_ysd])
    C.pop()

    C.push()
    ln2g, r_ln2g = C.sb("ln2g", [128, D], F32)
    ln2b, r_ln2b = C.sb("ln2b", [128, D], F32)
    gate2, r_gate2 = C.sb("gate2", [128, D], F32)
    S.dma("sp", ln2g[:], g_("ln2g_d").partition_broadcast(128), reads=[r_in], writes=[r_ln2g])
    S.dma("sp", ln2b[:], g_("ln2b_d").partition_broadcast(128), reads=[r_in], writes=[r_ln2b])
    x1r = C.ring("sb", "x1r", 2, [128, D], F32)
    yg = C.ring("sb", "yg", 8, [128, D], BF16)
    acc, r_acc = C.sb("accF", [128, D], F32)
    ot = C.ring("sb", "otF", 2, [128, D], F32)
    st, r_st = C.sb("stF", [128, 2, 6], F32)
    mv, r_mv = C.sb("mvF", [128, 2], F32)
    rs, r_rs = C.sb("rsF", [128, 1], F32)

    def loadF(t, slot):
        tok0 = t * 128
        S.dma("sp", x1r[slot][0][:], x1_d[tok0:tok0 + 128, :], reads=[r_x1], writes=[x1r[slot][1]])
        for j in range(4):
            y_t, r_y = yg[slot * 4 + j]
            S.op("pool", lambda e: e.indirect_dma_start(out=y_t[:], out_offset=None, in_=ys_d[:, :], in_offset=bass.IndirectOffsetOnAxis(ap=idx4[:, t, j:j + 1], axis=0)),
                 reads=[r_ysd, r_idx4], writes=[r_y], dma=True)

    loadF(0, 0)
    for t in range(NT):
        slot = t % 2
        tok0 = t * 128
        b = tok0 // SEQ
        if t % TPS == 0:
            S.dma("sp", gate2[:], mod_d[b:b + 1, 5 * D:6 * D].partition_broadcast(128), reads=[r_mod], writes=[r_gate2])
        if t + 1 < NT:
            loadF(t + 1, (t + 1) % 2)
        x1_t, r_x1t = x1r[slot]
        for j in range(4):
            y_t, r_y = yg[slot * 4 + j]
            if j == 0:
                S.op("dve", lambda e: e.tensor_scalar(out=acc[:], in0=y_t[:], scalar1=g4[:, t, 0:1], scalar2=None, op0=ALU.mult), reads=[r_y, r_g4], writes=[r_acc])
            else:
                S.op("dve", lambda e: e.scalar_tensor_tensor(out=acc[:], in0=y_t[:], scalar=g4[:, t, j:j + 1], in1=acc[:], op0=ALU.mult, op1=ALU.add), reads=[r_y, r_g4, r_acc], writes=[r_acc])
        S.op("pool", lambda e: e.tensor_tensor(out=acc[:], in0=acc[:], in1=gate2[:], op=ALU.mult), reads=[r_acc, r_gate2], writes=[r_acc])
        S.op("dve", lambda e: e.scalar_tensor_tensor(out=acc[:], in0=x1_t[:], scalar=float(ALPHA), in1=acc[:], op0=ALU.mult, op1=ALU.add), reads=[r_x1t, r_acc], writes=[r_acc])
        ln_stats(acc, r_acc, st, r_st, mv, r_mv, rs, r_rs)
        o_t, r_o = ot[slot]
        S.op("dve", lambda e: e.tensor_scalar(out=o_t[:], in0=acc[:], scalar1=mv[:, 0:1], scalar2=rs[:], op0=ALU.subtract, op1=ALU.mult), reads=[r_acc, r_mv, r_rs], writes=[r_o])
        S.op("pool", lambda e: e.tensor_tensor(out=o_t[:], in0=o_t[:], in1=ln2g[:], op=ALU.mult), reads=[r_o, r_ln2g], writes=[r_o])
        S.op("pool", lambda e: e.tensor_tensor(out=o_t[:], in0=o_t[:], in1=ln2b[:], op=ALU.add), reads=[r_o, r_ln2b], writes=[r_o])
        S.dma("sp", out_d[tok0:tok0 + 128, :], o_t[:], reads=[r_o], awrites=[r_out])
    C.pop()
    C.pop()


def phase_C(nc, S, C, dbg, L):
    g_ = lambda n: L[n]
    r_in = g_("r_in"); ident_b = g_("ident_b"); r_identb = g_("r_identb"); ident_f = g_("ident_f"); r_identf = g_("r_identf")
    qT_d, r_qT = g_("qT_d"), g_("r_qT"); iqT_d, r_iqT = g_("iqT_d"), g_("r_iqT")
    kva_d, r_kva = g_("kva_d"), g_("r_kva"); kvT_d, r_kvT = g_("kvT_d"), g_("r_kvT"); ikT_d, r_ikT = g_("ikT_d"), g_("r_ikT")
    iw_d, r_iw = g_("iw_d"), g_("r_iw"); oaT_d, r_oaT = g_("oaT_d"), g_("r_oaT")
    C.push()
    w1_d, w2_d = g_("w1_d"), g_("w2_d")

    def cast_weights(i):
        e_ = i // 2
        if i % 2 == 0:
            S.dma("pool", g_("w1b_d")[e_ * D:(e_ + 1) * D, :], w1_d[e_ * D:(e_ + 1) * D, :], reads=[r_in], awrites=[g_("r_w1bd")])
        else:
            S.dma("pool", g_("w2b_d")[e_ * D:(e_ + 1) * D, :], w2_d[e_ * D:(e_ + 1) * D, :], reads=[r_in], awrites=[g_("r_w2bd")])
    tzf, r_tzf = C.sb("tzf", [128, 2, 8, 128], F32)
    tz, r_tz = C.sb("tzb", [128, 2, 8, 128], BF16)
    cfar, r_cfar = C.sb("cfar", [128, 8], F32)
    identN, r_identN = C.sb("identN", [128, 128], BF16)
    S.dma("sp", tzf[:], g_("tz_d"), reads=[r_in], writes=[r_tzf])
    S.dma("sp", cfar[:], g_("cfar_d").partition_broadcast(128), reads=[r_in], writes=[r_cfar])
    S.op("dve", lambda e: e.tensor_scalar_mul(out=identN[:], in0=ident_f[:], scalar1=-NEG), reads=[r_identf], writes=[r_identN])
    first = True
    for dl in range(2):
        for h in range(8):
            S.op("dve", lambda e: e.tensor_scalar(out=tz[:, dl, h, :], in0=tzf[:, dl, h, :], scalar1=cfar[:, h:h + 1], scalar2=None, op0=ALU.subtract),
                 reads=[r_tzf, r_cfar], writes=[r_tz] if first else (), awrites=() if first else [r_tz])
            first = False
    seqb = [(C.sb("kvT%d" % i, [128, SEQ], BF16), C.sb("ikT%d" % i, [128, SEQ], BF16), C.sb("kvaC%d" % i, [128, TPS, 136], BF16)) for i in range(2)]
    qt = C.ring("sb", "qt", 5, [128, 8, 128], BF16)
    iqt = C.ring("sb", "iqt", 4, [128, 4, 128], BF16)
    iwt = C.ring("sb", "iwC", 4, [128, 8], F32)
    scorer = C.ring("sb", "score", 3, [128, SEQ], F32)
    penr = C.ring("sb", "pen", 2, [128, SEQ], BF16)
    rl = C.ring("sb", "rl", 2, [128, 512], F32)
    m8, r_m8 = C.sb("m8", [128, 8], F32)
    expT = C.ring("sb", "expT", 2, [128, TPS * 128], BF16)
    posb = C.ring("sb", "posb", 2, [128, 8, 132], F32)
    rc, r_rc = C.sb("rcC", [128, 8], F32)
    oa, r_oa = C.sb("oa", [128, D], BF16)
    oaT = C.ring("sb", "oaT", 2, [128, 8, 128], BF16)
    pi = C.ring("ps", "pi", 2, [128, 512], F32)
    pl = C.ring("ps", "pl", 3, [128, 512], F32)
    po = C.ring("ps", "po", 2, [128, 512], F32)
    ptr = C.ring("ps", "ptrC", 1, [128, 512], F32)
    NTILE = NB * TPS

    def load_seq(b):
        (kvT, r_kvTs), (ikT, r_ikTs), (kva, r_kvas) = seqb[b % 2]
        s0 = b * SEQ
        S.dma("sp", kvT[:], kvT_d[:, s0:s0 + SEQ], reads=[r_kvT], writes=[r_kvTs])
        S.dma("sp", ikT[:], ikT_d[:, s0:s0 + SEQ], reads=[r_ikT], writes=[r_ikTs])
        S.dma("sp", kva[:], kva_d[s0:s0 + SEQ, :].rearrange("(k p) c -> p k c", p=128), reads=[r_kva], writes=[r_kvas])

    def load_tile(i):
        tok0 = i * 128
        S.dma("sp", qt[i % 5][0][:], qT_d[:, :, tok0:tok0 + 128].rearrange("h p t -> p h t"), reads=[r_qT], writes=[qt[i % 5][1]])
        S.dma("sp", iqt[i % 4][0][:], iqT_d[:, :, tok0:tok0 + 128].rearrange("h p t -> p h t"), reads=[r_iqT], writes=[iqt[i % 4][1]])
        S.dma("sp", iwt[i % 4][0][:], iw_d[tok0:tok0 + 128, :], reads=[r_iw], writes=[iwt[i % 4][1]])

    NIT = 24
    pow2, r_pow2 = C.sb("pow2", [128, NIT + 1], F32)
    stepsr = C.ring("sb", "steps", 3, [128, NIT + 1], F32)
    bisr = C.ring("sb", "bis", 3, [128, 8], F32)
    junkb, r_junkb = C.sb("junkb", [128, SEQ], BF16)
    for j in range(NIT + 1):
        S.op("pool", lambda e: e.memset(pow2[:, j:j + 1], float(2.0 ** -(j + 1))), writes=[r_pow2] if j == 0 else (), awrites=() if j == 0 else [r_pow2])

    def prep_score_gen(i):
        b, t = divmod(i, TPS)
        (ikT, r_ikTs) = seqb[b % 2][1]
        iq_t, r_iq = iqt[i % 4]
        iw_t, r_iwt = iwt[i % 4]
        pen, r_pen = penr[i % 2]
        score, r_score = scorer[i % 3]
        steps, r_steps = stepsr[i % 3]
        bis, r_bis = bisr[i % 3]
        N = 128 * (t + 1)
        if t < 2:
            return
            yield
        for kg in range(0, N, 512):
            w = min(512, N - kg)
            for h in range(8):
                pr, hf = divmod(h, 2)
                p_t, r_p = pi[h % 2]
                S.op("pe", lambda e: e.matmul(p_t[:, 0:w], lhsT=iq_t[64 * hf:64 * hf + 64, pr, :], rhs=ikT[64 * hf:64 * hf + 64, kg:kg + w], start=True, stop=True),
                     reads=[r_iq, r_ikTs], writes=[r_p])
                r_t, r_r = rl[h % 2]
                if h == 0:
                    S.op("dve", lambda e: e.tensor_scalar(out=score[:, kg:kg + w], in0=p_t[:, 0:w], scalar1=0.0, scalar2=iw_t[:, 0:1], op0=ALU.max, op1=ALU.mult),
                         reads=[r_p, r_iwt], writes=[r_score] if kg == 0 else (), awrites=() if kg == 0 else [r_score])
                elif h % 2 == 1:
                    S.op("dve", lambda e: e.tensor_scalar(out=r_t[:, 0:w], in0=p_t[:, 0:w], scalar1=0.0, scalar2=iw_t[:, h:h + 1], op0=ALU.max, op1=ALU.mult),
                         reads=[r_p, r_iwt], writes=[r_r])
                    S.op("pool", lambda e: e.tensor_tensor(out=score[:, kg:kg + w], in0=score[:, kg:kg + w], in1=r_t[:, 0:w], op=ALU.add),
                         reads=[r_r, r_score], awrites=[r_score])
                else:
                    S.op("act", lambda e: e.activation(out=r_t[:, 0:w], in_=p_t[:, 0:w], func=AF.Relu), reads=[r_p], writes=[r_r])
                    S.op("dve", lambda e: e.scalar_tensor_tensor(out=score[:, kg:kg + w], in0=r_t[:, 0:w], scalar=iw_t[:, h:h + 1], in1=score[:, kg:kg + w], op0=ALU.mult, op1=ALU.add),
                         reads=[r_r, r_iwt, r_score], awrites=[r_score])
                yield
        S.op("dve", lambda e: e.tensor_reduce(out=bis[:, 0:1], in_=score[:, 0:N - 64], axis=AX.X, op=ALU.min), reads=[r_score], writes=[r_bis])
        S.op("dve", lambda e: e.memset(score[0:64, N - 64:N], -1e30), reads=[r_score], awrites=[r_score])
        S.op("dve", lambda e: e.max(out=m8[:], in_=score[:, 0:N]), reads=[r_score], writes=[r_m8])
        S.op("dve", lambda e: e.tensor_tensor(out=bis[:, 1:2], in0=m8[:, 0:1], in1=bis[:, 0:1], op=ALU.subtract), reads=[r_m8, r_bis], awrites=[r_bis])
        S.op("dve", lambda e: e.tensor_scalar(out=steps[:], in0=pow2[:], scalar1=bis[:, 1:2], scalar2=None, op0=ALU.mult), reads=[r_pow2, r_bis], writes=[r_steps])
        S.op("dve", lambda e: e.tensor_tensor(out=bis[:, 2:3], in0=bis[:, 0:1], in1=steps[:, 0:1], op=ALU.add), reads=[r_bis, r_steps], awrites=[r_bis])

    def prep_score(i):
        for _ in prep_score_gen(i):
            pass

    def prep_iter(i, j):
        b, t = divmod(i, TPS)
        if t < 2:
            return
        N = 128 * (t + 1)
        score, r_score = scorer[i % 3]
        steps, r_steps = stepsr[i % 3]
        bis, r_bis = bisr[i % 3]
        S.op("act", lambda e: e.activation(out=junkb[:, 0:N], in_=score[:, 0:N], func=AF.Sign, scale=-1.0, bias=bis[:, 2:3], accum_out=bis[:, 4:5]),
             reads=[r_score, r_bis], writes=[r_junkb], awrites=[r_bis])
        S.op("dve", lambda e: e.tensor_scalar(out=bis[:, 3:4], in0=bis[:, 4:5], scalar1=float(N - 511), scalar2=steps[:, j:j + 1], op0=ALU.is_le, op1=ALU.mult),
             reads=[r_bis, r_steps], awrites=[r_bis])
        S.op("dve", lambda e: e.scalar_tensor_tensor(out=bis[:, 2:3], in0=bis[:, 3:4], scalar=steps[:, j + 1:j + 2], in1=bis[:, 2:3], op0=ALU.subtract, op1=ALU.add),
             reads=[r_bis, r_steps], awrites=[r_bis])

    def prep_fin(i):
        b, t = divmod(i, TPS)
        N = 128 * (t + 1)
        pen, r_pen = penr[i % 2]
        if t < 2:
            S.op("dve", lambda e: e.memset(pen[:, 0:N], 0.0), writes=[r_pen])
            S.op("dve", lambda e: e.memset(pen[0:64, N - 64:N], -1.0), awrites=[r_pen])
            return
        score, r_score = scorer[i % 3]
        steps, r_steps = stepsr[i % 3]
        bis, r_bis = bisr[i % 3]
        S.op("dve", lambda e: e.tensor_tensor(out=bis[:, 5:6], in0=bis[:, 2:3], in1=steps[:, NIT:NIT + 1], op=ALU.subtract), reads=[r_bis, r_steps], awrites=[r_bis])
        S.op("dve", lambda e: e.tensor_scalar(out=pen[:, 0:N], in0=score[:, 0:N], scalar1=bis[:, 5:6], scalar2=1.0, op0=ALU.is_ge, op1=ALU.subtract),
             reads=[r_score, r_bis], writes=[r_pen])

    state = {"plc": 0, "hc": 0}

    def attend_head(i, h):
        b, t = divmod(i, TPS)
        (kvT, r_kvTs), _, (kva, r_kvas) = seqb[b % 2]
        q_t, r_q = qt[i % 5]
        pen, r_pen = penr[i % 2]
        ps_t, r_ps = posb[i % 2]
        nkb = t + 1
        if True:
            e_t, r_e = expT[state["hc"] % 2]
            state["hc"] += 1
            for kg in range(0, nkb, 4):
                nb_ = min(4, nkb - kg)
                p_t, r_p = pl[state["plc"] % 3]
                state["plc"] += 1
                for ii in range(nb_):
                    kb = kg + ii
                    near = kb >= t - 1
                    cs_ = slice(ii * 128, (ii + 1) * 128)
                    S.op("pe", lambda e: e.matmul(p_t[:, cs_], lhsT=kvT[:, kb * 128:(kb + 1) * 128], rhs=q_t[:, h, :], start=True, stop=False),
                         reads=[r_kvTs, r_q], writes=[r_p] if ii == 0 else (), awrites=() if ii == 0 else [r_p])
                    S.op("pe", lambda e: e.matmul(p_t[:, cs_], lhsT=pen[:, kb * 128:(kb + 1) * 128], rhs=identN[:], start=False, stop=(not near)),
                         reads=[r_pen, r_identN], awrites=[r_p])
                    if near:
                        S.op("pe", lambda e: e.matmul(p_t[:, cs_], lhsT=ident_b[:], rhs=tz[:, t - kb, h, :], start=False, stop=True),
                             reads=[r_identb, r_tz], awrites=[r_p])
                S.op("act", lambda e: e.activation(out=e_t[:, kg * 128:(kg + nb_) * 128], in_=p_t[:, 0:nb_ * 128], func=AF.Exp), reads=[r_p],
                     writes=[r_e] if kg == 0 else (), awrites=() if kg == 0 else [r_e])
            o_t, r_o = po[h % 2]
            for kb in range(nkb):
                S.op("pe", lambda e: e.matmul(o_t[:, 0:129], lhsT=e_t[:, kb * 128:(kb + 1) * 128], rhs=kva[:, kb, 0:129], start=(kb == 0), stop=(kb == nkb - 1)),
                     reads=[r_e, r_kvas], writes=[r_o] if kb == 0 else (), awrites=() if kb == 0 else [r_o])
            S.op("act", lambda e: e.activation(out=ps_t[:, h, 0:129], in_=o_t[:, 0:129], func=AF.Copy), reads=[r_o],
                 writes=[r_ps] if h == 0 else (), awrites=() if h == 0 else [r_ps])

    def finalize(i):
        tok0 = i * 128
        ps_t, r_ps = posb[i % 2]
        S.op("dve", lambda e: e.reciprocal(out=rc[:], in_=ps_t[:, :, 128]), reads=[r_ps], writes=[r_rc])
        S.op("dve", lambda e: e.tensor_tensor(out=oa[:].rearrange("p (h c) -> p h c", h=8), in0=ps_t[:, :, 0:128], in1=rc[:].unsqueeze(2).to_broadcast([128, 8, 128]), op=ALU.mult),
             reads=[r_ps, r_rc], writes=[r_oa])
        pT, r_pT = ptr[0]
        pT_b = pT[:].bitcast(BF16)
        for k in range(8):
            S.op("pe", lambda e: e.transpose(out=pT_b[:, k * 128:(k + 1) * 128], in_=oa[:, k * 128:(k + 1) * 128], identity=ident_b[:]),
                 reads=[r_oa, r_identb], writes=[r_pT] if k == 0 else (), awrites=() if k == 0 else [r_pT])
        oT, r_oT = oaT[i % 2]
        S.op("act", lambda e: e.activation(out=oT[:].rearrange("p k t -> p (k t)"), in_=pT_b, func=AF.Copy), reads=[r_pT], writes=[r_oT])
        S.dma("sp", oaT_d[:, :, tok0:tok0 + 128].rearrange("c p t -> p c t"), oT[:], reads=[r_oT], awrites=[r_oaT])

    HALF = NIT // 2
    load_seq(0)
    for i0 in range(4):
        load_tile(i0)
    prep_score(0)
    for j in range(NIT):
        prep_iter(0, j)
    prep_fin(0)
    prep_score(1)
    for j in range(HALF):
        prep_iter(1, j)
    prep_score(2)
    for i in range(NTILE):
        b, t = divmod(i, TPS)
        if t == 0 and b + 1 < NB:
            load_seq(b + 1)
        if i + 4 < NTILE:
            load_tile(i + 4)
        cast_weights(i)
        n1 = i + 1 < NTILE
        n2 = i + 2 < NTILE
        gen = prep_score_gen(i + 3) if i + 3 < NTILE else iter(())
        npieces = 8 * ((128 * (((i + 3) % TPS) + 1) + 511) // 512) if (i + 3 < NTILE and (i + 3) % TPS >= 2) else 0
        ppb = (npieces + 7) // 8
        sched = []
        for k in range(HALF):
            if n1:
                sched.append((i + 1, HALF + k))
            if n2:
                sched.append((i + 2, k))
        per = (len(sched) + 7) // 8
        for h in range(8):
            attend_head(i, h)
            its = sched[h * per:(h + 1) * per]
            for n_, (ti_, j) in enumerate(its):
                prep_iter(ti_, j)
                if n_ < ppb:
                    next(gen, None)
            for _ in range(max(0, ppb - len(its))):
                next(gen, None)
        for _ in gen:
            pass
        if n1:
            prep_fin(i + 1)
        if i >= 1:
            finalize(i - 1)
    finalize(NTILE - 1)
    C.pop()


def phase_B(nc, S, C, dbg, L):
    g_ = lambda n: L[n]
    r_in = g_("r_in"); ident_b = g_("ident_b"); r_identb = g_("r_identb")
    xbcT_d, r_xbcT = g_("xbcT_d"), g_("r_xbcT"); dt_d, r_dt = g_("dt_d"), g_("r_dt"); zs_d, r_zs = g_("zs_d"), g_("r_zs")
    obT_d, r_obT = g_("obT_d"), g_("r_obT")
    C.push()
    convw, r_convw = C.sb("convw", [128, 16, 4], F32)
    convb, r_convb = C.sb("convb", [128, 16], F32)
    dg, r_dg = C.sb("dg", [128, 16, 4, 128], BF16)
    identf2, r_identf2 = C.sb("identf2", [128, 128], F32)
    a_bc, r_abc = C.sb("a_bc", [128, 16], F32)
    dskip_bc, r_dskip = C.sb("dskip_bc", [128, 16], F32)
    normw_bc, r_normw = C.sb("normw_bc", [128, D], F32)
    triU, r_triU = C.sb("triU", [128, 128], F32)
    SLm, r_SL = C.sb("SLm", [128, 128], F32)
    onesf, r_onesf = C.sb("onesf", [128, 128], F32)
    negm4, r_negm4 = C.sb("negm4", [128, 512], BF16)
    S.dma("sp", convw[:], g_("convw_d"), reads=[r_in], writes=[r_convw])
    S.dma("sp", convb[:], g_("convb_d"), reads=[r_in], writes=[r_convb])
    S.dma("sp", identf2[:], g_("ident_d"), reads=[r_in], writes=[r_identf2])
    S.dma("sp", a_bc[:], g_("alog_d").partition_broadcast(128), reads=[r_in], writes=[r_abc])
    S.dma("sp", dskip_bc[:], g_("dskip_d").partition_broadcast(128), reads=[r_in], writes=[r_dskip])
    S.dma("sp", normw_bc[:], g_("normw_d").partition_broadcast(128), reads=[r_in], writes=[r_normw])
    S.dma("sp", triU[:], g_("triU_d"), reads=[r_in], writes=[r_triU])
    S.dma("sp", SLm[:], g_("SL_d"), reads=[r_in], writes=[r_SL])
    S.dma("pool", negm4[:], g_("negm4_d"), reads=[r_in], writes=[r_negm4])
    S.op("dve", lambda e: e.memset(onesf[:], 1.0), writes=[r_onesf])
    S.op("act", lambda e: e.activation(out=a_bc[:], in_=a_bc[:], func=AF.Exp), reads=[r_abc], writes=[r_abc])
    S.op("dve", lambda e: e.tensor_scalar_mul(out=a_bc[:], in0=a_bc[:], scalar1=-1.0), reads=[r_abc], writes=[r_abc])
    first = True
    for j in range(16):
        for k in range(4):
            S.op("dve", lambda e: e.tensor_scalar_mul(out=dg[:, j, k, :], in0=identf2[:], scalar1=convw[:, j, k:k + 1]),
                 reads=[r_identf2, r_convw], writes=[r_dg] if first else (), awrites=() if first else [r_dg])
            first = False

    bank = C.ring("ps", "bk", 8, [128, 512], F32)
    xh = C.ring("sb", "xh", 2, [128, 16, 131], BF16)
    dtl = C.ring("sb", "dtl", 2, [128, 16], F32)
    zl = C.ring("sb", "zl", 2, [128, D], BF16)
    xact, r_xact = C.sb("xact", [128, 16, 128], BF16)
    xs_tok, r_xs = C.sb("xs_tok", [128, D], BF16)
    B_tok, r_Bt = C.sb("B_tok", [128, 512], BF16)
    adt, r_adt = C.sb("adt", [128, 16], F32)
    sm, r_sm = C.sb("smB", [128, 8, 16], F32)
    Amat, r_A = C.sb("Amat", [128, 16, 128], F32)
    Lt, r_Lt = C.sb("Lt", [128, 2, 512], F32)
    Mt, r_Mt = C.sb("Mt", [128, 16, 128], BF16)
    xdt, r_xdt = C.sb("xdt", [128, D], BF16)
    xdd, r_xdd = C.sb("xdd", [128, D], BF16)
    prev_f, r_pf = C.sb("prev_f", [128, D], F32)
    prev_b, r_pb = C.sb("prev_b", [128, D], BF16)
    t1, r_t1 = C.sb("t1", [128, D], F32)
    t2, r_t2 = C.sb("t2", [128, D], F32)
    junk, r_junk = C.sb("junkB", [128, 256], F32)
    ob, r_ob = C.sb("ob", [128, D], BF16)
    obT = C.ring("sb", "obT", 2, [128, 8, 128], BF16)

    def bc3(ap2, n):
        return ap2.unsqueeze(2).to_broadcast([128, 16, n])

    def v3(t, n=64):
        return t.rearrange("p (h q) -> p h q", q=n)

    def load_chunk(b, t, slot):
        tok0 = b * SEQ + t * 128
        x_t, r_x = xh[slot]
        if t == 0:
            S.op("pool", lambda e: e.memset(x_t[:, :, 0:3], 0.0), writes=[r_x])
            S.dma("sp", x_t[:, :, 3:131], xbcT_d[:, :, tok0:tok0 + 128].rearrange("c p t -> p c t"), reads=[r_xbcT], awrites=[r_x])
        else:
            S.dma("sp", x_t[:, :, :], xbcT_d[:, :, tok0 - 3:tok0 + 128].rearrange("c p t -> p c t"), reads=[r_xbcT], writes=[r_x])
        S.dma("sp", dtl[slot][0][:], dt_d[tok0:tok0 + 128, :], reads=[r_dt], writes=[dtl[slot][1]])
        S.dma("sp", zl[slot][0][:], zs_d[tok0:tok0 + 128, :], reads=[r_zs], writes=[zl[slot][1]])

    nch = NB * TPS
    load_chunk(0, 0, 0)
    for ci in range(nch):
        b, t = divmod(ci, TPS)
        slot = ci % 2
        tok0 = b * SEQ + t * 128
        if ci + 1 < nch:
            load_chunk((ci + 1) // TPS, (ci + 1) % TPS, (ci + 1) % 2)
        x_t, r_x = xh[slot]
        d_t, r_d = dtl[slot]
        z_t, r_z = zl[slot]
        if t == 0:
            S.op("pool", lambda e: e.memset(prev_f[:], 0.0), writes=[r_pf])
            S.op("pool", lambda e: e.memset(prev_b[:], 0.0), writes=[r_pb])
        for jg in range(4):
            pc, r_pc = bank[jg % 2]
            for jj in range(4):
                j = jg * 4 + jj
                for k in range(4):
                    S.op("pe", lambda e: e.matmul(pc[:, jj * 128:(jj + 1) * 128], lhsT=dg[:, j, k, :], rhs=x_t[:, j, k:k + 128], start=(k == 0), stop=(k == 3)),
                         reads=[r_dg, r_x], writes=[r_pc] if (jj == 0 and k == 0) else (), awrites=() if (jj == 0 and k == 0) else [r_pc])
            for jj in range(4):
                j = jg * 4 + jj
                S.op("act", lambda e: e.activation(out=xact[:, j, :], in_=pc[:, jj * 128:(jj + 1) * 128], func=AF.Silu, bias=convb[:, j:j + 1]),
                     reads=[r_pc, r_convb], writes=[r_xact] if j == 0 else (), awrites=() if j == 0 else [r_xact])
        pxs, r_pxs = bank[2]
        pB, r_pB = bank[3]
        pxs_b = pxs[:].bitcast(BF16)
        pB_b = pB[:].bitcast(BF16)
        for k in range(8):
            S.op("pe", lambda e: e.transpose(out=pxs_b[:, k * 128:(k + 1) * 128], in_=xact[:, k, :], identity=ident_b[:]),
                 reads=[r_xact, r_identb], writes=[r_pxs] if k == 0 else (), awrites=() if k == 0 else [r_pxs])
        for k in range(4):
            S.op("pe", lambda e: e.transpose(out=pB_b[:, k * 128:(k + 1) * 128], in_=xact[:, 8 + k, :], identity=ident_b[:]),
                 reads=[r_xact, r_identb], writes=[r_pB] if k == 0 else (), awrites=() if k == 0 else [r_pB])
        S.op("act", lambda e: e.activation(out=xs_tok[:], in_=pxs_b, func=AF.Copy), reads=[r_pxs], writes=[r_xs])
        S.op("act", lambda e: e.activation(out=B_tok[:], in_=pB_b[:, 0:512], func=AF.Copy), reads=[r_pB], writes=[r_Bt])
        S.op("dve", lambda e: e.tensor_tensor(out=adt[:], in0=d_t[:], in1=a_bc[:], op=ALU.mult), reads=[r_d, r_abc], writes=[r_adt])
        S.op("pe", lambda e: e.matmul(pB[:, 256:272], lhsT=triU[:], rhs=adt[:], start=True, stop=True), reads=[r_triU, r_adt], awrites=[r_pB])
        S.op("pe", lambda e: e.matmul(pB[:, 272:288], lhsT=onesf[:], rhs=adt[:], start=True, stop=True), reads=[r_onesf, r_adt], awrites=[r_pB])
        S.op("act", lambda e: e.activation(out=sm[:, 0, :], in_=pB[:, 256:272], func=AF.Exp), reads=[r_pB], writes=[r_sm])
        S.op("act", lambda e: e.activation(out=sm[:, 1, :], in_=pB[:, 272:288], func=AF.Copy), reads=[r_pB], awrites=[r_sm])
        S.op("act", lambda e: e.activation(out=sm[:, 2, :], in_=pB[:, 272:288], func=AF.Exp), reads=[r_pB], awrites=[r_sm])
        S.op("dve", lambda e: e.tensor_tensor(out=sm[:, 3, :], in0=sm[:, 1, :], in1=pB[:, 256:272], op=ALU.subtract), reads=[r_sm, r_pB], awrites=[r_sm])
        S.op("act", lambda e: e.activation(out=sm[:, 4, :], in_=sm[:, 3, :], func=AF.Exp), reads=[r_sm], awrites=[r_sm])
        S.op("dve", lambda e: e.tensor_tensor(out=Amat[:], in0=triU[:].unsqueeze(1).to_broadcast([128, 16, 128]), in1=bc3(adt[:], 128), op=ALU.mult),
             reads=[r_triU, r_adt], writes=[r_A])
        pCB, r_pCB = bank[4]
        for g in range(4):
            S.op("pe", lambda e: e.matmul(pCB[:, g * 128:(g + 1) * 128], lhsT=xact[:, 8 + g, :], rhs=xact[:, 12 + g, :], start=True, stop=True),
                 reads=[r_xact], writes=[r_pCB] if g == 0 else (), awrites=() if g == 0 else [r_pCB])
        S.op("dve", lambda e: e.tensor_tensor(out=v3(xdt[:]), in0=v3(xs_tok[:]), in1=bc3(d_t[:], 64), op=ALU.mult), reads=[r_xs, r_d], writes=[r_xdt])
        S.op("pool", lambda e: e.tensor_tensor(out=v3(xdd[:]), in0=v3(xdt[:]), in1=bc3(sm[:, 4, :], 64), op=ALU.mult), reads=[r_xdt, r_sm], writes=[r_xdd])
        for g in range(4):
            pD, r_pD = bank[g % 2]
            S.op("pe", lambda e: e.matmul(pD[:], lhsT=SLm[:], rhs=Amat[:, 4 * g:4 * g + 4, :], start=True, stop=False), reads=[r_SL, r_A], writes=[r_pD])
            S.op("pe", lambda e: e.matmul(pD[:], lhsT=ident_b[:], rhs=negm4[:], start=False, stop=True), reads=[r_identb, r_negm4], awrites=[r_pD])
            S.op("act", lambda e: e.activation(out=Lt[:, g % 2, :], in_=pD[:], func=AF.Exp), reads=[r_pD], writes=[r_Lt] if g % 2 == 0 else (), awrites=() if g % 2 == 0 else [r_Lt])
            S.op("dve", lambda e: e.tensor_tensor(out=Mt[:, 4 * g:4 * g + 4, :], in0=Lt[:, g % 2, :].rearrange("p (h l) -> p h l", h=4),
                                                 in1=pCB[:, g * 128:(g + 1) * 128].unsqueeze(1).to_broadcast([128, 4, 128]), op=ALU.mult),
                 reads=[r_Lt, r_pCB], writes=[r_Mt] if g == 0 else (), awrites=() if g == 0 else [r_Mt])
        for hh in range(2):
            pY, r_pY = bank[5]
            pO, r_pO = bank[6]
            pS, r_pS = bank[7]
            c0 = hh * 512
            for h8 in range(8):
                h = hh * 8 + h8
                S.op("pe", lambda e: e.matmul(pY[:, h8 * 64:(h8 + 1) * 64], lhsT=Mt[:, h, :], rhs=xdt[:, h * 64:(h + 1) * 64], start=True, stop=True),
                     reads=[r_Mt, r_xdt], writes=[r_pY] if h8 == 0 else (), awrites=() if h8 == 0 else [r_pY])
            for g2 in range(2):
                g = hh * 2 + g2
                S.op("pe", lambda e: e.matmul(pO[:, g2 * 256:(g2 + 1) * 256], lhsT=xact[:, 12 + g, :], rhs=prev_b[:, g * 256:(g + 1) * 256], start=True, stop=True),
                     reads=[r_xact, r_pb], writes=[r_pO] if g2 == 0 else (), awrites=() if g2 == 0 else [r_pO])
            for g2 in range(2):
                g = hh * 2 + g2
                S.op("pe", lambda e: e.matmul(pS[:, g2 * 256:(g2 + 1) * 256], lhsT=B_tok[:, g * 128:(g + 1) * 128], rhs=xdd[:, g * 256:(g + 1) * 256], start=True, stop=True),
                     reads=[r_Bt, r_xdd], writes=[r_pS] if g2 == 0 else (), awrites=() if g2 == 0 else [r_pS])
            hs = slice(hh * 8, hh * 8 + 8)

            def v8(ap):
                return ap.rearrange("p (h q) -> p h q", q=64)
            ex8 = sm[:, 0, hs].unsqueeze(2).to_broadcast([128, 8, 64])
            cd8 = sm[:, 2, hs].unsqueeze(2).to_broadcast([128, 8, 64])
            ds8 = dskip_bc[:, hs].unsqueeze(2).to_broadcast([128, 8, 64])
            S.op("dve", lambda e: e.tensor_tensor(out=v8(t1[:, c0:c0 + 512]), in0=v8(pO[:]), in1=ex8, op=ALU.mult), reads=[r_pO, r_sm], writes=[r_t1] if hh == 0 else (), awrites=() if hh == 0 else [r_t1])
            S.op("dve", lambda e: e.tensor_tensor(out=t1[:, c0:c0 + 512], in0=t1[:, c0:c0 + 512], in1=pY[:], op=ALU.add), reads=[r_t1, r_pY], awrites=[r_t1])
            S.op("pool", lambda e: e.tensor_tensor(out=v8(t2[:, c0:c0 + 512]), in0=v8(xs_tok[:, c0:c0 + 512]), in1=ds8, op=ALU.mult), reads=[r_xs, r_dskip], writes=[r_t2] if hh == 0 else (), awrites=() if hh == 0 else [r_t2])
            S.op("dve", lambda e: e.tensor_tensor(out=v8(prev_f[:, c0:c0 + 512]), in0=v8(prev_f[:, c0:c0 + 512]), in1=cd8, op=ALU.mult), reads=[r_pf, r_sm], awrites=[r_pf])
            S.op("dve", lambda e: e.tensor_tensor(out=prev_f[:, c0:c0 + 512], in0=prev_f[:, c0:c0 + 512], in1=pS[:], op=ALU.add), reads=[r_pf, r_pS], awrites=[r_pf])
            S.op("act", lambda e: e.activation(out=prev_b[:, c0:c0 + 512], in_=prev_f[:, c0:c0 + 512], func=AF.Copy), reads=[r_pf, r_pO], awrites=[r_pb])
        S.op("pool", lambda e: e.tensor_tensor(out=t1[:], in0=t1[:], in1=t2[:], op=ALU.add), reads=[r_t1, r_t2], writes=[r_t1])
        S.op("pool", lambda e: e.tensor_tensor(out=t1[:], in0=t1[:], in1=z_t[:], op=ALU.mult), reads=[r_t1, r_z], writes=[r_t1])
        for g in range(4):
            S.op("act", lambda e: e.activation(out=junk[:], in_=t1[:, g * 256:(g + 1) * 256], func=AF.Square, accum_out=sm[:, 5, g:g + 1]),
                 reads=[r_t1], writes=[r_junk], awrites=[r_sm])
        S.op("act", lambda e: e.activation(out=sm[:, 5, 4:8], in_=sm[:, 5, 0:4], func=AF.Sqrt, scale=1.0 / 256.0, bias=EPS), reads=[r_sm], awrites=[r_sm])
        S.op("dve", lambda e: e.reciprocal(out=sm[:, 5, 8:12], in_=sm[:, 5, 4:8]), reads=[r_sm], awrites=[r_sm])
        S.op("dve", lambda e: e.tensor_tensor(out=t1[:].rearrange("p (g q) -> p g q", g=4), in0=t1[:].rearrange("p (g q) -> p g q", g=4),
                                             in1=sm[:, 5, 8:12].unsqueeze(2).to_broadcast([128, 4, 256]), op=ALU.mult), reads=[r_t1, r_sm], writes=[r_t1])
        S.op("dve", lambda e: e.tensor_tensor(out=ob[:], in0=t1[:], in1=normw_bc[:], op=ALU.mult), reads=[r_t1, r_normw], writes=[r_ob])
        pT, r_pT = bank[2]
        pT_b = pT[:].bitcast(BF16)
        for k in range(8):
            S.op("pe", lambda e: e.transpose(out=pT_b[:, k * 128:(k + 1) * 128], in_=ob[:, k * 128:(k + 1) * 128], identity=ident_b[:]),
                 reads=[r_ob, r_identb], writes=[r_pT] if k == 0 else (), awrites=() if k == 0 else [r_pT])
        o_t, r_o = obT[slot]
        S.op("act", lambda e: e.activation(out=o_t[:].rearrange("p k t -> p (k t)"), in_=pT_b, func=AF.Copy), reads=[r_pT], writes=[r_o])
        S.dma("sp", obT_d[:, :, tok0:tok0 + 128].rearrange("c p t -> p c t"), o_t[:], reads=[r_o], awrites=[r_obT])
    C.pop()


def _t5_bucket_np(rel):
    half, max_exact = 16, 8
    ret = (rel > 0).astype(np.int32) * half
    n = np.abs(rel)
    nf = np.maximum(n, 1).astype(np.float32)
    large = max_exact + (np.log(nf / np.float32(max_exact)) / np.float32(np.log(128.0 / 8.0)) * np.float32(half - max_exact)).astype(np.int32)
    large = np.minimum(large, half - 1)
    return ret + np.where(n < max_exact, n, large)


def _t5_blocks(rel_bias):
    k = np.arange(128)[:, None]
    q = np.arange(128)[None, :]
    out = np.zeros((128, 2, 8, 128), np.float32)
    for dl in range(2):
        bk = _t5_bucket_np((k - 128 * dl) - q)
        for h in range(8):
            out[:, dl, h, :] = rel_bias[bk, h]
    return out


def host_inputs(inputs, core):
    b0 = core * NB
    f = lambda a: np.ascontiguousarray(a, dtype=np.float32)
    c = inputs["c"][b0:b0 + NB]
    m = {
        "x": f(inputs["x"][b0:b0 + NB].reshape(NTOK, D)),
        "cT": f(c.reshape(NB, 8, 128).transpose(2, 1, 0)),
        "w_mod": f(inputs["w_mod"][0]),
        "b_mod": f(inputs["b_mod"][0].reshape(1, -1)),
        "w_in": f(inputs["w_in"][0]),
        "ident": np.eye(128, dtype=np.float32),
        "kv_norm_w": f(inputs["kv_norm_w"][0].reshape(1, -1)),
        "idx_k_norm_w": f(inputs["idx_k_norm_w"][0].reshape(1, -1)),
        "idx_k_norm_b": f(inputs["idx_k_norm_b"][0].reshape(1, -1)),
        "dt_bias": f(inputs["dt_bias"][0].reshape(1, -1)),
        "convw": f(inputs["conv_w"][0].reshape(4, 16, 128).transpose(2, 1, 0)),
        "convb": f(inputs["conv_b"][0].reshape(16, 128).T),
        "a_log": f(inputs["a_log"][0].reshape(1, -1)),
        "d_skip": f(inputs["d_skip"][0].reshape(1, -1)),
        "ssm_norm_w": f(inputs["ssm_norm_w"][0].reshape(1, -1)),
        "w_proj_a": f(inputs["w_proj_a"][0]), "w_proj_b": f(inputs["w_proj_b"][0]), "w_out": f(inputs["w_out"][0]),
        "ln1_g": f(inputs["ln1_g"][0].reshape(1, -1)), "ln1_b": f(inputs["ln1_b"][0].reshape(1, -1)),
        "ln2_g": f(inputs["ln2_g"][0].reshape(1, -1)), "ln2_b": f(inputs["ln2_b"][0].reshape(1, -1)),
        "w_router": f(inputs["w_router"][0]), "b_router": f(inputs["b_router"][0].reshape(1, -1)),
        "w1": f(inputs["w1"][0].reshape(NE * D, 2 * D)), "w2": f(inputs["w2"][0].reshape(NE * D, D)),
        "b1r": f(inputs["b1"][0].reshape(NE, 16, 128).transpose(0, 2, 1).reshape(NE * 128, 16)),
        "b2": f(inputs["b2"][0]),
        "sut": np.triu(np.ones((128, 128), np.float32), 1),
        "thr16": np.tile(512.0 * np.arange(16, dtype=np.float32), NE).reshape(1, -1),
        "bstart": (512.0 * np.arange(NBLK, dtype=np.float32)).reshape(1, -1),
        "kp": (np.arange(8, dtype=np.float32)[None, :] * 128 + np.arange(128, dtype=np.float32)[:, None]),
        "pcol": np.arange(128, dtype=np.float32).reshape(128, 1),
        "sut32": np.triu(np.ones((NE, NE), np.float32), 1),
        "tz": _t5_blocks(f(inputs["rel_bias"])),
        "cfar": f(inputs["rel_bias"][15:16, :]),
        "triU": np.triu(np.ones((128, 128), np.float32)),
        "SL": np.tril(np.ones((128, 128), np.float32), -1),
        "negm4": np.tile(np.tril(np.full((128, 128), NEG, np.float32), -1), (1, 4)),
    }
    return m


def kernel(**inputs):
    nc = build_program()
    in_maps = [host_inputs(inputs, c) for c in range(NCORES)]
    res = run_bass_kernel_spmd(nc, in_maps, core_ids=list(range(NCORES)))
    out = np.stack([np.asarray(r["out"]).reshape(NB, SEQ, D) for r in res.results], 0)
    return out.reshape(NCORES * NB, SEQ, D).astype(np.float32)
```

```python
import numpy as np
import concourse.bass as bass
import concourse.mybir as mybir
from concourse.bass_utils import run_bass_kernel_spmd

F32 = mybir.dt.float32
BF16 = mybir.dt.bfloat16
I32 = mybir.dt.int32
ALU = mybir.AluOpType
AF = mybir.ActivationFunctionType
AX = mybir.AxisListType

NCORES = 8
SEQ = 2048
D = 1024
NB = 4
NTOK = NB * SEQ
NT = NTOK // 128
TPS = SEQ // 128
DIN = 6872
C_Q, C_KV, C_IQ, C_IK, C_IW, C_Z, C_XBC, C_DT, C_GA, C_GB = 0, 1024, 1152, 1664, 1728, 1736, 2760, 4808, 4824, 5848
NE = 32
NBLK = NTOK * 4 // 512 + NE
ALPHA = 2.0 ** 0.25
EPS = 1e-5
NEG = -30000.0

B_STAGGER = 104
ENGS = ("pe", "act", "dve", "pool", "sp")


class Res:
    __slots__ = ("name", "writers", "readers", "dsem", "dcount", "dram")

    def __init__(self, name):
        self.name = name
        self.dram = False
        self.writers = {}
        self.readers = {}
        self.dsem = None
        self.dcount = 0


class Sched:
    def __init__(self, nc):
        self.nc = nc
        self.eng = {"pe": nc.tensor, "act": nc.scalar, "dve": nc.vector,
                    "pool": nc.gpsimd, "sp": nc.sync}
        self.sem = {e: nc.alloc_semaphore("prog_" + e) for e in ENGS}
        self.cnt = {e: 0 for e in ENGS}
        self.waited = {e: {} for e in ENGS}
        self.all_res = []
        self.nwaits = 0
        self.nops = 0
        self.sempool = []

    def retire(self, rs):
        for r in rs:
            if r.dsem is not None:
                self.sempool.append((r.dsem, r.dcount))
                r.dsem = None
            if r in self.all_res:
                self.all_res.remove(r)

    def res(self, name):
        r = Res(name)
        self.all_res.append(r)
        return r

    def _need(self, eng, tok, deps):
        sem, val = tok
        k = sem.num
        if self.waited[eng].get(k, 0) >= val:
            return
        if k not in deps or deps[k][1] < val:
            deps[k] = (sem, val)

    def op(self, eng, fn, reads=(), writes=(), awrites=(), dma=False):
        deps = {}
        mykey = None if dma else eng
        for r in reads:
            for k, tok in r.writers.items():
                if k == mykey and eng == "pe":
                    continue
                self._need(eng, tok, deps)
        for r in writes:
            for k, tok in list(r.writers.items()) + list(r.readers.items()):
                if k == mykey:
                    continue
                self._need(eng, tok, deps)
        for r in awrites:
            for k, tok in r.readers.items():
                if k == mykey:
                    continue
                self._need(eng, tok, deps)
        e = self.eng[eng]
        for k, (sem, val) in deps.items():
            e.wait_ge(sem, val)
            self.waited[eng][k] = val
            self.nwaits += 1
        ins = fn(e)
        self.nops += 1
        if dma:
            dst = (list(writes) + list(awrites))[0]
            if dst.dram:
                sb = [r for r in reads if not r.dram]
                if sb:
                    dst = sb[0]
            if dst.dsem is None:
                if self.sempool:
                    dst.dsem, dst.dcount = self.sempool.pop()
                else:
                    dst.dsem = self.nc.alloc_semaphore("d_" + dst.name)
            dst.dcount += 16
            ins.then_inc(dst.dsem, 16)
            tok = (dst.dsem, dst.dcount)
            key = "dma%d" % dst.dsem.num
        else:
            self.cnt[eng] += 1
            ins.then_inc(self.sem[eng], 1)
            tok = (self.sem[eng], self.cnt[eng])
            key = eng
        for r in reads:
            r.readers[key] = tok
        for r in writes:
            r.writers = {key: tok}
            r.readers = {}
        for r in awrites:
            r.writers[key] = tok
        return ins

    def dma(self, eng, out, in_, reads=(), writes=(), awrites=(), **kw):
        return self.op(eng, lambda e: e.dma_start(out=out, in_=in_, **kw),
                       reads=reads, writes=writes, awrites=awrites, dma=True)

    def barrier(self):
        toks = {}
        for e in ENGS:
            if self.cnt[e]:
                toks[self.sem[e].num] = (self.sem[e], self.cnt[e])
        for r in self.all_res:
            if r.dsem is not None and r.dcount:
                toks[r.dsem.num] = (r.dsem, r.dcount)
        for e in ENGS:
            for k, (sem, val) in toks.items():
                if self.waited[e].get(k, 0) >= val:
                    continue
                self.eng[e].wait_ge(sem, val)
                self.waited[e][k] = val
                self.nwaits += 1
        for r in self.all_res:
            r.writers = {}
            r.readers = {}


class Ctx:
    def __init__(self, nc, S):
        self.nc = nc
        self.S = S
        self.stack = []

    def push(self):
        self.stack.append([])

    def pop(self):
        self.S.barrier()
        gs = self.stack.pop()
        self.S.retire([r for (_, r) in gs])
        for g, _ in reversed(gs):
            g.__exit__(None, None, None)

    def sb(self, name, shape, dt):
        g = self.nc.sbuf_tensor("s_" + name, list(shape), dt)
        t = g.__enter__()
        r = self.S.res(name)
        self.stack[-1].append((g, r))
        return t, r

    def ps(self, name, shape, dt=F32):
        g = self.nc.psum_tensor("p_" + name, list(shape), dt)
        t = g.__enter__()
        r = self.S.res(name)
        self.stack[-1].append((g, r))
        return t, r

    def ring(self, kind, name, n, shape, dt):
        f = self.sb if kind == "sb" else self.ps
        return [f("%s%d" % (name, i), shape, dt) for i in range(n)]


def interleave(gens, stagger):
    active = []
    it = iter(gens)
    nxt = next(it, None)
    tick = 0
    while active or nxt is not None:
        if nxt is not None and tick % stagger == 0:
            active.append(nxt)
            nxt = next(it, None)
        for g in list(active):
            try:
                next(g)
            except StopIteration:
                active.remove(g)
        tick += 1


def build_program(debug=()):
    nc = bass.Bass("TRN2", target_bir_lowering=False)
    S = Sched(nc)
    C = Ctx(nc, S)
    dbg = set(debug)

    def din(name, shape, dt=F32):
        return nc.dram_tensor(name, list(shape), dt, kind="ExternalInput").ap()

    def scratch(name, shape, dt):
        kind = "ExternalOutput" if name in dbg else "Internal"
        r = S.res(name)
        r.dram = True
        return nc.dram_tensor(name, list(shape), dt, kind=kind).ap(), r

    x_d = din("x", [NTOK, D])
    cT_d = din("cT", [128, 8, NB])
    wmod_d = din("w_mod", [D, 6 * D])
    bmod_d = din("b_mod", [1, 6 * D])
    win_d = din("w_in", [D, DIN])
    ident_d = din("ident", [128, 128])
    kvw_d = din("kv_norm_w", [1, 128])
    ikw_d = din("idx_k_norm_w", [1, 64])
    ikb_d = din("idx_k_norm_b", [1, 64])
    dtb_d = din("dt_bias", [1, 16])
    convw_d = din("convw", [128, 16, 4])
    convb_d = din("convb", [128, 16])
    alog_d = din("a_log", [1, 16])
    dskip_d = din("d_skip", [1, 16])
    normw_d = din("ssm_norm_w", [1, D])
    triU_d = din("triU", [128, 128])
    SL_d = din("SL", [128, 128])
    negm4_d = din("negm4", [128, 512])
    tz_d = din("tz", [128, 2, 8, 128])
    cfar_d = din("cfar", [1, 8])
    wpa_d = din("w_proj_a", [D, D]); wpb_d = din("w_proj_b", [D, D]); wout_d = din("w_out", [D, D])
    ln1g_d = din("ln1_g", [1, D]); ln1b_d = din("ln1_b", [1, D]); ln2g_d = din("ln2_g", [1, D]); ln2b_d = din("ln2_b", [1, D])
    wr_d = din("w_router", [D, NE]); br_d = din("b_router", [1, NE])
    w1_d = din("w1", [NE * D, 2 * D]); w2_d = din("w2", [NE * D, D])
    b1r_d = din("b1r", [NE * 128, 16]); b2_d = din("b2", [NE, D])
    sut_d = din("sut", [128, 128]); thr16_d = din("thr16", [1, NE * 16]); bstart_d = din("bstart", [1, NBLK])
    kp_d = din("kp", [128, 8]); pcol_d = din("pcol", [128, 1]); sut32_d = din("sut32", [NE, NE])
    r_in = S.res("inputs")
    r_in.dram = True
    out_d = nc.dram_tensor("out", [NTOK, D], F32, kind="ExternalOutput").ap()
    r_out = S.res("out")
    r_out.dram = True

    mod_d, r_mod = scratch("mod_s", [NB, 6 * D], F32)
    qT_d, r_qT = scratch("qT_s", [8, 128, NTOK], BF16)
    iqT_d, r_iqT = scratch("iqT_s", [4, 128, NTOK], BF16)
    xbcT_d, r_xbcT = scratch("xbcT_s", [16, 128, NTOK], BF16)
    sgT_d, r_sgT = scratch("sgT_s", [16, 128, NTOK], BF16)
    kva_d, r_kva = scratch("kva_s", [NTOK, 136], BF16)
    kvT_d, r_kvT = scratch("kvT_s", [128, NTOK], BF16)
    ikT_d, r_ikT = scratch("ikT_s", [128, NTOK], BF16)
    iw_d, r_iw = scratch("iw_s", [NTOK, 8], F32)
    dt_d, r_dt = scratch("dt_s", [NTOK, 16], F32)
    zs_d, r_zs = scratch("zs_s", [NTOK, D], BF16)
    obT_d, r_obT = scratch("obT_s", [8, 128, NTOK], BF16)
    oaT_d, r_oaT = scratch("oaT_s", [8, 128, NTOK], BF16)
    mT_d, r_mTd = scratch("mT_s", [8, 128, NTOK], BF16)
    x1_d, r_x1 = scratch("x1_s", [NTOK, D], F32)
    u2_d, r_u2 = scratch("u2_s", [NTOK, D], BF16)
    xs_d, r_xsd = scratch("xsort_s", [NBLK * 512, D], BF16)
    ys_d, r_ysd = scratch("ysort_s", [NBLK * 512, D], BF16)

    w1b_d, r_w1bd = scratch("w1b_s", [NE * D, 2 * D], BF16)
    w2b_d, r_w2bd = scratch("w2b_s", [NE * D, D], BF16)

    C.push()
    ident_f, r_identf = C.sb("ident_f", [128, 128], F32)
    ident_b, r_identb = C.sb("ident_b", [128, 128], BF16)
    S.dma("sp", ident_f[:], ident_d, reads=[r_in], writes=[r_identf])
    S.op("dve", lambda e: e.tensor_copy(out=ident_b[:], in_=ident_f[:]), reads=[r_identf], writes=[r_identb])
    modT, r_modT = C.sb("modT", [128, 48, NB], F32)

    C.push()
    cT, r_cT = C.sb("cT", [128, 8, NB], F32)
    ones1, r_ones1 = C.sb("ones1", [1, NB], F32)
    bmod, r_bmod = C.sb("bmod", [1, 6 * D], F32)
    modrow, r_modrow = C.sb("modrow", [NB, 6 * D], F32)
    wm = C.ring("sb", "wm", 2, [128, 8, 512], F32)
    pmod = C.ring("ps", "pmod", 2, [NB, 512], F32)
    S.dma("sp", cT[:], cT_d, reads=[r_in], writes=[r_cT])
    S.dma("sp", bmod[:], bmod_d, reads=[r_in], writes=[r_bmod])
    S.op("act", lambda e: e.activation(out=cT[:], in_=cT[:], func=AF.Silu), reads=[r_cT], writes=[r_cT])
    S.op("dve", lambda e: e.memset(ones1[:], 1.0), writes=[r_ones1])
    for g in range(12):
        wt, r_wt = wm[g % 2]
        pt, r_pt = pmod[g % 2]
        S.dma("sp", wt[:], wmod_d[:, g * 512:(g + 1) * 512].rearrange("(k p) n -> p k n", p=128), reads=[r_in], writes=[r_wt])
        for k in range(8):
            S.op("pe", lambda e: e.matmul(pt[:], lhsT=cT[:, k, :], rhs=wt[:, k, :], start=(k == 0), stop=False),
                 reads=[r_cT, r_wt], writes=[r_pt] if k == 0 else (), awrites=() if k == 0 else [r_pt])
        S.op("pe", lambda e: e.matmul(pt[:], lhsT=ones1[:], rhs=bmod[:, g * 512:(g + 1) * 512], start=False, stop=True),
             reads=[r_ones1, r_bmod], awrites=[r_pt])
        S.op("act", lambda e: e.activation(out=modrow[:, g * 512:(g + 1) * 512], in_=pt[:], func=AF.Copy), reads=[r_pt], awrites=[r_modrow])
    S.dma("sp", mod_d, modrow[:], reads=[r_modrow], writes=[r_mod])
    pmt, r_pmt = pmod[0]
    pmt2, r_pmt2 = C.ps("pmodT", [128, 48 * NB], F32)
    for j in range(48):
        S.op("pe", lambda e: e.transpose(out=pmt2[:, j * NB:(j + 1) * NB], in_=modrow[:, j * 128:(j + 1) * 128], identity=ident_f[0:NB, 0:NB]),
             reads=[r_modrow, r_identf], writes=[r_pmt2] if j == 0 else (), awrites=() if j == 0 else [r_pmt2])
    S.op("act", lambda e: e.activation(out=modT[:].rearrange("p j b -> p (j b)"), in_=pmt2[:], func=AF.Copy), reads=[r_pmt2], writes=[r_modT])
    S.op("dve", lambda e: e.tensor_scalar_add(out=modT[:, 8:16, :], in0=modT[:, 8:16, :], scalar1=1.0), reads=[r_modT], awrites=[r_modT])
    S.op("dve", lambda e: e.tensor_scalar_add(out=modT[:, 32:40, :], in0=modT[:, 32:40, :], scalar1=1.0), reads=[r_modT], awrites=[r_modT])
    C.pop()
    if "stop0" in dbg:
        C.pop()
        return nc

    C.push()
    wI, r_wI = C.sb("wI", [128, 8, DIN], BF16)
    for i, (a, b_) in enumerate([(0, 1024), (1024, 1736), (1736, 2760), (2760, 3784), (3784, 4808), (4808, 5848), (5848, 6872)]):
        S.dma("pool", wI[:, :, a:b_], win_d[:, a:b_].rearrange("(k p) n -> p k n", p=128), reads=[r_in],
              writes=[r_wI] if i == 0 else (), awrites=() if i == 0 else [r_wI])
    kvw_bc, r_kvw = C.sb("kvw_bc", [128, 128], F32)
    ikw_bc, r_ikw = C.sb("ikw_bc", [128, 64], F32)
    ikb_bc, r_ikb = C.sb("ikb_bc", [128, 64], F32)
    dtb_bc, r_dtb = C.sb("dtb_bc", [128, 16], F32)
    S.dma("sp", kvw_bc[:], kvw_d.partition_broadcast(128), reads=[r_in], writes=[r_kvw])
    S.dma("sp", ikw_bc[:], ikw_d.partition_broadcast(128), reads=[r_in], writes=[r_ikw])
    S.dma("sp", ikb_bc[:], ikb_d.partition_broadcast(128), reads=[r_in], writes=[r_ikb])
    S.dma("sp", dtb_bc[:], dtb_d.partition_broadcast(128), reads=[r_in], writes=[r_dtb])

    xt = C.ring("sb", "xt", 2, [128, D], F32)
    xn = C.ring("sb", "xn", 2, [128, D], BF16)
    st = C.ring("sb", "st", 2, [128, 2, 6], F32)
    mv = C.ring("sb", "mv", 2, [128, 2], F32)
    rs = C.ring("sb", "rs", 2, [128, 1], F32)
    uT = C.ring("sb", "uT", 2, [128, 8, 512], BF16)
    ptr = C.ring("ps", "ptr", 1, [128, 8, 128], BF16)
    psm = C.ring("ps", "psm", 1, [128, 512], F32)
    pz = C.ring("ps", "pz", 2, [128, 512], F32)
    pf = C.ring("ps", "pf", 3, [128, 512], F32)
    pt2 = C.ring("ps", "pt2", 1, [128, 2, 128], BF16)
    kva = C.ring("sb", "kva", 2, [128, 136], BF16)
    ikn = C.ring("sb", "ikn", 2, [128, 128], BF16)
    sml = C.ring("sb", "sml", 2, [128, 64], F32)
    ikf = C.ring("sb", "ikf", 2, [128, 64], F32)
    iwt = C.ring("sb", "iwt", 2, [128, 8], F32)
    dtt = C.ring("sb", "dtt", 2, [128, 4, 16], F32)
    zst = C.ring("sb", "zst", 2, [128, D], BF16)
    tT = C.ring("sb", "tT", 2, [128, 2, 128], BF16)
    stg = C.ring("sb", "stg", 2, [128, 8, 512], BF16)
    for i in range(2):
        S.op("pool", lambda e: e.memset(kva[i][0][:, 128:136], 1.0), writes=[kva[i][1]])

    def ln_stats(src, r_src, i):
        st_t, r_st = st[i]
        mv_t, r_mv = mv[i]
        rs_t, r_rs = rs[i]
        for j in range(2):
            S.op("dve", lambda e: e.bn_stats(out=st_t[:, j, :], in_=src[:, j * 512:(j + 1) * 512]), reads=[r_src],
                 writes=[r_st] if j == 0 else (), awrites=() if j == 0 else [r_st])
        S.op("dve", lambda e: e.bn_aggr(out=mv_t[:], in_=st_t[:].rearrange("p a b -> p (a b)")), reads=[r_st], writes=[r_mv])
        S.op("act", lambda e: e.activation(out=rs_t[:], in_=mv_t[:, 1:2], func=AF.Sqrt, bias=EPS), reads=[r_mv], writes=[r_rs])
        S.op("dve", lambda e: e.reciprocal(out=rs_t[:], in_=rs_t[:]), reads=[r_rs], writes=[r_rs])
        return mv_t, r_mv, rs_t, r_rs

    tcount = 0
    for g in range(NTOK // 512):
        b = (g * 512) // SEQ
        u_t, r_u = uT[g % 2]
        for i4 in range(4):
            t = g * 4 + i4
            tok0 = t * 128
            ri = tcount % 2
            tcount += 1
            x_t, r_x = xt[ri]
            xn_t, r_xn = xn[ri]
            if t == 0:
                S.dma("sp", x_t[:], x_d[0:128, :], reads=[r_in], writes=[r_x])
            if t + 1 < NT:
                S.dma("sp", xt[(ri + 1) % 2][0][:], x_d[tok0 + 128:tok0 + 256, :], reads=[r_in], writes=[xt[(ri + 1) % 2][1]])
            mv_t, r_mv, rs_t, r_rs = ln_stats(x_t, r_x, ri)
            S.op("dve", lambda e: e.tensor_scalar(out=xn_t[:], in0=x_t[:], scalar1=mv_t[:, 0:1], scalar2=rs_t[:], op0=ALU.subtract, op1=ALU.mult),
                 reads=[r_x, r_mv, r_rs], writes=[r_xn])
            p_t, r_p = ptr[0]
            for k in range(8):
                S.op("pe", lambda e: e.transpose(out=p_t[:, k, :], in_=xn_t[:, k * 128:(k + 1) * 128], identity=ident_b[:]),
                     reads=[r_xn, r_identb], writes=[r_p] if k == 0 else (), awrites=() if k == 0 else [r_p])
            for k in range(8):
                S.op("act", lambda e: e.activation(out=u_t[:, k, i4 * 128:(i4 + 1) * 128], in_=p_t[:, k, :], func=AF.Identity,
                                                   scale=modT[:, 8 + k, b:b + 1], bias=modT[:, k, b:b + 1]),
                     reads=[r_p, r_modT], writes=[r_u] if (k == 0 and i4 == 0) else (), awrites=() if (k == 0 and i4 == 0) else [r_u])
            ps_t, r_ps = psm[0]
            for (c0, c1, o0) in [(C_KV, C_KV + 128, 0), (C_IK, C_IK + 72, 128), (C_DT, C_DT + 16, 200)]:
                for k in range(8):
                    S.op("pe", lambda e: e.matmul(ps_t[:, o0:o0 + (c1 - c0)], lhsT=u_t[:, k, i4 * 128:(i4 + 1) * 128], rhs=wI[:, k, c0:c1], start=(k == 0), stop=(k == 7)),
                         reads=[r_u, r_wI], writes=[r_ps] if (k == 0 and o0 == 0) else (), awrites=() if (k == 0 and o0 == 0) else [r_ps])
            zp = []
            for h in range(2):
                pz_t, r_pz = pz[h]
                zp.append((pz_t, r_pz))
                for k in range(8):
                    S.op("pe", lambda e: e.matmul(pz_t[:], lhsT=u_t[:, k, i4 * 128:(i4 + 1) * 128], rhs=wI[:, k, C_Z + h * 512:C_Z + (h + 1) * 512], start=(k == 0), stop=(k == 7)),
                         reads=[r_u, r_wI], writes=[r_pz] if k == 0 else (), awrites=() if k == 0 else [r_pz])
            sm_t, r_sm = sml[ri]
            kva_t, r_kva_t = kva[ri]
            ikf_t, r_ikf = ikf[ri]
            S.op("act", lambda e: e.activation(out=ikf_t[:, 0:64], in_=ps_t[:, 0:64], func=AF.Square, accum_out=sm_t[:, 0:1]), reads=[r_ps], writes=[r_ikf, r_sm])
            S.op("act", lambda e: e.activation(out=ikf_t[:, 0:64], in_=ps_t[:, 64:128], func=AF.Square, accum_out=sm_t[:, 1:2]), reads=[r_ps], writes=[r_ikf], awrites=[r_sm])
            S.op("dve", lambda e: e.tensor_tensor(out=sm_t[:, 0:1], in0=sm_t[:, 0:1], in1=sm_t[:, 1:2], op=ALU.add), reads=[r_sm], awrites=[r_sm])
            S.op("act", lambda e: e.activation(out=sm_t[:, 2:3], in_=sm_t[:, 0:1], func=AF.Sqrt, scale=1.0 / 128.0, bias=EPS), reads=[r_sm], awrites=[r_sm])
            S.op("dve", lambda e: e.reciprocal(out=sm_t[:, 3:4], in_=sm_t[:, 2:3]), reads=[r_sm], awrites=[r_sm])
            S.op("dve", lambda e: e.scalar_tensor_tensor(out=kva_t[:, 0:128], in0=ps_t[:, 0:128], scalar=sm_t[:, 3:4], in1=kvw_bc[:], op0=ALU.mult, op1=ALU.mult),
                 reads=[r_ps, r_sm, r_kvw], awrites=[r_kva_t])
            S.dma("sp", kva_d[tok0:tok0 + 128, :], kva_t[:], reads=[r_kva_t], awrites=[r_kva])
            ik_t, r_ik = ikn[ri]
            S.op("dve", lambda e: e.bn_stats(out=sm_t[:, 8:14], in_=ps_t[:, 128:192]), reads=[r_ps], awrites=[r_sm])
            S.op("dve", lambda e: e.bn_aggr(out=sm_t[:, 16:18], in_=sm_t[:, 8:14]), reads=[r_sm], awrites=[r_sm])
            S.op("act", lambda e: e.activation(out=sm_t[:, 18:19], in_=sm_t[:, 17:18], func=AF.Sqrt, bias=EPS), reads=[r_sm], awrites=[r_sm])
            S.op("dve", lambda e: e.reciprocal(out=sm_t[:, 19:20], in_=sm_t[:, 18:19]), reads=[r_sm], awrites=[r_sm])
            S.op("dve", lambda e: e.tensor_scalar(out=ikf_t[:], in0=ps_t[:, 128:192], scalar1=sm_t[:, 16:17], scalar2=sm_t[:, 19:20], op0=ALU.subtract, op1=ALU.mult),
                 reads=[r_ps, r_sm], writes=[r_ikf])
            S.op("dve", lambda e: e.tensor_tensor(out=ikf_t[:], in0=ikf_t[:], in1=ikw_bc[:], op=ALU.mult), reads=[r_ikf, r_ikw], writes=[r_ikf])
            S.op("dve", lambda e: e.tensor_tensor(out=ik_t[:, 0:64], in0=ikf_t[:], in1=ikb_bc[:], op=ALU.add), reads=[r_ikf, r_ikb], writes=[r_ik])
            S.op("dve", lambda e: e.tensor_copy(out=ik_t[:, 64:128], in_=ik_t[:, 0:64]), reads=[r_ik], awrites=[r_ik])
            iw_t, r_iwt = iwt[ri]
            S.op("act", lambda e: e.mul(out=iw_t[:], in_=ps_t[:, 192:200], mul=float(8 ** -0.5 * 64 ** -0.5)), reads=[r_ps], writes=[r_iwt])
            S.dma("sp", iw_d[tok0:tok0 + 128, :], iw_t[:], reads=[r_iwt], awrites=[r_iw])
            d_t, r_d = dtt[ri]
            S.op("dve", lambda e: e.tensor_tensor(out=d_t[:, 0, :], in0=ps_t[:, 200:216], in1=dtb_bc[:], op=ALU.add), reads=[r_ps, r_dtb], writes=[r_d])
            S.op("act", lambda e: e.activation(out=d_t[:, 1, :], in_=d_t[:, 0, :], func=AF.Abs), reads=[r_d], awrites=[r_d])
            S.op("act", lambda e: e.activation(out=d_t[:, 1, :], in_=d_t[:, 1, :], func=AF.Exp, scale=-1.0), reads=[r_d], awrites=[r_d])
            S.op("act", lambda e: e.activation(out=d_t[:, 1, :], in_=d_t[:, 1, :], func=AF.Ln, bias=1.0), reads=[r_d], awrites=[r_d])
            S.op("dve", lambda e: e.scalar_tensor_tensor(out=d_t[:, 2, :], in0=d_t[:, 0, :], scalar=0.0, in1=d_t[:, 1, :], op0=ALU.max, op1=ALU.add), reads=[r_d], awrites=[r_d])
            S.dma("sp", dt_d[tok0:tok0 + 128, :], d_t[:, 2, :], reads=[r_d], awrites=[r_dt])
            z_t, r_z = zst[ri]
            for h in range(2):
                S.op("act", lambda e: e.activation(out=z_t[:, h * 512:(h + 1) * 512], in_=zp[h][0][:], func=AF.Silu), reads=[zp[h][1]],
                     writes=[r_z] if h == 0 else (), awrites=() if h == 0 else [r_z])
            S.dma("sp", zs_d[tok0:tok0 + 128, :], z_t[:], reads=[r_z], awrites=[r_zs])
            p2, r_p2 = pt2[0]
            t_t, r_t = tT[ri]
            S.op("pe", lambda e: e.transpose(out=p2[:, 0, :], in_=kva_t[:, 0:128], identity=ident_b[:]), reads=[r_kva_t, r_identb], writes=[r_p2])
            S.op("pe", lambda e: e.transpose(out=p2[:, 1, :], in_=ik_t[:], identity=ident_b[:]), reads=[r_ik, r_identb], awrites=[r_p2])
            S.op("dve", lambda e: e.tensor_copy(out=t_t[:], in_=p2[:]), reads=[r_p2], writes=[r_t])
            S.dma("sp", kvT_d[:, tok0:tok0 + 128], t_t[:, 0, :], reads=[r_t], awrites=[r_kvT])
            S.dma("sp", ikT_d[:, tok0:tok0 + 128], t_t[:, 1, :], reads=[r_t], awrites=[r_ikT])
        g0 = g * 512
        fcount = 0
        for (c0, nch, dst, r_dst, ch0, func, scl) in [
                (C_Q, 8, qT_d, r_qT, 0, AF.Copy, float(128 ** -0.5)),
                (C_IQ, 4, iqT_d, r_iqT, 0, AF.Copy, 1.0),
                (C_XBC, 8, xbcT_d, r_xbcT, 0, AF.Copy, 1.0),
                (C_XBC + 1024, 8, xbcT_d, r_xbcT, 8, AF.Copy, 1.0),
                (C_GA, 8, sgT_d, r_sgT, 0, AF.Sigmoid, 1.0),
                (C_GB, 8, sgT_d, r_sgT, 8, AF.Sigmoid, 1.0)]:
            sg_t, r_sg = stg[fcount % 2]
            fcount += 1
            for j in range(nch):
                pf_t, r_pf = pf[j % 3]
                for k in range(8):
                    S.op("pe", lambda e: e.matmul(pf_t[:], lhsT=wI[:, k, c0 + j * 128:c0 + (j + 1) * 128], rhs=u_t[:, k, :], start=(k == 0), stop=(k == 7)),
                         reads=[r_u, r_wI], writes=[r_pf] if k == 0 else (), awrites=() if k == 0 else [r_pf])
                if func == AF.Copy and j % 2 == 1:
                    S.op("dve", lambda e: e.tensor_scalar_mul(out=sg_t[:, j, :], in0=pf_t[:], scalar1=scl), reads=[r_pf],
                         writes=[r_sg] if j == 0 else (), awrites=() if j == 0 else [r_sg])
                else:
                    S.op("act", lambda e: e.activation(out=sg_t[:, j, :], in_=pf_t[:], func=func, scale=scl), reads=[r_pf],
                         writes=[r_sg] if j == 0 else (), awrites=() if j == 0 else [r_sg])
            S.dma("sp", dst[ch0:ch0 + nch, :, g0:g0 + 512].rearrange("c p t -> p c t"), sg_t[:, 0:nch, :], reads=[r_sg], awrites=[r_dst])
    C.pop()
    if "stopA" in dbg:
        C.pop()
        return nc

    phase_B(nc, S, C, dbg, locals())
    if "stopB" in dbg:
        C.pop()
        return nc

    phase_C(nc, S, C, dbg, locals())
    if "stopC" in dbg:
        C.pop()
        return nc

    phase_DEF(nc, S, C, dbg, locals())
    C.pop()
    return nc


def phase_DEF(nc, S, C, dbg, L):
    g_ = lambda n: L[n]
    r_in = g_("r_in"); ident_b = g_("ident_b"); r_identb = g_("r_identb"); ident_f = g_("ident_f"); r_identf = g_("r_identf")
    modT = g_("modT"); r_modT = g_("r_modT"); mod_d = g_("mod_d"); r_mod = g_("r_mod")
    x_d = g_("x_d"); oaT_d, r_oaT = g_("oaT_d"), g_("r_oaT"); obT_d, r_obT = g_("obT_d"), g_("r_obT"); sgT_d, r_sgT = g_("sgT_d"), g_("r_sgT")
    x1_d, r_x1 = g_("x1_d"), g_("r_x1"); u2_d, r_u2 = g_("u2_d"), g_("r_u2"); xs_d, r_xsd = g_("xs_d"), g_("r_xsd"); ys_d, r_ysd = g_("ys_d"), g_("r_ysd")
    out_d, r_out = g_("out_d"), g_("r_out")
    b1r_d, b2_d = g_("b1r_d"), g_("b2_d")
    w1b_d, r_w1bd, w2b_d, r_w2bd = g_("w1b_d"), g_("r_w1bd"), g_("w2b_d"), g_("r_w2bd")

    C.push()
    idx4, r_idx4 = C.sb("idx4", [128, NT, 4], I32)
    g4, r_g4 = C.sb("g4", [128, NT, 4], F32)
    widx, r_widx = C.sb("widx", [128, NBLK, 8], I32)
    bidx, r_bidx = C.sb("bidx", [128, NBLK], I32)
    eidx, r_eidx = C.sb("eidx", [128, NBLK], I32)

    def ln_stats(src, r_src, st_t, r_st, mv_t, r_mv, rs_t, r_rs):
        for j in range(2):
            S.op("dve", lambda e: e.bn_stats(out=st_t[:, j, :], in_=src[:, j * 512:(j + 1) * 512]), reads=[r_src],
                 writes=[r_st] if j == 0 else (), awrites=() if j == 0 else [r_st])
        S.op("dve", lambda e: e.bn_aggr(out=mv_t[:], in_=st_t[:].rearrange("p a b -> p (a b)")), reads=[r_st], writes=[r_mv])
        S.op("act", lambda e: e.activation(out=rs_t[:], in_=mv_t[:, 1:2], func=AF.Sqrt, bias=EPS), reads=[r_mv], writes=[r_rs])
        S.op("dve", lambda e: e.reciprocal(out=rs_t[:], in_=rs_t[:]), reads=[r_rs], writes=[r_rs])

    mT_d, r_mTd = g_("mT_d"), g_("r_mTd")
    C.push()
    wpa, r_wpa = C.sb("wpa", [128, 8, D], BF16)
    wpb, r_wpb = C.sb("wpb", [128, 8, D], BF16)
    S.dma("pool", wpa[:], g_("wpa_d").rearrange("(k p) n -> p k n", p=128), reads=[r_in], writes=[r_wpa])
    S.dma("pool", wpb[:], g_("wpb_d").rearrange("(k p) n -> p k n", p=128), reads=[r_in], writes=[r_wpb])
    oaTr = C.ring("sb", "oaTd", 2, [128, 8, 512], BF16)
    obTr = C.ring("sb", "obTd", 2, [128, 8, 512], BF16)
    sgTr = C.ring("sb", "sgTd", 2, [128, 16, 512], BF16)
    mTr = C.ring("sb", "mT", 2, [128, 8, 512], BF16)
    ta = C.ring("sb", "ta", 3, [128, 512], F32)
    tb = C.ring("sb", "tb", 3, [128, 512], F32)
    pab = C.ring("ps", "pab", 4, [128, 512], F32)
    pbb = C.ring("ps", "pbb", 4, [128, 512], F32)

    def loadD1(g):
        g0 = g * 512
        S.dma("sp", oaTr[g % 2][0][:], oaT_d[:, :, g0:g0 + 512].rearrange("c p t -> p c t"), reads=[r_oaT], writes=[oaTr[g % 2][1]])
        S.dma("sp", obTr[g % 2][0][:], obT_d[:, :, g0:g0 + 512].rearrange("c p t -> p c t"), reads=[r_obT], writes=[obTr[g % 2][1]])
        S.dma("sp", sgTr[g % 2][0][:], sgT_d[:, :, g0:g0 + 512].rearrange("c p t -> p c t"), reads=[r_sgT], writes=[sgTr[g % 2][1]])

    NG = NTOK // 512
    loadD1(0)
    cn = 0
    for g in range(NG):
        g0 = g * 512
        if g + 1 < NG:
            loadD1(g + 1)
        oaT, r_oaTs = oaTr[g % 2]; obT, r_obTs = obTr[g % 2]; sgT, r_sgTs = sgTr[g % 2]; mT, r_mT = mTr[g % 2]
        for n in range(8):
            pa, r_pa = pab[cn % 4]
            pb, r_pb = pbb[cn % 4]
            ta_t, r_ta = ta[cn % 3]
            tb_t, r_tb = tb[cn % 3]
            cn += 1
            for k in range(8):
                S.op("pe", lambda e: e.matmul(pa[:], lhsT=wpa[:, k, n * 128:(n + 1) * 128], rhs=oaT[:, k, :], start=(k == 0), stop=(k == 7)),
                     reads=[r_wpa, r_oaTs], writes=[r_pa] if k == 0 else (), awrites=() if k == 0 else [r_pa])
            for k in range(8):
                S.op("pe", lambda e: e.matmul(pb[:], lhsT=wpb[:, k, n * 128:(n + 1) * 128], rhs=obT[:, k, :], start=(k == 0), stop=(k == 7)),
                     reads=[r_wpb, r_obTs], writes=[r_pb] if k == 0 else (), awrites=() if k == 0 else [r_pb])
            S.op("dve", lambda e: e.tensor_tensor(out=ta_t[:], in0=pa[:], in1=sgT[:, n, :], op=ALU.mult), reads=[r_pa, r_sgTs], writes=[r_ta])
            S.op("dve", lambda e: e.tensor_tensor(out=tb_t[:], in0=pb[:], in1=sgT[:, 8 + n, :], op=ALU.mult), reads=[r_pb, r_sgTs], writes=[r_tb])
            S.op("pool", lambda e: e.tensor_tensor(out=mT[:, n, :], in0=ta_t[:], in1=tb_t[:], op=ALU.add), reads=[r_ta, r_tb],
                 writes=[r_mT] if n == 0 else (), awrites=() if n == 0 else [r_mT])
        S.dma("sp", mT_d[:, :, g0:g0 + 512].rearrange("c p t -> p c t"), mT[:], reads=[r_mT], awrites=[r_mTd])
    C.pop()

    C.push()
    wout, r_wout = C.sb("wout", [128, 8, D], BF16)
    S.dma("pool", wout[:], g_("wout_d").rearrange("(k p) n -> p k n", p=128), reads=[r_in], writes=[r_wout])
    wr, r_wr = C.sb("wr", [128, 8, NE], F32)
    S.dma("sp", wr[:], g_("wr_d").rearrange("(k p) n -> p k n", p=128), reads=[r_in], writes=[r_wr])
    br_bc, r_br = C.sb("br_bc", [128, NE], F32)
    S.dma("sp", br_bc[:], g_("br_d").partition_broadcast(128), reads=[r_in], writes=[r_br])
    ln1g, r_ln1g = C.sb("ln1g", [128, D], F32)
    ln1b, r_ln1b = C.sb("ln1b", [128, D], F32)
    S.dma("sp", ln1g[:], g_("ln1g_d").partition_broadcast(128), reads=[r_in], writes=[r_ln1g])
    S.dma("sp", ln1b[:], g_("ln1b_d").partition_broadcast(128), reads=[r_in], writes=[r_ln1b])
    gater = C.ring("sb", "gate1", 2, [128, D], F32)
    sc2r = C.ring("sb", "sc2", 2, [128, D], F32)
    sh2r = C.ring("sb", "sh2", 2, [128, D], F32)
    sutf, r_sutf = C.sb("sutf", [128, 128], F32)
    sutb, r_sutb = C.sb("sutb", [128, 128], BF16)
    onesb, r_onesb = C.sb("onesb", [128, 128], BF16)
    S.dma("sp", sutf[:], g_("sut_d"), reads=[r_in], writes=[r_sutf])
    S.op("dve", lambda e: e.tensor_copy(out=sutb[:], in_=sutf[:]), reads=[r_sutf], writes=[r_sutb])
    S.op("dve", lambda e: e.memset(onesb[:], 1.0), writes=[r_onesb])
    maskall, r_maskall = C.sb("maskall", [128, NT, NE], F32)
    gall, r_gall = C.sb("gall", [128, NT, NE], F32)
    posall, r_posall = C.sb("posall", [128, NT, NE], F32)
    rrun, r_rrun = C.sb("rrun", [128, NE], F32)
    S.op("dve", lambda e: e.memset(rrun[:], 0.0), writes=[r_rrun])
    mtr = C.ring("sb", "mtl", 4, [128, 8, 128], BF16)
    xt = C.ring("sb", "xtd", 4, [128, D], F32)
    r1_r = C.ring("sb", "r1", 3, [128, D], F32)
    x1t_r = C.ring("sb", "x1t", 3, [128, D], F32)
    u2t_r = C.ring("sb", "u2t", 3, [128, D], F32)
    u2b = C.ring("sb", "u2b", 3, [128, D], BF16)
    u2T_r = C.ring("sb", "u2T", 3, [128, 8, 128], F32)
    st_r = C.ring("sb", "stD", 6, [128, 2, 6], F32)
    mv_r = C.ring("sb", "mvD", 6, [128, 2], F32)
    rs_r = C.ring("sb", "rsD", 6, [128, 1], F32)
    lg_r = C.ring("sb", "lg", 3, [128, NE], F32)
    m8_r = C.ring("sb", "m8D", 3, [128, 8], F32)
    sml_r = C.ring("sb", "smlD", 3, [128, 8], F32)
    ex_r = C.ring("sb", "exD", 3, [128, NE], F32)
    maskb_r = C.ring("sb", "maskb", 3, [128, NE], BF16)
    prs = C.ring("ps", "prs", 4, [128, 512], F32)
    ptT = C.ring("ps", "ptT", 2, [128, 512], F32)
    psl = C.ring("ps", "psl", 2, [128, 512], F32)

    def loadD2(t):
        tok0 = t * 128
        S.dma("sp", xt[t % 4][0][:], x_d[tok0:tok0 + 128, :], reads=[r_in], writes=[xt[t % 4][1]])
        S.dma("sp", mtr[t % 4][0][:], mT_d[:, :, tok0:tok0 + 128].rearrange("c p t -> p c t"), reads=[r_mTd], writes=[mtr[t % 4][1]])
        if t % TPS == 0:
            b = t // TPS
            gate1, r_gate1 = gater[b % 2]; sc2, r_sc2 = sc2r[b % 2]; sh2, r_sh2 = sh2r[b % 2]
            S.dma("sp", gate1[:], mod_d[b:b + 1, 2 * D:3 * D].partition_broadcast(128), reads=[r_mod], writes=[r_gate1])
            S.dma("sp", sh2[:], mod_d[b:b + 1, 3 * D:4 * D].partition_broadcast(128), reads=[r_mod], writes=[r_sh2])
            S.dma("sp", sc2[:], mod_d[b:b + 1, 4 * D:5 * D].partition_broadcast(128), reads=[r_mod], writes=[r_sc2])
            S.op("pool", lambda e: e.tensor_scalar_add(out=sc2[:], in0=sc2[:], scalar1=1.0), reads=[r_sc2], writes=[r_sc2])

    def stage1(t):
        tok0 = t * 128
        b = t // TPS
        gate1, r_gate1 = gater[b % 2]; sc2, r_sc2 = sc2r[b % 2]; sh2, r_sh2 = sh2r[b % 2]
        x_t, r_x = xt[t % 4]
        mt_t, r_mt = mtr[t % 4]
        r1, r_r1 = r1_r[t % 3]; x1t, r_x1t = x1t_r[t % 3]; u2t, r_u2t = u2t_r[t % 3]
        st, r_st = st_r[(2 * t) % 6]; mv, r_mv = mv_r[(2 * t) % 6]; rs, r_rs = rs_r[(2 * t) % 6]
        st2, r_st2 = st_r[(2 * t + 1) % 6]; mv2, r_mv2 = mv_r[(2 * t + 1) % 6]; rs2, r_rs2 = rs_r[(2 * t + 1) % 6]
        for hf in range(2):
            pr_t, r_pr = prs[(2 * t + hf) % 4]
            for n in range(8):
                S.op("pe", lambda e: e.matmul(pr_t[:], lhsT=mt_t[:, n, :], rhs=wout[:, n, hf * 512:(hf + 1) * 512], start=(n == 0), stop=(n == 7)),
                     reads=[r_mt, r_wout], writes=[r_pr] if n == 0 else (), awrites=() if n == 0 else [r_pr])
                yield
            S.op("dve", lambda e: e.tensor_tensor(out=r1[:, hf * 512:(hf + 1) * 512], in0=pr_t[:], in1=gate1[:, hf * 512:(hf + 1) * 512], op=ALU.mult),
                 reads=[r_pr, r_gate1], writes=[r_r1] if hf == 0 else (), awrites=() if hf == 0 else [r_r1])
            yield
        S.op("dve", lambda e: e.scalar_tensor_tensor(out=r1[:], in0=x_t[:], scalar=float(ALPHA), in1=r1[:], op0=ALU.mult, op1=ALU.add), reads=[r_x, r_r1], writes=[r_r1])
        yield
        ln_stats(r1, r_r1, st, r_st, mv, r_mv, rs, r_rs)
        yield
        S.op("dve", lambda e: e.tensor_scalar(out=x1t[:], in0=r1[:], scalar1=mv[:, 0:1], scalar2=rs[:], op0=ALU.subtract, op1=ALU.mult), reads=[r_r1, r_mv, r_rs], writes=[r_x1t])
        yield
        S.op("pool", lambda e: e.tensor_tensor(out=x1t[:], in0=x1t[:], in1=ln1g[:], op=ALU.mult), reads=[r_x1t, r_ln1g], writes=[r_x1t])
        yield
        S.op("pool", lambda e: e.tensor_tensor(out=x1t[:], in0=x1t[:], in1=ln1b[:], op=ALU.add), reads=[r_x1t, r_ln1b], writes=[r_x1t])
        yield
        S.dma("sp", x1_d[tok0:tok0 + 128, :], x1t[:], reads=[r_x1t], awrites=[r_x1])
        yield
        ln_stats(x1t, r_x1t, st2, r_st2, mv2, r_mv2, rs2, r_rs2)
        yield
        S.op("dve", lambda e: e.tensor_scalar(out=u2t[:], in0=x1t[:], scalar1=mv2[:, 0:1], scalar2=rs2[:], op0=ALU.subtract, op1=ALU.mult), reads=[r_x1t, r_mv2, r_rs2], writes=[r_u2t])
        yield
        S.op("pool", lambda e: e.tensor_tensor(out=u2t[:], in0=u2t[:], in1=sc2[:], op=ALU.mult), reads=[r_u2t, r_sc2], writes=[r_u2t])
        yield
        S.op("pool", lambda e: e.tensor_tensor(out=u2t[:], in0=u2t[:], in1=sh2[:], op=ALU.add), reads=[r_u2t, r_sh2], writes=[r_u2t])
        yield
        ub, r_ub = u2b[t % 3]
        S.op("act", lambda e: e.activation(out=ub[:], in_=u2t[:], func=AF.Copy), reads=[r_u2t], writes=[r_ub])
        yield
        S.dma("sp", u2_d[tok0:tok0 + 128, :], ub[:], reads=[r_ub], awrites=[r_u2])
        yield

    def stage2(t):
        u2t, r_u2t = u2t_r[t % 3]
        u2T, r_u2T = u2T_r[t % 3]
        lg, r_lg = lg_r[t % 3]; m8, r_m8 = m8_r[t % 3]; sml, r_sml = sml_r[t % 3]; ex, r_ex = ex_r[t % 3]; maskb, r_maskb = maskb_r[t % 3]
        for hf in range(2):
            pT, r_pT = (ptT[t % 2] if hf == 0 else psl[t % 2])
            for k4 in range(4):
                k = hf * 4 + k4
                S.op("pe", lambda e: e.transpose(out=pT[:, k4 * 128:(k4 + 1) * 128], in_=u2t[:, k * 128:(k + 1) * 128], identity=ident_f[:]),
                     reads=[r_u2t, r_identf], writes=[r_pT] if k4 == 0 else (), awrites=() if k4 == 0 else [r_pT])
                yield
            S.op("act", lambda e: e.activation(out=u2T[:, hf * 4:hf * 4 + 4, :].rearrange("p k t -> p (k t)"), in_=pT[:], func=AF.Copy), reads=[r_pT],
                 writes=[r_u2T] if hf == 0 else (), awrites=() if hf == 0 else [r_u2T])
            yield
        pl_, r_pl = psl[t % 2]
        for k in range(8):
            S.op("pe", lambda e: e.matmul(pl_[:, 0:NE], lhsT=u2T[:, k, :], rhs=wr[:, k, :], start=(k == 0), stop=(k == 7)),
                 reads=[r_u2T, r_wr], writes=[r_pl] if k == 0 else (), awrites=() if k == 0 else [r_pl])
            yield
        S.op("dve", lambda e: e.tensor_tensor(out=lg[:], in0=pl_[:, 0:NE], in1=br_bc[:], op=ALU.add), reads=[r_pl, r_br], writes=[r_lg])
        yield
        S.op("dve", lambda e: e.max(out=m8[:], in_=lg[:]), reads=[r_lg], writes=[r_m8])
        yield
        S.op("dve", lambda e: e.tensor_scalar(out=maskall[:, t, :], in0=lg[:], scalar1=m8[:, 3:4], scalar2=None, op0=ALU.is_ge), reads=[r_lg, r_m8], awrites=[r_maskall])
        yield
        S.op("dve", lambda e: e.tensor_scalar_mul(out=sml[:, 0:1], in0=m8[:, 0:1], scalar1=-1.0), reads=[r_m8], writes=[r_sml])
        yield
        S.op("act", lambda e: e.activation(out=ex[:], in_=lg[:], func=AF.Exp, bias=sml[:, 0:1]), reads=[r_lg, r_sml], writes=[r_ex])
        yield
        S.op("dve", lambda e: e.tensor_tensor(out=ex[:], in0=ex[:], in1=maskall[:, t, :], op=ALU.mult), reads=[r_ex, r_maskall], writes=[r_ex])
        yield
        S.op("dve", lambda e: e.reduce_sum(out=sml[:, 1:2], in_=ex[:], axis=AX.X), reads=[r_ex], awrites=[r_sml])
        yield
        S.op("dve", lambda e: e.reciprocal(out=sml[:, 2:3], in_=sml[:, 1:2]), reads=[r_sml], awrites=[r_sml])
        yield
        S.op("dve", lambda e: e.tensor_scalar(out=gall[:, t, :], in0=ex[:], scalar1=sml[:, 2:3], scalar2=None, op0=ALU.mult), reads=[r_ex, r_sml], awrites=[r_gall])
        yield
        S.op("dve", lambda e: e.tensor_copy(out=maskb[:], in_=maskall[:, t, :]), reads=[r_maskall], writes=[r_maskb])
        yield
        S.op("pe", lambda e: e.matmul(pl_[:, 64:64 + NE], lhsT=sutb[:], rhs=maskb[:], start=True, stop=True), reads=[r_sutb, r_maskb, r_lg], awrites=[r_pl])
        yield
        S.op("pe", lambda e: e.matmul(pl_[:, 128:128 + NE], lhsT=onesb[:], rhs=maskb[:], start=True, stop=True), reads=[r_onesb, r_maskb], awrites=[r_pl])
        yield
        S.op("dve", lambda e: e.tensor_tensor(out=posall[:, t, :], in0=pl_[:, 64:64 + NE], in1=rrun[:], op=ALU.add), reads=[r_pl, r_rrun], awrites=[r_posall])
        yield
        S.op("dve", lambda e: e.tensor_tensor(out=rrun[:], in0=pl_[:, 128:128 + NE], in1=rrun[:], op=ALU.add), reads=[r_pl, r_rrun], writes=[r_rrun])
        yield


    def tile_gen(t):
        if t + 3 < NT:
            loadD2(t + 3)
        yield from stage1(t)
        yield from stage2(t)

    for t0 in range(3):
        loadD2(t0)
    interleave((tile_gen(t) for t in range(NT)), 26)

    thr16, r_thr16 = C.sb("thr16", [128, NE, 16], F32)
    bstart, r_bstart = C.sb("bstart", [128, NBLK], F32)
    kp, r_kp = C.sb("kp", [128, 8], F32)
    pcol, r_pcol = C.sb("pcol", [128, 1], F32)
    sut32, r_sut32 = C.sb("sut32", [NE, NE], F32)
    S.dma("sp", thr16[:].rearrange("p e m -> p (e m)"), g_("thr16_d").partition_broadcast(128), reads=[r_in], writes=[r_thr16])
    S.dma("sp", bstart[:], g_("bstart_d").partition_broadcast(128), reads=[r_in], writes=[r_bstart])
    S.dma("sp", kp[:], g_("kp_d"), reads=[r_in], writes=[r_kp])
    S.dma("sp", pcol[:], g_("pcol_d"), reads=[r_in], writes=[r_pcol])
    S.dma("sp", sut32[:], g_("sut32_d"), reads=[r_in], writes=[r_sut32])
    big, r_big = C.sb("bigD", [128, NBLK * NE], F32)
    nbk, r_nbk = C.sb("nbk", [128, NE], F32)
    padT, r_padT = C.sb("padT", [NE, 128], F32)
    pstart, r_pstart = C.sb("pstart", [128, NE], F32)
    pend, r_pend = C.sb("pend", [128, NE], F32)
    bexp, r_bexp = C.sb("bexp", [128, NBLK], F32)
    wf, r_wf = C.sb("wf", [128, NBLK, 8], F32)
    S.op("dve", lambda e: e.tensor_tensor(out=big[:, 0:NE * 16].rearrange("p (e m) -> p e m", m=16), in0=rrun[:].unsqueeze(2).to_broadcast([128, NE, 16]), in1=thr16[:], op=ALU.is_gt),
         reads=[r_rrun, r_thr16], writes=[r_big])
    S.op("dve", lambda e: e.tensor_reduce(out=nbk[:], in_=big[:, 0:NE * 16].rearrange("p (e m) -> p e m", m=16), axis=AX.X, op=ALU.add), reads=[r_big], writes=[r_nbk])
    S.op("dve", lambda e: e.tensor_scalar_mul(out=nbk[:], in0=nbk[:], scalar1=512.0), reads=[r_nbk], writes=[r_nbk])
    pq, r_pq = psl[0]
    S.op("pe", lambda e: e.transpose(out=pq[0:NE, 0:128], in_=nbk[:], identity=ident_f[:]), reads=[r_nbk, r_identf], writes=[r_pq])
    S.op("act", lambda e: e.activation(out=padT[:], in_=pq[0:NE, 0:128], func=AF.Copy), reads=[r_pq], writes=[r_padT])
    S.op("pe", lambda e: e.matmul(pq[:, 256:256 + NE], lhsT=padT[:], rhs=sut32[:], start=True, stop=True), reads=[r_padT, r_sut32], awrites=[r_pq])
    S.op("act", lambda e: e.activation(out=pstart[:], in_=pq[:, 256:256 + NE], func=AF.Copy), reads=[r_pq], writes=[r_pstart])
    S.op("dve", lambda e: e.tensor_tensor(out=pend[:], in0=pstart[:], in1=nbk[:], op=ALU.add), reads=[r_pstart, r_nbk], writes=[r_pend])
    S.op("dve", lambda e: e.tensor_tensor(out=big[:].rearrange("p (i e) -> p i e", e=NE), in0=bstart[:].unsqueeze(2).to_broadcast([128, NBLK, NE]),
                                         in1=pend[:].unsqueeze(1).to_broadcast([128, NBLK, NE]), op=ALU.is_ge), reads=[r_bstart, r_pend], writes=[r_big])
    S.op("dve", lambda e: e.tensor_reduce(out=bexp[:], in_=big[:].rearrange("p (i e) -> p i e", e=NE), axis=AX.X, op=ALU.add), reads=[r_big], writes=[r_bexp])
    S.op("dve", lambda e: e.tensor_scalar_min(out=bexp[:], in0=bexp[:], scalar1=float(NE - 1)), reads=[r_bexp], writes=[r_bexp])
    S.op("dve", lambda e: e.tensor_copy(out=eidx[:], in_=bexp[:]), reads=[r_bexp], writes=[r_eidx])
    S.op("dve", lambda e: e.tensor_scalar(out=bidx[:], in0=bexp[:], scalar1=128.0, scalar2=pcol[:, 0:1], op0=ALU.mult, op1=ALU.add), reads=[r_bexp, r_pcol], writes=[r_bidx])
    S.op("dve", lambda e: e.tensor_scalar_mul(out=wf[:], in0=bexp[:].unsqueeze(2).to_broadcast([128, NBLK, 8]), scalar1=float(D)), reads=[r_bexp], writes=[r_wf])
    S.op("dve", lambda e: e.tensor_tensor(out=widx[:], in0=wf[:], in1=kp[:].unsqueeze(1).to_broadcast([128, NBLK, 8]), op=ALU.add), reads=[r_wf, r_kp], writes=[r_widx])
    sl_, r_sl = C.sb("slD", [128, NE], F32)
    eqt, r_eqt = C.sb("eqt", [128, NE], F32)
    m8b, r_m8b = C.sb("m8b", [128, 8], F32)
    S.op("dve", lambda e: e.tensor_scalar_add(out=pstart[:], in0=pstart[:], scalar1=1.0), reads=[r_pstart], writes=[r_pstart])
    for t in range(NT):
        tok0 = t * 128
        ub, r_ub = u2b[t % 2]
        S.dma("sp", ub[:], u2_d[tok0:tok0 + 128, :], reads=[r_u2], writes=[r_ub])
        S.op("dve", lambda e: e.tensor_tensor(out=sl_[:], in0=posall[:, t, :], in1=pstart[:], op=ALU.add), reads=[r_posall, r_pstart], writes=[r_sl])
        S.op("dve", lambda e: e.tensor_tensor(out=sl_[:], in0=sl_[:], in1=maskall[:, t, :], op=ALU.mult), reads=[r_sl, r_maskall], writes=[r_sl])
        S.op("dve", lambda e: e.max(out=m8b[:], in_=sl_[:]), reads=[r_sl], writes=[r_m8b])
        for j in range(4):
            S.op("dve", lambda e: e.scalar_tensor_tensor(out=eqt[:], in0=sl_[:], scalar=m8b[:, j:j + 1], in1=gall[:, t, :], op0=ALU.is_equal, op1=ALU.mult, accum_out=g4[:, t, j:j + 1]),
                 reads=[r_sl, r_m8b, r_gall], writes=[r_eqt], awrites=[r_g4])
        S.op("dve", lambda e: e.tensor_scalar_add(out=idx4[:, t, :], in0=m8b[:, 0:4], scalar1=-1.0), reads=[r_m8b], awrites=[r_idx4])
        for j in range(4):
            S.op("pool", lambda e: e.indirect_dma_start(out=xs_d[:, :], out_offset=bass.IndirectOffsetOnAxis(ap=idx4[:, t, j:j + 1], axis=0), in_=ub[:], in_offset=None),
                 reads=[r_ub, r_idx4], awrites=[r_xsd], dma=True)
    C.pop()
    if "stopD" in dbg:
        C.pop()
        return

    C.push()
    w1b = C.ring("sb", "w1b", 2, [128, 8, 2 * D], BF16)
    w2b = C.ring("sb", "w2b", 2, [128, 8, D], BF16)
    b1t = C.ring("sb", "b1t", 2, [128, 16], F32)
    b2t = C.ring("sb", "b2t", 2, [2, D], F32)
    ones1f, r_ones1f = C.sb("ones1f", [1, 128], BF16)
    S.op("dve", lambda e: e.memset(ones1f[:], 1.0), writes=[r_ones1f])
    b2b = C.ring("sb", "b2b", 2, [1, D], BF16)
    xr = C.ring("sb", "xr", 8, [128, D], BF16)
    XT, r_XT = C.sb("XT", [128, 8, 512], BF16)
    hg = C.ring("sb", "hg", 2, [128, 512], F32)
    hu = C.ring("sb", "hu", 2, [128, 512], F32)
    sgm = C.ring("sb", "sgm", 2, [128, 512], F32)
    actT, r_actT = C.sb("actT", [128, 8, 512], BF16)
    ysb = C.ring("sb", "ysb", 2, [128, D], BF16)
    pxt = C.ring("ps", "pxt", 2, [128, 512], F32)
    pg = C.ring("ps", "pg", 2, [128, 512], F32)
    pu = C.ring("ps", "pu", 2, [128, 512], F32)
    py = C.ring("ps", "py", 2, [128, 512], F32)

    def load_weights(i, slot):
        w1t, r_w1 = w1b[slot]
        w2t, r_w2 = w2b[slot]
        for k in range(8):
            S.op("pool", lambda e: e.indirect_dma_start(out=w1t[:, k, :], out_offset=None, in_=w1b_d[:, :], in_offset=bass.IndirectOffsetOnAxis(ap=widx[:, i, k:k + 1], axis=0)),
                 reads=[r_w1bd, r_widx], writes=[r_w1] if k == 0 else (), awrites=() if k == 0 else [r_w1], dma=True)
        for k in range(8):
            S.op("pool", lambda e: e.indirect_dma_start(out=w2t[:, k, :], out_offset=None, in_=w2b_d[:, :], in_offset=bass.IndirectOffsetOnAxis(ap=widx[:, i, k:k + 1], axis=0)),
                 reads=[r_w2bd, r_widx], writes=[r_w2] if k == 0 else (), awrites=() if k == 0 else [r_w2], dma=True)
        S.op("pool", lambda e: e.indirect_dma_start(out=b1t[slot][0][:], out_offset=None, in_=b1r_d[:, :], in_offset=bass.IndirectOffsetOnAxis(ap=bidx[:, i:i + 1], axis=0)),
             reads=[r_in, r_bidx], writes=[b1t[slot][1]], dma=True)
        S.op("pool", lambda e: e.indirect_dma_start(out=b2t[slot][0][:], out_offset=None, in_=b2_d[:, :], in_offset=bass.IndirectOffsetOnAxis(ap=eidx[0:2, i:i + 1], axis=0)),
             reads=[r_in, r_eidx], writes=[b2t[slot][1]], dma=True)

    def load_x(i):
        for s4 in range(4):
            x_t, r_x = xr[(i % 2) * 4 + s4]
            row0 = i * 512 + s4 * 128
            S.dma("sp", x_t[:], xs_d[row0:row0 + 128, :], reads=[r_xsd], writes=[r_x])

    nblk_run = NBLK if "nblk" not in L else L["nblk"]
    load_weights(0, 0)
    load_x(0)
    xc = 0
    for i in range(nblk_run):
        slot = i % 2
        if i + 1 < nblk_run:
            load_weights(i + 1, (i + 1) % 2)
            load_x(i + 1)
        w1t, r_w1 = w1b[slot]
        w2t, r_w2 = w2b[slot]
        b1_t, r_b1 = b1t[slot]
        b2f_t, r_b2f = b2t[slot]
        b2_t, r_b2 = b2b[slot]
        S.op("pool", lambda e: e.tensor_copy(out=b2_t[:], in_=b2f_t[0:1, :]), reads=[r_b2f], writes=[r_b2])
        for s4 in range(4):
            x_t, r_x = xr[(i % 2) * 4 + s4]
            p_t, r_p = pxt[xc % 2]
            xc += 1
            p_b = p_t[:].bitcast(BF16)
            for k in range(8):
                S.op("pe", lambda e: e.transpose(out=p_b[:, k * 128:(k + 1) * 128], in_=x_t[:, k * 128:(k + 1) * 128], identity=ident_b[:]),
                     reads=[r_x, r_identb], writes=[r_p] if k == 0 else (), awrites=() if k == 0 else [r_p])
            S.op("act", lambda e: e.activation(out=XT[:, :, s4 * 128:(s4 + 1) * 128], in_=p_b.rearrange("p (k t) -> p k t", k=8), func=AF.Copy), reads=[r_p],
                 writes=[r_XT] if s4 == 0 else (), awrites=() if s4 == 0 else [r_XT])
        for fc in range(8):
            pg_t, r_pg = pg[fc % 2]
            pu_t, r_pu = pu[fc % 2]
            for k in range(8):
                S.op("pe", lambda e: e.matmul(pg_t[:], lhsT=w1t[:, k, fc * 128:(fc + 1) * 128], rhs=XT[:, k, :], start=(k == 0), stop=(k == 7)),
                     reads=[r_w1, r_XT], writes=[r_pg] if k == 0 else (), awrites=() if k == 0 else [r_pg])
            for k in range(8):
                S.op("pe", lambda e: e.matmul(pu_t[:], lhsT=w1t[:, k, D + fc * 128:D + (fc + 1) * 128], rhs=XT[:, k, :], start=(k == 0), stop=(k == 7)),
                     reads=[r_w1, r_XT], writes=[r_pu] if k == 0 else (), awrites=() if k == 0 else [r_pu])
            hg_t, r_hg = hg[fc % 2]
            hu_t, r_hu = hu[fc % 2]
            sg_t, r_sgm = sgm[fc % 2]
            S.op("act", lambda e: e.activation(out=hg_t[:], in_=pg_t[:], func=AF.Identity, bias=b1_t[:, fc:fc + 1]), reads=[r_pg, r_b1], writes=[r_hg])
            S.op("act", lambda e: e.activation(out=hu_t[:], in_=pu_t[:], func=AF.Identity, bias=b1_t[:, 8 + fc:9 + fc]), reads=[r_pu, r_b1], writes=[r_hu])
            S.op("dve", lambda e: e.tensor_scalar_min(out=hg_t[:], in0=hg_t[:], scalar1=7.0), reads=[r_hg], writes=[r_hg])
            S.op("act", lambda e: e.activation(out=sg_t[:], in_=hg_t[:], func=AF.Sigmoid, scale=1.702), reads=[r_hg], writes=[r_sgm])
            S.op("pool", lambda e: e.tensor_scalar(out=hu_t[:], in0=hu_t[:], scalar1=7.0, scalar2=-7.0, op0=ALU.min, op1=ALU.max), reads=[r_hu], writes=[r_hu])
            S.op("dve", lambda e: e.scalar_tensor_tensor(out=hu_t[:], in0=hu_t[:], scalar=1.0, in1=hg_t[:], op0=ALU.add, op1=ALU.mult), reads=[r_hu, r_hg], writes=[r_hu])
            S.op("dve", lambda e: e.tensor_tensor(out=actT[:, fc, :], in0=hu_t[:], in1=sg_t[:], op=ALU.mult), reads=[r_hu, r_sgm],
                 writes=[r_actT] if fc == 0 else (), awrites=() if fc == 0 else [r_actT])
        for s4 in range(4):
            y_t, r_y = ysb[s4 % 2]
            for hf in range(2):
                py_t, r_py = py[hf]
                for fc in range(8):
                    S.op("pe", lambda e: e.matmul(py_t[:], lhsT=actT[:, fc, s4 * 128:(s4 + 1) * 128], rhs=w2t[:, fc, hf * 512:(hf + 1) * 512], start=(fc == 0), stop=False),
                         reads=[r_actT, r_w2], writes=[r_py] if fc == 0 else (), awrites=() if fc == 0 else [r_py])
                S.op("pe", lambda e: e.matmul(py_t[:], lhsT=ones1f[:], rhs=b2_t[0:1, hf * 512:(hf + 1) * 512], start=False, stop=True), reads=[r_ones1f, r_b2], awrites=[r_py])
                S.op("act", lambda e: e.activation(out=y_t[:, hf * 512:(hf + 1) * 512], in_=py_t[:], func=AF.Copy), reads=[r_py],
                     writes=[r_y] if hf == 0 else (), awrites=() if hf == 0 else [r_y])
            row0 = i * 512 + s4 * 128
            S.dma("act", ys_d[row0:row0 + 128, :], y_t[:], reads=[r_y], awrites=[r_ysd])
    C.pop()

    C.push()
    ln2g, r_ln2g = C.sb("ln2g", [128, D], F32)
    ln2b, r_ln2b = C.sb("ln2b", [128, D], F32)
    gate2, r_gate2 = C.sb("gate2", [128, D], F32)
    S.dma("sp", ln2g[:], g_("ln2g_d").partition_broadcast(128), reads=[r_in], writes=[r_ln2g])
    S.dma("sp", ln2b[:], g_("ln2b_d").partition_broadcast(128), reads=[r_in], writes=[r_ln2b])
    x1r = C.ring("sb", "x1r", 2, [128, D], F32)
    yg = C.ring("sb", "yg", 8, [128, D], BF16)
    acc, r_acc = C.sb("accF", [128, D], F32)
    ot = C.ring("sb", "otF", 2, [128, D], F32)
    st, r_st = C.sb("stF", [128, 2, 6], F32)
    mv, r_mv = C.sb("mvF", [128, 2], F32)
    rs, r_rs = C.sb("rsF", [128, 1], F32)

    def loadF(t, slot):
        tok0 = t * 128
        S.dma("sp", x1r[slot][0][:], x1_d[tok0:tok0 + 128, :], reads=[r_x1], writes=[x1r[slot][1]])
        for j in range(4):
            y_t, r_y = yg[slot * 4 + j]
            S.op("pool", lambda e: e.indirect_dma_start(out=y_t[:], out_offset=None, in_=ys_d[:, :], in_offset=bass.IndirectOffsetOnAxis(ap=idx4[:, t, j:j + 1], axis=0)),
                 reads=[r_ysd, r_idx4], writes=[r_y], dma=True)

    loadF(0, 0)
    for t in range(NT):
        slot = t % 2
        tok0 = t * 128
        b = tok0 // SEQ
        if t % TPS == 0:
            S.dma("sp", gate2[:], mod_d[b:b + 1, 5 * D:6 * D].partition_broadcast(128), reads=[r_mod], writes=[r_gate2])
        if t + 1 < NT:
            loadF(t + 1, (t + 1) % 2)
        x1_t, r_x1t = x1r[slot]
        for j in range(4):
            y_t, r_y = yg[slot * 4 + j]
            if j == 0:
                S.op("dve", lambda e: e.tensor_scalar(out=acc[:], in0=y_t[:], scalar1=g4[:, t, 0:1], scalar2=None, op0=ALU.mult), reads=[r_y, r_g4], writes=[r_acc])
            else:
                S.op("dve", lambda e: e.scalar_tensor_tensor(out=acc[:], in0=y_t[:], scalar=g4[:, t, j:j + 1], in1=acc[:], op0=ALU.mult, op1=ALU.add), reads=[r_y, r_g4, r_acc], writes=[r_acc])
        S.op("pool", lambda e: e.tensor_tensor(out=acc[:], in0=acc[:], in1=gate2[:], op=ALU.mult), reads=[r_acc, r_gate2], writes=[r_acc])
        S.op("dve", lambda e: e.scalar_tensor_tensor(out=acc[:], in0=x1_t[:], scalar=float(ALPHA), in1=acc[:], op0=ALU.mult, op1=ALU.add), reads=[r_x1t, r_acc], writes=[r_acc])
        ln_stats(acc, r_acc, st, r_st, mv, r_mv, rs, r_rs)
        o_t, r_o = ot[slot]
        S.op("dve", lambda e: e.tensor_scalar(out=o_t[:], in0=acc[:], scalar1=mv[:, 0:1], scalar2=rs[:], op0=ALU.subtract, op1=ALU.mult), reads=[r_acc, r_mv, r_rs], writes=[r_o])
        S.op("pool", lambda e: e.tensor_tensor(out=o_t[:], in0=o_t[:], in1=ln2g[:], op=ALU.mult), reads=[r_o, r_ln2g], writes=[r_o])
        S.op("pool", lambda e: e.tensor_tensor(out=o_t[:], in0=o_t[:], in1=ln2b[:], op=ALU.add), reads=[r_o, r_ln2b], writes=[r_o])
        S.dma("sp", out_d[tok0:tok0 + 128, :], o_t[:], reads=[r_o], awrites=[r_out])
    C.pop()
    C.pop()


def phase_C(nc, S, C, dbg, L):
    g_ = lambda n: L[n]
    r_in = g_("r_in"); ident_b = g_("ident_b"); r_identb = g_("r_identb"); ident_f = g_("ident_f"); r_identf = g_("r_identf")
    qT_d, r_qT = g_("qT_d"), g_("r_qT"); iqT_d, r_iqT = g_("iqT_d"), g_("r_iqT")
    kva_d, r_kva = g_("kva_d"), g_("r_kva"); kvT_d, r_kvT = g_("kvT_d"), g_("r_kvT"); ikT_d, r_ikT = g_("ikT_d"), g_("r_ikT")
    iw_d, r_iw = g_("iw_d"), g_("r_iw"); oaT_d, r_oaT = g_("oaT_d"), g_("r_oaT")
    C.push()
    w1_d, w2_d = g_("w1_d"), g_("w2_d")

    def cast_weights(i):
        e_ = i // 2
        if i % 2 == 0:
            S.dma("pool", g_("w1b_d")[e_ * D:(e_ + 1) * D, :], w1_d[e_ * D:(e_ + 1) * D, :], reads=[r_in], awrites=[g_("r_w1bd")])
        else:
            S.dma("pool", g_("w2b_d")[e_ * D:(e_ + 1) * D, :], w2_d[e_ * D:(e_ + 1) * D, :], reads=[r_in], awrites=[g_("r_w2bd")])
    tzf, r_tzf = C.sb("tzf", [128, 2, 8, 128], F32)
    tz, r_tz = C.sb("tzb", [128, 2, 8, 128], BF16)
    cfar, r_cfar = C.sb("cfar", [128, 8], F32)
    identN, r_identN = C.sb("identN", [128, 128], BF16)
    S.dma("sp", tzf[:], g_("tz_d"), reads=[r_in], writes=[r_tzf])
    S.dma("sp", cfar[:], g_("cfar_d").partition_broadcast(128), reads=[r_in], writes=[r_cfar])
    S.op("dve", lambda e: e.tensor_scalar_mul(out=identN[:], in0=ident_f[:], scalar1=-NEG), reads=[r_identf], writes=[r_identN])
    first = True
    for dl in range(2):
        for h in range(8):
            S.op("dve", lambda e: e.tensor_scalar(out=tz[:, dl, h, :], in0=tzf[:, dl, h, :], scalar1=cfar[:, h:h + 1], scalar2=None, op0=ALU.subtract),
                 reads=[r_tzf, r_cfar], writes=[r_tz] if first else (), awrites=() if first else [r_tz])
            first = False
    seqb = [(C.sb("kvT%d" % i, [128, SEQ], BF16), C.sb("ikT%d" % i, [128, SEQ], BF16), C.sb("kvaC%d" % i, [128, TPS, 136], BF16)) for i in range(2)]
    qt = C.ring("sb", "qt", 5, [128, 8, 128], BF16)
    iqt = C.ring("sb", "iqt", 4, [128, 4, 128], BF16)
    iwt = C.ring("sb", "iwC", 4, [128, 8], F32)
    scorer = C.ring("sb", "score", 3, [128, SEQ], F32)
    penr = C.ring("sb", "pen", 2, [128, SEQ], BF16)
    rl = C.ring("sb", "rl", 2, [128, 512], F32)
    m8, r_m8 = C.sb("m8", [128, 8], F32)
    expT = C.ring("sb", "expT", 2, [128, TPS * 128], BF16)
    posb = C.ring("sb", "posb", 2, [128, 8, 132], F32)
    rc, r_rc = C.sb("rcC", [128, 8], F32)
    oa, r_oa = C.sb("oa", [128, D], BF16)
    oaT = C.ring("sb", "oaT", 2, [128, 8, 128], BF16)
    pi = C.ring("ps", "pi", 2, [128, 512], F32)
    pl = C.ring("ps", "pl", 3, [128, 512], F32)
    po = C.ring("ps", "po", 2, [128, 512], F32)
    ptr = C.ring("ps", "ptrC", 1, [128, 512], F32)
    NTILE = NB * TPS

    def load_seq(b):
        (kvT, r_kvTs), (ikT, r_ikTs), (kva, r_kvas) = seqb[b % 2]
        s0 = b * SEQ
        S.dma("sp", kvT[:], kvT_d[:, s0:s0 + SEQ], reads=[r_kvT], writes=[r_kvTs])
        S.dma("sp", ikT[:], ikT_d[:, s0:s0 + SEQ], reads=[r_ikT], writes=[r_ikTs])
        S.dma("sp", kva[:], kva_d[s0:s0 + SEQ, :].rearrange("(k p) c -> p k c", p=128), reads=[r_kva], writes=[r_kvas])

    def load_tile(i):
        tok0 = i * 128
        S.dma("sp", qt[i % 5][0][:], qT_d[:, :, tok0:tok0 + 128].rearrange("h p t -> p h t"), reads=[r_qT], writes=[qt[i % 5][1]])
        S.dma("sp", iqt[i % 4][0][:], iqT_d[:, :, tok0:tok0 + 128].rearrange("h p t -> p h t"), reads=[r_iqT], writes=[iqt[i % 4][1]])
        S.dma("sp", iwt[i % 4][0][:], iw_d[tok0:tok0 + 128, :], reads=[r_iw], writes=[iwt[i % 4][1]])

    NIT = 24
    pow2, r_pow2 = C.sb("pow2", [128, NIT + 1], F32)
    stepsr = C.ring("sb", "steps", 3, [128, NIT + 1], F32)
    bisr = C.ring("sb", "bis", 3, [128, 8], F32)
    junkb, r_junkb = C.sb("junkb", [128, SEQ], BF16)
    for j in range(NIT + 1):
        S.op("pool", lambda e: e.memset(pow2[:, j:j + 1], float(2.0 ** -(j + 1))), writes=[r_pow2] if j == 0 else (), awrites=() if j == 0 else [r_pow2])

    def prep_score_gen(i):
        b, t = divmod(i, TPS)
        (ikT, r_ikTs) = seqb[b % 2][1]
        iq_t, r_iq = iqt[i % 4]
        iw_t, r_iwt = iwt[i % 4]
        pen, r_pen = penr[i % 2]
        score, r_score = scorer[i % 3]
        steps, r_steps = stepsr[i % 3]
        bis, r_bis = bisr[i % 3]
        N = 128 * (t + 1)
        if t < 2:
            return
            yield
        for kg in range(0, N, 512):
            w = min(512, N - kg)
            for h in range(8):
                pr, hf = divmod(h, 2)
                p_t, r_p = pi[h % 2]
                S.op("pe", lambda e: e.matmul(p_t[:, 0:w], lhsT=iq_t[64 * hf:64 * hf + 64, pr, :], rhs=ikT[64 * hf:64 * hf + 64, kg:kg + w], start=True, stop=True),
                     reads=[r_iq, r_ikTs], writes=[r_p])
                r_t, r_r = rl[h % 2]
                if h == 0:
                    S.op("dve", lambda e: e.tensor_scalar(out=score[:, kg:kg + w], in0=p_t[:, 0:w], scalar1=0.0, scalar2=iw_t[:, 0:1], op0=ALU.max, op1=ALU.mult),
                         reads=[r_p, r_iwt], writes=[r_score] if kg == 0 else (), awrites=() if kg == 0 else [r_score])
                elif h % 2 == 1:
                    S.op("dve", lambda e: e.tensor_scalar(out=r_t[:, 0:w], in0=p_t[:, 0:w], scalar1=0.0, scalar2=iw_t[:, h:h + 1], op0=ALU.max, op1=ALU.mult),
                         reads=[r_p, r_iwt], writes=[r_r])
                    S.op("pool", lambda e: e.tensor_tensor(out=score[:, kg:kg + w], in0=score[:, kg:kg + w], in1=r_t[:, 0:w], op=ALU.add),
                         reads=[r_r, r_score], awrites=[r_score])
                else:
                    S.op("act", lambda e: e.activation(out=r_t[:, 0:w], in_=p_t[:, 0:w], func=AF.Relu), reads=[r_p], writes=[r_r])
                    S.op("dve", lambda e: e.scalar_tensor_tensor(out=score[:, kg:kg + w], in0=r_t[:, 0:w], scalar=iw_t[:, h:h + 1], in1=score[:, kg:kg + w], op0=ALU.mult, op1=ALU.add),
                         reads=[r_r, r_iwt, r_score], awrites=[r_score])
                yield
        S.op("dve", lambda e: e.tensor_reduce(out=bis[:, 0:1], in_=score[:, 0:N - 64], axis=AX.X, op=ALU.min), reads=[r_score], writes=[r_bis])
        S.op("dve", lambda e: e.memset(score[0:64, N - 64:N], -1e30), reads=[r_score], awrites=[r_score])
        S.op("dve", lambda e: e.max(out=m8[:], in_=score[:, 0:N]), reads=[r_score], writes=[r_m8])
        S.op("dve", lambda e: e.tensor_tensor(out=bis[:, 1:2], in0=m8[:, 0:1], in1=bis[:, 0:1], op=ALU.subtract), reads=[r_m8, r_bis], awrites=[r_bis])
        S.op("dve", lambda e: e.tensor_scalar(out=steps[:], in0=pow2[:], scalar1=bis[:, 1:2], scalar2=None, op0=ALU.mult), reads=[r_pow2, r_bis], writes=[r_steps])
        S.op("dve", lambda e: e.tensor_tensor(out=bis[:, 2:3], in0=bis[:, 0:1], in1=steps[:, 0:1], op=ALU.add), reads=[r_bis, r_steps], awrites=[r_bis])

    def prep_score(i):
        for _ in prep_score_gen(i):
            pass

    def prep_iter(i, j):
        b, t = divmod(i, TPS)
        if t < 2:
            return
        N = 128 * (t + 1)
        score, r_score = scorer[i % 3]
        steps, r_steps = stepsr[i % 3]
        bis, r_bis = bisr[i % 3]
        S.op("act", lambda e: e.activation(out=junkb[:, 0:N], in_=score[:, 0:N], func=AF.Sign, scale=-1.0, bias=bis[:, 2:3], accum_out=bis[:, 4:5]),
             reads=[r_score, r_bis], writes=[r_junkb], awrites=[r_bis])
        S.op("dve", lambda e: e.tensor_scalar(out=bis[:, 3:4], in0=bis[:, 4:5], scalar1=float(N - 511), scalar2=steps[:, j:j + 1], op0=ALU.is_le, op1=ALU.mult),
             reads=[r_bis, r_steps], awrites=[r_bis])
        S.op("dve", lambda e: e.scalar_tensor_tensor(out=bis[:, 2:3], in0=bis[:, 3:4], scalar=steps[:, j + 1:j + 2], in1=bis[:, 2:3], op0=ALU.subtract, op1=ALU.add),
             reads=[r_bis, r_steps], awrites=[r_bis])

    def prep_fin(i):
        b, t = divmod(i, TPS)
        N = 128 * (t + 1)
        pen, r_pen = penr[i % 2]
        if t < 2:
            S.op("dve", lambda e: e.memset(pen[:, 0:N], 0.0), writes=[r_pen])
            S.op("dve", lambda e: e.memset(pen[0:64, N - 64:N], -1.0), awrites=[r_pen])
            return
        score, r_score = scorer[i % 3]
        steps, r_steps = stepsr[i % 3]
        bis, r_bis = bisr[i % 3]
        S.op("dve", lambda e: e.tensor_tensor(out=bis[:, 5:6], in0=bis[:, 2:3], in1=steps[:, NIT:NIT + 1], op=ALU.subtract), reads=[r_bis, r_steps], awrites=[r_bis])
        S.op("dve", lambda e: e.tensor_scalar(out=pen[:, 0:N], in0=score[:, 0:N], scalar1=bis[:, 5:6], scalar2=1.0, op0=ALU.is_ge, op1=ALU.subtract),
             reads=[r_score, r_bis], writes=[r_pen])

    state = {"plc": 0, "hc": 0}

    def attend_head(i, h):
        b, t = divmod(i, TPS)
        (kvT, r_kvTs), _, (kva, r_kvas) = seqb[b % 2]
        q_t, r_q = qt[i % 5]
        pen, r_pen = penr[i % 2]
        ps_t, r_ps = posb[i % 2]
        nkb = t + 1
        if True:
            e_t, r_e = expT[state["hc"] % 2]
            state["hc"] += 1
            for kg in range(0, nkb, 4):
                nb_ = min(4, nkb - kg)
                p_t, r_p = pl[state["plc"] % 3]
                state["plc"] += 1
                for ii in range(nb_):
                    kb = kg + ii
                    near = kb >= t - 1
                    cs_ = slice(ii * 128, (ii + 1) * 128)
                    S.op("pe", lambda e: e.matmul(p_t[:, cs_], lhsT=kvT[:, kb * 128:(kb + 1) * 128], rhs=q_t[:, h, :], start=True, stop=False),
                         reads=[r_kvTs, r_q], writes=[r_p] if ii == 0 else (), awrites=() if ii == 0 else [r_p])
                    S.op("pe", lambda e: e.matmul(p_t[:, cs_], lhsT=pen[:, kb * 128:(kb + 1) * 128], rhs=identN[:], start=False, stop=(not near)),
                         reads=[r_pen, r_identN], awrites=[r_p])
                    if near:
                        S.op("pe", lambda e: e.matmul(p_t[:, cs_], lhsT=ident_b[:], rhs=tz[:, t - kb, h, :], start=False, stop=True),
                             reads=[r_identb, r_tz], awrites=[r_p])
                S.op("act", lambda e: e.activation(out=e_t[:, kg * 128:(kg + nb_) * 128], in_=p_t[:, 0:nb_ * 128], func=AF.Exp), reads=[r_p],
                     writes=[r_e] if kg == 0 else (), awrites=() if kg == 0 else [r_e])
            o_t, r_o = po[h % 2]
            for kb in range(nkb):
                S.op("pe", lambda e: e.matmul(o_t[:, 0:129], lhsT=e_t[:, kb * 128:(kb + 1) * 128], rhs=kva[:, kb, 0:129], start=(kb == 0), stop=(kb == nkb - 1)),
                     reads=[r_e, r_kvas], writes=[r_o] if kb == 0 else (), awrites=() if kb == 0 else [r_o])
            S.op("act", lambda e: e.activation(out=ps_t[:, h, 0:129], in_=o_t[:, 0:129], func=AF.Copy), reads=[r_o],
                 writes=[r_ps] if h == 0 else (), awrites=() if h == 0 else [r_ps])

    def finalize(i):
        tok0 = i * 128
        ps_t, r_ps = posb[i % 2]
        S.op("dve", lambda e: e.reciprocal(out=rc[:], in_=ps_t[:, :, 128]), reads=[r_ps], writes=[r_rc])
        S.op("dve", lambda e: e.tensor_tensor(out=oa[:].rearrange("p (h c) -> p h c", h=8), in0=ps_t[:, :, 0:128], in1=rc[:].unsqueeze(2).to_broadcast([128, 8, 128]), op=ALU.mult),
             reads=[r_ps, r_rc], writes=[r_oa])
        pT, r_pT = ptr[0]
        pT_b = pT[:].bitcast(BF16)
        for k in range(8):
            S.op("pe", lambda e: e.transpose(out=pT_b[:, k * 128:(k + 1) * 128], in_=oa[:, k * 128:(k + 1) * 128], identity=ident_b[:]),
                 reads=[r_oa, r_identb], writes=[r_pT] if k == 0 else (), awrites=() if k == 0 else [r_pT])
        oT, r_oT = oaT[i % 2]
        S.op("act", lambda e: e.activation(out=oT[:].rearrange("p k t -> p (k t)"), in_=pT_b, func=AF.Copy), reads=[r_pT], writes=[r_oT])
        S.dma("sp", oaT_d[:, :, tok0:tok0 + 128].rearrange("c p t -> p c t"), oT[:], reads=[r_oT], awrites=[r_oaT])

    HALF = NIT // 2
    load_seq(0)
    for i0 in range(4):
        load_tile(i0)
    prep_score(0)
    for j in range(NIT):
        prep_iter(0, j)
    prep_fin(0)
    prep_score(1)
    for j in range(HALF):
        prep_iter(1, j)
    prep_score(2)
    for i in range(NTILE):
        b, t = divmod(i, TPS)
        if t == 0 and b + 1 < NB:
            load_seq(b + 1)
        if i + 4 < NTILE:
            load_tile(i + 4)
        cast_weights(i)
        n1 = i + 1 < NTILE
        n2 = i + 2 < NTILE
        gen = prep_score_gen(i + 3) if i + 3 < NTILE else iter(())
        npieces = 8 * ((128 * (((i + 3) % TPS) + 1) + 511) // 512) if (i + 3 < NTILE and (i + 3) % TPS >= 2) else 0
        ppb = (npieces + 7) // 8
        sched = []
        for k in range(HALF):
            if n1:
                sched.append((i + 1, HALF + k))
            if n2:
                sched.append((i + 2, k))
        per = (len(sched) + 7) // 8
        for h in range(8):
            attend_head(i, h)
            its = sched[h * per:(h + 1) * per]
            for n_, (ti_, j) in enumerate(its):
                prep_iter(ti_, j)
                if n_ < ppb:
                    next(gen, None)
            for _ in range(max(0, ppb - len(its))):
                next(gen, None)
        for _ in gen:
            pass
        if n1:
            prep_fin(i + 1)
        if i >= 1:
            finalize(i - 1)
    finalize(NTILE - 1)
    C.pop()


def phase_B(nc, S, C, dbg, L):
    g_ = lambda n: L[n]
    r_in = g_("r_in"); ident_b = g_("ident_b"); r_identb = g_("r_identb")
    xbcT_d, r_xbcT = g_("xbcT_d"), g_("r_xbcT"); dt_d, r_dt = g_("dt_d"), g_("r_dt"); zs_d, r_zs = g_("zs_d"), g_("r_zs")
    obT_d, r_obT = g_("obT_d"), g_("r_obT")
    C.push()
    convw, r_convw = C.sb("convw", [128, 16, 4], F32)
    convb, r_convb = C.sb("convb", [128, 16], F32)
    dg, r_dg = C.sb("dg", [128, 16, 4, 128], BF16)
    identf2, r_identf2 = C.sb("identf2", [128, 128], F32)
    a_bc, r_abc = C.sb("a_bc", [128, 16], F32)
    dskip_bc, r_dskip = C.sb("dskip_bc", [128, 16], F32)
    normw_bc, r_normw = C.sb("normw_bc", [128, D], F32)
    triU, r_triU = C.sb("triU", [128, 128], F32)
    SLm, r_SL = C.sb("SLm", [128, 128], F32)
    onesf, r_onesf = C.sb("onesf", [128, 128], F32)
    negm4, r_negm4 = C.sb("negm4", [128, 512], BF16)
    S.dma("sp", convw[:], g_("convw_d"), reads=[r_in], writes=[r_convw])
    S.dma("sp", convb[:], g_("convb_d"), reads=[r_in], writes=[r_convb])
    S.dma("sp", identf2[:], g_("ident_d"), reads=[r_in], writes=[r_identf2])
    S.dma("sp", a_bc[:], g_("alog_d").partition_broadcast(128), reads=[r_in], writes=[r_abc])
    S.dma("sp", dskip_bc[:], g_("dskip_d").partition_broadcast(128), reads=[r_in], writes=[r_dskip])
    S.dma("sp", normw_bc[:], g_("normw_d").partition_broadcast(128), reads=[r_in], writes=[r_normw])
    S.dma("sp", triU[:], g_("triU_d"), reads=[r_in], writes=[r_triU])
    S.dma("sp", SLm[:], g_("SL_d"), reads=[r_in], writes=[r_SL])
    S.dma("pool", negm4[:], g_("negm4_d"), reads=[r_in], writes=[r_negm4])
    S.op("dve", lambda e: e.memset(onesf[:], 1.0), writes=[r_onesf])
    S.op("act", lambda e: e.activation(out=a_bc[:], in_=a_bc[:], func=AF.Exp), reads=[r_abc], writes=[r_abc])
    S.op("dve", lambda e: e.tensor_scalar_mul(out=a_bc[:], in0=a_bc[:], scalar1=-1.0), reads=[r_abc], writes=[r_abc])
    first = True
    for j in range(16):
        for k in range(4):
            S.op("dve", lambda e: e.tensor_scalar_mul(out=dg[:, j, k, :], in0=identf2[:], scalar1=convw[:, j, k:k + 1]),
                 reads=[r_identf2, r_convw], writes=[r_dg] if first else (), awrites=() if first else [r_dg])
            first = False

    bank = C.ring("ps", "bk", 8, [128, 512], F32)
    xh = C.ring("sb", "xh", 4, [128, 16, 131], BF16)
    dtl = C.ring("sb", "dtl", 4, [128, 16], F32)
    zl = C.ring("sb", "zl", 4, [128, D], BF16)
    xact_r = C.ring("sb", "xact", 2, [128, 16, 128], BF16)
    xs_r = C.ring("sb", "xs_tok", 2, [128, D], BF16)
    Bt_r = C.ring("sb", "B_tok", 2, [128, 512], BF16)
    adt_r = C.ring("sb", "adt", 2, [128, 16], F32)
    sm_r = C.ring("sb", "smB", 2, [128, 8, 16], F32)
    A_r = C.ring("sb", "Amat", 2, [128, 16, 128], F32)
    Lt_r = C.ring("sb", "Lt", 2, [128, 2, 512], F32)
    Mt_r = C.ring("sb", "Mt", 2, [128, 16, 128], BF16)
    xdt_r = C.ring("sb", "xdt", 2, [128, D], BF16)
    xdd_r = C.ring("sb", "xdd", 2, [128, D], BF16)
    prev_f, r_pf = C.sb("prev_f", [128, D], F32)
    prev_b, r_pb = C.sb("prev_b", [128, D], BF16)
    t1_r = C.ring("sb", "t1", 2, [128, D], F32)
    t2_r = C.ring("sb", "t2", 2, [128, D], F32)
    junk, r_junk = C.sb("junkB", [128, 256], F32)
    ob_r = C.ring("sb", "ob", 2, [128, D], BF16)
    obT = C.ring("sb", "obT", 2, [128, 8, 128], BF16)

    def bc3(ap2, n):
        return ap2.unsqueeze(2).to_broadcast([128, 16, n])

    def v3(t, n=64):
        return t.rearrange("p (h q) -> p h q", q=n)

    def load_chunk(b, t, slot):
        tok0 = b * SEQ + t * 128
        x_t, r_x = xh[slot]
        if t == 0:
            S.op("pool", lambda e: e.memset(x_t[:, :, 0:3], 0.0), writes=[r_x])
            S.dma("sp", x_t[:, :, 3:131], xbcT_d[:, :, tok0:tok0 + 128].rearrange("c p t -> p c t"), reads=[r_xbcT], awrites=[r_x])
        else:
            S.dma("sp", x_t[:, :, :], xbcT_d[:, :, tok0 - 3:tok0 + 128].rearrange("c p t -> p c t"), reads=[r_xbcT], writes=[r_x])
        S.dma("sp", dtl[slot][0][:], dt_d[tok0:tok0 + 128, :], reads=[r_dt], writes=[dtl[slot][1]])
        S.dma("sp", zl[slot][0][:], zs_d[tok0:tok0 + 128, :], reads=[r_zs], writes=[zl[slot][1]])

    nch = NB * TPS

    def chunk_gen(ci):
        b, t = divmod(ci, TPS)
        slot = ci % 4
        sl2 = ci % 2
        tok0 = b * SEQ + t * 128
        if ci + 2 < nch:
            load_chunk((ci + 2) // TPS, (ci + 2) % TPS, (ci + 2) % 4)
        x_t, r_x = xh[slot]
        d_t, r_d = dtl[slot]
        z_t, r_z = zl[slot]
        xact, r_xact = xact_r[sl2]; xs_tok, r_xs = xs_r[sl2]; B_tok, r_Bt = Bt_r[sl2]; adt, r_adt = adt_r[sl2]; sm, r_sm = sm_r[sl2]
        Amat, r_A = A_r[sl2]; Lt, r_Lt = Lt_r[sl2]; Mt, r_Mt = Mt_r[sl2]; xdt, r_xdt = xdt_r[sl2]; xdd, r_xdd = xdd_r[sl2]
        t1, r_t1 = t1_r[sl2]; t2, r_t2 = t2_r[sl2]; ob, r_ob = ob_r[sl2]
        for jg in range(4):
            pc, r_pc = bank[jg % 2]
            for jj in range(4):
                j = jg * 4 + jj
                for k in range(4):
                    S.op("pe", lambda e: e.matmul(pc[:, jj * 128:(jj + 1) * 128], lhsT=dg[:, j, k, :], rhs=x_t[:, j, k:k + 128], start=(k == 0), stop=(k == 3)),
                         reads=[r_dg, r_x], writes=[r_pc] if (jj == 0 and k == 0) else (), awrites=() if (jj == 0 and k == 0) else [r_pc])
                    yield
            for jj in range(4):
                j = jg * 4 + jj
                S.op("act", lambda e: e.activation(out=xact[:, j, :], in_=pc[:, jj * 128:(jj + 1) * 128], func=AF.Silu, bias=convb[:, j:j + 1]),
                     reads=[r_pc, r_convb], writes=[r_xact] if j == 0 else (), awrites=() if j == 0 else [r_xact])
                yield
        pxs, r_pxs = bank[2]
        pB, r_pB = bank[3]
        pxs_b = pxs[:].bitcast(BF16)
        pB_b = pB[:].bitcast(BF16)
        for k in range(8):
            S.op("pe", lambda e: e.transpose(out=pxs_b[:, k * 128:(k + 1) * 128], in_=xact[:, k, :], identity=ident_b[:]),
                 reads=[r_xact, r_identb], writes=[r_pxs] if k == 0 else (), awrites=() if k == 0 else [r_pxs])
            yield
        for k in range(4):
            S.op("pe", lambda e: e.transpose(out=pB_b[:, k * 128:(k + 1) * 128], in_=xact[:, 8 + k, :], identity=ident_b[:]),
                 reads=[r_xact, r_identb], writes=[r_pB] if k == 0 else (), awrites=() if k == 0 else [r_pB])
            yield
        S.op("act", lambda e: e.activation(out=xs_tok[:], in_=pxs_b, func=AF.Copy), reads=[r_pxs], writes=[r_xs])
        yield
        S.op("act", lambda e: e.activation(out=B_tok[:], in_=pB_b[:, 0:512], func=AF.Copy), reads=[r_pB], writes=[r_Bt])
        yield
        S.op("dve", lambda e: e.tensor_tensor(out=adt[:], in0=d_t[:], in1=a_bc[:], op=ALU.mult), reads=[r_d, r_abc], writes=[r_adt])
        yield
        S.op("pe", lambda e: e.matmul(pB[:, 256:272], lhsT=triU[:], rhs=adt[:], start=True, stop=True), reads=[r_triU, r_adt], awrites=[r_pB])
        yield
        S.op("pe", lambda e: e.matmul(pB[:, 272:288], lhsT=onesf[:], rhs=adt[:], start=True, stop=True), reads=[r_onesf, r_adt], awrites=[r_pB])
        yield
        S.op("act", lambda e: e.activation(out=sm[:, 0, :], in_=pB[:, 256:272], func=AF.Exp), reads=[r_pB], writes=[r_sm])
        yield
        S.op("act", lambda e: e.activation(out=sm[:, 1, :], in_=pB[:, 272:288], func=AF.Copy), reads=[r_pB], awrites=[r_sm])
        yield
        S.op("act", lambda e: e.activation(out=sm[:, 2, :], in_=pB[:, 272:288], func=AF.Exp), reads=[r_pB], awrites=[r_sm])
        yield
        S.op("dve", lambda e: e.tensor_tensor(out=sm[:, 3, :], in0=sm[:, 1, :], in1=pB[:, 256:272], op=ALU.subtract), reads=[r_sm, r_pB], awrites=[r_sm])
        yield
        S.op("act", lambda e: e.activation(out=sm[:, 4, :], in_=sm[:, 3, :], func=AF.Exp), reads=[r_sm], awrites=[r_sm])
        yield
        S.op("dve", lambda e: e.tensor_tensor(out=Amat[:], in0=triU[:].unsqueeze(1).to_broadcast([128, 16, 128]), in1=bc3(adt[:], 128), op=ALU.mult),
             reads=[r_triU, r_adt], writes=[r_A])
        yield
        pCB, r_pCB = bank[4]
        for g in range(4):
            S.op("pe", lambda e: e.matmul(pCB[:, g * 128:(g + 1) * 128], lhsT=xact[:, 8 + g, :], rhs=xact[:, 12 + g, :], start=True, stop=True),
                 reads=[r_xact], writes=[r_pCB] if g == 0 else (), awrites=() if g == 0 else [r_pCB])
            yield
        S.op("dve", lambda e: e.tensor_tensor(out=v3(xdt[:]), in0=v3(xs_tok[:]), in1=bc3(d_t[:], 64), op=ALU.mult), reads=[r_xs, r_d], writes=[r_xdt])
        yield
        S.op("pool", lambda e: e.tensor_tensor(out=v3(xdd[:]), in0=v3(xdt[:]), in1=bc3(sm[:, 4, :], 64), op=ALU.mult), reads=[r_xdt, r_sm], writes=[r_xdd])
        yield
        for g in range(4):
            pD, r_pD = bank[5 + g % 2]
            S.op("pe", lambda e: e.matmul(pD[:], lhsT=SLm[:], rhs=Amat[:, 4 * g:4 * g + 4, :], start=True, stop=False), reads=[r_SL, r_A], writes=[r_pD])
            yield
            S.op("pe", lambda e: e.matmul(pD[:], lhsT=ident_b[:], rhs=negm4[:], start=False, stop=True), reads=[r_identb, r_negm4], awrites=[r_pD])
            yield
            S.op("act", lambda e: e.activation(out=Lt[:, g % 2, :], in_=pD[:], func=AF.Exp), reads=[r_pD], writes=[r_Lt] if g % 2 == 0 else (), awrites=() if g % 2 == 0 else [r_Lt])
            yield
            S.op("dve", lambda e: e.tensor_tensor(out=Mt[:, 4 * g:4 * g + 4, :], in0=Lt[:, g % 2, :].rearrange("p (h l) -> p h l", h=4),
                                                 in1=pCB[:, g * 128:(g + 1) * 128].unsqueeze(1).to_broadcast([128, 4, 128]), op=ALU.mult),
                 reads=[r_Lt, r_pCB], writes=[r_Mt] if g == 0 else (), awrites=() if g == 0 else [r_Mt])
            yield
        if t == 0:
            S.op("pool", lambda e: e.memset(prev_f[:], 0.0), writes=[r_pf])
            yield
            S.op("pool", lambda e: e.memset(prev_b[:], 0.0), writes=[r_pb])
            yield
        for hh in range(2):
            pY, r_pY = bank[5]
            pO, r_pO = bank[6]
            pS, r_pS = bank[7]
            c0 = hh * 512
            for h8 in range(8):
                h = hh * 8 + h8
                S.op("pe", lambda e: e.matmul(pY[:, h8 * 64:(h8 + 1) * 64], lhsT=Mt[:, h, :], rhs=xdt[:, h * 64:(h + 1) * 64], start=True, stop=True),
                     reads=[r_Mt, r_xdt], writes=[r_pY] if h8 == 0 else (), awrites=() if h8 == 0 else [r_pY])
                yield
            for g2 in range(2):
                g = hh * 2 + g2
                S.op("pe", lambda e: e.matmul(pO[:, g2 * 256:(g2 + 1) * 256], lhsT=xact[:, 12 + g, :], rhs=prev_b[:, g * 256:(g + 1) * 256], start=True, stop=True),
                     reads=[r_xact, r_pb], writes=[r_pO] if g2 == 0 else (), awrites=() if g2 == 0 else [r_pO])
                yield
            for g2 in range(2):
                g = hh * 2 + g2
                S.op("pe", lambda e: e.matmul(pS[:, g2 * 256:(g2 + 1) * 256], lhsT=B_tok[:, g * 128:(g + 1) * 128], rhs=xdd[:, g * 256:(g + 1) * 256], start=True, stop=True),
                     reads=[r_Bt, r_xdd], writes=[r_pS] if g2 == 0 else (), awrites=() if g2 == 0 else [r_pS])
                yield
            hs = slice(hh * 8, hh * 8 + 8)

            def v8(ap):
                return ap.rearrange("p (h q) -> p h q", q=64)
            ex8 = sm[:, 0, hs].unsqueeze(2).to_broadcast([128, 8, 64])
            cd8 = sm[:, 2, hs].unsqueeze(2).to_broadcast([128, 8, 64])
            ds8 = dskip_bc[:, hs].unsqueeze(2).to_broadcast([128, 8, 64])
            S.op("dve", lambda e: e.tensor_tensor(out=v8(t1[:, c0:c0 + 512]), in0=v8(pO[:]), in1=ex8, op=ALU.mult), reads=[r_pO, r_sm], writes=[r_t1] if hh == 0 else (), awrites=() if hh == 0 else [r_t1])
            yield
            S.op("dve", lambda e: e.tensor_tensor(out=t1[:, c0:c0 + 512], in0=t1[:, c0:c0 + 512], in1=pY[:], op=ALU.add), reads=[r_t1, r_pY], awrites=[r_t1])
            yield
            S.op("pool", lambda e: e.tensor_tensor(out=v8(t2[:, c0:c0 + 512]), in0=v8(xs_tok[:, c0:c0 + 512]), in1=ds8, op=ALU.mult), reads=[r_xs, r_dskip], writes=[r_t2] if hh == 0 else (), awrites=() if hh == 0 else [r_t2])
            yield
            S.op("dve", lambda e: e.tensor_tensor(out=v8(prev_f[:, c0:c0 + 512]), in0=v8(prev_f[:, c0:c0 + 512]), in1=cd8, op=ALU.mult), reads=[r_pf, r_sm], awrites=[r_pf])
            yield
            S.op("dve", lambda e: e.tensor_tensor(out=prev_f[:, c0:c0 + 512], in0=prev_f[:, c0:c0 + 512], in1=pS[:], op=ALU.add), reads=[r_pf, r_pS], awrites=[r_pf])
            yield
            S.op("act", lambda e: e.activation(out=prev_b[:, c0:c0 + 512], in_=prev_f[:, c0:c0 + 512], func=AF.Copy), reads=[r_pf, r_pO], awrites=[r_pb])
            yield
        S.op("pool", lambda e: e.tensor_tensor(out=t1[:], in0=t1[:], in1=t2[:], op=ALU.add), reads=[r_t1, r_t2], writes=[r_t1])
        yield
        S.op("pool", lambda e: e.tensor_tensor(out=t1[:], in0=t1[:], in1=z_t[:], op=ALU.mult), reads=[r_t1, r_z], writes=[r_t1])
        yield
        for g in range(4):
            S.op("act", lambda e: e.activation(out=junk[:], in_=t1[:, g * 256:(g + 1) * 256], func=AF.Square, accum_out=sm[:, 5, g:g + 1]),
                 reads=[r_t1], writes=[r_junk], awrites=[r_sm])
            yield
        S.op("act", lambda e: e.activation(out=sm[:, 5, 4:8], in_=sm[:, 5, 0:4], func=AF.Sqrt, scale=1.0 / 256.0, bias=EPS), reads=[r_sm], awrites=[r_sm])
        yield
        S.op("dve", lambda e: e.reciprocal(out=sm[:, 5, 8:12], in_=sm[:, 5, 4:8]), reads=[r_sm], awrites=[r_sm])
        yield
        S.op("dve", lambda e: e.tensor_tensor(out=t1[:].rearrange("p (g q) -> p g q", g=4), in0=t1[:].rearrange("p (g q) -> p g q", g=4),
                                             in1=sm[:, 5, 8:12].unsqueeze(2).to_broadcast([128, 4, 256]), op=ALU.mult), reads=[r_t1, r_sm], writes=[r_t1])
        yield
        S.op("dve", lambda e: e.tensor_tensor(out=ob[:], in0=t1[:], in1=normw_bc[:], op=ALU.mult), reads=[r_t1, r_normw], writes=[r_ob])
        yield
        pT, r_pT = bank[4]
        pT_b = pT[:].bitcast(BF16)
        for k in range(8):
            S.op("pe", lambda e: e.transpose(out=pT_b[:, k * 128:(k + 1) * 128], in_=ob[:, k * 128:(k + 1) * 128], identity=ident_b[:]),
                 reads=[r_ob, r_identb], writes=[r_pT] if k == 0 else (), awrites=() if k == 0 else [r_pT])
            yield
        o_t, r_o = obT[sl2]
        S.op("act", lambda e: e.activation(out=o_t[:].rearrange("p k t -> p (k t)"), in_=pT_b, func=AF.Copy), reads=[r_pT], writes=[r_o])
        yield
        S.dma("sp", obT_d[:, :, tok0:tok0 + 128].rearrange("c p t -> p c t"), o_t[:], reads=[r_o], awrites=[r_obT])
        yield

    load_chunk(0, 0, 0)
    load_chunk(0, 1, 1)
    interleave((chunk_gen(ci) for ci in range(nch)), B_STAGGER)
    C.pop()


def _t5_bucket_np(rel):
    half, max_exact = 16, 8
    ret = (rel > 0).astype(np.int32) * half
    n = np.abs(rel)
    nf = np.maximum(n, 1).astype(np.float32)
    large = max_exact + (np.log(nf / np.float32(max_exact)) / np.float32(np.log(128.0 / 8.0)) * np.float32(half - max_exact)).astype(np.int32)
    large = np.minimum(large, half - 1)
    return ret + np.where(n < max_exact, n, large)


def _t5_blocks(rel_bias):
    k = np.arange(128)[:, None]
    q = np.arange(128)[None, :]
    out = np.zeros((128, 2, 8, 128), np.float32)
    for dl in range(2):
        bk = _t5_bucket_np((k - 128 * dl) - q)
        for h in range(8):
            out[:, dl, h, :] = rel_bias[bk, h]
    return out


def host_inputs(inputs, core):
    b0 = core * NB
    f = lambda a: np.ascontiguousarray(a, dtype=np.float32)
    c = inputs["c"][b0:b0 + NB]
    m = {
        "x": f(inputs["x"][b0:b0 + NB].reshape(NTOK, D)),
        "cT": f(c.reshape(NB, 8, 128).transpose(2, 1, 0)),
        "w_mod": f(inputs["w_mod"][0]),
        "b_mod": f(inputs["b_mod"][0].reshape(1, -1)),
        "w_in": f(inputs["w_in"][0]),
        "ident": np.eye(128, dtype=np.float32),
        "kv_norm_w": f(inputs["kv_norm_w"][0].reshape(1, -1)),
        "idx_k_norm_w": f(inputs["idx_k_norm_w"][0].reshape(1, -1)),
        "idx_k_norm_b": f(inputs["idx_k_norm_b"][0].reshape(1, -1)),
        "dt_bias": f(inputs["dt_bias"][0].reshape(1, -1)),
        "convw": f(inputs["conv_w"][0].reshape(4, 16, 128).transpose(2, 1, 0)),
        "convb": f(inputs["conv_b"][0].reshape(16, 128).T),
        "a_log": f(inputs["a_log"][0].reshape(1, -1)),
        "d_skip": f(inputs["d_skip"][0].reshape(1, -1)),
        "ssm_norm_w": f(inputs["ssm_norm_w"][0].reshape(1, -1)),
        "w_proj_a": f(inputs["w_proj_a"][0]), "w_proj_b": f(inputs["w_proj_b"][0]), "w_out": f(inputs["w_out"][0]),
        "ln1_g": f(inputs["ln1_g"][0].reshape(1, -1)), "ln1_b": f(inputs["ln1_b"][0].reshape(1, -1)),
        "ln2_g": f(inputs["ln2_g"][0].reshape(1, -1)), "ln2_b": f(inputs["ln2_b"][0].reshape(1, -1)),
        "w_router": f(inputs["w_router"][0]), "b_router": f(inputs["b_router"][0].reshape(1, -1)),
        "w1": f(inputs["w1"][0].reshape(NE * D, 2 * D)), "w2": f(inputs["w2"][0].reshape(NE * D, D)),
        "b1r": f(inputs["b1"][0].reshape(NE, 16, 128).transpose(0, 2, 1).reshape(NE * 128, 16)),
        "b2": f(inputs["b2"][0]),
        "sut": np.triu(np.ones((128, 128), np.float32), 1),
        "thr16": np.tile(512.0 * np.arange(16, dtype=np.float32), NE).reshape(1, -1),
        "bstart": (512.0 * np.arange(NBLK, dtype=np.float32)).reshape(1, -1),
        "kp": (np.arange(8, dtype=np.float32)[None, :] * 128 + np.arange(128, dtype=np.float32)[:, None]),
        "pcol": np.arange(128, dtype=np.float32).reshape(128, 1),
        "sut32": np.triu(np.ones((NE, NE), np.float32), 1),
        "tz": _t5_blocks(f(inputs["rel_bias"])),
        "cfar": f(inputs["rel_bias"][15:16, :]),
        "triU": np.triu(np.ones((128, 128), np.float32)),
        "SL": np.tril(np.ones((128, 128), np.float32), -1),
        "negm4": np.tile(np.tril(np.full((128, 128), NEG, np.float32), -1), (1, 4)),
    }
    return m


def kernel(**inputs):
    nc = build_program()
    in_maps = [host_inputs(inputs, c) for c in range(NCORES)]
    res = run_bass_kernel_spmd(nc, in_maps, core_ids=list(range(NCORES)))
    out = np.stack([np.asarray(r["out"]).reshape(NB, SEQ, D) for r in res.results], 0)
    return out.reshape(NCORES * NB, SEQ, D).astype(np.float32)
```

```python
import numpy as np
import concourse.bass as bass
import concourse.mybir as mybir
from concourse.bass_utils import run_bass_kernel_spmd

F32 = mybir.dt.float32
BF16 = mybir.dt.bfloat16
I32 = mybir.dt.int32
ALU = mybir.AluOpType
AF = mybir.ActivationFunctionType
AX = mybir.AxisListType

NCORES = 8
SEQ = 2048
D = 1024
NB = 4
NTOK = NB * SEQ
NT = NTOK // 128
TPS = SEQ // 128
DIN = 6872
C_Q, C_KV, C_IQ, C_IK, C_IW, C_Z, C_XBC, C_DT, C_GA, C_GB = 0, 1024, 1152, 1664, 1728, 1736, 2760, 4808, 4824, 5848
NE = 32
NBLK = NTOK * 4 // 512 + NE
ALPHA = 2.0 ** 0.25
EPS = 1e-5
NEG = -30000.0

B_STAGGER = 104
ENGS = ("pe", "act", "dve", "pool", "sp")


class Res:
    __slots__ = ("name", "writers", "readers", "dsem", "dcount", "dram")

    def __init__(self, name):
        self.name = name
        self.dram = False
        self.writers = {}
        self.readers = {}
        self.dsem = None
        self.dcount = 0


class Sched:
    def __init__(self, nc):
        self.nc = nc
        self.eng = {"pe": nc.tensor, "act": nc.scalar, "dve": nc.vector,
                    "pool": nc.gpsimd, "sp": nc.sync}
        self.sem = {e: nc.alloc_semaphore("prog_" + e) for e in ENGS}
        self.cnt = {e: 0 for e in ENGS}
        self.waited = {e: {} for e in ENGS}
        self.all_res = []
        self.nwaits = 0
        self.nops = 0
        self.sempool = []

    def retire(self, rs):
        for r in rs:
            if r.dsem is not None:
                self.sempool.append((r.dsem, r.dcount))
                r.dsem = None
            if r in self.all_res:
                self.all_res.remove(r)

    def res(self, name):
        r = Res(name)
        self.all_res.append(r)
        return r

    def _need(self, eng, tok, deps):
        sem, val = tok
        k = sem.num
        if self.waited[eng].get(k, 0) >= val:
            return
        if k not in deps or deps[k][1] < val:
            deps[k] = (sem, val)

    def op(self, eng, fn, reads=(), writes=(), awrites=(), dma=False):
        deps = {}
        mykey = None if dma else eng
        for r in reads:
            for k, tok in r.writers.items():
                if k == mykey and eng == "pe":
                    continue
                self._need(eng, tok, deps)
        for r in writes:
            for k, tok in list(r.writers.items()) + list(r.readers.items()):
                if k == mykey:
                    continue
                self._need(eng, tok, deps)
        for r in awrites:
            for k, tok in r.readers.items():
                if k == mykey:
                    continue
                self._need(eng, tok, deps)
        e = self.eng[eng]
        for k, (sem, val) in deps.items():
            e.wait_ge(sem, val)
            self.waited[eng][k] = val
            self.nwaits += 1
        ins = fn(e)
        self.nops += 1
        if dma:
            dst = (list(writes) + list(awrites))[0]
            if dst.dram:
                sb = [r for r in reads if not r.dram]
                if sb:
                    dst = sb[0]
            if dst.dsem is None:
                if self.sempool:
                    dst.dsem, dst.dcount = self.sempool.pop()
                else:
                    dst.dsem = self.nc.alloc_semaphore("d_" + dst.name)
            dst.dcount += 16
            ins.then_inc(dst.dsem, 16)
            tok = (dst.dsem, dst.dcount)
            key = "dma%d" % dst.dsem.num
        else:
            self.cnt[eng] += 1
            ins.then_inc(self.sem[eng], 1)
            tok = (self.sem[eng], self.cnt[eng])
            key = eng
        for r in reads:
            r.readers[key] = tok
        for r in writes:
            r.writers = {key: tok}
            r.readers = {}
        for r in awrites:
            r.writers[key] = tok
        return ins

    def dma(self, eng, out, in_, reads=(), writes=(), awrites=(), **kw):
        return self.op(eng, lambda e: e.dma_start(out=out, in_=in_, **kw),
                       reads=reads, writes=writes, awrites=awrites, dma=True)

    def barrier(self):
        toks = {}
        for e in ENGS:
            if self.cnt[e]:
                toks[self.sem[e].num] = (self.sem[e], self.cnt[e])
        for r in self.all_res:
            if r.dsem is not None and r.dcount:
                toks[r.dsem.num] = (r.dsem, r.dcount)
        for e in ENGS:
            for k, (sem, val) in toks.items():
                if self.waited[e].get(k, 0) >= val:
                    continue
                self.eng[e].wait_ge(sem, val)
                self.waited[e][k] = val
                self.nwaits += 1
        for r in self.all_res:
            r.writers = {}
            r.readers = {}


class Ctx:
    def __init__(self, nc, S):
        self.nc = nc
        self.S = S
        self.stack = []

    def push(self):
        self.stack.append([])

    def pop(self):
        self.S.barrier()
        gs = self.stack.pop()
        self.S.retire([r for (_, r) in gs])
        for g, _ in reversed(gs):
            g.__exit__(None, None, None)

    def sb(self, name, shape, dt):
        g = self.nc.sbuf_tensor("s_" + name, list(shape), dt)
        t = g.__enter__()
        r = self.S.res(name)
        self.stack[-1].append((g, r))
        return t, r

    def ps(self, name, shape, dt=F32):
        g = self.nc.psum_tensor("p_" + name, list(shape), dt)
        t = g.__enter__()
        r = self.S.res(name)
        self.stack[-1].append((g, r))
        return t, r

    def ring(self, kind, name, n, shape, dt):
        f = self.sb if kind == "sb" else self.ps
        return [f("%s%d" % (name, i), shape, dt) for i in range(n)]


def interleave(gens, stagger):
    active = []
    it = iter(gens)
    nxt = next(it, None)
    tick = 0
    while active or nxt is not None:
        if nxt is not None and tick % stagger == 0:
            active.append(nxt)
            nxt = next(it, None)
        for g in list(active):
            try:
                next(g)
            except StopIteration:
                active.remove(g)
        tick += 1


def build_program(debug=()):
    nc = bass.Bass("TRN2", target_bir_lowering=False)
    S = Sched(nc)
    C = Ctx(nc, S)
    dbg = set(debug)

    def din(name, shape, dt=F32):
        return nc.dram_tensor(name, list(shape), dt, kind="ExternalInput").ap()

    def scratch(name, shape, dt):
        kind = "ExternalOutput" if name in dbg else "Internal"
        r = S.res(name)
        r.dram = True
        return nc.dram_tensor(name, list(shape), dt, kind=kind).ap(), r

    x_d = din("x", [NTOK, D])
    cT_d = din("cT", [128, 8, NB])
    wmod_d = din("w_mod", [D, 6 * D])
    bmod_d = din("b_mod", [1, 6 * D])
    win_d = din("w_in", [D, DIN])
    ident_d = din("ident", [128, 128])
    kvw_d = din("kv_norm_w", [1, 128])
    ikw_d = din("idx_k_norm_w", [1, 64])
    ikb_d = din("idx_k_norm_b", [1, 64])
    dtb_d = din("dt_bias", [1, 16])
    convw_d = din("convw", [128, 16, 4])
    convb_d = din("convb", [128, 16])
    alog_d = din("a_log", [1, 16])
    dskip_d = din("d_skip", [1, 16])
    normw_d = din("ssm_norm_w", [1, D])
    triU_d = din("triU", [128, 128])
    SL_d = din("SL", [128, 128])
    negm4_d = din("negm4", [128, 512])
    tz_d = din("tz", [128, 2, 8, 128])
    cfar_d = din("cfar", [1, 8])
    wpa_d = din("w_proj_a", [D, D]); wpb_d = din("w_proj_b", [D, D]); wout_d = din("w_out", [D, D])
    ln1g_d = din("ln1_g", [1, D]); ln1b_d = din("ln1_b", [1, D]); ln2g_d = din("ln2_g", [1, D]); ln2b_d = din("ln2_b", [1, D])
    wr_d = din("w_router", [D, NE]); br_d = din("b_router", [1, NE])
    w1_d = din("w1", [NE * D, 2 * D]); w2_d = din("w2", [NE * D, D])
    b1r_d = din("b1r", [NE * 128, 16]); b2_d = din("b2", [NE, D])
    sut_d = din("sut", [128, 128]); thr16_d = din("thr16", [1, NE * 16]); bstart_d = din("bstart", [1, NBLK])
    kp_d = din("kp", [128, 8]); pcol_d = din("pcol", [128, 1]); sut32_d = din("sut32", [NE, NE])
    r_in = S.res("inputs")
    r_in.dram = True
    out_d = nc.dram_tensor("out", [NTOK, D], F32, kind="ExternalOutput").ap()
    r_out = S.res("out")
    r_out.dram = True

    mod_d, r_mod = scratch("mod_s", [NB, 6 * D], F32)
    qT_d, r_qT = scratch("qT_s", [8, 128, NTOK], BF16)
    iqT_d, r_iqT = scratch("iqT_s", [4, 128, NTOK], BF16)
    xbcT_d, r_xbcT = scratch("xbcT_s", [16, 128, NTOK], BF16)
    sgT_d, r_sgT = scratch("sgT_s", [16, 128, NTOK], BF16)
    kva_d, r_kva = scratch("kva_s", [NTOK, 136], BF16)
    kvT_d, r_kvT = scratch("kvT_s", [128, NTOK], BF16)
    ikT_d, r_ikT = scratch("ikT_s", [128, NTOK], BF16)
    iw_d, r_iw = scratch("iw_s", [NTOK, 8], F32)
    dt_d, r_dt = scratch("dt_s", [NTOK, 16], F32)
    zs_d, r_zs = scratch("zs_s", [NTOK, D], BF16)
    obT_d, r_obT = scratch("obT_s", [8, 128, NTOK], BF16)
    oaT_d, r_oaT = scratch("oaT_s", [8, 128, NTOK], BF16)
    mT_d, r_mTd = scratch("mT_s", [8, 128, NTOK], BF16)
    x1_d, r_x1 = scratch("x1_s", [NTOK, D], F32)
    u2_d, r_u2 = scratch("u2_s", [NTOK, D], BF16)
    xs_d, r_xsd = scratch("xsort_s", [NBLK * 512, D], BF16)
    ys_d, r_ysd = scratch("ysort_s", [NBLK * 512, D], BF16)

    w1b_d, r_w1bd = scratch("w1b_s", [NE * D, 2 * D], BF16)
    w2b_d, r_w2bd = scratch("w2b_s", [NE * D, D], BF16)

    C.push()
    ident_f, r_identf = C.sb("ident_f", [128, 128], F32)
    ident_b, r_identb = C.sb("ident_b", [128, 128], BF16)
    S.dma("sp", ident_f[:], ident_d, reads=[r_in], writes=[r_identf])
    S.op("dve", lambda e: e.tensor_copy(out=ident_b[:], in_=ident_f[:]), reads=[r_identf], writes=[r_identb])
    modT, r_modT = C.sb("modT", [128, 48, NB], F32)

    C.push()
    cT, r_cT = C.sb("cT", [128, 8, NB], F32)
    ones1, r_ones1 = C.sb("ones1", [1, NB], F32)
    bmod, r_bmod = C.sb("bmod", [1, 6 * D], F32)
    modrow, r_modrow = C.sb("modrow", [NB, 6 * D], F32)
    wm = C.ring("sb", "wm", 2, [128, 8, 512], F32)
    pmod = C.ring("ps", "pmod", 2, [NB, 512], F32)
    S.dma("sp", cT[:], cT_d, reads=[r_in], writes=[r_cT])
    S.dma("sp", bmod[:], bmod_d, reads=[r_in], writes=[r_bmod])
    S.op("act", lambda e: e.activation(out=cT[:], in_=cT[:], func=AF.Silu), reads=[r_cT], writes=[r_cT])
    S.op("dve", lambda e: e.memset(ones1[:], 1.0), writes=[r_ones1])
    for g in range(12):
        wt, r_wt = wm[g % 2]
        pt, r_pt = pmod[g % 2]
        S.dma("sp", wt[:], wmod_d[:, g * 512:(g + 1) * 512].rearrange("(k p) n -> p k n", p=128), reads=[r_in], writes=[r_wt])
        for k in range(8):
            S.op("pe", lambda e: e.matmul(pt[:], lhsT=cT[:, k, :], rhs=wt[:, k, :], start=(k == 0), stop=False),
                 reads=[r_cT, r_wt], writes=[r_pt] if k == 0 else (), awrites=() if k == 0 else [r_pt])
        S.op("pe", lambda e: e.matmul(pt[:], lhsT=ones1[:], rhs=bmod[:, g * 512:(g + 1) * 512], start=False, stop=True),
             reads=[r_ones1, r_bmod], awrites=[r_pt])
        S.op("act", lambda e: e.activation(out=modrow[:, g * 512:(g + 1) * 512], in_=pt[:], func=AF.Copy), reads=[r_pt], awrites=[r_modrow])
    S.dma("sp", mod_d, modrow[:], reads=[r_modrow], writes=[r_mod])
    pmt, r_pmt = pmod[0]
    pmt2, r_pmt2 = C.ps("pmodT", [128, 48 * NB], F32)
    for j in range(48):
        S.op("pe", lambda e: e.transpose(out=pmt2[:, j * NB:(j + 1) * NB], in_=modrow[:, j * 128:(j + 1) * 128], identity=ident_f[0:NB, 0:NB]),
             reads=[r_modrow, r_identf], writes=[r_pmt2] if j == 0 else (), awrites=() if j == 0 else [r_pmt2])
    S.op("act", lambda e: e.activation(out=modT[:].rearrange("p j b -> p (j b)"), in_=pmt2[:], func=AF.Copy), reads=[r_pmt2], writes=[r_modT])
    S.op("dve", lambda e: e.tensor_scalar_add(out=modT[:, 8:16, :], in0=modT[:, 8:16, :], scalar1=1.0), reads=[r_modT], awrites=[r_modT])
    S.op("dve", lambda e: e.tensor_scalar_add(out=modT[:, 32:40, :], in0=modT[:, 32:40, :], scalar1=1.0), reads=[r_modT], awrites=[r_modT])
    C.pop()
    if "stop0" in dbg:
        C.pop()
        return nc

    C.push()
    wI, r_wI = C.sb("wI", [128, 8, DIN], BF16)
    for i, (a, b_) in enumerate([(0, 1024), (1024, 1736), (1736, 2760), (2760, 3784), (3784, 4808), (4808, 5848), (5848, 6872)]):
        S.dma("pool", wI[:, :, a:b_], win_d[:, a:b_].rearrange("(k p) n -> p k n", p=128), reads=[r_in],
              writes=[r_wI] if i == 0 else (), awrites=() if i == 0 else [r_wI])
    kvw_bc, r_kvw = C.sb("kvw_bc", [128, 128], F32)
    ikw_bc, r_ikw = C.sb("ikw_bc", [128, 64], F32)
    ikb_bc, r_ikb = C.sb("ikb_bc", [128, 64], F32)
    dtb_bc, r_dtb = C.sb("dtb_bc", [128, 16], F32)
    S.dma("sp", kvw_bc[:], kvw_d.partition_broadcast(128), reads=[r_in], writes=[r_kvw])
    S.dma("sp", ikw_bc[:], ikw_d.partition_broadcast(128), reads=[r_in], writes=[r_ikw])
    S.dma("sp", ikb_bc[:], ikb_d.partition_broadcast(128), reads=[r_in], writes=[r_ikb])
    S.dma("sp", dtb_bc[:], dtb_d.partition_broadcast(128), reads=[r_in], writes=[r_dtb])

    xt = C.ring("sb", "xt", 2, [128, D], F32)
    xn = C.ring("sb", "xn", 2, [128, D], BF16)
    st = C.ring("sb", "st", 2, [128, 2, 6], F32)
    mv = C.ring("sb", "mv", 2, [128, 2], F32)
    rs = C.ring("sb", "rs", 2, [128, 1], F32)
    uT = C.ring("sb", "uT", 2, [128, 8, 512], BF16)
    ptr = C.ring("ps", "ptr", 1, [128, 8, 128], BF16)
    psm = C.ring("ps", "psm", 1, [128, 512], F32)
    pz = C.ring("ps", "pz", 2, [128, 512], F32)
    pf = C.ring("ps", "pf", 3, [128, 512], F32)
    pt2 = C.ring("ps", "pt2", 1, [128, 2, 128], BF16)
    kva = C.ring("sb", "kva", 2, [128, 136], BF16)
    ikn = C.ring("sb", "ikn", 2, [128, 128], BF16)
    sml = C.ring("sb", "sml", 2, [128, 64], F32)
    ikf = C.ring("sb", "ikf", 2, [128, 64], F32)
    iwt = C.ring("sb", "iwt", 2, [128, 8], F32)
    dtt = C.ring("sb", "dtt", 2, [128, 4, 16], F32)
    zst = C.ring("sb", "zst", 2, [128, D], BF16)
    tT = C.ring("sb", "tT", 2, [128, 2, 128], BF16)
    stg = C.ring("sb", "stg", 2, [128, 8, 512], BF16)
    for i in range(2):
        S.op("pool", lambda e: e.memset(kva[i][0][:, 128:136], 1.0), writes=[kva[i][1]])

    def ln_stats(src, r_src, i):
        st_t, r_st = st[i]
        mv_t, r_mv = mv[i]
        rs_t, r_rs = rs[i]
        for j in range(2):
            S.op("dve", lambda e: e.bn_stats(out=st_t[:, j, :], in_=src[:, j * 512:(j + 1) * 512]), reads=[r_src],
                 writes=[r_st] if j == 0 else (), awrites=() if j == 0 else [r_st])
        S.op("dve", lambda e: e.bn_aggr(out=mv_t[:], in_=st_t[:].rearrange("p a b -> p (a b)")), reads=[r_st], writes=[r_mv])
        S.op("act", lambda e: e.activation(out=rs_t[:], in_=mv_t[:, 1:2], func=AF.Sqrt, bias=EPS), reads=[r_mv], writes=[r_rs])
        S.op("dve", lambda e: e.reciprocal(out=rs_t[:], in_=rs_t[:]), reads=[r_rs], writes=[r_rs])
        return mv_t, r_mv, rs_t, r_rs

    tcount = 0
    for g in range(NTOK // 512):
        b = (g * 512) // SEQ
        u_t, r_u = uT[g % 2]
        for i4 in range(4):
            t = g * 4 + i4
            tok0 = t * 128
            ri = tcount % 2
            tcount += 1
            x_t, r_x = xt[ri]
            xn_t, r_xn = xn[ri]
            if t == 0:
                S.dma("sp", x_t[:], x_d[0:128, :], reads=[r_in], writes=[r_x])
            if t + 1 < NT:
                S.dma("sp", xt[(ri + 1) % 2][0][:], x_d[tok0 + 128:tok0 + 256, :], reads=[r_in], writes=[xt[(ri + 1) % 2][1]])
            mv_t, r_mv, rs_t, r_rs = ln_stats(x_t, r_x, ri)
            S.op("dve", lambda e: e.tensor_scalar(out=xn_t[:], in0=x_t[:], scalar1=mv_t[:, 0:1], scalar2=rs_t[:], op0=ALU.subtract, op1=ALU.mult),
                 reads=[r_x, r_mv, r_rs], writes=[r_xn])
            p_t, r_p = ptr[0]
            for k in range(8):
                S.op("pe", lambda e: e.transpose(out=p_t[:, k, :], in_=xn_t[:, k * 128:(k + 1) * 128], identity=ident_b[:]),
                     reads=[r_xn, r_identb], writes=[r_p] if k == 0 else (), awrites=() if k == 0 else [r_p])
            for k in range(8):
                S.op("act", lambda e: e.activation(out=u_t[:, k, i4 * 128:(i4 + 1) * 128], in_=p_t[:, k, :], func=AF.Identity,
                                                   scale=modT[:, 8 + k, b:b + 1], bias=modT[:, k, b:b + 1]),
                     reads=[r_p, r_modT], writes=[r_u] if (k == 0 and i4 == 0) else (), awrites=() if (k == 0 and i4 == 0) else [r_u])
            ps_t, r_ps = psm[0]
            for (c0, c1, o0) in [(C_KV, C_KV + 128, 0), (C_IK, C_IK + 72, 128), (C_DT, C_DT + 16, 200)]:
                for k in range(8):
                    S.op("pe", lambda e: e.matmul(ps_t[:, o0:o0 + (c1 - c0)], lhsT=u_t[:, k, i4 * 128:(i4 + 1) * 128], rhs=wI[:, k, c0:c1], start=(k == 0), stop=(k == 7)),
                         reads=[r_u, r_wI], writes=[r_ps] if (k == 0 and o0 == 0) else (), awrites=() if (k == 0 and o0 == 0) else [r_ps])
            zp = []
            for h in range(2):
                pz_t, r_pz = pz[h]
                zp.append((pz_t, r_pz))
                for k in range(8):
                    S.op("pe", lambda e: e.matmul(pz_t[:], lhsT=u_t[:, k, i4 * 128:(i4 + 1) * 128], rhs=wI[:, k, C_Z + h * 512:C_Z + (h + 1) * 512], start=(k == 0), stop=(k == 7)),
                         reads=[r_u, r_wI], writes=[r_pz] if k == 0 else (), awrites=() if k == 0 else [r_pz])
            sm_t, r_sm = sml[ri]
            kva_t, r_kva_t = kva[ri]
            ikf_t, r_ikf = ikf[ri]
            S.op("act", lambda e: e.activation(out=ikf_t[:, 0:64], in_=ps_t[:, 0:64], func=AF.Square, accum_out=sm_t[:, 0:1]), reads=[r_ps], writes=[r_ikf, r_sm])
            S.op("act", lambda e: e.activation(out=ikf_t[:, 0:64], in_=ps_t[:, 64:128], func=AF.Square, accum_out=sm_t[:, 1:2]), reads=[r_ps], writes=[r_ikf], awrites=[r_sm])
            S.op("dve", lambda e: e.tensor_tensor(out=sm_t[:, 0:1], in0=sm_t[:, 0:1], in1=sm_t[:, 1:2], op=ALU.add), reads=[r_sm], awrites=[r_sm])
            S.op("act", lambda e: e.activation(out=sm_t[:, 2:3], in_=sm_t[:, 0:1], func=AF.Sqrt, scale=1.0 / 128.0, bias=EPS), reads=[r_sm], awrites=[r_sm])
            S.op("dve", lambda e: e.reciprocal(out=sm_t[:, 3:4], in_=sm_t[:, 2:3]), reads=[r_sm], awrites=[r_sm])
            S.op("dve", lambda e: e.scalar_tensor_tensor(out=kva_t[:, 0:128], in0=ps_t[:, 0:128], scalar=sm_t[:, 3:4], in1=kvw_bc[:], op0=ALU.mult, op1=ALU.mult),
                 reads=[r_ps, r_sm, r_kvw], awrites=[r_kva_t])
            S.dma("sp", kva_d[tok0:tok0 + 128, :], kva_t[:], reads=[r_kva_t], awrites=[r_kva])
            ik_t, r_ik = ikn[ri]
            S.op("dve", lambda e: e.bn_stats(out=sm_t[:, 8:14], in_=ps_t[:, 128:192]), reads=[r_ps], awrites=[r_sm])
            S.op("dve", lambda e: e.bn_aggr(out=sm_t[:, 16:18], in_=sm_t[:, 8:14]), reads=[r_sm], awrites=[r_sm])
            S.op("act", lambda e: e.activation(out=sm_t[:, 18:19], in_=sm_t[:, 17:18], func=AF.Sqrt, bias=EPS), reads=[r_sm], awrites=[r_sm])
            S.op("dve", lambda e: e.reciprocal(out=sm_t[:, 19:20], in_=sm_t[:, 18:19]), reads=[r_sm], awrites=[r_sm])
            S.op("dve", lambda e: e.tensor_scalar(out=ikf_t[:], in0=ps_t[:, 128:192], scalar1=sm_t[:, 16:17], scalar2=sm_t[:, 19:20], op0=ALU.subtract, op1=ALU.mult),
                 reads=[r_ps, r_sm], writes=[r_ikf])
            S.op("dve", lambda e: e.tensor_tensor(out=ikf_t[:], in0=ikf_t[:], in1=ikw_bc[:], op=ALU.mult), reads=[r_ikf, r_ikw], writes=[r_ikf])
            S.op("dve", lambda e: e.tensor_tensor(out=ik_t[:, 0:64], in0=ikf_t[:], in1=ikb_bc[:], op=ALU.add), reads=[r_ikf, r_ikb], writes=[r_ik])
            S.op("dve", lambda e: e.tensor_copy(out=ik_t[:, 64:128], in_=ik_t[:, 0:64]), reads=[r_ik], awrites=[r_ik])
            iw_t, r_iwt = iwt[ri]
            S.op("act", lambda e: e.mul(out=iw_t[:], in_=ps_t[:, 192:200], mul=float(8 ** -0.5 * 64 ** -0.5)), reads=[r_ps], writes=[r_iwt])
            S.dma("sp", iw_d[tok0:tok0 + 128, :], iw_t[:], reads=[r_iwt], awrites=[r_iw])
            d_t, r_d = dtt[ri]
            S.op("dve", lambda e: e.tensor_tensor(out=d_t[:, 0, :], in0=ps_t[:, 200:216], in1=dtb_bc[:], op=ALU.add), reads=[r_ps, r_dtb], writes=[r_d])
            S.op("act", lambda e: e.activation(out=d_t[:, 1, :], in_=d_t[:, 0, :], func=AF.Abs), reads=[r_d], awrites=[r_d])
            S.op("act", lambda e: e.activation(out=d_t[:, 1, :], in_=d_t[:, 1, :], func=AF.Exp, scale=-1.0), reads=[r_d], awrites=[r_d])
            S.op("act", lambda e: e.activation(out=d_t[:, 1, :], in_=d_t[:, 1, :], func=AF.Ln, bias=1.0), reads=[r_d], awrites=[r_d])
            S.op("dve", lambda e: e.scalar_tensor_tensor(out=d_t[:, 2, :], in0=d_t[:, 0, :], scalar=0.0, in1=d_t[:, 1, :], op0=ALU.max, op1=ALU.add), reads=[r_d], awrites=[r_d])
            S.dma("sp", dt_d[tok0:tok0 + 128, :], d_t[:, 2, :], reads=[r_d], awrites=[r_dt])
            z_t, r_z = zst[ri]
            for h in range(2):
                S.op("act", lambda e: e.activation(out=z_t[:, h * 512:(h + 1) * 512], in_=zp[h][0][:], func=AF.Silu), reads=[zp[h][1]],
                     writes=[r_z] if h == 0 else (), awrites=() if h == 0 else [r_z])
            S.dma("sp", zs_d[tok0:tok0 + 128, :], z_t[:], reads=[r_z], awrites=[r_zs])
            p2, r_p2 = pt2[0]
            t_t, r_t = tT[ri]
            S.op("pe", lambda e: e.transpose(out=p2[:, 0, :], in_=kva_t[:, 0:128], identity=ident_b[:]), reads=[r_kva_t, r_identb], writes=[r_p2])
            S.op("pe", lambda e: e.transpose(out=p2[:, 1, :], in_=ik_t[:], identity=ident_b[:]), reads=[r_ik, r_identb], awrites=[r_p2])
            S.op("dve", lambda e: e.tensor_copy(out=t_t[:], in_=p2[:]), reads=[r_p2], writes=[r_t])
            S.dma("sp", kvT_d[:, tok0:tok0 + 128], t_t[:, 0, :], reads=[r_t], awrites=[r_kvT])
            S.dma("sp", ikT_d[:, tok0:tok0 + 128], t_t[:, 1, :], reads=[r_t], awrites=[r_ikT])
        g0 = g * 512
        fcount = 0
        for (c0, nch, dst, r_dst, ch0, func, scl) in [
                (C_Q, 8, qT_d, r_qT, 0, AF.Copy, float(128 ** -0.5)),
                (C_IQ, 4, iqT_d, r_iqT, 0, AF.Copy, 1.0),
                (C_XBC, 8, xbcT_d, r_xbcT, 0, AF.Copy, 1.0),
                (C_XBC + 1024, 8, xbcT_d, r_xbcT, 8, AF.Copy, 1.0),
                (C_GA, 8, sgT_d, r_sgT, 0, AF.Sigmoid, 1.0),
                (C_GB, 8, sgT_d, r_sgT, 8, AF.Sigmoid, 1.0)]:
            sg_t, r_sg = stg[fcount % 2]
            fcount += 1
            for j in range(nch):
                pf_t, r_pf = pf[j % 3]
                for k in range(8):
                    S.op("pe", lambda e: e.matmul(pf_t[:], lhsT=wI[:, k, c0 + j * 128:c0 + (j + 1) * 128], rhs=u_t[:, k, :], start=(k == 0), stop=(k == 7)),
                         reads=[r_u, r_wI], writes=[r_pf] if k == 0 else (), awrites=() if k == 0 else [r_pf])
                if func == AF.Copy and j % 2 == 1:
                    S.op("dve", lambda e: e.tensor_scalar_mul(out=sg_t[:, j, :], in0=pf_t[:], scalar1=scl), reads=[r_pf],
                         writes=[r_sg] if j == 0 else (), awrites=() if j == 0 else [r_sg])
                else:
                    S.op("act", lambda e: e.activation(out=sg_t[:, j, :], in_=pf_t[:], func=func, scale=scl), reads=[r_pf],
                         writes=[r_sg] if j == 0 else (), awrites=() if j == 0 else [r_sg])
            S.dma("sp", dst[ch0:ch0 + nch, :, g0:g0 + 512].rearrange("c p t -> p c t"), sg_t[:, 0:nch, :], reads=[r_sg], awrites=[r_dst])
    C.pop()
    if "stopA" in dbg:
        C.pop()
        return nc

    phase_B(nc, S, C, dbg, locals())
    if "stopB" in dbg:
        C.pop()
        return nc

    phase_C(nc, S, C, dbg, locals())
    if "stopC" in dbg:
        C.pop()
        return nc

    phase_DEF(nc, S, C, dbg, locals())
    C.pop()
    return nc


def phase_DEF(nc, S, C, dbg, L):
    g_ = lambda n: L[n]
    r_in = g_("r_in"); ident_b = g_("ident_b"); r_identb = g_("r_identb"); ident_f = g_("ident_f"); r_identf = g_("r_identf")
    modT = g_("modT"); r_modT = g_("r_modT"); mod_d = g_("mod_d"); r_mod = g_("r_mod")
    x_d = g_("x_d"); oaT_d, r_oaT = g_("oaT_d"), g_("r_oaT"); obT_d, r_obT = g_("obT_d"), g_("r_obT"); sgT_d, r_sgT = g_("sgT_d"), g_("r_sgT")
    x1_d, r_x1 = g_("x1_d"), g_("r_x1"); u2_d, r_u2 = g_("u2_d"), g_("r_u2"); xs_d, r_xsd = g_("xs_d"), g_("r_xsd"); ys_d, r_ysd = g_("ys_d"), g_("r_ysd")
    out_d, r_out = g_("out_d"), g_("r_out")
    b1r_d, b2_d = g_("b1r_d"), g_("b2_d")
    w1b_d, r_w1bd, w2b_d, r_w2bd = g_("w1b_d"), g_("r_w1bd"), g_("w2b_d"), g_("r_w2bd")

    C.push()
    idx4, r_idx4 = C.sb("idx4", [128, NT, 4], I32)
    g4, r_g4 = C.sb("g4", [128, NT, 4], F32)
    widx, r_widx = C.sb("widx", [128, NBLK, 8], I32)
    bidx, r_bidx = C.sb("bidx", [128, NBLK], I32)
    eidx, r_eidx = C.sb("eidx", [128, NBLK], I32)

    def ln_stats(src, r_src, st_t, r_st, mv_t, r_mv, rs_t, r_rs):
        for j in range(2):
            S.op("dve", lambda e: e.bn_stats(out=st_t[:, j, :], in_=src[:, j * 512:(j + 1) * 512]), reads=[r_src],
                 writes=[r_st] if j == 0 else (), awrites=() if j == 0 else [r_st])
        S.op("dve", lambda e: e.bn_aggr(out=mv_t[:], in_=st_t[:].rearrange("p a b -> p (a b)")), reads=[r_st], writes=[r_mv])
        S.op("act", lambda e: e.activation(out=rs_t[:], in_=mv_t[:, 1:2], func=AF.Sqrt, bias=EPS), reads=[r_mv], writes=[r_rs])
        S.op("dve", lambda e: e.reciprocal(out=rs_t[:], in_=rs_t[:]), reads=[r_rs], writes=[r_rs])

    mT_d, r_mTd = g_("mT_d"), g_("r_mTd")
    C.push()
    wpa, r_wpa = C.sb("wpa", [128, 8, D], BF16)
    wpb, r_wpb = C.sb("wpb", [128, 8, D], BF16)
    S.dma("pool", wpa[:], g_("wpa_d").rearrange("(k p) n -> p k n", p=128), reads=[r_in], writes=[r_wpa])
    S.dma("pool", wpb[:], g_("wpb_d").rearrange("(k p) n -> p k n", p=128), reads=[r_in], writes=[r_wpb])
    oaTr = C.ring("sb", "oaTd", 2, [128, 8, 512], BF16)
    obTr = C.ring("sb", "obTd", 2, [128, 8, 512], BF16)
    sgTr = C.ring("sb", "sgTd", 2, [128, 16, 512], BF16)
    mTr = C.ring("sb", "mT", 2, [128, 8, 512], BF16)
    ta = C.ring("sb", "ta", 3, [128, 512], F32)
    tb = C.ring("sb", "tb", 3, [128, 512], F32)
    pab = C.ring("ps", "pab", 4, [128, 512], F32)
    pbb = C.ring("ps", "pbb", 4, [128, 512], F32)

    def loadD1(g):
        g0 = g * 512
        S.dma("sp", oaTr[g % 2][0][:], oaT_d[:, :, g0:g0 + 512].rearrange("c p t -> p c t"), reads=[r_oaT], writes=[oaTr[g % 2][1]])
        S.dma("sp", obTr[g % 2][0][:], obT_d[:, :, g0:g0 + 512].rearrange("c p t -> p c t"), reads=[r_obT], writes=[obTr[g % 2][1]])
        S.dma("sp", sgTr[g % 2][0][:], sgT_d[:, :, g0:g0 + 512].rearrange("c p t -> p c t"), reads=[r_sgT], writes=[sgTr[g % 2][1]])

    NG = NTOK // 512
    loadD1(0)
    cn = 0
    for g in range(NG):
        g0 = g * 512
        if g + 1 < NG:
            loadD1(g + 1)
        oaT, r_oaTs = oaTr[g % 2]; obT, r_obTs = obTr[g % 2]; sgT, r_sgTs = sgTr[g % 2]; mT, r_mT = mTr[g % 2]
        for n in range(8):
            pa, r_pa = pab[cn % 4]
            pb, r_pb = pbb[cn % 4]
            ta_t, r_ta = ta[cn % 3]
            tb_t, r_tb = tb[cn % 3]
            cn += 1
            for k in range(8):
                S.op("pe", lambda e: e.matmul(pa[:], lhsT=wpa[:, k, n * 128:(n + 1) * 128], rhs=oaT[:, k, :], start=(k == 0), stop=(k == 7)),
                     reads=[r_wpa, r_oaTs], writes=[r_pa] if k == 0 else (), awrites=() if k == 0 else [r_pa])
            for k in range(8):
                S.op("pe", lambda e: e.matmul(pb[:], lhsT=wpb[:, k, n * 128:(n + 1) * 128], rhs=obT[:, k, :], start=(k == 0), stop=(k == 7)),
                     reads=[r_wpb, r_obTs], writes=[r_pb] if k == 0 else (), awrites=() if k == 0 else [r_pb])
            S.op("dve", lambda e: e.tensor_tensor(out=ta_t[:], in0=pa[:], in1=sgT[:, n, :], op=ALU.mult), reads=[r_pa, r_sgTs], writes=[r_ta])
            S.op("dve", lambda e: e.tensor_tensor(out=tb_t[:], in0=pb[:], in1=sgT[:, 8 + n, :], op=ALU.mult), reads=[r_pb, r_sgTs], writes=[r_tb])
            S.op("pool", lambda e: e.tensor_tensor(out=mT[:, n, :], in0=ta_t[:], in1=tb_t[:], op=ALU.add), reads=[r_ta, r_tb],
                 writes=[r_mT] if n == 0 else (), awrites=() if n == 0 else [r_mT])
        S.dma("sp", mT_d[:, :, g0:g0 + 512].rearrange("c p t -> p c t"), mT[:], reads=[r_mT], awrites=[r_mTd])
    C.pop()

    C.push()
    wout, r_wout = C.sb("wout", [128, 8, D], BF16)
    S.dma("pool", wout[:], g_("wout_d").rearrange("(k p) n -> p k n", p=128), reads=[r_in], writes=[r_wout])
    wr, r_wr = C.sb("wr", [128, 8, NE], F32)
    S.dma("sp", wr[:], g_("wr_d").rearrange("(k p) n -> p k n", p=128), reads=[r_in], writes=[r_wr])
    br_bc, r_br = C.sb("br_bc", [128, NE], F32)
    S.dma("sp", br_bc[:], g_("br_d").partition_broadcast(128), reads=[r_in], writes=[r_br])
    ln1g, r_ln1g = C.sb("ln1g", [128, D], F32)
    ln1b, r_ln1b = C.sb("ln1b", [128, D], F32)
    S.dma("sp", ln1g[:], g_("ln1g_d").partition_broadcast(128), reads=[r_in], writes=[r_ln1g])
    S.dma("sp", ln1b[:], g_("ln1b_d").partition_broadcast(128), reads=[r_in], writes=[r_ln1b])
    gater = C.ring("sb", "gate1", 2, [128, D], F32)
    sc2r = C.ring("sb", "sc2", 2, [128, D], F32)
    sh2r = C.ring("sb", "sh2", 2, [128, D], F32)
    sutf, r_sutf = C.sb("sutf", [128, 128], F32)
    sutb, r_sutb = C.sb("sutb", [128, 128], BF16)
    onesb, r_onesb = C.sb("onesb", [128, 128], BF16)
    S.dma("sp", sutf[:], g_("sut_d"), reads=[r_in], writes=[r_sutf])
    S.op("dve", lambda e: e.tensor_copy(out=sutb[:], in_=sutf[:]), reads=[r_sutf], writes=[r_sutb])
    S.op("dve", lambda e: e.memset(onesb[:], 1.0), writes=[r_onesb])
    maskall, r_maskall = C.sb("maskall", [128, NT, NE], F32)
    gall, r_gall = C.sb("gall", [128, NT, NE], F32)
    posall, r_posall = C.sb("posall", [128, NT, NE], F32)
    rrun, r_rrun = C.sb("rrun", [128, NE], F32)
    S.op("dve", lambda e: e.memset(rrun[:], 0.0), writes=[r_rrun])
    mtr = C.ring("sb", "mtl", 4, [128, 8, 128], BF16)
    xt = C.ring("sb", "xtd", 4, [128, D], F32)
    r1_r = C.ring("sb", "r1", 3, [128, D], F32)
    x1t_r = C.ring("sb", "x1t", 3, [128, D], F32)
    u2t_r = C.ring("sb", "u2t", 3, [128, D], F32)
    u2b = C.ring("sb", "u2b", 3, [128, D], BF16)
    u2T_r = C.ring("sb", "u2T", 3, [128, 8, 128], F32)
    st_r = C.ring("sb", "stD", 6, [128, 2, 6], F32)
    mv_r = C.ring("sb", "mvD", 6, [128, 2], F32)
    rs_r = C.ring("sb", "rsD", 6, [128, 1], F32)
    lg_r = C.ring("sb", "lg", 3, [128, NE], F32)
    m8_r = C.ring("sb", "m8D", 3, [128, 8], F32)
    sml_r = C.ring("sb", "smlD", 3, [128, 8], F32)
    ex_r = C.ring("sb", "exD", 3, [128, NE], F32)
    maskb_r = C.ring("sb", "maskb", 3, [128, NE], BF16)
    prs = C.ring("ps", "prs", 4, [128, 512], F32)
    ptT = C.ring("ps", "ptT", 2, [128, 512], F32)
    psl = C.ring("ps", "psl", 2, [128, 512], F32)

    def loadD2(t):
        tok0 = t * 128
        S.dma("sp", xt[t % 4][0][:], x_d[tok0:tok0 + 128, :], reads=[r_in], writes=[xt[t % 4][1]])
        S.dma("sp", mtr[t % 4][0][:], mT_d[:, :, tok0:tok0 + 128].rearrange("c p t -> p c t"), reads=[r_mTd], writes=[mtr[t % 4][1]])
        if t % TPS == 0:
            b = t // TPS
            gate1, r_gate1 = gater[b % 2]; sc2, r_sc2 = sc2r[b % 2]; sh2, r_sh2 = sh2r[b % 2]
            S.dma("sp", gate1[:], mod_d[b:b + 1, 2 * D:3 * D].partition_broadcast(128), reads=[r_mod], writes=[r_gate1])
            S.dma("sp", sh2[:], mod_d[b:b + 1, 3 * D:4 * D].partition_broadcast(128), reads=[r_mod], writes=[r_sh2])
            S.dma("sp", sc2[:], mod_d[b:b + 1, 4 * D:5 * D].partition_broadcast(128), reads=[r_mod], writes=[r_sc2])
            S.op("pool", lambda e: e.tensor_scalar_add(out=sc2[:], in0=sc2[:], scalar1=1.0), reads=[r_sc2], writes=[r_sc2])

    def stage1(t):
        tok0 = t * 128
        b = t // TPS
        gate1, r_gate1 = gater[b % 2]; sc2, r_sc2 = sc2r[b % 2]; sh2, r_sh2 = sh2r[b % 2]
        x_t, r_x = xt[t % 4]
        mt_t, r_mt = mtr[t % 4]
        r1, r_r1 = r1_r[t % 3]; x1t, r_x1t = x1t_r[t % 3]; u2t, r_u2t = u2t_r[t % 3]
        st, r_st = st_r[(2 * t) % 6]; mv, r_mv = mv_r[(2 * t) % 6]; rs, r_rs = rs_r[(2 * t) % 6]
        st2, r_st2 = st_r[(2 * t + 1) % 6]; mv2, r_mv2 = mv_r[(2 * t + 1) % 6]; rs2, r_rs2 = rs_r[(2 * t + 1) % 6]
        for hf in range(2):
            pr_t, r_pr = prs[(2 * t + hf) % 4]
            for n in range(8):
                S.op("pe", lambda e: e.matmul(pr_t[:], lhsT=mt_t[:, n, :], rhs=wout[:, n, hf * 512:(hf + 1) * 512], start=(n == 0), stop=(n == 7)),
                     reads=[r_mt, r_wout], writes=[r_pr] if n == 0 else (), awrites=() if n == 0 else [r_pr])
                yield
            S.op("dve", lambda e: e.tensor_tensor(out=r1[:, hf * 512:(hf + 1) * 512], in0=pr_t[:], in1=gate1[:, hf * 512:(hf + 1) * 512], op=ALU.mult),
                 reads=[r_pr, r_gate1], writes=[r_r1] if hf == 0 else (), awrites=() if hf == 0 else [r_r1])
            yield
        S.op("dve", lambda e: e.scalar_tensor_tensor(out=r1[:], in0=x_t[:], scalar=float(ALPHA), in1=r1[:], op0=ALU.mult, op1=ALU.add), reads=[r_x, r_r1], writes=[r_r1])
        yield
        ln_stats(r1, r_r1, st, r_st, mv, r_mv, rs, r_rs)
        yield
        S.op("dve", lambda e: e.tensor_scalar(out=x1t[:], in0=r1[:], scalar1=mv[:, 0:1], scalar2=rs[:], op0=ALU.subtract, op1=ALU.mult), reads=[r_r1, r_mv, r_rs], writes=[r_x1t])
        yield
        S.op("pool", lambda e: e.tensor_tensor(out=x1t[:], in0=x1t[:], in1=ln1g[:], op=ALU.mult), reads=[r_x1t, r_ln1g], writes=[r_x1t])
        yield
        S.op("pool", lambda e: e.tensor_tensor(out=x1t[:], in0=x1t[:], in1=ln1b[:], op=ALU.add), reads=[r_x1t, r_ln1b], writes=[r_x1t])
        yield
        S.dma("sp", x1_d[tok0:tok0 + 128, :], x1t[:], reads=[r_x1t], awrites=[r_x1])
        yield
        ln_stats(x1t, r_x1t, st2, r_st2, mv2, r_mv2, rs2, r_rs2)
        yield
        S.op("dve", lambda e: e.tensor_scalar(out=u2t[:], in0=x1t[:], scalar1=mv2[:, 0:1], scalar2=rs2[:], op0=ALU.subtract, op1=ALU.mult), reads=[r_x1t, r_mv2, r_rs2], writes=[r_u2t])
        yield
        S.op("pool", lambda e: e.tensor_tensor(out=u2t[:], in0=u2t[:], in1=sc2[:], op=ALU.mult), reads=[r_u2t, r_sc2], writes=[r_u2t])
        yield
        S.op("pool", lambda e: e.tensor_tensor(out=u2t[:], in0=u2t[:], in1=sh2[:], op=ALU.add), reads=[r_u2t, r_sh2], writes=[r_u2t])
        yield
        ub, r_ub = u2b[t % 3]
        S.op("act", lambda e: e.activation(out=ub[:], in_=u2t[:], func=AF.Copy), reads=[r_u2t], writes=[r_ub])
        yield
        S.dma("sp", u2_d[tok0:tok0 + 128, :], ub[:], reads=[r_ub], awrites=[r_u2])
        yield

    def stage2(t):
        u2t, r_u2t = u2t_r[t % 3]
        u2T, r_u2T = u2T_r[t % 3]
        lg, r_lg = lg_r[t % 3]; m8, r_m8 = m8_r[t % 3]; sml, r_sml = sml_r[t % 3]; ex, r_ex = ex_r[t % 3]; maskb, r_maskb = maskb_r[t % 3]
        for hf in range(2):
            pT, r_pT = (ptT[t % 2] if hf == 0 else psl[t % 2])
            for k4 in range(4):
                k = hf * 4 + k4
                S.op("pe", lambda e: e.transpose(out=pT[:, k4 * 128:(k4 + 1) * 128], in_=u2t[:, k * 128:(k + 1) * 128], identity=ident_f[:]),
                     reads=[r_u2t, r_identf], writes=[r_pT] if k4 == 0 else (), awrites=() if k4 == 0 else [r_pT])
                yield
            S.op("act", lambda e: e.activation(out=u2T[:, hf * 4:hf * 4 + 4, :].rearrange("p k t -> p (k t)"), in_=pT[:], func=AF.Copy), reads=[r_pT],
                 writes=[r_u2T] if hf == 0 else (), awrites=() if hf == 0 else [r_u2T])
            yield
        pl_, r_pl = psl[t % 2]
        for k in range(8):
            S.op("pe", lambda e: e.matmul(pl_[:, 0:NE], lhsT=u2T[:, k, :], rhs=wr[:, k, :], start=(k == 0), stop=(k == 7)),
                 reads=[r_u2T, r_wr], writes=[r_pl] if k == 0 else (), awrites=() if k == 0 else [r_pl])
            yield
        S.op("dve", lambda e: e.tensor_tensor(out=lg[:], in0=pl_[:, 0:NE], in1=br_bc[:], op=ALU.add), reads=[r_pl, r_br], writes=[r_lg])
        yield
        S.op("dve", lambda e: e.max(out=m8[:], in_=lg[:]), reads=[r_lg], writes=[r_m8])
        yield
        S.op("dve", lambda e: e.tensor_scalar(out=maskall[:, t, :], in0=lg[:], scalar1=m8[:, 3:4], scalar2=None, op0=ALU.is_ge), reads=[r_lg, r_m8], awrites=[r_maskall])
        yield
        S.op("dve", lambda e: e.tensor_scalar_mul(out=sml[:, 0:1], in0=m8[:, 0:1], scalar1=-1.0), reads=[r_m8], writes=[r_sml])
        yield
        S.op("act", lambda e: e.activation(out=ex[:], in_=lg[:], func=AF.Exp, bias=sml[:, 0:1]), reads=[r_lg, r_sml], writes=[r_ex])
        yield
        S.op("dve", lambda e: e.tensor_tensor(out=ex[:], in0=ex[:], in1=maskall[:, t, :], op=ALU.mult), reads=[r_ex, r_maskall], writes=[r_ex])
        yield
        S.op("dve", lambda e: e.reduce_sum(out=sml[:, 1:2], in_=ex[:], axis=AX.X), reads=[r_ex], awrites=[r_sml])
        yield
        S.op("dve", lambda e: e.reciprocal(out=sml[:, 2:3], in_=sml[:, 1:2]), reads=[r_sml], awrites=[r_sml])
        yield
        S.op("dve", lambda e: e.tensor_scalar(out=gall[:, t, :], in0=ex[:], scalar1=sml[:, 2:3], scalar2=None, op0=ALU.mult), reads=[r_ex, r_sml], awrites=[r_gall])
        yield
        S.op("dve", lambda e: e.tensor_copy(out=maskb[:], in_=maskall[:, t, :]), reads=[r_maskall], writes=[r_maskb])
        yield
        S.op("pe", lambda e: e.matmul(pl_[:, 64:64 + NE], lhsT=sutb[:], rhs=maskb[:], start=True, stop=True), reads=[r_sutb, r_maskb, r_lg], awrites=[r_pl])
        yield
        S.op("pe", lambda e: e.matmul(pl_[:, 128:128 + NE], lhsT=onesb[:], rhs=maskb[:], start=True, stop=True), reads=[r_onesb, r_maskb], awrites=[r_pl])
        yield
        S.op("dve", lambda e: e.tensor_tensor(out=posall[:, t, :], in0=pl_[:, 64:64 + NE], in1=rrun[:], op=ALU.add), reads=[r_pl, r_rrun], awrites=[r_posall])
        yield
        S.op("dve", lambda e: e.tensor_tensor(out=rrun[:], in0=pl_[:, 128:128 + NE], in1=rrun[:], op=ALU.add), reads=[r_pl, r_rrun], writes=[r_rrun])
        yield


    def tile_gen(t):
        if t + 3 < NT:
            loadD2(t + 3)
        yield from stage1(t)
        yield from stage2(t)

    for t0 in range(3):
        loadD2(t0)
    interleave((tile_gen(t) for t in range(NT)), 26)

    thr16, r_thr16 = C.sb("thr16", [128, NE, 16], F32)
    bstart, r_bstart = C.sb("bstart", [128, NBLK], F32)
    kp, r_kp = C.sb("kp", [128, 8], F32)
    pcol, r_pcol = C.sb("pcol", [128, 1], F32)
    sut32, r_sut32 = C.sb("sut32", [NE, NE], F32)
    S.dma("sp", thr16[:].rearrange("p e m -> p (e m)"), g_("thr16_d").partition_broadcast(128), reads=[r_in], writes=[r_thr16])
    S.dma("sp", bstart[:], g_("bstart_d").partition_broadcast(128), reads=[r_in], writes=[r_bstart])
    S.dma("sp", kp[:], g_("kp_d"), reads=[r_in], writes=[r_kp])
    S.dma("sp", pcol[:], g_("pcol_d"), reads=[r_in], writes=[r_pcol])
    S.dma("sp", sut32[:], g_("sut32_d"), reads=[r_in], writes=[r_sut32])
    big, r_big = C.sb("bigD", [128, NBLK * NE], F32)
    nbk, r_nbk = C.sb("nbk", [128, NE], F32)
    padT, r_padT = C.sb("padT", [NE, 128], F32)
    pstart, r_pstart = C.sb("pstart", [128, NE], F32)
    pend, r_pend = C.sb("pend", [128, NE], F32)
    bexp, r_bexp = C.sb("bexp", [128, NBLK], F32)
    wf, r_wf = C.sb("wf", [128, NBLK, 8], F32)
    S.op("dve", lambda e: e.tensor_tensor(out=big[:, 0:NE * 16].rearrange("p (e m) -> p e m", m=16), in0=rrun[:].unsqueeze(2).to_broadcast([128, NE, 16]), in1=thr16[:], op=ALU.is_gt),
         reads=[r_rrun, r_thr16], writes=[r_big])
    S.op("dve", lambda e: e.tensor_reduce(out=nbk[:], in_=big[:, 0:NE * 16].rearrange("p (e m) -> p e m", m=16), axis=AX.X, op=ALU.add), reads=[r_big], writes=[r_nbk])
    S.op("dve", lambda e: e.tensor_scalar_mul(out=nbk[:], in0=nbk[:], scalar1=512.0), reads=[r_nbk], writes=[r_nbk])
    pq, r_pq = psl[0]
    S.op("pe", lambda e: e.transpose(out=pq[0:NE, 0:128], in_=nbk[:], identity=ident_f[:]), reads=[r_nbk, r_identf], writes=[r_pq])
    S.op("act", lambda e: e.activation(out=padT[:], in_=pq[0:NE, 0:128], func=AF.Copy), reads=[r_pq], writes=[r_padT])
    S.op("pe", lambda e: e.matmul(pq[:, 256:256 + NE], lhsT=padT[:], rhs=sut32[:], start=True, stop=True), reads=[r_padT, r_sut32], awrites=[r_pq])
    S.op("act", lambda e: e.activation(out=pstart[:], in_=pq[:, 256:256 + NE], func=AF.Copy), reads=[r_pq], writes=[r_pstart])
    S.op("dve", lambda e: e.tensor_tensor(out=pend[:], in0=pstart[:], in1=nbk[:], op=ALU.add), reads=[r_pstart, r_nbk], writes=[r_pend])
    S.op("dve", lambda e: e.tensor_tensor(out=big[:].rearrange("p (i e) -> p i e", e=NE), in0=bstart[:].unsqueeze(2).to_broadcast([128, NBLK, NE]),
                                         in1=pend[:].unsqueeze(1).to_broadcast([128, NBLK, NE]), op=ALU.is_ge), reads=[r_bstart, r_pend], writes=[r_big])
    S.op("dve", lambda e: e.tensor_reduce(out=bexp[:], in_=big[:].rearrange("p (i e) -> p i e", e=NE), axis=AX.X, op=ALU.add), reads=[r_big], writes=[r_bexp])
    S.op("dve", lambda e: e.tensor_scalar_min(out=bexp[:], in0=bexp[:], scalar1=float(NE - 1)), reads=[r_bexp], writes=[r_bexp])
    S.op("dve", lambda e: e.tensor_copy(out=eidx[:], in_=bexp[:]), reads=[r_bexp], writes=[r_eidx])
    S.op("dve", lambda e: e.tensor_scalar(out=bidx[:], in0=bexp[:], scalar1=128.0, scalar2=pcol[:, 0:1], op0=ALU.mult, op1=ALU.add), reads=[r_bexp, r_pcol], writes=[r_bidx])
    S.op("dve", lambda e: e.tensor_scalar_mul(out=wf[:], in0=bexp[:].unsqueeze(2).to_broadcast([128, NBLK, 8]), scalar1=float(D)), reads=[r_bexp], writes=[r_wf])
    S.op("dve", lambda e: e.tensor_tensor(out=widx[:], in0=wf[:], in1=kp[:].unsqueeze(1).to_broadcast([128, NBLK, 8]), op=ALU.add), reads=[r_wf, r_kp], writes=[r_widx])
    sl_, r_sl = C.sb("slD", [128, NE], F32)
    eqt, r_eqt = C.sb("eqt", [128, NE], F32)
    m8b, r_m8b = C.sb("m8b", [128, 8], F32)
    S.op("dve", lambda e: e.tensor_scalar_add(out=pstart[:], in0=pstart[:], scalar1=1.0), reads=[r_pstart], writes=[r_pstart])
    for t in range(NT):
        tok0 = t * 128
        ub, r_ub = u2b[t % 2]
        S.dma("sp", ub[:], u2_d[tok0:tok0 + 128, :], reads=[r_u2], writes=[r_ub])
        S.op("dve", lambda e: e.tensor_tensor(out=sl_[:], in0=posall[:, t, :], in1=pstart[:], op=ALU.add), reads=[r_posall, r_pstart], writes=[r_sl])
        S.op("dve", lambda e: e.tensor_tensor(out=sl_[:], in0=sl_[:], in1=maskall[:, t, :], op=ALU.mult), reads=[r_sl, r_maskall], writes=[r_sl])
        S.op("dve", lambda e: e.max(out=m8b[:], in_=sl_[:]), reads=[r_sl], writes=[r_m8b])
        for j in range(4):
            S.op("dve", lambda e: e.scalar_tensor_tensor(out=eqt[:], in0=sl_[:], scalar=m8b[:, j:j + 1], in1=gall[:, t, :], op0=ALU.is_equal, op1=ALU.mult, accum_out=g4[:, t, j:j + 1]),
                 reads=[r_sl, r_m8b, r_gall], writes=[r_eqt], awrites=[r_g4])
        S.op("dve", lambda e: e.tensor_scalar_add(out=idx4[:, t, :], in0=m8b[:, 0:4], scalar1=-1.0), reads=[r_m8b], awrites=[r_idx4])
        for j in range(4):
            S.op("pool", lambda e: e.indirect_dma_start(out=xs_d[:, :], out_offset=bass.IndirectOffsetOnAxis(ap=idx4[:, t, j:j + 1], axis=0), in_=ub[:], in_offset=None),
                 reads=[r_ub, r_idx4], awrites=[r_xsd], dma=True)
    C.pop()
    if "stopD" in dbg:
        C.pop()
        return

    C.push()
    w1b = C.ring("sb", "w1b", 2, [128, 8, 2 * D], BF16)
    w2b = C.ring("sb", "w2b", 2, [128, 8, D], BF16)
    b1t = C.ring("sb", "b1t", 2, [128, 16], F32)
    b2t = C.ring("sb", "b2t", 2, [2, D], F32)
    ones1f, r_ones1f = C.sb("ones1f", [1, 128], BF16)
    S.op("dve", lambda e: e.memset(ones1f[:], 1.0), writes=[r_ones1f])
    b2b = C.ring("sb", "b2b", 2, [1, D], BF16)
    xr = C.ring("sb", "xr", 8, [128, D], BF16)
    XT, r_XT = C.sb("XT", [128, 8, 512], BF16)
    hg = C.ring("sb", "hg", 2, [128, 512], F32)
    hu = C.ring("sb", "hu", 2, [128, 512], F32)
    sgm = C.ring("sb", "sgm", 2, [128, 512], F32)
    actT, r_actT = C.sb("actT", [128, 8, 512], BF16)
    ysb = C.ring("sb", "ysb", 2, [128, D], BF16)
    pxt = C.ring("ps", "pxt", 2, [128, 512], F32)
    pg = C.ring("ps", "pg", 2, [128, 512], F32)
    pu = C.ring("ps", "pu", 2, [128, 512], F32)
    py = C.ring("ps", "py", 2, [128, 512], F32)

    def load_weights(i, slot):
        w1t, r_w1 = w1b[slot]
        w2t, r_w2 = w2b[slot]
        for k in range(8):
            S.op("pool", lambda e: e.indirect_dma_start(out=w1t[:, k, :], out_offset=None, in_=w1b_d[:, :], in_offset=bass.IndirectOffsetOnAxis(ap=widx[:, i, k:k + 1], axis=0)),
                 reads=[r_w1bd, r_widx], writes=[r_w1] if k == 0 else (), awrites=() if k == 0 else [r_w1], dma=True)
        for k in range(8):
            S.op("pool", lambda e: e.indirect_dma_start(out=w2t[:, k, :], out_offset=None, in_=w2b_d[:, :], in_offset=bass.IndirectOffsetOnAxis(ap=widx[:, i, k:k + 1], axis=0)),
                 reads=[r_w2bd, r_widx], writes=[r_w2] if k == 0 else (), awrites=() if k == 0 else [r_w2], dma=True)
        S.op("pool", lambda e: e.indirect_dma_start(out=b1t[slot][0][:], out_offset=None, in_=b1r_d[:, :], in_offset=bass.IndirectOffsetOnAxis(ap=bidx[:, i:i + 1], axis=0)),
             reads=[r_in, r_bidx], writes=[b1t[slot][1]], dma=True)
        S.op("pool", lambda e: e.indirect_dma_start(out=b2t[slot][0][:], out_offset=None, in_=b2_d[:, :], in_offset=bass.IndirectOffsetOnAxis(ap=eidx[0:2, i:i + 1], axis=0)),
             reads=[r_in, r_eidx], writes=[b2t[slot][1]], dma=True)

    def load_x(i):
        for s4 in range(4):
            x_t, r_x = xr[(i % 2) * 4 + s4]
            row0 = i * 512 + s4 * 128
            S.dma("sp", x_t[:], xs_d[row0:row0 + 128, :], reads=[r_xsd], writes=[r_x])

    nblk_run = NBLK if "nblk" not in L else L["nblk"]
    load_weights(0, 0)
    load_x(0)
    xc = 0
    for i in range(nblk_run):
        slot = i % 2
        if i + 1 < nblk_run:
            load_weights(i + 1, (i + 1) % 2)
            load_x(i + 1)
        w1t, r_w1 = w1b[slot]
        w2t, r_w2 = w2b[slot]
        b1_t, r_b1 = b1t[slot]
        b2f_t, r_b2f = b2t[slot]
        b2_t, r_b2 = b2b[slot]
        S.op("pool", lambda e: e.tensor_copy(out=b2_t[:], in_=b2f_t[0:1, :]), reads=[r_b2f], writes=[r_b2])
        for s4 in range(4):
            x_t, r_x = xr[(i % 2) * 4 + s4]
            p_t, r_p = pxt[xc % 2]
            xc += 1
            p_b = p_t[:].bitcast(BF16)
            for k in range(8):
                S.op("pe", lambda e: e.transpose(out=p_b[:, k * 128:(k + 1) * 128], in_=x_t[:, k * 128:(k + 1) * 128], identity=ident_b[:]),
                     reads=[r_x, r_identb], writes=[r_p] if k == 0 else (), awrites=() if k == 0 else [r_p])
            S.op("act", lambda e: e.activation(out=XT[:, :, s4 * 128:(s4 + 1) * 128], in_=p_b.rearrange("p (k t) -> p k t", k=8), func=AF.Copy), reads=[r_p],
                 writes=[r_XT] if s4 == 0 else (), awrites=() if s4 == 0 else [r_XT])
        for fc in range(8):
            pg_t, r_pg = pg[fc % 2]
            pu_t, r_pu = pu[fc % 2]
            for k in range(8):
                S.op("pe", lambda e: e.matmul(pg_t[:], lhsT=w1t[:, k, fc * 128:(fc + 1) * 128], rhs=XT[:, k, :], start=(k == 0), stop=(k == 7)),
                     reads=[r_w1, r_XT], writes=[r_pg] if k == 0 else (), awrites=() if k == 0 else [r_pg])
            for k in range(8):
                S.op("pe", lambda e: e.matmul(pu_t[:], lhsT=w1t[:, k, D + fc * 128:D + (fc + 1) * 128], rhs=XT[:, k, :], start=(k == 0), stop=(k == 7)),
                     reads=[r_w1, r_XT], writes=[r_pu] if k == 0 else (), awrites=() if k == 0 else [r_pu])
            hg_t, r_hg = hg[fc % 2]
            hu_t, r_hu = hu[fc % 2]
            sg_t, r_sgm = sgm[fc % 2]
            S.op("act", lambda e: e.activation(out=hg_t[:], in_=pg_t[:], func=AF.Identity, bias=b1_t[:, fc:fc + 1]), reads=[r_pg, r_b1], writes=[r_hg])
            S.op("act", lambda e: e.activation(out=hu_t[:], in_=pu_t[:], func=AF.Identity, bias=b1_t[:, 8 + fc:9 + fc]), reads=[r_pu, r_b1], writes=[r_hu])
            S.op("dve", lambda e: e.tensor_scalar_min(out=hg_t[:], in0=hg_t[:], scalar1=7.0), reads=[r_hg], writes=[r_hg])
            S.op("act", lambda e: e.activation(out=sg_t[:], in_=hg_t[:], func=AF.Sigmoid, scale=1.702), reads=[r_hg], writes=[r_sgm])
            S.op("pool", lambda e: e.tensor_scalar(out=hu_t[:], in0=hu_t[:], scalar1=7.0, scalar2=-7.0, op0=ALU.min, op1=ALU.max), reads=[r_hu], writes=[r_hu])
            S.op("dve", lambda e: e.scalar_tensor_tensor(out=hu_t[:], in0=hu_t[:], scalar=1.0, in1=hg_t[:], op0=ALU.add, op1=ALU.mult), reads=[r_hu, r_hg], writes=[r_hu])
            S.op("dve", lambda e: e.tensor_tensor(out=actT[:, fc, :], in0=hu_t[:], in1=sg_t[:], op=ALU.mult), reads=[r_hu, r_sgm],
                 writes=[r_actT] if fc == 0 else (), awrites=() if fc == 0 else [r_actT])
        for s4 in range(4):
            y_t, r_y = ysb[s4 % 2]
            for hf in range(2):
                py_t, r_py = py[hf]
                for fc in range(8):
                    S.op("pe", lambda e: e.matmul(py_t[:], lhsT=actT[:, fc, s4 * 128:(s4 + 1) * 128], rhs=w2t[:, fc, hf * 512:(hf + 1) * 512], start=(fc == 0), stop=False),
                         reads=[r_actT, r_w2], writes=[r_py] if fc == 0 else (), awrites=() if fc == 0 else [r_py])
                S.op("pe", lambda e: e.matmul(py_t[:], lhsT=ones1f[:], rhs=b2_t[0:1, hf * 512:(hf + 1) * 512], start=False, stop=True), reads=[r_ones1f, r_b2], awrites=[r_py])
                S.op("act", lambda e: e.activation(out=y_t[:, hf * 512:(hf + 1) * 512], in_=py_t[:], func=AF.Copy), reads=[r_py],
                     writes=[r_y] if hf == 0 else (), awrites=() if hf == 0 else [r_y])
            row0 = i * 512 + s4 * 128
            S.dma("act", ys_d[row0:row0 + 128, :], y_t[:], reads=[r_y], awrites=[r_ysd])
    C.pop()

    C.push()
    ln2g, r_ln2g = C.sb("ln2g", [128, D], F32)
    ln2b, r_ln2b = C.sb("ln2b", [128, D], F32)
    S.dma("sp", ln2g[:], g_("ln2g_d").partition_broadcast(128), reads=[r_in], writes=[r_ln2g])
    S.dma("sp", ln2b[:], g_("ln2b_d").partition_broadcast(128), reads=[r_in], writes=[r_ln2b])
    gate2r = C.ring("sb", "gate2r", 2, [128, D], F32)
    x1r = C.ring("sb", "x1r", 4, [128, D], F32)
    yg = C.ring("sb", "yg", 16, [128, D], BF16)
    accr = C.ring("sb", "accF", 3, [128, D], F32)
    ot = C.ring("sb", "otF", 3, [128, D], F32)
    stF = C.ring("sb", "stF", 3, [128, 2, 6], F32)
    mvF = C.ring("sb", "mvF", 3, [128, 4], F32)
    rsF = C.ring("sb", "rsF", 3, [128, 1], F32)

    def loadF(t):
        tok0 = t * 128
        slot = t % 4
        S.dma("sp", x1r[slot][0][:], x1_d[tok0:tok0 + 128, :], reads=[r_x1], writes=[x1r[slot][1]])
        for j in range(4):
            y_t, r_y = yg[slot * 4 + j]
            S.op("pool", lambda e: e.indirect_dma_start(out=y_t[:], out_offset=None, in_=ys_d[:, :], in_offset=bass.IndirectOffsetOnAxis(ap=idx4[:, t, j:j + 1], axis=0)),
                 reads=[r_ysd, r_idx4], writes=[r_y], dma=True)
        if t % TPS == 0:
            b_ = t // TPS
            S.dma("sp", gate2r[b_ % 2][0][:], mod_d[b_:b_ + 1, 5 * D:6 * D].partition_broadcast(128), reads=[r_mod], writes=[gate2r[b_ % 2][1]])

    def genF(t):
        if t + 3 < NT:
            loadF(t + 3)
        slot = t % 4
        tok0 = t * 128
        gate2, r_gate2 = gate2r[(t // TPS) % 2]
        x1_t, r_x1t = x1r[slot]
        acc, r_acc = accr[t % 3]
        st, r_st = stF[t % 3]; mv, r_mv = mvF[t % 3]; rs, r_rs = rsF[t % 3]
        o_t, r_o = ot[t % 3]
        for j in range(4):
            y_t, r_y = yg[slot * 4 + j]
            if j == 0:
                S.op("act", lambda e: e.activation(out=acc[:], in_=y_t[:], func=AF.Copy, scale=g4[:, t, 0:1]), reads=[r_y, r_g4], writes=[r_acc])
            else:
                S.op("dve", lambda e: e.scalar_tensor_tensor(out=acc[:], in0=y_t[:], scalar=g4[:, t, j:j + 1], in1=acc[:], op0=ALU.mult, op1=ALU.add), reads=[r_y, r_g4, r_acc], writes=[r_acc])
            yield
        S.op("pool", lambda e: e.tensor_tensor(out=acc[:], in0=acc[:], in1=gate2[:], op=ALU.mult), reads=[r_acc, r_gate2], writes=[r_acc])
        yield
        S.op("dve", lambda e: e.scalar_tensor_tensor(out=acc[:], in0=x1_t[:], scalar=float(ALPHA), in1=acc[:], op0=ALU.mult, op1=ALU.add), reads=[r_x1t, r_acc], writes=[r_acc])
        yield
        for j in range(2):
            S.op("dve", lambda e: e.bn_stats(out=st[:, j, :], in_=acc[:, j * 512:(j + 1) * 512]), reads=[r_acc], writes=[r_st] if j == 0 else (), awrites=() if j == 0 else [r_st])
            yield
        S.op("dve", lambda e: e.bn_aggr(out=mv[:, 0:2], in_=st[:].rearrange("p a b -> p (a b)")), reads=[r_st], writes=[r_mv])
        yield
        S.op("act", lambda e: e.activation(out=rs[:], in_=mv[:, 1:2], func=AF.Sqrt, bias=EPS), reads=[r_mv], writes=[r_rs])
        yield
        S.op("dve", lambda e: e.reciprocal(out=rs[:], in_=rs[:]), reads=[r_rs], writes=[r_rs])
        yield
        S.op("dve", lambda e: e.tensor_scalar(out=mv[:, 2:3], in0=mv[:, 0:1], scalar1=-1.0, scalar2=rs[:], op0=ALU.mult, op1=ALU.mult), reads=[r_mv, r_rs], awrites=[r_mv])
        yield
        S.op("act", lambda e: e.activation(out=o_t[:], in_=acc[:], func=AF.Identity, scale=rs[:], bias=mv[:, 2:3]), reads=[r_acc, r_mv, r_rs], writes=[r_o])
        yield
        S.op("dve", lambda e: e.tensor_tensor(out=o_t[:], in0=o_t[:], in1=ln2g[:], op=ALU.mult), reads=[r_o, r_ln2g], writes=[r_o])
        yield
        S.op("pool", lambda e: e.tensor_tensor(out=o_t[:], in0=o_t[:], in1=ln2b[:], op=ALU.add), reads=[r_o, r_ln2b], writes=[r_o])
        yield
        S.dma("sp", out_d[tok0:tok0 + 128, :], o_t[:], reads=[r_o], awrites=[r_out])
        yield

    for t0 in range(3):
        loadF(t0)
    interleave((genF(t) for t in range(NT)), 6)
    C.pop()
    C.pop()


def phase_C(nc, S, C, dbg, L):
    g_ = lambda n: L[n]
    r_in = g_("r_in"); ident_b = g_("ident_b"); r_identb = g_("r_identb"); ident_f = g_("ident_f"); r_identf = g_("r_identf")
    qT_d, r_qT = g_("qT_d"), g_("r_qT"); iqT_d, r_iqT = g_("iqT_d"), g_("r_iqT")
    kva_d, r_kva = g_("kva_d"), g_("r_kva"); kvT_d, r_kvT = g_("kvT_d"), g_("r_kvT"); ikT_d, r_ikT = g_("ikT_d"), g_("r_ikT")
    iw_d, r_iw = g_("iw_d"), g_("r_iw"); oaT_d, r_oaT = g_("oaT_d"), g_("r_oaT")
    C.push()
    w1_d, w2_d = g_("w1_d"), g_("w2_d")

    def cast_weights(i):
        e_ = i // 2
        if i % 2 == 0:
            S.dma("pool", g_("w1b_d")[e_ * D:(e_ + 1) * D, :], w1_d[e_ * D:(e_ + 1) * D, :], reads=[r_in], awrites=[g_("r_w1bd")])
        else:
            S.dma("pool", g_("w2b_d")[e_ * D:(e_ + 1) * D, :], w2_d[e_ * D:(e_ + 1) * D, :], reads=[r_in], awrites=[g_("r_w2bd")])
    tzf, r_tzf = C.sb("tzf", [128, 2, 8, 128], F32)
    tz, r_tz = C.sb("tzb", [128, 2, 8, 128], BF16)
    cfar, r_cfar = C.sb("cfar", [128, 8], F32)
    identN, r_identN = C.sb("identN", [128, 128], BF16)
    S.dma("sp", tzf[:], g_("tz_d"), reads=[r_in], writes=[r_tzf])
    S.dma("sp", cfar[:], g_("cfar_d").partition_broadcast(128), reads=[r_in], writes=[r_cfar])
    S.op("dve", lambda e: e.tensor_scalar_mul(out=identN[:], in0=ident_f[:], scalar1=-NEG), reads=[r_identf], writes=[r_identN])
    first = True
    for dl in range(2):
        for h in range(8):
            S.op("dve", lambda e: e.tensor_scalar(out=tz[:, dl, h, :], in0=tzf[:, dl, h, :], scalar1=cfar[:, h:h + 1], scalar2=None, op0=ALU.subtract),
                 reads=[r_tzf, r_cfar], writes=[r_tz] if first else (), awrites=() if first else [r_tz])
            first = False
    seqb = [(C.sb("kvT%d" % i, [128, SEQ], BF16), C.sb("ikT%d" % i, [128, SEQ], BF16), C.sb("kvaC%d" % i, [128, TPS, 136], BF16)) for i in range(2)]
    qt = C.ring("sb", "qt", 5, [128, 8, 128], BF16)
    iqt = C.ring("sb", "iqt", 4, [128, 4, 128], BF16)
    iwt = C.ring("sb", "iwC", 4, [128, 8], F32)
    scorer = C.ring("sb", "score", 3, [128, SEQ], F32)
    penr = C.ring("sb", "pen", 2, [128, SEQ], BF16)
    rl = C.ring("sb", "rl", 2, [128, 512], F32)
    m8, r_m8 = C.sb("m8", [128, 8], F32)
    expT = C.ring("sb", "expT", 2, [128, TPS * 128], BF16)
    posb = C.ring("sb", "posb", 2, [128, 8, 132], F32)
    rc, r_rc = C.sb("rcC", [128, 8], F32)
    oa, r_oa = C.sb("oa", [128, D], BF16)
    oaT = C.ring("sb", "oaT", 2, [128, 8, 128], BF16)
    pi = C.ring("ps", "pi", 2, [128, 512], F32)
    pl = C.ring("ps", "pl", 3, [128, 512], F32)
    po = C.ring("ps", "po", 2, [128, 512], F32)
    ptr = C.ring("ps", "ptrC", 1, [128, 512], F32)
    NTILE = NB * TPS

    def load_seq(b):
        (kvT, r_kvTs), (ikT, r_ikTs), (kva, r_kvas) = seqb[b % 2]
        s0 = b * SEQ
        S.dma("sp", kvT[:], kvT_d[:, s0:s0 + SEQ], reads=[r_kvT], writes=[r_kvTs])
        S.dma("sp", ikT[:], ikT_d[:, s0:s0 + SEQ], reads=[r_ikT], writes=[r_ikTs])
        S.dma("sp", kva[:], kva_d[s0:s0 + SEQ, :].rearrange("(k p) c -> p k c", p=128), reads=[r_kva], writes=[r_kvas])

    def load_tile(i):
        tok0 = i * 128
        S.dma("sp", qt[i % 5][0][:], qT_d[:, :, tok0:tok0 + 128].rearrange("h p t -> p h t"), reads=[r_qT], writes=[qt[i % 5][1]])
        S.dma("sp", iqt[i % 4][0][:], iqT_d[:, :, tok0:tok0 + 128].rearrange("h p t -> p h t"), reads=[r_iqT], writes=[iqt[i % 4][1]])
        S.dma("sp", iwt[i % 4][0][:], iw_d[tok0:tok0 + 128, :], reads=[r_iw], writes=[iwt[i % 4][1]])

    NIT = 24
    pow2, r_pow2 = C.sb("pow2", [128, NIT + 1], F32)
    stepsr = C.ring("sb", "steps", 3, [128, NIT + 1], F32)
    bisr = C.ring("sb", "bis", 3, [128, 8], F32)
    junkb, r_junkb = C.sb("junkb", [128, SEQ], BF16)
    junkd, r_junkd = C.sb("junkd", [128, SEQ], BF16)
    for j in range(NIT + 1):
        S.op("pool", lambda e: e.memset(pow2[:, j:j + 1], float(2.0 ** -(j + 1))), writes=[r_pow2] if j == 0 else (), awrites=() if j == 0 else [r_pow2])

    def prep_score_gen(i):
        b, t = divmod(i, TPS)
        (ikT, r_ikTs) = seqb[b % 2][1]
        iq_t, r_iq = iqt[i % 4]
        iw_t, r_iwt = iwt[i % 4]
        pen, r_pen = penr[i % 2]
        score, r_score = scorer[i % 3]
        steps, r_steps = stepsr[i % 3]
        bis, r_bis = bisr[i % 3]
        N = 128 * (t + 1)
        if t < 2:
            return
            yield
        for kg in range(0, N, 512):
            w = min(512, N - kg)
            for h in range(8):
                pr, hf = divmod(h, 2)
                p_t, r_p = pi[h % 2]
                S.op("pe", lambda e: e.matmul(p_t[:, 0:w], lhsT=iq_t[64 * hf:64 * hf + 64, pr, :], rhs=ikT[64 * hf:64 * hf + 64, kg:kg + w], start=True, stop=True),
                     reads=[r_iq, r_ikTs], writes=[r_p])
                r_t, r_r = rl[h % 2]
                if h == 0:
                    S.op("dve", lambda e: e.tensor_scalar(out=score[:, kg:kg + w], in0=p_t[:, 0:w], scalar1=0.0, scalar2=iw_t[:, 0:1], op0=ALU.max, op1=ALU.mult),
                         reads=[r_p, r_iwt], writes=[r_score] if kg == 0 else (), awrites=() if kg == 0 else [r_score])
                elif h % 2 == 1:
                    S.op("dve", lambda e: e.tensor_scalar(out=r_t[:, 0:w], in0=p_t[:, 0:w], scalar1=0.0, scalar2=iw_t[:, h:h + 1], op0=ALU.max, op1=ALU.mult),
                         reads=[r_p, r_iwt], writes=[r_r])
                    S.op("pool", lambda e: e.tensor_tensor(out=score[:, kg:kg + w], in0=score[:, kg:kg + w], in1=r_t[:, 0:w], op=ALU.add),
                         reads=[r_r, r_score], awrites=[r_score])
                else:
                    S.op("act", lambda e: e.activation(out=r_t[:, 0:w], in_=p_t[:, 0:w], func=AF.Relu), reads=[r_p], writes=[r_r])
                    S.op("dve", lambda e: e.scalar_tensor_tensor(out=score[:, kg:kg + w], in0=r_t[:, 0:w], scalar=iw_t[:, h:h + 1], in1=score[:, kg:kg + w], op0=ALU.mult, op1=ALU.add),
                         reads=[r_r, r_iwt, r_score], awrites=[r_score])
                yield
        S.op("dve", lambda e: e.tensor_reduce(out=bis[:, 0:1], in_=score[:, 0:N - 64], axis=AX.X, op=ALU.min), reads=[r_score], writes=[r_bis])
        S.op("dve", lambda e: e.memset(score[0:64, N - 64:N], -1e30), reads=[r_score], awrites=[r_score])
        S.op("dve", lambda e: e.max(out=m8[:], in_=score[:, 0:N]), reads=[r_score], writes=[r_m8])
        S.op("dve", lambda e: e.tensor_tensor(out=bis[:, 1:2], in0=m8[:, 0:1], in1=bis[:, 0:1], op=ALU.subtract), reads=[r_m8, r_bis], awrites=[r_bis])
        S.op("dve", lambda e: e.tensor_scalar(out=steps[:], in0=pow2[:], scalar1=bis[:, 1:2], scalar2=None, op0=ALU.mult), reads=[r_pow2, r_bis], writes=[r_steps])
        S.op("dve", lambda e: e.tensor_tensor(out=bis[:, 2:3], in0=bis[:, 0:1], in1=steps[:, 0:1], op=ALU.add), reads=[r_bis, r_steps], awrites=[r_bis])

    def prep_score(i):
        for _ in prep_score_gen(i):
            pass

    def prep_iter(i, j):
        b, t = divmod(i, TPS)
        if t < 2:
            return
        N = 128 * (t + 1)
        score, r_score = scorer[i % 3]
        steps, r_steps = stepsr[i % 3]
        bis, r_bis = bisr[i % 3]
        if j % 2 == 0:
            S.op("act", lambda e: e.activation(out=junkb[:, 0:N], in_=score[:, 0:N], func=AF.Sign, scale=-1.0, bias=bis[:, 2:3], accum_out=bis[:, 4:5]),
                 reads=[r_score, r_bis], writes=[r_junkb], awrites=[r_bis])
            S.op("dve", lambda e: e.tensor_scalar(out=bis[:, 3:4], in0=bis[:, 4:5], scalar1=float(N - 511), scalar2=steps[:, j:j + 1], op0=ALU.is_le, op1=ALU.mult),
                 reads=[r_bis, r_steps], awrites=[r_bis])
        else:
            S.op("dve", lambda e: e.tensor_scalar(out=junkd[:, 0:N], in0=score[:, 0:N], scalar1=bis[:, 2:3], scalar2=None, op0=ALU.is_ge, op1=ALU.add, accum_out=bis[:, 6:7]),
                 reads=[r_score, r_bis], writes=[r_junkd], awrites=[r_bis])
            S.op("dve", lambda e: e.tensor_scalar(out=bis[:, 3:4], in0=bis[:, 6:7], scalar1=255.5, scalar2=steps[:, j:j + 1], op0=ALU.is_ge, op1=ALU.mult),
                 reads=[r_bis, r_steps], awrites=[r_bis])
        S.op("dve", lambda e: e.scalar_tensor_tensor(out=bis[:, 2:3], in0=bis[:, 3:4], scalar=steps[:, j + 1:j + 2], in1=bis[:, 2:3], op0=ALU.subtract, op1=ALU.add),
             reads=[r_bis, r_steps], awrites=[r_bis])

    def prep_fin(i):
        b, t = divmod(i, TPS)
        N = 128 * (t + 1)
        pen, r_pen = penr[i % 2]
        if t < 2:
            S.op("dve", lambda e: e.memset(pen[:, 0:N], 0.0), writes=[r_pen])
            S.op("dve", lambda e: e.memset(pen[0:64, N - 64:N], -1.0), awrites=[r_pen])
            return
        score, r_score = scorer[i % 3]
        steps, r_steps = stepsr[i % 3]
        bis, r_bis = bisr[i % 3]
        S.op("dve", lambda e: e.tensor_tensor(out=bis[:, 5:6], in0=bis[:, 2:3], in1=steps[:, NIT:NIT + 1], op=ALU.subtract), reads=[r_bis, r_steps], awrites=[r_bis])
        S.op("dve", lambda e: e.tensor_scalar(out=pen[:, 0:N], in0=score[:, 0:N], scalar1=bis[:, 5:6], scalar2=1.0, op0=ALU.is_ge, op1=ALU.subtract),
             reads=[r_score, r_bis], writes=[r_pen])

    state = {"plc": 0, "hc": 0}

    def attend_head(i, h):
        b, t = divmod(i, TPS)
        (kvT, r_kvTs), _, (kva, r_kvas) = seqb[b % 2]
        q_t, r_q = qt[i % 5]
        pen, r_pen = penr[i % 2]
        ps_t, r_ps = posb[i % 2]
        nkb = t + 1
        if True:
            e_t, r_e = expT[state["hc"] % 2]
            state["hc"] += 1
            for kg in range(0, nkb, 4):
                nb_ = min(4, nkb - kg)
                p_t, r_p = pl[state["plc"] % 3]
                state["plc"] += 1
                for ii in range(nb_):
                    kb = kg + ii
                    near = kb >= t - 1
                    cs_ = slice(ii * 128, (ii + 1) * 128)
                    S.op("pe", lambda e: e.matmul(p_t[:, cs_], lhsT=kvT[:, kb * 128:(kb + 1) * 128], rhs=q_t[:, h, :], start=True, stop=False),
                         reads=[r_kvTs, r_q], writes=[r_p] if ii == 0 else (), awrites=() if ii == 0 else [r_p])
                    S.op("pe", lambda e: e.matmul(p_t[:, cs_], lhsT=pen[:, kb * 128:(kb + 1) * 128], rhs=identN[:], start=False, stop=(not near)),
                         reads=[r_pen, r_identN], awrites=[r_p])
                    if near:
                        S.op("pe", lambda e: e.matmul(p_t[:, cs_], lhsT=ident_b[:], rhs=tz[:, t - kb, h, :], start=False, stop=True),
                             reads=[r_identb, r_tz], awrites=[r_p])
                S.op("act", lambda e: e.activation(out=e_t[:, kg * 128:(kg + nb_) * 128], in_=p_t[:, 0:nb_ * 128], func=AF.Exp), reads=[r_p],
                     writes=[r_e] if kg == 0 else (), awrites=() if kg == 0 else [r_e])
            o_t, r_o = po[h % 2]
            for kb in range(nkb):
                S.op("pe", lambda e: e.matmul(o_t[:, 0:129], lhsT=e_t[:, kb * 128:(kb + 1) * 128], rhs=kva[:, kb, 0:129], start=(kb == 0), stop=(kb == nkb - 1)),
                     reads=[r_e, r_kvas], writes=[r_o] if kb == 0 else (), awrites=() if kb == 0 else [r_o])
            S.op("act", lambda e: e.activation(out=ps_t[:, h, 0:129], in_=o_t[:, 0:129], func=AF.Copy), reads=[r_o],
                 writes=[r_ps] if h == 0 else (), awrites=() if h == 0 else [r_ps])

    def finalize(i):
        tok0 = i * 128
        ps_t, r_ps = posb[i % 2]
        S.op("dve", lambda e: e.reciprocal(out=rc[:], in_=ps_t[:, :, 128]), reads=[r_ps], writes=[r_rc])
        S.op("dve", lambda e: e.tensor_tensor(out=oa[:].rearrange("p (h c) -> p h c", h=8), in0=ps_t[:, :, 0:128], in1=rc[:].unsqueeze(2).to_broadcast([128, 8, 128]), op=ALU.mult),
             reads=[r_ps, r_rc], writes=[r_oa])
        pT, r_pT = ptr[0]
        pT_b = pT[:].bitcast(BF16)
        for k in range(8):
            S.op("pe", lambda e: e.transpose(out=pT_b[:, k * 128:(k + 1) * 128], in_=oa[:, k * 128:(k + 1) * 128], identity=ident_b[:]),
                 reads=[r_oa, r_identb], writes=[r_pT] if k == 0 else (), awrites=() if k == 0 else [r_pT])
        oT, r_oT = oaT[i % 2]
        S.op("act", lambda e: e.activation(out=oT[:].rearrange("p k t -> p (k t)"), in_=pT_b, func=AF.Copy), reads=[r_pT], writes=[r_oT])
        S.dma("sp", oaT_d[:, :, tok0:tok0 + 128].rearrange("c p t -> p c t"), oT[:], reads=[r_oT], awrites=[r_oaT])

    HALF = NIT // 2
    load_seq(0)
    for i0 in range(4):
        load_tile(i0)
    prep_score(0)
    for j in range(NIT):
        prep_iter(0, j)
    prep_fin(0)
    prep_score(1)
    for j in range(HALF):
        prep_iter(1, j)
    prep_score(2)
    for i in range(NTILE):
        b, t = divmod(i, TPS)
        if t == 0 and b + 1 < NB:
            load_seq(b + 1)
        if i + 4 < NTILE:
            load_tile(i + 4)
        cast_weights(i)
        n1 = i + 1 < NTILE
        n2 = i + 2 < NTILE
        gen = prep_score_gen(i + 3) if i + 3 < NTILE else iter(())
        npieces = 8 * ((128 * (((i + 3) % TPS) + 1) + 511) // 512) if (i + 3 < NTILE and (i + 3) % TPS >= 2) else 0
        ppb = (npieces + 7) // 8
        sched = []
        for k in range(HALF):
            if n1:
                sched.append((i + 1, HALF + k))
            if n2:
                sched.append((i + 2, k))
        per = (len(sched) + 7) // 8
        for h in range(8):
            attend_head(i, h)
            its = sched[h * per:(h + 1) * per]
            for n_, (ti_, j) in enumerate(its):
                prep_iter(ti_, j)
                if n_ < ppb:
                    next(gen, None)
            for _ in range(max(0, ppb - len(its))):
                next(gen, None)
        for _ in gen:
            pass
        if n1:
            prep_fin(i + 1)
        if i >= 1:
            finalize(i - 1)
    finalize(NTILE - 1)
    C.pop()


def phase_B(nc, S, C, dbg, L):
    g_ = lambda n: L[n]
    r_in = g_("r_in"); ident_b = g_("ident_b"); r_identb = g_("r_identb")
    xbcT_d, r_xbcT = g_("xbcT_d"), g_("r_xbcT"); dt_d, r_dt = g_("dt_d"), g_("r_dt"); zs_d, r_zs = g_("zs_d"), g_("r_zs")
    obT_d, r_obT = g_("obT_d"), g_("r_obT")
    C.push()
    convw, r_convw = C.sb("convw", [128, 16, 4], F32)
    convb, r_convb = C.sb("convb", [128, 16], F32)
    dg, r_dg = C.sb("dg", [128, 16, 4, 128], BF16)
    identf2, r_identf2 = C.sb("identf2", [128, 128], F32)
    a_bc, r_abc = C.sb("a_bc", [128, 16], F32)
    dskip_bc, r_dskip = C.sb("dskip_bc", [128, 16], F32)
    normw_bc, r_normw = C.sb("normw_bc", [128, D], F32)
    triU, r_triU = C.sb("triU", [128, 128], F32)
    SLm, r_SL = C.sb("SLm", [128, 128], F32)
    onesf, r_onesf = C.sb("onesf", [128, 128], F32)
    negm4, r_negm4 = C.sb("negm4", [128, 512], BF16)
    S.dma("sp", convw[:], g_("convw_d"), reads=[r_in], writes=[r_convw])
    S.dma("sp", convb[:], g_("convb_d"), reads=[r_in], writes=[r_convb])
    S.dma("sp", identf2[:], g_("ident_d"), reads=[r_in], writes=[r_identf2])
    S.dma("sp", a_bc[:], g_("alog_d").partition_broadcast(128), reads=[r_in], writes=[r_abc])
    S.dma("sp", dskip_bc[:], g_("dskip_d").partition_broadcast(128), reads=[r_in], writes=[r_dskip])
    S.dma("sp", normw_bc[:], g_("normw_d").partition_broadcast(128), reads=[r_in], writes=[r_normw])
    S.dma("sp", triU[:], g_("triU_d"), reads=[r_in], writes=[r_triU])
    S.dma("sp", SLm[:], g_("SL_d"), reads=[r_in], writes=[r_SL])
    S.dma("pool", negm4[:], g_("negm4_d"), reads=[r_in], writes=[r_negm4])
    S.op("dve", lambda e: e.memset(onesf[:], 1.0), writes=[r_onesf])
    S.op("act", lambda e: e.activation(out=a_bc[:], in_=a_bc[:], func=AF.Exp), reads=[r_abc], writes=[r_abc])
    S.op("dve", lambda e: e.tensor_scalar_mul(out=a_bc[:], in0=a_bc[:], scalar1=-1.0), reads=[r_abc], writes=[r_abc])
    first = True
    for j in range(16):
        for k in range(4):
            S.op("dve", lambda e: e.tensor_scalar_mul(out=dg[:, j, k, :], in0=identf2[:], scalar1=convw[:, j, k:k + 1]),
                 reads=[r_identf2, r_convw], writes=[r_dg] if first else (), awrites=() if first else [r_dg])
            first = False

    bank = C.ring("ps", "bk", 8, [128, 512], F32)
    xh = C.ring("sb", "xh", 4, [128, 16, 131], BF16)
    dtl = C.ring("sb", "dtl", 4, [128, 16], F32)
    zl = C.ring("sb", "zl", 4, [128, D], BF16)
    xact_r = C.ring("sb", "xact", 2, [128, 16, 128], BF16)
    xs_r = C.ring("sb", "xs_tok", 2, [128, D], BF16)
    Bt_r = C.ring("sb", "B_tok", 2, [128, 512], BF16)
    adt_r = C.ring("sb", "adt", 2, [128, 16], F32)
    sm_r = C.ring("sb", "smB", 2, [128, 8, 16], F32)
    A_r = C.ring("sb", "Amat", 2, [128, 16, 128], F32)
    Lt_r = C.ring("sb", "Lt", 2, [128, 2, 512], F32)
    Mt_r = C.ring("sb", "Mt", 2, [128, 16, 128], BF16)
    xdt_r = C.ring("sb", "xdt", 2, [128, D], BF16)
    xdd_r = C.ring("sb", "xdd", 2, [128, D], BF16)
    prev_f, r_pf = C.sb("prev_f", [128, D], F32)
    prev_b, r_pb = C.sb("prev_b", [128, D], BF16)
    t1_r = C.ring("sb", "t1", 2, [128, D], F32)
    t2_r = C.ring("sb", "t2", 2, [128, D], F32)
    junk, r_junk = C.sb("junkB", [128, 256], F32)
    ob_r = C.ring("sb", "ob", 2, [128, D], BF16)
    obT = C.ring("sb", "obT", 2, [128, 8, 128], BF16)

    def bc3(ap2, n):
        return ap2.unsqueeze(2).to_broadcast([128, 16, n])

    def v3(t, n=64):
        return t.rearrange("p (h q) -> p h q", q=n)

    def load_chunk(b, t, slot):
        tok0 = b * SEQ + t * 128
        x_t, r_x = xh[slot]
        if t == 0:
            S.op("pool", lambda e: e.memset(x_t[:, :, 0:3], 0.0), writes=[r_x])
            S.dma("sp", x_t[:, :, 3:131], xbcT_d[:, :, tok0:tok0 + 128].rearrange("c p t -> p c t"), reads=[r_xbcT], awrites=[r_x])
        else:
            S.dma("sp", x_t[:, :, :], xbcT_d[:, :, tok0 - 3:tok0 + 128].rearrange("c p t -> p c t"), reads=[r_xbcT], writes=[r_x])
        S.dma("sp", dtl[slot][0][:], dt_d[tok0:tok0 + 128, :], reads=[r_dt], writes=[dtl[slot][1]])
        S.dma("sp", zl[slot][0][:], zs_d[tok0:tok0 + 128, :], reads=[r_zs], writes=[zl[slot][1]])

    nch = NB * TPS

    def chunk_gen(ci):
        b, t = divmod(ci, TPS)
        slot = ci % 4
        sl2 = ci % 2
        tok0 = b * SEQ + t * 128
        if ci + 2 < nch:
            load_chunk((ci + 2) // TPS, (ci + 2) % TPS, (ci + 2) % 4)
        x_t, r_x = xh[slot]
        d_t, r_d = dtl[slot]
        z_t, r_z = zl[slot]
        xact, r_xact = xact_r[sl2]; xs_tok, r_xs = xs_r[sl2]; B_tok, r_Bt = Bt_r[sl2]; adt, r_adt = adt_r[sl2]; sm, r_sm = sm_r[sl2]
        Amat, r_A = A_r[sl2]; Lt, r_Lt = Lt_r[sl2]; Mt, r_Mt = Mt_r[sl2]; xdt, r_xdt = xdt_r[sl2]; xdd, r_xdd = xdd_r[sl2]
        t1, r_t1 = t1_r[sl2]; t2, r_t2 = t2_r[sl2]; ob, r_ob = ob_r[sl2]
        for jg in range(4):
            pc, r_pc = bank[jg % 2]
            for jj in range(4):
                j = jg * 4 + jj
                for k in range(4):
                    S.op("pe", lambda e: e.matmul(pc[:, jj * 128:(jj + 1) * 128], lhsT=dg[:, j, k, :], rhs=x_t[:, j, k:k + 128], start=(k == 0), stop=(k == 3)),
                         reads=[r_dg, r_x], writes=[r_pc] if (jj == 0 and k == 0) else (), awrites=() if (jj == 0 and k == 0) else [r_pc])
                    yield
            for jj in range(4):
                j = jg * 4 + jj
                S.op("act", lambda e: e.activation(out=xact[:, j, :], in_=pc[:, jj * 128:(jj + 1) * 128], func=AF.Silu, bias=convb[:, j:j + 1]),
                     reads=[r_pc, r_convb], writes=[r_xact] if j == 0 else (), awrites=() if j == 0 else [r_xact])
                yield
        pxs, r_pxs = bank[2]
        pB, r_pB = bank[3]
        pxs_b = pxs[:].bitcast(BF16)
        pB_b = pB[:].bitcast(BF16)
        for k in range(8):
            S.op("pe", lambda e: e.transpose(out=pxs_b[:, k * 128:(k + 1) * 128], in_=xact[:, k, :], identity=ident_b[:]),
                 reads=[r_xact, r_identb], writes=[r_pxs] if k == 0 else (), awrites=() if k == 0 else [r_pxs])
            yield
        for k in range(4):
            S.op("pe", lambda e: e.transpose(out=pB_b[:, k * 128:(k + 1) * 128], in_=xact[:, 8 + k, :], identity=ident_b[:]),
                 reads=[r_xact, r_identb], writes=[r_pB] if k == 0 else (), awrites=() if k == 0 else [r_pB])
            yield
        S.op("act", lambda e: e.activation(out=xs_tok[:], in_=pxs_b, func=AF.Copy), reads=[r_pxs], writes=[r_xs])
        yield
        S.op("act", lambda e: e.activation(out=B_tok[:], in_=pB_b[:, 0:512], func=AF.Copy), reads=[r_pB], writes=[r_Bt])
        yield
        S.op("dve", lambda e: e.tensor_tensor(out=adt[:], in0=d_t[:], in1=a_bc[:], op=ALU.mult), reads=[r_d, r_abc], writes=[r_adt])
        yield
        S.op("pe", lambda e: e.matmul(pB[:, 256:272], lhsT=triU[:], rhs=adt[:], start=True, stop=True), reads=[r_triU, r_adt], awrites=[r_pB])
        yield
        S.op("pe", lambda e: e.matmul(pB[:, 272:288], lhsT=onesf[:], rhs=adt[:], start=True, stop=True), reads=[r_onesf, r_adt], awrites=[r_pB])
        yield
        S.op("act", lambda e: e.activation(out=sm[:, 0, :], in_=pB[:, 256:272], func=AF.Exp), reads=[r_pB], writes=[r_sm])
        yield
        S.op("act", lambda e: e.activation(out=sm[:, 1, :], in_=pB[:, 272:288], func=AF.Copy), reads=[r_pB], awrites=[r_sm])
        yield
        S.op("act", lambda e: e.activation(out=sm[:, 2, :], in_=pB[:, 272:288], func=AF.Exp), reads=[r_pB], awrites=[r_sm])
        yield
        S.op("dve", lambda e: e.tensor_tensor(out=sm[:, 3, :], in0=sm[:, 1, :], in1=pB[:, 256:272], op=ALU.subtract), reads=[r_sm, r_pB], awrites=[r_sm])
        yield
        S.op("act", lambda e: e.activation(out=sm[:, 4, :], in_=sm[:, 3, :], func=AF.Exp), reads=[r_sm], awrites=[r_sm])
        yield
        S.op("dve", lambda e: e.tensor_tensor(out=Amat[:], in0=triU[:].unsqueeze(1).to_broadcast([128, 16, 128]), in1=bc3(adt[:], 128), op=ALU.mult),
             reads=[r_triU, r_adt], writes=[r_A])
        yield
        pCB, r_pCB = bank[4]
        for g in range(4):
            S.op("pe", lambda e: e.matmul(pCB[:, g * 128:(g + 1) * 128], lhsT=xact[:, 8 + g, :], rhs=xact[:, 12 + g, :], start=True, stop=True),
                 reads=[r_xact], writes=[r_pCB] if g == 0 else (), awrites=() if g == 0 else [r_pCB])
            yield
        S.op("dve", lambda e: e.tensor_tensor(out=v3(xdt[:]), in0=v3(xs_tok[:]), in1=bc3(d_t[:], 64), op=ALU.mult), reads=[r_xs, r_d], writes=[r_xdt])
        yield
        S.op("pool", lambda e: e.tensor_tensor(out=v3(xdd[:]), in0=v3(xdt[:]), in1=bc3(sm[:, 4, :], 64), op=ALU.mult), reads=[r_xdt, r_sm], writes=[r_xdd])
        yield
        for g in range(4):
            pD, r_pD = bank[5 + g % 2]
            S.op("pe", lambda e: e.matmul(pD[:], lhsT=SLm[:], rhs=Amat[:, 4 * g:4 * g + 4, :], start=True, stop=False), reads=[r_SL, r_A], writes=[r_pD])
            yield
            S.op("pe", lambda e: e.matmul(pD[:], lhsT=ident_b[:], rhs=negm4[:], start=False, stop=True), reads=[r_identb, r_negm4], awrites=[r_pD])
            yield
            S.op("act", lambda e: e.activation(out=Lt[:, g % 2, :], in_=pD[:], func=AF.Exp), reads=[r_pD], writes=[r_Lt] if g % 2 == 0 else (), awrites=() if g % 2 == 0 else [r_Lt])
            yield
            S.op("dve", lambda e: e.tensor_tensor(out=Mt[:, 4 * g:4 * g + 4, :], in0=Lt[:, g % 2, :].rearrange("p (h l) -> p h l", h=4),
                                                 in1=pCB[:, g * 128:(g + 1) * 128].unsqueeze(1).to_broadcast([128, 4, 128]), op=ALU.mult),
                 reads=[r_Lt, r_pCB], writes=[r_Mt] if g == 0 else (), awrites=() if g == 0 else [r_Mt])
            yield
        if t == 0:
            S.op("pool", lambda e: e.memset(prev_f[:], 0.0), writes=[r_pf])
            yield
            S.op("pool", lambda e: e.memset(prev_b[:], 0.0), writes=[r_pb])
            yield
        for hh in range(2):
            pY, r_pY = bank[5]
            pO, r_pO = bank[6]
            pS, r_pS = bank[7]
            c0 = hh * 512
            for h8 in range(8):
                h = hh * 8 + h8
                S.op("pe", lambda e: e.matmul(pY[:, h8 * 64:(h8 + 1) * 64], lhsT=Mt[:, h, :], rhs=xdt[:, h * 64:(h + 1) * 64], start=True, stop=True),
                     reads=[r_Mt, r_xdt], writes=[r_pY] if h8 == 0 else (), awrites=() if h8 == 0 else [r_pY])
                yield
            for g2 in range(2):
                g = hh * 2 + g2
                S.op("pe", lambda e: e.matmul(pO[:, g2 * 256:(g2 + 1) * 256], lhsT=xact[:, 12 + g, :], rhs=prev_b[:, g * 256:(g + 1) * 256], start=True, stop=True),
                     reads=[r_xact, r_pb], writes=[r_pO] if g2 == 0 else (), awrites=() if g2 == 0 else [r_pO])
                yield
            for g2 in range(2):
                g = hh * 2 + g2
                S.op("pe", lambda e: e.matmul(pS[:, g2 * 256:(g2 + 1) * 256], lhsT=B_tok[:, g * 128:(g + 1) * 128], rhs=xdd[:, g * 256:(g + 1) * 256], start=True, stop=True),
                     reads=[r_Bt, r_xdd], writes=[r_pS] if g2 == 0 else (), awrites=() if g2 == 0 else [r_pS])
                yield
            hs = slice(hh * 8, hh * 8 + 8)

            def v8(ap):
                return ap.rearrange("p (h q) -> p h q", q=64)
            ex8 = sm[:, 0, hs].unsqueeze(2).to_broadcast([128, 8, 64])
            cd8 = sm[:, 2, hs].unsqueeze(2).to_broadcast([128, 8, 64])
            ds8 = dskip_bc[:, hs].unsqueeze(2).to_broadcast([128, 8, 64])
            S.op("dve", lambda e: e.tensor_tensor(out=v8(t1[:, c0:c0 + 512]), in0=v8(pO[:]), in1=ex8, op=ALU.mult), reads=[r_pO, r_sm], writes=[r_t1] if hh == 0 else (), awrites=() if hh == 0 else [r_t1])
            yield
            S.op("dve", lambda e: e.tensor_tensor(out=t1[:, c0:c0 + 512], in0=t1[:, c0:c0 + 512], in1=pY[:], op=ALU.add), reads=[r_t1, r_pY], awrites=[r_t1])
            yield
            S.op("pool", lambda e: e.tensor_tensor(out=v8(t2[:, c0:c0 + 512]), in0=v8(xs_tok[:, c0:c0 + 512]), in1=ds8, op=ALU.mult), reads=[r_xs, r_dskip], writes=[r_t2] if hh == 0 else (), awrites=() if hh == 0 else [r_t2])
            yield
            S.op("dve", lambda e: e.tensor_tensor(out=v8(prev_f[:, c0:c0 + 512]), in0=v8(prev_f[:, c0:c0 + 512]), in1=cd8, op=ALU.mult), reads=[r_pf, r_sm], awrites=[r_pf])
            yield
            S.op("dve", lambda e: e.tensor_tensor(out=prev_f[:, c0:c0 + 512], in0=prev_f[:, c0:c0 + 512], in1=pS[:], op=ALU.add), reads=[r_pf, r_pS], awrites=[r_pf])
            yield
            S.op("act", lambda e: e.activation(out=prev_b[:, c0:c0 + 512], in_=prev_f[:, c0:c0 + 512], func=AF.Copy), reads=[r_pf, r_pO], awrites=[r_pb])
            yield
        S.op("pool", lambda e: e.tensor_tensor(out=t1[:], in0=t1[:], in1=t2[:], op=ALU.add), reads=[r_t1, r_t2], writes=[r_t1])
        yield
        S.op("pool", lambda e: e.tensor_tensor(out=t1[:], in0=t1[:], in1=z_t[:], op=ALU.mult), reads=[r_t1, r_z], writes=[r_t1])
        yield
        for g in range(4):
            S.op("act", lambda e: e.activation(out=junk[:], in_=t1[:, g * 256:(g + 1) * 256], func=AF.Square, accum_out=sm[:, 5, g:g + 1]),
                 reads=[r_t1], writes=[r_junk], awrites=[r_sm])
            yield
        S.op("act", lambda e: e.activation(out=sm[:, 5, 4:8], in_=sm[:, 5, 0:4], func=AF.Sqrt, scale=1.0 / 256.0, bias=EPS), reads=[r_sm], awrites=[r_sm])
        yield
        S.op("dve", lambda e: e.reciprocal(out=sm[:, 5, 8:12], in_=sm[:, 5, 4:8]), reads=[r_sm], awrites=[r_sm])
        yield
        S.op("dve", lambda e: e.tensor_tensor(out=t1[:].rearrange("p (g q) -> p g q", g=4), in0=t1[:].rearrange("p (g q) -> p g q", g=4),
                                             in1=sm[:, 5, 8:12].unsqueeze(2).to_broadcast([128, 4, 256]), op=ALU.mult), reads=[r_t1, r_sm], writes=[r_t1])
        yield
        S.op("dve", lambda e: e.tensor_tensor(out=ob[:], in0=t1[:], in1=normw_bc[:], op=ALU.mult), reads=[r_t1, r_normw], writes=[r_ob])
        yield
        pT, r_pT = bank[4]
        pT_b = pT[:].bitcast(BF16)
        for k in range(8):
            S.op("pe", lambda e: e.transpose(out=pT_b[:, k * 128:(k + 1) * 128], in_=ob[:, k * 128:(k + 1) * 128], identity=ident_b[:]),
                 reads=[r_ob, r_identb], writes=[r_pT] if k == 0 else (), awrites=() if k == 0 else [r_pT])
            yield
        o_t, r_o = obT[sl2]
        S.op("act", lambda e: e.activation(out=o_t[:].rearrange("p k t -> p (k t)"), in_=pT_b, func=AF.Copy), reads=[r_pT], writes=[r_o])
        yield
        S.dma("sp", obT_d[:, :, tok0:tok0 + 128].rearrange("c p t -> p c t"), o_t[:], reads=[r_o], awrites=[r_obT])
        yield

    load_chunk(0, 0, 0)
    load_chunk(0, 1, 1)
    interleave((chunk_gen(ci) for ci in range(nch)), B_STAGGER)
    C.pop()


def _t5_bucket_np(rel):
    half, max_exact = 16, 8
    ret = (rel > 0).astype(np.int32) * half
    n = np.abs(rel)
    nf = np.maximum(n, 1).astype(np.float32)
    large = max_exact + (np.log(nf / np.float32(max_exact)) / np.float32(np.log(128.0 / 8.0)) * np.float32(half - max_exact)).astype(np.int32)
    large = np.minimum(large, half - 1)
    return ret + np.where(n < max_exact, n, large)


def _t5_blocks(rel_bias):
    k = np.arange(128)[:, None]
    q = np.arange(128)[None, :]
    out = np.zeros((128, 2, 8, 128), np.float32)
    for dl in range(2):
        bk = _t5_bucket_np((k - 128 * dl) - q)
        for h in range(8):
            out[:, dl, h, :] = rel_bias[bk, h]
    return out


def host_inputs(inputs, core):
    b0 = core * NB
    f = lambda a: np.ascontiguousarray(a, dtype=np.float32)
    c = inputs["c"][b0:b0 + NB]
    m = {
        "x": f(inputs["x"][b0:b0 + NB].reshape(NTOK, D)),
        "cT": f(c.reshape(NB, 8, 128).transpose(2, 1, 0)),
        "w_mod": f(inputs["w_mod"][0]),
        "b_mod": f(inputs["b_mod"][0].reshape(1, -1)),
        "w_in": f(inputs["w_in"][0]),
        "ident": np.eye(128, dtype=np.float32),
        "kv_norm_w": f(inputs["kv_norm_w"][0].reshape(1, -1)),
        "idx_k_norm_w": f(inputs["idx_k_norm_w"][0].reshape(1, -1)),
        "idx_k_norm_b": f(inputs["idx_k_norm_b"][0].reshape(1, -1)),
        "dt_bias": f(inputs["dt_bias"][0].reshape(1, -1)),
        "convw": f(inputs["conv_w"][0].reshape(4, 16, 128).transpose(2, 1, 0)),
        "convb": f(inputs["conv_b"][0].reshape(16, 128).T),
        "a_log": f(inputs["a_log"][0].reshape(1, -1)),
        "d_skip": f(inputs["d_skip"][0].reshape(1, -1)),
        "ssm_norm_w": f(inputs["ssm_norm_w"][0].reshape(1, -1)),
        "w_proj_a": f(inputs["w_proj_a"][0]), "w_proj_b": f(inputs["w_proj_b"][0]), "w_out": f(inputs["w_out"][0]),
        "ln1_g": f(inputs["ln1_g"][0].reshape(1, -1)), "ln1_b": f(inputs["ln1_b"][0].reshape(1, -1)),
        "ln2_g": f(inputs["ln2_g"][0].reshape(1, -1)), "ln2_b": f(inputs["ln2_b"][0].reshape(1, -1)),
        "w_router": f(inputs["w_router"][0]), "b_router": f(inputs["b_router"][0].reshape(1, -1)),
        "w1": f(inputs["w1"][0].reshape(NE * D, 2 * D)), "w2": f(inputs["w2"][0].reshape(NE * D, D)),
        "b1r": f(inputs["b1"][0].reshape(NE, 16, 128).transpose(0, 2, 1).reshape(NE * 128, 16)),
        "b2": f(inputs["b2"][0]),
        "sut": np.triu(np.ones((128, 128), np.float32), 1),
        "thr16": np.tile(512.0 * np.arange(16, dtype=np.float32), NE).reshape(1, -1),
        "bstart": (512.0 * np.arange(NBLK, dtype=np.float32)).reshape(1, -1),
        "kp": (np.arange(8, dtype=np.float32)[None, :] * 128 + np.arange(128, dtype=np.float32)[:, None]),
        "pcol": np.arange(128, dtype=np.float32).reshape(128, 1),
        "sut32": np.triu(np.ones((NE, NE), np.float32), 1),
        "tz": _t5_blocks(f(inputs["rel_bias"])),
        "cfar": f(inputs["rel_bias"][15:16, :]),
        "triU": np.triu(np.ones((128, 128), np.float32)),
        "SL": np.tril(np.ones((128, 128), np.float32), -1),
        "negm4": np.tile(np.tril(np.full((128, 128), NEG, np.float32), -1), (1, 4)),
    }
    return m


def kernel(**inputs):
    nc = build_program()
    in_maps = [host_inputs(inputs, c) for c in range(NCORES)]
    res = run_bass_kernel_spmd(nc, in_maps, core_ids=list(range(NCORES)))
    out = np.stack([np.asarray(r["out"]).reshape(NB, SEQ, D) for r in res.results], 0)
    return out.reshape(NCORES * NB, SEQ, D).astype(np.float32)
```

```python
import numpy as np
import concourse.bass as bass
import concourse.mybir as mybir
from concourse.bass_utils import run_bass_kernel_spmd

F32 = mybir.dt.float32
BF16 = mybir.dt.bfloat16
I32 = mybir.dt.int32
ALU = mybir.AluOpType
AF = mybir.ActivationFunctionType
AX = mybir.AxisListType

NCORES = 8
SEQ = 2048
D = 1024
NB = 4
NTOK = NB * SEQ
NT = NTOK // 128
TPS = SEQ // 128
DIN = 6872
C_Q, C_KV, C_IQ, C_IK, C_IW, C_Z, C_XBC, C_DT, C_GA, C_GB = 0, 1024, 1152, 1664, 1728, 1736, 2760, 4808, 4824, 5848
NE = 32
NBLK = NTOK * 4 // 512 + NE
ALPHA = 2.0 ** 0.25
EPS = 1e-5
NEG = -30000.0

B_STAGGER = 104
ENGS = ("pe", "act", "dve", "pool", "sp")


class Res:
    __slots__ = ("name", "writers", "readers", "dsem", "dcount", "dram")

    def __init__(self, name):
        self.name = name
        self.dram = False
        self.writers = {}
        self.readers = {}
        self.dsem = None
        self.dcount = 0


class Sched:
    def __init__(self, nc):
        self.nc = nc
        self.eng = {"pe": nc.tensor, "act": nc.scalar, "dve": nc.vector,
                    "pool": nc.gpsimd, "sp": nc.sync}
        self.sem = {e: nc.alloc_semaphore("prog_" + e) for e in ENGS}
        self.cnt = {e: 0 for e in ENGS}
        self.waited = {e: {} for e in ENGS}
        self.all_res = []
        self.nwaits = 0
        self.nops = 0
        self.sempool = []

    def retire(self, rs):
        for r in rs:
            if r.dsem is not None:
                self.sempool.append((r.dsem, r.dcount))
                r.dsem = None
            if r in self.all_res:
                self.all_res.remove(r)

    def res(self, name):
        r = Res(name)
        self.all_res.append(r)
        return r

    def _need(self, eng, tok, deps):
        sem, val = tok
        k = sem.num
        if self.waited[eng].get(k, 0) >= val:
            return
        if k not in deps or deps[k][1] < val:
            deps[k] = (sem, val)

    def op(self, eng, fn, reads=(), writes=(), awrites=(), dma=False):
        deps = {}
        mykey = None if dma else eng
        for r in reads:
            for k, tok in r.writers.items():
                if k == mykey and eng == "pe":
                    continue
                self._need(eng, tok, deps)
        for r in writes:
            for k, tok in list(r.writers.items()) + list(r.readers.items()):
                if k == mykey:
                    continue
                self._need(eng, tok, deps)
        for r in awrites:
            for k, tok in r.readers.items():
                if k == mykey:
                    continue
                self._need(eng, tok, deps)
        e = self.eng[eng]
        for k, (sem, val) in deps.items():
            e.wait_ge(sem, val)
            self.waited[eng][k] = val
            self.nwaits += 1
        ins = fn(e)
        self.nops += 1
        if dma:
            dst = (list(writes) + list(awrites))[0]
            if dst.dram:
                sb = [r for r in reads if not r.dram]
                if sb:
                    dst = sb[0]
            if dst.dsem is None:
                if self.sempool:
                    dst.dsem, dst.dcount = self.sempool.pop()
                else:
                    dst.dsem = self.nc.alloc_semaphore("d_" + dst.name)
            dst.dcount += 16
            ins.then_inc(dst.dsem, 16)
            tok = (dst.dsem, dst.dcount)
            key = "dma%d" % dst.dsem.num
        else:
            self.cnt[eng] += 1
            ins.then_inc(self.sem[eng], 1)
            tok = (self.sem[eng], self.cnt[eng])
            key = eng
        for r in reads:
            r.readers[key] = tok
        for r in writes:
            r.writers = {key: tok}
            r.readers = {}
        for r in awrites:
            r.writers[key] = tok
        return ins

    def dma(self, eng, out, in_, reads=(), writes=(), awrites=(), **kw):
        return self.op(eng, lambda e: e.dma_start(out=out, in_=in_, **kw),
                       reads=reads, writes=writes, awrites=awrites, dma=True)

    def barrier(self):
        toks = {}
        for e in ENGS:
            if self.cnt[e]:
                toks[self.sem[e].num] = (self.sem[e], self.cnt[e])
        for r in self.all_res:
            if r.dsem is not None and r.dcount:
                toks[r.dsem.num] = (r.dsem, r.dcount)
        for e in ENGS:
            for k, (sem, val) in toks.items():
                if self.waited[e].get(k, 0) >= val:
                    continue
                self.eng[e].wait_ge(sem, val)
                self.waited[e][k] = val
                self.nwaits += 1
        for r in self.all_res:
            r.writers = {}
            r.readers = {}


class Ctx:
    def __init__(self, nc, S):
        self.nc = nc
        self.S = S
        self.stack = []

    def push(self):
        self.stack.append([])

    def pop(self):
        self.S.barrier()
        gs = self.stack.pop()
        self.S.retire([r for (_, r) in gs])
        for g, _ in reversed(gs):
            g.__exit__(None, None, None)

    def sb(self, name, shape, dt):
        g = self.nc.sbuf_tensor("s_" + name, list(shape), dt)
        t = g.__enter__()
        r = self.S.res(name)
        self.stack[-1].append((g, r))
        return t, r

    def ps(self, name, shape, dt=F32):
        g = self.nc.psum_tensor("p_" + name, list(shape), dt)
        t = g.__enter__()
        r = self.S.res(name)
        self.stack[-1].append((g, r))
        return t, r

    def ring(self, kind, name, n, shape, dt):
        f = self.sb if kind == "sb" else self.ps
        return [f("%s%d" % (name, i), shape, dt) for i in range(n)]


def interleave(gens, stagger):
    active = []
    it = iter(gens)
    nxt = next(it, None)
    tick = 0
    while active or nxt is not None:
        if nxt is not None and tick % stagger == 0:
            active.append(nxt)
            nxt = next(it, None)
        for g in list(active):
            try:
                next(g)
            except StopIteration:
                active.remove(g)
        tick += 1


def build_program(debug=()):
    nc = bass.Bass("TRN2", target_bir_lowering=False)
    S = Sched(nc)
    C = Ctx(nc, S)
    dbg = set(debug)

    def din(name, shape, dt=F32):
        return nc.dram_tensor(name, list(shape), dt, kind="ExternalInput").ap()

    def scratch(name, shape, dt):
        kind = "ExternalOutput" if name in dbg else "Internal"
        r = S.res(name)
        r.dram = True
        return nc.dram_tensor(name, list(shape), dt, kind=kind).ap(), r

    x_d = din("x", [NTOK, D])
    cT_d = din("cT", [128, 8, NB])
    wmod_d = din("w_mod", [D, 6 * D])
    bmod_d = din("b_mod", [1, 6 * D])
    win_d = din("w_in", [D, DIN])
    ident_d = din("ident", [128, 128])
    kvw_d = din("kv_norm_w", [1, 128])
    ikw_d = din("idx_k_norm_w", [1, 64])
    ikb_d = din("idx_k_norm_b", [1, 64])
    dtb_d = din("dt_bias", [1, 16])
    convw_d = din("convw", [128, 16, 4])
    convb_d = din("convb", [128, 16])
    alog_d = din("a_log", [1, 16])
    dskip_d = din("d_skip", [1, 16])
    normw_d = din("ssm_norm_w", [1, D])
    triU_d = din("triU", [128, 128])
    SL_d = din("SL", [128, 128])
    negm4_d = din("negm4", [128, 512])
    tz_d = din("tz", [128, 2, 8, 128])
    cfar_d = din("cfar", [1, 8])
    wpa_d = din("w_proj_a", [D, D]); wpb_d = din("w_proj_b", [D, D]); wout_d = din("w_out", [D, D])
    ln1g_d = din("ln1_g", [1, D]); ln1b_d = din("ln1_b", [1, D]); ln2g_d = din("ln2_g", [1, D]); ln2b_d = din("ln2_b", [1, D])
    wr_d = din("w_router", [D, NE]); br_d = din("b_router", [1, NE])
    w1_d = din("w1", [NE * D, 2 * D]); w2_d = din("w2", [NE * D, D])
    b1r_d = din("b1r", [NE * 128, 16]); b2_d = din("b2", [NE, D])
    sut_d = din("sut", [128, 128]); thr16_d = din("thr16", [1, NE * 16]); bstart_d = din("bstart", [1, NBLK])
    kp_d = din("kp", [128, 8]); pcol_d = din("pcol", [128, 1]); sut32_d = din("sut32", [NE, NE])
    r_in = S.res("inputs")
    r_in.dram = True
    out_d = nc.dram_tensor("out", [NTOK, D], F32, kind="ExternalOutput").ap()
    r_out = S.res("out")
    r_out.dram = True

    mod_d, r_mod = scratch("mod_s", [NB, 6 * D], F32)
    qT_d, r_qT = scratch("qT_s", [8, 128, NTOK], BF16)
    iqT_d, r_iqT = scratch("iqT_s", [4, 128, NTOK], BF16)
    xbcT_d, r_xbcT = scratch("xbcT_s", [16, 128, NTOK], BF16)
    sgT_d, r_sgT = scratch("sgT_s", [16, 128, NTOK], BF16)
    kva_d, r_kva = scratch("kva_s", [NTOK, 136], BF16)
    kvT_d, r_kvT = scratch("kvT_s", [128, NTOK], BF16)
    ikT_d, r_ikT = scratch("ikT_s", [128, NTOK], BF16)
    iw_d, r_iw = scratch("iw_s", [NTOK, 8], F32)
    dt_d, r_dt = scratch("dt_s", [NTOK, 16], F32)
    zs_d, r_zs = scratch("zs_s", [NTOK, D], BF16)
    obT_d, r_obT = scratch("obT_s", [8, 128, NTOK], BF16)
    oaT_d, r_oaT = scratch("oaT_s", [8, 128, NTOK], BF16)
    mT_d, r_mTd = scratch("mT_s", [8, 128, NTOK], BF16)
    x1_d, r_x1 = scratch("x1_s", [NTOK, D], F32)
    u2_d, r_u2 = scratch("u2_s", [NTOK, D], BF16)
    xs_d, r_xsd = scratch("xsort_s", [NBLK * 512, D], BF16)
    ys_d, r_ysd = scratch("ysort_s", [NBLK * 512, D], BF16)

    w1b_d, r_w1bd = scratch("w1b_s", [NE * D, 2 * D], BF16)
    w2b_d, r_w2bd = scratch("w2b_s", [NE * D, D], BF16)

    C.push()
    ident_f, r_identf = C.sb("ident_f", [128, 128], F32)
    ident_b, r_identb = C.sb("ident_b", [128, 128], BF16)
    S.dma("sp", ident_f[:], ident_d, reads=[r_in], writes=[r_identf])
    S.op("dve", lambda e: e.tensor_copy(out=ident_b[:], in_=ident_f[:]), reads=[r_identf], writes=[r_identb])
    modT, r_modT = C.sb("modT", [128, 48, NB], F32)

    C.push()
    cT, r_cT = C.sb("cT", [128, 8, NB], F32)
    ones1, r_ones1 = C.sb("ones1", [1, NB], F32)
    bmod, r_bmod = C.sb("bmod", [1, 6 * D], F32)
    modrow, r_modrow = C.sb("modrow", [NB, 6 * D], F32)
    wm = C.ring("sb", "wm", 2, [128, 8, 512], F32)
    pmod = C.ring("ps", "pmod", 2, [NB, 512], F32)
    S.dma("sp", cT[:], cT_d, reads=[r_in], writes=[r_cT])
    S.dma("sp", bmod[:], bmod_d, reads=[r_in], writes=[r_bmod])
    S.op("act", lambda e: e.activation(out=cT[:], in_=cT[:], func=AF.Silu), reads=[r_cT], writes=[r_cT])
    S.op("dve", lambda e: e.memset(ones1[:], 1.0), writes=[r_ones1])
    for g in range(12):
        wt, r_wt = wm[g % 2]
        pt, r_pt = pmod[g % 2]
        S.dma("sp", wt[:], wmod_d[:, g * 512:(g + 1) * 512].rearrange("(k p) n -> p k n", p=128), reads=[r_in], writes=[r_wt])
        for k in range(8):
            S.op("pe", lambda e: e.matmul(pt[:], lhsT=cT[:, k, :], rhs=wt[:, k, :], start=(k == 0), stop=False),
                 reads=[r_cT, r_wt], writes=[r_pt] if k == 0 else (), awrites=() if k == 0 else [r_pt])
        S.op("pe", lambda e: e.matmul(pt[:], lhsT=ones1[:], rhs=bmod[:, g * 512:(g + 1) * 512], start=False, stop=True),
             reads=[r_ones1, r_bmod], awrites=[r_pt])
        S.op("act", lambda e: e.activation(out=modrow[:, g * 512:(g + 1) * 512], in_=pt[:], func=AF.Copy), reads=[r_pt], awrites=[r_modrow])
    S.dma("sp", mod_d, modrow[:], reads=[r_modrow], writes=[r_mod])
    pmt, r_pmt = pmod[0]
    pmt2, r_pmt2 = C.ps("pmodT", [128, 48 * NB], F32)
    for j in range(48):
        S.op("pe", lambda e: e.transpose(out=pmt2[:, j * NB:(j + 1) * NB], in_=modrow[:, j * 128:(j + 1) * 128], identity=ident_f[0:NB, 0:NB]),
             reads=[r_modrow, r_identf], writes=[r_pmt2] if j == 0 else (), awrites=() if j == 0 else [r_pmt2])
    S.op("act", lambda e: e.activation(out=modT[:].rearrange("p j b -> p (j b)"), in_=pmt2[:], func=AF.Copy), reads=[r_pmt2], writes=[r_modT])
    S.op("dve", lambda e: e.tensor_scalar_add(out=modT[:, 8:16, :], in0=modT[:, 8:16, :], scalar1=1.0), reads=[r_modT], awrites=[r_modT])
    S.op("dve", lambda e: e.tensor_scalar_add(out=modT[:, 32:40, :], in0=modT[:, 32:40, :], scalar1=1.0), reads=[r_modT], awrites=[r_modT])
    C.pop()
    if "stop0" in dbg:
        C.pop()
        return nc

    C.push()
    wI, r_wI = C.sb("wI", [128, 8, DIN], BF16)
    for i, (a, b_) in enumerate([(0, 1024), (1024, 1736), (1736, 2760), (2760, 3784), (3784, 4808), (4808, 5848), (5848, 6872)]):
        S.dma("pool", wI[:, :, a:b_], win_d[:, a:b_].rearrange("(k p) n -> p k n", p=128), reads=[r_in],
              writes=[r_wI] if i == 0 else (), awrites=() if i == 0 else [r_wI])
    kvw_bc, r_kvw = C.sb("kvw_bc", [128, 128], F32)
    ikw_bc, r_ikw = C.sb("ikw_bc", [128, 64], F32)
    ikb_bc, r_ikb = C.sb("ikb_bc", [128, 64], F32)
    dtb_bc, r_dtb = C.sb("dtb_bc", [128, 16], F32)
    S.dma("sp", kvw_bc[:], kvw_d.partition_broadcast(128), reads=[r_in], writes=[r_kvw])
    S.dma("sp", ikw_bc[:], ikw_d.partition_broadcast(128), reads=[r_in], writes=[r_ikw])
    S.dma("sp", ikb_bc[:], ikb_d.partition_broadcast(128), reads=[r_in], writes=[r_ikb])
    S.dma("sp", dtb_bc[:], dtb_d.partition_broadcast(128), reads=[r_in], writes=[r_dtb])

    xt = C.ring("sb", "xt", 2, [128, D], F32)
    xn = C.ring("sb", "xn", 2, [128, D], BF16)
    st = C.ring("sb", "st", 2, [128, 2, 6], F32)
    mv = C.ring("sb", "mv", 2, [128, 2], F32)
    rs = C.ring("sb", "rs", 2, [128, 1], F32)
    uT = C.ring("sb", "uT", 2, [128, 8, 512], BF16)
    ptr = C.ring("ps", "ptr", 1, [128, 8, 128], BF16)
    psm = C.ring("ps", "psm", 1, [128, 512], F32)
    pz = C.ring("ps", "pz", 2, [128, 512], F32)
    pf = C.ring("ps", "pf", 3, [128, 512], F32)
    pt2 = C.ring("ps", "pt2", 1, [128, 2, 128], BF16)
    kva = C.ring("sb", "kva", 2, [128, 136], BF16)
    ikn = C.ring("sb", "ikn", 2, [128, 128], BF16)
    sml = C.ring("sb", "sml", 2, [128, 64], F32)
    ikf = C.ring("sb", "ikf", 2, [128, 64], F32)
    iwt = C.ring("sb", "iwt", 2, [128, 8], F32)
    dtt = C.ring("sb", "dtt", 2, [128, 4, 16], F32)
    zst = C.ring("sb", "zst", 2, [128, D], BF16)
    tT = C.ring("sb", "tT", 2, [128, 2, 128], BF16)
    stg = C.ring("sb", "stg", 2, [128, 8, 512], BF16)
    for i in range(2):
        S.op("pool", lambda e: e.memset(kva[i][0][:, 128:136], 1.0), writes=[kva[i][1]])

    def ln_stats(src, r_src, i):
        st_t, r_st = st[i]
        mv_t, r_mv = mv[i]
        rs_t, r_rs = rs[i]
        for j in range(2):
            S.op("dve", lambda e: e.bn_stats(out=st_t[:, j, :], in_=src[:, j * 512:(j + 1) * 512]), reads=[r_src],
                 writes=[r_st] if j == 0 else (), awrites=() if j == 0 else [r_st])
        S.op("dve", lambda e: e.bn_aggr(out=mv_t[:], in_=st_t[:].rearrange("p a b -> p (a b)")), reads=[r_st], writes=[r_mv])
        S.op("act", lambda e: e.activation(out=rs_t[:], in_=mv_t[:, 1:2], func=AF.Sqrt, bias=EPS), reads=[r_mv], writes=[r_rs])
        S.op("dve", lambda e: e.reciprocal(out=rs_t[:], in_=rs_t[:]), reads=[r_rs], writes=[r_rs])
        return mv_t, r_mv, rs_t, r_rs

    tcount = 0
    for g in range(NTOK // 512):
        b = (g * 512) // SEQ
        u_t, r_u = uT[g % 2]
        for i4 in range(4):
            t = g * 4 + i4
            tok0 = t * 128
            ri = tcount % 2
            tcount += 1
            x_t, r_x = xt[ri]
            xn_t, r_xn = xn[ri]
            if t == 0:
                S.dma("sp", x_t[:], x_d[0:128, :], reads=[r_in], writes=[r_x])
            if t + 1 < NT:
                S.dma("sp", xt[(ri + 1) % 2][0][:], x_d[tok0 + 128:tok0 + 256, :], reads=[r_in], writes=[xt[(ri + 1) % 2][1]])
            mv_t, r_mv, rs_t, r_rs = ln_stats(x_t, r_x, ri)
            S.op("dve", lambda e: e.tensor_scalar(out=xn_t[:], in0=x_t[:], scalar1=mv_t[:, 0:1], scalar2=rs_t[:], op0=ALU.subtract, op1=ALU.mult),
                 reads=[r_x, r_mv, r_rs], writes=[r_xn])
            p_t, r_p = ptr[0]
            for k in range(8):
                S.op("pe", lambda e: e.transpose(out=p_t[:, k, :], in_=xn_t[:, k * 128:(k + 1) * 128], identity=ident_b[:]),
                     reads=[r_xn, r_identb], writes=[r_p] if k == 0 else (), awrites=() if k == 0 else [r_p])
            for k in range(8):
                S.op("act", lambda e: e.activation(out=u_t[:, k, i4 * 128:(i4 + 1) * 128], in_=p_t[:, k, :], func=AF.Identity,
                                                   scale=modT[:, 8 + k, b:b + 1], bias=modT[:, k, b:b + 1]),
                     reads=[r_p, r_modT], writes=[r_u] if (k == 0 and i4 == 0) else (), awrites=() if (k == 0 and i4 == 0) else [r_u])
            ps_t, r_ps = psm[0]
            for (c0, c1, o0) in [(C_KV, C_KV + 128, 0), (C_IK, C_IK + 72, 128), (C_DT, C_DT + 16, 200)]:
                for k in range(8):
                    S.op("pe", lambda e: e.matmul(ps_t[:, o0:o0 + (c1 - c0)], lhsT=u_t[:, k, i4 * 128:(i4 + 1) * 128], rhs=wI[:, k, c0:c1], start=(k == 0), stop=(k == 7)),
                         reads=[r_u, r_wI], writes=[r_ps] if (k == 0 and o0 == 0) else (), awrites=() if (k == 0 and o0 == 0) else [r_ps])
            zp = []
            for h in range(2):
                pz_t, r_pz = pz[h]
                zp.append((pz_t, r_pz))
                for k in range(8):
                    S.op("pe", lambda e: e.matmul(pz_t[:], lhsT=u_t[:, k, i4 * 128:(i4 + 1) * 128], rhs=wI[:, k, C_Z + h * 512:C_Z + (h + 1) * 512], start=(k == 0), stop=(k == 7)),
                         reads=[r_u, r_wI], writes=[r_pz] if k == 0 else (), awrites=() if k == 0 else [r_pz])
            sm_t, r_sm = sml[ri]
            kva_t, r_kva_t = kva[ri]
            ikf_t, r_ikf = ikf[ri]
            S.op("act", lambda e: e.activation(out=ikf_t[:, 0:64], in_=ps_t[:, 0:64], func=AF.Square, accum_out=sm_t[:, 0:1]), reads=[r_ps], writes=[r_ikf, r_sm])
            S.op("act", lambda e: e.activation(out=ikf_t[:, 0:64], in_=ps_t[:, 64:128], func=AF.Square, accum_out=sm_t[:, 1:2]), reads=[r_ps], writes=[r_ikf], awrites=[r_sm])
            S.op("dve", lambda e: e.tensor_tensor(out=sm_t[:, 0:1], in0=sm_t[:, 0:1], in1=sm_t[:, 1:2], op=ALU.add), reads=[r_sm], awrites=[r_sm])
            S.op("act", lambda e: e.activation(out=sm_t[:, 2:3], in_=sm_t[:, 0:1], func=AF.Sqrt, scale=1.0 / 128.0, bias=EPS), reads=[r_sm], awrites=[r_sm])
            S.op("dve", lambda e: e.reciprocal(out=sm_t[:, 3:4], in_=sm_t[:, 2:3]), reads=[r_sm], awrites=[r_sm])
            S.op("dve", lambda e: e.scalar_tensor_tensor(out=kva_t[:, 0:128], in0=ps_t[:, 0:128], scalar=sm_t[:, 3:4], in1=kvw_bc[:], op0=ALU.mult, op1=ALU.mult),
                 reads=[r_ps, r_sm, r_kvw], awrites=[r_kva_t])
            S.dma("sp", kva_d[tok0:tok0 + 128, :], kva_t[:], reads=[r_kva_t], awrites=[r_kva])
            ik_t, r_ik = ikn[ri]
            S.op("dve", lambda e: e.bn_stats(out=sm_t[:, 8:14], in_=ps_t[:, 128:192]), reads=[r_ps], awrites=[r_sm])
            S.op("dve", lambda e: e.bn_aggr(out=sm_t[:, 16:18], in_=sm_t[:, 8:14]), reads=[r_sm], awrites=[r_sm])
            S.op("act", lambda e: e.activation(out=sm_t[:, 18:19], in_=sm_t[:, 17:18], func=AF.Sqrt, bias=EPS), reads=[r_sm], awrites=[r_sm])
            S.op("dve", lambda e: e.reciprocal(out=sm_t[:, 19:20], in_=sm_t[:, 18:19]), reads=[r_sm], awrites=[r_sm])
            S.op("dve", lambda e: e.tensor_scalar(out=ikf_t[:], in0=ps_t[:, 128:192], scalar1=sm_t[:, 16:17], scalar2=sm_t[:, 19:20], op0=ALU.subtract, op1=ALU.mult),
                 reads=[r_ps, r_sm], writes=[r_ikf])
            S.op("dve", lambda e: e.tensor_tensor(out=ikf_t[:], in0=ikf_t[:], in1=ikw_bc[:], op=ALU.mult), reads=[r_ikf, r_ikw], writes=[r_ikf])
            S.op("dve", lambda e: e.tensor_tensor(out=ik_t[:, 0:64], in0=ikf_t[:], in1=ikb_bc[:], op=ALU.add), reads=[r_ikf, r_ikb], writes=[r_ik])
            S.op("dve", lambda e: e.tensor_copy(out=ik_t[:, 64:128], in_=ik_t[:, 0:64]), reads=[r_ik], awrites=[r_ik])
            iw_t, r_iwt = iwt[ri]
            S.op("act", lambda e: e.mul(out=iw_t[:], in_=ps_t[:, 192:200], mul=float(8 ** -0.5 * 64 ** -0.5)), reads=[r_ps], writes=[r_iwt])
            S.dma("sp", iw_d[tok0:tok0 + 128, :], iw_t[:], reads=[r_iwt], awrites=[r_iw])
            d_t, r_d = dtt[ri]
            S.op("dve", lambda e: e.tensor_tensor(out=d_t[:, 0, :], in0=ps_t[:, 200:216], in1=dtb_bc[:], op=ALU.add), reads=[r_ps, r_dtb], writes=[r_d])
            S.op("act", lambda e: e.activation(out=d_t[:, 1, :], in_=d_t[:, 0, :], func=AF.Abs), reads=[r_d], awrites=[r_d])
            S.op("act", lambda e: e.activation(out=d_t[:, 1, :], in_=d_t[:, 1, :], func=AF.Exp, scale=-1.0), reads=[r_d], awrites=[r_d])
            S.op("act", lambda e: e.activation(out=d_t[:, 1, :], in_=d_t[:, 1, :], func=AF.Ln, bias=1.0), reads=[r_d], awrites=[r_d])
            S.op("dve", lambda e: e.scalar_tensor_tensor(out=d_t[:, 2, :], in0=d_t[:, 0, :], scalar=0.0, in1=d_t[:, 1, :], op0=ALU.max, op1=ALU.add), reads=[r_d], awrites=[r_d])
            S.dma("sp", dt_d[tok0:tok0 + 128, :], d_t[:, 2, :], reads=[r_d], awrites=[r_dt])
            z_t, r_z = zst[ri]
            for h in range(2):
                S.op("act", lambda e: e.activation(out=z_t[:, h * 512:(h + 1) * 512], in_=zp[h][0][:], func=AF.Silu), reads=[zp[h][1]],
                     writes=[r_z] if h == 0 else (), awrites=() if h == 0 else [r_z])
            S.dma("sp", zs_d[tok0:tok0 + 128, :], z_t[:], reads=[r_z], awrites=[r_zs])
            p2, r_p2 = pt2[0]
            t_t, r_t = tT[ri]
            S.op("pe", lambda e: e.transpose(out=p2[:, 0, :], in_=kva_t[:, 0:128], identity=ident_b[:]), reads=[r_kva_t, r_identb], writes=[r_p2])
            S.op("pe", lambda e: e.transpose(out=p2[:, 1, :], in_=ik_t[:], identity=ident_b[:]), reads=[r_ik, r_identb], awrites=[r_p2])
            S.op("dve", lambda e: e.tensor_copy(out=t_t[:], in_=p2[:]), reads=[r_p2], writes=[r_t])
            S.dma("sp", kvT_d[:, tok0:tok0 + 128], t_t[:, 0, :], reads=[r_t], awrites=[r_kvT])
            S.dma("sp", ikT_d[:, tok0:tok0 + 128], t_t[:, 1, :], reads=[r_t], awrites=[r_ikT])
        g0 = g * 512
        fcount = 0
        for (c0, nch, dst, r_dst, ch0, func, scl) in [
                (C_Q, 8, qT_d, r_qT, 0, AF.Copy, float(128 ** -0.5)),
                (C_IQ, 4, iqT_d, r_iqT, 0, AF.Copy, 1.0),
                (C_XBC, 8, xbcT_d, r_xbcT, 0, AF.Copy, 1.0),
                (C_XBC + 1024, 8, xbcT_d, r_xbcT, 8, AF.Copy, 1.0),
                (C_GA, 8, sgT_d, r_sgT, 0, AF.Sigmoid, 1.0),
                (C_GB, 8, sgT_d, r_sgT, 8, AF.Sigmoid, 1.0)]:
            sg_t, r_sg = stg[fcount % 2]
            fcount += 1
            for j in range(nch):
                pf_t, r_pf = pf[j % 3]
                for k in range(8):
                    S.op("pe", lambda e: e.matmul(pf_t[:], lhsT=wI[:, k, c0 + j * 128:c0 + (j + 1) * 128], rhs=u_t[:, k, :], start=(k == 0), stop=(k == 7)),
                         reads=[r_u, r_wI], writes=[r_pf] if k == 0 else (), awrites=() if k == 0 else [r_pf])
                if func == AF.Copy and j % 2 == 1:
                    S.op("dve", lambda e: e.tensor_scalar_mul(out=sg_t[:, j, :], in0=pf_t[:], scalar1=scl), reads=[r_pf],
                         writes=[r_sg] if j == 0 else (), awrites=() if j == 0 else [r_sg])
                else:
                    S.op("act", lambda e: e.activation(out=sg_t[:, j, :], in_=pf_t[:], func=func, scale=scl), reads=[r_pf],
                         writes=[r_sg] if j == 0 else (), awrites=() if j == 0 else [r_sg])
            S.dma("sp", dst[ch0:ch0 + nch, :, g0:g0 + 512].rearrange("c p t -> p c t"), sg_t[:, 0:nch, :], reads=[r_sg], awrites=[r_dst])
    C.pop()
    if "stopA" in dbg:
        C.pop()
        return nc

    phase_B(nc, S, C, dbg, locals())
    if "stopB" in dbg:
        C.pop()
        return nc

    phase_C(nc, S, C, dbg, locals())
    if "stopC" in dbg:
        C.pop()
        return nc

    phase_DEF(nc, S, C, dbg, locals())
    C.pop()
    return nc


def phase_DEF(nc, S, C, dbg, L):
    g_ = lambda n: L[n]
    r_in = g_("r_in"); ident_b = g_("ident_b"); r_identb = g_("r_identb"); ident_f = g_("ident_f"); r_identf = g_("r_identf")
    modT = g_("modT"); r_modT = g_("r_modT"); mod_d = g_("mod_d"); r_mod = g_("r_mod")
    x_d = g_("x_d"); oaT_d, r_oaT = g_("oaT_d"), g_("r_oaT"); obT_d, r_obT = g_("obT_d"), g_("r_obT"); sgT_d, r_sgT = g_("sgT_d"), g_("r_sgT")
    x1_d, r_x1 = g_("x1_d"), g_("r_x1"); u2_d, r_u2 = g_("u2_d"), g_("r_u2"); xs_d, r_xsd = g_("xs_d"), g_("r_xsd"); ys_d, r_ysd = g_("ys_d"), g_("r_ysd")
    out_d, r_out = g_("out_d"), g_("r_out")
    b1r_d, b2_d = g_("b1r_d"), g_("b2_d")
    w1b_d, r_w1bd, w2b_d, r_w2bd = g_("w1b_d"), g_("r_w1bd"), g_("w2b_d"), g_("r_w2bd")

    C.push()
    idx4, r_idx4 = C.sb("idx4", [128, NT, 4], I32)
    g4, r_g4 = C.sb("g4", [128, NT, 4], F32)
    widx, r_widx = C.sb("widx", [128, NBLK, 8], I32)
    bidx, r_bidx = C.sb("bidx", [128, NBLK], I32)
    eidx, r_eidx = C.sb("eidx", [128, NBLK], I32)

    def ln_stats(src, r_src, st_t, r_st, mv_t, r_mv, rs_t, r_rs):
        for j in range(2):
            S.op("dve", lambda e: e.bn_stats(out=st_t[:, j, :], in_=src[:, j * 512:(j + 1) * 512]), reads=[r_src],
                 writes=[r_st] if j == 0 else (), awrites=() if j == 0 else [r_st])
        S.op("dve", lambda e: e.bn_aggr(out=mv_t[:], in_=st_t[:].rearrange("p a b -> p (a b)")), reads=[r_st], writes=[r_mv])
        S.op("act", lambda e: e.activation(out=rs_t[:], in_=mv_t[:, 1:2], func=AF.Sqrt, bias=EPS), reads=[r_mv], writes=[r_rs])
        S.op("dve", lambda e: e.reciprocal(out=rs_t[:], in_=rs_t[:]), reads=[r_rs], writes=[r_rs])

    mT_d, r_mTd = g_("mT_d"), g_("r_mTd")
    C.push()
    wpa, r_wpa = C.sb("wpa", [128, 8, D], BF16)
    wpb, r_wpb = C.sb("wpb", [128, 8, D], BF16)
    S.dma("pool", wpa[:], g_("wpa_d").rearrange("(k p) n -> p k n", p=128), reads=[r_in], writes=[r_wpa])
    S.dma("pool", wpb[:], g_("wpb_d").rearrange("(k p) n -> p k n", p=128), reads=[r_in], writes=[r_wpb])
    oaTr = C.ring("sb", "oaTd", 2, [128, 8, 512], BF16)
    obTr = C.ring("sb", "obTd", 2, [128, 8, 512], BF16)
    sgTr = C.ring("sb", "sgTd", 2, [128, 16, 512], BF16)
    mTr = C.ring("sb", "mT", 2, [128, 8, 512], BF16)
    ta = C.ring("sb", "ta", 3, [128, 512], F32)
    tb = C.ring("sb", "tb", 3, [128, 512], F32)
    pab = C.ring("ps", "pab", 4, [128, 512], F32)
    pbb = C.ring("ps", "pbb", 4, [128, 512], F32)

    def loadD1(g):
        g0 = g * 512
        S.dma("sp", oaTr[g % 2][0][:], oaT_d[:, :, g0:g0 + 512].rearrange("c p t -> p c t"), reads=[r_oaT], writes=[oaTr[g % 2][1]])
        S.dma("sp", obTr[g % 2][0][:], obT_d[:, :, g0:g0 + 512].rearrange("c p t -> p c t"), reads=[r_obT], writes=[obTr[g % 2][1]])
        S.dma("sp", sgTr[g % 2][0][:], sgT_d[:, :, g0:g0 + 512].rearrange("c p t -> p c t"), reads=[r_sgT], writes=[sgTr[g % 2][1]])

    NG = NTOK // 512
    loadD1(0)
    cn = 0
    for g in range(NG):
        g0 = g * 512
        if g + 1 < NG:
            loadD1(g + 1)
        oaT, r_oaTs = oaTr[g % 2]; obT, r_obTs = obTr[g % 2]; sgT, r_sgTs = sgTr[g % 2]; mT, r_mT = mTr[g % 2]
        for n in range(8):
            pa, r_pa = pab[cn % 4]
            pb, r_pb = pbb[cn % 4]
            ta_t, r_ta = ta[cn % 3]
            tb_t, r_tb = tb[cn % 3]
            cn += 1
            for k in range(8):
                S.op("pe", lambda e: e.matmul(pa[:], lhsT=wpa[:, k, n * 128:(n + 1) * 128], rhs=oaT[:, k, :], start=(k == 0), stop=(k == 7)),
                     reads=[r_wpa, r_oaTs], writes=[r_pa] if k == 0 else (), awrites=() if k == 0 else [r_pa])
            for k in range(8):
                S.op("pe", lambda e: e.matmul(pb[:], lhsT=wpb[:, k, n * 128:(n + 1) * 128], rhs=obT[:, k, :], start=(k == 0), stop=(k == 7)),
                     reads=[r_wpb, r_obTs], writes=[r_pb] if k == 0 else (), awrites=() if k == 0 else [r_pb])
            S.op("dve", lambda e: e.tensor_tensor(out=ta_t[:], in0=pa[:], in1=sgT[:, n, :], op=ALU.mult), reads=[r_pa, r_sgTs], writes=[r_ta])
            S.op("dve", lambda e: e.tensor_tensor(out=tb_t[:], in0=pb[:], in1=sgT[:, 8 + n, :], op=ALU.mult), reads=[r_pb, r_sgTs], writes=[r_tb])
            S.op("pool", lambda e: e.tensor_tensor(out=mT[:, n, :], in0=ta_t[:], in1=tb_t[:], op=ALU.add), reads=[r_ta, r_tb],
                 writes=[r_mT] if n == 0 else (), awrites=() if n == 0 else [r_mT])
        S.dma("sp", mT_d[:, :, g0:g0 + 512].rearrange("c p t -> p c t"), mT[:], reads=[r_mT], awrites=[r_mTd])
    C.pop()

    C.push()
    wout, r_wout = C.sb("wout", [128, 8, D], BF16)
    S.dma("pool", wout[:], g_("wout_d").rearrange("(k p) n -> p k n", p=128), reads=[r_in], writes=[r_wout])
    wr, r_wr = C.sb("wr", [128, 8, NE], F32)
    S.dma("sp", wr[:], g_("wr_d").rearrange("(k p) n -> p k n", p=128), reads=[r_in], writes=[r_wr])
    br_bc, r_br = C.sb("br_bc", [128, NE], F32)
    S.dma("sp", br_bc[:], g_("br_d").partition_broadcast(128), reads=[r_in], writes=[r_br])
    ln1g, r_ln1g = C.sb("ln1g", [128, D], F32)
    ln1b, r_ln1b = C.sb("ln1b", [128, D], F32)
    S.dma("sp", ln1g[:], g_("ln1g_d").partition_broadcast(128), reads=[r_in], writes=[r_ln1g])
    S.dma("sp", ln1b[:], g_("ln1b_d").partition_broadcast(128), reads=[r_in], writes=[r_ln1b])
    gater = C.ring("sb", "gate1", 2, [128, D], F32)
    sc2r = C.ring("sb", "sc2", 2, [128, D], F32)
    sh2r = C.ring("sb", "sh2", 2, [128, D], F32)
    sutf, r_sutf = C.sb("sutf", [128, 128], F32)
    sutb, r_sutb = C.sb("sutb", [128, 128], BF16)
    onesb, r_onesb = C.sb("onesb", [128, 128], BF16)
    S.dma("sp", sutf[:], g_("sut_d"), reads=[r_in], writes=[r_sutf])
    S.op("dve", lambda e: e.tensor_copy(out=sutb[:], in_=sutf[:]), reads=[r_sutf], writes=[r_sutb])
    S.op("dve", lambda e: e.memset(onesb[:], 1.0), writes=[r_onesb])
    maskall, r_maskall = C.sb("maskall", [128, NT, NE], F32)
    gall, r_gall = C.sb("gall", [128, NT, NE], F32)
    posall, r_posall = C.sb("posall", [128, NT, NE], F32)
    rrun, r_rrun = C.sb("rrun", [128, NE], F32)
    S.op("dve", lambda e: e.memset(rrun[:], 0.0), writes=[r_rrun])
    mtr = C.ring("sb", "mtl", 4, [128, 8, 128], BF16)
    xt = C.ring("sb", "xtd", 4, [128, D], F32)
    r1_r = C.ring("sb", "r1", 3, [128, D], F32)
    x1t_r = C.ring("sb", "x1t", 3, [128, D], F32)
    u2t_r = C.ring("sb", "u2t", 3, [128, D], F32)
    u2b = C.ring("sb", "u2b", 3, [128, D], BF16)
    u2T_r = C.ring("sb", "u2T", 3, [128, 8, 128], F32)
    st_r = C.ring("sb", "stD", 6, [128, 2, 6], F32)
    mv_r = C.ring("sb", "mvD", 6, [128, 2], F32)
    rs_r = C.ring("sb", "rsD", 6, [128, 1], F32)
    lg_r = C.ring("sb", "lg", 3, [128, NE], F32)
    m8_r = C.ring("sb", "m8D", 3, [128, 8], F32)
    sml_r = C.ring("sb", "smlD", 3, [128, 8], F32)
    ex_r = C.ring("sb", "exD", 3, [128, NE], F32)
    maskb_r = C.ring("sb", "maskb", 3, [128, NE], BF16)
    prs = C.ring("ps", "prs", 4, [128, 512], F32)
    ptT = C.ring("ps", "ptT", 2, [128, 512], F32)
    psl = C.ring("ps", "psl", 2, [128, 512], F32)

    def loadD2(t):
        tok0 = t * 128
        S.dma("sp", xt[t % 4][0][:], x_d[tok0:tok0 + 128, :], reads=[r_in], writes=[xt[t % 4][1]])
        S.dma("sp", mtr[t % 4][0][:], mT_d[:, :, tok0:tok0 + 128].rearrange("c p t -> p c t"), reads=[r_mTd], writes=[mtr[t % 4][1]])
        if t % TPS == 0:
            b = t // TPS
            gate1, r_gate1 = gater[b % 2]; sc2, r_sc2 = sc2r[b % 2]; sh2, r_sh2 = sh2r[b % 2]
            S.dma("sp", gate1[:], mod_d[b:b + 1, 2 * D:3 * D].partition_broadcast(128), reads=[r_mod], writes=[r_gate1])
            S.dma("sp", sh2[:], mod_d[b:b + 1, 3 * D:4 * D].partition_broadcast(128), reads=[r_mod], writes=[r_sh2])
            S.dma("sp", sc2[:], mod_d[b:b + 1, 4 * D:5 * D].partition_broadcast(128), reads=[r_mod], writes=[r_sc2])
            S.op("pool", lambda e: e.tensor_scalar_add(out=sc2[:], in0=sc2[:], scalar1=1.0), reads=[r_sc2], writes=[r_sc2])

    def stage1(t):
        tok0 = t * 128
        b = t // TPS
        gate1, r_gate1 = gater[b % 2]; sc2, r_sc2 = sc2r[b % 2]; sh2, r_sh2 = sh2r[b % 2]
        x_t, r_x = xt[t % 4]
        mt_t, r_mt = mtr[t % 4]
        r1, r_r1 = r1_r[t % 3]; x1t, r_x1t = x1t_r[t % 3]; u2t, r_u2t = u2t_r[t % 3]
        st, r_st = st_r[(2 * t) % 6]; mv, r_mv = mv_r[(2 * t) % 6]; rs, r_rs = rs_r[(2 * t) % 6]
        st2, r_st2 = st_r[(2 * t + 1) % 6]; mv2, r_mv2 = mv_r[(2 * t + 1) % 6]; rs2, r_rs2 = rs_r[(2 * t + 1) % 6]
        for hf in range(2):
            pr_t, r_pr = prs[(2 * t + hf) % 4]
            for n in range(8):
                S.op("pe", lambda e: e.matmul(pr_t[:], lhsT=mt_t[:, n, :], rhs=wout[:, n, hf * 512:(hf + 1) * 512], start=(n == 0), stop=(n == 7)),
                     reads=[r_mt, r_wout], writes=[r_pr] if n == 0 else (), awrites=() if n == 0 else [r_pr])
                yield
            S.op("dve", lambda e: e.tensor_tensor(out=r1[:, hf * 512:(hf + 1) * 512], in0=pr_t[:], in1=gate1[:, hf * 512:(hf + 1) * 512], op=ALU.mult),
                 reads=[r_pr, r_gate1], writes=[r_r1] if hf == 0 else (), awrites=() if hf == 0 else [r_r1])
            yield
        S.op("dve", lambda e: e.scalar_tensor_tensor(out=r1[:], in0=x_t[:], scalar=float(ALPHA), in1=r1[:], op0=ALU.mult, op1=ALU.add), reads=[r_x, r_r1], writes=[r_r1])
        yield
        ln_stats(r1, r_r1, st, r_st, mv, r_mv, rs, r_rs)
        yield
        S.op("dve", lambda e: e.tensor_scalar(out=x1t[:], in0=r1[:], scalar1=mv[:, 0:1], scalar2=rs[:], op0=ALU.subtract, op1=ALU.mult), reads=[r_r1, r_mv, r_rs], writes=[r_x1t])
        yield
        S.op("pool", lambda e: e.tensor_tensor(out=x1t[:], in0=x1t[:], in1=ln1g[:], op=ALU.mult), reads=[r_x1t, r_ln1g], writes=[r_x1t])
        yield
        S.op("pool", lambda e: e.tensor_tensor(out=x1t[:], in0=x1t[:], in1=ln1b[:], op=ALU.add), reads=[r_x1t, r_ln1b], writes=[r_x1t])
        yield
        S.dma("sp", x1_d[tok0:tok0 + 128, :], x1t[:], reads=[r_x1t], awrites=[r_x1])
        yield
        ln_stats(x1t, r_x1t, st2, r_st2, mv2, r_mv2, rs2, r_rs2)
        yield
        S.op("dve", lambda e: e.tensor_scalar(out=u2t[:], in0=x1t[:], scalar1=mv2[:, 0:1], scalar2=rs2[:], op0=ALU.subtract, op1=ALU.mult), reads=[r_x1t, r_mv2, r_rs2], writes=[r_u2t])
        yield
        S.op("pool", lambda e: e.tensor_tensor(out=u2t[:], in0=u2t[:], in1=sc2[:], op=ALU.mult), reads=[r_u2t, r_sc2], writes=[r_u2t])
        yield
        S.op("pool", lambda e: e.tensor_tensor(out=u2t[:], in0=u2t[:], in1=sh2[:], op=ALU.add), reads=[r_u2t, r_sh2], writes=[r_u2t])
        yield
        ub, r_ub = u2b[t % 3]
        S.op("act", lambda e: e.activation(out=ub[:], in_=u2t[:], func=AF.Copy), reads=[r_u2t], writes=[r_ub])
        yield
        S.dma("sp", u2_d[tok0:tok0 + 128, :], ub[:], reads=[r_ub], awrites=[r_u2])
        yield

    def stage2(t):
        u2t, r_u2t = u2t_r[t % 3]
        u2T, r_u2T = u2T_r[t % 3]
        lg, r_lg = lg_r[t % 3]; m8, r_m8 = m8_r[t % 3]; sml, r_sml = sml_r[t % 3]; ex, r_ex = ex_r[t % 3]; maskb, r_maskb = maskb_r[t % 3]
        for hf in range(2):
            pT, r_pT = (ptT[t % 2] if hf == 0 else psl[t % 2])
            for k4 in range(4):
                k = hf * 4 + k4
                S.op("pe", lambda e: e.transpose(out=pT[:, k4 * 128:(k4 + 1) * 128], in_=u2t[:, k * 128:(k + 1) * 128], identity=ident_f[:]),
                     reads=[r_u2t, r_identf], writes=[r_pT] if k4 == 0 else (), awrites=() if k4 == 0 else [r_pT])
                yield
            S.op("act", lambda e: e.activation(out=u2T[:, hf * 4:hf * 4 + 4, :].rearrange("p k t -> p (k t)"), in_=pT[:], func=AF.Copy), reads=[r_pT],
                 writes=[r_u2T] if hf == 0 else (), awrites=() if hf == 0 else [r_u2T])
            yield
        pl_, r_pl = psl[t % 2]
        for k in range(8):
            S.op("pe", lambda e: e.matmul(pl_[:, 0:NE], lhsT=u2T[:, k, :], rhs=wr[:, k, :], start=(k == 0), stop=(k == 7)),
                 reads=[r_u2T, r_wr], writes=[r_pl] if k == 0 else (), awrites=() if k == 0 else [r_pl])
            yield
        S.op("dve", lambda e: e.tensor_tensor(out=lg[:], in0=pl_[:, 0:NE], in1=br_bc[:], op=ALU.add), reads=[r_pl, r_br], writes=[r_lg])
        yield
        S.op("dve", lambda e: e.max(out=m8[:], in_=lg[:]), reads=[r_lg], writes=[r_m8])
        yield
        S.op("dve", lambda e: e.tensor_scalar(out=maskall[:, t, :], in0=lg[:], scalar1=m8[:, 3:4], scalar2=None, op0=ALU.is_ge), reads=[r_lg, r_m8], awrites=[r_maskall])
        yield
        S.op("dve", lambda e: e.tensor_scalar_mul(out=sml[:, 0:1], in0=m8[:, 0:1], scalar1=-1.0), reads=[r_m8], writes=[r_sml])
        yield
        S.op("act", lambda e: e.activation(out=ex[:], in_=lg[:], func=AF.Exp, bias=sml[:, 0:1]), reads=[r_lg, r_sml], writes=[r_ex])
        yield
        S.op("dve", lambda e: e.tensor_tensor(out=ex[:], in0=ex[:], in1=maskall[:, t, :], op=ALU.mult), reads=[r_ex, r_maskall], writes=[r_ex])
        yield
        S.op("dve", lambda e: e.reduce_sum(out=sml[:, 1:2], in_=ex[:], axis=AX.X), reads=[r_ex], awrites=[r_sml])
        yield
        S.op("dve", lambda e: e.reciprocal(out=sml[:, 2:3], in_=sml[:, 1:2]), reads=[r_sml], awrites=[r_sml])
        yield
        S.op("dve", lambda e: e.tensor_scalar(out=gall[:, t, :], in0=ex[:], scalar1=sml[:, 2:3], scalar2=None, op0=ALU.mult), reads=[r_ex, r_sml], awrites=[r_gall])
        yield
        S.op("dve", lambda e: e.tensor_copy(out=maskb[:], in_=maskall[:, t, :]), reads=[r_maskall], writes=[r_maskb])
        yield
        S.op("pe", lambda e: e.matmul(pl_[:, 64:64 + NE], lhsT=sutb[:], rhs=maskb[:], start=True, stop=True), reads=[r_sutb, r_maskb, r_lg], awrites=[r_pl])
        yield
        S.op("pe", lambda e: e.matmul(pl_[:, 128:128 + NE], lhsT=onesb[:], rhs=maskb[:], start=True, stop=True), reads=[r_onesb, r_maskb], awrites=[r_pl])
        yield
        S.op("dve", lambda e: e.tensor_tensor(out=posall[:, t, :], in0=pl_[:, 64:64 + NE], in1=rrun[:], op=ALU.add), reads=[r_pl, r_rrun], awrites=[r_posall])
        yield
        S.op("dve", lambda e: e.tensor_tensor(out=rrun[:], in0=pl_[:, 128:128 + NE], in1=rrun[:], op=ALU.add), reads=[r_pl, r_rrun], writes=[r_rrun])
        yield


    def tile_gen(t):
        if t + 3 < NT:
            loadD2(t + 3)
        yield from stage1(t)
        yield from stage2(t)

    for t0 in range(3):
        loadD2(t0)
    interleave((tile_gen(t) for t in range(NT)), 26)

    thr16, r_thr16 = C.sb("thr16", [128, NE, 16], F32)
    bstart, r_bstart = C.sb("bstart", [128, NBLK], F32)
    kp, r_kp = C.sb("kp", [128, 8], F32)
    pcol, r_pcol = C.sb("pcol", [128, 1], F32)
    sut32, r_sut32 = C.sb("sut32", [NE, NE], F32)
    S.dma("sp", thr16[:].rearrange("p e m -> p (e m)"), g_("thr16_d").partition_broadcast(128), reads=[r_in], writes=[r_thr16])
    S.dma("sp", bstart[:], g_("bstart_d").partition_broadcast(128), reads=[r_in], writes=[r_bstart])
    S.dma("sp", kp[:], g_("kp_d"), reads=[r_in], writes=[r_kp])
    S.dma("sp", pcol[:], g_("pcol_d"), reads=[r_in], writes=[r_pcol])
    S.dma("sp", sut32[:], g_("sut32_d"), reads=[r_in], writes=[r_sut32])
    big, r_big = C.sb("bigD", [128, NBLK * NE], F32)
    nbk, r_nbk = C.sb("nbk", [128, NE], F32)
    padT, r_padT = C.sb("padT", [NE, 128], F32)
    pstart, r_pstart = C.sb("pstart", [128, NE], F32)
    pend, r_pend = C.sb("pend", [128, NE], F32)
    bexp, r_bexp = C.sb("bexp", [128, NBLK], F32)
    wf, r_wf = C.sb("wf", [128, NBLK, 8], F32)
    S.op("dve", lambda e: e.tensor_tensor(out=big[:, 0:NE * 16].rearrange("p (e m) -> p e m", m=16), in0=rrun[:].unsqueeze(2).to_broadcast([128, NE, 16]), in1=thr16[:], op=ALU.is_gt),
         reads=[r_rrun, r_thr16], writes=[r_big])
    S.op("dve", lambda e: e.tensor_reduce(out=nbk[:], in_=big[:, 0:NE * 16].rearrange("p (e m) -> p e m", m=16), axis=AX.X, op=ALU.add), reads=[r_big], writes=[r_nbk])
    S.op("dve", lambda e: e.tensor_scalar_mul(out=nbk[:], in0=nbk[:], scalar1=512.0), reads=[r_nbk], writes=[r_nbk])
    pq, r_pq = psl[0]
    S.op("pe", lambda e: e.transpose(out=pq[0:NE, 0:128], in_=nbk[:], identity=ident_f[:]), reads=[r_nbk, r_identf], writes=[r_pq])
    S.op("act", lambda e: e.activation(out=padT[:], in_=pq[0:NE, 0:128], func=AF.Copy), reads=[r_pq], writes=[r_padT])
    S.op("pe", lambda e: e.matmul(pq[:, 256:256 + NE], lhsT=padT[:], rhs=sut32[:], start=True, stop=True), reads=[r_padT, r_sut32], awrites=[r_pq])
    S.op("act", lambda e: e.activation(out=pstart[:], in_=pq[:, 256:256 + NE], func=AF.Copy), reads=[r_pq], writes=[r_pstart])
    S.op("dve", lambda e: e.tensor_tensor(out=pend[:], in0=pstart[:], in1=nbk[:], op=ALU.add), reads=[r_pstart, r_nbk], writes=[r_pend])
    S.op("dve", lambda e: e.tensor_tensor(out=big[:].rearrange("p (i e) -> p i e", e=NE), in0=bstart[:].unsqueeze(2).to_broadcast([128, NBLK, NE]),
                                         in1=pend[:].unsqueeze(1).to_broadcast([128, NBLK, NE]), op=ALU.is_ge), reads=[r_bstart, r_pend], writes=[r_big])
    S.op("dve", lambda e: e.tensor_reduce(out=bexp[:], in_=big[:].rearrange("p (i e) -> p i e", e=NE), axis=AX.X, op=ALU.add), reads=[r_big], writes=[r_bexp])
    S.op("dve", lambda e: e.tensor_scalar_min(out=bexp[:], in0=bexp[:], scalar1=float(NE - 1)), reads=[r_bexp], writes=[r_bexp])
    S.op("dve", lambda e: e.tensor_copy(out=eidx[:], in_=bexp[:]), reads=[r_bexp], writes=[r_eidx])
    S.op("dve", lambda e: e.tensor_scalar(out=bidx[:], in0=bexp[:], scalar1=128.0, scalar2=pcol[:, 0:1], op0=ALU.mult, op1=ALU.add), reads=[r_bexp, r_pcol], writes=[r_bidx])
    S.op("dve", lambda e: e.tensor_scalar_mul(out=wf[:], in0=bexp[:].unsqueeze(2).to_broadcast([128, NBLK, 8]), scalar1=float(D)), reads=[r_bexp], writes=[r_wf])
    S.op("dve", lambda e: e.tensor_tensor(out=widx[:], in0=wf[:], in1=kp[:].unsqueeze(1).to_broadcast([128, NBLK, 8]), op=ALU.add), reads=[r_wf, r_kp], writes=[r_widx])
    sl_, r_sl = C.sb("slD", [128, NE], F32)
    eqt, r_eqt = C.sb("eqt", [128, NE], F32)
    m8b, r_m8b = C.sb("m8b", [128, 8], F32)
    S.op("dve", lambda e: e.tensor_scalar_add(out=pstart[:], in0=pstart[:], scalar1=1.0), reads=[r_pstart], writes=[r_pstart])
    for t0 in range(2):
        S.dma("sp", u2b[t0 % 3][0][:], u2_d[t0 * 128:t0 * 128 + 128, :], reads=[r_u2], writes=[u2b[t0 % 3][1]])
    for t in range(NT):
        tok0 = t * 128
        ub, r_ub = u2b[t % 3]
        if t + 2 < NT:
            S.dma("sp", u2b[(t + 2) % 3][0][:], u2_d[tok0 + 256:tok0 + 384, :], reads=[r_u2], writes=[u2b[(t + 2) % 3][1]])
        S.op("dve", lambda e: e.tensor_tensor(out=sl_[:], in0=posall[:, t, :], in1=pstart[:], op=ALU.add), reads=[r_posall, r_pstart], writes=[r_sl])
        S.op("dve", lambda e: e.tensor_tensor(out=sl_[:], in0=sl_[:], in1=maskall[:, t, :], op=ALU.mult), reads=[r_sl, r_maskall], writes=[r_sl])
        S.op("dve", lambda e: e.max(out=m8b[:], in_=sl_[:]), reads=[r_sl], writes=[r_m8b])
        for j in range(4):
            S.op("dve", lambda e: e.scalar_tensor_tensor(out=eqt[:], in0=sl_[:], scalar=m8b[:, j:j + 1], in1=gall[:, t, :], op0=ALU.is_equal, op1=ALU.mult, accum_out=g4[:, t, j:j + 1]),
                 reads=[r_sl, r_m8b, r_gall], writes=[r_eqt], awrites=[r_g4])
        S.op("dve", lambda e: e.tensor_scalar_add(out=idx4[:, t, :], in0=m8b[:, 0:4], scalar1=-1.0), reads=[r_m8b], awrites=[r_idx4])
        for j in range(4):
            S.op("pool", lambda e: e.indirect_dma_start(out=xs_d[:, :], out_offset=bass.IndirectOffsetOnAxis(ap=idx4[:, t, j:j + 1], axis=0), in_=ub[:], in_offset=None),
                 reads=[r_ub, r_idx4], awrites=[r_xsd], dma=True)
    C.pop()
    if "stopD" in dbg:
        C.pop()
        return

    C.push()
    w1b = C.ring("sb", "w1b", 2, [128, 8, 2 * D], BF16)
    w2b = C.ring("sb", "w2b", 2, [128, 8, D], BF16)
    b1t = C.ring("sb", "b1t", 2, [128, 16], F32)
    b2t = C.ring("sb", "b2t", 2, [2, D], F32)
    ones1f, r_ones1f = C.sb("ones1f", [1, 128], BF16)
    S.op("dve", lambda e: e.memset(ones1f[:], 1.0), writes=[r_ones1f])
    b2b = C.ring("sb", "b2b", 2, [1, D], BF16)
    xr = C.ring("sb", "xr", 8, [128, D], BF16)
    XT, r_XT = C.sb("XT", [128, 8, 512], BF16)
    hg = C.ring("sb", "hg", 2, [128, 512], F32)
    hu = C.ring("sb", "hu", 2, [128, 512], F32)
    sgm = C.ring("sb", "sgm", 2, [128, 512], F32)
    actT, r_actT = C.sb("actT", [128, 8, 512], BF16)
    ysb = C.ring("sb", "ysb", 2, [128, D], BF16)
    pxt = C.ring("ps", "pxt", 2, [128, 512], F32)
    pg = C.ring("ps", "pg", 2, [128, 512], F32)
    pu = C.ring("ps", "pu", 2, [128, 512], F32)
    py = C.ring("ps", "py", 2, [128, 512], F32)

    def load_weights(i, slot):
        w1t, r_w1 = w1b[slot]
        w2t, r_w2 = w2b[slot]
        for k in range(8):
            S.op("pool", lambda e: e.indirect_dma_start(out=w1t[:, k, :], out_offset=None, in_=w1b_d[:, :], in_offset=bass.IndirectOffsetOnAxis(ap=widx[:, i, k:k + 1], axis=0)),
                 reads=[r_w1bd, r_widx], writes=[r_w1] if k == 0 else (), awrites=() if k == 0 else [r_w1], dma=True)
        for k in range(8):
            S.op("pool", lambda e: e.indirect_dma_start(out=w2t[:, k, :], out_offset=None, in_=w2b_d[:, :], in_offset=bass.IndirectOffsetOnAxis(ap=widx[:, i, k:k + 1], axis=0)),
                 reads=[r_w2bd, r_widx], writes=[r_w2] if k == 0 else (), awrites=() if k == 0 else [r_w2], dma=True)
        S.op("pool", lambda e: e.indirect_dma_start(out=b1t[slot][0][:], out_offset=None, in_=b1r_d[:, :], in_offset=bass.IndirectOffsetOnAxis(ap=bidx[:, i:i + 1], axis=0)),
             reads=[r_in, r_bidx], writes=[b1t[slot][1]], dma=True)
        S.op("pool", lambda e: e.indirect_dma_start(out=b2t[slot][0][:], out_offset=None, in_=b2_d[:, :], in_offset=bass.IndirectOffsetOnAxis(ap=eidx[0:2, i:i + 1], axis=0)),
             reads=[r_in, r_eidx], writes=[b2t[slot][1]], dma=True)

    def load_x(i):
        for s4 in range(4):
            x_t, r_x = xr[(i % 2) * 4 + s4]
            row0 = i * 512 + s4 * 128
            S.dma("sp", x_t[:], xs_d[row0:row0 + 128, :], reads=[r_xsd], writes=[r_x])

    nblk_run = NBLK if "nblk" not in L else L["nblk"]
    load_weights(0, 0)
    load_x(0)
    xc = 0
    for i in range(nblk_run):
        slot = i % 2
        if i + 1 < nblk_run:
            load_weights(i + 1, (i + 1) % 2)
            load_x(i + 1)
        w1t, r_w1 = w1b[slot]
        w2t, r_w2 = w2b[slot]
        b1_t, r_b1 = b1t[slot]
        b2f_t, r_b2f = b2t[slot]
        b2_t, r_b2 = b2b[slot]
        S.op("pool", lambda e: e.tensor_copy(out=b2_t[:], in_=b2f_t[0:1, :]), reads=[r_b2f], writes=[r_b2])
        for s4 in range(4):
            x_t, r_x = xr[(i % 2) * 4 + s4]
            p_t, r_p = pxt[xc % 2]
            xc += 1
            p_b = p_t[:].bitcast(BF16)
            for k in range(8):
                S.op("pe", lambda e: e.transpose(out=p_b[:, k * 128:(k + 1) * 128], in_=x_t[:, k * 128:(k + 1) * 128], identity=ident_b[:]),
                     reads=[r_x, r_identb], writes=[r_p] if k == 0 else (), awrites=() if k == 0 else [r_p])
            S.op("act", lambda e: e.activation(out=XT[:, :, s4 * 128:(s4 + 1) * 128], in_=p_b.rearrange("p (k t) -> p k t", k=8), func=AF.Copy), reads=[r_p],
                 writes=[r_XT] if s4 == 0 else (), awrites=() if s4 == 0 else [r_XT])
        for fc in range(8):
            pg_t, r_pg = pg[fc % 2]
            pu_t, r_pu = pu[fc % 2]
            for k in range(8):
                S.op("pe", lambda e: e.matmul(pg_t[:], lhsT=w1t[:, k, fc * 128:(fc + 1) * 128], rhs=XT[:, k, :], start=(k == 0), stop=(k == 7)),
                     reads=[r_w1, r_XT], writes=[r_pg] if k == 0 else (), awrites=() if k == 0 else [r_pg])
            for k in range(8):
                S.op("pe", lambda e: e.matmul(pu_t[:], lhsT=w1t[:, k, D + fc * 128:D + (fc + 1) * 128], rhs=XT[:, k, :], start=(k == 0), stop=(k == 7)),
                     reads=[r_w1, r_XT], writes=[r_pu] if k == 0 else (), awrites=() if k == 0 else [r_pu])
            hg_t, r_hg = hg[fc % 2]
            hu_t, r_hu = hu[fc % 2]
            sg_t, r_sgm = sgm[fc % 2]
            S.op("act", lambda e: e.activation(out=hg_t[:], in_=pg_t[:], func=AF.Identity, bias=b1_t[:, fc:fc + 1]), reads=[r_pg, r_b1], writes=[r_hg])
            S.op("act", lambda e: e.activation(out=hu_t[:], in_=pu_t[:], func=AF.Identity, bias=b1_t[:, 8 + fc:9 + fc]), reads=[r_pu, r_b1], writes=[r_hu])
            S.op("dve", lambda e: e.tensor_scalar_min(out=hg_t[:], in0=hg_t[:], scalar1=7.0), reads=[r_hg], writes=[r_hg])
            S.op("act", lambda e: e.activation(out=sg_t[:], in_=hg_t[:], func=AF.Sigmoid, scale=1.702), reads=[r_hg], writes=[r_sgm])
            S.op("pool", lambda e: e.tensor_scalar(out=hu_t[:], in0=hu_t[:], scalar1=7.0, scalar2=-7.0, op0=ALU.min, op1=ALU.max), reads=[r_hu], writes=[r_hu])
            S.op("dve", lambda e: e.scalar_tensor_tensor(out=hu_t[:], in0=hu_t[:], scalar=1.0, in1=hg_t[:], op0=ALU.add, op1=ALU.mult), reads=[r_hu, r_hg], writes=[r_hu])
            S.op("dve", lambda e: e.tensor_tensor(out=actT[:, fc, :], in0=hu_t[:], in1=sg_t[:], op=ALU.mult), reads=[r_hu, r_sgm],
                 writes=[r_actT] if fc == 0 else (), awrites=() if fc == 0 else [r_actT])
        for s4 in range(4):
            y_t, r_y = ysb[s4 % 2]
            for hf in range(2):
                py_t, r_py = py[hf]
                for fc in range(8):
                    S.op("pe", lambda e: e.matmul(py_t[:], lhsT=actT[:, fc, s4 * 128:(s4 + 1) * 128], rhs=w2t[:, fc, hf * 512:(hf + 1) * 512], start=(fc == 0), stop=False),
                         reads=[r_actT, r_w2], writes=[r_py] if fc == 0 else (), awrites=() if fc == 0 else [r_py])
                S.op("pe", lambda e: e.matmul(py_t[:], lhsT=ones1f[:], rhs=b2_t[0:1, hf * 512:(hf + 1) * 512], start=False, stop=True), reads=[r_ones1f, r_b2], awrites=[r_py])
                S.op("act", lambda e: e.activation(out=y_t[:, hf * 512:(hf + 1) * 512], in_=py_t[:], func=AF.Copy), reads=[r_py],
                     writes=[r_y] if hf == 0 else (), awrites=() if hf == 0 else [r_y])
            row0 = i * 512 + s4 * 128
            S.dma("act", ys_d[row0:row0 + 128, :], y_t[:], reads=[r_y], awrites=[r_ysd])
    C.pop()

    C.push()
    ln2g, r_ln2g = C.sb("ln2g", [128, D], F32)
    ln2b, r_ln2b = C.sb("ln2b", [128, D], F32)
    S.dma("sp", ln2g[:], g_("ln2g_d").partition_broadcast(128), reads=[r_in], writes=[r_ln2g])
    S.dma("sp", ln2b[:], g_("ln2b_d").partition_broadcast(128), reads=[r_in], writes=[r_ln2b])
    gate2r = C.ring("sb", "gate2r", 2, [128, D], F32)
    x1r = C.ring("sb", "x1r", 4, [128, D], F32)
    yg = C.ring("sb", "yg", 16, [128, D], BF16)
    accr = C.ring("sb", "accF", 3, [128, D], F32)
    ot = C.ring("sb", "otF", 3, [128, D], F32)
    stF = C.ring("sb", "stF", 3, [128, 2, 6], F32)
    mvF = C.ring("sb", "mvF", 3, [128, 4], F32)
    rsF = C.ring("sb", "rsF", 3, [128, 1], F32)

    def loadF(t):
        tok0 = t * 128
        slot = t % 4
        S.dma("sp", x1r[slot][0][:], x1_d[tok0:tok0 + 128, :], reads=[r_x1], writes=[x1r[slot][1]])
        for j in range(4):
            y_t, r_y = yg[slot * 4 + j]
            S.op("pool", lambda e: e.indirect_dma_start(out=y_t[:], out_offset=None, in_=ys_d[:, :], in_offset=bass.IndirectOffsetOnAxis(ap=idx4[:, t, j:j + 1], axis=0)),
                 reads=[r_ysd, r_idx4], writes=[r_y], dma=True)
        if t % TPS == 0:
            b_ = t // TPS
            S.dma("sp", gate2r[b_ % 2][0][:], mod_d[b_:b_ + 1, 5 * D:6 * D].partition_broadcast(128), reads=[r_mod], writes=[gate2r[b_ % 2][1]])

    def genF(t):
        if t + 3 < NT:
            loadF(t + 3)
        slot = t % 4
        tok0 = t * 128
        gate2, r_gate2 = gate2r[(t // TPS) % 2]
        x1_t, r_x1t = x1r[slot]
        acc, r_acc = accr[t % 3]
        st, r_st = stF[t % 3]; mv, r_mv = mvF[t % 3]; rs, r_rs = rsF[t % 3]
        o_t, r_o = ot[t % 3]
        for j in range(4):
            y_t, r_y = yg[slot * 4 + j]
            if j == 0:
                S.op("act", lambda e: e.activation(out=acc[:], in_=y_t[:], func=AF.Copy, scale=g4[:, t, 0:1]), reads=[r_y, r_g4], writes=[r_acc])
            else:
                S.op("dve", lambda e: e.scalar_tensor_tensor(out=acc[:], in0=y_t[:], scalar=g4[:, t, j:j + 1], in1=acc[:], op0=ALU.mult, op1=ALU.add), reads=[r_y, r_g4, r_acc], writes=[r_acc])
            yield
        S.op("pool", lambda e: e.tensor_tensor(out=acc[:], in0=acc[:], in1=gate2[:], op=ALU.mult), reads=[r_acc, r_gate2], writes=[r_acc])
        yield
        S.op("dve", lambda e: e.scalar_tensor_tensor(out=acc[:], in0=x1_t[:], scalar=float(ALPHA), in1=acc[:], op0=ALU.mult, op1=ALU.add), reads=[r_x1t, r_acc], writes=[r_acc])
        yield
        for j in range(2):
            S.op("dve", lambda e: e.bn_stats(out=st[:, j, :], in_=acc[:, j * 512:(j + 1) * 512]), reads=[r_acc], writes=[r_st] if j == 0 else (), awrites=() if j == 0 else [r_st])
            yield
        S.op("dve", lambda e: e.bn_aggr(out=mv[:, 0:2], in_=st[:].rearrange("p a b -> p (a b)")), reads=[r_st], writes=[r_mv])
        yield
        S.op("act", lambda e: e.activation(out=rs[:], in_=mv[:, 1:2], func=AF.Sqrt, bias=EPS), reads=[r_mv], writes=[r_rs])
        yield
        S.op("dve", lambda e: e.reciprocal(out=rs[:], in_=rs[:]), reads=[r_rs], writes=[r_rs])
        yield
        S.op("dve", lambda e: e.tensor_scalar(out=mv[:, 2:3], in0=mv[:, 0:1], scalar1=-1.0, scalar2=rs[:], op0=ALU.mult, op1=ALU.mult), reads=[r_mv, r_rs], awrites=[r_mv])
        yield
        S.op("act", lambda e: e.activation(out=o_t[:], in_=acc[:], func=AF.Identity, scale=rs[:], bias=mv[:, 2:3]), reads=[r_acc, r_mv, r_rs], writes=[r_o])
        yield
        S.op("dve", lambda e: e.tensor_tensor(out=o_t[:], in0=o_t[:], in1=ln2g[:], op=ALU.mult), reads=[r_o, r_ln2g], writes=[r_o])
        yield
        S.op("pool", lambda e: e.tensor_tensor(out=o_t[:], in0=o_t[:], in1=ln2b[:], op=ALU.add), reads=[r_o, r_ln2b], writes=[r_o])
        yield
        S.dma("sp", out_d[tok0:tok0 + 128, :], o_t[:], reads=[r_o], awrites=[r_out])
        yield

    for t0 in range(3):
        loadF(t0)
    interleave((genF(t) for t in range(NT)), 6)
    C.pop()
    C.pop()


def phase_C(nc, S, C, dbg, L):
    g_ = lambda n: L[n]
    r_in = g_("r_in"); ident_b = g_("ident_b"); r_identb = g_("r_identb"); ident_f = g_("ident_f"); r_identf = g_("r_identf")
    qT_d, r_qT = g_("qT_d"), g_("r_qT"); iqT_d, r_iqT = g_("iqT_d"), g_("r_iqT")
    kva_d, r_kva = g_("kva_d"), g_("r_kva"); kvT_d, r_kvT = g_("kvT_d"), g_("r_kvT"); ikT_d, r_ikT = g_("ikT_d"), g_("r_ikT")
    iw_d, r_iw = g_("iw_d"), g_("r_iw"); oaT_d, r_oaT = g_("oaT_d"), g_("r_oaT")
    C.push()
    w1_d, w2_d = g_("w1_d"), g_("w2_d")

    def cast_weights(i):
        e_ = i // 2
        if i % 2 == 0:
            S.dma("pool", g_("w1b_d")[e_ * D:(e_ + 1) * D, :], w1_d[e_ * D:(e_ + 1) * D, :], reads=[r_in], awrites=[g_("r_w1bd")])
        else:
            S.dma("pool", g_("w2b_d")[e_ * D:(e_ + 1) * D, :], w2_d[e_ * D:(e_ + 1) * D, :], reads=[r_in], awrites=[g_("r_w2bd")])
    tzf, r_tzf = C.sb("tzf", [128, 2, 8, 128], F32)
    tz, r_tz = C.sb("tzb", [128, 2, 8, 128], BF16)
    cfar, r_cfar = C.sb("cfar", [128, 8], F32)
    identN, r_identN = C.sb("identN", [128, 128], BF16)
    S.dma("sp", tzf[:], g_("tz_d"), reads=[r_in], writes=[r_tzf])
    S.dma("sp", cfar[:], g_("cfar_d").partition_broadcast(128), reads=[r_in], writes=[r_cfar])
    S.op("dve", lambda e: e.tensor_scalar_mul(out=identN[:], in0=ident_f[:], scalar1=-NEG), reads=[r_identf], writes=[r_identN])
    first = True
    for dl in range(2):
        for h in range(8):
            S.op("dve", lambda e: e.tensor_scalar(out=tz[:, dl, h, :], in0=tzf[:, dl, h, :], scalar1=cfar[:, h:h + 1], scalar2=None, op0=ALU.subtract),
                 reads=[r_tzf, r_cfar], writes=[r_tz] if first else (), awrites=() if first else [r_tz])
            first = False
    seqb = [(C.sb("kvT%d" % i, [128, SEQ], BF16), C.sb("ikT%d" % i, [128, SEQ], BF16), C.sb("kvaC%d" % i, [128, TPS, 136], BF16)) for i in range(2)]
    qt = C.ring("sb", "qt", 5, [128, 8, 128], BF16)
    iqt = C.ring("sb", "iqt", 4, [128, 4, 128], BF16)
    iwt = C.ring("sb", "iwC", 4, [128, 8], F32)
    scorer = C.ring("sb", "score", 3, [128, SEQ], F32)
    penr = C.ring("sb", "pen", 2, [128, SEQ], BF16)
    rl = C.ring("sb", "rl", 2, [128, 512], F32)
    m8, r_m8 = C.sb("m8", [128, 8], F32)
    expT = C.ring("sb", "expT", 2, [128, TPS * 128], BF16)
    posb = C.ring("sb", "posb", 2, [128, 8, 132], F32)
    rc, r_rc = C.sb("rcC", [128, 8], F32)
    oa, r_oa = C.sb("oa", [128, D], BF16)
    oaT = C.ring("sb", "oaT", 2, [128, 8, 128], BF16)
    pi = C.ring("ps", "pi", 2, [128, 512], F32)
    pl = C.ring("ps", "pl", 3, [128, 512], F32)
    po = C.ring("ps", "po", 2, [128, 512], F32)
    ptr = C.ring("ps", "ptrC", 1, [128, 512], F32)
    NTILE = NB * TPS

    def load_seq(b):
        (kvT, r_kvTs), (ikT, r_ikTs), (kva, r_kvas) = seqb[b % 2]
        s0 = b * SEQ
        S.dma("sp", kvT[:], kvT_d[:, s0:s0 + SEQ], reads=[r_kvT], writes=[r_kvTs])
        S.dma("sp", ikT[:], ikT_d[:, s0:s0 + SEQ], reads=[r_ikT], writes=[r_ikTs])
        S.dma("sp", kva[:], kva_d[s0:s0 + SEQ, :].rearrange("(k p) c -> p k c", p=128), reads=[r_kva], writes=[r_kvas])

    def load_tile(i):
        tok0 = i * 128
        S.dma("sp", qt[i % 5][0][:], qT_d[:, :, tok0:tok0 + 128].rearrange("h p t -> p h t"), reads=[r_qT], writes=[qt[i % 5][1]])
        S.dma("sp", iqt[i % 4][0][:], iqT_d[:, :, tok0:tok0 + 128].rearrange("h p t -> p h t"), reads=[r_iqT], writes=[iqt[i % 4][1]])
        S.dma("sp", iwt[i % 4][0][:], iw_d[tok0:tok0 + 128, :], reads=[r_iw], writes=[iwt[i % 4][1]])

    NIT = 24
    pow2, r_pow2 = C.sb("pow2", [128, NIT + 1], F32)
    stepsr = C.ring("sb", "steps", 3, [128, NIT + 1], F32)
    bisr = C.ring("sb", "bis", 3, [128, 8], F32)
    junkb, r_junkb = C.sb("junkb", [128, SEQ], BF16)
    junkd, r_junkd = C.sb("junkd", [128, SEQ], BF16)
    for j in range(NIT + 1):
        S.op("pool", lambda e: e.memset(pow2[:, j:j + 1], float(2.0 ** -(j + 1))), writes=[r_pow2] if j == 0 else (), awrites=() if j == 0 else [r_pow2])

    def prep_score_gen(i):
        b, t = divmod(i, TPS)
        (ikT, r_ikTs) = seqb[b % 2][1]
        iq_t, r_iq = iqt[i % 4]
        iw_t, r_iwt = iwt[i % 4]
        pen, r_pen = penr[i % 2]
        score, r_score = scorer[i % 3]
        steps, r_steps = stepsr[i % 3]
        bis, r_bis = bisr[i % 3]
        N = 128 * (t + 1)
        if t < 2:
            return
            yield
        for kg in range(0, N, 512):
            w = min(512, N - kg)
            for h in range(8):
                pr, hf = divmod(h, 2)
                p_t, r_p = pi[h % 2]
                S.op("pe", lambda e: e.matmul(p_t[:, 0:w], lhsT=iq_t[64 * hf:64 * hf + 64, pr, :], rhs=ikT[64 * hf:64 * hf + 64, kg:kg + w], start=True, stop=True),
                     reads=[r_iq, r_ikTs], writes=[r_p])
                r_t, r_r = rl[h % 2]
                if h == 0:
                    S.op("dve", lambda e: e.tensor_scalar(out=score[:, kg:kg + w], in0=p_t[:, 0:w], scalar1=0.0, scalar2=iw_t[:, 0:1], op0=ALU.max, op1=ALU.mult),
                         reads=[r_p, r_iwt], writes=[r_score] if kg == 0 else (), awrites=() if kg == 0 else [r_score])
                elif h % 2 == 1:
                    S.op("dve", lambda e: e.tensor_scalar(out=r_t[:, 0:w], in0=p_t[:, 0:w], scalar1=0.0, scalar2=iw_t[:, h:h + 1], op0=ALU.max, op1=ALU.mult),
                         reads=[r_p, r_iwt], writes=[r_r])
                    S.op("pool", lambda e: e.tensor_tensor(out=score[:, kg:kg + w], in0=score[:, kg:kg + w], in1=r_t[:, 0:w], op=ALU.add),
                         reads=[r_r, r_score], awrites=[r_score])
                else:
                    S.op("act", lambda e: e.activation(out=r_t[:, 0:w], in_=p_t[:, 0:w], func=AF.Relu), reads=[r_p], writes=[r_r])
                    S.op("dve", lambda e: e.scalar_tensor_tensor(out=score[:, kg:kg + w], in0=r_t[:, 0:w], scalar=iw_t[:, h:h + 1], in1=score[:, kg:kg + w], op0=ALU.mult, op1=ALU.add),
                         reads=[r_r, r_iwt, r_score], awrites=[r_score])
                yield
        S.op("dve", lambda e: e.tensor_reduce(out=bis[:, 0:1], in_=score[:, 0:N - 64], axis=AX.X, op=ALU.min), reads=[r_score], writes=[r_bis])
        S.op("dve", lambda e: e.memset(score[0:64, N - 64:N], -1e30), reads=[r_score], awrites=[r_score])
        S.op("dve", lambda e: e.max(out=m8[:], in_=score[:, 0:N]), reads=[r_score], writes=[r_m8])
        S.op("dve", lambda e: e.tensor_tensor(out=bis[:, 1:2], in0=m8[:, 0:1], in1=bis[:, 0:1], op=ALU.subtract), reads=[r_m8, r_bis], awrites=[r_bis])
        S.op("dve", lambda e: e.tensor_scalar(out=steps[:], in0=pow2[:], scalar1=bis[:, 1:2], scalar2=None, op0=ALU.mult), reads=[r_pow2, r_bis], writes=[r_steps])
        S.op("dve", lambda e: e.tensor_tensor(out=bis[:, 2:3], in0=bis[:, 0:1], in1=steps[:, 0:1], op=ALU.add), reads=[r_bis, r_steps], awrites=[r_bis])

    def prep_score(i):
        for _ in prep_score_gen(i):
            pass

    def prep_iter(i, j):
        b, t = divmod(i, TPS)
        if t < 2:
            return
        N = 128 * (t + 1)
        score, r_score = scorer[i % 3]
        steps, r_steps = stepsr[i % 3]
        bis, r_bis = bisr[i % 3]
        if j % 3 != 2:
            S.op("act", lambda e: e.activation(out=junkb[:, 0:N], in_=score[:, 0:N], func=AF.Sign, scale=-1.0, bias=bis[:, 2:3], accum_out=bis[:, 4:5]),
                 reads=[r_score, r_bis], writes=[r_junkb], awrites=[r_bis])
            S.op("dve", lambda e: e.tensor_scalar(out=bis[:, 3:4], in0=bis[:, 4:5], scalar1=float(N - 511), scalar2=steps[:, j:j + 1], op0=ALU.is_le, op1=ALU.mult),
                 reads=[r_bis, r_steps], awrites=[r_bis])
        else:
            S.op("dve", lambda e: e.tensor_scalar(out=junkd[:, 0:N], in0=score[:, 0:N], scalar1=bis[:, 2:3], scalar2=None, op0=ALU.is_ge, op1=ALU.add, accum_out=bis[:, 6:7]),
                 reads=[r_score, r_bis], writes=[r_junkd], awrites=[r_bis])
            S.op("dve", lambda e: e.tensor_scalar(out=bis[:, 3:4], in0=bis[:, 6:7], scalar1=255.5, scalar2=steps[:, j:j + 1], op0=ALU.is_ge, op1=ALU.mult),
                 reads=[r_bis, r_steps], awrites=[r_bis])
        S.op("dve", lambda e: e.scalar_tensor_tensor(out=bis[:, 2:3], in0=bis[:, 3:4], scalar=steps[:, j + 1:j + 2], in1=bis[:, 2:3], op0=ALU.subtract, op1=ALU.add),
             reads=[r_bis, r_steps], awrites=[r_bis])

    def prep_fin(i):
        b, t = divmod(i, TPS)
        N = 128 * (t + 1)
        pen, r_pen = penr[i % 2]
        if t < 2:
            S.op("dve", lambda e: e.memset(pen[:, 0:N], 0.0), writes=[r_pen])
            S.op("dve", lambda e: e.memset(pen[0:64, N - 64:N], -1.0), awrites=[r_pen])
            return
        score, r_score = scorer[i % 3]
        steps, r_steps = stepsr[i % 3]
        bis, r_bis = bisr[i % 3]
        S.op("dve", lambda e: e.tensor_tensor(out=bis[:, 5:6], in0=bis[:, 2:3], in1=steps[:, NIT:NIT + 1], op=ALU.subtract), reads=[r_bis, r_steps], awrites=[r_bis])
        S.op("dve", lambda e: e.tensor_scalar(out=pen[:, 0:N], in0=score[:, 0:N], scalar1=bis[:, 5:6], scalar2=1.0, op0=ALU.is_ge, op1=ALU.subtract),
             reads=[r_score, r_bis], writes=[r_pen])

    state = {"plc": 0, "hc": 0}

    def attend_head(i, h):
        b, t = divmod(i, TPS)
        (kvT, r_kvTs), _, (kva, r_kvas) = seqb[b % 2]
        q_t, r_q = qt[i % 5]
        pen, r_pen = penr[i % 2]
        ps_t, r_ps = posb[i % 2]
        nkb = t + 1
        if True:
            e_t, r_e = expT[state["hc"] % 2]
            state["hc"] += 1
            for kg in range(0, nkb, 4):
                nb_ = min(4, nkb - kg)
                p_t, r_p = pl[state["plc"] % 3]
                state["plc"] += 1
                for ii in range(nb_):
                    kb = kg + ii
                    near = kb >= t - 1
                    cs_ = slice(ii * 128, (ii + 1) * 128)
                    S.op("pe", lambda e: e.matmul(p_t[:, cs_], lhsT=kvT[:, kb * 128:(kb + 1) * 128], rhs=q_t[:, h, :], start=True, stop=False),
                         reads=[r_kvTs, r_q], writes=[r_p] if ii == 0 else (), awrites=() if ii == 0 else [r_p])
                    S.op("pe", lambda e: e.matmul(p_t[:, cs_], lhsT=pen[:, kb * 128:(kb + 1) * 128], rhs=identN[:], start=False, stop=(not near)),
                         reads=[r_pen, r_identN], awrites=[r_p])
                    if near:
                        S.op("pe", lambda e: e.matmul(p_t[:, cs_], lhsT=ident_b[:], rhs=tz[:, t - kb, h, :], start=False, stop=True),
                             reads=[r_identb, r_tz], awrites=[r_p])
                S.op("act", lambda e: e.activation(out=e_t[:, kg * 128:(kg + nb_) * 128], in_=p_t[:, 0:nb_ * 128], func=AF.Exp), reads=[r_p],
                     writes=[r_e] if kg == 0 else (), awrites=() if kg == 0 else [r_e])
            o_t, r_o = po[h % 2]
            for kb in range(nkb):
                S.op("pe", lambda e: e.matmul(o_t[:, 0:129], lhsT=e_t[:, kb * 128:(kb + 1) * 128], rhs=kva[:, kb, 0:129], start=(kb == 0), stop=(kb == nkb - 1)),
                     reads=[r_e, r_kvas], writes=[r_o] if kb == 0 else (), awrites=() if kb == 0 else [r_o])
            S.op("act", lambda e: e.activation(out=ps_t[:, h, 0:129], in_=o_t[:, 0:129], func=AF.Copy), reads=[r_o],
                 writes=[r_ps] if h == 0 else (), awrites=() if h == 0 else [r_ps])

    def finalize(i):
        tok0 = i * 128
        ps_t, r_ps = posb[i % 2]
        S.op("dve", lambda e: e.reciprocal(out=rc[:], in_=ps_t[:, :, 128]), reads=[r_ps], writes=[r_rc])
        S.op("dve", lambda e: e.tensor_tensor(out=oa[:].rearrange("p (h c) -> p h c", h=8), in0=ps_t[:, :, 0:128], in1=rc[:].unsqueeze(2).to_broadcast([128, 8, 128]), op=ALU.mult),
             reads=[r_ps, r_rc], writes=[r_oa])
        pT, r_pT = ptr[0]
        pT_b = pT[:].bitcast(BF16)
        for k in range(8):
            S.op("pe", lambda e: e.transpose(out=pT_b[:, k * 128:(k + 1) * 128], in_=oa[:, k * 128:(k + 1) * 128], identity=ident_b[:]),
                 reads=[r_oa, r_identb], writes=[r_pT] if k == 0 else (), awrites=() if k == 0 else [r_pT])
        oT, r_oT = oaT[i % 2]
        S.op("act", lambda e: e.activation(out=oT[:].rearrange("p k t -> p (k t)"), in_=pT_b, func=AF.Copy), reads=[r_pT], writes=[r_oT])
        S.dma("sp", oaT_d[:, :, tok0:tok0 + 128].rearrange("c p t -> p c t"), oT[:], reads=[r_oT], awrites=[r_oaT])

    HALF = NIT // 2
    load_seq(0)
    for i0 in range(4):
        load_tile(i0)
    prep_score(0)
    for j in range(NIT):
        prep_iter(0, j)
    prep_fin(0)
    prep_score(1)
    for j in range(HALF):
        prep_iter(1, j)
    prep_score(2)
    for i in range(NTILE):
        b, t = divmod(i, TPS)
        if t == 0 and b + 1 < NB:
            load_seq(b + 1)
        if i + 4 < NTILE:
            load_tile(i + 4)
        cast_weights(i)
        n1 = i + 1 < NTILE
        n2 = i + 2 < NTILE
        gen = prep_score_gen(i + 3) if i + 3 < NTILE else iter(())
        npieces = 8 * ((128 * (((i + 3) % TPS) + 1) + 511) // 512) if (i + 3 < NTILE and (i + 3) % TPS >= 2) else 0
        ppb = (npieces + 7) // 8
        sched = []
        for k in range(HALF):
            if n1:
                sched.append((i + 1, HALF + k))
            if n2:
                sched.append((i + 2, k))
        per = (len(sched) + 7) // 8
        for h in range(8):
            attend_head(i, h)
            its = sched[h * per:(h + 1) * per]
            for n_, (ti_, j) in enumerate(its):
                prep_iter(ti_, j)
                if n_ < ppb:
                    next(gen, None)
            for _ in range(max(0, ppb - len(its))):
                next(gen, None)
        for _ in gen:
            pass
        if n1:
            prep_fin(i + 1)
        if i >= 1:
            finalize(i - 1)
    finalize(NTILE - 1)
    C.pop()


def phase_B(nc, S, C, dbg, L):
    g_ = lambda n: L[n]
    r_in = g_("r_in"); ident_b = g_("ident_b"); r_identb = g_("r_identb")
    xbcT_d, r_xbcT = g_("xbcT_d"), g_("r_xbcT"); dt_d, r_dt = g_("dt_d"), g_("r_dt"); zs_d, r_zs = g_("zs_d"), g_("r_zs")
    obT_d, r_obT = g_("obT_d"), g_("r_obT")
    C.push()
    convw, r_convw = C.sb("convw", [128, 16, 4], F32)
    convb, r_convb = C.sb("convb", [128, 16], F32)
    dg, r_dg = C.sb("dg", [128, 16, 4, 128], BF16)
    identf2, r_identf2 = C.sb("identf2", [128, 128], F32)
    a_bc, r_abc = C.sb("a_bc", [128, 16], F32)
    dskip_bc, r_dskip = C.sb("dskip_bc", [128, 16], F32)
    normw_bc, r_normw = C.sb("normw_bc", [128, D], F32)
    triU, r_triU = C.sb("triU", [128, 128], F32)
    SLm, r_SL = C.sb("SLm", [128, 128], F32)
    onesf, r_onesf = C.sb("onesf", [128, 128], F32)
    negm4, r_negm4 = C.sb("negm4", [128, 512], BF16)
    S.dma("sp", convw[:], g_("convw_d"), reads=[r_in], writes=[r_convw])
    S.dma("sp", convb[:], g_("convb_d"), reads=[r_in], writes=[r_convb])
    S.dma("sp", identf2[:], g_("ident_d"), reads=[r_in], writes=[r_identf2])
    S.dma("sp", a_bc[:], g_("alog_d").partition_broadcast(128), reads=[r_in], writes=[r_abc])
    S.dma("sp", dskip_bc[:], g_("dskip_d").partition_broadcast(128), reads=[r_in], writes=[r_dskip])
    S.dma("sp", normw_bc[:], g_("normw_d").partition_broadcast(128), reads=[r_in], writes=[r_normw])
    S.dma("sp", triU[:], g_("triU_d"), reads=[r_in], writes=[r_triU])
    S.dma("sp", SLm[:], g_("SL_d"), reads=[r_in], writes=[r_SL])
    S.dma("pool", negm4[:], g_("negm4_d"), reads=[r_in], writes=[r_negm4])
    S.op("dve", lambda e: e.memset(onesf[:], 1.0), writes=[r_onesf])
    S.op("act", lambda e: e.activation(out=a_bc[:], in_=a_bc[:], func=AF.Exp), reads=[r_abc], writes=[r_abc])
    S.op("dve", lambda e: e.tensor_scalar_mul(out=a_bc[:], in0=a_bc[:], scalar1=-1.0), reads=[r_abc], writes=[r_abc])
    first = True
    for j in range(16):
        for k in range(4):
            S.op("dve", lambda e: e.tensor_scalar_mul(out=dg[:, j, k, :], in0=identf2[:], scalar1=convw[:, j, k:k + 1]),
                 reads=[r_identf2, r_convw], writes=[r_dg] if first else (), awrites=() if first else [r_dg])
            first = False

    bank = C.ring("ps", "bk", 8, [128, 512], F32)
    xh = C.ring("sb", "xh", 4, [128, 16, 131], BF16)
    dtl = C.ring("sb", "dtl", 4, [128, 16], F32)
    zl = C.ring("sb", "zl", 4, [128, D], BF16)
    xact_r = C.ring("sb", "xact", 2, [128, 16, 128], BF16)
    xs_r = C.ring("sb", "xs_tok", 2, [128, D], BF16)
    Bt_r = C.ring("sb", "B_tok", 2, [128, 512], BF16)
    adt_r = C.ring("sb", "adt", 2, [128, 16], F32)
    sm_r = C.ring("sb", "smB", 2, [128, 8, 16], F32)
    A_r = C.ring("sb", "Amat", 2, [128, 16, 128], F32)
    Lt_r = C.ring("sb", "Lt", 2, [128, 2, 512], F32)
    Mt_r = C.ring("sb", "Mt", 2, [128, 16, 128], BF16)
    xdt_r = C.ring("sb", "xdt", 2, [128, D], BF16)
    xdd_r = C.ring("sb", "xdd", 2, [128, D], BF16)
    prev_f, r_pf = C.sb("prev_f", [128, D], F32)
    prev_b, r_pb = C.sb("prev_b", [128, D], BF16)
    t1_r = C.ring("sb", "t1", 2, [128, D], F32)
    t2_r = C.ring("sb", "t2", 2, [128, D], F32)
    junk, r_junk = C.sb("junkB", [128, 256], F32)
    ob_r = C.ring("sb", "ob", 2, [128, D], BF16)
    obT = C.ring("sb", "obT", 2, [128, 8, 128], BF16)

    def bc3(ap2, n):
        return ap2.unsqueeze(2).to_broadcast([128, 16, n])

    def v3(t, n=64):
        return t.rearrange("p (h q) -> p h q", q=n)

    def load_chunk(b, t, slot):
        tok0 = b * SEQ + t * 128
        x_t, r_x = xh[slot]
        if t == 0:
            S.op("pool", lambda e: e.memset(x_t[:, :, 0:3], 0.0), writes=[r_x])
            S.dma("sp", x_t[:, :, 3:131], xbcT_d[:, :, tok0:tok0 + 128].rearrange("c p t -> p c t"), reads=[r_xbcT], awrites=[r_x])
        else:
            S.dma("sp", x_t[:, :, :], xbcT_d[:, :, tok0 - 3:tok0 + 128].rearrange("c p t -> p c t"), reads=[r_xbcT], writes=[r_x])
        S.dma("sp", dtl[slot][0][:], dt_d[tok0:tok0 + 128, :], reads=[r_dt], writes=[dtl[slot][1]])
        S.dma("sp", zl[slot][0][:], zs_d[tok0:tok0 + 128, :], reads=[r_zs], writes=[zl[slot][1]])

    nch = NB * TPS

    def chunk_gen(ci):
        b, t = divmod(ci, TPS)
        slot = ci % 4
        sl2 = ci % 2
        tok0 = b * SEQ + t * 128
        if ci + 2 < nch:
            load_chunk((ci + 2) // TPS, (ci + 2) % TPS, (ci + 2) % 4)
        x_t, r_x = xh[slot]
        d_t, r_d = dtl[slot]
        z_t, r_z = zl[slot]
        xact, r_xact = xact_r[sl2]; xs_tok, r_xs = xs_r[sl2]; B_tok, r_Bt = Bt_r[sl2]; adt, r_adt = adt_r[sl2]; sm, r_sm = sm_r[sl2]
        Amat, r_A = A_r[sl2]; Lt, r_Lt = Lt_r[sl2]; Mt, r_Mt = Mt_r[sl2]; xdt, r_xdt = xdt_r[sl2]; xdd, r_xdd = xdd_r[sl2]
        t1, r_t1 = t1_r[sl2]; t2, r_t2 = t2_r[sl2]; ob, r_ob = ob_r[sl2]
        for jg in range(4):
            pc, r_pc = bank[jg % 2]
            for jj in range(4):
                j = jg * 4 + jj
                for k in range(4):
                    S.op("pe", lambda e: e.matmul(pc[:, jj * 128:(jj + 1) * 128], lhsT=dg[:, j, k, :], rhs=x_t[:, j, k:k + 128], start=(k == 0), stop=(k == 3)),
                         reads=[r_dg, r_x], writes=[r_pc] if (jj == 0 and k == 0) else (), awrites=() if (jj == 0 and k == 0) else [r_pc])
                    yield
            for jj in range(4):
                j = jg * 4 + jj
                S.op("act", lambda e: e.activation(out=xact[:, j, :], in_=pc[:, jj * 128:(jj + 1) * 128], func=AF.Silu, bias=convb[:, j:j + 1]),
                     reads=[r_pc, r_convb], writes=[r_xact] if j == 0 else (), awrites=() if j == 0 else [r_xact])
                yield
        pxs, r_pxs = bank[2]
        pB, r_pB = bank[3]
        pxs_b = pxs[:].bitcast(BF16)
        pB_b = pB[:].bitcast(BF16)
        for k in range(8):
            S.op("pe", lambda e: e.transpose(out=pxs_b[:, k * 128:(k + 1) * 128], in_=xact[:, k, :], identity=ident_b[:]),
                 reads=[r_xact, r_identb], writes=[r_pxs] if k == 0 else (), awrites=() if k == 0 else [r_pxs])
            yield
        for k in range(4):
            S.op("pe", lambda e: e.transpose(out=pB_b[:, k * 128:(k + 1) * 128], in_=xact[:, 8 + k, :], identity=ident_b[:]),
                 reads=[r_xact, r_identb], writes=[r_pB] if k == 0 else (), awrites=() if k == 0 else [r_pB])
            yield
        S.op("act", lambda e: e.activation(out=xs_tok[:], in_=pxs_b, func=AF.Copy), reads=[r_pxs], writes=[r_xs])
        yield
        S.op("act", lambda e: e.activation(out=B_tok[:], in_=pB_b[:, 0:512], func=AF.Copy), reads=[r_pB], writes=[r_Bt])
        yield
        S.op("dve", lambda e: e.tensor_tensor(out=adt[:], in0=d_t[:], in1=a_bc[:], op=ALU.mult), reads=[r_d, r_abc], writes=[r_adt])
        yield
        S.op("pe", lambda e: e.matmul(pB[:, 256:272], lhsT=triU[:], rhs=adt[:], start=True, stop=True), reads=[r_triU, r_adt], awrites=[r_pB])
        yield
        S.op("pe", lambda e: e.matmul(pB[:, 272:288], lhsT=onesf[:], rhs=adt[:], start=True, stop=True), reads=[r_onesf, r_adt], awrites=[r_pB])
        yield
        S.op("act", lambda e: e.activation(out=sm[:, 0, :], in_=pB[:, 256:272], func=AF.Exp), reads=[r_pB], writes=[r_sm])
        yield
        S.op("act", lambda e: e.activation(out=sm[:, 1, :], in_=pB[:, 272:288], func=AF.Copy), reads=[r_pB], awrites=[r_sm])
        yield
        S.op("act", lambda e: e.activation(out=sm[:, 2, :], in_=pB[:, 272:288], func=AF.Exp), reads=[r_pB], awrites=[r_sm])
        yield
        S.op("dve", lambda e: e.tensor_tensor(out=sm[:, 3, :], in0=sm[:, 1, :], in1=pB[:, 256:272], op=ALU.subtract), reads=[r_sm, r_pB], awrites=[r_sm])
        yield
        S.op("act", lambda e: e.activation(out=sm[:, 4, :], in_=sm[:, 3, :], func=AF.Exp), reads=[r_sm], awrites=[r_sm])
        yield
        S.op("dve", lambda e: e.tensor_tensor(out=Amat[:], in0=triU[:].unsqueeze(1).to_broadcast([128, 16, 128]), in1=bc3(adt[:], 128), op=ALU.mult),
             reads=[r_triU, r_adt], writes=[r_A])
        yield
        pCB, r_pCB = bank[4]
        for g in range(4):
            S.op("pe", lambda e: e.matmul(pCB[:, g * 128:(g + 1) * 128], lhsT=xact[:, 8 + g, :], rhs=xact[:, 12 + g, :], start=True, stop=True),
                 reads=[r_xact], writes=[r_pCB] if g == 0 else (), awrites=() if g == 0 else [r_pCB])
            yield
        S.op("dve", lambda e: e.tensor_tensor(out=v3(xdt[:]), in0=v3(xs_tok[:]), in1=bc3(d_t[:], 64), op=ALU.mult), reads=[r_xs, r_d], writes=[r_xdt])
        yield
        S.op("pool", lambda e: e.tensor_tensor(out=v3(xdd[:]), in0=v3(xdt[:]), in1=bc3(sm[:, 4, :], 64), op=ALU.mult), reads=[r_xdt, r_sm], writes=[r_xdd])
        yield
        for g in range(4):
            pD, r_pD = bank[5 + g % 2]
            S.op("pe", lambda e: e.matmul(pD[:], lhsT=SLm[:], rhs=Amat[:, 4 * g:4 * g + 4, :], start=True, stop=False), reads=[r_SL, r_A], writes=[r_pD])
            yield
            S.op("pe", lambda e: e.matmul(pD[:], lhsT=ident_b[:], rhs=negm4[:], start=False, stop=True), reads=[r_identb, r_negm4], awrites=[r_pD])
            yield
            S.op("act", lambda e: e.activation(out=Lt[:, g % 2, :], in_=pD[:], func=AF.Exp), reads=[r_pD], writes=[r_Lt] if g % 2 == 0 else (), awrites=() if g % 2 == 0 else [r_Lt])
            yield
            S.op("dve", lambda e: e.tensor_tensor(out=Mt[:, 4 * g:4 * g + 4, :], in0=Lt[:, g % 2, :].rearrange("p (h l) -> p h l", h=4),
                                                 in1=pCB[:, g * 128:(g + 1) * 128].unsqueeze(1).to_broadcast([128, 4, 128]), op=ALU.mult),
                 reads=[r_Lt, r_pCB], writes=[r_Mt] if g == 0 else (), awrites=() if g == 0 else [r_Mt])
            yield
        if t == 0:
            S.op("pool", lambda e: e.memset(prev_f[:], 0.0), writes=[r_pf])
            yield
            S.op("pool", lambda e: e.memset(prev_b[:], 0.0), writes=[r_pb])
            yield
        for hh in range(2):
            pY, r_pY = bank[5]
            pO, r_pO = bank[6]
            pS, r_pS = bank[7]
            c0 = hh * 512
            for h8 in range(8):
                h = hh * 8 + h8
                S.op("pe", lambda e: e.matmul(pY[:, h8 * 64:(h8 + 1) * 64], lhsT=Mt[:, h, :], rhs=xdt[:, h * 64:(h + 1) * 64], start=True, stop=True),
                     reads=[r_Mt, r_xdt], writes=[r_pY] if h8 == 0 else (), awrites=() if h8 == 0 else [r_pY])
                yield
            for g2 in range(2):
                g = hh * 2 + g2
                S.op("pe", lambda e: e.matmul(pO[:, g2 * 256:(g2 + 1) * 256], lhsT=xact[:, 12 + g, :], rhs=prev_b[:, g * 256:(g + 1) * 256], start=True, stop=True),
                     reads=[r_xact, r_pb], writes=[r_pO] if g2 == 0 else (), awrites=() if g2 == 0 else [r_pO])
                yield
            for g2 in range(2):
                g = hh * 2 + g2
                S.op("pe", lambda e: e.matmul(pS[:, g2 * 256:(g2 + 1) * 256], lhsT=B_tok[:, g * 128:(g + 1) * 128], rhs=xdd[:, g * 256:(g + 1) * 256], start=True, stop=True),
                     reads=[r_Bt, r_xdd], writes=[r_pS] if g2 == 0 else (), awrites=() if g2 == 0 else [r_pS])
                yield
            hs = slice(hh * 8, hh * 8 + 8)

            def v8(ap):
                return ap.rearrange("p (h q) -> p h q", q=64)
            ex8 = sm[:, 0, hs].unsqueeze(2).to_broadcast([128, 8, 64])
            cd8 = sm[:, 2, hs].unsqueeze(2).to_broadcast([128, 8, 64])
            ds8 = dskip_bc[:, hs].unsqueeze(2).to_broadcast([128, 8, 64])
            S.op("dve", lambda e: e.tensor_tensor(out=v8(t1[:, c0:c0 + 512]), in0=v8(pO[:]), in1=ex8, op=ALU.mult), reads=[r_pO, r_sm], writes=[r_t1] if hh == 0 else (), awrites=() if hh == 0 else [r_t1])
            yield
            S.op("dve", lambda e: e.tensor_tensor(out=t1[:, c0:c0 + 512], in0=t1[:, c0:c0 + 512], in1=pY[:], op=ALU.add), reads=[r_t1, r_pY], awrites=[r_t1])
            yield
            S.op("pool", lambda e: e.tensor_tensor(out=v8(t2[:, c0:c0 + 512]), in0=v8(xs_tok[:, c0:c0 + 512]), in1=ds8, op=ALU.mult), reads=[r_xs, r_dskip], writes=[r_t2] if hh == 0 else (), awrites=() if hh == 0 else [r_t2])
            yield
            S.op("dve", lambda e: e.tensor_tensor(out=v8(prev_f[:, c0:c0 + 512]), in0=v8(prev_f[:, c0:c0 + 512]), in1=cd8, op=ALU.mult), reads=[r_pf, r_sm], awrites=[r_pf])
            yield
            S.op("dve", lambda e: e.tensor_tensor(out=prev_f[:, c0:c0 + 512], in0=prev_f[:, c0:c0 + 512], in1=pS[:], op=ALU.add), reads=[r_pf, r_pS], awrites=[r_pf])
            yield
            S.op("act", lambda e: e.activation(out=prev_b[:, c0:c0 + 512], in_=prev_f[:, c0:c0 + 512], func=AF.Copy), reads=[r_pf, r_pO], awrites=[r_pb])
            yield
        S.op("pool", lambda e: e.tensor_tensor(out=t1[:], in0=t1[:], in1=t2[:], op=ALU.add), reads=[r_t1, r_t2], writes=[r_t1])
        yield
        S.op("pool", lambda e: e.tensor_tensor(out=t1[:], in0=t1[:], in1=z_t[:], op=ALU.mult), reads=[r_t1, r_z], writes=[r_t1])
        yield
        for g in range(4):
            S.op("act", lambda e: e.activation(out=junk[:], in_=t1[:, g * 256:(g + 1) * 256], func=AF.Square, accum_out=sm[:, 5, g:g + 1]),
                 reads=[r_t1], writes=[r_junk], awrites=[r_sm])
            yield
        S.op("act", lambda e: e.activation(out=sm[:, 5, 4:8], in_=sm[:, 5, 0:4], func=AF.Sqrt, scale=1.0 / 256.0, bias=EPS), reads=[r_sm], awrites=[r_sm])
        yield
        S.op("dve", lambda e: e.reciprocal(out=sm[:, 5, 8:12], in_=sm[:, 5, 4:8]), reads=[r_sm], awrites=[r_sm])
        yield
        S.op("dve", lambda e: e.tensor_tensor(out=t1[:].rearrange("p (g q) -> p g q", g=4), in0=t1[:].rearrange("p (g q) -> p g q", g=4),
                                             in1=sm[:, 5, 8:12].unsqueeze(2).to_broadcast([128, 4, 256]), op=ALU.mult), reads=[r_t1, r_sm], writes=[r_t1])
        yield
        S.op("dve", lambda e: e.tensor_tensor(out=ob[:], in0=t1[:], in1=normw_bc[:], op=ALU.mult), reads=[r_t1, r_normw], writes=[r_ob])
        yield
        pT, r_pT = bank[4]
        pT_b = pT[:].bitcast(BF16)
        for k in range(8):
            S.op("pe", lambda e: e.transpose(out=pT_b[:, k * 128:(k + 1) * 128], in_=ob[:, k * 128:(k + 1) * 128], identity=ident_b[:]),
                 reads=[r_ob, r_identb], writes=[r_pT] if k == 0 else (), awrites=() if k == 0 else [r_pT])
            yield
        o_t, r_o = obT[sl2]
        S.op("act", lambda e: e.activation(out=o_t[:].rearrange("p k t -> p (k t)"), in_=pT_b, func=AF.Copy), reads=[r_pT], writes=[r_o])
        yield
        S.dma("sp", obT_d[:, :, tok0:tok0 + 128].rearrange("c p t -> p c t"), o_t[:], reads=[r_o], awrites=[r_obT])
        yield

    load_chunk(0, 0, 0)
    load_chunk(0, 1, 1)
    interleave((chunk_gen(ci) for ci in range(nch)), B_STAGGER)
    C.pop()


def _t5_bucket_np(rel):
    half, max_exact = 16, 8
    ret = (rel > 0).astype(np.int32) * half
    n = np.abs(rel)
    nf = np.maximum(n, 1).astype(np.float32)
    large = max_exact + (np.log(nf / np.float32(max_exact)) / np.float32(np.log(128.0 / 8.0)) * np.float32(half - max_exact)).astype(np.int32)
    large = np.minimum(large, half - 1)
    return ret + np.where(n < max_exact, n, large)


def _t5_blocks(rel_bias):
    k = np.arange(128)[:, None]
    q = np.arange(128)[None, :]
    out = np.zeros((128, 2, 8, 128), np.float32)
    for dl in range(2):
        bk = _t5_bucket_np((k - 128 * dl) - q)
        for h in range(8):
            out[:, dl, h, :] = rel_bias[bk, h]
    return out


def host_inputs(inputs, core):
    b0 = core * NB
    f = lambda a: np.ascontiguousarray(a, dtype=np.float32)
    c = inputs["c"][b0:b0 + NB]
    m = {
        "x": f(inputs["x"][b0:b0 + NB].reshape(NTOK, D)),
        "cT": f(c.reshape(NB, 8, 128).transpose(2, 1, 0)),
        "w_mod": f(inputs["w_mod"][0]),
        "b_mod": f(inputs["b_mod"][0].reshape(1, -1)),
        "w_in": f(inputs["w_in"][0]),
        "ident": np.eye(128, dtype=np.float32),
        "kv_norm_w": f(inputs["kv_norm_w"][0].reshape(1, -1)),
        "idx_k_norm_w": f(inputs["idx_k_norm_w"][0].reshape(1, -1)),
        "idx_k_norm_b": f(inputs["idx_k_norm_b"][0].reshape(1, -1)),
        "dt_bias": f(inputs["dt_bias"][0].reshape(1, -1)),
        "convw": f(inputs["conv_w"][0].reshape(4, 16, 128).transpose(2, 1, 0)),
        "convb": f(inputs["conv_b"][0].reshape(16, 128).T),
        "a_log": f(inputs["a_log"][0].reshape(1, -1)),
        "d_skip": f(inputs["d_skip"][0].reshape(1, -1)),
        "ssm_norm_w": f(inputs["ssm_norm_w"][0].reshape(1, -1)),
        "w_proj_a": f(inputs["w_proj_a"][0]), "w_proj_b": f(inputs["w_proj_b"][0]), "w_out": f(inputs["w_out"][0]),
        "ln1_g": f(inputs["ln1_g"][0].reshape(1, -1)), "ln1_b": f(inputs["ln1_b"][0].reshape(1, -1)),
        "ln2_g": f(inputs["ln2_g"][0].reshape(1, -1)), "ln2_b": f(inputs["ln2_b"][0].reshape(1, -1)),
        "w_router": f(inputs["w_router"][0]), "b_router": f(inputs["b_router"][0].reshape(1, -1)),
        "w1": f(inputs["w1"][0].reshape(NE * D, 2 * D)), "w2": f(inputs["w2"][0].reshape(NE * D, D)),
        "b1r": f(inputs["b1"][0].reshape(NE, 16, 128).transpose(0, 2, 1).reshape(NE * 128, 16)),
        "b2": f(inputs["b2"][0]),
        "sut": np.triu(np.ones((128, 128), np.float32), 1),
        "thr16": np.tile(512.0 * np.arange(16, dtype=np.float32), NE).reshape(1, -1),
        "bstart": (512.0 * np.arange(NBLK, dtype=np.float32)).reshape(1, -1),
        "kp": (np.arange(8, dtype=np.float32)[None, :] * 128 + np.arange(128, dtype=np.float32)[:, None]),
        "pcol": np.arange(128, dtype=np.float32).reshape(128, 1),
        "sut32": np.triu(np.ones((NE, NE), np.float32), 1),
        "tz": _t5_blocks(f(inputs["rel_bias"])),
        "cfar": f(inputs["rel_bias"][15:16, :]),
        "triU": np.triu(np.ones((128, 128), np.float32)),
        "SL": np.tril(np.ones((128, 128), np.float32), -1),
        "negm4": np.tile(np.tril(np.full((128, 128), NEG, np.float32), -1), (1, 4)),
    }
    return m


def kernel(**inputs):
    nc = build_program()
    in_maps = [host_inputs(inputs, c) for c in range(NCORES)]
    res = run_bass_kernel_spmd(nc, in_maps, core_ids=list(range(NCORES)))
    out = np.stack([np.asarray(r["out"]).reshape(NB, SEQ, D) for r in res.results], 0)
    return out.reshape(NCORES * NB, SEQ, D).astype(np.float32)
```

```python
import numpy as np
import concourse.bass as bass
import concourse.mybir as mybir
from concourse.bass_utils import run_bass_kernel_spmd

F32 = mybir.dt.float32
BF16 = mybir.dt.bfloat16
I32 = mybir.dt.int32
ALU = mybir.AluOpType
AF = mybir.ActivationFunctionType
AX = mybir.AxisListType

NCORES = 8
SEQ = 2048
D = 1024
NB = 4
NTOK = NB * SEQ
NT = NTOK // 128
TPS = SEQ // 128
DIN = 6872
C_Q, C_KV, C_IQ, C_IK, C_IW, C_Z, C_XBC, C_DT, C_GA, C_GB = 0, 1024, 1152, 1664, 1728, 1736, 2760, 4808, 4824, 5848
NE = 32
NBLK = NTOK * 4 // 512 + NE
ALPHA = 2.0 ** 0.25
EPS = 1e-5
NEG = -30000.0

B_STAGGER = 104
ENGS = ("pe", "act", "dve", "pool", "sp")


class Res:
    __slots__ = ("name", "writers", "readers", "dsem", "dcount", "dram")

    def __init__(self, name):
        self.name = name
        self.dram = False
        self.writers = {}
        self.readers = {}
        self.dsem = None
        self.dcount = 0


class Sched:
    def __init__(self, nc):
        self.nc = nc
        self.eng = {"pe": nc.tensor, "act": nc.scalar, "dve": nc.vector,
                    "pool": nc.gpsimd, "sp": nc.sync}
        self.sem = {e: nc.alloc_semaphore("prog_" + e) for e in ENGS}
        self.cnt = {e: 0 for e in ENGS}
        self.waited = {e: {} for e in ENGS}
        self.all_res = []
        self.nwaits = 0
        self.nops = 0
        self.sempool = []

    def retire(self, rs):
        for r in rs:
            if r.dsem is not None:
                self.sempool.append((r.dsem, r.dcount))
                r.dsem = None
            if r in self.all_res:
                self.all_res.remove(r)

    def res(self, name):
        r = Res(name)
        self.all_res.append(r)
        return r

    def _need(self, eng, tok, deps):
        sem, val = tok
        k = sem.num
        if self.waited[eng].get(k, 0) >= val:
            return
        if k not in deps or deps[k][1] < val:
            deps[k] = (sem, val)

    def op(self, eng, fn, reads=(), writes=(), awrites=(), dma=False):
        deps = {}
        mykey = None if dma else eng
        for r in reads:
            for k, tok in r.writers.items():
                if k == mykey and eng == "pe":
                    continue
                self._need(eng, tok, deps)
        for r in writes:
            for k, tok in list(r.writers.items()) + list(r.readers.items()):
                if k == mykey:
                    continue
                self._need(eng, tok, deps)
        for r in awrites:
            for k, tok in r.readers.items():
                if k == mykey:
                    continue
                self._need(eng, tok, deps)
        e = self.eng[eng]
        for k, (sem, val) in deps.items():
            e.wait_ge(sem, val)
            self.waited[eng][k] = val
            self.nwaits += 1
        ins = fn(e)
        self.nops += 1
        if dma:
            dst = (list(writes) + list(awrites))[0]
            if dst.dram:
                sb = [r for r in reads if not r.dram]
                if sb:
                    dst = sb[0]
            if dst.dsem is None:
                if self.sempool:
                    dst.dsem, dst.dcount = self.sempool.pop()
                else:
                    dst.dsem = self.nc.alloc_semaphore("d_" + dst.name)
            dst.dcount += 16
            ins.then_inc(dst.dsem, 16)
            tok = (dst.dsem, dst.dcount)
            key = "dma%d" % dst.dsem.num
        else:
            self.cnt[eng] += 1
            ins.then_inc(self.sem[eng], 1)
            tok = (self.sem[eng], self.cnt[eng])
            key = eng
        for r in reads:
            r.readers[key] = tok
        for r in writes:
            r.writers = {key: tok}
            r.readers = {}
        for r in awrites:
            r.writers[key] = tok
        return ins

    def dma(self, eng, out, in_, reads=(), writes=(), awrites=(), **kw):
        return self.op(eng, lambda e: e.dma_start(out=out, in_=in_, **kw),
                       reads=reads, writes=writes, awrites=awrites, dma=True)

    def barrier(self):
        toks = {}
        for e in ENGS:
            if self.cnt[e]:
                toks[self.sem[e].num] = (self.sem[e], self.cnt[e])
        for r in self.all_res:
            if r.dsem is not None and r.dcount:
                toks[r.dsem.num] = (r.dsem, r.dcount)
        for e in ENGS:
            for k, (sem, val) in toks.items():
                if self.waited[e].get(k, 0) >= val:
                    continue
                self.eng[e].wait_ge(sem, val)
                self.waited[e][k] = val
                self.nwaits += 1
        for r in self.all_res:
            r.writers = {}
            r.readers = {}


class Ctx:
    def __init__(self, nc, S):
        self.nc = nc
        self.S = S
        self.stack = []

    def push(self):
        self.stack.append([])

    def pop(self):
        self.S.barrier()
        gs = self.stack.pop()
        self.S.retire([r for (_, r) in gs])
        for g, _ in reversed(gs):
            g.__exit__(None, None, None)

    def sb(self, name, shape, dt):
        g = self.nc.sbuf_tensor("s_" + name, list(shape), dt)
        t = g.__enter__()
        r = self.S.res(name)
        self.stack[-1].append((g, r))
        return t, r

    def ps(self, name, shape, dt=F32):
        g = self.nc.psum_tensor("p_" + name, list(shape), dt)
        t = g.__enter__()
        r = self.S.res(name)
        self.stack[-1].append((g, r))
        return t, r

    def ring(self, kind, name, n, shape, dt):
        f = self.sb if kind == "sb" else self.ps
        return [f("%s%d" % (name, i), shape, dt) for i in range(n)]


def interleave(gens, stagger):
    active = []
    it = iter(gens)
    nxt = next(it, None)
    tick = 0
    while active or nxt is not None:
        if nxt is not None and tick % stagger == 0:
            active.append(nxt)
            nxt = next(it, None)
        for g in list(active):
            try:
                next(g)
            except StopIteration:
                active.remove(g)
        tick += 1


def build_program(debug=()):
    nc = bass.Bass("TRN2", target_bir_lowering=False)
    S = Sched(nc)
    C = Ctx(nc, S)
    dbg = set(debug)

    def din(name, shape, dt=F32):
        return nc.dram_tensor(name, list(shape), dt, kind="ExternalInput").ap()

    def scratch(name, shape, dt):
        kind = "ExternalOutput" if name in dbg else "Internal"
        r = S.res(name)
        r.dram = True
        return nc.dram_tensor(name, list(shape), dt, kind=kind).ap(), r

    x_d = din("x", [NTOK, D])
    cT_d = din("cT", [128, 8, NB])
    wmod_d = din("w_mod", [D, 6 * D])
    bmod_d = din("b_mod", [1, 6 * D])
    win_d = din("w_in", [D, DIN])
    ident_d = din("ident", [128, 128])
    kvw_d = din("kv_norm_w", [1, 128])
    ikw_d = din("idx_k_norm_w", [1, 64])
    ikb_d = din("idx_k_norm_b", [1, 64])
    dtb_d = din("dt_bias", [1, 16])
    convw_d = din("convw", [128, 16, 4])
    convb_d = din("convb", [128, 16])
    alog_d = din("a_log", [1, 16])
    dskip_d = din("d_skip", [1, 16])
    normw_d = din("ssm_norm_w", [1, D])
    triU_d = din("triU", [128, 128])
    SL_d = din("SL", [128, 128])
    negm4_d = din("negm4", [128, 512])
    tz_d = din("tz", [128, 2, 8, 128])
    cfar_d = din("cfar", [1, 8])
    wpa_d = din("w_proj_a", [D, D]); wpb_d = din("w_proj_b", [D, D]); wout_d = din("w_out", [D, D])
    ln1g_d = din("ln1_g", [1, D]); ln1b_d = din("ln1_b", [1, D]); ln2g_d = din("ln2_g", [1, D]); ln2b_d = din("ln2_b", [1, D])
    wr_d = din("w_router", [D, NE]); br_d = din("b_router", [1, NE])
    w1_d = din("w1", [NE * D, 2 * D]); w2_d = din("w2", [NE * D, D])
    b1r_d = din("b1r", [NE * 128, 16]); b2_d = din("b2", [NE, D])
    sut_d = din("sut", [128, 128]); thr16_d = din("thr16", [1, NE * 16]); bstart_d = din("bstart", [1, NBLK])
    kp_d = din("kp", [128, 8]); pcol_d = din("pcol", [128, 1]); sut32_d = din("sut32", [NE, NE])
    r_in = S.res("inputs")
    r_in.dram = True
    out_d = nc.dram_tensor("out", [NTOK, D], F32, kind="ExternalOutput").ap()
    r_out = S.res("out")
    r_out.dram = True

    mod_d, r_mod = scratch("mod_s", [NB, 6 * D], F32)
    qT_d, r_qT = scratch("qT_s", [8, 128, NTOK], BF16)
    iqT_d, r_iqT = scratch("iqT_s", [4, 128, NTOK], BF16)
    xbcT_d, r_xbcT = scratch("xbcT_s", [16, 128, NTOK], BF16)
    sgT_d, r_sgT = scratch("sgT_s", [16, 128, NTOK], BF16)
    kva_d, r_kva = scratch("kva_s", [NTOK, 136], BF16)
    kvT_d, r_kvT = scratch("kvT_s", [128, NTOK], BF16)
    ikT_d, r_ikT = scratch("ikT_s", [128, NTOK], BF16)
    iw_d, r_iw = scratch("iw_s", [NTOK, 8], F32)
    dt_d, r_dt = scratch("dt_s", [NTOK, 16], F32)
    zs_d, r_zs = scratch("zs_s", [NTOK, D], BF16)
    obT_d, r_obT = scratch("obT_s", [8, 128, NTOK], BF16)
    oaT_d, r_oaT = scratch("oaT_s", [8, 128, NTOK], BF16)
    mT_d, r_mTd = scratch("mT_s", [8, 128, NTOK], BF16)
    x1_d, r_x1 = scratch("x1_s", [NTOK, D], F32)
    u2_d, r_u2 = scratch("u2_s", [NTOK, D], BF16)
    xs_d, r_xsd = scratch("xsort_s", [NBLK * 512, D], BF16)
    ys_d, r_ysd = scratch("ysort_s", [NBLK * 512, D], BF16)

    w1b_d, r_w1bd = scratch("w1b_s", [NE * D, 2 * D], BF16)
    w2b_d, r_w2bd = scratch("w2b_s", [NE * D, D], BF16)

    C.push()
    ident_f, r_identf = C.sb("ident_f", [128, 128], F32)
    ident_b, r_identb = C.sb("ident_b", [128, 128], BF16)
    S.dma("sp", ident_f[:], ident_d, reads=[r_in], writes=[r_identf])
    S.op("dve", lambda e: e.tensor_copy(out=ident_b[:], in_=ident_f[:]), reads=[r_identf], writes=[r_identb])
    modT, r_modT = C.sb("modT", [128, 48, NB], F32)

    C.push()
    cT, r_cT = C.sb("cT", [128, 8, NB], F32)
    ones1, r_ones1 = C.sb("ones1", [1, NB], F32)
    bmod, r_bmod = C.sb("bmod", [1, 6 * D], F32)
    modrow, r_modrow = C.sb("modrow", [NB, 6 * D], F32)
    wm = C.ring("sb", "wm", 2, [128, 8, 512], F32)
    pmod = C.ring("ps", "pmod", 2, [NB, 512], F32)
    S.dma("sp", cT[:], cT_d, reads=[r_in], writes=[r_cT])
    S.dma("sp", bmod[:], bmod_d, reads=[r_in], writes=[r_bmod])
    S.op("act", lambda e: e.activation(out=cT[:], in_=cT[:], func=AF.Silu), reads=[r_cT], writes=[r_cT])
    S.op("dve", lambda e: e.memset(ones1[:], 1.0), writes=[r_ones1])
    for g in range(12):
        wt, r_wt = wm[g % 2]
        pt, r_pt = pmod[g % 2]
        S.dma("sp", wt[:], wmod_d[:, g * 512:(g + 1) * 512].rearrange("(k p) n -> p k n", p=128), reads=[r_in], writes=[r_wt])
        for k in range(8):
            S.op("pe", lambda e: e.matmul(pt[:], lhsT=cT[:, k, :], rhs=wt[:, k, :], start=(k == 0), stop=False),
                 reads=[r_cT, r_wt], writes=[r_pt] if k == 0 else (), awrites=() if k == 0 else [r_pt])
        S.op("pe", lambda e: e.matmul(pt[:], lhsT=ones1[:], rhs=bmod[:, g * 512:(g + 1) * 512], start=False, stop=True),
             reads=[r_ones1, r_bmod], awrites=[r_pt])
        S.op("act", lambda e: e.activation(out=modrow[:, g * 512:(g + 1) * 512], in_=pt[:], func=AF.Copy), reads=[r_pt], awrites=[r_modrow])
    S.dma("sp", mod_d, modrow[:], reads=[r_modrow], writes=[r_mod])
    pmt, r_pmt = pmod[0]
    pmt2, r_pmt2 = C.ps("pmodT", [128, 48 * NB], F32)
    for j in range(48):
        S.op("pe", lambda e: e.transpose(out=pmt2[:, j * NB:(j + 1) * NB], in_=modrow[:, j * 128:(j + 1) * 128], identity=ident_f[0:NB, 0:NB]),
             reads=[r_modrow, r_identf], writes=[r_pmt2] if j == 0 else (), awrites=() if j == 0 else [r_pmt2])
    S.op("act", lambda e: e.activation(out=modT[:].rearrange("p j b -> p (j b)"), in_=pmt2[:], func=AF.Copy), reads=[r_pmt2], writes=[r_modT])
    S.op("dve", lambda e: e.tensor_scalar_add(out=modT[:, 8:16, :], in0=modT[:, 8:16, :], scalar1=1.0), reads=[r_modT], awrites=[r_modT])
    S.op("dve", lambda e: e.tensor_scalar_add(out=modT[:, 32:40, :], in0=modT[:, 32:40, :], scalar1=1.0), reads=[r_modT], awrites=[r_modT])
    C.pop()
    if "stop0" in dbg:
        C.pop()
        return nc

    C.push()
    wI, r_wI = C.sb("wI", [128, 8, DIN], BF16)
    for i, (a, b_) in enumerate([(0, 1024), (1024, 1736), (1736, 2760), (2760, 3784), (3784, 4808), (4808, 5848), (5848, 6872)]):
        S.dma("pool", wI[:, :, a:b_], win_d[:, a:b_].rearrange("(k p) n -> p k n", p=128), reads=[r_in],
              writes=[r_wI] if i == 0 else (), awrites=() if i == 0 else [r_wI])
    kvw_bc, r_kvw = C.sb("kvw_bc", [128, 128], F32)
    ikw_bc, r_ikw = C.sb("ikw_bc", [128, 64], F32)
    ikb_bc, r_ikb = C.sb("ikb_bc", [128, 64], F32)
    dtb_bc, r_dtb = C.sb("dtb_bc", [128, 16], F32)
    S.dma("sp", kvw_bc[:], kvw_d.partition_broadcast(128), reads=[r_in], writes=[r_kvw])
    S.dma("sp", ikw_bc[:], ikw_d.partition_broadcast(128), reads=[r_in], writes=[r_ikw])
    S.dma("sp", ikb_bc[:], ikb_d.partition_broadcast(128), reads=[r_in], writes=[r_ikb])
    S.dma("sp", dtb_bc[:], dtb_d.partition_broadcast(128), reads=[r_in], writes=[r_dtb])

    xt = C.ring("sb", "xt", 2, [128, D], F32)
    xn = C.ring("sb", "xn", 2, [128, D], BF16)
    st = C.ring("sb", "st", 2, [128, 2, 6], F32)
    mv = C.ring("sb", "mv", 2, [128, 2], F32)
    rs = C.ring("sb", "rs", 2, [128, 1], F32)
    uT = C.ring("sb", "uT", 2, [128, 8, 512], BF16)
    ptr = C.ring("ps", "ptr", 1, [128, 8, 128], BF16)
    psm = C.ring("ps", "psm", 1, [128, 512], F32)
    pz = C.ring("ps", "pz", 2, [128, 512], F32)
    pf = C.ring("ps", "pf", 3, [128, 512], F32)
    pt2 = C.ring("ps", "pt2", 1, [128, 2, 128], BF16)
    kva = C.ring("sb", "kva", 2, [128, 136], BF16)
    ikn = C.ring("sb", "ikn", 2, [128, 128], BF16)
    sml = C.ring("sb", "sml", 2, [128, 64], F32)
    ikf = C.ring("sb", "ikf", 2, [128, 64], F32)
    iwt = C.ring("sb", "iwt", 2, [128, 8], F32)
    dtt = C.ring("sb", "dtt", 2, [128, 4, 16], F32)
    zst = C.ring("sb", "zst", 2, [128, D], BF16)
    tT = C.ring("sb", "tT", 2, [128, 2, 128], BF16)
    stg = C.ring("sb", "stg", 2, [128, 8, 512], BF16)
    for i in range(2):
        S.op("pool", lambda e: e.memset(kva[i][0][:, 128:136], 1.0), writes=[kva[i][1]])

    def ln_stats(src, r_src, i):
        st_t, r_st = st[i]
        mv_t, r_mv = mv[i]
        rs_t, r_rs = rs[i]
        for j in range(2):
            S.op("dve", lambda e: e.bn_stats(out=st_t[:, j, :], in_=src[:, j * 512:(j + 1) * 512]), reads=[r_src],
                 writes=[r_st] if j == 0 else (), awrites=() if j == 0 else [r_st])
        S.op("dve", lambda e: e.bn_aggr(out=mv_t[:], in_=st_t[:].rearrange("p a b -> p (a b)")), reads=[r_st], writes=[r_mv])
        S.op("act", lambda e: e.activation(out=rs_t[:], in_=mv_t[:, 1:2], func=AF.Sqrt, bias=EPS), reads=[r_mv], writes=[r_rs])
        S.op("dve", lambda e: e.reciprocal(out=rs_t[:], in_=rs_t[:]), reads=[r_rs], writes=[r_rs])
        return mv_t, r_mv, rs_t, r_rs

    tcount = 0
    for g in range(NTOK // 512):
        b = (g * 512) // SEQ
        u_t, r_u = uT[g % 2]
        for i4 in range(4):
            t = g * 4 + i4
            tok0 = t * 128
            ri = tcount % 2
            tcount += 1
            x_t, r_x = xt[ri]
            xn_t, r_xn = xn[ri]
            if t == 0:
                S.dma("sp", x_t[:], x_d[0:128, :], reads=[r_in], writes=[r_x])
            if t + 1 < NT:
                S.dma("sp", xt[(ri + 1) % 2][0][:], x_d[tok0 + 128:tok0 + 256, :], reads=[r_in], writes=[xt[(ri + 1) % 2][1]])
            mv_t, r_mv, rs_t, r_rs = ln_stats(x_t, r_x, ri)
            S.op("dve", lambda e: e.tensor_scalar(out=xn_t[:], in0=x_t[:], scalar1=mv_t[:, 0:1], scalar2=rs_t[:], op0=ALU.subtract, op1=ALU.mult),
                 reads=[r_x, r_mv, r_rs], writes=[r_xn])
            p_t, r_p = ptr[0]
            for k in range(8):
                S.op("pe", lambda e: e.transpose(out=p_t[:, k, :], in_=xn_t[:, k * 128:(k + 1) * 128], identity=ident_b[:]),
                     reads=[r_xn, r_identb], writes=[r_p] if k == 0 else (), awrites=() if k == 0 else [r_p])
            for k in range(8):
                S.op("act", lambda e: e.activation(out=u_t[:, k, i4 * 128:(i4 + 1) * 128], in_=p_t[:, k, :], func=AF.Identity,
                                                   scale=modT[:, 8 + k, b:b + 1], bias=modT[:, k, b:b + 1]),
                     reads=[r_p, r_modT], writes=[r_u] if (k == 0 and i4 == 0) else (), awrites=() if (k == 0 and i4 == 0) else [r_u])
            ps_t, r_ps = psm[0]
            for (c0, c1, o0) in [(C_KV, C_KV + 128, 0), (C_IK, C_IK + 72, 128), (C_DT, C_DT + 16, 200)]:
                for k in range(8):
                    S.op("pe", lambda e: e.matmul(ps_t[:, o0:o0 + (c1 - c0)], lhsT=u_t[:, k, i4 * 128:(i4 + 1) * 128], rhs=wI[:, k, c0:c1], start=(k == 0), stop=(k == 7)),
                         reads=[r_u, r_wI], writes=[r_ps] if (k == 0 and o0 == 0) else (), awrites=() if (k == 0 and o0 == 0) else [r_ps])
            zp = []
            for h in range(2):
                pz_t, r_pz = pz[h]
                zp.append((pz_t, r_pz))
                for k in range(8):
                    S.op("pe", lambda e: e.matmul(pz_t[:], lhsT=u_t[:, k, i4 * 128:(i4 + 1) * 128], rhs=wI[:, k, C_Z + h * 512:C_Z + (h + 1) * 512], start=(k == 0), stop=(k == 7)),
                         reads=[r_u, r_wI], writes=[r_pz] if k == 0 else (), awrites=() if k == 0 else [r_pz])
            sm_t, r_sm = sml[ri]
            kva_t, r_kva_t = kva[ri]
            ikf_t, r_ikf = ikf[ri]
            S.op("act", lambda e: e.activation(out=ikf_t[:, 0:64], in_=ps_t[:, 0:64], func=AF.Square, accum_out=sm_t[:, 0:1]), reads=[r_ps], writes=[r_ikf, r_sm])
            S.op("act", lambda e: e.activation(out=ikf_t[:, 0:64], in_=ps_t[:, 64:128], func=AF.Square, accum_out=sm_t[:, 1:2]), reads=[r_ps], writes=[r_ikf], awrites=[r_sm])
            S.op("dve", lambda e: e.tensor_tensor(out=sm_t[:, 0:1], in0=sm_t[:, 0:1], in1=sm_t[:, 1:2], op=ALU.add), reads=[r_sm], awrites=[r_sm])
            S.op("act", lambda e: e.activation(out=sm_t[:, 2:3], in_=sm_t[:, 0:1], func=AF.Sqrt, scale=1.0 / 128.0, bias=EPS), reads=[r_sm], awrites=[r_sm])
            S.op("dve", lambda e: e.reciprocal(out=sm_t[:, 3:4], in_=sm_t[:, 2:3]), reads=[r_sm], awrites=[r_sm])
            S.op("dve", lambda e: e.scalar_tensor_tensor(out=kva_t[:, 0:128], in0=ps_t[:, 0:128], scalar=sm_t[:, 3:4], in1=kvw_bc[:], op0=ALU.mult, op1=ALU.mult),
                 reads=[r_ps, r_sm, r_kvw], awrites=[r_kva_t])
            S.dma("sp", kva_d[tok0:tok0 + 128, :], kva_t[:], reads=[r_kva_t], awrites=[r_kva])
            ik_t, r_ik = ikn[ri]
            S.op("dve", lambda e: e.bn_stats(out=sm_t[:, 8:14], in_=ps_t[:, 128:192]), reads=[r_ps], awrites=[r_sm])
            S.op("dve", lambda e: e.bn_aggr(out=sm_t[:, 16:18], in_=sm_t[:, 8:14]), reads=[r_sm], awrites=[r_sm])
            S.op("act", lambda e: e.activation(out=sm_t[:, 18:19], in_=sm_t[:, 17:18], func=AF.Sqrt, bias=EPS), reads=[r_sm], awrites=[r_sm])
            S.op("dve", lambda e: e.reciprocal(out=sm_t[:, 19:20], in_=sm_t[:, 18:19]), reads=[r_sm], awrites=[r_sm])
            S.op("dve", lambda e: e.tensor_scalar(out=ikf_t[:], in0=ps_t[:, 128:192], scalar1=sm_t[:, 16:17], scalar2=sm_t[:, 19:20], op0=ALU.subtract, op1=ALU.mult),
                 reads=[r_ps, r_sm], writes=[r_ikf])
            S.op("dve", lambda e: e.tensor_tensor(out=ikf_t[:], in0=ikf_t[:], in1=ikw_bc[:], op=ALU.mult), reads=[r_ikf, r_ikw], writes=[r_ikf])
            S.op("dve", lambda e: e.tensor_tensor(out=ik_t[:, 0:64], in0=ikf_t[:], in1=ikb_bc[:], op=ALU.add), reads=[r_ikf, r_ikb], writes=[r_ik])
            S.op("dve", lambda e: e.tensor_copy(out=ik_t[:, 64:128], in_=ik_t[:, 0:64]), reads=[r_ik], awrites=[r_ik])
            iw_t, r_iwt = iwt[ri]
            S.op("act", lambda e: e.mul(out=iw_t[:], in_=ps_t[:, 192:200], mul=float(8 ** -0.5 * 64 ** -0.5)), reads=[r_ps], writes=[r_iwt])
            S.dma("sp", iw_d[tok0:tok0 + 128, :], iw_t[:], reads=[r_iwt], awrites=[r_iw])
            d_t, r_d = dtt[ri]
            S.op("dve", lambda e: e.tensor_tensor(out=d_t[:, 0, :], in0=ps_t[:, 200:216], in1=dtb_bc[:], op=ALU.add), reads=[r_ps, r_dtb], writes=[r_d])
            S.op("act", lambda e: e.activation(out=d_t[:, 1, :], in_=d_t[:, 0, :], func=AF.Abs), reads=[r_d], awrites=[r_d])
            S.op("act", lambda e: e.activation(out=d_t[:, 1, :], in_=d_t[:, 1, :], func=AF.Exp, scale=-1.0), reads=[r_d], awrites=[r_d])
            S.op("act", lambda e: e.activation(out=d_t[:, 1, :], in_=d_t[:, 1, :], func=AF.Ln, bias=1.0), reads=[r_d], awrites=[r_d])
            S.op("dve", lambda e: e.scalar_tensor_tensor(out=d_t[:, 2, :], in0=d_t[:, 0, :], scalar=0.0, in1=d_t[:, 1, :], op0=ALU.max, op1=ALU.add), reads=[r_d], awrites=[r_d])
            S.dma("sp", dt_d[tok0:tok0 + 128, :], d_t[:, 2, :], reads=[r_d], awrites=[r_dt])
            z_t, r_z = zst[ri]
            for h in range(2):
                S.op("act", lambda e: e.activation(out=z_t[:, h * 512:(h + 1) * 512], in_=zp[h][0][:], func=AF.Silu), reads=[zp[h][1]],
                     writes=[r_z] if h == 0 else (), awrites=() if h == 0 else [r_z])
            S.dma("sp", zs_d[tok0:tok0 + 128, :], z_t[:], reads=[r_z], awrites=[r_zs])
            p2, r_p2 = pt2[0]
            t_t, r_t = tT[ri]
            S.op("pe", lambda e: e.transpose(out=p2[:, 0, :], in_=kva_t[:, 0:128], identity=ident_b[:]), reads=[r_kva_t, r_identb], writes=[r_p2])
            S.op("pe", lambda e: e.transpose(out=p2[:, 1, :], in_=ik_t[:], identity=ident_b[:]), reads=[r_ik, r_identb], awrites=[r_p2])
            S.op("dve", lambda e: e.tensor_copy(out=t_t[:], in_=p2[:]), reads=[r_p2], writes=[r_t])
            S.dma("sp", kvT_d[:, tok0:tok0 + 128], t_t[:, 0, :], reads=[r_t], awrites=[r_kvT])
            S.dma("sp", ikT_d[:, tok0:tok0 + 128], t_t[:, 1, :], reads=[r_t], awrites=[r_ikT])
        g0 = g * 512
        fcount = 0
        for (c0, nch, dst, r_dst, ch0, func, scl) in [
                (C_Q, 8, qT_d, r_qT, 0, AF.Copy, float(128 ** -0.5)),
                (C_IQ, 4, iqT_d, r_iqT, 0, AF.Copy, 1.0),
                (C_XBC, 8, xbcT_d, r_xbcT, 0, AF.Copy, 1.0),
                (C_XBC + 1024, 8, xbcT_d, r_xbcT, 8, AF.Copy, 1.0),
                (C_GA, 8, sgT_d, r_sgT, 0, AF.Sigmoid, 1.0),
                (C_GB, 8, sgT_d, r_sgT, 8, AF.Sigmoid, 1.0)]:
            sg_t, r_sg = stg[fcount % 2]
            fcount += 1
            for j in range(nch):
                pf_t, r_pf = pf[j % 3]
                for k in range(8):
                    S.op("pe", lambda e: e.matmul(pf_t[:], lhsT=wI[:, k, c0 + j * 128:c0 + (j + 1) * 128], rhs=u_t[:, k, :], start=(k == 0), stop=(k == 7)),
                         reads=[r_u, r_wI], writes=[r_pf] if k == 0 else (), awrites=() if k == 0 else [r_pf])
                if func == AF.Copy and j % 2 == 1:
                    S.op("dve", lambda e: e.tensor_scalar_mul(out=sg_t[:, j, :], in0=pf_t[:], scalar1=scl), reads=[r_pf],
                         writes=[r_sg] if j == 0 else (), awrites=() if j == 0 else [r_sg])
                else:
                    S.op("act", lambda e: e.activation(out=sg_t[:, j, :], in_=pf_t[:], func=func, scale=scl), reads=[r_pf],
                         writes=[r_sg] if j == 0 else (), awrites=() if j == 0 else [r_sg])
            S.dma("sp", dst[ch0:ch0 + nch, :, g0:g0 + 512].rearrange("c p t -> p c t"), sg_t[:, 0:nch, :], reads=[r_sg], awrites=[r_dst])
    C.pop()
    if "stopA" in dbg:
        C.pop()
        return nc

    phase_B(nc, S, C, dbg, locals())
    if "stopB" in dbg:
        C.pop()
        return nc

    phase_C(nc, S, C, dbg, locals())
    if "stopC" in dbg:
        C.pop()
        return nc

    phase_DEF(nc, S, C, dbg, locals())
    C.pop()
    return nc


def phase_DEF(nc, S, C, dbg, L):
    g_ = lambda n: L[n]
    r_in = g_("r_in"); ident_b = g_("ident_b"); r_identb = g_("r_identb"); ident_f = g_("ident_f"); r_identf = g_("r_identf")
    modT = g_("modT"); r_modT = g_("r_modT"); mod_d = g_("mod_d"); r_mod = g_("r_mod")
    x_d = g_("x_d"); oaT_d, r_oaT = g_("oaT_d"), g_("r_oaT"); obT_d, r_obT = g_("obT_d"), g_("r_obT"); sgT_d, r_sgT = g_("sgT_d"), g_("r_sgT")
    x1_d, r_x1 = g_("x1_d"), g_("r_x1"); u2_d, r_u2 = g_("u2_d"), g_("r_u2"); xs_d, r_xsd = g_("xs_d"), g_("r_xsd"); ys_d, r_ysd = g_("ys_d"), g_("r_ysd")
    out_d, r_out = g_("out_d"), g_("r_out")
    b1r_d, b2_d = g_("b1r_d"), g_("b2_d")
    w1b_d, r_w1bd, w2b_d, r_w2bd = g_("w1b_d"), g_("r_w1bd"), g_("w2b_d"), g_("r_w2bd")

    C.push()
    idx4, r_idx4 = C.sb("idx4", [128, NT, 4], I32)
    g4, r_g4 = C.sb("g4", [128, NT, 4], F32)
    widx, r_widx = C.sb("widx", [128, NBLK, 8], I32)
    bidx, r_bidx = C.sb("bidx", [128, NBLK], I32)
    eidx, r_eidx = C.sb("eidx", [128, NBLK], I32)

    def ln_stats(src, r_src, st_t, r_st, mv_t, r_mv, rs_t, r_rs):
        for j in range(2):
            S.op("dve", lambda e: e.bn_stats(out=st_t[:, j, :], in_=src[:, j * 512:(j + 1) * 512]), reads=[r_src],
                 writes=[r_st] if j == 0 else (), awrites=() if j == 0 else [r_st])
        S.op("dve", lambda e: e.bn_aggr(out=mv_t[:], in_=st_t[:].rearrange("p a b -> p (a b)")), reads=[r_st], writes=[r_mv])
        S.op("act", lambda e: e.activation(out=rs_t[:], in_=mv_t[:, 1:2], func=AF.Sqrt, bias=EPS), reads=[r_mv], writes=[r_rs])
        S.op("dve", lambda e: e.reciprocal(out=rs_t[:], in_=rs_t[:]), reads=[r_rs], writes=[r_rs])

    mT_d, r_mTd = g_("mT_d"), g_("r_mTd")
    C.push()
    wpa, r_wpa = C.sb("wpa", [128, 8, D], BF16)
    wpb, r_wpb = C.sb("wpb", [128, 8, D], BF16)
    S.dma("pool", wpa[:], g_("wpa_d").rearrange("(k p) n -> p k n", p=128), reads=[r_in], writes=[r_wpa])
    S.dma("pool", wpb[:], g_("wpb_d").rearrange("(k p) n -> p k n", p=128), reads=[r_in], writes=[r_wpb])
    oaTr = C.ring("sb", "oaTd", 2, [128, 8, 512], BF16)
    obTr = C.ring("sb", "obTd", 2, [128, 8, 512], BF16)
    sgTr = C.ring("sb", "sgTd", 2, [128, 16, 512], BF16)
    mTr = C.ring("sb", "mT", 2, [128, 8, 512], BF16)
    ta = C.ring("sb", "ta", 3, [128, 512], F32)
    tb = C.ring("sb", "tb", 3, [128, 512], F32)
    pab = C.ring("ps", "pab", 4, [128, 512], F32)
    pbb = C.ring("ps", "pbb", 4, [128, 512], F32)

    def loadD1(g):
        g0 = g * 512
        S.dma("sp", oaTr[g % 2][0][:], oaT_d[:, :, g0:g0 + 512].rearrange("c p t -> p c t"), reads=[r_oaT], writes=[oaTr[g % 2][1]])
        S.dma("sp", obTr[g % 2][0][:], obT_d[:, :, g0:g0 + 512].rearrange("c p t -> p c t"), reads=[r_obT], writes=[obTr[g % 2][1]])
        S.dma("sp", sgTr[g % 2][0][:], sgT_d[:, :, g0:g0 + 512].rearrange("c p t -> p c t"), reads=[r_sgT], writes=[sgTr[g % 2][1]])

    NG = NTOK // 512
    loadD1(0)
    cn = 0
    for g in range(NG):
        g0 = g * 512
        if g + 1 < NG:
            loadD1(g + 1)
        oaT, r_oaTs = oaTr[g % 2]; obT, r_obTs = obTr[g % 2]; sgT, r_sgTs = sgTr[g % 2]; mT, r_mT = mTr[g % 2]
        for n in range(8):
            pa, r_pa = pab[cn % 4]
            pb, r_pb = pbb[cn % 4]
            ta_t, r_ta = ta[cn % 3]
            tb_t, r_tb = tb[cn % 3]
            cn += 1
            for k in range(8):
                S.op("pe", lambda e: e.matmul(pa[:], lhsT=wpa[:, k, n * 128:(n + 1) * 128], rhs=oaT[:, k, :], start=(k == 0), stop=(k == 7)),
                     reads=[r_wpa, r_oaTs], writes=[r_pa] if k == 0 else (), awrites=() if k == 0 else [r_pa])
            for k in range(8):
                S.op("pe", lambda e: e.matmul(pb[:], lhsT=wpb[:, k, n * 128:(n + 1) * 128], rhs=obT[:, k, :], start=(k == 0), stop=(k == 7)),
                     reads=[r_wpb, r_obTs], writes=[r_pb] if k == 0 else (), awrites=() if k == 0 else [r_pb])
            S.op("dve", lambda e: e.tensor_tensor(out=ta_t[:], in0=pa[:], in1=sgT[:, n, :], op=ALU.mult), reads=[r_pa, r_sgTs], writes=[r_ta])
            S.op("dve", lambda e: e.tensor_tensor(out=tb_t[:], in0=pb[:], in1=sgT[:, 8 + n, :], op=ALU.mult), reads=[r_pb, r_sgTs], writes=[r_tb])
            S.op("pool", lambda e: e.tensor_tensor(out=mT[:, n, :], in0=ta_t[:], in1=tb_t[:], op=ALU.add), reads=[r_ta, r_tb],
                 writes=[r_mT] if n == 0 else (), awrites=() if n == 0 else [r_mT])
        S.dma("sp", mT_d[:, :, g0:g0 + 512].rearrange("c p t -> p c t"), mT[:], reads=[r_mT], awrites=[r_mTd])
    C.pop()

    C.push()
    wout, r_wout = C.sb("wout", [128, 8, D], BF16)
    S.dma("pool", wout[:], g_("wout_d").rearrange("(k p) n -> p k n", p=128), reads=[r_in], writes=[r_wout])
    wr, r_wr = C.sb("wr", [128, 8, NE], F32)
    S.dma("sp", wr[:], g_("wr_d").rearrange("(k p) n -> p k n", p=128), reads=[r_in], writes=[r_wr])
    br_bc, r_br = C.sb("br_bc", [128, NE], F32)
    S.dma("sp", br_bc[:], g_("br_d").partition_broadcast(128), reads=[r_in], writes=[r_br])
    ln1g, r_ln1g = C.sb("ln1g", [128, D], F32)
    ln1b, r_ln1b = C.sb("ln1b", [128, D], F32)
    S.dma("sp", ln1g[:], g_("ln1g_d").partition_broadcast(128), reads=[r_in], writes=[r_ln1g])
    S.dma("sp", ln1b[:], g_("ln1b_d").partition_broadcast(128), reads=[r_in], writes=[r_ln1b])
    gater = C.ring("sb", "gate1", 2, [128, D], F32)
    sc2r = C.ring("sb", "sc2", 2, [128, D], F32)
    sh2r = C.ring("sb", "sh2", 2, [128, D], F32)
    sutf, r_sutf = C.sb("sutf", [128, 128], F32)
    sutb, r_sutb = C.sb("sutb", [128, 128], BF16)
    onesb, r_onesb = C.sb("onesb", [128, 128], BF16)
    S.dma("sp", sutf[:], g_("sut_d"), reads=[r_in], writes=[r_sutf])
    S.op("dve", lambda e: e.tensor_copy(out=sutb[:], in_=sutf[:]), reads=[r_sutf], writes=[r_sutb])
    S.op("dve", lambda e: e.memset(onesb[:], 1.0), writes=[r_onesb])
    maskall, r_maskall = C.sb("maskall", [128, NT, NE], F32)
    gall, r_gall = C.sb("gall", [128, NT, NE], F32)
    posall, r_posall = C.sb("posall", [128, NT, NE], F32)
    rrun, r_rrun = C.sb("rrun", [128, NE], F32)
    S.op("dve", lambda e: e.memset(rrun[:], 0.0), writes=[r_rrun])
    mtr = C.ring("sb", "mtl", 4, [128, 8, 128], BF16)
    xt = C.ring("sb", "xtd", 4, [128, D], F32)
    r1_r = C.ring("sb", "r1", 3, [128, D], F32)
    x1t_r = C.ring("sb", "x1t", 3, [128, D], F32)
    u2t_r = C.ring("sb", "u2t", 3, [128, D], F32)
    u2b = C.ring("sb", "u2b", 3, [128, D], BF16)
    u2T_r = C.ring("sb", "u2T", 3, [128, 8, 128], F32)
    st_r = C.ring("sb", "stD", 6, [128, 2, 6], F32)
    mv_r = C.ring("sb", "mvD", 6, [128, 2], F32)
    rs_r = C.ring("sb", "rsD", 6, [128, 1], F32)
    lg_r = C.ring("sb", "lg", 3, [128, NE], F32)
    m8_r = C.ring("sb", "m8D", 3, [128, 8], F32)
    sml_r = C.ring("sb", "smlD", 3, [128, 8], F32)
    ex_r = C.ring("sb", "exD", 3, [128, NE], F32)
    maskb_r = C.ring("sb", "maskb", 3, [128, NE], BF16)
    prs = C.ring("ps", "prs", 4, [128, 512], F32)
    ptT = C.ring("ps", "ptT", 2, [128, 512], F32)
    psl = C.ring("ps", "psl", 2, [128, 512], F32)

    def loadD2(t):
        tok0 = t * 128
        S.dma("sp", xt[t % 4][0][:], x_d[tok0:tok0 + 128, :], reads=[r_in], writes=[xt[t % 4][1]])
        S.dma("sp", mtr[t % 4][0][:], mT_d[:, :, tok0:tok0 + 128].rearrange("c p t -> p c t"), reads=[r_mTd], writes=[mtr[t % 4][1]])
        if t % TPS == 0:
            b = t // TPS
            gate1, r_gate1 = gater[b % 2]; sc2, r_sc2 = sc2r[b % 2]; sh2, r_sh2 = sh2r[b % 2]
            S.dma("sp", gate1[:], mod_d[b:b + 1, 2 * D:3 * D].partition_broadcast(128), reads=[r_mod], writes=[r_gate1])
            S.dma("sp", sh2[:], mod_d[b:b + 1, 3 * D:4 * D].partition_broadcast(128), reads=[r_mod], writes=[r_sh2])
            S.dma("sp", sc2[:], mod_d[b:b + 1, 4 * D:5 * D].partition_broadcast(128), reads=[r_mod], writes=[r_sc2])
            S.op("pool", lambda e: e.tensor_scalar_add(out=sc2[:], in0=sc2[:], scalar1=1.0), reads=[r_sc2], writes=[r_sc2])

    def stage1(t):
        tok0 = t * 128
        b = t // TPS
        gate1, r_gate1 = gater[b % 2]; sc2, r_sc2 = sc2r[b % 2]; sh2, r_sh2 = sh2r[b % 2]
        x_t, r_x = xt[t % 4]
        mt_t, r_mt = mtr[t % 4]
        r1, r_r1 = r1_r[t % 3]; x1t, r_x1t = x1t_r[t % 3]; u2t, r_u2t = u2t_r[t % 3]
        st, r_st = st_r[(2 * t) % 6]; mv, r_mv = mv_r[(2 * t) % 6]; rs, r_rs = rs_r[(2 * t) % 6]
        st2, r_st2 = st_r[(2 * t + 1) % 6]; mv2, r_mv2 = mv_r[(2 * t + 1) % 6]; rs2, r_rs2 = rs_r[(2 * t + 1) % 6]
        for hf in range(2):
            pr_t, r_pr = prs[(2 * t + hf) % 4]
            for n in range(8):
                S.op("pe", lambda e: e.matmul(pr_t[:], lhsT=mt_t[:, n, :], rhs=wout[:, n, hf * 512:(hf + 1) * 512], start=(n == 0), stop=(n == 7)),
                     reads=[r_mt, r_wout], writes=[r_pr] if n == 0 else (), awrites=() if n == 0 else [r_pr])
                yield
            S.op("dve", lambda e: e.tensor_tensor(out=r1[:, hf * 512:(hf + 1) * 512], in0=pr_t[:], in1=gate1[:, hf * 512:(hf + 1) * 512], op=ALU.mult),
                 reads=[r_pr, r_gate1], writes=[r_r1] if hf == 0 else (), awrites=() if hf == 0 else [r_r1])
            yield
        S.op("dve", lambda e: e.scalar_tensor_tensor(out=r1[:], in0=x_t[:], scalar=float(ALPHA), in1=r1[:], op0=ALU.mult, op1=ALU.add), reads=[r_x, r_r1], writes=[r_r1])
        yield
        ln_stats(r1, r_r1, st, r_st, mv, r_mv, rs, r_rs)
        yield
        S.op("dve", lambda e: e.tensor_scalar(out=x1t[:], in0=r1[:], scalar1=mv[:, 0:1], scalar2=rs[:], op0=ALU.subtract, op1=ALU.mult), reads=[r_r1, r_mv, r_rs], writes=[r_x1t])
        yield
        S.op("pool", lambda e: e.tensor_tensor(out=x1t[:], in0=x1t[:], in1=ln1g[:], op=ALU.mult), reads=[r_x1t, r_ln1g], writes=[r_x1t])
        yield
        S.op("pool", lambda e: e.tensor_tensor(out=x1t[:], in0=x1t[:], in1=ln1b[:], op=ALU.add), reads=[r_x1t, r_ln1b], writes=[r_x1t])
        yield
        S.dma("sp", x1_d[tok0:tok0 + 128, :], x1t[:], reads=[r_x1t], awrites=[r_x1])
        yield
        ln_stats(x1t, r_x1t, st2, r_st2, mv2, r_mv2, rs2, r_rs2)
        yield
        S.op("dve", lambda e: e.tensor_scalar(out=u2t[:], in0=x1t[:], scalar1=mv2[:, 0:1], scalar2=rs2[:], op0=ALU.subtract, op1=ALU.mult), reads=[r_x1t, r_mv2, r_rs2], writes=[r_u2t])
        yield
        S.op("pool", lambda e: e.tensor_tensor(out=u2t[:], in0=u2t[:], in1=sc2[:], op=ALU.mult), reads=[r_u2t, r_sc2], writes=[r_u2t])
        yield
        S.op("pool", lambda e: e.tensor_tensor(out=u2t[:], in0=u2t[:], in1=sh2[:], op=ALU.add), reads=[r_u2t, r_sh2], writes=[r_u2t])
        yield
        ub, r_ub = u2b[t % 3]
        S.op("act", lambda e: e.activation(out=ub[:], in_=u2t[:], func=AF.Copy), reads=[r_u2t], writes=[r_ub])
        yield
        S.dma("sp", u2_d[tok0:tok0 + 128, :], ub[:], reads=[r_ub], awrites=[r_u2])
        yield

    def stage2(t):
        u2t, r_u2t = u2t_r[t % 3]
        u2T, r_u2T = u2T_r[t % 3]
        lg, r_lg = lg_r[t % 3]; m8, r_m8 = m8_r[t % 3]; sml, r_sml = sml_r[t % 3]; ex, r_ex = ex_r[t % 3]; maskb, r_maskb = maskb_r[t % 3]
        for hf in range(2):
            pT, r_pT = (ptT[t % 2] if hf == 0 else psl[t % 2])
            for k4 in range(4):
                k = hf * 4 + k4
                S.op("pe", lambda e: e.transpose(out=pT[:, k4 * 128:(k4 + 1) * 128], in_=u2t[:, k * 128:(k + 1) * 128], identity=ident_f[:]),
                     reads=[r_u2t, r_identf], writes=[r_pT] if k4 == 0 else (), awrites=() if k4 == 0 else [r_pT])
                yield
            S.op("act", lambda e: e.activation(out=u2T[:, hf * 4:hf * 4 + 4, :].rearrange("p k t -> p (k t)"), in_=pT[:], func=AF.Copy), reads=[r_pT],
                 writes=[r_u2T] if hf == 0 else (), awrites=() if hf == 0 else [r_u2T])
            yield
        pl_, r_pl = psl[t % 2]
        for k in range(8):
            S.op("pe", lambda e: e.matmul(pl_[:, 0:NE], lhsT=u2T[:, k, :], rhs=wr[:, k, :], start=(k == 0), stop=(k == 7)),
                 reads=[r_u2T, r_wr], writes=[r_pl] if k == 0 else (), awrites=() if k == 0 else [r_pl])
            yield
        S.op("dve", lambda e: e.tensor_tensor(out=lg[:], in0=pl_[:, 0:NE], in1=br_bc[:], op=ALU.add), reads=[r_pl, r_br], writes=[r_lg])
        yield
        S.op("dve", lambda e: e.max(out=m8[:], in_=lg[:]), reads=[r_lg], writes=[r_m8])
        yield
        S.op("dve", lambda e: e.tensor_scalar(out=maskall[:, t, :], in0=lg[:], scalar1=m8[:, 3:4], scalar2=None, op0=ALU.is_ge), reads=[r_lg, r_m8], awrites=[r_maskall])
        yield
        S.op("dve", lambda e: e.tensor_scalar_mul(out=sml[:, 0:1], in0=m8[:, 0:1], scalar1=-1.0), reads=[r_m8], writes=[r_sml])
        yield
        S.op("act", lambda e: e.activation(out=ex[:], in_=lg[:], func=AF.Exp, bias=sml[:, 0:1]), reads=[r_lg, r_sml], writes=[r_ex])
        yield
        S.op("dve", lambda e: e.tensor_tensor(out=ex[:], in0=ex[:], in1=maskall[:, t, :], op=ALU.mult), reads=[r_ex, r_maskall], writes=[r_ex])
        yield
        S.op("dve", lambda e: e.reduce_sum(out=sml[:, 1:2], in_=ex[:], axis=AX.X), reads=[r_ex], awrites=[r_sml])
        yield
        S.op("dve", lambda e: e.reciprocal(out=sml[:, 2:3], in_=sml[:, 1:2]), reads=[r_sml], awrites=[r_sml])
        yield
        S.op("dve", lambda e: e.tensor_scalar(out=gall[:, t, :], in0=ex[:], scalar1=sml[:, 2:3], scalar2=None, op0=ALU.mult), reads=[r_ex, r_sml], awrites=[r_gall])
        yield
        S.op("dve", lambda e: e.tensor_copy(out=maskb[:], in_=maskall[:, t, :]), reads=[r_maskall], writes=[r_maskb])
        yield
        S.op("pe", lambda e: e.matmul(pl_[:, 64:64 + NE], lhsT=sutb[:], rhs=maskb[:], start=True, stop=True), reads=[r_sutb, r_maskb, r_lg], awrites=[r_pl])
        yield
        S.op("pe", lambda e: e.matmul(pl_[:, 128:128 + NE], lhsT=onesb[:], rhs=maskb[:], start=True, stop=True), reads=[r_onesb, r_maskb], awrites=[r_pl])
        yield
        S.op("dve", lambda e: e.tensor_tensor(out=posall[:, t, :], in0=pl_[:, 64:64 + NE], in1=rrun[:], op=ALU.add), reads=[r_pl, r_rrun], awrites=[r_posall])
        yield
        S.op("dve", lambda e: e.tensor_tensor(out=rrun[:], in0=pl_[:, 128:128 + NE], in1=rrun[:], op=ALU.add), reads=[r_pl, r_rrun], writes=[r_rrun])
        yield


    def tile_gen(t):
        if t + 2 < NT:
            loadD2(t + 2)
        yield from stage1(t)
        yield from stage2(t)

    for t0 in range(2):
        loadD2(t0)
    interleave((tile_gen(t) for t in range(NT)), 16)

    thr16, r_thr16 = C.sb("thr16", [128, NE, 16], F32)
    bstart, r_bstart = C.sb("bstart", [128, NBLK], F32)
    kp, r_kp = C.sb("kp", [128, 8], F32)
    pcol, r_pcol = C.sb("pcol", [128, 1], F32)
    sut32, r_sut32 = C.sb("sut32", [NE, NE], F32)
    S.dma("sp", thr16[:].rearrange("p e m -> p (e m)"), g_("thr16_d").partition_broadcast(128), reads=[r_in], writes=[r_thr16])
    S.dma("sp", bstart[:], g_("bstart_d").partition_broadcast(128), reads=[r_in], writes=[r_bstart])
    S.dma("sp", kp[:], g_("kp_d"), reads=[r_in], writes=[r_kp])
    S.dma("sp", pcol[:], g_("pcol_d"), reads=[r_in], writes=[r_pcol])
    S.dma("sp", sut32[:], g_("sut32_d"), reads=[r_in], writes=[r_sut32])
    big, r_big = C.sb("bigD", [128, NBLK * NE], F32)
    nbk, r_nbk = C.sb("nbk", [128, NE], F32)
    padT, r_padT = C.sb("padT", [NE, 128], F32)
    pstart, r_pstart = C.sb("pstart", [128, NE], F32)
    pend, r_pend = C.sb("pend", [128, NE], F32)
    bexp, r_bexp = C.sb("bexp", [128, NBLK], F32)
    wf, r_wf = C.sb("wf", [128, NBLK, 8], F32)
    S.op("dve", lambda e: e.tensor_tensor(out=big[:, 0:NE * 16].rearrange("p (e m) -> p e m", m=16), in0=rrun[:].unsqueeze(2).to_broadcast([128, NE, 16]), in1=thr16[:], op=ALU.is_gt),
         reads=[r_rrun, r_thr16], writes=[r_big])
    S.op("dve", lambda e: e.tensor_reduce(out=nbk[:], in_=big[:, 0:NE * 16].rearrange("p (e m) -> p e m", m=16), axis=AX.X, op=ALU.add), reads=[r_big], writes=[r_nbk])
    S.op("dve", lambda e: e.tensor_scalar_mul(out=nbk[:], in0=nbk[:], scalar1=512.0), reads=[r_nbk], writes=[r_nbk])
    pq, r_pq = psl[0]
    S.op("pe", lambda e: e.transpose(out=pq[0:NE, 0:128], in_=nbk[:], identity=ident_f[:]), reads=[r_nbk, r_identf], writes=[r_pq])
    S.op("act", lambda e: e.activation(out=padT[:], in_=pq[0:NE, 0:128], func=AF.Copy), reads=[r_pq], writes=[r_padT])
    S.op("pe", lambda e: e.matmul(pq[:, 256:256 + NE], lhsT=padT[:], rhs=sut32[:], start=True, stop=True), reads=[r_padT, r_sut32], awrites=[r_pq])
    S.op("act", lambda e: e.activation(out=pstart[:], in_=pq[:, 256:256 + NE], func=AF.Copy), reads=[r_pq], writes=[r_pstart])
    S.op("dve", lambda e: e.tensor_tensor(out=pend[:], in0=pstart[:], in1=nbk[:], op=ALU.add), reads=[r_pstart, r_nbk], writes=[r_pend])
    S.op("dve", lambda e: e.tensor_tensor(out=big[:].rearrange("p (i e) -> p i e", e=NE), in0=bstart[:].unsqueeze(2).to_broadcast([128, NBLK, NE]),
                                         in1=pend[:].unsqueeze(1).to_broadcast([128, NBLK, NE]), op=ALU.is_ge), reads=[r_bstart, r_pend], writes=[r_big])
    S.op("dve", lambda e: e.tensor_reduce(out=bexp[:], in_=big[:].rearrange("p (i e) -> p i e", e=NE), axis=AX.X, op=ALU.add), reads=[r_big], writes=[r_bexp])
    S.op("dve", lambda e: e.tensor_scalar_min(out=bexp[:], in0=bexp[:], scalar1=float(NE - 1)), reads=[r_bexp], writes=[r_bexp])
    S.op("dve", lambda e: e.tensor_copy(out=eidx[:], in_=bexp[:]), reads=[r_bexp], writes=[r_eidx])
    S.op("dve", lambda e: e.tensor_scalar(out=bidx[:], in0=bexp[:], scalar1=128.0, scalar2=pcol[:, 0:1], op0=ALU.mult, op1=ALU.add), reads=[r_bexp, r_pcol], writes=[r_bidx])
    S.op("dve", lambda e: e.tensor_scalar_mul(out=wf[:], in0=bexp[:].unsqueeze(2).to_broadcast([128, NBLK, 8]), scalar1=float(D)), reads=[r_bexp], writes=[r_wf])
    S.op("dve", lambda e: e.tensor_tensor(out=widx[:], in0=wf[:], in1=kp[:].unsqueeze(1).to_broadcast([128, NBLK, 8]), op=ALU.add), reads=[r_wf, r_kp], writes=[r_widx])
    sl_, r_sl = C.sb("slD", [128, NE], F32)
    eqt, r_eqt = C.sb("eqt", [128, NE], F32)
    m8b, r_m8b = C.sb("m8b", [128, 8], F32)
    S.op("dve", lambda e: e.tensor_scalar_add(out=pstart[:], in0=pstart[:], scalar1=1.0), reads=[r_pstart], writes=[r_pstart])
    for t0 in range(2):
        S.dma("sp", u2b[t0 % 3][0][:], u2_d[t0 * 128:t0 * 128 + 128, :], reads=[r_u2], writes=[u2b[t0 % 3][1]])
    for t in range(NT):
        tok0 = t * 128
        ub, r_ub = u2b[t % 3]
        if t + 2 < NT:
            S.dma("sp", u2b[(t + 2) % 3][0][:], u2_d[tok0 + 256:tok0 + 384, :], reads=[r_u2], writes=[u2b[(t + 2) % 3][1]])
        S.op("dve", lambda e: e.tensor_tensor(out=sl_[:], in0=posall[:, t, :], in1=pstart[:], op=ALU.add), reads=[r_posall, r_pstart], writes=[r_sl])
        S.op("dve", lambda e: e.tensor_tensor(out=sl_[:], in0=sl_[:], in1=maskall[:, t, :], op=ALU.mult), reads=[r_sl, r_maskall], writes=[r_sl])
        S.op("dve", lambda e: e.max(out=m8b[:], in_=sl_[:]), reads=[r_sl], writes=[r_m8b])
        for j in range(4):
            S.op("dve", lambda e: e.scalar_tensor_tensor(out=eqt[:], in0=sl_[:], scalar=m8b[:, j:j + 1], in1=gall[:, t, :], op0=ALU.is_equal, op1=ALU.mult, accum_out=g4[:, t, j:j + 1]),
                 reads=[r_sl, r_m8b, r_gall], writes=[r_eqt], awrites=[r_g4])
        S.op("dve", lambda e: e.tensor_scalar_add(out=idx4[:, t, :], in0=m8b[:, 0:4], scalar1=-1.0), reads=[r_m8b], awrites=[r_idx4])
        for j in range(4):
            S.op("pool", lambda e: e.indirect_dma_start(out=xs_d[:, :], out_offset=bass.IndirectOffsetOnAxis(ap=idx4[:, t, j:j + 1], axis=0), in_=ub[:], in_offset=None),
                 reads=[r_ub, r_idx4], awrites=[r_xsd], dma=True)
    C.pop()
    if "stopD" in dbg:
        C.pop()
        return

    C.push()
    w1b = C.ring("sb", "w1b", 2, [128, 8, 2 * D], BF16)
    w2b = C.ring("sb", "w2b", 2, [128, 8, D], BF16)
    b1t = C.ring("sb", "b1t", 2, [128, 16], F32)
    b2t = C.ring("sb", "b2t", 2, [2, D], F32)
    ones1f, r_ones1f = C.sb("ones1f", [1, 128], BF16)
    S.op("dve", lambda e: e.memset(ones1f[:], 1.0), writes=[r_ones1f])
    b2b = C.ring("sb", "b2b", 2, [1, D], BF16)
    xr = C.ring("sb", "xr", 8, [128, D], BF16)
    XT, r_XT = C.sb("XT", [128, 8, 512], BF16)
    hg = C.ring("sb", "hg", 2, [128, 512], F32)
    hu = C.ring("sb", "hu", 2, [128, 512], F32)
    sgm = C.ring("sb", "sgm", 2, [128, 512], F32)
    actT, r_actT = C.sb("actT", [128, 8, 512], BF16)
    ysb = C.ring("sb", "ysb", 2, [128, D], BF16)
    pxt = C.ring("ps", "pxt", 2, [128, 512], F32)
    pg = C.ring("ps", "pg", 2, [128, 512], F32)
    pu = C.ring("ps", "pu", 2, [128, 512], F32)
    py = C.ring("ps", "py", 2, [128, 512], F32)

    def load_weights(i, slot):
        w1t, r_w1 = w1b[slot]
        w2t, r_w2 = w2b[slot]
        for k in range(8):
            S.op("pool", lambda e: e.indirect_dma_start(out=w1t[:, k, :], out_offset=None, in_=w1b_d[:, :], in_offset=bass.IndirectOffsetOnAxis(ap=widx[:, i, k:k + 1], axis=0)),
                 reads=[r_w1bd, r_widx], writes=[r_w1] if k == 0 else (), awrites=() if k == 0 else [r_w1], dma=True)
        for k in range(8):
            S.op("pool", lambda e: e.indirect_dma_start(out=w2t[:, k, :], out_offset=None, in_=w2b_d[:, :], in_offset=bass.IndirectOffsetOnAxis(ap=widx[:, i, k:k + 1], axis=0)),
                 reads=[r_w2bd, r_widx], writes=[r_w2] if k == 0 else (), awrites=() if k == 0 else [r_w2], dma=True)
        S.op("pool", lambda e: e.indirect_dma_start(out=b1t[slot][0][:], out_offset=None, in_=b1r_d[:, :], in_offset=bass.IndirectOffsetOnAxis(ap=bidx[:, i:i + 1], axis=0)),
             reads=[r_in, r_bidx], writes=[b1t[slot][1]], dma=True)
        S.op("pool", lambda e: e.indirect_dma_start(out=b2t[slot][0][:], out_offset=None, in_=b2_d[:, :], in_offset=bass.IndirectOffsetOnAxis(ap=eidx[0:2, i:i + 1], axis=0)),
             reads=[r_in, r_eidx], writes=[b2t[slot][1]], dma=True)

    def load_x(i):
        for s4 in range(4):
            x_t, r_x = xr[(i % 2) * 4 + s4]
            row0 = i * 512 + s4 * 128
            S.dma("sp", x_t[:], xs_d[row0:row0 + 128, :], reads=[r_xsd], writes=[r_x])

    nblk_run = NBLK if "nblk" not in L else L["nblk"]
    load_weights(0, 0)
    load_x(0)
    xc = 0
    for i in range(nblk_run):
        slot = i % 2
        if i + 1 < nblk_run:
            load_weights(i + 1, (i + 1) % 2)
            load_x(i + 1)
        w1t, r_w1 = w1b[slot]
        w2t, r_w2 = w2b[slot]
        b1_t, r_b1 = b1t[slot]
        b2f_t, r_b2f = b2t[slot]
        b2_t, r_b2 = b2b[slot]
        S.op("pool", lambda e: e.tensor_copy(out=b2_t[:], in_=b2f_t[0:1, :]), reads=[r_b2f], writes=[r_b2])
        for s4 in range(4):
            x_t, r_x = xr[(i % 2) * 4 + s4]
            p_t, r_p = pxt[xc % 2]
            xc += 1
            p_b = p_t[:].bitcast(BF16)
            for k in range(8):
                S.op("pe", lambda e: e.transpose(out=p_b[:, k * 128:(k + 1) * 128], in_=x_t[:, k * 128:(k + 1) * 128], identity=ident_b[:]),
                     reads=[r_x, r_identb], writes=[r_p] if k == 0 else (), awrites=() if k == 0 else [r_p])
            S.op("act", lambda e: e.activation(out=XT[:, :, s4 * 128:(s4 + 1) * 128], in_=p_b.rearrange("p (k t) -> p k t", k=8), func=AF.Copy), reads=[r_p],
                 writes=[r_XT] if s4 == 0 else (), awrites=() if s4 == 0 else [r_XT])
        for fc in range(8):
            pg_t, r_pg = pg[fc % 2]
            pu_t, r_pu = pu[fc % 2]
            for k in range(8):
                S.op("pe", lambda e: e.matmul(pg_t[:], lhsT=w1t[:, k, fc * 128:(fc + 1) * 128], rhs=XT[:, k, :], start=(k == 0), stop=(k == 7)),
                     reads=[r_w1, r_XT], writes=[r_pg] if k == 0 else (), awrites=() if k == 0 else [r_pg])
            for k in range(8):
                S.op("pe", lambda e: e.matmul(pu_t[:], lhsT=w1t[:, k, D + fc * 128:D + (fc + 1) * 128], rhs=XT[:, k, :], start=(k == 0), stop=(k == 7)),
                     reads=[r_w1, r_XT], writes=[r_pu] if k == 0 else (), awrites=() if k == 0 else [r_pu])
            hg_t, r_hg = hg[fc % 2]
            hu_t, r_hu = hu[fc % 2]
            sg_t, r_sgm = sgm[fc % 2]
            S.op("act", lambda e: e.activation(out=hg_t[:], in_=pg_t[:], func=AF.Identity, bias=b1_t[:, fc:fc + 1]), reads=[r_pg, r_b1], writes=[r_hg])
            S.op("act", lambda e: e.activation(out=hu_t[:], in_=pu_t[:], func=AF.Identity, bias=b1_t[:, 8 + fc:9 + fc]), reads=[r_pu, r_b1], writes=[r_hu])
            S.op("dve", lambda e: e.tensor_scalar_min(out=hg_t[:], in0=hg_t[:], scalar1=7.0), reads=[r_hg], writes=[r_hg])
            S.op("act", lambda e: e.activation(out=sg_t[:], in_=hg_t[:], func=AF.Sigmoid, scale=1.702), reads=[r_hg], writes=[r_sgm])
            S.op("pool", lambda e: e.tensor_scalar(out=hu_t[:], in0=hu_t[:], scalar1=7.0, scalar2=-7.0, op0=ALU.min, op1=ALU.max), reads=[r_hu], writes=[r_hu])
            S.op("dve", lambda e: e.scalar_tensor_tensor(out=hu_t[:], in0=hu_t[:], scalar=1.0, in1=hg_t[:], op0=ALU.add, op1=ALU.mult), reads=[r_hu, r_hg], writes=[r_hu])
            S.op("dve", lambda e: e.tensor_tensor(out=actT[:, fc, :], in0=hu_t[:], in1=sg_t[:], op=ALU.mult), reads=[r_hu, r_sgm],
                 writes=[r_actT] if fc == 0 else (), awrites=() if fc == 0 else [r_actT])
        for s4 in range(4):
            y_t, r_y = ysb[s4 % 2]
            for hf in range(2):
                py_t, r_py = py[hf]
                for fc in range(8):
                    S.op("pe", lambda e: e.matmul(py_t[:], lhsT=actT[:, fc, s4 * 128:(s4 + 1) * 128], rhs=w2t[:, fc, hf * 512:(hf + 1) * 512], start=(fc == 0), stop=False),
                         reads=[r_actT, r_w2], writes=[r_py] if fc == 0 else (), awrites=() if fc == 0 else [r_py])
                S.op("pe", lambda e: e.matmul(py_t[:], lhsT=ones1f[:], rhs=b2_t[0:1, hf * 512:(hf + 1) * 512], start=False, stop=True), reads=[r_ones1f, r_b2], awrites=[r_py])
                S.op("act", lambda e: e.activation(out=y_t[:, hf * 512:(hf + 1) * 512], in_=py_t[:], func=AF.Copy), reads=[r_py],
                     writes=[r_y] if hf == 0 else (), awrites=() if hf == 0 else [r_y])
            row0 = i * 512 + s4 * 128
            S.dma("act", ys_d[row0:row0 + 128, :], y_t[:], reads=[r_y], awrites=[r_ysd])
    C.pop()

    C.push()
    ln2g, r_ln2g = C.sb("ln2g", [128, D], F32)
    ln2b, r_ln2b = C.sb("ln2b", [128, D], F32)
    S.dma("sp", ln2g[:], g_("ln2g_d").partition_broadcast(128), reads=[r_in], writes=[r_ln2g])
    S.dma("sp", ln2b[:], g_("ln2b_d").partition_broadcast(128), reads=[r_in], writes=[r_ln2b])
    gate2r = C.ring("sb", "gate2r", 2, [128, D], F32)
    x1r = C.ring("sb", "x1r", 4, [128, D], F32)
    yg = C.ring("sb", "yg", 16, [128, D], BF16)
    accr = C.ring("sb", "accF", 3, [128, D], F32)
    ot = C.ring("sb", "otF", 3, [128, D], F32)
    stF = C.ring("sb", "stF", 3, [128, 2, 6], F32)
    mvF = C.ring("sb", "mvF", 3, [128, 4], F32)
    rsF = C.ring("sb", "rsF", 3, [128, 1], F32)

    def loadF(t):
        tok0 = t * 128
        slot = t % 4
        S.dma("sp", x1r[slot][0][:], x1_d[tok0:tok0 + 128, :], reads=[r_x1], writes=[x1r[slot][1]])
        for j in range(4):
            y_t, r_y = yg[slot * 4 + j]
            S.op("pool", lambda e: e.indirect_dma_start(out=y_t[:], out_offset=None, in_=ys_d[:, :], in_offset=bass.IndirectOffsetOnAxis(ap=idx4[:, t, j:j + 1], axis=0)),
                 reads=[r_ysd, r_idx4], writes=[r_y], dma=True)
        if t % TPS == 0:
            b_ = t // TPS
            S.dma("sp", gate2r[b_ % 2][0][:], mod_d[b_:b_ + 1, 5 * D:6 * D].partition_broadcast(128), reads=[r_mod], writes=[gate2r[b_ % 2][1]])

    def genF(t):
        if t + 3 < NT:
            loadF(t + 3)
        slot = t % 4
        tok0 = t * 128
        gate2, r_gate2 = gate2r[(t // TPS) % 2]
        x1_t, r_x1t = x1r[slot]
        acc, r_acc = accr[t % 3]
        st, r_st = stF[t % 3]; mv, r_mv = mvF[t % 3]; rs, r_rs = rsF[t % 3]
        o_t, r_o = ot[t % 3]
        for j in range(4):
            y_t, r_y = yg[slot * 4 + j]
            if j == 0:
                S.op("act", lambda e: e.activation(out=acc[:], in_=y_t[:], func=AF.Copy, scale=g4[:, t, 0:1]), reads=[r_y, r_g4], writes=[r_acc])
            else:
                S.op("dve", lambda e: e.scalar_tensor_tensor(out=acc[:], in0=y_t[:], scalar=g4[:, t, j:j + 1], in1=acc[:], op0=ALU.mult, op1=ALU.add), reads=[r_y, r_g4, r_acc], writes=[r_acc])
            yield
        S.op("pool", lambda e: e.tensor_tensor(out=acc[:], in0=acc[:], in1=gate2[:], op=ALU.mult), reads=[r_acc, r_gate2], writes=[r_acc])
        yield
        S.op("dve", lambda e: e.scalar_tensor_tensor(out=acc[:], in0=x1_t[:], scalar=float(ALPHA), in1=acc[:], op0=ALU.mult, op1=ALU.add), reads=[r_x1t, r_acc], writes=[r_acc])
        yield
        for j in range(2):
            S.op("dve", lambda e: e.bn_stats(out=st[:, j, :], in_=acc[:, j * 512:(j + 1) * 512]), reads=[r_acc], writes=[r_st] if j == 0 else (), awrites=() if j == 0 else [r_st])
            yield
        S.op("dve", lambda e: e.bn_aggr(out=mv[:, 0:2], in_=st[:].rearrange("p a b -> p (a b)")), reads=[r_st], writes=[r_mv])
        yield
        S.op("act", lambda e: e.activation(out=rs[:], in_=mv[:, 1:2], func=AF.Sqrt, bias=EPS), reads=[r_mv], writes=[r_rs])
        yield
        S.op("dve", lambda e: e.reciprocal(out=rs[:], in_=rs[:]), reads=[r_rs], writes=[r_rs])
        yield
        S.op("dve", lambda e: e.tensor_scalar(out=mv[:, 2:3], in0=mv[:, 0:1], scalar1=-1.0, scalar2=rs[:], op0=ALU.mult, op1=ALU.mult), reads=[r_mv, r_rs], awrites=[r_mv])
        yield
        S.op("act", lambda e: e.activation(out=o_t[:], in_=acc[:], func=AF.Identity, scale=rs[:], bias=mv[:, 2:3]), reads=[r_acc, r_mv, r_rs], writes=[r_o])
        yield
        S.op("dve", lambda e: e.tensor_tensor(out=o_t[:], in0=o_t[:], in1=ln2g[:], op=ALU.mult), reads=[r_o, r_ln2g], writes=[r_o])
        yield
        S.op("dve", lambda e: e.tensor_tensor(out=o_t[:], in0=o_t[:], in1=ln2b[:], op=ALU.add), reads=[r_o, r_ln2b], writes=[r_o])
        yield
        S.dma("sp", out_d[tok0:tok0 + 128, :], o_t[:], reads=[r_o], awrites=[r_out])
        yield

    for t0 in range(3):
        loadF(t0)
    interleave((genF(t) for t in range(NT)), 6)
    C.pop()
    C.pop()


def phase_C(nc, S, C, dbg, L):
    g_ = lambda n: L[n]
    r_in = g_("r_in"); ident_b = g_("ident_b"); r_identb = g_("r_identb"); ident_f = g_("ident_f"); r_identf = g_("r_identf")
    qT_d, r_qT = g_("qT_d"), g_("r_qT"); iqT_d, r_iqT = g_("iqT_d"), g_("r_iqT")
    kva_d, r_kva = g_("kva_d"), g_("r_kva"); kvT_d, r_kvT = g_("kvT_d"), g_("r_kvT"); ikT_d, r_ikT = g_("ikT_d"), g_("r_ikT")
    iw_d, r_iw = g_("iw_d"), g_("r_iw"); oaT_d, r_oaT = g_("oaT_d"), g_("r_oaT")
    C.push()
    w1_d, w2_d = g_("w1_d"), g_("w2_d")

    def cast_weights(i):
        e_ = i // 2
        if i % 2 == 0:
            S.dma("pool", g_("w1b_d")[e_ * D:(e_ + 1) * D, :], w1_d[e_ * D:(e_ + 1) * D, :], reads=[r_in], awrites=[g_("r_w1bd")])
        else:
            S.dma("pool", g_("w2b_d")[e_ * D:(e_ + 1) * D, :], w2_d[e_ * D:(e_ + 1) * D, :], reads=[r_in], awrites=[g_("r_w2bd")])
    tzf, r_tzf = C.sb("tzf", [128, 2, 8, 128], F32)
    tz, r_tz = C.sb("tzb", [128, 2, 8, 128], BF16)
    cfar, r_cfar = C.sb("cfar", [128, 8], F32)
    identN, r_identN = C.sb("identN", [128, 128], BF16)
    S.dma("sp", tzf[:], g_("tz_d"), reads=[r_in], writes=[r_tzf])
    S.dma("sp", cfar[:], g_("cfar_d").partition_broadcast(128), reads=[r_in], writes=[r_cfar])
    S.op("dve", lambda e: e.tensor_scalar_mul(out=identN[:], in0=ident_f[:], scalar1=-NEG), reads=[r_identf], writes=[r_identN])
    first = True
    for dl in range(2):
        for h in range(8):
            S.op("dve", lambda e: e.tensor_scalar(out=tz[:, dl, h, :], in0=tzf[:, dl, h, :], scalar1=cfar[:, h:h + 1], scalar2=None, op0=ALU.subtract),
                 reads=[r_tzf, r_cfar], writes=[r_tz] if first else (), awrites=() if first else [r_tz])
            first = False
    seqb = [(C.sb("kvT%d" % i, [128, SEQ], BF16), C.sb("ikT%d" % i, [128, SEQ], BF16), C.sb("kvaC%d" % i, [128, TPS, 136], BF16)) for i in range(2)]
    qt = C.ring("sb", "qt", 5, [128, 8, 128], BF16)
    iqt = C.ring("sb", "iqt", 4, [128, 4, 128], BF16)
    iwt = C.ring("sb", "iwC", 4, [128, 8], F32)
    scorer = C.ring("sb", "score", 3, [128, SEQ], F32)
    penr = C.ring("sb", "pen", 2, [128, SEQ], BF16)
    rl = C.ring("sb", "rl", 2, [128, 512], F32)
    m8, r_m8 = C.sb("m8", [128, 8], F32)
    expT = C.ring("sb", "expT", 2, [128, TPS * 128], BF16)
    posb = C.ring("sb", "posb", 2, [128, 8, 132], F32)
    rc, r_rc = C.sb("rcC", [128, 8], F32)
    oa, r_oa = C.sb("oa", [128, D], BF16)
    oaT = C.ring("sb", "oaT", 2, [128, 8, 128], BF16)
    pi = C.ring("ps", "pi", 2, [128, 512], F32)
    pl = C.ring("ps", "pl", 3, [128, 512], F32)
    po = C.ring("ps", "po", 2, [128, 512], F32)
    ptr = C.ring("ps", "ptrC", 1, [128, 512], F32)
    NTILE = NB * TPS

    def load_seq(b):
        (kvT, r_kvTs), (ikT, r_ikTs), (kva, r_kvas) = seqb[b % 2]
        s0 = b * SEQ
        S.dma("sp", kvT[:], kvT_d[:, s0:s0 + SEQ], reads=[r_kvT], writes=[r_kvTs])
        S.dma("sp", ikT[:], ikT_d[:, s0:s0 + SEQ], reads=[r_ikT], writes=[r_ikTs])
        S.dma("sp", kva[:], kva_d[s0:s0 + SEQ, :].rearrange("(k p) c -> p k c", p=128), reads=[r_kva], writes=[r_kvas])

    def load_tile(i):
        tok0 = i * 128
        S.dma("sp", qt[i % 5][0][:], qT_d[:, :, tok0:tok0 + 128].rearrange("h p t -> p h t"), reads=[r_qT], writes=[qt[i % 5][1]])
        S.dma("sp", iqt[i % 4][0][:], iqT_d[:, :, tok0:tok0 + 128].rearrange("h p t -> p h t"), reads=[r_iqT], writes=[iqt[i % 4][1]])
        S.dma("sp", iwt[i % 4][0][:], iw_d[tok0:tok0 + 128, :], reads=[r_iw], writes=[iwt[i % 4][1]])

    NIT = 24
    pow2, r_pow2 = C.sb("pow2", [128, NIT + 1], F32)
    stepsr = C.ring("sb", "steps", 3, [128, NIT + 1], F32)
    bisr = C.ring("sb", "bis", 3, [128, 8], F32)
    junkb, r_junkb = C.sb("junkb", [128, SEQ], BF16)
    junkd, r_junkd = C.sb("junkd", [128, SEQ], BF16)
    for j in range(NIT + 1):
        S.op("pool", lambda e: e.memset(pow2[:, j:j + 1], float(2.0 ** -(j + 1))), writes=[r_pow2] if j == 0 else (), awrites=() if j == 0 else [r_pow2])

    def prep_score_gen(i):
        b, t = divmod(i, TPS)
        (ikT, r_ikTs) = seqb[b % 2][1]
        iq_t, r_iq = iqt[i % 4]
        iw_t, r_iwt = iwt[i % 4]
        pen, r_pen = penr[i % 2]
        score, r_score = scorer[i % 3]
        steps, r_steps = stepsr[i % 3]
        bis, r_bis = bisr[i % 3]
        N = 128 * (t + 1)
        if t < 2:
            return
            yield
        for kg in range(0, N, 512):
            w = min(512, N - kg)
            for h in range(8):
                pr, hf = divmod(h, 2)
                p_t, r_p = pi[h % 2]
                S.op("pe", lambda e: e.matmul(p_t[:, 0:w], lhsT=iq_t[64 * hf:64 * hf + 64, pr, :], rhs=ikT[64 * hf:64 * hf + 64, kg:kg + w], start=True, stop=True),
                     reads=[r_iq, r_ikTs], writes=[r_p])
                r_t, r_r = rl[h % 2]
                if h == 0:
                    S.op("dve", lambda e: e.tensor_scalar(out=score[:, kg:kg + w], in0=p_t[:, 0:w], scalar1=0.0, scalar2=iw_t[:, 0:1], op0=ALU.max, op1=ALU.mult),
                         reads=[r_p, r_iwt], writes=[r_score] if kg == 0 else (), awrites=() if kg == 0 else [r_score])
                elif h % 2 == 1:
                    S.op("dve", lambda e: e.tensor_scalar(out=r_t[:, 0:w], in0=p_t[:, 0:w], scalar1=0.0, scalar2=iw_t[:, h:h + 1], op0=ALU.max, op1=ALU.mult),
                         reads=[r_p, r_iwt], writes=[r_r])
                    S.op("pool", lambda e: e.tensor_tensor(out=score[:, kg:kg + w], in0=score[:, kg:kg + w], in1=r_t[:, 0:w], op=ALU.add),
                         reads=[r_r, r_score], awrites=[r_score])
                else:
                    S.op("act", lambda e: e.activation(out=r_t[:, 0:w], in_=p_t[:, 0:w], func=AF.Relu), reads=[r_p], writes=[r_r])
                    S.op("dve", lambda e: e.scalar_tensor_tensor(out=score[:, kg:kg + w], in0=r_t[:, 0:w], scalar=iw_t[:, h:h + 1], in1=score[:, kg:kg + w], op0=ALU.mult, op1=ALU.add),
                         reads=[r_r, r_iwt, r_score], awrites=[r_score])
                yield
        S.op("dve", lambda e: e.tensor_reduce(out=bis[:, 0:1], in_=score[:, 0:N - 64], axis=AX.X, op=ALU.min), reads=[r_score], writes=[r_bis])
        S.op("dve", lambda e: e.memset(score[0:64, N - 64:N], -1e30), reads=[r_score], awrites=[r_score])
        S.op("dve", lambda e: e.max(out=m8[:], in_=score[:, 0:N]), reads=[r_score], writes=[r_m8])
        S.op("dve", lambda e: e.tensor_tensor(out=bis[:, 1:2], in0=m8[:, 0:1], in1=bis[:, 0:1], op=ALU.subtract), reads=[r_m8, r_bis], awrites=[r_bis])
        S.op("dve", lambda e: e.tensor_scalar(out=steps[:], in0=pow2[:], scalar1=bis[:, 1:2], scalar2=None, op0=ALU.mult), reads=[r_pow2, r_bis], writes=[r_steps])
        S.op("dve", lambda e: e.tensor_tensor(out=bis[:, 2:3], in0=bis[:, 0:1], in1=steps[:, 0:1], op=ALU.add), reads=[r_bis, r_steps], awrites=[r_bis])

    def prep_score(i):
        for _ in prep_score_gen(i):
            pass

    def prep_iter(i, j):
        b, t = divmod(i, TPS)
        if t < 2:
            return
        N = 128 * (t + 1)
        score, r_score = scorer[i % 3]
        steps, r_steps = stepsr[i % 3]
        bis, r_bis = bisr[i % 3]
        if j % 3 != 2:
            S.op("act", lambda e: e.activation(out=junkb[:, 0:N], in_=score[:, 0:N], func=AF.Sign, scale=-1.0, bias=bis[:, 2:3], accum_out=bis[:, 4:5]),
                 reads=[r_score, r_bis], writes=[r_junkb], awrites=[r_bis])
            S.op("dve", lambda e: e.tensor_scalar(out=bis[:, 3:4], in0=bis[:, 4:5], scalar1=float(N - 511), scalar2=steps[:, j:j + 1], op0=ALU.is_le, op1=ALU.mult),
                 reads=[r_bis, r_steps], awrites=[r_bis])
        else:
            S.op("dve", lambda e: e.tensor_scalar(out=junkd[:, 0:N], in0=score[:, 0:N], scalar1=bis[:, 2:3], scalar2=None, op0=ALU.is_ge, op1=ALU.add, accum_out=bis[:, 6:7]),
                 reads=[r_score, r_bis], writes=[r_junkd], awrites=[r_bis])
            S.op("dve", lambda e: e.tensor_scalar(out=bis[:, 3:4], in0=bis[:, 6:7], scalar1=255.5, scalar2=steps[:, j:j + 1], op0=ALU.is_ge, op1=ALU.mult),
                 reads=[r_bis, r_steps], awrites=[r_bis])
        S.op("dve", lambda e: e.scalar_tensor_tensor(out=bis[:, 2:3], in0=bis[:, 3:4], scalar=steps[:, j + 1:j + 2], in1=bis[:, 2:3], op0=ALU.subtract, op1=ALU.add),
             reads=[r_bis, r_steps], awrites=[r_bis])

    def prep_fin(i):
        b, t = divmod(i, TPS)
        N = 128 * (t + 1)
        pen, r_pen = penr[i % 2]
        if t < 2:
            S.op("dve", lambda e: e.memset(pen[:, 0:N], 0.0), writes=[r_pen])
            S.op("dve", lambda e: e.memset(pen[0:64, N - 64:N], -1.0), awrites=[r_pen])
            return
        score, r_score = scorer[i % 3]
        steps, r_steps = stepsr[i % 3]
        bis, r_bis = bisr[i % 3]
        S.op("dve", lambda e: e.tensor_tensor(out=bis[:, 5:6], in0=bis[:, 2:3], in1=steps[:, NIT:NIT + 1], op=ALU.subtract), reads=[r_bis, r_steps], awrites=[r_bis])
        S.op("dve", lambda e: e.tensor_scalar(out=pen[:, 0:N], in0=score[:, 0:N], scalar1=bis[:, 5:6], scalar2=1.0, op0=ALU.is_ge, op1=ALU.subtract),
             reads=[r_score, r_bis], writes=[r_pen])

    state = {"plc": 0, "hc": 0}

    def attend_head(i, h):
        b, t = divmod(i, TPS)
        (kvT, r_kvTs), _, (kva, r_kvas) = seqb[b % 2]
        q_t, r_q = qt[i % 5]
        pen, r_pen = penr[i % 2]
        ps_t, r_ps = posb[i % 2]
        nkb = t + 1
        if True:
            e_t, r_e = expT[state["hc"] % 2]
            state["hc"] += 1
            for kg in range(0, nkb, 4):
                nb_ = min(4, nkb - kg)
                p_t, r_p = pl[state["plc"] % 3]
                state["plc"] += 1
                for ii in range(nb_):
                    kb = kg + ii
                    near = kb >= t - 1
                    cs_ = slice(ii * 128, (ii + 1) * 128)
                    S.op("pe", lambda e: e.matmul(p_t[:, cs_], lhsT=kvT[:, kb * 128:(kb + 1) * 128], rhs=q_t[:, h, :], start=True, stop=False),
                         reads=[r_kvTs, r_q], writes=[r_p] if ii == 0 else (), awrites=() if ii == 0 else [r_p])
                    S.op("pe", lambda e: e.matmul(p_t[:, cs_], lhsT=pen[:, kb * 128:(kb + 1) * 128], rhs=identN[:], start=False, stop=(not near)),
                         reads=[r_pen, r_identN], awrites=[r_p])
                    if near:
                        S.op("pe", lambda e: e.matmul(p_t[:, cs_], lhsT=ident_b[:], rhs=tz[:, t - kb, h, :], start=False, stop=True),
                             reads=[r_identb, r_tz], awrites=[r_p])
                S.op("act", lambda e: e.activation(out=e_t[:, kg * 128:(kg + nb_) * 128], in_=p_t[:, 0:nb_ * 128], func=AF.Exp), reads=[r_p],
                     writes=[r_e] if kg == 0 else (), awrites=() if kg == 0 else [r_e])
            o_t, r_o = po[h % 2]
            for kb in range(nkb):
                S.op("pe", lambda e: e.matmul(o_t[:, 0:129], lhsT=e_t[:, kb * 128:(kb + 1) * 128], rhs=kva[:, kb, 0:129], start=(kb == 0), stop=(kb == nkb - 1)),
                     reads=[r_e, r_kvas], writes=[r_o] if kb == 0 else (), awrites=() if kb == 0 else [r_o])
            S.op("act", lambda e: e.activation(out=ps_t[:, h, 0:129], in_=o_t[:, 0:129], func=AF.Copy), reads=[r_o],
                 writes=[r_ps] if h == 0 else (), awrites=() if h == 0 else [r_ps])

    def finalize(i):
        tok0 = i * 128
        ps_t, r_ps = posb[i % 2]
        S.op("dve", lambda e: e.reciprocal(out=rc[:], in_=ps_t[:, :, 128]), reads=[r_ps], writes=[r_rc])
        S.op("dve", lambda e: e.tensor_tensor(out=oa[:].rearrange("p (h c) -> p h c", h=8), in0=ps_t[:, :, 0:128], in1=rc[:].unsqueeze(2).to_broadcast([128, 8, 128]), op=ALU.mult),
             reads=[r_ps, r_rc], writes=[r_oa])
        pT, r_pT = ptr[0]
        pT_b = pT[:].bitcast(BF16)
        for k in range(8):
            S.op("pe", lambda e: e.transpose(out=pT_b[:, k * 128:(k + 1) * 128], in_=oa[:, k * 128:(k + 1) * 128], identity=ident_b[:]),
                 reads=[r_oa, r_identb], writes=[r_pT] if k == 0 else (), awrites=() if k == 0 else [r_pT])
        oT, r_oT = oaT[i % 2]
        S.op("act", lambda e: e.activation(out=oT[:].rearrange("p k t -> p (k t)"), in_=pT_b, func=AF.Copy), reads=[r_pT], writes=[r_oT])
        S.dma("sp", oaT_d[:, :, tok0:tok0 + 128].rearrange("c p t -> p c t"), oT[:], reads=[r_oT], awrites=[r_oaT])

    HALF = NIT // 2
    load_seq(0)
    for i0 in range(4):
        load_tile(i0)
    prep_score(0)
    for j in range(NIT):
        prep_iter(0, j)
    prep_fin(0)
    prep_score(1)
    for j in range(HALF):
        prep_iter(1, j)
    prep_score(2)
    for i in range(NTILE):
        b, t = divmod(i, TPS)
        if t == 0 and b + 1 < NB:
            load_seq(b + 1)
        if i + 4 < NTILE:
            load_tile(i + 4)
        cast_weights(i)
        n1 = i + 1 < NTILE
        n2 = i + 2 < NTILE
        gen = prep_score_gen(i + 3) if i + 3 < NTILE else iter(())
        npieces = 8 * ((128 * (((i + 3) % TPS) + 1) + 511) // 512) if (i + 3 < NTILE and (i + 3) % TPS >= 2) else 0
        ppb = (npieces + 7) // 8
        sched = []
        for k in range(HALF):
            if n1:
                sched.append((i + 1, HALF + k))
            if n2:
                sched.append((i + 2, k))
        per = (len(sched) + 7) // 8
        for h in range(8):
            attend_head(i, h)
            its = sched[h * per:(h + 1) * per]
            for n_, (ti_, j) in enumerate(its):
                prep_iter(ti_, j)
                if n_ < ppb:
                    next(gen, None)
            for _ in range(max(0, ppb - len(its))):
                next(gen, None)
        for _ in gen:
            pass
        if n1:
            prep_fin(i + 1)
        if i >= 1:
            finalize(i - 1)
    finalize(NTILE - 1)
    C.pop()


def phase_B(nc, S, C, dbg, L):
    g_ = lambda n: L[n]
    r_in = g_("r_in"); ident_b = g_("ident_b"); r_identb = g_("r_identb")
    xbcT_d, r_xbcT = g_("xbcT_d"), g_("r_xbcT"); dt_d, r_dt = g_("dt_d"), g_("r_dt"); zs_d, r_zs = g_("zs_d"), g_("r_zs")
    obT_d, r_obT = g_("obT_d"), g_("r_obT")
    C.push()
    convw, r_convw = C.sb("convw", [128, 16, 4], F32)
    convb, r_convb = C.sb("convb", [128, 16], F32)
    dg, r_dg = C.sb("dg", [128, 16, 4, 128], BF16)
    identf2, r_identf2 = C.sb("identf2", [128, 128], F32)
    a_bc, r_abc = C.sb("a_bc", [128, 16], F32)
    dskip_bc, r_dskip = C.sb("dskip_bc", [128, 16], F32)
    normw_bc, r_normw = C.sb("normw_bc", [128, D], F32)
    triU, r_triU = C.sb("triU", [128, 128], F32)
    SLm, r_SL = C.sb("SLm", [128, 128], F32)
    onesf, r_onesf = C.sb("onesf", [128, 128], F32)
    negm4, r_negm4 = C.sb("negm4", [128, 512], BF16)
    S.dma("sp", convw[:], g_("convw_d"), reads=[r_in], writes=[r_convw])
    S.dma("sp", convb[:], g_("convb_d"), reads=[r_in], writes=[r_convb])
    S.dma("sp", identf2[:], g_("ident_d"), reads=[r_in], writes=[r_identf2])
    S.dma("sp", a_bc[:], g_("alog_d").partition_broadcast(128), reads=[r_in], writes=[r_abc])
    S.dma("sp", dskip_bc[:], g_("dskip_d").partition_broadcast(128), reads=[r_in], writes=[r_dskip])
    S.dma("sp", normw_bc[:], g_("normw_d").partition_broadcast(128), reads=[r_in], writes=[r_normw])
    S.dma("sp", triU[:], g_("triU_d"), reads=[r_in], writes=[r_triU])
    S.dma("sp", SLm[:], g_("SL_d"), reads=[r_in], writes=[r_SL])
    S.dma("pool", negm4[:], g_("negm4_d"), reads=[r_in], writes=[r_negm4])
    S.op("dve", lambda e: e.memset(onesf[:], 1.0), writes=[r_onesf])
    S.op("act", lambda e: e.activation(out=a_bc[:], in_=a_bc[:], func=AF.Exp), reads=[r_abc], writes=[r_abc])
    S.op("dve", lambda e: e.tensor_scalar_mul(out=a_bc[:], in0=a_bc[:], scalar1=-1.0), reads=[r_abc], writes=[r_abc])
    first = True
    for j in range(16):
        for k in range(4):
            S.op("dve", lambda e: e.tensor_scalar_mul(out=dg[:, j, k, :], in0=identf2[:], scalar1=convw[:, j, k:k + 1]),
                 reads=[r_identf2, r_convw], writes=[r_dg] if first else (), awrites=() if first else [r_dg])
            first = False

    bank = C.ring("ps", "bk", 8, [128, 512], F32)
    xh = C.ring("sb", "xh", 4, [128, 16, 131], BF16)
    dtl = C.ring("sb", "dtl", 4, [128, 16], F32)
    zl = C.ring("sb", "zl", 4, [128, D], BF16)
    xact_r = C.ring("sb", "xact", 2, [128, 16, 128], BF16)
    xs_r = C.ring("sb", "xs_tok", 2, [128, D], BF16)
    Bt_r = C.ring("sb", "B_tok", 2, [128, 512], BF16)
    adt_r = C.ring("sb", "adt", 2, [128, 16], F32)
    sm_r = C.ring("sb", "smB", 2, [128, 8, 16], F32)
    A_r = C.ring("sb", "Amat", 2, [128, 16, 128], F32)
    Lt_r = C.ring("sb", "Lt", 2, [128, 2, 512], F32)
    Mt_r = C.ring("sb", "Mt", 2, [128, 16, 128], BF16)
    xdt_r = C.ring("sb", "xdt", 2, [128, D], BF16)
    xdd_r = C.ring("sb", "xdd", 2, [128, D], BF16)
    prev_f, r_pf = C.sb("prev_f", [128, D], F32)
    prev_b, r_pb = C.sb("prev_b", [128, D], BF16)
    t1_r = C.ring("sb", "t1", 2, [128, D], F32)
    t2_r = C.ring("sb", "t2", 2, [128, D], F32)
    junk, r_junk = C.sb("junkB", [128, 256], F32)
    ob_r = C.ring("sb", "ob", 2, [128, D], BF16)
    obT = C.ring("sb", "obT", 2, [128, 8, 128], BF16)

    def bc3(ap2, n):
        return ap2.unsqueeze(2).to_broadcast([128, 16, n])

    def v3(t, n=64):
        return t.rearrange("p (h q) -> p h q", q=n)

    def load_chunk(b, t, slot):
        tok0 = b * SEQ + t * 128
        x_t, r_x = xh[slot]
        if t == 0:
            S.op("pool", lambda e: e.memset(x_t[:, :, 0:3], 0.0), writes=[r_x])
            S.dma("sp", x_t[:, :, 3:131], xbcT_d[:, :, tok0:tok0 + 128].rearrange("c p t -> p c t"), reads=[r_xbcT], awrites=[r_x])
        else:
            S.dma("sp", x_t[:, :, :], xbcT_d[:, :, tok0 - 3:tok0 + 128].rearrange("c p t -> p c t"), reads=[r_xbcT], writes=[r_x])
        S.dma("sp", dtl[slot][0][:], dt_d[tok0:tok0 + 128, :], reads=[r_dt], writes=[dtl[slot][1]])
        S.dma("sp", zl[slot][0][:], zs_d[tok0:tok0 + 128, :], reads=[r_zs], writes=[zl[slot][1]])

    nch = NB * TPS

    def chunk_gen(ci):
        b, t = divmod(ci, TPS)
        slot = ci % 4
        sl2 = ci % 2
        tok0 = b * SEQ + t * 128
        if ci + 2 < nch:
            load_chunk((ci + 2) // TPS, (ci + 2) % TPS, (ci + 2) % 4)
        x_t, r_x = xh[slot]
        d_t, r_d = dtl[slot]
        z_t, r_z = zl[slot]
        xact, r_xact = xact_r[sl2]; xs_tok, r_xs = xs_r[sl2]; B_tok, r_Bt = Bt_r[sl2]; adt, r_adt = adt_r[sl2]; sm, r_sm = sm_r[sl2]
        Amat, r_A = A_r[sl2]; Lt, r_Lt = Lt_r[sl2]; Mt, r_Mt = Mt_r[sl2]; xdt, r_xdt = xdt_r[sl2]; xdd, r_xdd = xdd_r[sl2]
        t1, r_t1 = t1_r[sl2]; t2, r_t2 = t2_r[sl2]; ob, r_ob = ob_r[sl2]
        for jg in range(4):
            pc, r_pc = bank[jg % 2]
            for jj in range(4):
                j = jg * 4 + jj
                for k in range(4):
                    S.op("pe", lambda e: e.matmul(pc[:, jj * 128:(jj + 1) * 128], lhsT=dg[:, j, k, :], rhs=x_t[:, j, k:k + 128], start=(k == 0), stop=(k == 3)),
                         reads=[r_dg, r_x], writes=[r_pc] if (jj == 0 and k == 0) else (), awrites=() if (jj == 0 and k == 0) else [r_pc])
                    yield
            for jj in range(4):
                j = jg * 4 + jj
                S.op("act", lambda e: e.activation(out=xact[:, j, :], in_=pc[:, jj * 128:(jj + 1) * 128], func=AF.Silu, bias=convb[:, j:j + 1]),
                     reads=[r_pc, r_convb], writes=[r_xact] if j == 0 else (), awrites=() if j == 0 else [r_xact])
                yield
        pxs, r_pxs = bank[2]
        pB, r_pB = bank[3]
        pxs_b = pxs[:].bitcast(BF16)
        pB_b = pB[:].bitcast(BF16)
        for k in range(8):
            S.op("pe", lambda e: e.transpose(out=pxs_b[:, k * 128:(k + 1) * 128], in_=xact[:, k, :], identity=ident_b[:]),
                 reads=[r_xact, r_identb], writes=[r_pxs] if k == 0 else (), awrites=() if k == 0 else [r_pxs])
            yield
        for k in range(4):
            S.op("pe", lambda e: e.transpose(out=pB_b[:, k * 128:(k + 1) * 128], in_=xact[:, 8 + k, :], identity=ident_b[:]),
                 reads=[r_xact, r_identb], writes=[r_pB] if k == 0 else (), awrites=() if k == 0 else [r_pB])
            yield
        S.op("act", lambda e: e.activation(out=xs_tok[:], in_=pxs_b, func=AF.Copy), reads=[r_pxs], writes=[r_xs])
        yield
        S.op("act", lambda e: e.activation(out=B_tok[:], in_=pB_b[:, 0:512], func=AF.Copy), reads=[r_pB], writes=[r_Bt])
        yield
        S.op("dve", lambda e: e.tensor_tensor(out=adt[:], in0=d_t[:], in1=a_bc[:], op=ALU.mult), reads=[r_d, r_abc], writes=[r_adt])
        yield
        S.op("pe", lambda e: e.matmul(pB[:, 256:272], lhsT=triU[:], rhs=adt[:], start=True, stop=True), reads=[r_triU, r_adt], awrites=[r_pB])
        yield
        S.op("pe", lambda e: e.matmul(pB[:, 272:288], lhsT=onesf[:], rhs=adt[:], start=True, stop=True), reads=[r_onesf, r_adt], awrites=[r_pB])
        yield
        S.op("act", lambda e: e.activation(out=sm[:, 0, :], in_=pB[:, 256:272], func=AF.Exp), reads=[r_pB], writes=[r_sm])
        yield
        S.op("act", lambda e: e.activation(out=sm[:, 1, :], in_=pB[:, 272:288], func=AF.Copy), reads=[r_pB], awrites=[r_sm])
        yield
        S.op("act", lambda e: e.activation(out=sm[:, 2, :], in_=pB[:, 272:288], func=AF.Exp), reads=[r_pB], awrites=[r_sm])
        yield
        S.op("dve", lambda e: e.tensor_tensor(out=sm[:, 3, :], in0=sm[:, 1, :], in1=pB[:, 256:272], op=ALU.subtract), reads=[r_sm, r_pB], awrites=[r_sm])
        yield
        S.op("act", lambda e: e.activation(out=sm[:, 4, :], in_=sm[:, 3, :], func=AF.Exp), reads=[r_sm], awrites=[r_sm])
        yield
        S.op("dve", lambda e: e.tensor_tensor(out=Amat[:], in0=triU[:].unsqueeze(1).to_broadcast([128, 16, 128]), in1=bc3(adt[:], 128), op=ALU.mult),
             reads=[r_triU, r_adt], writes=[r_A])
        yield
        pCB, r_pCB = bank[4]
        for g in range(4):
            S.op("pe", lambda e: e.matmul(pCB[:, g * 128:(g + 1) * 128], lhsT=xact[:, 8 + g, :], rhs=xact[:, 12 + g, :], start=True, stop=True),
                 reads=[r_xact], writes=[r_pCB] if g == 0 else (), awrites=() if g == 0 else [r_pCB])
            yield
        S.op("dve", lambda e: e.tensor_tensor(out=v3(xdt[:]), in0=v3(xs_tok[:]), in1=bc3(d_t[:], 64), op=ALU.mult), reads=[r_xs, r_d], writes=[r_xdt])
        yield
        S.op("pool", lambda e: e.tensor_tensor(out=v3(xdd[:]), in0=v3(xdt[:]), in1=bc3(sm[:, 4, :], 64), op=ALU.mult), reads=[r_xdt, r_sm], writes=[r_xdd])
        yield
        for g in range(4):
            pD, r_pD = bank[5 + g % 2]
            S.op("pe", lambda e: e.matmul(pD[:], lhsT=SLm[:], rhs=Amat[:, 4 * g:4 * g + 4, :], start=True, stop=False), reads=[r_SL, r_A], writes=[r_pD])
            yield
            S.op("pe", lambda e: e.matmul(pD[:], lhsT=ident_b[:], rhs=negm4[:], start=False, stop=True), reads=[r_identb, r_negm4], awrites=[r_pD])
            yield
            S.op("act", lambda e: e.activation(out=Lt[:, g % 2, :], in_=pD[:], func=AF.Exp), reads=[r_pD], writes=[r_Lt] if g % 2 == 0 else (), awrites=() if g % 2 == 0 else [r_Lt])
            yield
            S.op("dve", lambda e: e.tensor_tensor(out=Mt[:, 4 * g:4 * g + 4, :], in0=Lt[:, g % 2, :].rearrange("p (h l) -> p h l", h=4),
                                                 in1=pCB[:, g * 128:(g + 1) * 128].unsqueeze(1).to_broadcast([128, 4, 128]), op=ALU.mult),
                 reads=[r_Lt, r_pCB], writes=[r_Mt] if g == 0 else (), awrites=() if g == 0 else [r_Mt])
            yield
        if t == 0:
            S.op("pool", lambda e: e.memset(prev_f[:], 0.0), writes=[r_pf])
            yield
            S.op("pool", lambda e: e.memset(prev_b[:], 0.0), writes=[r_pb])
            yield
        for hh in range(2):
            pY, r_pY = bank[5]
            pO, r_pO = bank[6]
            pS, r_pS = bank[7]
            c0 = hh * 512
            for h8 in range(8):
                h = hh * 8 + h8
                S.op("pe", lambda e: e.matmul(pY[:, h8 * 64:(h8 + 1) * 64], lhsT=Mt[:, h, :], rhs=xdt[:, h * 64:(h + 1) * 64], start=True, stop=True),
                     reads=[r_Mt, r_xdt], writes=[r_pY] if h8 == 0 else (), awrites=() if h8 == 0 else [r_pY])
                yield
            for g2 in range(2):
                g = hh * 2 + g2
                S.op("pe", lambda e: e.matmul(pO[:, g2 * 256:(g2 + 1) * 256], lhsT=xact[:, 12 + g, :], rhs=prev_b[:, g * 256:(g + 1) * 256], start=True, stop=True),
                     reads=[r_xact, r_pb], writes=[r_pO] if g2 == 0 else (), awrites=() if g2 == 0 else [r_pO])
                yield
            for g2 in range(2):
                g = hh * 2 + g2
                S.op("pe", lambda e: e.matmul(pS[:, g2 * 256:(g2 + 1) * 256], lhsT=B_tok[:, g * 128:(g + 1) * 128], rhs=xdd[:, g * 256:(g + 1) * 256], start=True, stop=True),
                     reads=[r_Bt, r_xdd], writes=[r_pS] if g2 == 0 else (), awrites=() if g2 == 0 else [r_pS])
                yield
            hs = slice(hh * 8, hh * 8 + 8)

            def v8(ap):
                return ap.rearrange("p (h q) -> p h q", q=64)
            ex8 = sm[:, 0, hs].unsqueeze(2).to_broadcast([128, 8, 64])
            cd8 = sm[:, 2, hs].unsqueeze(2).to_broadcast([128, 8, 64])
            ds8 = dskip_bc[:, hs].unsqueeze(2).to_broadcast([128, 8, 64])
            S.op("dve", lambda e: e.tensor_tensor(out=v8(t1[:, c0:c0 + 512]), in0=v8(pO[:]), in1=ex8, op=ALU.mult), reads=[r_pO, r_sm], writes=[r_t1] if hh == 0 else (), awrites=() if hh == 0 else [r_t1])
            yield
            S.op("dve", lambda e: e.tensor_tensor(out=t1[:, c0:c0 + 512], in0=t1[:, c0:c0 + 512], in1=pY[:], op=ALU.add), reads=[r_t1, r_pY], awrites=[r_t1])
            yield
            S.op("pool", lambda e: e.tensor_tensor(out=v8(t2[:, c0:c0 + 512]), in0=v8(xs_tok[:, c0:c0 + 512]), in1=ds8, op=ALU.mult), reads=[r_xs, r_dskip], writes=[r_t2] if hh == 0 else (), awrites=() if hh == 0 else [r_t2])
            yield
            S.op("dve", lambda e: e.tensor_tensor(out=v8(prev_f[:, c0:c0 + 512]), in0=v8(prev_f[:, c0:c0 + 512]), in1=cd8, op=ALU.mult), reads=[r_pf, r_sm], awrites=[r_pf])
            yield
            S.op("dve", lambda e: e.tensor_tensor(out=prev_f[:, c0:c0 + 512], in0=prev_f[:, c0:c0 + 512], in1=pS[:], op=ALU.add), reads=[r_pf, r_pS], awrites=[r_pf])
            yield
            S.op("act", lambda e: e.activation(out=prev_b[:, c0:c0 + 512], in_=prev_f[:, c0:c0 + 512], func=AF.Copy), reads=[r_pf, r_pO], awrites=[r_pb])
            yield
        S.op("pool", lambda e: e.tensor_tensor(out=t1[:], in0=t1[:], in1=t2[:], op=ALU.add), reads=[r_t1, r_t2], writes=[r_t1])
        yield
        S.op("pool", lambda e: e.tensor_tensor(out=t1[:], in0=t1[:], in1=z_t[:], op=ALU.mult), reads=[r_t1, r_z], writes=[r_t1])
        yield
        for g in range(4):
            S.op("act", lambda e: e.activation(out=junk[:], in_=t1[:, g * 256:(g + 1) * 256], func=AF.Square, accum_out=sm[:, 5, g:g + 1]),
                 reads=[r_t1], writes=[r_junk], awrites=[r_sm])
            yield
        S.op("act", lambda e: e.activation(out=sm[:, 5, 4:8], in_=sm[:, 5, 0:4], func=AF.Sqrt, scale=1.0 / 256.0, bias=EPS), reads=[r_sm], awrites=[r_sm])
        yield
        S.op("dve", lambda e: e.reciprocal(out=sm[:, 5, 8:12], in_=sm[:, 5, 4:8]), reads=[r_sm], awrites=[r_sm])
        yield
        S.op("dve", lambda e: e.tensor_tensor(out=t1[:].rearrange("p (g q) -> p g q", g=4), in0=t1[:].rearrange("p (g q) -> p g q", g=4),
                                             in1=sm[:, 5, 8:12].unsqueeze(2).to_broadcast([128, 4, 256]), op=ALU.mult), reads=[r_t1, r_sm], writes=[r_t1])
        yield
        S.op("dve", lambda e: e.tensor_tensor(out=ob[:], in0=t1[:], in1=normw_bc[:], op=ALU.mult), reads=[r_t1, r_normw], writes=[r_ob])
        yield
        pT, r_pT = bank[4]
        pT_b = pT[:].bitcast(BF16)
        for k in range(8):
            S.op("pe", lambda e: e.transpose(out=pT_b[:, k * 128:(k + 1) * 128], in_=ob[:, k * 128:(k + 1) * 128], identity=ident_b[:]),
                 reads=[r_ob, r_identb], writes=[r_pT] if k == 0 else (), awrites=() if k == 0 else [r_pT])
            yield
        o_t, r_o = obT[sl2]
        S.op("act", lambda e: e.activation(out=o_t[:].rearrange("p k t -> p (k t)"), in_=pT_b, func=AF.Copy), reads=[r_pT], writes=[r_o])
        yield
        S.dma("sp", obT_d[:, :, tok0:tok0 + 128].rearrange("c p t -> p c t"), o_t[:], reads=[r_o], awrites=[r_obT])
        yield

    load_chunk(0, 0, 0)
    load_chunk(0, 1, 1)
    interleave((chunk_gen(ci) for ci in range(nch)), B_STAGGER)
    C.pop()


def _t5_bucket_np(rel):
    half, max_exact = 16, 8
    ret = (rel > 0).astype(np.int32) * half
    n = np.abs(rel)
    nf = np.maximum(n, 1).astype(np.float32)
    large = max_exact + (np.log(nf / np.float32(max_exact)) / np.float32(np.log(128.0 / 8.0)) * np.float32(half - max_exact)).astype(np.int32)
    large = np.minimum(large, half - 1)
    return ret + np.where(n < max_exact, n, large)


def _t5_blocks(rel_bias):
    k = np.arange(128)[:, None]
    q = np.arange(128)[None, :]
    out = np.zeros((128, 2, 8, 128), np.float32)
    for dl in range(2):
        bk = _t5_bucket_np((k - 128 * dl) - q)
        for h in range(8):
            out[:, dl, h, :] = rel_bias[bk, h]
    return out


def host_inputs(inputs, core):
    b0 = core * NB
    f = lambda a: np.ascontiguousarray(a, dtype=np.float32)
    c = inputs["c"][b0:b0 + NB]
    m = {
        "x": f(inputs["x"][b0:b0 + NB].reshape(NTOK, D)),
        "cT": f(c.reshape(NB, 8, 128).transpose(2, 1, 0)),
        "w_mod": f(inputs["w_mod"][0]),
        "b_mod": f(inputs["b_mod"][0].reshape(1, -1)),
        "w_in": f(inputs["w_in"][0]),
        "ident": np.eye(128, dtype=np.float32),
        "kv_norm_w": f(inputs["kv_norm_w"][0].reshape(1, -1)),
        "idx_k_norm_w": f(inputs["idx_k_norm_w"][0].reshape(1, -1)),
        "idx_k_norm_b": f(inputs["idx_k_norm_b"][0].reshape(1, -1)),
        "dt_bias": f(inputs["dt_bias"][0].reshape(1, -1)),
        "convw": f(inputs["conv_w"][0].reshape(4, 16, 128).transpose(2, 1, 0)),
        "convb": f(inputs["conv_b"][0].reshape(16, 128).T),
        "a_log": f(inputs["a_log"][0].reshape(1, -1)),
        "d_skip": f(inputs["d_skip"][0].reshape(1, -1)),
        "ssm_norm_w": f(inputs["ssm_norm_w"][0].reshape(1, -1)),
        "w_proj_a": f(inputs["w_proj_a"][0]), "w_proj_b": f(inputs["w_proj_b"][0]), "w_out": f(inputs["w_out"][0]),
        "ln1_g": f(inputs["ln1_g"][0].reshape(1, -1)), "ln1_b": f(inputs["ln1_b"][0].reshape(1, -1)),
        "ln2_g": f(inputs["ln2_g"][0].reshape(1, -1)), "ln2_b": f(inputs["ln2_b"][0].reshape(1, -1)),
        "w_router": f(inputs["w_router"][0]), "b_router": f(inputs["b_router"][0].reshape(1, -1)),
        "w1": f(inputs["w1"][0].reshape(NE * D, 2 * D)), "w2": f(inputs["w2"][0].reshape(NE * D, D)),
        "b1r": f(inputs["b1"][0].reshape(NE, 16, 128).transpose(0, 2, 1).reshape(NE * 128, 16)),
        "b2": f(inputs["b2"][0]),
        "sut": np.triu(np.ones((128, 128), np.float32), 1),
        "thr16": np.tile(512.0 * np.arange(16, dtype=np.float32), NE).reshape(1, -1),
        "bstart": (512.0 * np.arange(NBLK, dtype=np.float32)).reshape(1, -1),
        "kp": (np.arange(8, dtype=np.float32)[None, :] * 128 + np.arange(128, dtype=np.float32)[:, None]),
        "pcol": np.arange(128, dtype=np.float32).reshape(128, 1),
        "sut32": np.triu(np.ones((NE, NE), np.float32), 1),
        "tz": _t5_blocks(f(inputs["rel_bias"])),
        "cfar": f(inputs["rel_bias"][15:16, :]),
        "triU": np.triu(np.ones((128, 128), np.float32)),
        "SL": np.tril(np.ones((128, 128), np.float32), -1),
        "negm4": np.tile(np.tril(np.full((128, 128), NEG, np.float32), -1), (1, 4)),
    }
    return m


def kernel(**inputs):
    nc = build_program()
    in_maps = [host_inputs(inputs, c) for c in range(NCORES)]
    res = run_bass_kernel_spmd(nc, in_maps, core_ids=list(range(NCORES)))
    out = np.stack([np.asarray(r["out"]).reshape(NB, SEQ, D) for r in res.results], 0)
    return out.reshape(NCORES * NB, SEQ, D).astype(np.float32)
```

```python
import numpy as np
import concourse.bass as bass
import concourse.mybir as mybir
from concourse.bass_utils import run_bass_kernel_spmd

F32 = mybir.dt.float32
BF16 = mybir.dt.bfloat16
I32 = mybir.dt.int32
ALU = mybir.AluOpType
AF = mybir.ActivationFunctionType
AX = mybir.AxisListType

NCORES = 8
SEQ = 2048
D = 1024
NB = 4
NTOK = NB * SEQ
NT = NTOK // 128
TPS = SEQ // 128
DIN = 6872
C_Q, C_KV, C_IQ, C_IK, C_IW, C_Z, C_XBC, C_DT, C_GA, C_GB = 0, 1024, 1152, 1664, 1728, 1736, 2760, 4808, 4824, 5848
NE = 32
NBLK = NTOK * 4 // 512 + NE
ALPHA = 2.0 ** 0.25
EPS = 1e-5
NEG = -30000.0

B_STAGGER = 104
ENGS = ("pe", "act", "dve", "pool", "sp")


class Res:
    __slots__ = ("name", "writers", "readers", "dsem", "dcount", "dram")

    def __init__(self, name):
        self.name = name
        self.dram = False
        self.writers = {}
        self.readers = {}
        self.dsem = None
        self.dcount = 0


class Sched:
    def __init__(self, nc):
        self.nc = nc
        self.eng = {"pe": nc.tensor, "act": nc.scalar, "dve": nc.vector,
                    "pool": nc.gpsimd, "sp": nc.sync}
        self.sem = {e: nc.alloc_semaphore("prog_" + e) for e in ENGS}
        self.cnt = {e: 0 for e in ENGS}
        self.waited = {e: {} for e in ENGS}
        self.all_res = []
        self.nwaits = 0
        self.nops = 0
        self.sempool = []

    def retire(self, rs):
        for r in rs:
            if r.dsem is not None:
                self.sempool.append((r.dsem, r.dcount))
                r.dsem = None
            if r in self.all_res:
                self.all_res.remove(r)

    def res(self, name):
        r = Res(name)
        self.all_res.append(r)
        return r

    def _need(self, eng, tok, deps):
        sem, val = tok
        k = sem.num
        if self.waited[eng].get(k, 0) >= val:
            return
        if k not in deps or deps[k][1] < val:
            deps[k] = (sem, val)

    def op(self, eng, fn, reads=(), writes=(), awrites=(), dma=False):
        deps = {}
        mykey = None if dma else eng
        for r in reads:
            for k, tok in r.writers.items():
                if k == mykey and eng == "pe":
                    continue
                self._need(eng, tok, deps)
        for r in writes:
            for k, tok in list(r.writers.items()) + list(r.readers.items()):
                if k == mykey:
                    continue
                self._need(eng, tok, deps)
        for r in awrites:
            for k, tok in r.readers.items():
                if k == mykey:
                    continue
                self._need(eng, tok, deps)
        e = self.eng[eng]
        for k, (sem, val) in deps.items():
            e.wait_ge(sem, val)
            self.waited[eng][k] = val
            self.nwaits += 1
        ins = fn(e)
        self.nops += 1
        if dma:
            dst = (list(writes) + list(awrites))[0]
            if dst.dram:
                sb = [r for r in reads if not r.dram]
                if sb:
                    dst = sb[0]
            if dst.dsem is None:
                if self.sempool:
                    dst.dsem, dst.dcount = self.sempool.pop()
                else:
                    dst.dsem = self.nc.alloc_semaphore("d_" + dst.name)
            dst.dcount += 16
            ins.then_inc(dst.dsem, 16)
            tok = (dst.dsem, dst.dcount)
            key = "dma%d" % dst.dsem.num
        else:
            self.cnt[eng] += 1
            ins.then_inc(self.sem[eng], 1)
            tok = (self.sem[eng], self.cnt[eng])
            key = eng
        for r in reads:
            r.readers[key] = tok
        for r in writes:
            r.writers = {key: tok}
            r.readers = {}
        for r in awrites:
            r.writers[key] = tok
        return ins

    def dma(self, eng, out, in_, reads=(), writes=(), awrites=(), **kw):
        return self.op(eng, lambda e: e.dma_start(out=out, in_=in_, **kw),
                       reads=reads, writes=writes, awrites=awrites, dma=True)

    def barrier(self):
        toks = {}
        for e in ENGS:
            if self.cnt[e]:
                toks[self.sem[e].num] = (self.sem[e], self.cnt[e])
        for r in self.all_res:
            if r.dsem is not None and r.dcount:
                toks[r.dsem.num] = (r.dsem, r.dcount)
        for e in ENGS:
            for k, (sem, val) in toks.items():
                if self.waited[e].get(k, 0) >= val:
                    continue
                self.eng[e].wait_ge(sem, val)
                self.waited[e][k] = val
                self.nwaits += 1
        for r in self.all_res:
            r.writers = {}
            r.readers = {}


class Ctx:
    def __init__(self, nc, S):
        self.nc = nc
        self.S = S
        self.stack = []

    def push(self):
        self.stack.append([])

    def pop(self):
        self.S.barrier()
        gs = self.stack.pop()
        self.S.retire([r for (_, r) in gs])
        for g, _ in reversed(gs):
            g.__exit__(None, None, None)

    def sb(self, name, shape, dt):
        g = self.nc.sbuf_tensor("s_" + name, list(shape), dt)
        t = g.__enter__()
        r = self.S.res(name)
        self.stack[-1].append((g, r))
        return t, r

    def ps(self, name, shape, dt=F32):
        g = self.nc.psum_tensor("p_" + name, list(shape), dt)
        t = g.__enter__()
        r = self.S.res(name)
        self.stack[-1].append((g, r))
        return t, r

    def ring(self, kind, name, n, shape, dt):
        f = self.sb if kind == "sb" else self.ps
        return [f("%s%d" % (name, i), shape, dt) for i in range(n)]


def interleave(gens, stagger):
    active = []
    it = iter(gens)
    nxt = next(it, None)
    tick = 0
    while active or nxt is not None:
        if nxt is not None and tick % stagger == 0:
            active.append(nxt)
            nxt = next(it, None)
        for g in list(active):
            try:
                next(g)
            except StopIteration:
                active.remove(g)
        tick += 1


def build_program(debug=()):
    nc = bass.Bass("TRN2", target_bir_lowering=False)
    S = Sched(nc)
    C = Ctx(nc, S)
    dbg = set(debug)

    def din(name, shape, dt=F32):
        return nc.dram_tensor(name, list(shape), dt, kind="ExternalInput").ap()

    def scratch(name, shape, dt):
        kind = "ExternalOutput" if name in dbg else "Internal"
        r = S.res(name)
        r.dram = True
        return nc.dram_tensor(name, list(shape), dt, kind=kind).ap(), r

    x_d = din("x", [NTOK, D])
    cT_d = din("cT", [128, 8, NB])
    wmod_d = din("w_mod", [D, 6 * D])
    bmod_d = din("b_mod", [1, 6 * D])
    win_d = din("w_in", [D, DIN])
    ident_d = din("ident", [128, 128])
    kvw_d = din("kv_norm_w", [1, 128])
    ikw_d = din("idx_k_norm_w", [1, 64])
    ikb_d = din("idx_k_norm_b", [1, 64])
    dtb_d = din("dt_bias", [1, 16])
    convw_d = din("convw", [128, 16, 4])
    convb_d = din("convb", [128, 16])
    alog_d = din("a_log", [1, 16])
    dskip_d = din("d_skip", [1, 16])
    normw_d = din("ssm_norm_w", [1, D])
    triU_d = din("triU", [128, 128])
    SL_d = din("SL", [128, 128])
    negm4_d = din("negm4", [128, 512])
    tz_d = din("tz", [128, 2, 8, 128])
    cfar_d = din("cfar", [1, 8])
    wpa_d = din("w_proj_a", [D, D]); wpb_d = din("w_proj_b", [D, D]); wout_d = din("w_out", [D, D])
    ln1g_d = din("ln1_g", [1, D]); ln1b_d = din("ln1_b", [1, D]); ln2g_d = din("ln2_g", [1, D]); ln2b_d = din("ln2_b", [1, D])
    wr_d = din("w_router", [D, NE]); br_d = din("b_router", [1, NE])
    w1_d = din("w1", [NE * D, 2 * D]); w2_d = din("w2", [NE * D, D])
    b1r_d = din("b1r", [NE * 128, 16]); b2_d = din("b2", [NE, D])
    sut_d = din("sut", [128, 128]); thr16_d = din("thr16", [1, NE * 16]); bstart_d = din("bstart", [1, NBLK])
    kp_d = din("kp", [128, 8]); pcol_d = din("pcol", [128, 1]); sut32_d = din("sut32", [NE, NE])
    r_in = S.res("inputs")
    r_in.dram = True
    out_d = nc.dram_tensor("out", [NTOK, D], F32, kind="ExternalOutput").ap()
    r_out = S.res("out")
    r_out.dram = True

    mod_d, r_mod = scratch("mod_s", [NB, 6 * D], F32)
    qT_d, r_qT = scratch("qT_s", [8, 128, NTOK], BF16)
    iqT_d, r_iqT = scratch("iqT_s", [4, 128, NTOK], BF16)
    xbcT_d, r_xbcT = scratch("xbcT_s", [16, 128, NTOK], BF16)
    sgT_d, r_sgT = scratch("sgT_s", [16, 128, NTOK], BF16)
    kva_d, r_kva = scratch("kva_s", [NTOK, 136], BF16)
    kvT_d, r_kvT = scratch("kvT_s", [128, NTOK], BF16)
    ikT_d, r_ikT = scratch("ikT_s", [128, NTOK], BF16)
    iw_d, r_iw = scratch("iw_s", [NTOK, 8], F32)
    dt_d, r_dt = scratch("dt_s", [NTOK, 16], F32)
    zs_d, r_zs = scratch("zs_s", [NTOK, D], BF16)
    obT_d, r_obT = scratch("obT_s", [8, 128, NTOK], BF16)
    oaT_d, r_oaT = scratch("oaT_s", [8, 128, NTOK], BF16)
    mT_d, r_mTd = scratch("mT_s", [8, 128, NTOK], BF16)
    x1_d, r_x1 = scratch("x1_s", [NTOK, D], F32)
    u2_d, r_u2 = scratch("u2_s", [NTOK, D], BF16)
    xs_d, r_xsd = scratch("xsort_s", [NBLK * 512, D], BF16)
    ys_d, r_ysd = scratch("ysort_s", [NBLK * 512, D], BF16)

    w1b_d, r_w1bd = scratch("w1b_s", [NE * D, 2 * D], BF16)
    w2b_d, r_w2bd = scratch("w2b_s", [NE * D, D], BF16)

    C.push()
    ident_f, r_identf = C.sb("ident_f", [128, 128], F32)
    ident_b, r_identb = C.sb("ident_b", [128, 128], BF16)
    S.dma("sp", ident_f[:], ident_d, reads=[r_in], writes=[r_identf])
    S.op("dve", lambda e: e.tensor_copy(out=ident_b[:], in_=ident_f[:]), reads=[r_identf], writes=[r_identb])
    modT, r_modT = C.sb("modT", [128, 48, NB], F32)

    C.push()
    cT, r_cT = C.sb("cT", [128, 8, NB], F32)
    ones1, r_ones1 = C.sb("ones1", [1, NB], F32)
    bmod, r_bmod = C.sb("bmod", [1, 6 * D], F32)
    modrow, r_modrow = C.sb("modrow", [NB, 6 * D], F32)
    wm = C.ring("sb", "wm", 2, [128, 8, 512], F32)
    pmod = C.ring("ps", "pmod", 2, [NB, 512], F32)
    S.dma("sp", cT[:], cT_d, reads=[r_in], writes=[r_cT])
    S.dma("sp", bmod[:], bmod_d, reads=[r_in], writes=[r_bmod])
    S.op("act", lambda e: e.activation(out=cT[:], in_=cT[:], func=AF.Silu), reads=[r_cT], writes=[r_cT])
    S.op("dve", lambda e: e.memset(ones1[:], 1.0), writes=[r_ones1])
    for g in range(12):
        wt, r_wt = wm[g % 2]
        pt, r_pt = pmod[g % 2]
        S.dma("sp", wt[:], wmod_d[:, g * 512:(g + 1) * 512].rearrange("(k p) n -> p k n", p=128), reads=[r_in], writes=[r_wt])
        for k in range(8):
            S.op("pe", lambda e: e.matmul(pt[:], lhsT=cT[:, k, :], rhs=wt[:, k, :], start=(k == 0), stop=False),
                 reads=[r_cT, r_wt], writes=[r_pt] if k == 0 else (), awrites=() if k == 0 else [r_pt])
        S.op("pe", lambda e: e.matmul(pt[:], lhsT=ones1[:], rhs=bmod[:, g * 512:(g + 1) * 512], start=False, stop=True),
             reads=[r_ones1, r_bmod], awrites=[r_pt])
        S.op("act", lambda e: e.activation(out=modrow[:, g * 512:(g + 1) * 512], in_=pt[:], func=AF.Copy), reads=[r_pt], awrites=[r_modrow])
    S.dma("sp", mod_d, modrow[:], reads=[r_modrow], writes=[r_mod])
    pmt, r_pmt = pmod[0]
    pmt2, r_pmt2 = C.ps("pmodT", [128, 48 * NB], F32)
    for j in range(48):
        S.op("pe", lambda e: e.transpose(out=pmt2[:, j * NB:(j + 1) * NB], in_=modrow[:, j * 128:(j + 1) * 128], identity=ident_f[0:NB, 0:NB]),
             reads=[r_modrow, r_identf], writes=[r_pmt2] if j == 0 else (), awrites=() if j == 0 else [r_pmt2])
    S.op("act", lambda e: e.activation(out=modT[:].rearrange("p j b -> p (j b)"), in_=pmt2[:], func=AF.Copy), reads=[r_pmt2], writes=[r_modT])
    S.op("dve", lambda e: e.tensor_scalar_add(out=modT[:, 8:16, :], in0=modT[:, 8:16, :], scalar1=1.0), reads=[r_modT], awrites=[r_modT])
    S.op("dve", lambda e: e.tensor_scalar_add(out=modT[:, 32:40, :], in0=modT[:, 32:40, :], scalar1=1.0), reads=[r_modT], awrites=[r_modT])
    C.pop()
    if "stop0" in dbg:
        C.pop()
        return nc

    C.push()
    wI, r_wI = C.sb("wI", [128, 8, DIN], BF16)
    for i, (a, b_) in enumerate([(0, 1024), (1024, 1736), (1736, 2760), (2760, 3784), (3784, 4808), (4808, 5848), (5848, 6872)]):
        S.dma("pool", wI[:, :, a:b_], win_d[:, a:b_].rearrange("(k p) n -> p k n", p=128), reads=[r_in],
              writes=[r_wI] if i == 0 else (), awrites=() if i == 0 else [r_wI])
    kvw_bc, r_kvw = C.sb("kvw_bc", [128, 128], F32)
    ikw_bc, r_ikw = C.sb("ikw_bc", [128, 64], F32)
    ikb_bc, r_ikb = C.sb("ikb_bc", [128, 64], F32)
    dtb_bc, r_dtb = C.sb("dtb_bc", [128, 16], F32)
    S.dma("sp", kvw_bc[:], kvw_d.partition_broadcast(128), reads=[r_in], writes=[r_kvw])
    S.dma("sp", ikw_bc[:], ikw_d.partition_broadcast(128), reads=[r_in], writes=[r_ikw])
    S.dma("sp", ikb_bc[:], ikb_d.partition_broadcast(128), reads=[r_in], writes=[r_ikb])
    S.dma("sp", dtb_bc[:], dtb_d.partition_broadcast(128), reads=[r_in], writes=[r_dtb])

    xt = C.ring("sb", "xt", 2, [128, D], F32)
    xn = C.ring("sb", "xn", 2, [128, D], BF16)
    st = C.ring("sb", "st", 2, [128, 2, 6], F32)
    mv = C.ring("sb", "mv", 2, [128, 2], F32)
    rs = C.ring("sb", "rs", 2, [128, 1], F32)
    uT = C.ring("sb", "uT", 2, [128, 8, 512], BF16)
    ptr = C.ring("ps", "ptr", 1, [128, 8, 128], BF16)
    psm = C.ring("ps", "psm", 1, [128, 512], F32)
    pz = C.ring("ps", "pz", 2, [128, 512], F32)
    pf = C.ring("ps", "pf", 3, [128, 512], F32)
    pt2 = C.ring("ps", "pt2", 1, [128, 2, 128], BF16)
    kva = C.ring("sb", "kva", 2, [128, 136], BF16)
    ikn = C.ring("sb", "ikn", 2, [128, 128], BF16)
    sml = C.ring("sb", "sml", 2, [128, 64], F32)
    ikf = C.ring("sb", "ikf", 2, [128, 64], F32)
    iwt = C.ring("sb", "iwt", 2, [128, 8], F32)
    dtt = C.ring("sb", "dtt", 2, [128, 4, 16], F32)
    zst = C.ring("sb", "zst", 2, [128, D], BF16)
    tT = C.ring("sb", "tT", 2, [128, 2, 128], BF16)
    stg = C.ring("sb", "stg", 2, [128, 8, 512], BF16)
    for i in range(2):
        S.op("pool", lambda e: e.memset(kva[i][0][:, 128:136], 1.0), writes=[kva[i][1]])

    def ln_stats(src, r_src, i):
        st_t, r_st = st[i]
        mv_t, r_mv = mv[i]
        rs_t, r_rs = rs[i]
        for j in range(2):
            S.op("dve", lambda e: e.bn_stats(out=st_t[:, j, :], in_=src[:, j * 512:(j + 1) * 512]), reads=[r_src],
                 writes=[r_st] if j == 0 else (), awrites=() if j == 0 else [r_st])
        S.op("dve", lambda e: e.bn_aggr(out=mv_t[:], in_=st_t[:].rearrange("p a b -> p (a b)")), reads=[r_st], writes=[r_mv])
        S.op("act", lambda e: e.activation(out=rs_t[:], in_=mv_t[:, 1:2], func=AF.Sqrt, bias=EPS), reads=[r_mv], writes=[r_rs])
        S.op("dve", lambda e: e.reciprocal(out=rs_t[:], in_=rs_t[:]), reads=[r_rs], writes=[r_rs])
        return mv_t, r_mv, rs_t, r_rs

    tcount = 0
    for g in range(NTOK // 512):
        b = (g * 512) // SEQ
        u_t, r_u = uT[g % 2]
        for i4 in range(4):
            t = g * 4 + i4
            tok0 = t * 128
            ri = tcount % 2
            tcount += 1
            x_t, r_x = xt[ri]
            xn_t, r_xn = xn[ri]
            if t == 0:
                S.dma("sp", x_t[:], x_d[0:128, :], reads=[r_in], writes=[r_x])
            if t + 1 < NT:
                S.dma("sp", xt[(ri + 1) % 2][0][:], x_d[tok0 + 128:tok0 + 256, :], reads=[r_in], writes=[xt[(ri + 1) % 2][1]])
            mv_t, r_mv, rs_t, r_rs = ln_stats(x_t, r_x, ri)
            S.op("dve", lambda e: e.tensor_scalar(out=xn_t[:], in0=x_t[:], scalar1=mv_t[:, 0:1], scalar2=rs_t[:], op0=ALU.subtract, op1=ALU.mult),
                 reads=[r_x, r_mv, r_rs], writes=[r_xn])
            p_t, r_p = ptr[0]
            for k in range(8):
                S.op("pe", lambda e: e.transpose(out=p_t[:, k, :], in_=xn_t[:, k * 128:(k + 1) * 128], identity=ident_b[:]),
                     reads=[r_xn, r_identb], writes=[r_p] if k == 0 else (), awrites=() if k == 0 else [r_p])
            for k in range(8):
                S.op("act", lambda e: e.activation(out=u_t[:, k, i4 * 128:(i4 + 1) * 128], in_=p_t[:, k, :], func=AF.Identity,
                                                   scale=modT[:, 8 + k, b:b + 1], bias=modT[:, k, b:b + 1]),
                     reads=[r_p, r_modT], writes=[r_u] if (k == 0 and i4 == 0) else (), awrites=() if (k == 0 and i4 == 0) else [r_u])
            ps_t, r_ps = psm[0]
            for (c0, c1, o0) in [(C_KV, C_KV + 128, 0), (C_IK, C_IK + 72, 128), (C_DT, C_DT + 16, 200)]:
                for k in range(8):
                    S.op("pe", lambda e: e.matmul(ps_t[:, o0:o0 + (c1 - c0)], lhsT=u_t[:, k, i4 * 128:(i4 + 1) * 128], rhs=wI[:, k, c0:c1], start=(k == 0), stop=(k == 7)),
                         reads=[r_u, r_wI], writes=[r_ps] if (k == 0 and o0 == 0) else (), awrites=() if (k == 0 and o0 == 0) else [r_ps])
            zp = []
            for h in range(2):
                pz_t, r_pz = pz[h]
                zp.append((pz_t, r_pz))
                for k in range(8):
                    S.op("pe", lambda e: e.matmul(pz_t[:], lhsT=u_t[:, k, i4 * 128:(i4 + 1) * 128], rhs=wI[:, k, C_Z + h * 512:C_Z + (h + 1) * 512], start=(k == 0), stop=(k == 7)),
                         reads=[r_u, r_wI], writes=[r_pz] if k == 0 else (), awrites=() if k == 0 else [r_pz])
            sm_t, r_sm = sml[ri]
            kva_t, r_kva_t = kva[ri]
            ikf_t, r_ikf = ikf[ri]
            S.op("act", lambda e: e.activation(out=ikf_t[:, 0:64], in_=ps_t[:, 0:64], func=AF.Square, accum_out=sm_t[:, 0:1]), reads=[r_ps], writes=[r_ikf, r_sm])
            S.op("act", lambda e: e.activation(out=ikf_t[:, 0:64], in_=ps_t[:, 64:128], func=AF.Square, accum_out=sm_t[:, 1:2]), reads=[r_ps], writes=[r_ikf], awrites=[r_sm])
            S.op("dve", lambda e: e.tensor_tensor(out=sm_t[:, 0:1], in0=sm_t[:, 0:1], in1=sm_t[:, 1:2], op=ALU.add), reads=[r_sm], awrites=[r_sm])
            S.op("act", lambda e: e.activation(out=sm_t[:, 2:3], in_=sm_t[:, 0:1], func=AF.Sqrt, scale=1.0 / 128.0, bias=EPS), reads=[r_sm], awrites=[r_sm])
            S.op("dve", lambda e: e.reciprocal(out=sm_t[:, 3:4], in_=sm_t[:, 2:3]), reads=[r_sm], awrites=[r_sm])
            S.op("dve", lambda e: e.scalar_tensor_tensor(out=kva_t[:, 0:128], in0=ps_t[:, 0:128], scalar=sm_t[:, 3:4], in1=kvw_bc[:], op0=ALU.mult, op1=ALU.mult),
                 reads=[r_ps, r_sm, r_kvw], awrites=[r_kva_t])
            S.dma("sp", kva_d[tok0:tok0 + 128, :], kva_t[:], reads=[r_kva_t], awrites=[r_kva])
            ik_t, r_ik = ikn[ri]
            S.op("dve", lambda e: e.bn_stats(out=sm_t[:, 8:14], in_=ps_t[:, 128:192]), reads=[r_ps], awrites=[r_sm])
            S.op("dve", lambda e: e.bn_aggr(out=sm_t[:, 16:18], in_=sm_t[:, 8:14]), reads=[r_sm], awrites=[r_sm])
            S.op("act", lambda e: e.activation(out=sm_t[:, 18:19], in_=sm_t[:, 17:18], func=AF.Sqrt, bias=EPS), reads=[r_sm], awrites=[r_sm])
            S.op("dve", lambda e: e.reciprocal(out=sm_t[:, 19:20], in_=sm_t[:, 18:19]), reads=[r_sm], awrites=[r_sm])
            S.op("dve", lambda e: e.tensor_scalar(out=ikf_t[:], in0=ps_t[:, 128:192], scalar1=sm_t[:, 16:17], scalar2=sm_t[:, 19:20], op0=ALU.subtract, op1=ALU.mult),
                 reads=[r_ps, r_sm], writes=[r_ikf])
            S.op("dve", lambda e: e.tensor_tensor(out=ikf_t[:], in0=ikf_t[:], in1=ikw_bc[:], op=ALU.mult), reads=[r_ikf, r_ikw], writes=[r_ikf])
            S.op("dve", lambda e: e.tensor_tensor(out=ik_t[:, 0:64], in0=ikf_t[:], in1=ikb_bc[:], op=ALU.add), reads=[r_ikf, r_ikb], writes=[r_ik])
            S.op("dve", lambda e: e.tensor_copy(out=ik_t[:, 64:128], in_=ik_t[:, 0:64]), reads=[r_ik], awrites=[r_ik])
            iw_t, r_iwt = iwt[ri]
            S.op("act", lambda e: e.mul(out=iw_t[:], in_=ps_t[:, 192:200], mul=float(8 ** -0.5 * 64 ** -0.5)), reads=[r_ps], writes=[r_iwt])
            S.dma("sp", iw_d[tok0:tok0 + 128, :], iw_t[:], reads=[r_iwt], awrites=[r_iw])
            d_t, r_d = dtt[ri]
            S.op("dve", lambda e: e.tensor_tensor(out=d_t[:, 0, :], in0=ps_t[:, 200:216], in1=dtb_bc[:], op=ALU.add), reads=[r_ps, r_dtb], writes=[r_d])
            S.op("act", lambda e: e.activation(out=d_t[:, 1, :], in_=d_t[:, 0, :], func=AF.Abs), reads=[r_d], awrites=[r_d])
            S.op("act", lambda e: e.activation(out=d_t[:, 1, :], in_=d_t[:, 1, :], func=AF.Exp, scale=-1.0), reads=[r_d], awrites=[r_d])
            S.op("act", lambda e: e.activation(out=d_t[:, 1, :], in_=d_t[:, 1, :], func=AF.Ln, bias=1.0), reads=[r_d], awrites=[r_d])
            S.op("dve", lambda e: e.scalar_tensor_tensor(out=d_t[:, 2, :], in0=d_t[:, 0, :], scalar=0.0, in1=d_t[:, 1, :], op0=ALU.max, op1=ALU.add), reads=[r_d], awrites=[r_d])
            S.dma("sp", dt_d[tok0:tok0 + 128, :], d_t[:, 2, :], reads=[r_d], awrites=[r_dt])
            z_t, r_z = zst[ri]
            for h in range(2):
                S.op("act", lambda e: e.activation(out=z_t[:, h * 512:(h + 1) * 512], in_=zp[h][0][:], func=AF.Silu), reads=[zp[h][1]],
                     writes=[r_z] if h == 0 else (), awrites=() if h == 0 else [r_z])
            S.dma("sp", zs_d[tok0:tok0 + 128, :], z_t[:], reads=[r_z], awrites=[r_zs])
            p2, r_p2 = pt2[0]
            t_t, r_t = tT[ri]
            S.op("pe", lambda e: e.transpose(out=p2[:, 0, :], in_=kva_t[:, 0:128], identity=ident_b[:]), reads=[r_kva_t, r_identb], writes=[r_p2])
            S.op("pe", lambda e: e.transpose(out=p2[:, 1, :], in_=ik_t[:], identity=ident_b[:]), reads=[r_ik, r_identb], awrites=[r_p2])
            S.op("dve", lambda e: e.tensor_copy(out=t_t[:], in_=p2[:]), reads=[r_p2], writes=[r_t])
            S.dma("sp", kvT_d[:, tok0:tok0 + 128], t_t[:, 0, :], reads=[r_t], awrites=[r_kvT])
            S.dma("sp", ikT_d[:, tok0:tok0 + 128], t_t[:, 1, :], reads=[r_t], awrites=[r_ikT])
        g0 = g * 512
        fcount = 0
        for (c0, nch, dst, r_dst, ch0, func, scl) in [
                (C_Q, 8, qT_d, r_qT, 0, AF.Copy, float(128 ** -0.5)),
                (C_IQ, 4, iqT_d, r_iqT, 0, AF.Copy, 1.0),
                (C_XBC, 8, xbcT_d, r_xbcT, 0, AF.Copy, 1.0),
                (C_XBC + 1024, 8, xbcT_d, r_xbcT, 8, AF.Copy, 1.0),
                (C_GA, 8, sgT_d, r_sgT, 0, AF.Sigmoid, 1.0),
                (C_GB, 8, sgT_d, r_sgT, 8, AF.Sigmoid, 1.0)]:
            sg_t, r_sg = stg[fcount % 2]
            fcount += 1
            for j in range(nch):
                pf_t, r_pf = pf[j % 3]
                for k in range(8):
                    S.op("pe", lambda e: e.matmul(pf_t[:], lhsT=wI[:, k, c0 + j * 128:c0 + (j + 1) * 128], rhs=u_t[:, k, :], start=(k == 0), stop=(k == 7)),
                         reads=[r_u, r_wI], writes=[r_pf] if k == 0 else (), awrites=() if k == 0 else [r_pf])
                if func == AF.Copy and j % 2 == 1:
                    S.op("dve", lambda e: e.tensor_scalar_mul(out=sg_t[:, j, :], in0=pf_t[:], scalar1=scl), reads=[r_pf],
                         writes=[r_sg] if j == 0 else (), awrites=() if j == 0 else [r_sg])
                else:
                    S.op("act", lambda e: e.activation(out=sg_t[:, j, :], in_=pf_t[:], func=func, scale=scl), reads=[r_pf],
                         writes=[r_sg] if j == 0 else (), awrites=() if j == 0 else [r_sg])
            S.dma("sp", dst[ch0:ch0 + nch, :, g0:g0 + 512].rearrange("c p t -> p c t"), sg_t[:, 0:nch, :], reads=[r_sg], awrites=[r_dst])
    C.pop()
    if "stopA" in dbg:
        C.pop()
        return nc

    phase_B(nc, S, C, dbg, locals())
    if "stopB" in dbg:
        C.pop()
        return nc

    phase_C(nc, S, C, dbg, locals())
    if "stopC" in dbg:
        C.pop()
        return nc

    phase_DEF(nc, S, C, dbg, locals())
    C.pop()
    return nc


def phase_DEF(nc, S, C, dbg, L):
    g_ = lambda n: L[n]
    r_in = g_("r_in"); ident_b = g_("ident_b"); r_identb = g_("r_identb"); ident_f = g_("ident_f"); r_identf = g_("r_identf")
    modT = g_("modT"); r_modT = g_("r_modT"); mod_d = g_("mod_d"); r_mod = g_("r_mod")
    x_d = g_("x_d"); oaT_d, r_oaT = g_("oaT_d"), g_("r_oaT"); obT_d, r_obT = g_("obT_d"), g_("r_obT"); sgT_d, r_sgT = g_("sgT_d"), g_("r_sgT")
    x1_d, r_x1 = g_("x1_d"), g_("r_x1"); u2_d, r_u2 = g_("u2_d"), g_("r_u2"); xs_d, r_xsd = g_("xs_d"), g_("r_xsd"); ys_d, r_ysd = g_("ys_d"), g_("r_ysd")
    out_d, r_out = g_("out_d"), g_("r_out")
    b1r_d, b2_d = g_("b1r_d"), g_("b2_d")
    w1b_d, r_w1bd, w2b_d, r_w2bd = g_("w1b_d"), g_("r_w1bd"), g_("w2b_d"), g_("r_w2bd")

    C.push()
    idx4, r_idx4 = C.sb("idx4", [128, NT, 4], I32)
    g4, r_g4 = C.sb("g4", [128, NT, 4], F32)
    widx, r_widx = C.sb("widx", [128, NBLK, 8], I32)
    bidx, r_bidx = C.sb("bidx", [128, NBLK], I32)
    eidx, r_eidx = C.sb("eidx", [128, NBLK], I32)

    def ln_stats(src, r_src, st_t, r_st, mv_t, r_mv, rs_t, r_rs):
        for j in range(2):
            S.op("dve", lambda e: e.bn_stats(out=st_t[:, j, :], in_=src[:, j * 512:(j + 1) * 512]), reads=[r_src],
                 writes=[r_st] if j == 0 else (), awrites=() if j == 0 else [r_st])
        S.op("dve", lambda e: e.bn_aggr(out=mv_t[:], in_=st_t[:].rearrange("p a b -> p (a b)")), reads=[r_st], writes=[r_mv])
        S.op("act", lambda e: e.activation(out=rs_t[:], in_=mv_t[:, 1:2], func=AF.Sqrt, bias=EPS), reads=[r_mv], writes=[r_rs])
        S.op("dve", lambda e: e.reciprocal(out=rs_t[:], in_=rs_t[:]), reads=[r_rs], writes=[r_rs])

    mT_d, r_mTd = g_("mT_d"), g_("r_mTd")
    C.push()
    wpa, r_wpa = C.sb("wpa", [128, 8, D], BF16)
    wpb, r_wpb = C.sb("wpb", [128, 8, D], BF16)
    S.dma("pool", wpa[:], g_("wpa_d").rearrange("(k p) n -> p k n", p=128), reads=[r_in], writes=[r_wpa])
    S.dma("pool", wpb[:], g_("wpb_d").rearrange("(k p) n -> p k n", p=128), reads=[r_in], writes=[r_wpb])
    oaTr = C.ring("sb", "oaTd", 2, [128, 8, 512], BF16)
    obTr = C.ring("sb", "obTd", 2, [128, 8, 512], BF16)
    sgTr = C.ring("sb", "sgTd", 2, [128, 16, 512], BF16)
    mTr = C.ring("sb", "mT", 2, [128, 8, 512], BF16)
    ta = C.ring("sb", "ta", 3, [128, 512], F32)
    tb = C.ring("sb", "tb", 3, [128, 512], F32)
    pab = C.ring("ps", "pab", 4, [128, 512], F32)
    pbb = C.ring("ps", "pbb", 4, [128, 512], F32)

    def loadD1(g):
        g0 = g * 512
        S.dma("sp", oaTr[g % 2][0][:], oaT_d[:, :, g0:g0 + 512].rearrange("c p t -> p c t"), reads=[r_oaT], writes=[oaTr[g % 2][1]])
        S.dma("sp", obTr[g % 2][0][:], obT_d[:, :, g0:g0 + 512].rearrange("c p t -> p c t"), reads=[r_obT], writes=[obTr[g % 2][1]])
        S.dma("sp", sgTr[g % 2][0][:], sgT_d[:, :, g0:g0 + 512].rearrange("c p t -> p c t"), reads=[r_sgT], writes=[sgTr[g % 2][1]])

    NG = NTOK // 512
    loadD1(0)
    cn = 0
    for g in range(NG):
        g0 = g * 512
        if g + 1 < NG:
            loadD1(g + 1)
        oaT, r_oaTs = oaTr[g % 2]; obT, r_obTs = obTr[g % 2]; sgT, r_sgTs = sgTr[g % 2]; mT, r_mT = mTr[g % 2]
        for n in range(8):
            pa, r_pa = pab[cn % 4]
            pb, r_pb = pbb[cn % 4]
            ta_t, r_ta = ta[cn % 3]
            tb_t, r_tb = tb[cn % 3]
            cn += 1
            for k in range(8):
                S.op("pe", lambda e: e.matmul(pa[:], lhsT=wpa[:, k, n * 128:(n + 1) * 128], rhs=oaT[:, k, :], start=(k == 0), stop=(k == 7)),
                     reads=[r_wpa, r_oaTs], writes=[r_pa] if k == 0 else (), awrites=() if k == 0 else [r_pa])
            for k in range(8):
                S.op("pe", lambda e: e.matmul(pb[:], lhsT=wpb[:, k, n * 128:(n + 1) * 128], rhs=obT[:, k, :], start=(k == 0), stop=(k == 7)),
                     reads=[r_wpb, r_obTs], writes=[r_pb] if k == 0 else (), awrites=() if k == 0 else [r_pb])
            S.op("dve", lambda e: e.tensor_tensor(out=ta_t[:], in0=pa[:], in1=sgT[:, n, :], op=ALU.mult), reads=[r_pa, r_sgTs], writes=[r_ta])
            S.op("dve", lambda e: e.tensor_tensor(out=tb_t[:], in0=pb[:], in1=sgT[:, 8 + n, :], op=ALU.mult), reads=[r_pb, r_sgTs], writes=[r_tb])
            S.op("pool", lambda e: e.tensor_tensor(out=mT[:, n, :], in0=ta_t[:], in1=tb_t[:], op=ALU.add), reads=[r_ta, r_tb],
                 writes=[r_mT] if n == 0 else (), awrites=() if n == 0 else [r_mT])
        S.dma("sp", mT_d[:, :, g0:g0 + 512].rearrange("c p t -> p c t"), mT[:], reads=[r_mT], awrites=[r_mTd])
    C.pop()

    C.push()
    wout, r_wout = C.sb("wout", [128, 8, D], BF16)
    S.dma("pool", wout[:], g_("wout_d").rearrange("(k p) n -> p k n", p=128), reads=[r_in], writes=[r_wout])
    wr, r_wr = C.sb("wr", [128, 8, NE], F32)
    S.dma("sp", wr[:], g_("wr_d").rearrange("(k p) n -> p k n", p=128), reads=[r_in], writes=[r_wr])
    br_bc, r_br = C.sb("br_bc", [128, NE], F32)
    S.dma("sp", br_bc[:], g_("br_d").partition_broadcast(128), reads=[r_in], writes=[r_br])
    ln1g, r_ln1g = C.sb("ln1g", [128, D], F32)
    ln1b, r_ln1b = C.sb("ln1b", [128, D], F32)
    S.dma("sp", ln1g[:], g_("ln1g_d").partition_broadcast(128), reads=[r_in], writes=[r_ln1g])
    S.dma("sp", ln1b[:], g_("ln1b_d").partition_broadcast(128), reads=[r_in], writes=[r_ln1b])
    gater = C.ring("sb", "gate1", 2, [128, D], F32)
    sc2r = C.ring("sb", "sc2", 2, [128, D], F32)
    sh2r = C.ring("sb", "sh2", 2, [128, D], F32)
    sutf, r_sutf = C.sb("sutf", [128, 128], F32)
    sutb, r_sutb = C.sb("sutb", [128, 128], BF16)
    onesb, r_onesb = C.sb("onesb", [128, 128], BF16)
    S.dma("sp", sutf[:], g_("sut_d"), reads=[r_in], writes=[r_sutf])
    S.op("dve", lambda e: e.tensor_copy(out=sutb[:], in_=sutf[:]), reads=[r_sutf], writes=[r_sutb])
    S.op("dve", lambda e: e.memset(onesb[:], 1.0), writes=[r_onesb])
    maskall, r_maskall = C.sb("maskall", [128, NT, NE], F32)
    gall, r_gall = C.sb("gall", [128, NT, NE], F32)
    posall, r_posall = C.sb("posall", [128, NT, NE], F32)
    rrun, r_rrun = C.sb("rrun", [128, NE], F32)
    S.op("dve", lambda e: e.memset(rrun[:], 0.0), writes=[r_rrun])
    mtr = C.ring("sb", "mtl", 4, [128, 8, 128], BF16)
    xt = C.ring("sb", "xtd", 4, [128, D], F32)
    r1_r = C.ring("sb", "r1", 3, [128, D], F32)
    x1t_r = C.ring("sb", "x1t", 3, [128, D], F32)
    u2t_r = C.ring("sb", "u2t", 3, [128, D], F32)
    u2b = C.ring("sb", "u2b", 3, [128, D], BF16)
    u2T_r = C.ring("sb", "u2T", 3, [128, 8, 128], F32)
    st_r = C.ring("sb", "stD", 6, [128, 2, 6], F32)
    mv_r = C.ring("sb", "mvD", 6, [128, 2], F32)
    rs_r = C.ring("sb", "rsD", 6, [128, 1], F32)
    lg_r = C.ring("sb", "lg", 3, [128, NE], F32)
    m8_r = C.ring("sb", "m8D", 3, [128, 8], F32)
    sml_r = C.ring("sb", "smlD", 3, [128, 8], F32)
    ex_r = C.ring("sb", "exD", 3, [128, NE], F32)
    maskb_r = C.ring("sb", "maskb", 3, [128, NE], BF16)
    prs = C.ring("ps", "prs", 4, [128, 512], F32)
    ptT = C.ring("ps", "ptT", 2, [128, 512], F32)
    psl = C.ring("ps", "psl", 2, [128, 512], F32)

    def loadD2(t):
        tok0 = t * 128
        S.dma("sp", xt[t % 4][0][:], x_d[tok0:tok0 + 128, :], reads=[r_in], writes=[xt[t % 4][1]])
        S.dma("sp", mtr[t % 4][0][:], mT_d[:, :, tok0:tok0 + 128].rearrange("c p t -> p c t"), reads=[r_mTd], writes=[mtr[t % 4][1]])
        if t % TPS == 0:
            b = t // TPS
            gate1, r_gate1 = gater[b % 2]; sc2, r_sc2 = sc2r[b % 2]; sh2, r_sh2 = sh2r[b % 2]
            S.dma("sp", gate1[:], mod_d[b:b + 1, 2 * D:3 * D].partition_broadcast(128), reads=[r_mod], writes=[r_gate1])
            S.dma("sp", sh2[:], mod_d[b:b + 1, 3 * D:4 * D].partition_broadcast(128), reads=[r_mod], writes=[r_sh2])
            S.dma("sp", sc2[:], mod_d[b:b + 1, 4 * D:5 * D].partition_broadcast(128), reads=[r_mod], writes=[r_sc2])
            S.op("pool", lambda e: e.tensor_scalar_add(out=sc2[:], in0=sc2[:], scalar1=1.0), reads=[r_sc2], writes=[r_sc2])

    def stage1(t):
        tok0 = t * 128
        b = t // TPS
        gate1, r_gate1 = gater[b % 2]; sc2, r_sc2 = sc2r[b % 2]; sh2, r_sh2 = sh2r[b % 2]
        x_t, r_x = xt[t % 4]
        mt_t, r_mt = mtr[t % 4]
        r1, r_r1 = r1_r[t % 3]; x1t, r_x1t = x1t_r[t % 3]; u2t, r_u2t = u2t_r[t % 3]
        st, r_st = st_r[(2 * t) % 6]; mv, r_mv = mv_r[(2 * t) % 6]; rs, r_rs = rs_r[(2 * t) % 6]
        st2, r_st2 = st_r[(2 * t + 1) % 6]; mv2, r_mv2 = mv_r[(2 * t + 1) % 6]; rs2, r_rs2 = rs_r[(2 * t + 1) % 6]
        for hf in range(2):
            pr_t, r_pr = prs[(2 * t + hf) % 4]
            for n in range(8):
                S.op("pe", lambda e: e.matmul(pr_t[:], lhsT=mt_t[:, n, :], rhs=wout[:, n, hf * 512:(hf + 1) * 512], start=(n == 0), stop=(n == 7)),
                     reads=[r_mt, r_wout], writes=[r_pr] if n == 0 else (), awrites=() if n == 0 else [r_pr])
                yield
            S.op("dve", lambda e: e.tensor_tensor(out=r1[:, hf * 512:(hf + 1) * 512], in0=pr_t[:], in1=gate1[:, hf * 512:(hf + 1) * 512], op=ALU.mult),
                 reads=[r_pr, r_gate1], writes=[r_r1] if hf == 0 else (), awrites=() if hf == 0 else [r_r1])
            yield
        S.op("dve", lambda e: e.scalar_tensor_tensor(out=r1[:], in0=x_t[:], scalar=float(ALPHA), in1=r1[:], op0=ALU.mult, op1=ALU.add), reads=[r_x, r_r1], writes=[r_r1])
        yield
        ln_stats(r1, r_r1, st, r_st, mv, r_mv, rs, r_rs)
        yield
        S.op("dve", lambda e: e.tensor_scalar(out=x1t[:], in0=r1[:], scalar1=mv[:, 0:1], scalar2=rs[:], op0=ALU.subtract, op1=ALU.mult), reads=[r_r1, r_mv, r_rs], writes=[r_x1t])
        yield
        S.op("pool", lambda e: e.tensor_tensor(out=x1t[:], in0=x1t[:], in1=ln1g[:], op=ALU.mult), reads=[r_x1t, r_ln1g], writes=[r_x1t])
        yield
        S.op("pool", lambda e: e.tensor_tensor(out=x1t[:], in0=x1t[:], in1=ln1b[:], op=ALU.add), reads=[r_x1t, r_ln1b], writes=[r_x1t])
        yield
        S.dma("sp", x1_d[tok0:tok0 + 128, :], x1t[:], reads=[r_x1t], awrites=[r_x1])
        yield
        ln_stats(x1t, r_x1t, st2, r_st2, mv2, r_mv2, rs2, r_rs2)
        yield
        S.op("dve", lambda e: e.tensor_scalar(out=u2t[:], in0=x1t[:], scalar1=mv2[:, 0:1], scalar2=rs2[:], op0=ALU.subtract, op1=ALU.mult), reads=[r_x1t, r_mv2, r_rs2], writes=[r_u2t])
        yield
        S.op("pool", lambda e: e.tensor_tensor(out=u2t[:], in0=u2t[:], in1=sc2[:], op=ALU.mult), reads=[r_u2t, r_sc2], writes=[r_u2t])
        yield
        S.op("pool", lambda e: e.tensor_tensor(out=u2t[:], in0=u2t[:], in1=sh2[:], op=ALU.add), reads=[r_u2t, r_sh2], writes=[r_u2t])
        yield
        ub, r_ub = u2b[t % 3]
        S.op("act", lambda e: e.activation(out=ub[:], in_=u2t[:], func=AF.Copy), reads=[r_u2t], writes=[r_ub])
        yield
        S.dma("sp", u2_d[tok0:tok0 + 128, :], ub[:], reads=[r_ub], awrites=[r_u2])
        yield

    def stage2(t):
        u2t, r_u2t = u2t_r[t % 3]
        u2T, r_u2T = u2T_r[t % 3]
        lg, r_lg = lg_r[t % 3]; m8, r_m8 = m8_r[t % 3]; sml, r_sml = sml_r[t % 3]; ex, r_ex = ex_r[t % 3]; maskb, r_maskb = maskb_r[t % 3]
        for hf in range(2):
            pT, r_pT = (ptT[t % 2] if hf == 0 else psl[t % 2])
            for k4 in range(4):
                k = hf * 4 + k4
                S.op("pe", lambda e: e.transpose(out=pT[:, k4 * 128:(k4 + 1) * 128], in_=u2t[:, k * 128:(k + 1) * 128], identity=ident_f[:]),
                     reads=[r_u2t, r_identf], writes=[r_pT] if k4 == 0 else (), awrites=() if k4 == 0 else [r_pT])
                yield
            S.op("act", lambda e: e.activation(out=u2T[:, hf * 4:hf * 4 + 4, :].rearrange("p k t -> p (k t)"), in_=pT[:], func=AF.Copy), reads=[r_pT],
                 writes=[r_u2T] if hf == 0 else (), awrites=() if hf == 0 else [r_u2T])
            yield
        pl_, r_pl = psl[t % 2]
        for k in range(8):
            S.op("pe", lambda e: e.matmul(pl_[:, 0:NE], lhsT=u2T[:, k, :], rhs=wr[:, k, :], start=(k == 0), stop=(k == 7)),
                 reads=[r_u2T, r_wr], writes=[r_pl] if k == 0 else (), awrites=() if k == 0 else [r_pl])
            yield
        S.op("dve", lambda e: e.tensor_tensor(out=lg[:], in0=pl_[:, 0:NE], in1=br_bc[:], op=ALU.add), reads=[r_pl, r_br], writes=[r_lg])
        yield
        S.op("dve", lambda e: e.max(out=m8[:], in_=lg[:]), reads=[r_lg], writes=[r_m8])
        yield
        S.op("dve", lambda e: e.tensor_scalar(out=maskall[:, t, :], in0=lg[:], scalar1=m8[:, 3:4], scalar2=None, op0=ALU.is_ge), reads=[r_lg, r_m8], awrites=[r_maskall])
        yield
        S.op("dve", lambda e: e.tensor_scalar_mul(out=sml[:, 0:1], in0=m8[:, 0:1], scalar1=-1.0), reads=[r_m8], writes=[r_sml])
        yield
        S.op("act", lambda e: e.activation(out=ex[:], in_=lg[:], func=AF.Exp, bias=sml[:, 0:1]), reads=[r_lg, r_sml], writes=[r_ex])
        yield
        S.op("dve", lambda e: e.tensor_tensor(out=ex[:], in0=ex[:], in1=maskall[:, t, :], op=ALU.mult), reads=[r_ex, r_maskall], writes=[r_ex])
        yield
        S.op("dve", lambda e: e.reduce_sum(out=sml[:, 1:2], in_=ex[:], axis=AX.X), reads=[r_ex], awrites=[r_sml])
        yield
        S.op("dve", lambda e: e.reciprocal(out=sml[:, 2:3], in_=sml[:, 1:2]), reads=[r_sml], awrites=[r_sml])
        yield
        S.op("dve", lambda e: e.tensor_scalar(out=gall[:, t, :], in0=ex[:], scalar1=sml[:, 2:3], scalar2=None, op0=ALU.mult), reads=[r_ex, r_sml], awrites=[r_gall])
        yield
        S.op("dve", lambda e: e.tensor_copy(out=maskb[:], in_=maskall[:, t, :]), reads=[r_maskall], writes=[r_maskb])
        yield
        S.op("pe", lambda e: e.matmul(pl_[:, 64:64 + NE], lhsT=sutb[:], rhs=maskb[:], start=True, stop=True), reads=[r_sutb, r_maskb, r_lg], awrites=[r_pl])
        yield
        S.op("pe", lambda e: e.matmul(pl_[:, 128:128 + NE], lhsT=onesb[:], rhs=maskb[:], start=True, stop=True), reads=[r_onesb, r_maskb], awrites=[r_pl])
        yield
        S.op("dve", lambda e: e.tensor_tensor(out=posall[:, t, :], in0=pl_[:, 64:64 + NE], in1=rrun[:], op=ALU.add), reads=[r_pl, r_rrun], awrites=[r_posall])
        yield
        S.op("dve", lambda e: e.tensor_tensor(out=rrun[:], in0=pl_[:, 128:128 + NE], in1=rrun[:], op=ALU.add), reads=[r_pl, r_rrun], writes=[r_rrun])
        yield


    def tile_gen(t):
        if t + 2 < NT:
            loadD2(t + 2)
        yield from stage1(t)
        yield from stage2(t)

    for t0 in range(2):
        loadD2(t0)
    interleave((tile_gen(t) for t in range(NT)), 16)

    thr16, r_thr16 = C.sb("thr16", [128, NE, 16], F32)
    bstart, r_bstart = C.sb("bstart", [128, NBLK], F32)
    kp, r_kp = C.sb("kp", [128, 8], F32)
    pcol, r_pcol = C.sb("pcol", [128, 1], F32)
    sut32, r_sut32 = C.sb("sut32", [NE, NE], F32)
    S.dma("sp", thr16[:].rearrange("p e m -> p (e m)"), g_("thr16_d").partition_broadcast(128), reads=[r_in], writes=[r_thr16])
    S.dma("sp", bstart[:], g_("bstart_d").partition_broadcast(128), reads=[r_in], writes=[r_bstart])
    S.dma("sp", kp[:], g_("kp_d"), reads=[r_in], writes=[r_kp])
    S.dma("sp", pcol[:], g_("pcol_d"), reads=[r_in], writes=[r_pcol])
    S.dma("sp", sut32[:], g_("sut32_d"), reads=[r_in], writes=[r_sut32])
    big, r_big = C.sb("bigD", [128, NBLK * NE], F32)
    nbk, r_nbk = C.sb("nbk", [128, NE], F32)
    padT, r_padT = C.sb("padT", [NE, 128], F32)
    pstart, r_pstart = C.sb("pstart", [128, NE], F32)
    pend, r_pend = C.sb("pend", [128, NE], F32)
    bexp, r_bexp = C.sb("bexp", [128, NBLK], F32)
    wf, r_wf = C.sb("wf", [128, NBLK, 8], F32)
    S.op("dve", lambda e: e.tensor_tensor(out=big[:, 0:NE * 16].rearrange("p (e m) -> p e m", m=16), in0=rrun[:].unsqueeze(2).to_broadcast([128, NE, 16]), in1=thr16[:], op=ALU.is_gt),
         reads=[r_rrun, r_thr16], writes=[r_big])
    S.op("dve", lambda e: e.tensor_reduce(out=nbk[:], in_=big[:, 0:NE * 16].rearrange("p (e m) -> p e m", m=16), axis=AX.X, op=ALU.add), reads=[r_big], writes=[r_nbk])
    S.op("dve", lambda e: e.tensor_scalar_mul(out=nbk[:], in0=nbk[:], scalar1=512.0), reads=[r_nbk], writes=[r_nbk])
    pq, r_pq = psl[0]
    S.op("pe", lambda e: e.transpose(out=pq[0:NE, 0:128], in_=nbk[:], identity=ident_f[:]), reads=[r_nbk, r_identf], writes=[r_pq])
    S.op("act", lambda e: e.activation(out=padT[:], in_=pq[0:NE, 0:128], func=AF.Copy), reads=[r_pq], writes=[r_padT])
    S.op("pe", lambda e: e.matmul(pq[:, 256:256 + NE], lhsT=padT[:], rhs=sut32[:], start=True, stop=True), reads=[r_padT, r_sut32], awrites=[r_pq])
    S.op("act", lambda e: e.activation(out=pstart[:], in_=pq[:, 256:256 + NE], func=AF.Copy), reads=[r_pq], writes=[r_pstart])
    S.op("dve", lambda e: e.tensor_tensor(out=pend[:], in0=pstart[:], in1=nbk[:], op=ALU.add), reads=[r_pstart, r_nbk], writes=[r_pend])
    S.op("dve", lambda e: e.tensor_tensor(out=big[:].rearrange("p (i e) -> p i e", e=NE), in0=bstart[:].unsqueeze(2).to_broadcast([128, NBLK, NE]),
                                         in1=pend[:].unsqueeze(1).to_broadcast([128, NBLK, NE]), op=ALU.is_ge), reads=[r_bstart, r_pend], writes=[r_big])
    S.op("dve", lambda e: e.tensor_reduce(out=bexp[:], in_=big[:].rearrange("p (i e) -> p i e", e=NE), axis=AX.X, op=ALU.add), reads=[r_big], writes=[r_bexp])
    S.op("dve", lambda e: e.tensor_scalar_min(out=bexp[:], in0=bexp[:], scalar1=float(NE - 1)), reads=[r_bexp], writes=[r_bexp])
    S.op("dve", lambda e: e.tensor_copy(out=eidx[:], in_=bexp[:]), reads=[r_bexp], writes=[r_eidx])
    S.op("dve", lambda e: e.tensor_scalar(out=bidx[:], in0=bexp[:], scalar1=128.0, scalar2=pcol[:, 0:1], op0=ALU.mult, op1=ALU.add), reads=[r_bexp, r_pcol], writes=[r_bidx])
    S.op("dve", lambda e: e.tensor_scalar_mul(out=wf[:], in0=bexp[:].unsqueeze(2).to_broadcast([128, NBLK, 8]), scalar1=float(D)), reads=[r_bexp], writes=[r_wf])
    S.op("dve", lambda e: e.tensor_tensor(out=widx[:], in0=wf[:], in1=kp[:].unsqueeze(1).to_broadcast([128, NBLK, 8]), op=ALU.add), reads=[r_wf, r_kp], writes=[r_widx])
    sl_, r_sl = C.sb("slD", [128, NE], F32)
    eqt, r_eqt = C.sb("eqt", [128, NE], F32)
    m8b, r_m8b = C.sb("m8b", [128, 8], F32)
    S.op("dve", lambda e: e.tensor_scalar_add(out=pstart[:], in0=pstart[:], scalar1=1.0), reads=[r_pstart], writes=[r_pstart])
    for t0 in range(2):
        S.dma("sp", u2b[t0 % 3][0][:], u2_d[t0 * 128:t0 * 128 + 128, :], reads=[r_u2], writes=[u2b[t0 % 3][1]])
    for t in range(NT):
        tok0 = t * 128
        ub, r_ub = u2b[t % 3]
        if t + 2 < NT:
            S.dma("sp", u2b[(t + 2) % 3][0][:], u2_d[tok0 + 256:tok0 + 384, :], reads=[r_u2], writes=[u2b[(t + 2) % 3][1]])
        S.op("dve", lambda e: e.tensor_tensor(out=sl_[:], in0=posall[:, t, :], in1=pstart[:], op=ALU.add), reads=[r_posall, r_pstart], writes=[r_sl])
        S.op("dve", lambda e: e.tensor_tensor(out=sl_[:], in0=sl_[:], in1=maskall[:, t, :], op=ALU.mult), reads=[r_sl, r_maskall], writes=[r_sl])
        S.op("dve", lambda e: e.max(out=m8b[:], in_=sl_[:]), reads=[r_sl], writes=[r_m8b])
        for j in range(4):
            S.op("dve", lambda e: e.scalar_tensor_tensor(out=eqt[:], in0=sl_[:], scalar=m8b[:, j:j + 1], in1=gall[:, t, :], op0=ALU.is_equal, op1=ALU.mult, accum_out=g4[:, t, j:j + 1]),
                 reads=[r_sl, r_m8b, r_gall], writes=[r_eqt], awrites=[r_g4])
        S.op("dve", lambda e: e.tensor_scalar_add(out=idx4[:, t, :], in0=m8b[:, 0:4], scalar1=-1.0), reads=[r_m8b], awrites=[r_idx4])
        for j in range(4):
            S.op("pool", lambda e: e.indirect_dma_start(out=xs_d[:, :], out_offset=bass.IndirectOffsetOnAxis(ap=idx4[:, t, j:j + 1], axis=0), in_=ub[:], in_offset=None),
                 reads=[r_ub, r_idx4], awrites=[r_xsd], dma=True)
    C.pop()
    if "stopD" in dbg:
        C.pop()
        return

    C.push()
    w1b = C.ring("sb", "w1b", 2, [128, 8, 2 * D], BF16)
    w2b = C.ring("sb", "w2b", 2, [128, 8, D], BF16)
    b1t = C.ring("sb", "b1t", 2, [128, 16], F32)
    b2t = C.ring("sb", "b2t", 2, [128, D], F32)
    ones1f, r_ones1f = C.sb("ones1f", [1, 128], BF16)
    S.op("dve", lambda e: e.memset(ones1f[:], 1.0), writes=[r_ones1f])
    b2b = C.ring("sb", "b2b", 2, [1, D], BF16)
    xr = C.ring("sb", "xr", 8, [128, D], BF16)
    XT, r_XT = C.sb("XT", [128, 8, 512], BF16)
    hg = C.ring("sb", "hg", 2, [128, 512], F32)
    hu = C.ring("sb", "hu", 2, [128, 512], F32)
    sgm = C.ring("sb", "sgm", 2, [128, 512], F32)
    actT, r_actT = C.sb("actT", [128, 8, 512], BF16)
    ysb = C.ring("sb", "ysb", 2, [128, D], BF16)
    pxt = C.ring("ps", "pxt", 2, [128, 512], F32)
    pg = C.ring("ps", "pg", 2, [128, 512], F32)
    pu = C.ring("ps", "pu", 2, [128, 512], F32)
    py = C.ring("ps", "py", 2, [128, 512], F32)

    def load_weights(i, slot):
        w1t, r_w1 = w1b[slot]
        w2t, r_w2 = w2b[slot]
        for k in range(8):
            S.op("pool", lambda e: e.indirect_dma_start(out=w1t[:, k, :], out_offset=None, in_=w1b_d[:, :], in_offset=bass.IndirectOffsetOnAxis(ap=widx[:, i, k:k + 1], axis=0)),
                 reads=[r_w1bd, r_widx], writes=[r_w1] if k == 0 else (), awrites=() if k == 0 else [r_w1], dma=True)
        for k in range(8):
            S.op("pool", lambda e: e.indirect_dma_start(out=w2t[:, k, :], out_offset=None, in_=w2b_d[:, :], in_offset=bass.IndirectOffsetOnAxis(ap=widx[:, i, k:k + 1], axis=0)),
                 reads=[r_w2bd, r_widx], writes=[r_w2] if k == 0 else (), awrites=() if k == 0 else [r_w2], dma=True)
        S.op("pool", lambda e: e.indirect_dma_start(out=b1t[slot][0][:], out_offset=None, in_=b1r_d[:, :], in_offset=bass.IndirectOffsetOnAxis(ap=bidx[:, i:i + 1], axis=0)),
             reads=[r_in, r_bidx], writes=[b1t[slot][1]], dma=True)
        S.op("pool", lambda e: e.indirect_dma_start(out=b2t[slot][0][:], out_offset=None, in_=b2_d[:, :], in_offset=bass.IndirectOffsetOnAxis(ap=eidx[:, i:i + 1], axis=0)),
             reads=[r_in, r_eidx], writes=[b2t[slot][1]], dma=True)

    def load_x(i):
        for s4 in range(4):
            x_t, r_x = xr[(i % 2) * 4 + s4]
            row0 = i * 512 + s4 * 128
            S.dma("sp", x_t[:], xs_d[row0:row0 + 128, :], reads=[r_xsd], writes=[r_x])

    nblk_run = NBLK if "nblk" not in L else L["nblk"]
    load_weights(0, 0)
    load_x(0)
    xc = 0
    for i in range(nblk_run):
        slot = i % 2
        if i + 1 < nblk_run:
            load_weights(i + 1, (i + 1) % 2)
            load_x(i + 1)
        w1t, r_w1 = w1b[slot]
        w2t, r_w2 = w2b[slot]
        b1_t, r_b1 = b1t[slot]
        b2f_t, r_b2f = b2t[slot]
        b2_t, r_b2 = b2b[slot]
        for s4 in range(4):
            x_t, r_x = xr[(i % 2) * 4 + s4]
            p_t, r_p = pxt[xc % 2]
            xc += 1
            p_b = p_t[:].bitcast(BF16)
            for k in range(8):
                S.op("pe", lambda e: e.transpose(out=p_b[:, k * 128:(k + 1) * 128], in_=x_t[:, k * 128:(k + 1) * 128], identity=ident_b[:]),
                     reads=[r_x, r_identb], writes=[r_p] if k == 0 else (), awrites=() if k == 0 else [r_p])
            S.op("act", lambda e: e.activation(out=XT[:, :, s4 * 128:(s4 + 1) * 128], in_=p_b.rearrange("p (k t) -> p k t", k=8), func=AF.Copy), reads=[r_p],
                 writes=[r_XT] if s4 == 0 else (), awrites=() if s4 == 0 else [r_XT])
        for fc in range(8):
            pg_t, r_pg = pg[fc % 2]
            pu_t, r_pu = pu[fc % 2]
            for k in range(8):
                S.op("pe", lambda e: e.matmul(pg_t[:], lhsT=w1t[:, k, fc * 128:(fc + 1) * 128], rhs=XT[:, k, :], start=(k == 0), stop=(k == 7)),
                     reads=[r_w1, r_XT], writes=[r_pg] if k == 0 else (), awrites=() if k == 0 else [r_pg])
            for k in range(8):
                S.op("pe", lambda e: e.matmul(pu_t[:], lhsT=w1t[:, k, D + fc * 128:D + (fc + 1) * 128], rhs=XT[:, k, :], start=(k == 0), stop=(k == 7)),
                     reads=[r_w1, r_XT], writes=[r_pu] if k == 0 else (), awrites=() if k == 0 else [r_pu])
            hg_t, r_hg = hg[fc % 2]
            hu_t, r_hu = hu[fc % 2]
            sg_t, r_sgm = sgm[fc % 2]
            S.op("act", lambda e: e.activation(out=hg_t[:], in_=pg_t[:], func=AF.Identity, bias=b1_t[:, fc:fc + 1]), reads=[r_pg, r_b1], writes=[r_hg])
            S.op("act", lambda e: e.activation(out=hu_t[:], in_=pu_t[:], func=AF.Identity, bias=b1_t[:, 8 + fc:9 + fc]), reads=[r_pu, r_b1], writes=[r_hu])
            S.op("dve", lambda e: e.tensor_scalar_min(out=hg_t[:], in0=hg_t[:], scalar1=7.0), reads=[r_hg], writes=[r_hg])
            S.op("act", lambda e: e.activation(out=sg_t[:], in_=hg_t[:], func=AF.Sigmoid, scale=1.702), reads=[r_hg], writes=[r_sgm])
            S.op("pool", lambda e: e.tensor_scalar(out=hu_t[:], in0=hu_t[:], scalar1=7.0, scalar2=-7.0, op0=ALU.min, op1=ALU.max), reads=[r_hu], writes=[r_hu])
            S.op("dve", lambda e: e.scalar_tensor_tensor(out=hu_t[:], in0=hu_t[:], scalar=1.0, in1=hg_t[:], op0=ALU.add, op1=ALU.mult), reads=[r_hu, r_hg], writes=[r_hu])
            S.op("dve", lambda e: e.tensor_tensor(out=actT[:, fc, :], in0=hu_t[:], in1=sg_t[:], op=ALU.mult), reads=[r_hu, r_sgm],
                 writes=[r_actT] if fc == 0 else (), awrites=() if fc == 0 else [r_actT])
        for s4 in range(4):
            y_t, r_y = ysb[s4 % 2]
            for hf in range(2):
                py_t, r_py = py[hf]
                for fc in range(8):
                    S.op("pe", lambda e: e.matmul(py_t[:], lhsT=actT[:, fc, s4 * 128:(s4 + 1) * 128], rhs=w2t[:, fc, hf * 512:(hf + 1) * 512], start=(fc == 0), stop=(fc == 7)),
                         reads=[r_actT, r_w2], writes=[r_py] if fc == 0 else (), awrites=() if fc == 0 else [r_py])
                S.op("dve", lambda e: e.tensor_tensor(out=y_t[:, hf * 512:(hf + 1) * 512], in0=py_t[:], in1=b2f_t[:, hf * 512:(hf + 1) * 512], op=ALU.add), reads=[r_py, r_b2f],
                     writes=[r_y] if hf == 0 else (), awrites=() if hf == 0 else [r_y])
            row0 = i * 512 + s4 * 128
            S.dma("act", ys_d[row0:row0 + 128, :], y_t[:], reads=[r_y], awrites=[r_ysd])
    C.pop()

    C.push()
    ln2g, r_ln2g = C.sb("ln2g", [128, D], F32)
    ln2b, r_ln2b = C.sb("ln2b", [128, D], F32)
    S.dma("sp", ln2g[:], g_("ln2g_d").partition_broadcast(128), reads=[r_in], writes=[r_ln2g])
    S.dma("sp", ln2b[:], g_("ln2b_d").partition_broadcast(128), reads=[r_in], writes=[r_ln2b])
    gate2r = C.ring("sb", "gate2r", 2, [128, D], F32)
    x1r = C.ring("sb", "x1r", 4, [128, D], F32)
    yg = C.ring("sb", "yg", 16, [128, D], BF16)
    accr = C.ring("sb", "accF", 3, [128, D], F32)
    ot = C.ring("sb", "otF", 3, [128, D], F32)
    stF = C.ring("sb", "stF", 3, [128, 2, 6], F32)
    mvF = C.ring("sb", "mvF", 3, [128, 4], F32)
    rsF = C.ring("sb", "rsF", 3, [128, 1], F32)

    def loadF(t):
        tok0 = t * 128
        slot = t % 4
        S.dma("sp", x1r[slot][0][:], x1_d[tok0:tok0 + 128, :], reads=[r_x1], writes=[x1r[slot][1]])
        for j in range(4):
            y_t, r_y = yg[slot * 4 + j]
            S.op("pool", lambda e: e.indirect_dma_start(out=y_t[:], out_offset=None, in_=ys_d[:, :], in_offset=bass.IndirectOffsetOnAxis(ap=idx4[:, t, j:j + 1], axis=0)),
                 reads=[r_ysd, r_idx4], writes=[r_y], dma=True)
        if t % TPS == 0:
            b_ = t // TPS
            S.dma("sp", gate2r[b_ % 2][0][:], mod_d[b_:b_ + 1, 5 * D:6 * D].partition_broadcast(128), reads=[r_mod], writes=[gate2r[b_ % 2][1]])

    def genF(t):
        if t + 3 < NT:
            loadF(t + 3)
        slot = t % 4
        tok0 = t * 128
        gate2, r_gate2 = gate2r[(t // TPS) % 2]
        x1_t, r_x1t = x1r[slot]
        acc, r_acc = accr[t % 3]
        st, r_st = stF[t % 3]; mv, r_mv = mvF[t % 3]; rs, r_rs = rsF[t % 3]
        o_t, r_o = ot[t % 3]
        for j in range(4):
            y_t, r_y = yg[slot * 4 + j]
            if j == 0:
                S.op("act", lambda e: e.activation(out=acc[:], in_=y_t[:], func=AF.Copy, scale=g4[:, t, 0:1]), reads=[r_y, r_g4], writes=[r_acc])
            else:
                S.op("dve", lambda e: e.scalar_tensor_tensor(out=acc[:], in0=y_t[:], scalar=g4[:, t, j:j + 1], in1=acc[:], op0=ALU.mult, op1=ALU.add), reads=[r_y, r_g4, r_acc], writes=[r_acc])
            yield
        S.op("pool", lambda e: e.tensor_tensor(out=acc[:], in0=acc[:], in1=gate2[:], op=ALU.mult), reads=[r_acc, r_gate2], writes=[r_acc])
        yield
        S.op("dve", lambda e: e.scalar_tensor_tensor(out=acc[:], in0=x1_t[:], scalar=float(ALPHA), in1=acc[:], op0=ALU.mult, op1=ALU.add), reads=[r_x1t, r_acc], writes=[r_acc])
        yield
        for j in range(2):
            S.op("dve", lambda e: e.bn_stats(out=st[:, j, :], in_=acc[:, j * 512:(j + 1) * 512]), reads=[r_acc], writes=[r_st] if j == 0 else (), awrites=() if j == 0 else [r_st])
            yield
        S.op("dve", lambda e: e.bn_aggr(out=mv[:, 0:2], in_=st[:].rearrange("p a b -> p (a b)")), reads=[r_st], writes=[r_mv])
        yield
        S.op("act", lambda e: e.activation(out=rs[:], in_=mv[:, 1:2], func=AF.Sqrt, bias=EPS), reads=[r_mv], writes=[r_rs])
        yield
        S.op("dve", lambda e: e.reciprocal(out=rs[:], in_=rs[:]), reads=[r_rs], writes=[r_rs])
        yield
        S.op("dve", lambda e: e.tensor_scalar(out=mv[:, 2:3], in0=mv[:, 0:1], scalar1=-1.0, scalar2=rs[:], op0=ALU.mult, op1=ALU.mult), reads=[r_mv, r_rs], awrites=[r_mv])
        yield
        S.op("act", lambda e: e.activation(out=o_t[:], in_=acc[:], func=AF.Identity, scale=rs[:], bias=mv[:, 2:3]), reads=[r_acc, r_mv, r_rs], writes=[r_o])
        yield
        S.op("dve", lambda e: e.tensor_tensor(out=o_t[:], in0=o_t[:], in1=ln2g[:], op=ALU.mult), reads=[r_o, r_ln2g], writes=[r_o])
        yield
        S.op("dve", lambda e: e.tensor_tensor(out=o_t[:], in0=o_t[:], in1=ln2b[:], op=ALU.add), reads=[r_o, r_ln2b], writes=[r_o])
        yield
        S.dma("sp", out_d[tok0:tok0 + 128, :], o_t[:], reads=[r_o], awrites=[r_out])
        yield

    for t0 in range(3):
        loadF(t0)
    interleave((genF(t) for t in range(NT)), 6)
    C.pop()
    C.pop()


def phase_C(nc, S, C, dbg, L):
    g_ = lambda n: L[n]
    r_in = g_("r_in"); ident_b = g_("ident_b"); r_identb = g_("r_identb"); ident_f = g_("ident_f"); r_identf = g_("r_identf")
    qT_d, r_qT = g_("qT_d"), g_("r_qT"); iqT_d, r_iqT = g_("iqT_d"), g_("r_iqT")
    kva_d, r_kva = g_("kva_d"), g_("r_kva"); kvT_d, r_kvT = g_("kvT_d"), g_("r_kvT"); ikT_d, r_ikT = g_("ikT_d"), g_("r_ikT")
    iw_d, r_iw = g_("iw_d"), g_("r_iw"); oaT_d, r_oaT = g_("oaT_d"), g_("r_oaT")
    C.push()
    w1_d, w2_d = g_("w1_d"), g_("w2_d")

    def cast_weights(i):
        e_ = i // 2
        if i % 2 == 0:
            S.dma("pool", g_("w1b_d")[e_ * D:(e_ + 1) * D, :], w1_d[e_ * D:(e_ + 1) * D, :], reads=[r_in], awrites=[g_("r_w1bd")])
        else:
            S.dma("pool", g_("w2b_d")[e_ * D:(e_ + 1) * D, :], w2_d[e_ * D:(e_ + 1) * D, :], reads=[r_in], awrites=[g_("r_w2bd")])
    tzf, r_tzf = C.sb("tzf", [128, 2, 8, 128], F32)
    tz, r_tz = C.sb("tzb", [128, 2, 8, 128], BF16)
    cfar, r_cfar = C.sb("cfar", [128, 8], F32)
    identN, r_identN = C.sb("identN", [128, 128], BF16)
    S.dma("sp", tzf[:], g_("tz_d"), reads=[r_in], writes=[r_tzf])
    S.dma("sp", cfar[:], g_("cfar_d").partition_broadcast(128), reads=[r_in], writes=[r_cfar])
    S.op("dve", lambda e: e.tensor_scalar_mul(out=identN[:], in0=ident_f[:], scalar1=-NEG), reads=[r_identf], writes=[r_identN])
    first = True
    for dl in range(2):
        for h in range(8):
            S.op("dve", lambda e: e.tensor_scalar(out=tz[:, dl, h, :], in0=tzf[:, dl, h, :], scalar1=cfar[:, h:h + 1], scalar2=None, op0=ALU.subtract),
                 reads=[r_tzf, r_cfar], writes=[r_tz] if first else (), awrites=() if first else [r_tz])
            first = False
    seqb = [(C.sb("kvT%d" % i, [128, SEQ], BF16), C.sb("ikT%d" % i, [128, SEQ], BF16), C.sb("kvaC%d" % i, [128, TPS, 136], BF16)) for i in range(2)]
    qt = C.ring("sb", "qt", 5, [128, 8, 128], BF16)
    iqt = C.ring("sb", "iqt", 4, [128, 4, 128], BF16)
    iwt = C.ring("sb", "iwC", 4, [128, 8], F32)
    scorer = C.ring("sb", "score", 3, [128, SEQ], F32)
    penr = C.ring("sb", "pen", 2, [128, SEQ], BF16)
    rl = C.ring("sb", "rl", 2, [128, 512], F32)
    m8, r_m8 = C.sb("m8", [128, 8], F32)
    expT = C.ring("sb", "expT", 2, [128, TPS * 128], BF16)
    posb = C.ring("sb", "posb", 2, [128, 8, 132], F32)
    rc, r_rc = C.sb("rcC", [128, 8], F32)
    oa, r_oa = C.sb("oa", [128, D], BF16)
    oaT = C.ring("sb", "oaT", 2, [128, 8, 128], BF16)
    pi = C.ring("ps", "pi", 2, [128, 512], F32)
    pl = C.ring("ps", "pl", 3, [128, 512], F32)
    po = C.ring("ps", "po", 2, [128, 512], F32)
    ptr = C.ring("ps", "ptrC", 1, [128, 512], F32)
    NTILE = NB * TPS

    def load_seq(b):
        (kvT, r_kvTs), (ikT, r_ikTs), (kva, r_kvas) = seqb[b % 2]
        s0 = b * SEQ
        S.dma("sp", kvT[:], kvT_d[:, s0:s0 + SEQ], reads=[r_kvT], writes=[r_kvTs])
        S.dma("sp", ikT[:], ikT_d[:, s0:s0 + SEQ], reads=[r_ikT], writes=[r_ikTs])
        S.dma("sp", kva[:], kva_d[s0:s0 + SEQ, :].rearrange("(k p) c -> p k c", p=128), reads=[r_kva], writes=[r_kvas])

    def load_tile(i):
        tok0 = i * 128
        S.dma("sp", qt[i % 5][0][:], qT_d[:, :, tok0:tok0 + 128].rearrange("h p t -> p h t"), reads=[r_qT], writes=[qt[i % 5][1]])
        S.dma("sp", iqt[i % 4][0][:], iqT_d[:, :, tok0:tok0 + 128].rearrange("h p t -> p h t"), reads=[r_iqT], writes=[iqt[i % 4][1]])
        S.dma("sp", iwt[i % 4][0][:], iw_d[tok0:tok0 + 128, :], reads=[r_iw], writes=[iwt[i % 4][1]])

    NIT = 24
    pow2, r_pow2 = C.sb("pow2", [128, NIT + 1], F32)
    stepsr = C.ring("sb", "steps", 3, [128, NIT + 1], F32)
    bisr = C.ring("sb", "bis", 3, [128, 8], F32)
    junkb, r_junkb = C.sb("junkb", [128, SEQ], BF16)
    junkd, r_junkd = C.sb("junkd", [128, SEQ], BF16)
    for j in range(NIT + 1):
        S.op("pool", lambda e: e.memset(pow2[:, j:j + 1], float(2.0 ** -(j + 1))), writes=[r_pow2] if j == 0 else (), awrites=() if j == 0 else [r_pow2])

    def prep_score_gen(i):
        b, t = divmod(i, TPS)
        (ikT, r_ikTs) = seqb[b % 2][1]
        iq_t, r_iq = iqt[i % 4]
        iw_t, r_iwt = iwt[i % 4]
        pen, r_pen = penr[i % 2]
        score, r_score = scorer[i % 3]
        steps, r_steps = stepsr[i % 3]
        bis, r_bis = bisr[i % 3]
        N = 128 * (t + 1)
        if t < 2:
            return
            yield
        for kg in range(0, N, 512):
            w = min(512, N - kg)
            for h in range(8):
                pr, hf = divmod(h, 2)
                p_t, r_p = pi[h % 2]
                S.op("pe", lambda e: e.matmul(p_t[:, 0:w], lhsT=iq_t[64 * hf:64 * hf + 64, pr, :], rhs=ikT[64 * hf:64 * hf + 64, kg:kg + w], start=True, stop=True),
                     reads=[r_iq, r_ikTs], writes=[r_p])
                r_t, r_r = rl[h % 2]
                if h == 0:
                    S.op("dve", lambda e: e.tensor_scalar(out=score[:, kg:kg + w], in0=p_t[:, 0:w], scalar1=0.0, scalar2=iw_t[:, 0:1], op0=ALU.max, op1=ALU.mult),
                         reads=[r_p, r_iwt], writes=[r_score] if kg == 0 else (), awrites=() if kg == 0 else [r_score])
                elif h % 2 == 1:
                    S.op("dve", lambda e: e.tensor_scalar(out=r_t[:, 0:w], in0=p_t[:, 0:w], scalar1=0.0, scalar2=iw_t[:, h:h + 1], op0=ALU.max, op1=ALU.mult),
                         reads=[r_p, r_iwt], writes=[r_r])
                    S.op("pool", lambda e: e.tensor_tensor(out=score[:, kg:kg + w], in0=score[:, kg:kg + w], in1=r_t[:, 0:w], op=ALU.add),
                         reads=[r_r, r_score], awrites=[r_score])
                else:
                    S.op("act", lambda e: e.activation(out=r_t[:, 0:w], in_=p_t[:, 0:w], func=AF.Relu), reads=[r_p], writes=[r_r])
                    S.op("dve", lambda e: e.scalar_tensor_tensor(out=score[:, kg:kg + w], in0=r_t[:, 0:w], scalar=iw_t[:, h:h + 1], in1=score[:, kg:kg + w], op0=ALU.mult, op1=ALU.add),
                         reads=[r_r, r_iwt, r_score], awrites=[r_score])
                yield
        S.op("dve", lambda e: e.tensor_reduce(out=bis[:, 0:1], in_=score[:, 0:N - 64], axis=AX.X, op=ALU.min), reads=[r_score], writes=[r_bis])
        S.op("dve", lambda e: e.memset(score[0:64, N - 64:N], -1e30), reads=[r_score], awrites=[r_score])
        S.op("dve", lambda e: e.max(out=m8[:], in_=score[:, 0:N]), reads=[r_score], writes=[r_m8])
        S.op("dve", lambda e: e.tensor_tensor(out=bis[:, 1:2], in0=m8[:, 0:1], in1=bis[:, 0:1], op=ALU.subtract), reads=[r_m8, r_bis], awrites=[r_bis])
        S.op("dve", lambda e: e.tensor_scalar(out=steps[:], in0=pow2[:], scalar1=bis[:, 1:2], scalar2=None, op0=ALU.mult), reads=[r_pow2, r_bis], writes=[r_steps])
        S.op("dve", lambda e: e.tensor_tensor(out=bis[:, 2:3], in0=bis[:, 0:1], in1=steps[:, 0:1], op=ALU.add), reads=[r_bis, r_steps], awrites=[r_bis])

    def prep_score(i):
        for _ in prep_score_gen(i):
            pass

    def prep_iter(i, j):
        b, t = divmod(i, TPS)
        if t < 2:
            return
        N = 128 * (t + 1)
        score, r_score = scorer[i % 3]
        steps, r_steps = stepsr[i % 3]
        bis, r_bis = bisr[i % 3]
        if j % 3 != 2:
            S.op("act", lambda e: e.activation(out=junkb[:, 0:N], in_=score[:, 0:N], func=AF.Sign, scale=-1.0, bias=bis[:, 2:3], accum_out=bis[:, 4:5]),
                 reads=[r_score, r_bis], writes=[r_junkb], awrites=[r_bis])
            S.op("dve", lambda e: e.tensor_scalar(out=bis[:, 3:4], in0=bis[:, 4:5], scalar1=float(N - 511), scalar2=steps[:, j:j + 1], op0=ALU.is_le, op1=ALU.mult),
                 reads=[r_bis, r_steps], awrites=[r_bis])
        else:
            S.op("dve", lambda e: e.tensor_scalar(out=junkd[:, 0:N], in0=score[:, 0:N], scalar1=bis[:, 2:3], scalar2=None, op0=ALU.is_ge, op1=ALU.add, accum_out=bis[:, 6:7]),
                 reads=[r_score, r_bis], writes=[r_junkd], awrites=[r_bis])
            S.op("dve", lambda e: e.tensor_scalar(out=bis[:, 3:4], in0=bis[:, 6:7], scalar1=255.5, scalar2=steps[:, j:j + 1], op0=ALU.is_ge, op1=ALU.mult),
                 reads=[r_bis, r_steps], awrites=[r_bis])
        S.op("dve", lambda e: e.scalar_tensor_tensor(out=bis[:, 2:3], in0=bis[:, 3:4], scalar=steps[:, j + 1:j + 2], in1=bis[:, 2:3], op0=ALU.subtract, op1=ALU.add),
             reads=[r_bis, r_steps], awrites=[r_bis])

    def prep_fin(i):
        b, t = divmod(i, TPS)
        N = 128 * (t + 1)
        pen, r_pen = penr[i % 2]
        if t < 2:
            S.op("dve", lambda e: e.memset(pen[:, 0:N], 0.0), writes=[r_pen])
            S.op("dve", lambda e: e.memset(pen[0:64, N - 64:N], -1.0), awrites=[r_pen])
            return
        score, r_score = scorer[i % 3]
        steps, r_steps = stepsr[i % 3]
        bis, r_bis = bisr[i % 3]
        S.op("dve", lambda e: e.tensor_tensor(out=bis[:, 5:6], in0=bis[:, 2:3], in1=steps[:, NIT:NIT + 1], op=ALU.subtract), reads=[r_bis, r_steps], awrites=[r_bis])
        S.op("dve", lambda e: e.tensor_scalar(out=pen[:, 0:N], in0=score[:, 0:N], scalar1=bis[:, 5:6], scalar2=1.0, op0=ALU.is_ge, op1=ALU.subtract),
             reads=[r_score, r_bis], writes=[r_pen])

    state = {"plc": 0, "hc": 0}

    def attend_head(i, h):
        b, t = divmod(i, TPS)
        (kvT, r_kvTs), _, (kva, r_kvas) = seqb[b % 2]
        q_t, r_q = qt[i % 5]
        pen, r_pen = penr[i % 2]
        ps_t, r_ps = posb[i % 2]
        nkb = t + 1
        if True:
            e_t, r_e = expT[state["hc"] % 2]
            state["hc"] += 1
            for kg in range(0, nkb, 4):
                nb_ = min(4, nkb - kg)
                p_t, r_p = pl[state["plc"] % 3]
                state["plc"] += 1
                for ii in range(nb_):
                    kb = kg + ii
                    near = kb >= t - 1
                    cs_ = slice(ii * 128, (ii + 1) * 128)
                    S.op("pe", lambda e: e.matmul(p_t[:, cs_], lhsT=kvT[:, kb * 128:(kb + 1) * 128], rhs=q_t[:, h, :], start=True, stop=False),
                         reads=[r_kvTs, r_q], writes=[r_p] if ii == 0 else (), awrites=() if ii == 0 else [r_p])
                    S.op("pe", lambda e: e.matmul(p_t[:, cs_], lhsT=pen[:, kb * 128:(kb + 1) * 128], rhs=identN[:], start=False, stop=(not near)),
                         reads=[r_pen, r_identN], awrites=[r_p])
                    if near:
                        S.op("pe", lambda e: e.matmul(p_t[:, cs_], lhsT=ident_b[:], rhs=tz[:, t - kb, h, :], start=False, stop=True),
                             reads=[r_identb, r_tz], awrites=[r_p])
                S.op("act", lambda e: e.activation(out=e_t[:, kg * 128:(kg + nb_) * 128], in_=p_t[:, 0:nb_ * 128], func=AF.Exp), reads=[r_p],
                     writes=[r_e] if kg == 0 else (), awrites=() if kg == 0 else [r_e])
            o_t, r_o = po[h % 2]
            for kb in range(nkb):
                S.op("pe", lambda e: e.matmul(o_t[:, 0:129], lhsT=e_t[:, kb * 128:(kb + 1) * 128], rhs=kva[:, kb, 0:129], start=(kb == 0), stop=(kb == nkb - 1)),
                     reads=[r_e, r_kvas], writes=[r_o] if kb == 0 else (), awrites=() if kb == 0 else [r_o])
            S.op("act", lambda e: e.activation(out=ps_t[:, h, 0:129], in_=o_t[:, 0:129], func=AF.Copy), reads=[r_o],
                 writes=[r_ps] if h == 0 else (), awrites=() if h == 0 else [r_ps])

    def finalize(i):
        tok0 = i * 128
        ps_t, r_ps = posb[i % 2]
        S.op("dve", lambda e: e.reciprocal(out=rc[:], in_=ps_t[:, :, 128]), reads=[r_ps], writes=[r_rc])
        S.op("dve", lambda e: e.tensor_tensor(out=oa[:].rearrange("p (h c) -> p h c", h=8), in0=ps_t[:, :, 0:128], in1=rc[:].unsqueeze(2).to_broadcast([128, 8, 128]), op=ALU.mult),
             reads=[r_ps, r_rc], writes=[r_oa])
        pT, r_pT = ptr[0]
        pT_b = pT[:].bitcast(BF16)
        for k in range(8):
            S.op("pe", lambda e: e.transpose(out=pT_b[:, k * 128:(k + 1) * 128], in_=oa[:, k * 128:(k + 1) * 128], identity=ident_b[:]),
                 reads=[r_oa, r_identb], writes=[r_pT] if k == 0 else (), awrites=() if k == 0 else [r_pT])
        oT, r_oT = oaT[i % 2]
        S.op("act", lambda e: e.activation(out=oT[:].rearrange("p k t -> p (k t)"), in_=pT_b, func=AF.Copy), reads=[r_pT], writes=[r_oT])
        S.dma("sp", oaT_d[:, :, tok0:tok0 + 128].rearrange("c p t -> p c t"), oT[:], reads=[r_oT], awrites=[r_oaT])

    HALF = NIT // 2
    load_seq(0)
    for i0 in range(4):
        load_tile(i0)
    prep_score(0)
    for j in range(NIT):
        prep_iter(0, j)
    prep_fin(0)
    prep_score(1)
    for j in range(HALF):
        prep_iter(1, j)
    prep_score(2)
    for i in range(NTILE):
        b, t = divmod(i, TPS)
        if t == 0 and b + 1 < NB:
            load_seq(b + 1)
        if i + 4 < NTILE:
            load_tile(i + 4)
        cast_weights(i)
        n1 = i + 1 < NTILE
        n2 = i + 2 < NTILE
        gen = prep_score_gen(i + 3) if i + 3 < NTILE else iter(())
        npieces = 8 * ((128 * (((i + 3) % TPS) + 1) + 511) // 512) if (i + 3 < NTILE and (i + 3) % TPS >= 2) else 0
        ppb = (npieces + 7) // 8
        sched = []
        for k in range(HALF):
            if n1:
                sched.append((i + 1, HALF + k))
            if n2:
                sched.append((i + 2, k))
        per = (len(sched) + 7) // 8
        for h in range(8):
            attend_head(i, h)
            its = sched[h * per:(h + 1) * per]
            for n_, (ti_, j) in enumerate(its):
                prep_iter(ti_, j)
                if n_ < ppb:
                    next(gen, None)
            for _ in range(max(0, ppb - len(its))):
                next(gen, None)
        for _ in gen:
            pass
        if n1:
            prep_fin(i + 1)
        if i >= 1:
            finalize(i - 1)
    finalize(NTILE - 1)
    C.pop()


def phase_B(nc, S, C, dbg, L):
    g_ = lambda n: L[n]
    r_in = g_("r_in"); ident_b = g_("ident_b"); r_identb = g_("r_identb")
    xbcT_d, r_xbcT = g_("xbcT_d"), g_("r_xbcT"); dt_d, r_dt = g_("dt_d"), g_("r_dt"); zs_d, r_zs = g_("zs_d"), g_("r_zs")
    obT_d, r_obT = g_("obT_d"), g_("r_obT")
    C.push()
    convw, r_convw = C.sb("convw", [128, 16, 4], F32)
    convb, r_convb = C.sb("convb", [128, 16], F32)
    dg, r_dg = C.sb("dg", [128, 16, 4, 128], BF16)
    identf2, r_identf2 = C.sb("identf2", [128, 128], F32)
    a_bc, r_abc = C.sb("a_bc", [128, 16], F32)
    dskip_bc, r_dskip = C.sb("dskip_bc", [128, 16], F32)
    normw_bc, r_normw = C.sb("normw_bc", [128, D], F32)
    triU, r_triU = C.sb("triU", [128, 128], F32)
    SLm, r_SL = C.sb("SLm", [128, 128], F32)
    onesf, r_onesf = C.sb("onesf", [128, 128], F32)
    negm4, r_negm4 = C.sb("negm4", [128, 512], BF16)
    S.dma("sp", convw[:], g_("convw_d"), reads=[r_in], writes=[r_convw])
    S.dma("sp", convb[:], g_("convb_d"), reads=[r_in], writes=[r_convb])
    S.dma("sp", identf2[:], g_("ident_d"), reads=[r_in], writes=[r_identf2])
    S.dma("sp", a_bc[:], g_("alog_d").partition_broadcast(128), reads=[r_in], writes=[r_abc])
    S.dma("sp", dskip_bc[:], g_("dskip_d").partition_broadcast(128), reads=[r_in], writes=[r_dskip])
    S.dma("sp", normw_bc[:], g_("normw_d").partition_broadcast(128), reads=[r_in], writes=[r_normw])
    S.dma("sp", triU[:], g_("triU_d"), reads=[r_in], writes=[r_triU])
    S.dma("sp", SLm[:], g_("SL_d"), reads=[r_in], writes=[r_SL])
    S.dma("pool", negm4[:], g_("negm4_d"), reads=[r_in], writes=[r_negm4])
    S.op("dve", lambda e: e.memset(onesf[:], 1.0), writes=[r_onesf])
    S.op("act", lambda e: e.activation(out=a_bc[:], in_=a_bc[:], func=AF.Exp), reads=[r_abc], writes=[r_abc])
    S.op("dve", lambda e: e.tensor_scalar_mul(out=a_bc[:], in0=a_bc[:], scalar1=-1.0), reads=[r_abc], writes=[r_abc])
    first = True
    for j in range(16):
        for k in range(4):
            S.op("dve", lambda e: e.tensor_scalar_mul(out=dg[:, j, k, :], in0=identf2[:], scalar1=convw[:, j, k:k + 1]),
                 reads=[r_identf2, r_convw], writes=[r_dg] if first else (), awrites=() if first else [r_dg])
            first = False

    bank = C.ring("ps", "bk", 8, [128, 512], F32)
    xh = C.ring("sb", "xh", 4, [128, 16, 131], BF16)
    dtl = C.ring("sb", "dtl", 4, [128, 16], F32)
    zl = C.ring("sb", "zl", 4, [128, D], BF16)
    xact_r = C.ring("sb", "xact", 2, [128, 16, 128], BF16)
    xs_r = C.ring("sb", "xs_tok", 2, [128, D], BF16)
    Bt_r = C.ring("sb", "B_tok", 2, [128, 512], BF16)
    adt_r = C.ring("sb", "adt", 2, [128, 16], F32)
    sm_r = C.ring("sb", "smB", 2, [128, 8, 16], F32)
    A_r = C.ring("sb", "Amat", 2, [128, 16, 128], F32)
    Lt_r = C.ring("sb", "Lt", 2, [128, 2, 512], F32)
    Mt_r = C.ring("sb", "Mt", 2, [128, 16, 128], BF16)
    xdt_r = C.ring("sb", "xdt", 2, [128, D], BF16)
    xdd_r = C.ring("sb", "xdd", 2, [128, D], BF16)
    prev_f, r_pf = C.sb("prev_f", [128, D], F32)
    prev_b, r_pb = C.sb("prev_b", [128, D], BF16)
    t1_r = C.ring("sb", "t1", 2, [128, D], F32)
    t2_r = C.ring("sb", "t2", 2, [128, D], F32)
    junk, r_junk = C.sb("junkB", [128, 256], F32)
    ob_r = C.ring("sb", "ob", 2, [128, D], BF16)
    obT = C.ring("sb", "obT", 2, [128, 8, 128], BF16)

    def bc3(ap2, n):
        return ap2.unsqueeze(2).to_broadcast([128, 16, n])

    def v3(t, n=64):
        return t.rearrange("p (h q) -> p h q", q=n)

    def load_chunk(b, t, slot):
        tok0 = b * SEQ + t * 128
        x_t, r_x = xh[slot]
        if t == 0:
            S.op("pool", lambda e: e.memset(x_t[:, :, 0:3], 0.0), writes=[r_x])
            S.dma("sp", x_t[:, :, 3:131], xbcT_d[:, :, tok0:tok0 + 128].rearrange("c p t -> p c t"), reads=[r_xbcT], awrites=[r_x])
        else:
            S.dma("sp", x_t[:, :, :], xbcT_d[:, :, tok0 - 3:tok0 + 128].rearrange("c p t -> p c t"), reads=[r_xbcT], writes=[r_x])
        S.dma("sp", dtl[slot][0][:], dt_d[tok0:tok0 + 128, :], reads=[r_dt], writes=[dtl[slot][1]])
        S.dma("sp", zl[slot][0][:], zs_d[tok0:tok0 + 128, :], reads=[r_zs], writes=[zl[slot][1]])

    nch = NB * TPS

    def chunk_gen(ci):
        b, t = divmod(ci, TPS)
        slot = ci % 4
        sl2 = ci % 2
        tok0 = b * SEQ + t * 128
        if ci + 2 < nch:
            load_chunk((ci + 2) // TPS, (ci + 2) % TPS, (ci + 2) % 4)
        x_t, r_x = xh[slot]
        d_t, r_d = dtl[slot]
        z_t, r_z = zl[slot]
        xact, r_xact = xact_r[sl2]; xs_tok, r_xs = xs_r[sl2]; B_tok, r_Bt = Bt_r[sl2]; adt, r_adt = adt_r[sl2]; sm, r_sm = sm_r[sl2]
        Amat, r_A = A_r[sl2]; Lt, r_Lt = Lt_r[sl2]; Mt, r_Mt = Mt_r[sl2]; xdt, r_xdt = xdt_r[sl2]; xdd, r_xdd = xdd_r[sl2]
        t1, r_t1 = t1_r[sl2]; t2, r_t2 = t2_r[sl2]; ob, r_ob = ob_r[sl2]
        for jg in range(4):
            pc, r_pc = bank[jg % 2]
            for jj in range(4):
                j = jg * 4 + jj
                for k in range(4):
                    S.op("pe", lambda e: e.matmul(pc[:, jj * 128:(jj + 1) * 128], lhsT=dg[:, j, k, :], rhs=x_t[:, j, k:k + 128], start=(k == 0), stop=(k == 3)),
                         reads=[r_dg, r_x], writes=[r_pc] if (jj == 0 and k == 0) else (), awrites=() if (jj == 0 and k == 0) else [r_pc])
                    yield
            for jj in range(4):
                j = jg * 4 + jj
                S.op("act", lambda e: e.activation(out=xact[:, j, :], in_=pc[:, jj * 128:(jj + 1) * 128], func=AF.Silu, bias=convb[:, j:j + 1]),
                     reads=[r_pc, r_convb], writes=[r_xact] if j == 0 else (), awrites=() if j == 0 else [r_xact])
                yield
        pxs, r_pxs = bank[2]
        pB, r_pB = bank[3]
        pxs_b = pxs[:].bitcast(BF16)
        pB_b = pB[:].bitcast(BF16)
        for k in range(8):
            S.op("pe", lambda e: e.transpose(out=pxs_b[:, k * 128:(k + 1) * 128], in_=xact[:, k, :], identity=ident_b[:]),
                 reads=[r_xact, r_identb], writes=[r_pxs] if k == 0 else (), awrites=() if k == 0 else [r_pxs])
            yield
        for k in range(4):
            S.op("pe", lambda e: e.transpose(out=pB_b[:, k * 128:(k + 1) * 128], in_=xact[:, 8 + k, :], identity=ident_b[:]),
                 reads=[r_xact, r_identb], writes=[r_pB] if k == 0 else (), awrites=() if k == 0 else [r_pB])
            yield
        S.op("act", lambda e: e.activation(out=xs_tok[:], in_=pxs_b, func=AF.Copy), reads=[r_pxs], writes=[r_xs])
        yield
        S.op("act", lambda e: e.activation(out=B_tok[:], in_=pB_b[:, 0:512], func=AF.Copy), reads=[r_pB], writes=[r_Bt])
        yield
        S.op("dve", lambda e: e.tensor_tensor(out=adt[:], in0=d_t[:], in1=a_bc[:], op=ALU.mult), reads=[r_d, r_abc], writes=[r_adt])
        yield
        S.op("pe", lambda e: e.matmul(pB[:, 256:272], lhsT=triU[:], rhs=adt[:], start=True, stop=True), reads=[r_triU, r_adt], awrites=[r_pB])
        yield
        S.op("pe", lambda e: e.matmul(pB[:, 272:288], lhsT=onesf[:], rhs=adt[:], start=True, stop=True), reads=[r_onesf, r_adt], awrites=[r_pB])
        yield
        S.op("act", lambda e: e.activation(out=sm[:, 0, :], in_=pB[:, 256:272], func=AF.Exp), reads=[r_pB], writes=[r_sm])
        yield
        S.op("act", lambda e: e.activation(out=sm[:, 1, :], in_=pB[:, 272:288], func=AF.Copy), reads=[r_pB], awrites=[r_sm])
        yield
        S.op("act", lambda e: e.activation(out=sm[:, 2, :], in_=pB[:, 272:288], func=AF.Exp), reads=[r_pB], awrites=[r_sm])
        yield
        S.op("dve", lambda e: e.tensor_tensor(out=sm[:, 3, :], in0=sm[:, 1, :], in1=pB[:, 256:272], op=ALU.subtract), reads=[r_sm, r_pB], awrites=[r_sm])
        yield
        S.op("act", lambda e: e.activation(out=sm[:, 4, :], in_=sm[:, 3, :], func=AF.Exp), reads=[r_sm], awrites=[r_sm])
        yield
        S.op("dve", lambda e: e.tensor_tensor(out=Amat[:], in0=triU[:].unsqueeze(1).to_broadcast([128, 16, 128]), in1=bc3(adt[:], 128), op=ALU.mult),
             reads=[r_triU, r_adt], writes=[r_A])
        yield
        pCB, r_pCB = bank[4]
        for g in range(4):
            S.op("pe", lambda e: e.matmul(pCB[:, g * 128:(g + 1) * 128], lhsT=xact[:, 8 + g, :], rhs=xact[:, 12 + g, :], start=True, stop=True),
                 reads=[r_xact], writes=[r_pCB] if g == 0 else (), awrites=() if g == 0 else [r_pCB])
            yield
        S.op("dve", lambda e: e.tensor_tensor(out=v3(xdt[:]), in0=v3(xs_tok[:]), in1=bc3(d_t[:], 64), op=ALU.mult), reads=[r_xs, r_d], writes=[r_xdt])
        yield
        S.op("pool", lambda e: e.tensor_tensor(out=v3(xdd[:]), in0=v3(xdt[:]), in1=bc3(sm[:, 4, :], 64), op=ALU.mult), reads=[r_xdt, r_sm], writes=[r_xdd])
        yield
        for g in range(4):
            pD, r_pD = bank[5 + g % 2]
            S.op("pe", lambda e: e.matmul(pD[:], lhsT=SLm[:], rhs=Amat[:, 4 * g:4 * g + 4, :], start=True, stop=False), reads=[r_SL, r_A], writes=[r_pD])
            yield
            S.op("pe", lambda e: e.matmul(pD[:], lhsT=ident_b[:], rhs=negm4[:], start=False, stop=True), reads=[r_identb, r_negm4], awrites=[r_pD])
            yield
            S.op("act", lambda e: e.activation(out=Lt[:, g % 2, :], in_=pD[:], func=AF.Exp), reads=[r_pD], writes=[r_Lt] if g % 2 == 0 else (), awrites=() if g % 2 == 0 else [r_Lt])
            yield
            S.op("dve", lambda e: e.tensor_tensor(out=Mt[:, 4 * g:4 * g + 4, :], in0=Lt[:, g % 2, :].rearrange("p (h l) -> p h l", h=4),
                                                 in1=pCB[:, g * 128:(g + 1) * 128].unsqueeze(1).to_broadcast([128, 4, 128]), op=ALU.mult),
                 reads=[r_Lt, r_pCB], writes=[r_Mt] if g == 0 else (), awrites=() if g == 0 else [r_Mt])
            yield
        if t == 0:
            S.op("pool", lambda e: e.memset(prev_f[:], 0.0), writes=[r_pf])
            yield
            S.op("pool", lambda e: e.memset(prev_b[:], 0.0), writes=[r_pb])
            yield
        for hh in range(2):
            pY, r_pY = bank[5]
            pO, r_pO = bank[6]
            pS, r_pS = bank[7]
            c0 = hh * 512
            for h8 in range(8):
                h = hh * 8 + h8
                S.op("pe", lambda e: e.matmul(pY[:, h8 * 64:(h8 + 1) * 64], lhsT=Mt[:, h, :], rhs=xdt[:, h * 64:(h + 1) * 64], start=True, stop=True),
                     reads=[r_Mt, r_xdt], writes=[r_pY] if h8 == 0 else (), awrites=() if h8 == 0 else [r_pY])
                yield
            for g2 in range(2):
                g = hh * 2 + g2
                S.op("pe", lambda e: e.matmul(pO[:, g2 * 256:(g2 + 1) * 256], lhsT=xact[:, 12 + g, :], rhs=prev_b[:, g * 256:(g + 1) * 256], start=True, stop=True),
                     reads=[r_xact, r_pb], writes=[r_pO] if g2 == 0 else (), awrites=() if g2 == 0 else [r_pO])
                yield
            for g2 in range(2):
                g = hh * 2 + g2
                S.op("pe", lambda e: e.matmul(pS[:, g2 * 256:(g2 + 1) * 256], lhsT=B_tok[:, g * 128:(g + 1) * 128], rhs=xdd[:, g * 256:(g + 1) * 256], start=True, stop=True),
                     reads=[r_Bt, r_xdd], writes=[r_pS] if g2 == 0 else (), awrites=() if g2 == 0 else [r_pS])
                yield
            hs = slice(hh * 8, hh * 8 + 8)

            def v8(ap):
                return ap.rearrange("p (h q) -> p h q", q=64)
            ex8 = sm[:, 0, hs].unsqueeze(2).to_broadcast([128, 8, 64])
            cd8 = sm[:, 2, hs].unsqueeze(2).to_broadcast([128, 8, 64])
            ds8 = dskip_bc[:, hs].unsqueeze(2).to_broadcast([128, 8, 64])
            S.op("dve", lambda e: e.tensor_tensor(out=v8(t1[:, c0:c0 + 512]), in0=v8(pO[:]), in1=ex8, op=ALU.mult), reads=[r_pO, r_sm], writes=[r_t1] if hh == 0 else (), awrites=() if hh == 0 else [r_t1])
            yield
            S.op("dve", lambda e: e.tensor_tensor(out=t1[:, c0:c0 + 512], in0=t1[:, c0:c0 + 512], in1=pY[:], op=ALU.add), reads=[r_t1, r_pY], awrites=[r_t1])
            yield
            S.op("pool", lambda e: e.tensor_tensor(out=v8(t2[:, c0:c0 + 512]), in0=v8(xs_tok[:, c0:c0 + 512]), in1=ds8, op=ALU.mult), reads=[r_xs, r_dskip], writes=[r_t2] if hh == 0 else (), awrites=() if hh == 0 else [r_t2])
            yield
            S.op("dve", lambda e: e.tensor_tensor(out=v8(prev_f[:, c0:c0 + 512]), in0=v8(prev_f[:, c0:c0 + 512]), in1=cd8, op=ALU.mult), reads=[r_pf, r_sm], awrites=[r_pf])
            yield
            S.op("dve", lambda e: e.tensor_tensor(out=prev_f[:, c0:c0 + 512], in0=prev_f[:, c0:c0 + 512], in1=pS[:], op=ALU.add), reads=[r_pf, r_pS], awrites=[r_pf])
            yield
            S.op("act", lambda e: e.activation(out=prev_b[:, c0:c0 + 512], in_=prev_f[:, c0:c0 + 512], func=AF.Copy), reads=[r_pf, r_pO], awrites=[r_pb])
            yield
        S.op("pool", lambda e: e.tensor_tensor(out=t1[:], in0=t1[:], in1=t2[:], op=ALU.add), reads=[r_t1, r_t2], writes=[r_t1])
        yield
        S.op("pool", lambda e: e.tensor_tensor(out=t1[:], in0=t1[:], in1=z_t[:], op=ALU.mult), reads=[r_t1, r_z], writes=[r_t1])
        yield
        for g in range(4):
            S.op("act", lambda e: e.activation(out=junk[:], in_=t1[:, g * 256:(g + 1) * 256], func=AF.Square, accum_out=sm[:, 5, g:g + 1]),
                 reads=[r_t1], writes=[r_junk], awrites=[r_sm])
            yield
        S.op("act", lambda e: e.activation(out=sm[:, 5, 4:8], in_=sm[:, 5, 0:4], func=AF.Sqrt, scale=1.0 / 256.0, bias=EPS), reads=[r_sm], awrites=[r_sm])
        yield
        S.op("dve", lambda e: e.reciprocal(out=sm[:, 5, 8:12], in_=sm[:, 5, 4:8]), reads=[r_sm], awrites=[r_sm])
        yield
        S.op("dve", lambda e: e.tensor_tensor(out=t1[:].rearrange("p (g q) -> p g q", g=4), in0=t1[:].rearrange("p (g q) -> p g q", g=4),
                                             in1=sm[:, 5, 8:12].unsqueeze(2).to_broadcast([128, 4, 256]), op=ALU.mult), reads=[r_t1, r_sm], writes=[r_t1])
        yield
        S.op("dve", lambda e: e.tensor_tensor(out=ob[:], in0=t1[:], in1=normw_bc[:], op=ALU.mult), reads=[r_t1, r_normw], writes=[r_ob])
        yield
        pT, r_pT = bank[4]
        pT_b = pT[:].bitcast(BF16)
        for k in range(8):
            S.op("pe", lambda e: e.transpose(out=pT_b[:, k * 128:(k + 1) * 128], in_=ob[:, k * 128:(k + 1) * 128], identity=ident_b[:]),
                 reads=[r_ob, r_identb], writes=[r_pT] if k == 0 else (), awrites=() if k == 0 else [r_pT])
            yield
        o_t, r_o = obT[sl2]
        S.op("act", lambda e: e.activation(out=o_t[:].rearrange("p k t -> p (k t)"), in_=pT_b, func=AF.Copy), reads=[r_pT], writes=[r_o])
        yield
        S.dma("sp", obT_d[:, :, tok0:tok0 + 128].rearrange("c p t -> p c t"), o_t[:], reads=[r_o], awrites=[r_obT])
        yield

    load_chunk(0, 0, 0)
    load_chunk(0, 1, 1)
    interleave((chunk_gen(ci) for ci in range(nch)), B_STAGGER)
    C.pop()


def _t5_bucket_np(rel):
    half, max_exact = 16, 8
    ret = (rel > 0).astype(np.int32) * half
    n = np.abs(rel)
    nf = np.maximum(n, 1).astype(np.float32)
    large = max_exact + (np.log(nf / np.float32(max_exact)) / np.float32(np.log(128.0 / 8.0)) * np.float32(half - max_exact)).astype(np.int32)
    large = np.minimum(large, half - 1)
    return ret + np.where(n < max_exact, n, large)


def _t5_blocks(rel_bias):
    k = np.arange(128)[:, None]
    q = np.arange(128)[None, :]
    out = np.zeros((128, 2, 8, 128), np.float32)
    for dl in range(2):
        bk = _t5_bucket_np((k - 128 * dl) - q)
        for h in range(8):
            out[:, dl, h, :] = rel_bias[bk, h]
    return out


def host_inputs(inputs, core):
    b0 = core * NB
    f = lambda a: np.ascontiguousarray(a, dtype=np.float32)
    c = inputs["c"][b0:b0 + NB]
    m = {
        "x": f(inputs["x"][b0:b0 + NB].reshape(NTOK, D)),
        "cT": f(c.reshape(NB, 8, 128).transpose(2, 1, 0)),
        "w_mod": f(inputs["w_mod"][0]),
        "b_mod": f(inputs["b_mod"][0].reshape(1, -1)),
        "w_in": f(inputs["w_in"][0]),
        "ident": np.eye(128, dtype=np.float32),
        "kv_norm_w": f(inputs["kv_norm_w"][0].reshape(1, -1)),
        "idx_k_norm_w": f(inputs["idx_k_norm_w"][0].reshape(1, -1)),
        "idx_k_norm_b": f(inputs["idx_k_norm_b"][0].reshape(1, -1)),
        "dt_bias": f(inputs["dt_bias"][0].reshape(1, -1)),
        "convw": f(inputs["conv_w"][0].reshape(4, 16, 128).transpose(2, 1, 0)),
        "convb": f(inputs["conv_b"][0].reshape(16, 128).T),
        "a_log": f(inputs["a_log"][0].reshape(1, -1)),
        "d_skip": f(inputs["d_skip"][0].reshape(1, -1)),
        "ssm_norm_w": f(inputs["ssm_norm_w"][0].reshape(1, -1)),
        "w_proj_a": f(inputs["w_proj_a"][0]), "w_proj_b": f(inputs["w_proj_b"][0]), "w_out": f(inputs["w_out"][0]),
        "ln1_g": f(inputs["ln1_g"][0].reshape(1, -1)), "ln1_b": f(inputs["ln1_b"][0].reshape(1, -1)),
        "ln2_g": f(inputs["ln2_g"][0].reshape(1, -1)), "ln2_b": f(inputs["ln2_b"][0].reshape(1, -1)),
        "w_router": f(inputs["w_router"][0]), "b_router": f(inputs["b_router"][0].reshape(1, -1)),
        "w1": f(inputs["w1"][0].reshape(NE * D, 2 * D)), "w2": f(inputs["w2"][0].reshape(NE * D, D)),
        "b1r": f(inputs["b1"][0].reshape(NE, 16, 128).transpose(0, 2, 1).reshape(NE * 128, 16)),
        "b2": f(inputs["b2"][0]),
        "sut": np.triu(np.ones((128, 128), np.float32), 1),
        "thr16": np.tile(512.0 * np.arange(16, dtype=np.float32), NE).reshape(1, -1),
        "bstart": (512.0 * np.arange(NBLK, dtype=np.float32)).reshape(1, -1),
        "kp": (np.arange(8, dtype=np.float32)[None, :] * 128 + np.arange(128, dtype=np.float32)[:, None]),
        "pcol": np.arange(128, dtype=np.float32).reshape(128, 1),
        "sut32": np.triu(np.ones((NE, NE), np.float32), 1),
        "tz": _t5_blocks(f(inputs["rel_bias"])),
        "cfar": f(inputs["rel_bias"][15:16, :]),
        "triU": np.triu(np.ones((128, 128), np.float32)),
        "SL": np.tril(np.ones((128, 128), np.float32), -1),
        "negm4": np.tile(np.tril(np.full((128, 128), NEG, np.float32), -1), (1, 4)),
    }
    return m


def kernel(**inputs):
    nc = build_program()
    in_maps = [host_inputs(inputs, c) for c in range(NCORES)]
    res = run_bass_kernel_spmd(nc, in_maps, core_ids=list(range(NCORES)))
    out = np.stack([np.asarray(r["out"]).reshape(NB, SEQ, D) for r in res.results], 0)
    return out.reshape(NCORES * NB, SEQ, D).astype(np.float32)
```

```python
import numpy as np
import concourse.bass as bass
import concourse.mybir as mybir
from concourse.bass_utils import run_bass_kernel_spmd

F32 = mybir.dt.float32
BF16 = mybir.dt.bfloat16
I32 = mybir.dt.int32
ALU = mybir.AluOpType
AF = mybir.ActivationFunctionType
AX = mybir.AxisListType

NCORES = 8
SEQ = 2048
D = 1024
NB = 4
NTOK = NB * SEQ
NT = NTOK // 128
TPS = SEQ // 128
DIN = 6872
C_Q, C_KV, C_IQ, C_IK, C_IW, C_Z, C_XBC, C_DT, C_GA, C_GB = 0, 1024, 1152, 1664, 1728, 1736, 2760, 4808, 4824, 5848
NE = 32
NBLK = NTOK * 4 // 512 + NE
ALPHA = 2.0 ** 0.25
EPS = 1e-5
NEG = -30000.0

B_STAGGER = 104
ENGS = ("pe", "act", "dve", "pool", "sp")


class Res:
    __slots__ = ("name", "writers", "readers", "dsem", "dcount", "dram")

    def __init__(self, name):
        self.name = name
        self.dram = False
        self.writers = {}
        self.readers = {}
        self.dsem = None
        self.dcount = 0


class Sched:
    def __init__(self, nc):
        self.nc = nc
        self.eng = {"pe": nc.tensor, "act": nc.scalar, "dve": nc.vector,
                    "pool": nc.gpsimd, "sp": nc.sync}
        self.sem = {e: nc.alloc_semaphore("prog_" + e) for e in ENGS}
        self.cnt = {e: 0 for e in ENGS}
        self.waited = {e: {} for e in ENGS}
        self.all_res = []
        self.nwaits = 0
        self.nops = 0
        self.sempool = []

    def retire(self, rs):
        for r in rs:
            if r.dsem is not None:
                self.sempool.append((r.dsem, r.dcount))
                r.dsem = None
            if r in self.all_res:
                self.all_res.remove(r)

    def res(self, name):
        r = Res(name)
        self.all_res.append(r)
        return r

    def _need(self, eng, tok, deps):
        sem, val = tok
        k = sem.num
        if self.waited[eng].get(k, 0) >= val:
            return
        if k not in deps or deps[k][1] < val:
            deps[k] = (sem, val)

    def op(self, eng, fn, reads=(), writes=(), awrites=(), dma=False):
        deps = {}
        mykey = None if dma else eng
        for r in reads:
            for k, tok in r.writers.items():
                if k == mykey and eng == "pe":
                    continue
                self._need(eng, tok, deps)
        for r in writes:
            for k, tok in list(r.writers.items()) + list(r.readers.items()):
                if k == mykey:
                    continue
                self._need(eng, tok, deps)
        for r in awrites:
            for k, tok in r.readers.items():
                if k == mykey:
                    continue
                self._need(eng, tok, deps)
        e = self.eng[eng]
        for k, (sem, val) in deps.items():
            e.wait_ge(sem, val)
            self.waited[eng][k] = val
            self.nwaits += 1
        ins = fn(e)
        self.nops += 1
        if dma:
            dst = (list(writes) + list(awrites))[0]
            if dst.dram:
                sb = [r for r in reads if not r.dram]
                if sb:
                    dst = sb[0]
            if dst.dsem is None:
                if self.sempool:
                    dst.dsem, dst.dcount = self.sempool.pop()
                else:
                    dst.dsem = self.nc.alloc_semaphore("d_" + dst.name)
            dst.dcount += 16
            ins.then_inc(dst.dsem, 16)
            tok = (dst.dsem, dst.dcount)
            key = "dma%d" % dst.dsem.num
        else:
            self.cnt[eng] += 1
            ins.then_inc(self.sem[eng], 1)
            tok = (self.sem[eng], self.cnt[eng])
            key = eng
        for r in reads:
            r.readers[key] = tok
        for r in writes:
            r.writers = {key: tok}
            r.readers = {}
        for r in awrites:
            r.writers[key] = tok
        return ins

    def dma(self, eng, out, in_, reads=(), writes=(), awrites=(), **kw):
        return self.op(eng, lambda e: e.dma_start(out=out, in_=in_, **kw),
                       reads=reads, writes=writes, awrites=awrites, dma=True)

    def barrier(self):
        toks = {}
        for e in ENGS:
            if self.cnt[e]:
                toks[self.sem[e].num] = (self.sem[e], self.cnt[e])
        for r in self.all_res:
            if r.dsem is not None and r.dcount:
                toks[r.dsem.num] = (r.dsem, r.dcount)
        for e in ENGS:
            for k, (sem, val) in toks.items():
                if self.waited[e].get(k, 0) >= val:
                    continue
                self.eng[e].wait_ge(sem, val)
                self.waited[e][k] = val
                self.nwaits += 1
        for r in self.all_res:
            r.writers = {}
            r.readers = {}


class Ctx:
    def __init__(self, nc, S):
        self.nc = nc
        self.S = S
        self.stack = []

    def push(self):
        self.stack.append([])

    def pop(self):
        self.S.barrier()
        gs = self.stack.pop()
        self.S.retire([r for (_, r) in gs])
        for g, _ in reversed(gs):
            g.__exit__(None, None, None)

    def sb(self, name, shape, dt):
        g = self.nc.sbuf_tensor("s_" + name, list(shape), dt)
        t = g.__enter__()
        r = self.S.res(name)
        self.stack[-1].append((g, r))
        return t, r

    def ps(self, name, shape, dt=F32):
        g = self.nc.psum_tensor("p_" + name, list(shape), dt)
        t = g.__enter__()
        r = self.S.res(name)
        self.stack[-1].append((g, r))
        return t, r

    def ring(self, kind, name, n, shape, dt):
        f = self.sb if kind == "sb" else self.ps
        return [f("%s%d" % (name, i), shape, dt) for i in range(n)]


def interleave(gens, stagger):
    active = []
    it = iter(gens)
    nxt = next(it, None)
    tick = 0
    while active or nxt is not None:
        if nxt is not None and tick % stagger == 0:
            active.append(nxt)
            nxt = next(it, None)
        for g in list(active):
            try:
                next(g)
            except StopIteration:
                active.remove(g)
        tick += 1


def build_program(debug=()):
    nc = bass.Bass("TRN2", target_bir_lowering=False)
    S = Sched(nc)
    C = Ctx(nc, S)
    dbg = set(debug)

    def din(name, shape, dt=F32):
        return nc.dram_tensor(name, list(shape), dt, kind="ExternalInput").ap()

    def scratch(name, shape, dt):
        kind = "ExternalOutput" if name in dbg else "Internal"
        r = S.res(name)
        r.dram = True
        return nc.dram_tensor(name, list(shape), dt, kind=kind).ap(), r

    x_d = din("x", [NTOK, D])
    cT_d = din("cT", [128, 8, NB])
    wmod_d = din("w_mod", [D, 6 * D])
    bmod_d = din("b_mod", [1, 6 * D])
    win_d = din("w_in", [D, DIN])
    ident_d = din("ident", [128, 128])
    kvw_d = din("kv_norm_w", [1, 128])
    ikw_d = din("idx_k_norm_w", [1, 64])
    ikb_d = din("idx_k_norm_b", [1, 64])
    dtb_d = din("dt_bias", [1, 16])
    convw_d = din("convw", [128, 16, 4])
    convb_d = din("convb", [128, 16])
    alog_d = din("a_log", [1, 16])
    dskip_d = din("d_skip", [1, 16])
    normw_d = din("ssm_norm_w", [1, D])
    triU_d = din("triU", [128, 128])
    SL_d = din("SL", [128, 128])
    negm4_d = din("negm4", [128, 512])
    tz_d = din("tz", [128, 2, 8, 128])
    cfar_d = din("cfar", [1, 8])
    wpa_d = din("w_proj_a", [D, D]); wpb_d = din("w_proj_b", [D, D]); wout_d = din("w_out", [D, D])
    ln1g_d = din("ln1_g", [1, D]); ln1b_d = din("ln1_b", [1, D]); ln2g_d = din("ln2_g", [1, D]); ln2b_d = din("ln2_b", [1, D])
    wr_d = din("w_router", [D, NE]); br_d = din("b_router", [1, NE])
    w1_d = din("w1", [NE * D, 2 * D]); w2_d = din("w2", [NE * D, D])
    b1r_d = din("b1r", [NE * 128, 16]); b2_d = din("b2", [NE, D])
    sut_d = din("sut", [128, 128]); thr16_d = din("thr16", [1, NE * 16]); bstart_d = din("bstart", [1, NBLK])
    kp_d = din("kp", [128, 8]); pcol_d = din("pcol", [128, 1]); sut32_d = din("sut32", [NE, NE])
    r_in = S.res("inputs")
    r_in.dram = True
    out_d = nc.dram_tensor("out", [NTOK, D], F32, kind="ExternalOutput").ap()
    r_out = S.res("out")
    r_out.dram = True

    mod_d, r_mod = scratch("mod_s", [NB, 6 * D], F32)
    qT_d, r_qT = scratch("qT_s", [8, 128, NTOK], BF16)
    iqT_d, r_iqT = scratch("iqT_s", [4, 128, NTOK], BF16)
    xbcT_d, r_xbcT = scratch("xbcT_s", [16, 128, NTOK], BF16)
    sgT_d, r_sgT = scratch("sgT_s", [16, 128, NTOK], BF16)
    kva_d, r_kva = scratch("kva_s", [NTOK, 136], BF16)
    kvT_d, r_kvT = scratch("kvT_s", [128, NTOK], BF16)
    ikT_d, r_ikT = scratch("ikT_s", [128, NTOK], BF16)
    iw_d, r_iw = scratch("iw_s", [NTOK, 8], F32)
    dt_d, r_dt = scratch("dt_s", [NTOK, 16], F32)
    zs_d, r_zs = scratch("zs_s", [NTOK, D], BF16)
    obT_d, r_obT = scratch("obT_s", [8, 128, NTOK], BF16)
    oaT_d, r_oaT = scratch("oaT_s", [8, 128, NTOK], BF16)
    mT_d, r_mTd = scratch("mT_s", [8, 128, NTOK], BF16)
    x1_d, r_x1 = scratch("x1_s", [NTOK, D], F32)
    u2_d, r_u2 = scratch("u2_s", [NTOK, D], BF16)
    xs_d, r_xsd = scratch("xsort_s", [NBLK * 512, D], BF16)
    ys_d, r_ysd = scratch("ysort_s", [NBLK * 512, D], BF16)

    w1b_d, r_w1bd = scratch("w1b_s", [NE * D, 2 * D], BF16)
    w2b_d, r_w2bd = scratch("w2b_s", [NE * D, D], BF16)

    C.push()
    ident_f, r_identf = C.sb("ident_f", [128, 128], F32)
    ident_b, r_identb = C.sb("ident_b", [128, 128], BF16)
    S.dma("sp", ident_f[:], ident_d, reads=[r_in], writes=[r_identf])
    S.op("dve", lambda e: e.tensor_copy(out=ident_b[:], in_=ident_f[:]), reads=[r_identf], writes=[r_identb])
    modT, r_modT = C.sb("modT", [128, 48, NB], F32)

    C.push()
    cT, r_cT = C.sb("cT", [128, 8, NB], F32)
    ones1, r_ones1 = C.sb("ones1", [1, NB], F32)
    bmod, r_bmod = C.sb("bmod", [1, 6 * D], F32)
    modrow, r_modrow = C.sb("modrow", [NB, 6 * D], F32)
    wm = C.ring("sb", "wm", 2, [128, 8, 512], F32)
    pmod = C.ring("ps", "pmod", 2, [NB, 512], F32)
    S.dma("sp", cT[:], cT_d, reads=[r_in], writes=[r_cT])
    S.dma("sp", bmod[:], bmod_d, reads=[r_in], writes=[r_bmod])
    S.op("act", lambda e: e.activation(out=cT[:], in_=cT[:], func=AF.Silu), reads=[r_cT], writes=[r_cT])
    S.op("dve", lambda e: e.memset(ones1[:], 1.0), writes=[r_ones1])
    for g in range(12):
        wt, r_wt = wm[g % 2]
        pt, r_pt = pmod[g % 2]
        S.dma("sp", wt[:], wmod_d[:, g * 512:(g + 1) * 512].rearrange("(k p) n -> p k n", p=128), reads=[r_in], writes=[r_wt])
        for k in range(8):
            S.op("pe", lambda e: e.matmul(pt[:], lhsT=cT[:, k, :], rhs=wt[:, k, :], start=(k == 0), stop=False),
                 reads=[r_cT, r_wt], writes=[r_pt] if k == 0 else (), awrites=() if k == 0 else [r_pt])
        S.op("pe", lambda e: e.matmul(pt[:], lhsT=ones1[:], rhs=bmod[:, g * 512:(g + 1) * 512], start=False, stop=True),
             reads=[r_ones1, r_bmod], awrites=[r_pt])
        S.op("act", lambda e: e.activation(out=modrow[:, g * 512:(g + 1) * 512], in_=pt[:], func=AF.Copy), reads=[r_pt], awrites=[r_modrow])
    S.dma("sp", mod_d, modrow[:], reads=[r_modrow], writes=[r_mod])
    pmt, r_pmt = pmod[0]
    pmt2, r_pmt2 = C.ps("pmodT", [128, 48 * NB], F32)
    for j in range(48):
        S.op("pe", lambda e: e.transpose(out=pmt2[:, j * NB:(j + 1) * NB], in_=modrow[:, j * 128:(j + 1) * 128], identity=ident_f[0:NB, 0:NB]),
             reads=[r_modrow, r_identf], writes=[r_pmt2] if j == 0 else (), awrites=() if j == 0 else [r_pmt2])
    S.op("act", lambda e: e.activation(out=modT[:].rearrange("p j b -> p (j b)"), in_=pmt2[:], func=AF.Copy), reads=[r_pmt2], writes=[r_modT])
    S.op("dve", lambda e: e.tensor_scalar_add(out=modT[:, 8:16, :], in0=modT[:, 8:16, :], scalar1=1.0), reads=[r_modT], awrites=[r_modT])
    S.op("dve", lambda e: e.tensor_scalar_add(out=modT[:, 32:40, :], in0=modT[:, 32:40, :], scalar1=1.0), reads=[r_modT], awrites=[r_modT])
    C.pop()
    if "stop0" in dbg:
        C.pop()
        return nc

    C.push()
    wI, r_wI = C.sb("wI", [128, 8, DIN], BF16)
    for i, (a, b_) in enumerate([(0, 1024), (1024, 1736), (1736, 2760), (2760, 3784), (3784, 4808), (4808, 5848), (5848, 6872)]):
        S.dma("pool", wI[:, :, a:b_], win_d[:, a:b_].rearrange("(k p) n -> p k n", p=128), reads=[r_in],
              writes=[r_wI] if i == 0 else (), awrites=() if i == 0 else [r_wI])
    kvw_bc, r_kvw = C.sb("kvw_bc", [128, 128], F32)
    ikw_bc, r_ikw = C.sb("ikw_bc", [128, 64], F32)
    ikb_bc, r_ikb = C.sb("ikb_bc", [128, 64], F32)
    dtb_bc, r_dtb = C.sb("dtb_bc", [128, 16], F32)
    S.dma("sp", kvw_bc[:], kvw_d.partition_broadcast(128), reads=[r_in], writes=[r_kvw])
    S.dma("sp", ikw_bc[:], ikw_d.partition_broadcast(128), reads=[r_in], writes=[r_ikw])
    S.dma("sp", ikb_bc[:], ikb_d.partition_broadcast(128), reads=[r_in], writes=[r_ikb])
    S.dma("sp", dtb_bc[:], dtb_d.partition_broadcast(128), reads=[r_in], writes=[r_dtb])

    xt = C.ring("sb", "xt", 2, [128, D], F32)
    xn = C.ring("sb", "xn", 2, [128, D], BF16)
    st = C.ring("sb", "st", 2, [128, 2, 6], F32)
    mv = C.ring("sb", "mv", 2, [128, 2], F32)
    rs = C.ring("sb", "rs", 2, [128, 1], F32)
    uT = C.ring("sb", "uT", 2, [128, 8, 512], BF16)
    ptr = C.ring("ps", "ptr", 1, [128, 8, 128], BF16)
    psm = C.ring("ps", "psm", 1, [128, 512], F32)
    pz = C.ring("ps", "pz", 2, [128, 512], F32)
    pf = C.ring("ps", "pf", 3, [128, 512], F32)
    pt2 = C.ring("ps", "pt2", 1, [128, 2, 128], BF16)
    kva = C.ring("sb", "kva", 2, [128, 136], BF16)
    ikn = C.ring("sb", "ikn", 2, [128, 128], BF16)
    sml = C.ring("sb", "sml", 2, [128, 64], F32)
    ikf = C.ring("sb", "ikf", 2, [128, 64], F32)
    iwt = C.ring("sb", "iwt", 2, [128, 8], F32)
    dtt = C.ring("sb", "dtt", 2, [128, 4, 16], F32)
    zst = C.ring("sb", "zst", 2, [128, D], BF16)
    tT = C.ring("sb", "tT", 2, [128, 2, 128], BF16)
    stg = C.ring("sb", "stg", 2, [128, 8, 512], BF16)
    for i in range(2):
        S.op("pool", lambda e: e.memset(kva[i][0][:, 128:136], 1.0), writes=[kva[i][1]])

    def ln_stats(src, r_src, i):
        st_t, r_st = st[i]
        mv_t, r_mv = mv[i]
        rs_t, r_rs = rs[i]
        for j in range(2):
            S.op("dve", lambda e: e.bn_stats(out=st_t[:, j, :], in_=src[:, j * 512:(j + 1) * 512]), reads=[r_src],
                 writes=[r_st] if j == 0 else (), awrites=() if j == 0 else [r_st])
        S.op("dve", lambda e: e.bn_aggr(out=mv_t[:], in_=st_t[:].rearrange("p a b -> p (a b)")), reads=[r_st], writes=[r_mv])
        S.op("act", lambda e: e.activation(out=rs_t[:], in_=mv_t[:, 1:2], func=AF.Sqrt, bias=EPS), reads=[r_mv], writes=[r_rs])
        S.op("dve", lambda e: e.reciprocal(out=rs_t[:], in_=rs_t[:]), reads=[r_rs], writes=[r_rs])
        return mv_t, r_mv, rs_t, r_rs

    tcount = 0
    for g in range(NTOK // 512):
        b = (g * 512) // SEQ
        u_t, r_u = uT[g % 2]
        for i4 in range(4):
            t = g * 4 + i4
            tok0 = t * 128
            ri = tcount % 2
            tcount += 1
            x_t, r_x = xt[ri]
            xn_t, r_xn = xn[ri]
            if t == 0:
                S.dma("sp", x_t[:], x_d[0:128, :], reads=[r_in], writes=[r_x])
            if t + 1 < NT:
                S.dma("sp", xt[(ri + 1) % 2][0][:], x_d[tok0 + 128:tok0 + 256, :], reads=[r_in], writes=[xt[(ri + 1) % 2][1]])
            mv_t, r_mv, rs_t, r_rs = ln_stats(x_t, r_x, ri)
            S.op("dve", lambda e: e.tensor_scalar(out=xn_t[:], in0=x_t[:], scalar1=mv_t[:, 0:1], scalar2=rs_t[:], op0=ALU.subtract, op1=ALU.mult),
                 reads=[r_x, r_mv, r_rs], writes=[r_xn])
            p_t, r_p = ptr[0]
            for k in range(8):
                S.op("pe", lambda e: e.transpose(out=p_t[:, k, :], in_=xn_t[:, k * 128:(k + 1) * 128], identity=ident_b[:]),
                     reads=[r_xn, r_identb], writes=[r_p] if k == 0 else (), awrites=() if k == 0 else [r_p])
            for k in range(8):
                S.op("act", lambda e: e.activation(out=u_t[:, k, i4 * 128:(i4 + 1) * 128], in_=p_t[:, k, :], func=AF.Identity,
                                                   scale=modT[:, 8 + k, b:b + 1], bias=modT[:, k, b:b + 1]),
                     reads=[r_p, r_modT], writes=[r_u] if (k == 0 and i4 == 0) else (), awrites=() if (k == 0 and i4 == 0) else [r_u])
            ps_t, r_ps = psm[0]
            for (c0, c1, o0) in [(C_KV, C_KV + 128, 0), (C_IK, C_IK + 72, 128), (C_DT, C_DT + 16, 200)]:
                for k in range(8):
                    S.op("pe", lambda e: e.matmul(ps_t[:, o0:o0 + (c1 - c0)], lhsT=u_t[:, k, i4 * 128:(i4 + 1) * 128], rhs=wI[:, k, c0:c1], start=(k == 0), stop=(k == 7)),
                         reads=[r_u, r_wI], writes=[r_ps] if (k == 0 and o0 == 0) else (), awrites=() if (k == 0 and o0 == 0) else [r_ps])
            zp = []
            for h in range(2):
                pz_t, r_pz = pz[h]
                zp.append((pz_t, r_pz))
                for k in range(8):
                    S.op("pe", lambda e: e.matmul(pz_t[:], lhsT=u_t[:, k, i4 * 128:(i4 + 1) * 128], rhs=wI[:, k, C_Z + h * 512:C_Z + (h + 1) * 512], start=(k == 0), stop=(k == 7)),
                         reads=[r_u, r_wI], writes=[r_pz] if k == 0 else (), awrites=() if k == 0 else [r_pz])
            sm_t, r_sm = sml[ri]
            kva_t, r_kva_t = kva[ri]
            ikf_t, r_ikf = ikf[ri]
            S.op("act", lambda e: e.activation(out=ikf_t[:, 0:64], in_=ps_t[:, 0:64], func=AF.Square, accum_out=sm_t[:, 0:1]), reads=[r_ps], writes=[r_ikf, r_sm])
            S.op("act", lambda e: e.activation(out=ikf_t[:, 0:64], in_=ps_t[:, 64:128], func=AF.Square, accum_out=sm_t[:, 1:2]), reads=[r_ps], writes=[r_ikf], awrites=[r_sm])
            S.op("dve", lambda e: e.tensor_tensor(out=sm_t[:, 0:1], in0=sm_t[:, 0:1], in1=sm_t[:, 1:2], op=ALU.add), reads=[r_sm], awrites=[r_sm])
            S.op("act", lambda e: e.activation(out=sm_t[:, 2:3], in_=sm_t[:, 0:1], func=AF.Sqrt, scale=1.0 / 128.0, bias=EPS), reads=[r_sm], awrites=[r_sm])
            S.op("dve", lambda e: e.reciprocal(out=sm_t[:, 3:4], in_=sm_t[:, 2:3]), reads=[r_sm], awrites=[r_sm])
            S.op("dve", lambda e: e.scalar_tensor_tensor(out=kva_t[:, 0:128], in0=ps_t[:, 0:128], scalar=sm_t[:, 3:4], in1=kvw_bc[:], op0=ALU.mult, op1=ALU.mult),
                 reads=[r_ps, r_sm, r_kvw], awrites=[r_kva_t])
            S.dma("sp", kva_d[tok0:tok0 + 128, :], kva_t[:], reads=[r_kva_t], awrites=[r_kva])
            ik_t, r_ik = ikn[ri]
            S.op("dve", lambda e: e.bn_stats(out=sm_t[:, 8:14], in_=ps_t[:, 128:192]), reads=[r_ps], awrites=[r_sm])
            S.op("dve", lambda e: e.bn_aggr(out=sm_t[:, 16:18], in_=sm_t[:, 8:14]), reads=[r_sm], awrites=[r_sm])
            S.op("act", lambda e: e.activation(out=sm_t[:, 18:19], in_=sm_t[:, 17:18], func=AF.Sqrt, bias=EPS), reads=[r_sm], awrites=[r_sm])
            S.op("dve", lambda e: e.reciprocal(out=sm_t[:, 19:20], in_=sm_t[:, 18:19]), reads=[r_sm], awrites=[r_sm])
            S.op("dve", lambda e: e.tensor_scalar(out=ikf_t[:], in0=ps_t[:, 128:192], scalar1=sm_t[:, 16:17], scalar2=sm_t[:, 19:20], op0=ALU.subtract, op1=ALU.mult),
                 reads=[r_ps, r_sm], writes=[r_ikf])
            S.op("dve", lambda e: e.tensor_tensor(out=ikf_t[:], in0=ikf_t[:], in1=ikw_bc[:], op=ALU.mult), reads=[r_ikf, r_ikw], writes=[r_ikf])
            S.op("dve", lambda e: e.tensor_tensor(out=ik_t[:, 0:64], in0=ikf_t[:], in1=ikb_bc[:], op=ALU.add), reads=[r_ikf, r_ikb], writes=[r_ik])
            S.op("dve", lambda e: e.tensor_copy(out=ik_t[:, 64:128], in_=ik_t[:, 0:64]), reads=[r_ik], awrites=[r_ik])
            iw_t, r_iwt = iwt[ri]
            S.op("act", lambda e: e.mul(out=iw_t[:], in_=ps_t[:, 192:200], mul=float(8 ** -0.5 * 64 ** -0.5)), reads=[r_ps], writes=[r_iwt])
            S.dma("sp", iw_d[tok0:tok0 + 128, :], iw_t[:], reads=[r_iwt], awrites=[r_iw])
            d_t, r_d = dtt[ri]
            S.op("dve", lambda e: e.tensor_tensor(out=d_t[:, 0, :], in0=ps_t[:, 200:216], in1=dtb_bc[:], op=ALU.add), reads=[r_ps, r_dtb], writes=[r_d])
            S.op("act", lambda e: e.activation(out=d_t[:, 1, :], in_=d_t[:, 0, :], func=AF.Abs), reads=[r_d], awrites=[r_d])
            S.op("act", lambda e: e.activation(out=d_t[:, 1, :], in_=d_t[:, 1, :], func=AF.Exp, scale=-1.0), reads=[r_d], awrites=[r_d])
            S.op("act", lambda e: e.activation(out=d_t[:, 1, :], in_=d_t[:, 1, :], func=AF.Ln, bias=1.0), reads=[r_d], awrites=[r_d])
            S.op("dve", lambda e: e.scalar_tensor_tensor(out=d_t[:, 2, :], in0=d_t[:, 0, :], scalar=0.0, in1=d_t[:, 1, :], op0=ALU.max, op1=ALU.add), reads=[r_d], awrites=[r_d])
            S.dma("sp", dt_d[tok0:tok0 + 128, :], d_t[:, 2, :], reads=[r_d], awrites=[r_dt])
            z_t, r_z = zst[ri]
            for h in range(2):
                S.op("act", lambda e: e.activation(out=z_t[:, h * 512:(h + 1) * 512], in_=zp[h][0][:], func=AF.Silu), reads=[zp[h][1]],
                     writes=[r_z] if h == 0 else (), awrites=() if h == 0 else [r_z])
            S.dma("sp", zs_d[tok0:tok0 + 128, :], z_t[:], reads=[r_z], awrites=[r_zs])
            p2, r_p2 = pt2[0]
            t_t, r_t = tT[ri]
            S.op("pe", lambda e: e.transpose(out=p2[:, 0, :], in_=kva_t[:, 0:128], identity=ident_b[:]), reads=[r_kva_t, r_identb], writes=[r_p2])
            S.op("pe", lambda e: e.transpose(out=p2[:, 1, :], in_=ik_t[:], identity=ident_b[:]), reads=[r_ik, r_identb], awrites=[r_p2])
            S.op("dve", lambda e: e.tensor_copy(out=t_t[:], in_=p2[:]), reads=[r_p2], writes=[r_t])
            S.dma("sp", kvT_d[:, tok0:tok0 + 128], t_t[:, 0, :], reads=[r_t], awrites=[r_kvT])
            S.dma("sp", ikT_d[:, tok0:tok0 + 128], t_t[:, 1, :], reads=[r_t], awrites=[r_ikT])
        g0 = g * 512
        fcount = 0
        for (c0, nch, dst, r_dst, ch0, func, scl) in [
                (C_Q, 8, qT_d, r_qT, 0, AF.Copy, float(128 ** -0.5)),
                (C_IQ, 4, iqT_d, r_iqT, 0, AF.Copy, 1.0),
                (C_XBC, 8, xbcT_d, r_xbcT, 0, AF.Copy, 1.0),
                (C_XBC + 1024, 8, xbcT_d, r_xbcT, 8, AF.Copy, 1.0),
                (C_GA, 8, sgT_d, r_sgT, 0, AF.Sigmoid, 1.0),
                (C_GB, 8, sgT_d, r_sgT, 8, AF.Sigmoid, 1.0)]:
            sg_t, r_sg = stg[fcount % 2]
            fcount += 1
            for j in range(nch):
                pf_t, r_pf = pf[j % 3]
                for k in range(8):
                    S.op("pe", lambda e: e.matmul(pf_t[:], lhsT=wI[:, k, c0 + j * 128:c0 + (j + 1) * 128], rhs=u_t[:, k, :], start=(k == 0), stop=(k == 7)),
                         reads=[r_u, r_wI], writes=[r_pf] if k == 0 else (), awrites=() if k == 0 else [r_pf])
                if func == AF.Copy and j % 2 == 1:
                    S.op("dve", lambda e: e.tensor_scalar_mul(out=sg_t[:, j, :], in0=pf_t[:], scalar1=scl), reads=[r_pf],
                         writes=[r_sg] if j == 0 else (), awrites=() if j == 0 else [r_sg])
                else:
                    S.op("act", lambda e: e.activation(out=sg_t[:, j, :], in_=pf_t[:], func=func, scale=scl), reads=[r_pf],
                         writes=[r_sg] if j == 0 else (), awrites=() if j == 0 else [r_sg])
            S.dma("sp", dst[ch0:ch0 + nch, :, g0:g0 + 512].rearrange("c p t -> p c t"), sg_t[:, 0:nch, :], reads=[r_sg], awrites=[r_dst])
    C.pop()
    if "stopA" in dbg:
        C.pop()
        return nc

    phase_B(nc, S, C, dbg, locals())
    if "stopB" in dbg:
        C.pop()
        return nc

    phase_C(nc, S, C, dbg, locals())
    if "stopC" in dbg:
        C.pop()
        return nc

    phase_DEF(nc, S, C, dbg, locals())
    C.pop()
    return nc


def phase_DEF(nc, S, C, dbg, L):
    g_ = lambda n: L[n]
    r_in = g_("r_in"); ident_b = g_("ident_b"); r_identb = g_("r_identb"); ident_f = g_("ident_f"); r_identf = g_("r_identf")
    modT = g_("modT"); r_modT = g_("r_modT"); mod_d = g_("mod_d"); r_mod = g_("r_mod")
    x_d = g_("x_d"); oaT_d, r_oaT = g_("oaT_d"), g_("r_oaT"); obT_d, r_obT = g_("obT_d"), g_("r_obT"); sgT_d, r_sgT = g_("sgT_d"), g_("r_sgT")
    x1_d, r_x1 = g_("x1_d"), g_("r_x1"); u2_d, r_u2 = g_("u2_d"), g_("r_u2"); xs_d, r_xsd = g_("xs_d"), g_("r_xsd"); ys_d, r_ysd = g_("ys_d"), g_("r_ysd")
    out_d, r_out = g_("out_d"), g_("r_out")
    b1r_d, b2_d = g_("b1r_d"), g_("b2_d")
    w1b_d, r_w1bd, w2b_d, r_w2bd = g_("w1b_d"), g_("r_w1bd"), g_("w2b_d"), g_("r_w2bd")

    C.push()
    idx4, r_idx4 = C.sb("idx4", [128, NT, 4], I32)
    g4, r_g4 = C.sb("g4", [128, NT, 4], F32)
    widx, r_widx = C.sb("widx", [128, NBLK, 8], I32)
    bidx, r_bidx = C.sb("bidx", [128, NBLK], I32)
    eidx, r_eidx = C.sb("eidx", [128, NBLK], I32)

    def ln_stats(src, r_src, st_t, r_st, mv_t, r_mv, rs_t, r_rs):
        for j in range(2):
            S.op("dve", lambda e: e.bn_stats(out=st_t[:, j, :], in_=src[:, j * 512:(j + 1) * 512]), reads=[r_src],
                 writes=[r_st] if j == 0 else (), awrites=() if j == 0 else [r_st])
        S.op("dve", lambda e: e.bn_aggr(out=mv_t[:], in_=st_t[:].rearrange("p a b -> p (a b)")), reads=[r_st], writes=[r_mv])
        S.op("act", lambda e: e.activation(out=rs_t[:], in_=mv_t[:, 1:2], func=AF.Sqrt, bias=EPS), reads=[r_mv], writes=[r_rs])
        S.op("dve", lambda e: e.reciprocal(out=rs_t[:], in_=rs_t[:]), reads=[r_rs], writes=[r_rs])

    mT_d, r_mTd = g_("mT_d"), g_("r_mTd")
    C.push()
    wpa, r_wpa = C.sb("wpa", [128, 8, D], BF16)
    wpb, r_wpb = C.sb("wpb", [128, 8, D], BF16)
    S.dma("pool", wpa[:], g_("wpa_d").rearrange("(k p) n -> p k n", p=128), reads=[r_in], writes=[r_wpa])
    S.dma("pool", wpb[:], g_("wpb_d").rearrange("(k p) n -> p k n", p=128), reads=[r_in], writes=[r_wpb])
    oaTr = C.ring("sb", "oaTd", 2, [128, 8, 512], BF16)
    obTr = C.ring("sb", "obTd", 2, [128, 8, 512], BF16)
    sgTr = C.ring("sb", "sgTd", 2, [128, 16, 512], BF16)
    mTr = C.ring("sb", "mT", 2, [128, 8, 512], BF16)
    ta = C.ring("sb", "ta", 3, [128, 512], F32)
    tb = C.ring("sb", "tb", 3, [128, 512], F32)
    pab = C.ring("ps", "pab", 4, [128, 512], F32)
    pbb = C.ring("ps", "pbb", 4, [128, 512], F32)

    def loadD1(g):
        g0 = g * 512
        S.dma("sp", oaTr[g % 2][0][:], oaT_d[:, :, g0:g0 + 512].rearrange("c p t -> p c t"), reads=[r_oaT], writes=[oaTr[g % 2][1]])
        S.dma("sp", obTr[g % 2][0][:], obT_d[:, :, g0:g0 + 512].rearrange("c p t -> p c t"), reads=[r_obT], writes=[obTr[g % 2][1]])
        S.dma("sp", sgTr[g % 2][0][:], sgT_d[:, :, g0:g0 + 512].rearrange("c p t -> p c t"), reads=[r_sgT], writes=[sgTr[g % 2][1]])

    NG = NTOK // 512
    loadD1(0)
    cn = 0
    for g in range(NG):
        g0 = g * 512
        if g + 1 < NG:
            loadD1(g + 1)
        oaT, r_oaTs = oaTr[g % 2]; obT, r_obTs = obTr[g % 2]; sgT, r_sgTs = sgTr[g % 2]; mT, r_mT = mTr[g % 2]
        for n in range(8):
            pa, r_pa = pab[cn % 4]
            pb, r_pb = pbb[cn % 4]
            ta_t, r_ta = ta[cn % 3]
            tb_t, r_tb = tb[cn % 3]
            cn += 1
            for k in range(8):
                S.op("pe", lambda e: e.matmul(pa[:], lhsT=wpa[:, k, n * 128:(n + 1) * 128], rhs=oaT[:, k, :], start=(k == 0), stop=(k == 7)),
                     reads=[r_wpa, r_oaTs], writes=[r_pa] if k == 0 else (), awrites=() if k == 0 else [r_pa])
            for k in range(8):
                S.op("pe", lambda e: e.matmul(pb[:], lhsT=wpb[:, k, n * 128:(n + 1) * 128], rhs=obT[:, k, :], start=(k == 0), stop=(k == 7)),
                     reads=[r_wpb, r_obTs], writes=[r_pb] if k == 0 else (), awrites=() if k == 0 else [r_pb])
            S.op("dve", lambda e: e.tensor_tensor(out=ta_t[:], in0=pa[:], in1=sgT[:, n, :], op=ALU.mult), reads=[r_pa, r_sgTs], writes=[r_ta])
            S.op("dve", lambda e: e.tensor_tensor(out=tb_t[:], in0=pb[:], in1=sgT[:, 8 + n, :], op=ALU.mult), reads=[r_pb, r_sgTs], writes=[r_tb])
            S.op("pool", lambda e: e.tensor_tensor(out=mT[:, n, :], in0=ta_t[:], in1=tb_t[:], op=ALU.add), reads=[r_ta, r_tb],
                 writes=[r_mT] if n == 0 else (), awrites=() if n == 0 else [r_mT])
        S.dma("sp", mT_d[:, :, g0:g0 + 512].rearrange("c p t -> p c t"), mT[:], reads=[r_mT], awrites=[r_mTd])
    C.pop()

    C.push()
    wout, r_wout = C.sb("wout", [128, 8, D], BF16)
    S.dma("pool", wout[:], g_("wout_d").rearrange("(k p) n -> p k n", p=128), reads=[r_in], writes=[r_wout])
    wr, r_wr = C.sb("wr", [128, 8, NE], F32)
    S.dma("sp", wr[:], g_("wr_d").rearrange("(k p) n -> p k n", p=128), reads=[r_in], writes=[r_wr])
    br_bc, r_br = C.sb("br_bc", [128, NE], F32)
    S.dma("sp", br_bc[:], g_("br_d").partition_broadcast(128), reads=[r_in], writes=[r_br])
    ln1g, r_ln1g = C.sb("ln1g", [128, D], F32)
    ln1b, r_ln1b = C.sb("ln1b", [128, D], F32)
    S.dma("sp", ln1g[:], g_("ln1g_d").partition_broadcast(128), reads=[r_in], writes=[r_ln1g])
    S.dma("sp", ln1b[:], g_("ln1b_d").partition_broadcast(128), reads=[r_in], writes=[r_ln1b])
    gater = C.ring("sb", "gate1", 2, [128, D], F32)
    sc2r = C.ring("sb", "sc2", 2, [128, D], F32)
    sh2r = C.ring("sb", "sh2", 2, [128, D], F32)
    sutf, r_sutf = C.sb("sutf", [128, 128], F32)
    sutb, r_sutb = C.sb("sutb", [128, 128], BF16)
    onesb, r_onesb = C.sb("onesb", [128, 128], BF16)
    S.dma("sp", sutf[:], g_("sut_d"), reads=[r_in], writes=[r_sutf])
    S.op("dve", lambda e: e.tensor_copy(out=sutb[:], in_=sutf[:]), reads=[r_sutf], writes=[r_sutb])
    S.op("dve", lambda e: e.memset(onesb[:], 1.0), writes=[r_onesb])
    maskall, r_maskall = C.sb("maskall", [128, NT, NE], F32)
    gall, r_gall = C.sb("gall", [128, NT, NE], F32)
    posall, r_posall = C.sb("posall", [128, NT, NE], F32)
    rrun, r_rrun = C.sb("rrun", [128, NE], F32)
    S.op("dve", lambda e: e.memset(rrun[:], 0.0), writes=[r_rrun])
    mtr = C.ring("sb", "mtl", 4, [128, 8, 128], BF16)
    xt = C.ring("sb", "xtd", 4, [128, D], F32)
    r1_r = C.ring("sb", "r1", 3, [128, D], F32)
    x1t_r = C.ring("sb", "x1t", 3, [128, D], F32)
    u2t_r = C.ring("sb", "u2t", 3, [128, D], F32)
    u2b = C.ring("sb", "u2b", 3, [128, D], BF16)
    u2T_r = C.ring("sb", "u2T", 3, [128, 8, 128], F32)
    st_r = C.ring("sb", "stD", 6, [128, 2, 6], F32)
    mv_r = C.ring("sb", "mvD", 6, [128, 2], F32)
    rs_r = C.ring("sb", "rsD", 6, [128, 1], F32)
    lg_r = C.ring("sb", "lg", 3, [128, NE], F32)
    m8_r = C.ring("sb", "m8D", 3, [128, 8], F32)
    sml_r = C.ring("sb", "smlD", 3, [128, 8], F32)
    ex_r = C.ring("sb", "exD", 3, [128, NE], F32)
    maskb_r = C.ring("sb", "maskb", 3, [128, NE], BF16)
    prs = C.ring("ps", "prs", 4, [128, 512], F32)
    ptT = C.ring("ps", "ptT", 2, [128, 512], F32)
    psl = C.ring("ps", "psl", 2, [128, 512], F32)

    def loadD2(t):
        tok0 = t * 128
        S.dma("sp", xt[t % 4][0][:], x_d[tok0:tok0 + 128, :], reads=[r_in], writes=[xt[t % 4][1]])
        S.dma("sp", mtr[t % 4][0][:], mT_d[:, :, tok0:tok0 + 128].rearrange("c p t -> p c t"), reads=[r_mTd], writes=[mtr[t % 4][1]])
        if t % TPS == 0:
            b = t // TPS
            gate1, r_gate1 = gater[b % 2]; sc2, r_sc2 = sc2r[b % 2]; sh2, r_sh2 = sh2r[b % 2]
            S.dma("sp", gate1[:], mod_d[b:b + 1, 2 * D:3 * D].partition_broadcast(128), reads=[r_mod], writes=[r_gate1])
            S.dma("sp", sh2[:], mod_d[b:b + 1, 3 * D:4 * D].partition_broadcast(128), reads=[r_mod], writes=[r_sh2])
            S.dma("sp", sc2[:], mod_d[b:b + 1, 4 * D:5 * D].partition_broadcast(128), reads=[r_mod], writes=[r_sc2])
            S.op("pool", lambda e: e.tensor_scalar_add(out=sc2[:], in0=sc2[:], scalar1=1.0), reads=[r_sc2], writes=[r_sc2])

    def stage1(t):
        tok0 = t * 128
        b = t // TPS
        gate1, r_gate1 = gater[b % 2]; sc2, r_sc2 = sc2r[b % 2]; sh2, r_sh2 = sh2r[b % 2]
        x_t, r_x = xt[t % 4]
        mt_t, r_mt = mtr[t % 4]
        r1, r_r1 = r1_r[t % 3]; x1t, r_x1t = x1t_r[t % 3]; u2t, r_u2t = u2t_r[t % 3]
        st, r_st = st_r[(2 * t) % 6]; mv, r_mv = mv_r[(2 * t) % 6]; rs, r_rs = rs_r[(2 * t) % 6]
        st2, r_st2 = st_r[(2 * t + 1) % 6]; mv2, r_mv2 = mv_r[(2 * t + 1) % 6]; rs2, r_rs2 = rs_r[(2 * t + 1) % 6]
        for hf in range(2):
            pr_t, r_pr = prs[(2 * t + hf) % 4]
            for n in range(8):
                S.op("pe", lambda e: e.matmul(pr_t[:], lhsT=mt_t[:, n, :], rhs=wout[:, n, hf * 512:(hf + 1) * 512], start=(n == 0), stop=(n == 7)),
                     reads=[r_mt, r_wout], writes=[r_pr] if n == 0 else (), awrites=() if n == 0 else [r_pr])
                yield
            S.op("dve", lambda e: e.tensor_tensor(out=r1[:, hf * 512:(hf + 1) * 512], in0=pr_t[:], in1=gate1[:, hf * 512:(hf + 1) * 512], op=ALU.mult),
                 reads=[r_pr, r_gate1], writes=[r_r1] if hf == 0 else (), awrites=() if hf == 0 else [r_r1])
            yield
        S.op("dve", lambda e: e.scalar_tensor_tensor(out=r1[:], in0=x_t[:], scalar=float(ALPHA), in1=r1[:], op0=ALU.mult, op1=ALU.add), reads=[r_x, r_r1], writes=[r_r1])
        yield
        ln_stats(r1, r_r1, st, r_st, mv, r_mv, rs, r_rs)
        yield
        S.op("dve", lambda e: e.tensor_scalar(out=x1t[:], in0=r1[:], scalar1=mv[:, 0:1], scalar2=rs[:], op0=ALU.subtract, op1=ALU.mult), reads=[r_r1, r_mv, r_rs], writes=[r_x1t])
        yield
        S.op("pool", lambda e: e.tensor_tensor(out=x1t[:], in0=x1t[:], in1=ln1g[:], op=ALU.mult), reads=[r_x1t, r_ln1g], writes=[r_x1t])
        yield
        S.op("pool", lambda e: e.tensor_tensor(out=x1t[:], in0=x1t[:], in1=ln1b[:], op=ALU.add), reads=[r_x1t, r_ln1b], writes=[r_x1t])
        yield
        S.dma("sp", x1_d[tok0:tok0 + 128, :], x1t[:], reads=[r_x1t], awrites=[r_x1])
        yield
        ln_stats(x1t, r_x1t, st2, r_st2, mv2, r_mv2, rs2, r_rs2)
        yield
        S.op("dve", lambda e: e.tensor_scalar(out=u2t[:], in0=x1t[:], scalar1=mv2[:, 0:1], scalar2=rs2[:], op0=ALU.subtract, op1=ALU.mult), reads=[r_x1t, r_mv2, r_rs2], writes=[r_u2t])
        yield
        S.op("pool", lambda e: e.tensor_tensor(out=u2t[:], in0=u2t[:], in1=sc2[:], op=ALU.mult), reads=[r_u2t, r_sc2], writes=[r_u2t])
        yield
        S.op("pool", lambda e: e.tensor_tensor(out=u2t[:], in0=u2t[:], in1=sh2[:], op=ALU.add), reads=[r_u2t, r_sh2], writes=[r_u2t])
        yield
        ub, r_ub = u2b[t % 3]
        S.op("act", lambda e: e.activation(out=ub[:], in_=u2t[:], func=AF.Copy), reads=[r_u2t], writes=[r_ub])
        yield
        S.dma("sp", u2_d[tok0:tok0 + 128, :], ub[:], reads=[r_ub], awrites=[r_u2])
        yield

    def stage2(t):
        u2t, r_u2t = u2t_r[t % 3]
        u2T, r_u2T = u2T_r[t % 3]
        lg, r_lg = lg_r[t % 3]; m8, r_m8 = m8_r[t % 3]; sml, r_sml = sml_r[t % 3]; ex, r_ex = ex_r[t % 3]; maskb, r_maskb = maskb_r[t % 3]
        for hf in range(2):
            pT, r_pT = (ptT[t % 2] if hf == 0 else psl[t % 2])
            for k4 in range(4):
                k = hf * 4 + k4
                S.op("pe", lambda e: e.transpose(out=pT[:, k4 * 128:(k4 + 1) * 128], in_=u2t[:, k * 128:(k + 1) * 128], identity=ident_f[:]),
                     reads=[r_u2t, r_identf], writes=[r_pT] if k4 == 0 else (), awrites=() if k4 == 0 else [r_pT])
                yield
            S.op("act", lambda e: e.activation(out=u2T[:, hf * 4:hf * 4 + 4, :].rearrange("p k t -> p (k t)"), in_=pT[:], func=AF.Copy), reads=[r_pT],
                 writes=[r_u2T] if hf == 0 else (), awrites=() if hf == 0 else [r_u2T])
            yield
        pl_, r_pl = psl[t % 2]
        for k in range(8):
            S.op("pe", lambda e: e.matmul(pl_[:, 0:NE], lhsT=u2T[:, k, :], rhs=wr[:, k, :], start=(k == 0), stop=(k == 7)),
                 reads=[r_u2T, r_wr], writes=[r_pl] if k == 0 else (), awrites=() if k == 0 else [r_pl])
            yield
        S.op("dve", lambda e: e.tensor_tensor(out=lg[:], in0=pl_[:, 0:NE], in1=br_bc[:], op=ALU.add), reads=[r_pl, r_br], writes=[r_lg])
        yield
        S.op("dve", lambda e: e.max(out=m8[:], in_=lg[:]), reads=[r_lg], writes=[r_m8])
        yield
        S.op("dve", lambda e: e.tensor_scalar(out=maskall[:, t, :], in0=lg[:], scalar1=m8[:, 3:4], scalar2=None, op0=ALU.is_ge), reads=[r_lg, r_m8], awrites=[r_maskall])
        yield
        S.op("dve", lambda e: e.tensor_scalar_mul(out=sml[:, 0:1], in0=m8[:, 0:1], scalar1=-1.0), reads=[r_m8], writes=[r_sml])
        yield
        S.op("act", lambda e: e.activation(out=ex[:], in_=lg[:], func=AF.Exp, bias=sml[:, 0:1]), reads=[r_lg, r_sml], writes=[r_ex])
        yield
        S.op("dve", lambda e: e.tensor_tensor(out=ex[:], in0=ex[:], in1=maskall[:, t, :], op=ALU.mult), reads=[r_ex, r_maskall], writes=[r_ex])
        yield
        S.op("dve", lambda e: e.reduce_sum(out=sml[:, 1:2], in_=ex[:], axis=AX.X), reads=[r_ex], awrites=[r_sml])
        yield
        S.op("dve", lambda e: e.reciprocal(out=sml[:, 2:3], in_=sml[:, 1:2]), reads=[r_sml], awrites=[r_sml])
        yield
        S.op("dve", lambda e: e.tensor_scalar(out=gall[:, t, :], in0=ex[:], scalar1=sml[:, 2:3], scalar2=None, op0=ALU.mult), reads=[r_ex, r_sml], awrites=[r_gall])
        yield
        S.op("dve", lambda e: e.tensor_copy(out=maskb[:], in_=maskall[:, t, :]), reads=[r_maskall], writes=[r_maskb])
        yield
        S.op("pe", lambda e: e.matmul(pl_[:, 64:64 + NE], lhsT=sutb[:], rhs=maskb[:], start=True, stop=True), reads=[r_sutb, r_maskb, r_lg], awrites=[r_pl])
        yield
        S.op("pe", lambda e: e.matmul(pl_[:, 128:128 + NE], lhsT=onesb[:], rhs=maskb[:], start=True, stop=True), reads=[r_onesb, r_maskb], awrites=[r_pl])
        yield
        S.op("dve", lambda e: e.tensor_tensor(out=posall[:, t, :], in0=pl_[:, 64:64 + NE], in1=rrun[:], op=ALU.add), reads=[r_pl, r_rrun], awrites=[r_posall])
        yield
        S.op("dve", lambda e: e.tensor_tensor(out=rrun[:], in0=pl_[:, 128:128 + NE], in1=rrun[:], op=ALU.add), reads=[r_pl, r_rrun], writes=[r_rrun])
        yield


    def tile_gen(t):
        if t + 2 < NT:
            loadD2(t + 2)
        yield from stage1(t)
        yield from stage2(t)

    for t0 in range(2):
        loadD2(t0)
    interleave((tile_gen(t) for t in range(NT)), 16)

    thr16, r_thr16 = C.sb("thr16", [128, NE, 16], F32)
    bstart, r_bstart = C.sb("bstart", [128, NBLK], F32)
    kp, r_kp = C.sb("kp", [128, 8], F32)
    pcol, r_pcol = C.sb("pcol", [128, 1], F32)
    sut32, r_sut32 = C.sb("sut32", [NE, NE], F32)
    S.dma("sp", thr16[:].rearrange("p e m -> p (e m)"), g_("thr16_d").partition_broadcast(128), reads=[r_in], writes=[r_thr16])
    S.dma("sp", bstart[:], g_("bstart_d").partition_broadcast(128), reads=[r_in], writes=[r_bstart])
    S.dma("sp", kp[:], g_("kp_d"), reads=[r_in], writes=[r_kp])
    S.dma("sp", pcol[:], g_("pcol_d"), reads=[r_in], writes=[r_pcol])
    S.dma("sp", sut32[:], g_("sut32_d"), reads=[r_in], writes=[r_sut32])
    big, r_big = C.sb("bigD", [128, NBLK * NE], F32)
    nbk, r_nbk = C.sb("nbk", [128, NE], F32)
    padT, r_padT = C.sb("padT", [NE, 128], F32)
    pstart, r_pstart = C.sb("pstart", [128, NE], F32)
    pend, r_pend = C.sb("pend", [128, NE], F32)
    bexp, r_bexp = C.sb("bexp", [128, NBLK], F32)
    wf, r_wf = C.sb("wf", [128, NBLK, 8], F32)
    S.op("dve", lambda e: e.tensor_tensor(out=big[:, 0:NE * 16].rearrange("p (e m) -> p e m", m=16), in0=rrun[:].unsqueeze(2).to_broadcast([128, NE, 16]), in1=thr16[:], op=ALU.is_gt),
         reads=[r_rrun, r_thr16], writes=[r_big])
    S.op("dve", lambda e: e.tensor_reduce(out=nbk[:], in_=big[:, 0:NE * 16].rearrange("p (e m) -> p e m", m=16), axis=AX.X, op=ALU.add), reads=[r_big], writes=[r_nbk])
    S.op("dve", lambda e: e.tensor_scalar_mul(out=nbk[:], in0=nbk[:], scalar1=512.0), reads=[r_nbk], writes=[r_nbk])
    pq, r_pq = psl[0]
    S.op("pe", lambda e: e.transpose(out=pq[0:NE, 0:128], in_=nbk[:], identity=ident_f[:]), reads=[r_nbk, r_identf], writes=[r_pq])
    S.op("act", lambda e: e.activation(out=padT[:], in_=pq[0:NE, 0:128], func=AF.Copy), reads=[r_pq], writes=[r_padT])
    S.op("pe", lambda e: e.matmul(pq[:, 256:256 + NE], lhsT=padT[:], rhs=sut32[:], start=True, stop=True), reads=[r_padT, r_sut32], awrites=[r_pq])
    S.op("act", lambda e: e.activation(out=pstart[:], in_=pq[:, 256:256 + NE], func=AF.Copy), reads=[r_pq], writes=[r_pstart])
    S.op("dve", lambda e: e.tensor_tensor(out=pend[:], in0=pstart[:], in1=nbk[:], op=ALU.add), reads=[r_pstart, r_nbk], writes=[r_pend])
    S.op("dve", lambda e: e.tensor_tensor(out=big[:].rearrange("p (i e) -> p i e", e=NE), in0=bstart[:].unsqueeze(2).to_broadcast([128, NBLK, NE]),
                                         in1=pend[:].unsqueeze(1).to_broadcast([128, NBLK, NE]), op=ALU.is_ge), reads=[r_bstart, r_pend], writes=[r_big])
    S.op("dve", lambda e: e.tensor_reduce(out=bexp[:], in_=big[:].rearrange("p (i e) -> p i e", e=NE), axis=AX.X, op=ALU.add), reads=[r_big], writes=[r_bexp])
    S.op("dve", lambda e: e.tensor_scalar_min(out=bexp[:], in0=bexp[:], scalar1=float(NE - 1)), reads=[r_bexp], writes=[r_bexp])
    S.op("dve", lambda e: e.tensor_copy(out=eidx[:], in_=bexp[:]), reads=[r_bexp], writes=[r_eidx])
    S.op("dve", lambda e: e.tensor_scalar(out=bidx[:], in0=bexp[:], scalar1=128.0, scalar2=pcol[:, 0:1], op0=ALU.mult, op1=ALU.add), reads=[r_bexp, r_pcol], writes=[r_bidx])
    S.op("dve", lambda e: e.tensor_scalar_mul(out=wf[:], in0=bexp[:].unsqueeze(2).to_broadcast([128, NBLK, 8]), scalar1=float(D)), reads=[r_bexp], writes=[r_wf])
    S.op("dve", lambda e: e.tensor_tensor(out=widx[:], in0=wf[:], in1=kp[:].unsqueeze(1).to_broadcast([128, NBLK, 8]), op=ALU.add), reads=[r_wf, r_kp], writes=[r_widx])
    sl_, r_sl = C.sb("slD", [128, NE], F32)
    eqt, r_eqt = C.sb("eqt", [128, NE], F32)
    m8b, r_m8b = C.sb("m8b", [128, 8], F32)
    S.op("dve", lambda e: e.tensor_scalar_add(out=pstart[:], in0=pstart[:], scalar1=1.0), reads=[r_pstart], writes=[r_pstart])
    for t0 in range(2):
        S.dma("sp", u2b[t0 % 3][0][:], u2_d[t0 * 128:t0 * 128 + 128, :], reads=[r_u2], writes=[u2b[t0 % 3][1]])
    for t in range(NT):
        tok0 = t * 128
        ub, r_ub = u2b[t % 3]
        if t + 2 < NT:
            S.dma("sp", u2b[(t + 2) % 3][0][:], u2_d[tok0 + 256:tok0 + 384, :], reads=[r_u2], writes=[u2b[(t + 2) % 3][1]])
        S.op("dve", lambda e: e.tensor_tensor(out=sl_[:], in0=posall[:, t, :], in1=pstart[:], op=ALU.add), reads=[r_posall, r_pstart], writes=[r_sl])
        S.op("dve", lambda e: e.tensor_tensor(out=sl_[:], in0=sl_[:], in1=maskall[:, t, :], op=ALU.mult), reads=[r_sl, r_maskall], writes=[r_sl])
        S.op("dve", lambda e: e.max(out=m8b[:], in_=sl_[:]), reads=[r_sl], writes=[r_m8b])
        for j in range(4):
            S.op("dve", lambda e: e.scalar_tensor_tensor(out=eqt[:], in0=sl_[:], scalar=m8b[:, j:j + 1], in1=gall[:, t, :], op0=ALU.is_equal, op1=ALU.mult, accum_out=g4[:, t, j:j + 1]),
                 reads=[r_sl, r_m8b, r_gall], writes=[r_eqt], awrites=[r_g4])
        S.op("dve", lambda e: e.tensor_scalar_add(out=idx4[:, t, :], in0=m8b[:, 0:4], scalar1=-1.0), reads=[r_m8b], awrites=[r_idx4])
        for j in range(4):
            S.op("pool", lambda e: e.indirect_dma_start(out=xs_d[:, :], out_offset=bass.IndirectOffsetOnAxis(ap=idx4[:, t, j:j + 1], axis=0), in_=ub[:], in_offset=None),
                 reads=[r_ub, r_idx4], awrites=[r_xsd], dma=True)
    C.pop()
    if "stopD" in dbg:
        C.pop()
        return

    C.push()
    w1b = C.ring("sb", "w1b", 2, [128, 8, 2 * D], BF16)
    w2b = C.ring("sb", "w2b", 2, [128, 8, D], BF16)
    b1t = C.ring("sb", "b1t", 2, [128, 16], F32)
    b2t = C.ring("sb", "b2t", 2, [128, D], F32)
    ones1f, r_ones1f = C.sb("ones1f", [1, 128], BF16)
    S.op("dve", lambda e: e.memset(ones1f[:], 1.0), writes=[r_ones1f])
    b2b = C.ring("sb", "b2b", 2, [1, D], BF16)
    xr = C.ring("sb", "xr", 8, [128, D], BF16)
    XTr = C.ring("sb", "XT", 2, [128, 8, 512], BF16)
    hg = C.ring("sb", "hg", 2, [128, 512], F32)
    hu = C.ring("sb", "hu", 2, [128, 512], F32)
    sgm = C.ring("sb", "sgm", 2, [128, 512], F32)
    actT, r_actT = C.sb("actT", [128, 8, 512], BF16)
    ysb = C.ring("sb", "ysb", 2, [128, D], BF16)
    pxt = C.ring("ps", "pxt", 2, [128, 512], F32)
    pg = C.ring("ps", "pg", 2, [128, 512], F32)
    pu = C.ring("ps", "pu", 2, [128, 512], F32)
    py = C.ring("ps", "py", 2, [128, 512], F32)

    def load_weights(i, slot):
        w1t, r_w1 = w1b[slot]
        w2t, r_w2 = w2b[slot]
        for k in range(8):
            S.op("pool", lambda e: e.indirect_dma_start(out=w1t[:, k, :], out_offset=None, in_=w1b_d[:, :], in_offset=bass.IndirectOffsetOnAxis(ap=widx[:, i, k:k + 1], axis=0)),
                 reads=[r_w1bd, r_widx], writes=[r_w1] if k == 0 else (), awrites=() if k == 0 else [r_w1], dma=True)
        for k in range(8):
            S.op("pool", lambda e: e.indirect_dma_start(out=w2t[:, k, :], out_offset=None, in_=w2b_d[:, :], in_offset=bass.IndirectOffsetOnAxis(ap=widx[:, i, k:k + 1], axis=0)),
                 reads=[r_w2bd, r_widx], writes=[r_w2] if k == 0 else (), awrites=() if k == 0 else [r_w2], dma=True)
        S.op("pool", lambda e: e.indirect_dma_start(out=b1t[slot][0][:], out_offset=None, in_=b1r_d[:, :], in_offset=bass.IndirectOffsetOnAxis(ap=bidx[:, i:i + 1], axis=0)),
             reads=[r_in, r_bidx], writes=[b1t[slot][1]], dma=True)
        S.op("pool", lambda e: e.indirect_dma_start(out=b2t[slot][0][:], out_offset=None, in_=b2_d[:, :], in_offset=bass.IndirectOffsetOnAxis(ap=eidx[:, i:i + 1], axis=0)),
             reads=[r_in, r_eidx], writes=[b2t[slot][1]], dma=True)

    def load_x(i):
        for s4 in range(4):
            x_t, r_x = xr[(i % 2) * 4 + s4]
            row0 = i * 512 + s4 * 128
            S.dma("sp", x_t[:], xs_d[row0:row0 + 128, :], reads=[r_xsd], writes=[r_x])

    nblk_run = NBLK if "nblk" not in L else L["nblk"]
    load_weights(0, 0)
    load_x(0)
    xstate = {"xc": 0}

    def xpose(i):
        XT, r_XT = XTr[i % 2]
        for s4 in range(4):
            x_t, r_x = xr[(i % 2) * 4 + s4]
            p_t, r_p = pxt[xstate["xc"] % 2]
            xstate["xc"] += 1
            p_b = p_t[:].bitcast(BF16)
            for k in range(8):
                S.op("pe", lambda e: e.transpose(out=p_b[:, k * 128:(k + 1) * 128], in_=x_t[:, k * 128:(k + 1) * 128], identity=ident_b[:]),
                     reads=[r_x, r_identb], writes=[r_p] if k == 0 else (), awrites=() if k == 0 else [r_p])
            S.op("act", lambda e: e.activation(out=XT[:, :, s4 * 128:(s4 + 1) * 128], in_=p_b.rearrange("p (k t) -> p k t", k=8), func=AF.Copy), reads=[r_p],
                 writes=[r_XT] if s4 == 0 else (), awrites=() if s4 == 0 else [r_XT])

    xpose(0)
    for i in range(nblk_run):
        slot = i % 2
        if i + 1 < nblk_run:
            load_weights(i + 1, (i + 1) % 2)
            load_x(i + 1)
        w1t, r_w1 = w1b[slot]
        w2t, r_w2 = w2b[slot]
        b1_t, r_b1 = b1t[slot]
        b2f_t, r_b2f = b2t[slot]
        b2_t, r_b2 = b2b[slot]
        XT, r_XT = XTr[i % 2]
        for fc in range(8):
            pg_t, r_pg = pg[fc % 2]
            pu_t, r_pu = pu[fc % 2]
            for k in range(8):
                S.op("pe", lambda e: e.matmul(pg_t[:], lhsT=w1t[:, k, fc * 128:(fc + 1) * 128], rhs=XT[:, k, :], start=(k == 0), stop=(k == 7)),
                     reads=[r_w1, r_XT], writes=[r_pg] if k == 0 else (), awrites=() if k == 0 else [r_pg])
            for k in range(8):
                S.op("pe", lambda e: e.matmul(pu_t[:], lhsT=w1t[:, k, D + fc * 128:D + (fc + 1) * 128], rhs=XT[:, k, :], start=(k == 0), stop=(k == 7)),
                     reads=[r_w1, r_XT], writes=[r_pu] if k == 0 else (), awrites=() if k == 0 else [r_pu])
            hg_t, r_hg = hg[fc % 2]
            hu_t, r_hu = hu[fc % 2]
            sg_t, r_sgm = sgm[fc % 2]
            S.op("act", lambda e: e.activation(out=hg_t[:], in_=pg_t[:], func=AF.Identity, bias=b1_t[:, fc:fc + 1]), reads=[r_pg, r_b1], writes=[r_hg])
            S.op("act", lambda e: e.activation(out=hu_t[:], in_=pu_t[:], func=AF.Identity, bias=b1_t[:, 8 + fc:9 + fc]), reads=[r_pu, r_b1], writes=[r_hu])
            S.op("dve", lambda e: e.tensor_scalar_min(out=hg_t[:], in0=hg_t[:], scalar1=7.0), reads=[r_hg], writes=[r_hg])
            S.op("act", lambda e: e.activation(out=sg_t[:], in_=hg_t[:], func=AF.Sigmoid, scale=1.702), reads=[r_hg], writes=[r_sgm])
            S.op("pool", lambda e: e.tensor_scalar(out=hu_t[:], in0=hu_t[:], scalar1=7.0, scalar2=-7.0, op0=ALU.min, op1=ALU.max), reads=[r_hu], writes=[r_hu])
            S.op("dve", lambda e: e.scalar_tensor_tensor(out=hu_t[:], in0=hu_t[:], scalar=1.0, in1=hg_t[:], op0=ALU.add, op1=ALU.mult), reads=[r_hu, r_hg], writes=[r_hu])
            S.op("dve", lambda e: e.tensor_tensor(out=actT[:, fc, :], in0=hu_t[:], in1=sg_t[:], op=ALU.mult), reads=[r_hu, r_sgm],
                 writes=[r_actT] if fc == 0 else (), awrites=() if fc == 0 else [r_actT])
        if i + 1 < nblk_run:
            xpose(i + 1)
        for s4 in range(4):
            y_t, r_y = ysb[s4 % 2]
            for hf in range(2):
                py_t, r_py = py[hf]
                for fc in range(8):
                    S.op("pe", lambda e: e.matmul(py_t[:], lhsT=actT[:, fc, s4 * 128:(s4 + 1) * 128], rhs=w2t[:, fc, hf * 512:(hf + 1) * 512], start=(fc == 0), stop=(fc == 7)),
                         reads=[r_actT, r_w2], writes=[r_py] if fc == 0 else (), awrites=() if fc == 0 else [r_py])
                S.op("dve", lambda e: e.tensor_tensor(out=y_t[:, hf * 512:(hf + 1) * 512], in0=py_t[:], in1=b2f_t[:, hf * 512:(hf + 1) * 512], op=ALU.add), reads=[r_py, r_b2f],
                     writes=[r_y] if hf == 0 else (), awrites=() if hf == 0 else [r_y])
            row0 = i * 512 + s4 * 128
            S.dma("act", ys_d[row0:row0 + 128, :], y_t[:], reads=[r_y], awrites=[r_ysd])
    C.pop()

    C.push()
    ln2g, r_ln2g = C.sb("ln2g", [128, D], F32)
    ln2b, r_ln2b = C.sb("ln2b", [128, D], F32)
    S.dma("sp", ln2g[:], g_("ln2g_d").partition_broadcast(128), reads=[r_in], writes=[r_ln2g])
    S.dma("sp", ln2b[:], g_("ln2b_d").partition_broadcast(128), reads=[r_in], writes=[r_ln2b])
    gate2r = C.ring("sb", "gate2r", 2, [128, D], F32)
    x1r = C.ring("sb", "x1r", 4, [128, D], F32)
    yg = C.ring("sb", "yg", 16, [128, D], BF16)
    accr = C.ring("sb", "accF", 3, [128, D], F32)
    ot = C.ring("sb", "otF", 3, [128, D], F32)
    stF = C.ring("sb", "stF", 3, [128, 2, 6], F32)
    mvF = C.ring("sb", "mvF", 3, [128, 4], F32)
    rsF = C.ring("sb", "rsF", 3, [128, 1], F32)

    def loadF(t):
        tok0 = t * 128
        slot = t % 4
        S.dma("sp", x1r[slot][0][:], x1_d[tok0:tok0 + 128, :], reads=[r_x1], writes=[x1r[slot][1]])
        for j in range(4):
            y_t, r_y = yg[slot * 4 + j]
            S.op("pool", lambda e: e.indirect_dma_start(out=y_t[:], out_offset=None, in_=ys_d[:, :], in_offset=bass.IndirectOffsetOnAxis(ap=idx4[:, t, j:j + 1], axis=0)),
                 reads=[r_ysd, r_idx4], writes=[r_y], dma=True)
        if t % TPS == 0:
            b_ = t // TPS
            S.dma("sp", gate2r[b_ % 2][0][:], mod_d[b_:b_ + 1, 5 * D:6 * D].partition_broadcast(128), reads=[r_mod], writes=[gate2r[b_ % 2][1]])

    def genF(t):
        if t + 3 < NT:
            loadF(t + 3)
        slot = t % 4
        tok0 = t * 128
        gate2, r_gate2 = gate2r[(t // TPS) % 2]
        x1_t, r_x1t = x1r[slot]
        acc, r_acc = accr[t % 3]
        st, r_st = stF[t % 3]; mv, r_mv = mvF[t % 3]; rs, r_rs = rsF[t % 3]
        o_t, r_o = ot[t % 3]
        for j in range(4):
            y_t, r_y = yg[slot * 4 + j]
            if j == 0:
                S.op("act", lambda e: e.activation(out=acc[:], in_=y_t[:], func=AF.Copy, scale=g4[:, t, 0:1]), reads=[r_y, r_g4], writes=[r_acc])
            else:
                S.op("dve", lambda e: e.scalar_tensor_tensor(out=acc[:], in0=y_t[:], scalar=g4[:, t, j:j + 1], in1=acc[:], op0=ALU.mult, op1=ALU.add), reads=[r_y, r_g4, r_acc], writes=[r_acc])
            yield
        S.op("pool", lambda e: e.tensor_tensor(out=acc[:], in0=acc[:], in1=gate2[:], op=ALU.mult), reads=[r_acc, r_gate2], writes=[r_acc])
        yield
        S.op("dve", lambda e: e.scalar_tensor_tensor(out=acc[:], in0=x1_t[:], scalar=float(ALPHA), in1=acc[:], op0=ALU.mult, op1=ALU.add), reads=[r_x1t, r_acc], writes=[r_acc])
        yield
        for j in range(2):
            S.op("dve", lambda e: e.bn_stats(out=st[:, j, :], in_=acc[:, j * 512:(j + 1) * 512]), reads=[r_acc], writes=[r_st] if j == 0 else (), awrites=() if j == 0 else [r_st])
            yield
        S.op("dve", lambda e: e.bn_aggr(out=mv[:, 0:2], in_=st[:].rearrange("p a b -> p (a b)")), reads=[r_st], writes=[r_mv])
        yield
        S.op("act", lambda e: e.activation(out=rs[:], in_=mv[:, 1:2], func=AF.Sqrt, bias=EPS), reads=[r_mv], writes=[r_rs])
        yield
        S.op("dve", lambda e: e.reciprocal(out=rs[:], in_=rs[:]), reads=[r_rs], writes=[r_rs])
        yield
        S.op("dve", lambda e: e.tensor_scalar(out=mv[:, 2:3], in0=mv[:, 0:1], scalar1=-1.0, scalar2=rs[:], op0=ALU.mult, op1=ALU.mult), reads=[r_mv, r_rs], awrites=[r_mv])
        yield
        S.op("act", lambda e: e.activation(out=o_t[:], in_=acc[:], func=AF.Identity, scale=rs[:], bias=mv[:, 2:3]), reads=[r_acc, r_mv, r_rs], writes=[r_o])
        yield
        S.op("dve", lambda e: e.tensor_tensor(out=o_t[:], in0=o_t[:], in1=ln2g[:], op=ALU.mult), reads=[r_o, r_ln2g], writes=[r_o])
        yield
        S.op("dve", lambda e: e.tensor_tensor(out=o_t[:], in0=o_t[:], in1=ln2b[:], op=ALU.add), reads=[r_o, r_ln2b], writes=[r_o])
        yield
        S.dma("sp", out_d[tok0:tok0 + 128, :], o_t[:], reads=[r_o], awrites=[r_out])
        yield

    for t0 in range(3):
        loadF(t0)
    interleave((genF(t) for t in range(NT)), 6)
    C.pop()
    C.pop()


def phase_C(nc, S, C, dbg, L):
    g_ = lambda n: L[n]
    r_in = g_("r_in"); ident_b = g_("ident_b"); r_identb = g_("r_identb"); ident_f = g_("ident_f"); r_identf = g_("r_identf")
    qT_d, r_qT = g_("qT_d"), g_("r_qT"); iqT_d, r_iqT = g_("iqT_d"), g_("r_iqT")
    kva_d, r_kva = g_("kva_d"), g_("r_kva"); kvT_d, r_kvT = g_("kvT_d"), g_("r_kvT"); ikT_d, r_ikT = g_("ikT_d"), g_("r_ikT")
    iw_d, r_iw = g_("iw_d"), g_("r_iw"); oaT_d, r_oaT = g_("oaT_d"), g_("r_oaT")
    C.push()
    w1_d, w2_d = g_("w1_d"), g_("w2_d")

    def cast_weights(i):
        e_ = i // 2
        if i % 2 == 0:
            S.dma("pool", g_("w1b_d")[e_ * D:(e_ + 1) * D, :], w1_d[e_ * D:(e_ + 1) * D, :], reads=[r_in], awrites=[g_("r_w1bd")])
        else:
            S.dma("pool", g_("w2b_d")[e_ * D:(e_ + 1) * D, :], w2_d[e_ * D:(e_ + 1) * D, :], reads=[r_in], awrites=[g_("r_w2bd")])
    tzf, r_tzf = C.sb("tzf", [128, 2, 8, 128], F32)
    tz, r_tz = C.sb("tzb", [128, 2, 8, 128], BF16)
    cfar, r_cfar = C.sb("cfar", [128, 8], F32)
    identN, r_identN = C.sb("identN", [128, 128], BF16)
    S.dma("sp", tzf[:], g_("tz_d"), reads=[r_in], writes=[r_tzf])
    S.dma("sp", cfar[:], g_("cfar_d").partition_broadcast(128), reads=[r_in], writes=[r_cfar])
    S.op("dve", lambda e: e.tensor_scalar_mul(out=identN[:], in0=ident_f[:], scalar1=-NEG), reads=[r_identf], writes=[r_identN])
    first = True
    for dl in range(2):
        for h in range(8):
            S.op("dve", lambda e: e.tensor_scalar(out=tz[:, dl, h, :], in0=tzf[:, dl, h, :], scalar1=cfar[:, h:h + 1], scalar2=None, op0=ALU.subtract),
                 reads=[r_tzf, r_cfar], writes=[r_tz] if first else (), awrites=() if first else [r_tz])
            first = False
    seqb = [(C.sb("kvT%d" % i, [128, SEQ], BF16), C.sb("ikT%d" % i, [128, SEQ], BF16), C.sb("kvaC%d" % i, [128, TPS, 136], BF16)) for i in range(2)]
    qt = C.ring("sb", "qt", 5, [128, 8, 128], BF16)
    iqt = C.ring("sb", "iqt", 4, [128, 4, 128], BF16)
    iwt = C.ring("sb", "iwC", 4, [128, 8], F32)
    scorer = C.ring("sb", "score", 3, [128, SEQ], F32)
    penr = C.ring("sb", "pen", 2, [128, SEQ], BF16)
    rl = C.ring("sb", "rl", 2, [128, 512], F32)
    m8, r_m8 = C.sb("m8", [128, 8], F32)
    expT = C.ring("sb", "expT", 2, [128, TPS * 128], BF16)
    posb = C.ring("sb", "posb", 2, [128, 8, 132], F32)
    rc, r_rc = C.sb("rcC", [128, 8], F32)
    oa, r_oa = C.sb("oa", [128, D], BF16)
    oaT = C.ring("sb", "oaT", 2, [128, 8, 128], BF16)
    pi = C.ring("ps", "pi", 2, [128, 512], F32)
    pl = C.ring("ps", "pl", 3, [128, 512], F32)
    po = C.ring("ps", "po", 2, [128, 512], F32)
    ptr = C.ring("ps", "ptrC", 1, [128, 512], F32)
    NTILE = NB * TPS

    def load_seq(b):
        (kvT, r_kvTs), (ikT, r_ikTs), (kva, r_kvas) = seqb[b % 2]
        s0 = b * SEQ
        S.dma("sp", kvT[:], kvT_d[:, s0:s0 + SEQ], reads=[r_kvT], writes=[r_kvTs])
        S.dma("sp", ikT[:], ikT_d[:, s0:s0 + SEQ], reads=[r_ikT], writes=[r_ikTs])
        S.dma("sp", kva[:], kva_d[s0:s0 + SEQ, :].rearrange("(k p) c -> p k c", p=128), reads=[r_kva], writes=[r_kvas])

    def load_tile(i):
        tok0 = i * 128
        S.dma("sp", qt[i % 5][0][:], qT_d[:, :, tok0:tok0 + 128].rearrange("h p t -> p h t"), reads=[r_qT], writes=[qt[i % 5][1]])
        S.dma("sp", iqt[i % 4][0][:], iqT_d[:, :, tok0:tok0 + 128].rearrange("h p t -> p h t"), reads=[r_iqT], writes=[iqt[i % 4][1]])
        S.dma("sp", iwt[i % 4][0][:], iw_d[tok0:tok0 + 128, :], reads=[r_iw], writes=[iwt[i % 4][1]])

    NIT = 24
    pow2, r_pow2 = C.sb("pow2", [128, NIT + 1], F32)
    stepsr = C.ring("sb", "steps", 3, [128, NIT + 1], F32)
    bisr = C.ring("sb", "bis", 3, [128, 8], F32)
    junkb, r_junkb = C.sb("junkb", [128, SEQ], BF16)
    junkd, r_junkd = C.sb("junkd", [128, SEQ], BF16)
    for j in range(NIT + 1):
        S.op("pool", lambda e: e.memset(pow2[:, j:j + 1], float(2.0 ** -(j + 1))), writes=[r_pow2] if j == 0 else (), awrites=() if j == 0 else [r_pow2])

    def prep_score_gen(i):
        b, t = divmod(i, TPS)
        (ikT, r_ikTs) = seqb[b % 2][1]
        iq_t, r_iq = iqt[i % 4]
        iw_t, r_iwt = iwt[i % 4]
        pen, r_pen = penr[i % 2]
        score, r_score = scorer[i % 3]
        steps, r_steps = stepsr[i % 3]
        bis, r_bis = bisr[i % 3]
        N = 128 * (t + 1)
        if t < 2:
            return
            yield
        for kg in range(0, N, 512):
            w = min(512, N - kg)
            for h in range(8):
                pr, hf = divmod(h, 2)
                p_t, r_p = pi[h % 2]
                S.op("pe", lambda e: e.matmul(p_t[:, 0:w], lhsT=iq_t[64 * hf:64 * hf + 64, pr, :], rhs=ikT[64 * hf:64 * hf + 64, kg:kg + w], start=True, stop=True),
                     reads=[r_iq, r_ikTs], writes=[r_p])
                r_t, r_r = rl[h % 2]
                if h == 0:
                    S.op("dve", lambda e: e.tensor_scalar(out=score[:, kg:kg + w], in0=p_t[:, 0:w], scalar1=0.0, scalar2=iw_t[:, 0:1], op0=ALU.max, op1=ALU.mult),
                         reads=[r_p, r_iwt], writes=[r_score] if kg == 0 else (), awrites=() if kg == 0 else [r_score])
                elif h % 2 == 1:
                    S.op("dve", lambda e: e.tensor_scalar(out=r_t[:, 0:w], in0=p_t[:, 0:w], scalar1=0.0, scalar2=iw_t[:, h:h + 1], op0=ALU.max, op1=ALU.mult),
                         reads=[r_p, r_iwt], writes=[r_r])
                    S.op("pool", lambda e: e.tensor_tensor(out=score[:, kg:kg + w], in0=score[:, kg:kg + w], in1=r_t[:, 0:w], op=ALU.add),
                         reads=[r_r, r_score], awrites=[r_score])
                else:
                    S.op("act", lambda e: e.activation(out=r_t[:, 0:w], in_=p_t[:, 0:w], func=AF.Relu), reads=[r_p], writes=[r_r])
                    S.op("dve", lambda e: e.scalar_tensor_tensor(out=score[:, kg:kg + w], in0=r_t[:, 0:w], scalar=iw_t[:, h:h + 1], in1=score[:, kg:kg + w], op0=ALU.mult, op1=ALU.add),
                         reads=[r_r, r_iwt, r_score], awrites=[r_score])
                yield
        S.op("dve", lambda e: e.tensor_reduce(out=bis[:, 0:1], in_=score[:, 0:N - 64], axis=AX.X, op=ALU.min), reads=[r_score], writes=[r_bis])
        S.op("dve", lambda e: e.memset(score[0:64, N - 64:N], -1e30), reads=[r_score], awrites=[r_score])
        S.op("dve", lambda e: e.max(out=m8[:], in_=score[:, 0:N]), reads=[r_score], writes=[r_m8])
        S.op("dve", lambda e: e.tensor_tensor(out=bis[:, 1:2], in0=m8[:, 0:1], in1=bis[:, 0:1], op=ALU.subtract), reads=[r_m8, r_bis], awrites=[r_bis])
        S.op("dve", lambda e: e.tensor_scalar(out=steps[:], in0=pow2[:], scalar1=bis[:, 1:2], scalar2=None, op0=ALU.mult), reads=[r_pow2, r_bis], writes=[r_steps])
        S.op("dve", lambda e: e.tensor_tensor(out=bis[:, 2:3], in0=bis[:, 0:1], in1=steps[:, 0:1], op=ALU.add), reads=[r_bis, r_steps], awrites=[r_bis])

    def prep_score(i):
        for _ in prep_score_gen(i):
            pass

    def prep_iter(i, j):
        b, t = divmod(i, TPS)
        if t < 2:
            return
        N = 128 * (t + 1)
        score, r_score = scorer[i % 3]
        steps, r_steps = stepsr[i % 3]
        bis, r_bis = bisr[i % 3]
        if j % 3 != 2:
            S.op("act", lambda e: e.activation(out=junkb[:, 0:N], in_=score[:, 0:N], func=AF.Sign, scale=-1.0, bias=bis[:, 2:3], accum_out=bis[:, 4:5]),
                 reads=[r_score, r_bis], writes=[r_junkb], awrites=[r_bis])
            S.op("dve", lambda e: e.tensor_scalar(out=bis[:, 3:4], in0=bis[:, 4:5], scalar1=float(N - 511), scalar2=steps[:, j:j + 1], op0=ALU.is_le, op1=ALU.mult),
                 reads=[r_bis, r_steps], awrites=[r_bis])
        else:
            S.op("dve", lambda e: e.tensor_scalar(out=junkd[:, 0:N], in0=score[:, 0:N], scalar1=bis[:, 2:3], scalar2=None, op0=ALU.is_ge, op1=ALU.add, accum_out=bis[:, 6:7]),
                 reads=[r_score, r_bis], writes=[r_junkd], awrites=[r_bis])
            S.op("dve", lambda e: e.tensor_scalar(out=bis[:, 3:4], in0=bis[:, 6:7], scalar1=255.5, scalar2=steps[:, j:j + 1], op0=ALU.is_ge, op1=ALU.mult),
                 reads=[r_bis, r_steps], awrites=[r_bis])
        S.op("dve", lambda e: e.scalar_tensor_tensor(out=bis[:, 2:3], in0=bis[:, 3:4], scalar=steps[:, j + 1:j + 2], in1=bis[:, 2:3], op0=ALU.subtract, op1=ALU.add),
             reads=[r_bis, r_steps], awrites=[r_bis])

    def prep_fin(i):
        b, t = divmod(i, TPS)
        N = 128 * (t + 1)
        pen, r_pen = penr[i % 2]
        if t < 2:
            S.op("dve", lambda e: e.memset(pen[:, 0:N], 0.0), writes=[r_pen])
            S.op("dve", lambda e: e.memset(pen[0:64, N - 64:N], -1.0), awrites=[r_pen])
            return
        score, r_score = scorer[i % 3]
        steps, r_steps = stepsr[i % 3]
        bis, r_bis = bisr[i % 3]
        S.op("dve", lambda e: e.tensor_tensor(out=bis[:, 5:6], in0=bis[:, 2:3], in1=steps[:, NIT:NIT + 1], op=ALU.subtract), reads=[r_bis, r_steps], awrites=[r_bis])
        S.op("dve", lambda e: e.tensor_scalar(out=pen[:, 0:N], in0=score[:, 0:N], scalar1=bis[:, 5:6], scalar2=1.0, op0=ALU.is_ge, op1=ALU.subtract),
             reads=[r_score, r_bis], writes=[r_pen])

    state = {"plc": 0, "hc": 0}

    def attend_head(i, h):
        b, t = divmod(i, TPS)
        (kvT, r_kvTs), _, (kva, r_kvas) = seqb[b % 2]
        q_t, r_q = qt[i % 5]
        pen, r_pen = penr[i % 2]
        ps_t, r_ps = posb[i % 2]
        nkb = t + 1
        if True:
            e_t, r_e = expT[state["hc"] % 2]
            state["hc"] += 1
            for kg in range(0, nkb, 4):
                nb_ = min(4, nkb - kg)
                p_t, r_p = pl[state["plc"] % 3]
                state["plc"] += 1
                for ii in range(nb_):
                    kb = kg + ii
                    near = kb >= t - 1
                    cs_ = slice(ii * 128, (ii + 1) * 128)
                    S.op("pe", lambda e: e.matmul(p_t[:, cs_], lhsT=kvT[:, kb * 128:(kb + 1) * 128], rhs=q_t[:, h, :], start=True, stop=False),
                         reads=[r_kvTs, r_q], writes=[r_p] if ii == 0 else (), awrites=() if ii == 0 else [r_p])
                    S.op("pe", lambda e: e.matmul(p_t[:, cs_], lhsT=pen[:, kb * 128:(kb + 1) * 128], rhs=identN[:], start=False, stop=(not near)),
                         reads=[r_pen, r_identN], awrites=[r_p])
                    if near:
                        S.op("pe", lambda e: e.matmul(p_t[:, cs_], lhsT=ident_b[:], rhs=tz[:, t - kb, h, :], start=False, stop=True),
                             reads=[r_identb, r_tz], awrites=[r_p])
                S.op("act", lambda e: e.activation(out=e_t[:, kg * 128:(kg + nb_) * 128], in_=p_t[:, 0:nb_ * 128], func=AF.Exp), reads=[r_p],
                     writes=[r_e] if kg == 0 else (), awrites=() if kg == 0 else [r_e])
            o_t, r_o = po[h % 2]
            for kb in range(nkb):
                S.op("pe", lambda e: e.matmul(o_t[:, 0:129], lhsT=e_t[:, kb * 128:(kb + 1) * 128], rhs=kva[:, kb, 0:129], start=(kb == 0), stop=(kb == nkb - 1)),
                     reads=[r_e, r_kvas], writes=[r_o] if kb == 0 else (), awrites=() if kb == 0 else [r_o])
            S.op("act", lambda e: e.activation(out=ps_t[:, h, 0:129], in_=o_t[:, 0:129], func=AF.Copy), reads=[r_o],
                 writes=[r_ps] if h == 0 else (), awrites=() if h == 0 else [r_ps])

    def finalize(i):
        tok0 = i * 128
        ps_t, r_ps = posb[i % 2]
        S.op("dve", lambda e: e.reciprocal(out=rc[:], in_=ps_t[:, :, 128]), reads=[r_ps], writes=[r_rc])
        S.op("dve", lambda e: e.tensor_tensor(out=oa[:].rearrange("p (h c) -> p h c", h=8), in0=ps_t[:, :, 0:128], in1=rc[:].unsqueeze(2).to_broadcast([128, 8, 128]), op=ALU.mult),
             reads=[r_ps, r_rc], writes=[r_oa])
        pT, r_pT = ptr[0]
        pT_b = pT[:].bitcast(BF16)
        for k in range(8):
            S.op("pe", lambda e: e.transpose(out=pT_b[:, k * 128:(k + 1) * 128], in_=oa[:, k * 128:(k + 1) * 128], identity=ident_b[:]),
                 reads=[r_oa, r_identb], writes=[r_pT] if k == 0 else (), awrites=() if k == 0 else [r_pT])
        oT, r_oT = oaT[i % 2]
        S.op("act", lambda e: e.activation(out=oT[:].rearrange("p k t -> p (k t)"), in_=pT_b, func=AF.Copy), reads=[r_pT], writes=[r_oT])
        S.dma("sp", oaT_d[:, :, tok0:tok0 + 128].rearrange("c p t -> p c t"), oT[:], reads=[r_oT], awrites=[r_oaT])

    HALF = NIT // 2
    load_seq(0)
    for i0 in range(4):
        load_tile(i0)
    prep_score(0)
    for j in range(NIT):
        prep_iter(0, j)
    prep_fin(0)
    prep_score(1)
    for j in range(HALF):
        prep_iter(1, j)
    prep_score(2)
    for i in range(NTILE):
        b, t = divmod(i, TPS)
        if t == 0 and b + 1 < NB:
            load_seq(b + 1)
        if i + 4 < NTILE:
            load_tile(i + 4)
        cast_weights(i)
        n1 = i + 1 < NTILE
        n2 = i + 2 < NTILE
        gen = prep_score_gen(i + 3) if i + 3 < NTILE else iter(())
        npieces = 8 * ((128 * (((i + 3) % TPS) + 1) + 511) // 512) if (i + 3 < NTILE and (i + 3) % TPS >= 2) else 0
        ppb = (npieces + 7) // 8
        sched = []
        for k in range(HALF):
            if n1:
                sched.append((i + 1, HALF + k))
            if n2:
                sched.append((i + 2, k))
        per = (len(sched) + 7) // 8
        for h in range(8):
            attend_head(i, h)
            its = sched[h * per:(h + 1) * per]
            for n_, (ti_, j) in enumerate(its):
                prep_iter(ti_, j)
                if n_ < ppb:
                    next(gen, None)
            for _ in range(max(0, ppb - len(its))):
                next(gen, None)
        for _ in gen:
            pass
        if n1:
            prep_fin(i + 1)
        if i >= 1:
            finalize(i - 1)
    finalize(NTILE - 1)
    C.pop()


def phase_B(nc, S, C, dbg, L):
    g_ = lambda n: L[n]
    r_in = g_("r_in"); ident_b = g_("ident_b"); r_identb = g_("r_identb")
    xbcT_d, r_xbcT = g_("xbcT_d"), g_("r_xbcT"); dt_d, r_dt = g_("dt_d"), g_("r_dt"); zs_d, r_zs = g_("zs_d"), g_("r_zs")
    obT_d, r_obT = g_("obT_d"), g_("r_obT")
    C.push()
    convw, r_convw = C.sb("convw", [128, 16, 4], F32)
    convb, r_convb = C.sb("convb", [128, 16], F32)
    dg, r_dg = C.sb("dg", [128, 16, 4, 128], BF16)
    identf2, r_identf2 = C.sb("identf2", [128, 128], F32)
    a_bc, r_abc = C.sb("a_bc", [128, 16], F32)
    dskip_bc, r_dskip = C.sb("dskip_bc", [128, 16], F32)
    normw_bc, r_normw = C.sb("normw_bc", [128, D], F32)
    triU, r_triU = C.sb("triU", [128, 128], F32)
    SLm, r_SL = C.sb("SLm", [128, 128], F32)
    onesf, r_onesf = C.sb("onesf", [128, 128], F32)
    negm4, r_negm4 = C.sb("negm4", [128, 512], BF16)
    S.dma("sp", convw[:], g_("convw_d"), reads=[r_in], writes=[r_convw])
    S.dma("sp", convb[:], g_("convb_d"), reads=[r_in], writes=[r_convb])
    S.dma("sp", identf2[:], g_("ident_d"), reads=[r_in], writes=[r_identf2])
    S.dma("sp", a_bc[:], g_("alog_d").partition_broadcast(128), reads=[r_in], writes=[r_abc])
    S.dma("sp", dskip_bc[:], g_("dskip_d").partition_broadcast(128), reads=[r_in], writes=[r_dskip])
    S.dma("sp", normw_bc[:], g_("normw_d").partition_broadcast(128), reads=[r_in], writes=[r_normw])
    S.dma("sp", triU[:], g_("triU_d"), reads=[r_in], writes=[r_triU])
    S.dma("sp", SLm[:], g_("SL_d"), reads=[r_in], writes=[r_SL])
    S.dma("pool", negm4[:], g_("negm4_d"), reads=[r_in], writes=[r_negm4])
    S.op("dve", lambda e: e.memset(onesf[:], 1.0), writes=[r_onesf])
    S.op("act", lambda e: e.activation(out=a_bc[:], in_=a_bc[:], func=AF.Exp), reads=[r_abc], writes=[r_abc])
    S.op("dve", lambda e: e.tensor_scalar_mul(out=a_bc[:], in0=a_bc[:], scalar1=-1.0), reads=[r_abc], writes=[r_abc])
    first = True
    for j in range(16):
        for k in range(4):
            S.op("dve", lambda e: e.tensor_scalar_mul(out=dg[:, j, k, :], in0=identf2[:], scalar1=convw[:, j, k:k + 1]),
                 reads=[r_identf2, r_convw], writes=[r_dg] if first else (), awrites=() if first else [r_dg])
            first = False

    bank = C.ring("ps", "bk", 8, [128, 512], F32)
    xh = C.ring("sb", "xh", 4, [128, 16, 131], BF16)
    dtl = C.ring("sb", "dtl", 4, [128, 16], F32)
    zl = C.ring("sb", "zl", 4, [128, D], BF16)
    xact_r = C.ring("sb", "xact", 2, [128, 16, 128], BF16)
    xs_r = C.ring("sb", "xs_tok", 2, [128, D], BF16)
    Bt_r = C.ring("sb", "B_tok", 2, [128, 512], BF16)
    adt_r = C.ring("sb", "adt", 2, [128, 16], F32)
    sm_r = C.ring("sb", "smB", 2, [128, 8, 16], F32)
    A_r = C.ring("sb", "Amat", 2, [128, 16, 128], F32)
    Lt_r = C.ring("sb", "Lt", 2, [128, 2, 512], F32)
    Mt_r = C.ring("sb", "Mt", 2, [128, 16, 128], BF16)
    xdt_r = C.ring("sb", "xdt", 2, [128, D], BF16)
    xdd_r = C.ring("sb", "xdd", 2, [128, D], BF16)
    prev_f, r_pf = C.sb("prev_f", [128, D], F32)
    prev_b, r_pb = C.sb("prev_b", [128, D], BF16)
    t1_r = C.ring("sb", "t1", 2, [128, D], F32)
    t2_r = C.ring("sb", "t2", 2, [128, D], F32)
    junk, r_junk = C.sb("junkB", [128, 256], F32)
    ob_r = C.ring("sb", "ob", 2, [128, D], BF16)
    obT = C.ring("sb", "obT", 2, [128, 8, 128], BF16)

    def bc3(ap2, n):
        return ap2.unsqueeze(2).to_broadcast([128, 16, n])

    def v3(t, n=64):
        return t.rearrange("p (h q) -> p h q", q=n)

    def load_chunk(b, t, slot):
        tok0 = b * SEQ + t * 128
        x_t, r_x = xh[slot]
        if t == 0:
            S.op("pool", lambda e: e.memset(x_t[:, :, 0:3], 0.0), writes=[r_x])
            S.dma("sp", x_t[:, :, 3:131], xbcT_d[:, :, tok0:tok0 + 128].rearrange("c p t -> p c t"), reads=[r_xbcT], awrites=[r_x])
        else:
            S.dma("sp", x_t[:, :, :], xbcT_d[:, :, tok0 - 3:tok0 + 128].rearrange("c p t -> p c t"), reads=[r_xbcT], writes=[r_x])
        S.dma("sp", dtl[slot][0][:], dt_d[tok0:tok0 + 128, :], reads=[r_dt], writes=[dtl[slot][1]])
        S.dma("sp", zl[slot][0][:], zs_d[tok0:tok0 + 128, :], reads=[r_zs], writes=[zl[slot][1]])

    nch = NB * TPS

    def chunk_gen(ci):
        b, t = divmod(ci, TPS)
        slot = ci % 4
        sl2 = ci % 2
        tok0 = b * SEQ + t * 128
        if ci + 2 < nch:
            load_chunk((ci + 2) // TPS, (ci + 2) % TPS, (ci + 2) % 4)
        x_t, r_x = xh[slot]
        d_t, r_d = dtl[slot]
        z_t, r_z = zl[slot]
        xact, r_xact = xact_r[sl2]; xs_tok, r_xs = xs_r[sl2]; B_tok, r_Bt = Bt_r[sl2]; adt, r_adt = adt_r[sl2]; sm, r_sm = sm_r[sl2]
        Amat, r_A = A_r[sl2]; Lt, r_Lt = Lt_r[sl2]; Mt, r_Mt = Mt_r[sl2]; xdt, r_xdt = xdt_r[sl2]; xdd, r_xdd = xdd_r[sl2]
        t1, r_t1 = t1_r[sl2]; t2, r_t2 = t2_r[sl2]; ob, r_ob = ob_r[sl2]
        for jg in range(4):
            pc, r_pc = bank[jg % 2]
            for jj in range(4):
                j = jg * 4 + jj
                for k in range(4):
                    S.op("pe", lambda e: e.matmul(pc[:, jj * 128:(jj + 1) * 128], lhsT=dg[:, j, k, :], rhs=x_t[:, j, k:k + 128], start=(k == 0), stop=(k == 3)),
                         reads=[r_dg, r_x], writes=[r_pc] if (jj == 0 and k == 0) else (), awrites=() if (jj == 0 and k == 0) else [r_pc])
                    yield
            for jj in range(4):
                j = jg * 4 + jj
                S.op("act", lambda e: e.activation(out=xact[:, j, :], in_=pc[:, jj * 128:(jj + 1) * 128], func=AF.Silu, bias=convb[:, j:j + 1]),
                     reads=[r_pc, r_convb], writes=[r_xact] if j == 0 else (), awrites=() if j == 0 else [r_xact])
                yield
        pxs, r_pxs = bank[2]
        pB, r_pB = bank[3]
        pxs_b = pxs[:].bitcast(BF16)
        pB_b = pB[:].bitcast(BF16)
        for k in range(8):
            S.op("pe", lambda e: e.transpose(out=pxs_b[:, k * 128:(k + 1) * 128], in_=xact[:, k, :], identity=ident_b[:]),
                 reads=[r_xact, r_identb], writes=[r_pxs] if k == 0 else (), awrites=() if k == 0 else [r_pxs])
            yield
        for k in range(4):
            S.op("pe", lambda e: e.transpose(out=pB_b[:, k * 128:(k + 1) * 128], in_=xact[:, 8 + k, :], identity=ident_b[:]),
                 reads=[r_xact, r_identb], writes=[r_pB] if k == 0 else (), awrites=() if k == 0 else [r_pB])
            yield
        S.op("act", lambda e: e.activation(out=xs_tok[:], in_=pxs_b, func=AF.Copy), reads=[r_pxs], writes=[r_xs])
        yield
        S.op("act", lambda e: e.activation(out=B_tok[:], in_=pB_b[:, 0:512], func=AF.Copy), reads=[r_pB], writes=[r_Bt])
        yield
        S.op("dve", lambda e: e.tensor_tensor(out=adt[:], in0=d_t[:], in1=a_bc[:], op=ALU.mult), reads=[r_d, r_abc], writes=[r_adt])
        yield
        S.op("pe", lambda e: e.matmul(pB[:, 256:272], lhsT=triU[:], rhs=adt[:], start=True, stop=True), reads=[r_triU, r_adt], awrites=[r_pB])
        yield
        S.op("pe", lambda e: e.matmul(pB[:, 272:288], lhsT=onesf[:], rhs=adt[:], start=True, stop=True), reads=[r_onesf, r_adt], awrites=[r_pB])
        yield
        S.op("act", lambda e: e.activation(out=sm[:, 0, :], in_=pB[:, 256:272], func=AF.Exp), reads=[r_pB], writes=[r_sm])
        yield
        S.op("act", lambda e: e.activation(out=sm[:, 1, :], in_=pB[:, 272:288], func=AF.Copy), reads=[r_pB], awrites=[r_sm])
        yield
        S.op("act", lambda e: e.activation(out=sm[:, 2, :], in_=pB[:, 272:288], func=AF.Exp), reads=[r_pB], awrites=[r_sm])
        yield
        S.op("dve", lambda e: e.tensor_tensor(out=sm[:, 3, :], in0=sm[:, 1, :], in1=pB[:, 256:272], op=ALU.subtract), reads=[r_sm, r_pB], awrites=[r_sm])
        yield
        S.op("act", lambda e: e.activation(out=sm[:, 4, :], in_=sm[:, 3, :], func=AF.Exp), reads=[r_sm], awrites=[r_sm])
        yield
        S.op("dve", lambda e: e.tensor_tensor(out=Amat[:], in0=triU[:].unsqueeze(1).to_broadcast([128, 16, 128]), in1=bc3(adt[:], 128), op=ALU.mult),
             reads=[r_triU, r_adt], writes=[r_A])
        yield
        pCB, r_pCB = bank[4]
        for g in range(4):
            S.op("pe", lambda e: e.matmul(pCB[:, g * 128:(g + 1) * 128], lhsT=xact[:, 8 + g, :], rhs=xact[:, 12 + g, :], start=True, stop=True),
                 reads=[r_xact], writes=[r_pCB] if g == 0 else (), awrites=() if g == 0 else [r_pCB])
            yield
        S.op("dve", lambda e: e.tensor_tensor(out=v3(xdt[:]), in0=v3(xs_tok[:]), in1=bc3(d_t[:], 64), op=ALU.mult), reads=[r_xs, r_d], writes=[r_xdt])
        yield
        S.op("pool", lambda e: e.tensor_tensor(out=v3(xdd[:]), in0=v3(xdt[:]), in1=bc3(sm[:, 4, :], 64), op=ALU.mult), reads=[r_xdt, r_sm], writes=[r_xdd])
        yield
        for g in range(4):
            pD, r_pD = bank[5 + g % 2]
            S.op("pe", lambda e: e.matmul(pD[:], lhsT=SLm[:], rhs=Amat[:, 4 * g:4 * g + 4, :], start=True, stop=False), reads=[r_SL, r_A], writes=[r_pD])
            yield
            S.op("pe", lambda e: e.matmul(pD[:], lhsT=ident_b[:], rhs=negm4[:], start=False, stop=True), reads=[r_identb, r_negm4], awrites=[r_pD])
            yield
            S.op("act", lambda e: e.activation(out=Lt[:, g % 2, :], in_=pD[:], func=AF.Exp), reads=[r_pD], writes=[r_Lt] if g % 2 == 0 else (), awrites=() if g % 2 == 0 else [r_Lt])
            yield
            S.op("dve", lambda e: e.tensor_tensor(out=Mt[:, 4 * g:4 * g + 4, :], in0=Lt[:, g % 2, :].rearrange("p (h l) -> p h l", h=4),
                                                 in1=pCB[:, g * 128:(g + 1) * 128].unsqueeze(1).to_broadcast([128, 4, 128]), op=ALU.mult),
                 reads=[r_Lt, r_pCB], writes=[r_Mt] if g == 0 else (), awrites=() if g == 0 else [r_Mt])
            yield
        if t == 0:
            S.op("pool", lambda e: e.memset(prev_f[:], 0.0), writes=[r_pf])
            yield
            S.op("pool", lambda e: e.memset(prev_b[:], 0.0), writes=[r_pb])
            yield
        for hh in range(2):
            pY, r_pY = bank[5]
            pO, r_pO = bank[6]
            pS, r_pS = bank[7]
            c0 = hh * 512
            for h8 in range(8):
                h = hh * 8 + h8
                S.op("pe", lambda e: e.matmul(pY[:, h8 * 64:(h8 + 1) * 64], lhsT=Mt[:, h, :], rhs=xdt[:, h * 64:(h + 1) * 64], start=True, stop=True),
                     reads=[r_Mt, r_xdt], writes=[r_pY] if h8 == 0 else (), awrites=() if h8 == 0 else [r_pY])
                yield
            for g2 in range(2):
                g = hh * 2 + g2
                S.op("pe", lambda e: e.matmul(pO[:, g2 * 256:(g2 + 1) * 256], lhsT=xact[:, 12 + g, :], rhs=prev_b[:, g * 256:(g + 1) * 256], start=True, stop=True),
                     reads=[r_xact, r_pb], writes=[r_pO] if g2 == 0 else (), awrites=() if g2 == 0 else [r_pO])
                yield
            for g2 in range(2):
                g = hh * 2 + g2
                S.op("pe", lambda e: e.matmul(pS[:, g2 * 256:(g2 + 1) * 256], lhsT=B_tok[:, g * 128:(g + 1) * 128], rhs=xdd[:, g * 256:(g + 1) * 256], start=True, stop=True),
                     reads=[r_Bt, r_xdd], writes=[r_pS] if g2 == 0 else (), awrites=() if g2 == 0 else [r_pS])
                yield
            hs = slice(hh * 8, hh * 8 + 8)

            def v8(ap):
                return ap.rearrange("p (h q) -> p h q", q=64)
            ex8 = sm[:, 0, hs].unsqueeze(2).to_broadcast([128, 8, 64])
            cd8 = sm[:, 2, hs].unsqueeze(2).to_broadcast([128, 8, 64])
            ds8 = dskip_bc[:, hs].unsqueeze(2).to_broadcast([128, 8, 64])
            S.op("dve", lambda e: e.tensor_tensor(out=v8(t1[:, c0:c0 + 512]), in0=v8(pO[:]), in1=ex8, op=ALU.mult), reads=[r_pO, r_sm], writes=[r_t1] if hh == 0 else (), awrites=() if hh == 0 else [r_t1])
            yield
            S.op("dve", lambda e: e.tensor_tensor(out=t1[:, c0:c0 + 512], in0=t1[:, c0:c0 + 512], in1=pY[:], op=ALU.add), reads=[r_t1, r_pY], awrites=[r_t1])
            yield
            S.op("pool", lambda e: e.tensor_tensor(out=v8(t2[:, c0:c0 + 512]), in0=v8(xs_tok[:, c0:c0 + 512]), in1=ds8, op=ALU.mult), reads=[r_xs, r_dskip], writes=[r_t2] if hh == 0 else (), awrites=() if hh == 0 else [r_t2])
            yield
            S.op("dve", lambda e: e.tensor_tensor(out=v8(prev_f[:, c0:c0 + 512]), in0=v8(prev_f[:, c0:c0 + 512]), in1=cd8, op=ALU.mult), reads=[r_pf, r_sm], awrites=[r_pf])
            yield
            S.op("dve", lambda e: e.tensor_tensor(out=prev_f[:, c0:c0 + 512], in0=prev_f[:, c0:c0 + 512], in1=pS[:], op=ALU.add), reads=[r_pf, r_pS], awrites=[r_pf])
            yield
            S.op("act", lambda e: e.activation(out=prev_b[:, c0:c0 + 512], in_=prev_f[:, c0:c0 + 512], func=AF.Copy), reads=[r_pf, r_pO], awrites=[r_pb])
            yield
        S.op("pool", lambda e: e.tensor_tensor(out=t1[:], in0=t1[:], in1=t2[:], op=ALU.add), reads=[r_t1, r_t2], writes=[r_t1])
        yield
        S.op("pool", lambda e: e.tensor_tensor(out=t1[:], in0=t1[:], in1=z_t[:], op=ALU.mult), reads=[r_t1, r_z], writes=[r_t1])
        yield
        for g in range(4):
            S.op("act", lambda e: e.activation(out=junk[:], in_=t1[:, g * 256:(g + 1) * 256], func=AF.Square, accum_out=sm[:, 5, g:g + 1]),
                 reads=[r_t1], writes=[r_junk], awrites=[r_sm])
            yield
        S.op("act", lambda e: e.activation(out=sm[:, 5, 4:8], in_=sm[:, 5, 0:4], func=AF.Sqrt, scale=1.0 / 256.0, bias=EPS), reads=[r_sm], awrites=[r_sm])
        yield
        S.op("dve", lambda e: e.reciprocal(out=sm[:, 5, 8:12], in_=sm[:, 5, 4:8]), reads=[r_sm], awrites=[r_sm])
        yield
        S.op("dve", lambda e: e.tensor_tensor(out=t1[:].rearrange("p (g q) -> p g q", g=4), in0=t1[:].rearrange("p (g q) -> p g q", g=4),
                                             in1=sm[:, 5, 8:12].unsqueeze(2).to_broadcast([128, 4, 256]), op=ALU.mult), reads=[r_t1, r_sm], writes=[r_t1])
        yield
        S.op("dve", lambda e: e.tensor_tensor(out=ob[:], in0=t1[:], in1=normw_bc[:], op=ALU.mult), reads=[r_t1, r_normw], writes=[r_ob])
        yield
        pT, r_pT = bank[4]
        pT_b = pT[:].bitcast(BF16)
        for k in range(8):
            S.op("pe", lambda e: e.transpose(out=pT_b[:, k * 128:(k + 1) * 128], in_=ob[:, k * 128:(k + 1) * 128], identity=ident_b[:]),
                 reads=[r_ob, r_identb], writes=[r_pT] if k == 0 else (), awrites=() if k == 0 else [r_pT])
            yield
        o_t, r_o = obT[sl2]
        S.op("act", lambda e: e.activation(out=o_t[:].rearrange("p k t -> p (k t)"), in_=pT_b, func=AF.Copy), reads=[r_pT], writes=[r_o])
        yield
        S.dma("sp", obT_d[:, :, tok0:tok0 + 128].rearrange("c p t -> p c t"), o_t[:], reads=[r_o], awrites=[r_obT])
        yield

    load_chunk(0, 0, 0)
    load_chunk(0, 1, 1)
    interleave((chunk_gen(ci) for ci in range(nch)), B_STAGGER)
    C.pop()


def _t5_bucket_np(rel):
    half, max_exact = 16, 8
    ret = (rel > 0).astype(np.int32) * half
    n = np.abs(rel)
    nf = np.maximum(n, 1).astype(np.float32)
    large = max_exact + (np.log(nf / np.float32(max_exact)) / np.float32(np.log(128.0 / 8.0)) * np.float32(half - max_exact)).astype(np.int32)
    large = np.minimum(large, half - 1)
    return ret + np.where(n < max_exact, n, large)


def _t5_blocks(rel_bias):
    k = np.arange(128)[:, None]
    q = np.arange(128)[None, :]
    out = np.zeros((128, 2, 8, 128), np.float32)
    for dl in range(2):
        bk = _t5_bucket_np((k - 128 * dl) - q)
        for h in range(8):
            out[:, dl, h, :] = rel_bias[bk, h]
    return out


def host_inputs(inputs, core):
    b0 = core * NB
    f = lambda a: np.ascontiguousarray(a, dtype=np.float32)
    c = inputs["c"][b0:b0 + NB]
    m = {
        "x": f(inputs["x"][b0:b0 + NB].reshape(NTOK, D)),
        "cT": f(c.reshape(NB, 8, 128).transpose(2, 1, 0)),
        "w_mod": f(inputs["w_mod"][0]),
        "b_mod": f(inputs["b_mod"][0].reshape(1, -1)),
        "w_in": f(inputs["w_in"][0]),
        "ident": np.eye(128, dtype=np.float32),
        "kv_norm_w": f(inputs["kv_norm_w"][0].reshape(1, -1)),
        "idx_k_norm_w": f(inputs["idx_k_norm_w"][0].reshape(1, -1)),
        "idx_k_norm_b": f(inputs["idx_k_norm_b"][0].reshape(1, -1)),
        "dt_bias": f(inputs["dt_bias"][0].reshape(1, -1)),
        "convw": f(inputs["conv_w"][0].reshape(4, 16, 128).transpose(2, 1, 0)),
        "convb": f(inputs["conv_b"][0].reshape(16, 128).T),
        "a_log": f(inputs["a_log"][0].reshape(1, -1)),
        "d_skip": f(inputs["d_skip"][0].reshape(1, -1)),
        "ssm_norm_w": f(inputs["ssm_norm_w"][0].reshape(1, -1)),
        "w_proj_a": f(inputs["w_proj_a"][0]), "w_proj_b": f(inputs["w_proj_b"][0]), "w_out": f(inputs["w_out"][0]),
        "ln1_g": f(inputs["ln1_g"][0].reshape(1, -1)), "ln1_b": f(inputs["ln1_b"][0].reshape(1, -1)),
        "ln2_g": f(inputs["ln2_g"][0].reshape(1, -1)), "ln2_b": f(inputs["ln2_b"][0].reshape(1, -1)),
        "w_router": f(inputs["w_router"][0]), "b_router": f(inputs["b_router"][0].reshape(1, -1)),
        "w1": f(inputs["w1"][0].reshape(NE * D, 2 * D)), "w2": f(inputs["w2"][0].reshape(NE * D, D)),
        "b1r": f(inputs["b1"][0].reshape(NE, 16, 128).transpose(0, 2, 1).reshape(NE * 128, 16)),
        "b2": f(inputs["b2"][0]),
        "sut": np.triu(np.ones((128, 128), np.float32), 1),
        "thr16": np.tile(512.0 * np.arange(16, dtype=np.float32), NE).reshape(1, -1),
        "bstart": (512.0 * np.arange(NBLK, dtype=np.float32)).reshape(1, -1),
        "kp": (np.arange(8, dtype=np.float32)[None, :] * 128 + np.arange(128, dtype=np.float32)[:, None]),
        "pcol": np.arange(128, dtype=np.float32).reshape(128, 1),
        "sut32": np.triu(np.ones((NE, NE), np.float32), 1),
        "tz": _t5_blocks(f(inputs["rel_bias"])),
        "cfar": f(inputs["rel_bias"][15:16, :]),
        "triU": np.triu(np.ones((128, 128), np.float32)),
        "SL": np.tril(np.ones((128, 128), np.float32), -1),
        "negm4": np.tile(np.tril(np.full((128, 128), NEG, np.float32), -1), (1, 4)),
    }
    return m


def kernel(**inputs):
    nc = build_program()
    in_maps = [host_inputs(inputs, c) for c in range(NCORES)]
    res = run_bass_kernel_spmd(nc, in_maps, core_ids=list(range(NCORES)))
    out = np.stack([np.asarray(r["out"]).reshape(NB, SEQ, D) for r in res.results], 0)
    return out.reshape(NCORES * NB, SEQ, D).astype(np.float32)
```

```python
import numpy as np
import concourse.bass as bass
import concourse.mybir as mybir
from concourse.bass_utils import run_bass_kernel_spmd

F32 = mybir.dt.float32
BF16 = mybir.dt.bfloat16
I32 = mybir.dt.int32
ALU = mybir.AluOpType
AF = mybir.ActivationFunctionType
AX = mybir.AxisListType

NCORES = 8
SEQ = 2048
D = 1024
NB = 4
NTOK = NB * SEQ
NT = NTOK // 128
TPS = SEQ // 128
DIN = 6872
C_Q, C_KV, C_IQ, C_IK, C_IW, C_Z, C_XBC, C_DT, C_GA, C_GB = 0, 1024, 1152, 1664, 1728, 1736, 2760, 4808, 4824, 5848
NE = 32
NBLK = NTOK * 4 // 512 + NE
ALPHA = 2.0 ** 0.25
EPS = 1e-5
NEG = -30000.0

B_STAGGER = 104
ENGS = ("pe", "act", "dve", "pool", "sp")


class Res:
    __slots__ = ("name", "writers", "readers", "dsem", "dcount", "dram")

    def __init__(self, name):
        self.name = name
        self.dram = False
        self.writers = {}
        self.readers = {}
        self.dsem = None
        self.dcount = 0


class Sched:
    def __init__(self, nc):
        self.nc = nc
        self.eng = {"pe": nc.tensor, "act": nc.scalar, "dve": nc.vector,
                    "pool": nc.gpsimd, "sp": nc.sync}
        self.sem = {e: nc.alloc_semaphore("prog_" + e) for e in ENGS}
        self.cnt = {e: 0 for e in ENGS}
        self.waited = {e: {} for e in ENGS}
        self.all_res = []
        self.nwaits = 0
        self.nops = 0
        self.sempool = []

    def retire(self, rs):
        for r in rs:
            if r.dsem is not None:
                self.sempool.append((r.dsem, r.dcount))
                r.dsem = None
            if r in self.all_res:
                self.all_res.remove(r)

    def res(self, name):
        r = Res(name)
        self.all_res.append(r)
        return r

    def _need(self, eng, tok, deps):
        sem, val = tok
        k = sem.num
        if self.waited[eng].get(k, 0) >= val:
            return
        if k not in deps or deps[k][1] < val:
            deps[k] = (sem, val)

    def op(self, eng, fn, reads=(), writes=(), awrites=(), dma=False):
        deps = {}
        mykey = None if dma else eng
        for r in reads:
            for k, tok in r.writers.items():
                if k == mykey and eng == "pe":
                    continue
                self._need(eng, tok, deps)
        for r in writes:
            for k, tok in list(r.writers.items()) + list(r.readers.items()):
                if k == mykey:
                    continue
                self._need(eng, tok, deps)
        for r in awrites:
            for k, tok in r.readers.items():
                if k == mykey:
                    continue
                self._need(eng, tok, deps)
        e = self.eng[eng]
        for k, (sem, val) in deps.items():
            e.wait_ge(sem, val)
            self.waited[eng][k] = val
            self.nwaits += 1
        ins = fn(e)
        self.nops += 1
        if dma:
            dst = (list(writes) + list(awrites))[0]
            if dst.dram:
                sb = [r for r in reads if not r.dram]
                if sb:
                    dst = sb[0]
            if dst.dsem is None:
                if self.sempool:
                    dst.dsem, dst.dcount = self.sempool.pop()
                else:
                    dst.dsem = self.nc.alloc_semaphore("d_" + dst.name)
            dst.dcount += 16
            ins.then_inc(dst.dsem, 16)
            tok = (dst.dsem, dst.dcount)
            key = "dma%d" % dst.dsem.num
        else:
            self.cnt[eng] += 1
            ins.then_inc(self.sem[eng], 1)
            tok = (self.sem[eng], self.cnt[eng])
            key = eng
        for r in reads:
            r.readers[key] = tok
        for r in writes:
            r.writers = {key: tok}
            r.readers = {}
        for r in awrites:
            r.writers[key] = tok
        return ins

    def dma(self, eng, out, in_, reads=(), writes=(), awrites=(), **kw):
        return self.op(eng, lambda e: e.dma_start(out=out, in_=in_, **kw),
                       reads=reads, writes=writes, awrites=awrites, dma=True)

    def barrier(self):
        toks = {}
        for e in ENGS:
            if self.cnt[e]:
                toks[self.sem[e].num] = (self.sem[e], self.cnt[e])
        for r in self.all_res:
            if r.dsem is not None and r.dcount:
                toks[r.dsem.num] = (r.dsem, r.dcount)
        for e in ENGS:
            for k, (sem, val) in toks.items():
                if self.waited[e].get(k, 0) >= val:
                    continue
                self.eng[e].wait_ge(sem, val)
                self.waited[e][k] = val
                self.nwaits += 1
        for r in self.all_res:
            r.writers = {}
            r.readers = {}


class Ctx:
    def __init__(self, nc, S):
        self.nc = nc
        self.S = S
        self.stack = []

    def push(self):
        self.stack.append([])

    def pop(self):
        self.S.barrier()
        gs = self.stack.pop()
        self.S.retire([r for (_, r) in gs])
        for g, _ in reversed(gs):
            g.__exit__(None, None, None)

    def sb(self, name, shape, dt):
        g = self.nc.sbuf_tensor("s_" + name, list(shape), dt)
        t = g.__enter__()
        r = self.S.res(name)
        self.stack[-1].append((g, r))
        return t, r

    def ps(self, name, shape, dt=F32):
        g = self.nc.psum_tensor("p_" + name, list(shape), dt)
        t = g.__enter__()
        r = self.S.res(name)
        self.stack[-1].append((g, r))
        return t, r

    def ring(self, kind, name, n, shape, dt):
        f = self.sb if kind == "sb" else self.ps
        return [f("%s%d" % (name, i), shape, dt) for i in range(n)]


def interleave(gens, stagger):
    active = []
    it = iter(gens)
    nxt = next(it, None)
    tick = 0
    while active or nxt is not None:
        if nxt is not None and tick % stagger == 0:
            active.append(nxt)
            nxt = next(it, None)
        for g in list(active):
            try:
                next(g)
            except StopIteration:
                active.remove(g)
        tick += 1


def build_program(debug=()):
    nc = bass.Bass("TRN2", target_bir_lowering=False)
    S = Sched(nc)
    C = Ctx(nc, S)
    dbg = set(debug)

    def din(name, shape, dt=F32):
        return nc.dram_tensor(name, list(shape), dt, kind="ExternalInput").ap()

    def scratch(name, shape, dt):
        kind = "ExternalOutput" if name in dbg else "Internal"
        r = S.res(name)
        r.dram = True
        return nc.dram_tensor(name, list(shape), dt, kind=kind).ap(), r

    x_d = din("x", [NTOK, D])
    cT_d = din("cT", [128, 8, NB])
    wmod_d = din("w_mod", [D, 6 * D])
    bmod_d = din("b_mod", [1, 6 * D])
    win_d = din("w_in", [D, DIN])
    ident_d = din("ident", [128, 128])
    kvw_d = din("kv_norm_w", [1, 128])
    ikw_d = din("idx_k_norm_w", [1, 64])
    ikb_d = din("idx_k_norm_b", [1, 64])
    dtb_d = din("dt_bias", [1, 16])
    convw_d = din("convw", [128, 16, 4])
    convb_d = din("convb", [128, 16])
    alog_d = din("a_log", [1, 16])
    dskip_d = din("d_skip", [1, 16])
    normw_d = din("ssm_norm_w", [1, D])
    triU_d = din("triU", [128, 128])
    SL_d = din("SL", [128, 128])
    negm4_d = din("negm4", [128, 512])
    tz_d = din("tz", [128, 2, 8, 128])
    cfar_d = din("cfar", [1, 8])
    wpa_d = din("w_proj_a", [D, D]); wpb_d = din("w_proj_b", [D, D]); wout_d = din("w_out", [D, D])
    ln1g_d = din("ln1_g", [1, D]); ln1b_d = din("ln1_b", [1, D]); ln2g_d = din("ln2_g", [1, D]); ln2b_d = din("ln2_b", [1, D])
    wr_d = din("w_router", [D, NE]); br_d = din("b_router", [1, NE])
    w1_d = din("w1", [NE * D, 2 * D]); w2_d = din("w2", [NE * D, D])
    b1r_d = din("b1r", [NE * 128, 16]); b2_d = din("b2", [NE, D])
    sut_d = din("sut", [128, 128]); thr16_d = din("thr16", [1, NE * 16]); bstart_d = din("bstart", [1, NBLK])
    kp_d = din("kp", [128, 8]); pcol_d = din("pcol", [128, 1]); sut32_d = din("sut32", [NE, NE])
    r_in = S.res("inputs")
    r_in.dram = True
    out_d = nc.dram_tensor("out", [NTOK, D], F32, kind="ExternalOutput").ap()
    r_out = S.res("out")
    r_out.dram = True

    mod_d, r_mod = scratch("mod_s", [NB, 6 * D], F32)
    qT_d, r_qT = scratch("qT_s", [8, 128, NTOK], BF16)
    iqT_d, r_iqT = scratch("iqT_s", [4, 128, NTOK], BF16)
    xbcT_d, r_xbcT = scratch("xbcT_s", [16, 128, NTOK], BF16)
    sgT_d, r_sgT = scratch("sgT_s", [16, 128, NTOK], BF16)
    kva_d, r_kva = scratch("kva_s", [NTOK, 136], BF16)
    kvT_d, r_kvT = scratch("kvT_s", [128, NTOK], BF16)
    ikT_d, r_ikT = scratch("ikT_s", [128, NTOK], BF16)
    iw_d, r_iw = scratch("iw_s", [NTOK, 8], F32)
    dt_d, r_dt = scratch("dt_s", [NTOK, 16], F32)
    zs_d, r_zs = scratch("zs_s", [NTOK, D], BF16)
    obT_d, r_obT = scratch("obT_s", [8, 128, NTOK], BF16)
    oaT_d, r_oaT = scratch("oaT_s", [8, 128, NTOK], BF16)
    mT_d, r_mTd = scratch("mT_s", [8, 128, NTOK], BF16)
    x1_d, r_x1 = scratch("x1_s", [NTOK, D], F32)
    u2_d, r_u2 = scratch("u2_s", [NTOK, D], BF16)
    xs_d, r_xsd = scratch("xsort_s", [NBLK * 512, D], BF16)
    ys_d, r_ysd = scratch("ysort_s", [NBLK * 512, D], BF16)

    w1b_d, r_w1bd = scratch("w1b_s", [NE * D, 2 * D], BF16)
    w2b_d, r_w2bd = scratch("w2b_s", [NE * D, D], BF16)

    C.push()
    ident_f, r_identf = C.sb("ident_f", [128, 128], F32)
    ident_b, r_identb = C.sb("ident_b", [128, 128], BF16)
    S.dma("sp", ident_f[:], ident_d, reads=[r_in], writes=[r_identf])
    S.op("dve", lambda e: e.tensor_copy(out=ident_b[:], in_=ident_f[:]), reads=[r_identf], writes=[r_identb])
    modT, r_modT = C.sb("modT", [128, 48, NB], F32)

    C.push()
    cT, r_cT = C.sb("cT", [128, 8, NB], F32)
    ones1, r_ones1 = C.sb("ones1", [1, NB], F32)
    bmod, r_bmod = C.sb("bmod", [1, 6 * D], F32)
    modrow, r_modrow = C.sb("modrow", [NB, 6 * D], F32)
    wm = C.ring("sb", "wm", 2, [128, 8, 512], F32)
    pmod = C.ring("ps", "pmod", 2, [NB, 512], F32)
    S.dma("sp", cT[:], cT_d, reads=[r_in], writes=[r_cT])
    S.dma("sp", bmod[:], bmod_d, reads=[r_in], writes=[r_bmod])
    S.op("act", lambda e: e.activation(out=cT[:], in_=cT[:], func=AF.Silu), reads=[r_cT], writes=[r_cT])
    S.op("dve", lambda e: e.memset(ones1[:], 1.0), writes=[r_ones1])
    for g in range(12):
        wt, r_wt = wm[g % 2]
        pt, r_pt = pmod[g % 2]
        S.dma("sp", wt[:], wmod_d[:, g * 512:(g + 1) * 512].rearrange("(k p) n -> p k n", p=128), reads=[r_in], writes=[r_wt])
        for k in range(8):
            S.op("pe", lambda e: e.matmul(pt[:], lhsT=cT[:, k, :], rhs=wt[:, k, :], start=(k == 0), stop=False),
                 reads=[r_cT, r_wt], writes=[r_pt] if k == 0 else (), awrites=() if k == 0 else [r_pt])
        S.op("pe", lambda e: e.matmul(pt[:], lhsT=ones1[:], rhs=bmod[:, g * 512:(g + 1) * 512], start=False, stop=True),
             reads=[r_ones1, r_bmod], awrites=[r_pt])
        S.op("act", lambda e: e.activation(out=modrow[:, g * 512:(g + 1) * 512], in_=pt[:], func=AF.Copy), reads=[r_pt], awrites=[r_modrow])
    S.dma("sp", mod_d, modrow[:], reads=[r_modrow], writes=[r_mod])
    pmt, r_pmt = pmod[0]
    pmt2, r_pmt2 = C.ps("pmodT", [128, 48 * NB], F32)
    for j in range(48):
        S.op("pe", lambda e: e.transpose(out=pmt2[:, j * NB:(j + 1) * NB], in_=modrow[:, j * 128:(j + 1) * 128], identity=ident_f[0:NB, 0:NB]),
             reads=[r_modrow, r_identf], writes=[r_pmt2] if j == 0 else (), awrites=() if j == 0 else [r_pmt2])
    S.op("act", lambda e: e.activation(out=modT[:].rearrange("p j b -> p (j b)"), in_=pmt2[:], func=AF.Copy), reads=[r_pmt2], writes=[r_modT])
    S.op("dve", lambda e: e.tensor_scalar_add(out=modT[:, 8:16, :], in0=modT[:, 8:16, :], scalar1=1.0), reads=[r_modT], awrites=[r_modT])
    S.op("dve", lambda e: e.tensor_scalar_add(out=modT[:, 32:40, :], in0=modT[:, 32:40, :], scalar1=1.0), reads=[r_modT], awrites=[r_modT])
    C.pop()
    if "stop0" in dbg:
        C.pop()
        return nc

    C.push()
    wI, r_wI = C.sb("wI", [128, 8, DIN], BF16)
    for i, (a, b_) in enumerate([(0, 1024), (1024, 1736), (1736, 2760), (2760, 3784), (3784, 4808), (4808, 5848), (5848, 6872)]):
        S.dma("pool", wI[:, :, a:b_], win_d[:, a:b_].rearrange("(k p) n -> p k n", p=128), reads=[r_in],
              writes=[r_wI] if i == 0 else (), awrites=() if i == 0 else [r_wI])
    kvw_bc, r_kvw = C.sb("kvw_bc", [128, 128], F32)
    ikw_bc, r_ikw = C.sb("ikw_bc", [128, 64], F32)
    ikb_bc, r_ikb = C.sb("ikb_bc", [128, 64], F32)
    dtb_bc, r_dtb = C.sb("dtb_bc", [128, 16], F32)
    S.dma("sp", kvw_bc[:], kvw_d.partition_broadcast(128), reads=[r_in], writes=[r_kvw])
    S.dma("sp", ikw_bc[:], ikw_d.partition_broadcast(128), reads=[r_in], writes=[r_ikw])
    S.dma("sp", ikb_bc[:], ikb_d.partition_broadcast(128), reads=[r_in], writes=[r_ikb])
    S.dma("sp", dtb_bc[:], dtb_d.partition_broadcast(128), reads=[r_in], writes=[r_dtb])

    xt = C.ring("sb", "xt", 2, [128, D], F32)
    xn = C.ring("sb", "xn", 2, [128, D], BF16)
    st = C.ring("sb", "st", 2, [128, 2, 6], F32)
    mv = C.ring("sb", "mv", 2, [128, 2], F32)
    rs = C.ring("sb", "rs", 2, [128, 1], F32)
    uT = C.ring("sb", "uT", 2, [128, 8, 512], BF16)
    ptr = C.ring("ps", "ptr", 1, [128, 8, 128], BF16)
    psm = C.ring("ps", "psm", 1, [128, 512], F32)
    pz = C.ring("ps", "pz", 2, [128, 512], F32)
    pf = C.ring("ps", "pf", 3, [128, 512], F32)
    pt2 = C.ring("ps", "pt2", 1, [128, 2, 128], BF16)
    kva = C.ring("sb", "kva", 2, [128, 136], BF16)
    ikn = C.ring("sb", "ikn", 2, [128, 128], BF16)
    sml = C.ring("sb", "sml", 2, [128, 64], F32)
    ikf = C.ring("sb", "ikf", 2, [128, 64], F32)
    iwt = C.ring("sb", "iwt", 2, [128, 8], F32)
    dtt = C.ring("sb", "dtt", 2, [128, 4, 16], F32)
    zst = C.ring("sb", "zst", 2, [128, D], BF16)
    tT = C.ring("sb", "tT", 2, [128, 2, 128], BF16)
    stg = C.ring("sb", "stg", 2, [128, 8, 512], BF16)
    for i in range(2):
        S.op("pool", lambda e: e.memset(kva[i][0][:, 128:136], 1.0), writes=[kva[i][1]])

    def ln_stats(src, r_src, i):
        st_t, r_st = st[i]
        mv_t, r_mv = mv[i]
        rs_t, r_rs = rs[i]
        for j in range(2):
            S.op("dve", lambda e: e.bn_stats(out=st_t[:, j, :], in_=src[:, j * 512:(j + 1) * 512]), reads=[r_src],
                 writes=[r_st] if j == 0 else (), awrites=() if j == 0 else [r_st])
        S.op("dve", lambda e: e.bn_aggr(out=mv_t[:], in_=st_t[:].rearrange("p a b -> p (a b)")), reads=[r_st], writes=[r_mv])
        S.op("act", lambda e: e.activation(out=rs_t[:], in_=mv_t[:, 1:2], func=AF.Sqrt, bias=EPS), reads=[r_mv], writes=[r_rs])
        S.op("dve", lambda e: e.reciprocal(out=rs_t[:], in_=rs_t[:]), reads=[r_rs], writes=[r_rs])
        return mv_t, r_mv, rs_t, r_rs

    tcount = 0
    for g in range(NTOK // 512):
        b = (g * 512) // SEQ
        u_t, r_u = uT[g % 2]
        for i4 in range(4):
            t = g * 4 + i4
            tok0 = t * 128
            ri = tcount % 2
            tcount += 1
            x_t, r_x = xt[ri]
            xn_t, r_xn = xn[ri]
            if t == 0:
                S.dma("sp", x_t[:], x_d[0:128, :], reads=[r_in], writes=[r_x])
            if t + 1 < NT:
                S.dma("sp", xt[(ri + 1) % 2][0][:], x_d[tok0 + 128:tok0 + 256, :], reads=[r_in], writes=[xt[(ri + 1) % 2][1]])
            mv_t, r_mv, rs_t, r_rs = ln_stats(x_t, r_x, ri)
            S.op("dve", lambda e: e.tensor_scalar(out=xn_t[:], in0=x_t[:], scalar1=mv_t[:, 0:1], scalar2=rs_t[:], op0=ALU.subtract, op1=ALU.mult),
                 reads=[r_x, r_mv, r_rs], writes=[r_xn])
            p_t, r_p = ptr[0]
            for k in range(8):
                S.op("pe", lambda e: e.transpose(out=p_t[:, k, :], in_=xn_t[:, k * 128:(k + 1) * 128], identity=ident_b[:]),
                     reads=[r_xn, r_identb], writes=[r_p] if k == 0 else (), awrites=() if k == 0 else [r_p])
            for k in range(8):
                S.op("act", lambda e: e.activation(out=u_t[:, k, i4 * 128:(i4 + 1) * 128], in_=p_t[:, k, :], func=AF.Identity,
                                                   scale=modT[:, 8 + k, b:b + 1], bias=modT[:, k, b:b + 1]),
                     reads=[r_p, r_modT], writes=[r_u] if (k == 0 and i4 == 0) else (), awrites=() if (k == 0 and i4 == 0) else [r_u])
            ps_t, r_ps = psm[0]
            for (c0, c1, o0) in [(C_KV, C_KV + 128, 0), (C_IK, C_IK + 72, 128), (C_DT, C_DT + 16, 200)]:
                for k in range(8):
                    S.op("pe", lambda e: e.matmul(ps_t[:, o0:o0 + (c1 - c0)], lhsT=u_t[:, k, i4 * 128:(i4 + 1) * 128], rhs=wI[:, k, c0:c1], start=(k == 0), stop=(k == 7)),
                         reads=[r_u, r_wI], writes=[r_ps] if (k == 0 and o0 == 0) else (), awrites=() if (k == 0 and o0 == 0) else [r_ps])
            zp = []
            for h in range(2):
                pz_t, r_pz = pz[h]
                zp.append((pz_t, r_pz))
                for k in range(8):
                    S.op("pe", lambda e: e.matmul(pz_t[:], lhsT=u_t[:, k, i4 * 128:(i4 + 1) * 128], rhs=wI[:, k, C_Z + h * 512:C_Z + (h + 1) * 512], start=(k == 0), stop=(k == 7)),
                         reads=[r_u, r_wI], writes=[r_pz] if k == 0 else (), awrites=() if k == 0 else [r_pz])
            sm_t, r_sm = sml[ri]
            kva_t, r_kva_t = kva[ri]
            ikf_t, r_ikf = ikf[ri]
            S.op("act", lambda e: e.activation(out=ikf_t[:, 0:64], in_=ps_t[:, 0:64], func=AF.Square, accum_out=sm_t[:, 0:1]), reads=[r_ps], writes=[r_ikf, r_sm])
            S.op("act", lambda e: e.activation(out=ikf_t[:, 0:64], in_=ps_t[:, 64:128], func=AF.Square, accum_out=sm_t[:, 1:2]), reads=[r_ps], writes=[r_ikf], awrites=[r_sm])
            S.op("dve", lambda e: e.tensor_tensor(out=sm_t[:, 0:1], in0=sm_t[:, 0:1], in1=sm_t[:, 1:2], op=ALU.add), reads=[r_sm], awrites=[r_sm])
            S.op("act", lambda e: e.activation(out=sm_t[:, 2:3], in_=sm_t[:, 0:1], func=AF.Sqrt, scale=1.0 / 128.0, bias=EPS), reads=[r_sm], awrites=[r_sm])
            S.op("dve", lambda e: e.reciprocal(out=sm_t[:, 3:4], in_=sm_t[:, 2:3]), reads=[r_sm], awrites=[r_sm])
            S.op("dve", lambda e: e.scalar_tensor_tensor(out=kva_t[:, 0:128], in0=ps_t[:, 0:128], scalar=sm_t[:, 3:4], in1=kvw_bc[:], op0=ALU.mult, op1=ALU.mult),
                 reads=[r_ps, r_sm, r_kvw], awrites=[r_kva_t])
            S.dma("sp", kva_d[tok0:tok0 + 128, :], kva_t[:], reads=[r_kva_t], awrites=[r_kva])
            ik_t, r_ik = ikn[ri]
            S.op("dve", lambda e: e.bn_stats(out=sm_t[:, 8:14], in_=ps_t[:, 128:192]), reads=[r_ps], awrites=[r_sm])
            S.op("dve", lambda e: e.bn_aggr(out=sm_t[:, 16:18], in_=sm_t[:, 8:14]), reads=[r_sm], awrites=[r_sm])
            S.op("act", lambda e: e.activation(out=sm_t[:, 18:19], in_=sm_t[:, 17:18], func=AF.Sqrt, bias=EPS), reads=[r_sm], awrites=[r_sm])
            S.op("dve", lambda e: e.reciprocal(out=sm_t[:, 19:20], in_=sm_t[:, 18:19]), reads=[r_sm], awrites=[r_sm])
            S.op("dve", lambda e: e.tensor_scalar(out=ikf_t[:], in0=ps_t[:, 128:192], scalar1=sm_t[:, 16:17], scalar2=sm_t[:, 19:20], op0=ALU.subtract, op1=ALU.mult),
                 reads=[r_ps, r_sm], writes=[r_ikf])
            S.op("dve", lambda e: e.tensor_tensor(out=ikf_t[:], in0=ikf_t[:], in1=ikw_bc[:], op=ALU.mult), reads=[r_ikf, r_ikw], writes=[r_ikf])
            S.op("dve", lambda e: e.tensor_tensor(out=ik_t[:, 0:64], in0=ikf_t[:], in1=ikb_bc[:], op=ALU.add), reads=[r_ikf, r_ikb], writes=[r_ik])
            S.op("dve", lambda e: e.tensor_copy(out=ik_t[:, 64:128], in_=ik_t[:, 0:64]), reads=[r_ik], awrites=[r_ik])
            iw_t, r_iwt = iwt[ri]
            S.op("act", lambda e: e.mul(out=iw_t[:], in_=ps_t[:, 192:200], mul=float(8 ** -0.5 * 64 ** -0.5)), reads=[r_ps], writes=[r_iwt])
            S.dma("sp", iw_d[tok0:tok0 + 128, :], iw_t[:], reads=[r_iwt], awrites=[r_iw])
            d_t, r_d = dtt[ri]
            S.op("dve", lambda e: e.tensor_tensor(out=d_t[:, 0, :], in0=ps_t[:, 200:216], in1=dtb_bc[:], op=ALU.add), reads=[r_ps, r_dtb], writes=[r_d])
            S.op("act", lambda e: e.activation(out=d_t[:, 1, :], in_=d_t[:, 0, :], func=AF.Abs), reads=[r_d], awrites=[r_d])
            S.op("act", lambda e: e.activation(out=d_t[:, 1, :], in_=d_t[:, 1, :], func=AF.Exp, scale=-1.0), reads=[r_d], awrites=[r_d])
            S.op("act", lambda e: e.activation(out=d_t[:, 1, :], in_=d_t[:, 1, :], func=AF.Ln, bias=1.0), reads=[r_d], awrites=[r_d])
            S.op("dve", lambda e: e.scalar_tensor_tensor(out=d_t[:, 2, :], in0=d_t[:, 0, :], scalar=0.0, in1=d_t[:, 1, :], op0=ALU.max, op1=ALU.add), reads=[r_d], awrites=[r_d])
            S.dma("sp", dt_d[tok0:tok0 + 128, :], d_t[:, 2, :], reads=[r_d], awrites=[r_dt])
            z_t, r_z = zst[ri]
            for h in range(2):
                S.op("act", lambda e: e.activation(out=z_t[:, h * 512:(h + 1) * 512], in_=zp[h][0][:], func=AF.Silu), reads=[zp[h][1]],
                     writes=[r_z] if h == 0 else (), awrites=() if h == 0 else [r_z])
            S.dma("sp", zs_d[tok0:tok0 + 128, :], z_t[:], reads=[r_z], awrites=[r_zs])
            p2, r_p2 = pt2[0]
            t_t, r_t = tT[ri]
            S.op("pe", lambda e: e.transpose(out=p2[:, 0, :], in_=kva_t[:, 0:128], identity=ident_b[:]), reads=[r_kva_t, r_identb], writes=[r_p2])
            S.op("pe", lambda e: e.transpose(out=p2[:, 1, :], in_=ik_t[:], identity=ident_b[:]), reads=[r_ik, r_identb], awrites=[r_p2])
            S.op("dve", lambda e: e.tensor_copy(out=t_t[:], in_=p2[:]), reads=[r_p2], writes=[r_t])
            S.dma("sp", kvT_d[:, tok0:tok0 + 128], t_t[:, 0, :], reads=[r_t], awrites=[r_kvT])
            S.dma("sp", ikT_d[:, tok0:tok0 + 128], t_t[:, 1, :], reads=[r_t], awrites=[r_ikT])
        g0 = g * 512
        fcount = 0
        for (c0, nch, dst, r_dst, ch0, func, scl) in [
                (C_Q, 8, qT_d, r_qT, 0, AF.Copy, float(128 ** -0.5)),
                (C_IQ, 4, iqT_d, r_iqT, 0, AF.Copy, 1.0),
                (C_XBC, 8, xbcT_d, r_xbcT, 0, AF.Copy, 1.0),
                (C_XBC + 1024, 8, xbcT_d, r_xbcT, 8, AF.Copy, 1.0),
                (C_GA, 8, sgT_d, r_sgT, 0, AF.Sigmoid, 1.0),
                (C_GB, 8, sgT_d, r_sgT, 8, AF.Sigmoid, 1.0)]:
            sg_t, r_sg = stg[fcount % 2]
            fcount += 1
            for j in range(nch):
                pf_t, r_pf = pf[j % 3]
                for k in range(8):
                    S.op("pe", lambda e: e.matmul(pf_t[:], lhsT=wI[:, k, c0 + j * 128:c0 + (j + 1) * 128], rhs=u_t[:, k, :], start=(k == 0), stop=(k == 7)),
                         reads=[r_u, r_wI], writes=[r_pf] if k == 0 else (), awrites=() if k == 0 else [r_pf])
                if func == AF.Copy and j % 2 == 1:
                    S.op("dve", lambda e: e.tensor_scalar_mul(out=sg_t[:, j, :], in0=pf_t[:], scalar1=scl), reads=[r_pf],
                         writes=[r_sg] if j == 0 else (), awrites=() if j == 0 else [r_sg])
                else:
                    S.op("act", lambda e: e.activation(out=sg_t[:, j, :], in_=pf_t[:], func=func, scale=scl), reads=[r_pf],
                         writes=[r_sg] if j == 0 else (), awrites=() if j == 0 else [r_sg])
            S.dma("sp", dst[ch0:ch0 + nch, :, g0:g0 + 512].rearrange("c p t -> p c t"), sg_t[:, 0:nch, :], reads=[r_sg], awrites=[r_dst])
    C.pop()
    if "stopA" in dbg:
        C.pop()
        return nc

    phase_B(nc, S, C, dbg, locals())
    if "stopB" in dbg:
        C.pop()
        return nc

    phase_C(nc, S, C, dbg, locals())
    if "stopC" in dbg:
        C.pop()
        return nc

    phase_DEF(nc, S, C, dbg, locals())
    C.pop()
    return nc


def phase_DEF(nc, S, C, dbg, L):
    g_ = lambda n: L[n]
    r_in = g_("r_in"); ident_b = g_("ident_b"); r_identb = g_("r_identb"); ident_f = g_("ident_f"); r_identf = g_("r_identf")
    modT = g_("modT"); r_modT = g_("r_modT"); mod_d = g_("mod_d"); r_mod = g_("r_mod")
    x_d = g_("x_d"); oaT_d, r_oaT = g_("oaT_d"), g_("r_oaT"); obT_d, r_obT = g_("obT_d"), g_("r_obT"); sgT_d, r_sgT = g_("sgT_d"), g_("r_sgT")
    x1_d, r_x1 = g_("x1_d"), g_("r_x1"); u2_d, r_u2 = g_("u2_d"), g_("r_u2"); xs_d, r_xsd = g_("xs_d"), g_("r_xsd"); ys_d, r_ysd = g_("ys_d"), g_("r_ysd")
    out_d, r_out = g_("out_d"), g_("r_out")
    b1r_d, b2_d = g_("b1r_d"), g_("b2_d")
    w1b_d, r_w1bd, w2b_d, r_w2bd = g_("w1b_d"), g_("r_w1bd"), g_("w2b_d"), g_("r_w2bd")

    C.push()
    idx4, r_idx4 = C.sb("idx4", [128, NT, 4], I32)
    g4, r_g4 = C.sb("g4", [128, NT, 4], F32)
    widx, r_widx = C.sb("widx", [128, NBLK, 8], I32)
    bidx, r_bidx = C.sb("bidx", [128, NBLK], I32)
    eidx, r_eidx = C.sb("eidx", [128, NBLK], I32)

    def ln_stats(src, r_src, st_t, r_st, mv_t, r_mv, rs_t, r_rs):
        for j in range(2):
            S.op("dve", lambda e: e.bn_stats(out=st_t[:, j, :], in_=src[:, j * 512:(j + 1) * 512]), reads=[r_src],
                 writes=[r_st] if j == 0 else (), awrites=() if j == 0 else [r_st])
        S.op("dve", lambda e: e.bn_aggr(out=mv_t[:], in_=st_t[:].rearrange("p a b -> p (a b)")), reads=[r_st], writes=[r_mv])
        S.op("act", lambda e: e.activation(out=rs_t[:], in_=mv_t[:, 1:2], func=AF.Sqrt, bias=EPS), reads=[r_mv], writes=[r_rs])
        S.op("dve", lambda e: e.reciprocal(out=rs_t[:], in_=rs_t[:]), reads=[r_rs], writes=[r_rs])

    mT_d, r_mTd = g_("mT_d"), g_("r_mTd")
    C.push()
    wpa, r_wpa = C.sb("wpa", [128, 8, D], BF16)
    wpb, r_wpb = C.sb("wpb", [128, 8, D], BF16)
    S.dma("pool", wpa[:], g_("wpa_d").rearrange("(k p) n -> p k n", p=128), reads=[r_in], writes=[r_wpa])
    S.dma("pool", wpb[:], g_("wpb_d").rearrange("(k p) n -> p k n", p=128), reads=[r_in], writes=[r_wpb])
    oaTr = C.ring("sb", "oaTd", 2, [128, 8, 512], BF16)
    obTr = C.ring("sb", "obTd", 2, [128, 8, 512], BF16)
    sgTr = C.ring("sb", "sgTd", 2, [128, 16, 512], BF16)
    mTr = C.ring("sb", "mT", 2, [128, 8, 512], BF16)
    ta = C.ring("sb", "ta", 3, [128, 512], F32)
    tb = C.ring("sb", "tb", 3, [128, 512], F32)
    pab = C.ring("ps", "pab", 4, [128, 512], F32)
    pbb = C.ring("ps", "pbb", 4, [128, 512], F32)

    def loadD1(g):
        g0 = g * 512
        S.dma("sp", oaTr[g % 2][0][:], oaT_d[:, :, g0:g0 + 512].rearrange("c p t -> p c t"), reads=[r_oaT], writes=[oaTr[g % 2][1]])
        S.dma("sp", obTr[g % 2][0][:], obT_d[:, :, g0:g0 + 512].rearrange("c p t -> p c t"), reads=[r_obT], writes=[obTr[g % 2][1]])
        S.dma("sp", sgTr[g % 2][0][:], sgT_d[:, :, g0:g0 + 512].rearrange("c p t -> p c t"), reads=[r_sgT], writes=[sgTr[g % 2][1]])

    NG = NTOK // 512
    loadD1(0)
    cn = 0
    for g in range(NG):
        g0 = g * 512
        if g + 1 < NG:
            loadD1(g + 1)
        oaT, r_oaTs = oaTr[g % 2]; obT, r_obTs = obTr[g % 2]; sgT, r_sgTs = sgTr[g % 2]; mT, r_mT = mTr[g % 2]
        for n in range(8):
            pa, r_pa = pab[cn % 4]
            pb, r_pb = pbb[cn % 4]
            ta_t, r_ta = ta[cn % 3]
            tb_t, r_tb = tb[cn % 3]
            cn += 1
            for k in range(8):
                S.op("pe", lambda e: e.matmul(pa[:], lhsT=wpa[:, k, n * 128:(n + 1) * 128], rhs=oaT[:, k, :], start=(k == 0), stop=(k == 7)),
                     reads=[r_wpa, r_oaTs], writes=[r_pa] if k == 0 else (), awrites=() if k == 0 else [r_pa])
            for k in range(8):
                S.op("pe", lambda e: e.matmul(pb[:], lhsT=wpb[:, k, n * 128:(n + 1) * 128], rhs=obT[:, k, :], start=(k == 0), stop=(k == 7)),
                     reads=[r_wpb, r_obTs], writes=[r_pb] if k == 0 else (), awrites=() if k == 0 else [r_pb])
            S.op("dve", lambda e: e.tensor_tensor(out=ta_t[:], in0=pa[:], in1=sgT[:, n, :], op=ALU.mult), reads=[r_pa, r_sgTs], writes=[r_ta])
            S.op("dve", lambda e: e.tensor_tensor(out=tb_t[:], in0=pb[:], in1=sgT[:, 8 + n, :], op=ALU.mult), reads=[r_pb, r_sgTs], writes=[r_tb])
            S.op("pool", lambda e: e.tensor_tensor(out=mT[:, n, :], in0=ta_t[:], in1=tb_t[:], op=ALU.add), reads=[r_ta, r_tb],
                 writes=[r_mT] if n == 0 else (), awrites=() if n == 0 else [r_mT])
        S.dma("sp", mT_d[:, :, g0:g0 + 512].rearrange("c p t -> p c t"), mT[:], reads=[r_mT], awrites=[r_mTd])
    C.pop()

    C.push()
    wout, r_wout = C.sb("wout", [128, 8, D], BF16)
    S.dma("pool", wout[:], g_("wout_d").rearrange("(k p) n -> p k n", p=128), reads=[r_in], writes=[r_wout])
    wr, r_wr = C.sb("wr", [128, 8, NE], F32)
    S.dma("sp", wr[:], g_("wr_d").rearrange("(k p) n -> p k n", p=128), reads=[r_in], writes=[r_wr])
    br_bc, r_br = C.sb("br_bc", [128, NE], F32)
    S.dma("sp", br_bc[:], g_("br_d").partition_broadcast(128), reads=[r_in], writes=[r_br])
    ln1g, r_ln1g = C.sb("ln1g", [128, D], F32)
    ln1b, r_ln1b = C.sb("ln1b", [128, D], F32)
    S.dma("sp", ln1g[:], g_("ln1g_d").partition_broadcast(128), reads=[r_in], writes=[r_ln1g])
    S.dma("sp", ln1b[:], g_("ln1b_d").partition_broadcast(128), reads=[r_in], writes=[r_ln1b])
    gater = C.ring("sb", "gate1", 2, [128, D], F32)
    sc2r = C.ring("sb", "sc2", 2, [128, D], F32)
    sh2r = C.ring("sb", "sh2", 2, [128, D], F32)
    sutf, r_sutf = C.sb("sutf", [128, 128], F32)
    sutb, r_sutb = C.sb("sutb", [128, 128], BF16)
    onesb, r_onesb = C.sb("onesb", [128, 128], BF16)
    S.dma("sp", sutf[:], g_("sut_d"), reads=[r_in], writes=[r_sutf])
    S.op("dve", lambda e: e.tensor_copy(out=sutb[:], in_=sutf[:]), reads=[r_sutf], writes=[r_sutb])
    S.op("dve", lambda e: e.memset(onesb[:], 1.0), writes=[r_onesb])
    maskall, r_maskall = C.sb("maskall", [128, NT, NE], F32)
    gall, r_gall = C.sb("gall", [128, NT, NE], F32)
    posall, r_posall = C.sb("posall", [128, NT, NE], F32)
    rrun, r_rrun = C.sb("rrun", [128, NE], F32)
    S.op("dve", lambda e: e.memset(rrun[:], 0.0), writes=[r_rrun])
    mtr = C.ring("sb", "mtl", 4, [128, 8, 128], BF16)
    xt = C.ring("sb", "xtd", 4, [128, D], F32)
    r1_r = C.ring("sb", "r1", 3, [128, D], F32)
    x1t_r = C.ring("sb", "x1t", 3, [128, D], F32)
    u2t_r = C.ring("sb", "u2t", 3, [128, D], F32)
    u2b = C.ring("sb", "u2b", 3, [128, D], BF16)
    u2T_r = C.ring("sb", "u2T", 3, [128, 8, 128], F32)
    st_r = C.ring("sb", "stD", 6, [128, 2, 6], F32)
    mv_r = C.ring("sb", "mvD", 6, [128, 2], F32)
    rs_r = C.ring("sb", "rsD", 6, [128, 1], F32)
    lg_r = C.ring("sb", "lg", 3, [128, NE], F32)
    m8_r = C.ring("sb", "m8D", 3, [128, 8], F32)
    sml_r = C.ring("sb", "smlD", 3, [128, 8], F32)
    ex_r = C.ring("sb", "exD", 3, [128, NE], F32)
    maskb_r = C.ring("sb", "maskb", 3, [128, NE], BF16)
    prs = C.ring("ps", "prs", 4, [128, 512], F32)
    ptT = C.ring("ps", "ptT", 2, [128, 512], F32)
    psl = C.ring("ps", "psl", 2, [128, 512], F32)

    def loadD2(t):
        tok0 = t * 128
        S.dma("sp", xt[t % 4][0][:], x_d[tok0:tok0 + 128, :], reads=[r_in], writes=[xt[t % 4][1]])
        S.dma("sp", mtr[t % 4][0][:], mT_d[:, :, tok0:tok0 + 128].rearrange("c p t -> p c t"), reads=[r_mTd], writes=[mtr[t % 4][1]])
        if t % TPS == 0:
            b = t // TPS
            gate1, r_gate1 = gater[b % 2]; sc2, r_sc2 = sc2r[b % 2]; sh2, r_sh2 = sh2r[b % 2]
            S.dma("sp", gate1[:], mod_d[b:b + 1, 2 * D:3 * D].partition_broadcast(128), reads=[r_mod], writes=[r_gate1])
            S.dma("sp", sh2[:], mod_d[b:b + 1, 3 * D:4 * D].partition_broadcast(128), reads=[r_mod], writes=[r_sh2])
            S.dma("sp", sc2[:], mod_d[b:b + 1, 4 * D:5 * D].partition_broadcast(128), reads=[r_mod], writes=[r_sc2])
            S.op("pool", lambda e: e.tensor_scalar_add(out=sc2[:], in0=sc2[:], scalar1=1.0), reads=[r_sc2], writes=[r_sc2])

    def stage1(t):
        tok0 = t * 128
        b = t // TPS
        gate1, r_gate1 = gater[b % 2]; sc2, r_sc2 = sc2r[b % 2]; sh2, r_sh2 = sh2r[b % 2]
        x_t, r_x = xt[t % 4]
        mt_t, r_mt = mtr[t % 4]
        r1, r_r1 = r1_r[t % 3]; x1t, r_x1t = x1t_r[t % 3]; u2t, r_u2t = u2t_r[t % 3]
        st, r_st = st_r[(2 * t) % 6]; mv, r_mv = mv_r[(2 * t) % 6]; rs, r_rs = rs_r[(2 * t) % 6]
        st2, r_st2 = st_r[(2 * t + 1) % 6]; mv2, r_mv2 = mv_r[(2 * t + 1) % 6]; rs2, r_rs2 = rs_r[(2 * t + 1) % 6]
        for hf in range(2):
            pr_t, r_pr = prs[(2 * t + hf) % 4]
            for n in range(8):
                S.op("pe", lambda e: e.matmul(pr_t[:], lhsT=mt_t[:, n, :], rhs=wout[:, n, hf * 512:(hf + 1) * 512], start=(n == 0), stop=(n == 7)),
                     reads=[r_mt, r_wout], writes=[r_pr] if n == 0 else (), awrites=() if n == 0 else [r_pr])
                yield
            S.op("dve", lambda e: e.tensor_tensor(out=r1[:, hf * 512:(hf + 1) * 512], in0=pr_t[:], in1=gate1[:, hf * 512:(hf + 1) * 512], op=ALU.mult),
                 reads=[r_pr, r_gate1], writes=[r_r1] if hf == 0 else (), awrites=() if hf == 0 else [r_r1])
            yield
        S.op("dve", lambda e: e.scalar_tensor_tensor(out=r1[:], in0=x_t[:], scalar=float(ALPHA), in1=r1[:], op0=ALU.mult, op1=ALU.add), reads=[r_x, r_r1], writes=[r_r1])
        yield
        ln_stats(r1, r_r1, st, r_st, mv, r_mv, rs, r_rs)
        yield
        S.op("dve", lambda e: e.tensor_scalar(out=x1t[:], in0=r1[:], scalar1=mv[:, 0:1], scalar2=rs[:], op0=ALU.subtract, op1=ALU.mult), reads=[r_r1, r_mv, r_rs], writes=[r_x1t])
        yield
        S.op("pool", lambda e: e.tensor_tensor(out=x1t[:], in0=x1t[:], in1=ln1g[:], op=ALU.mult), reads=[r_x1t, r_ln1g], writes=[r_x1t])
        yield
        S.op("pool", lambda e: e.tensor_tensor(out=x1t[:], in0=x1t[:], in1=ln1b[:], op=ALU.add), reads=[r_x1t, r_ln1b], writes=[r_x1t])
        yield
        S.dma("sp", x1_d[tok0:tok0 + 128, :], x1t[:], reads=[r_x1t], awrites=[r_x1])
        yield
        ln_stats(x1t, r_x1t, st2, r_st2, mv2, r_mv2, rs2, r_rs2)
        yield
        S.op("dve", lambda e: e.tensor_scalar(out=u2t[:], in0=x1t[:], scalar1=mv2[:, 0:1], scalar2=rs2[:], op0=ALU.subtract, op1=ALU.mult), reads=[r_x1t, r_mv2, r_rs2], writes=[r_u2t])
        yield
        S.op("pool", lambda e: e.tensor_tensor(out=u2t[:], in0=u2t[:], in1=sc2[:], op=ALU.mult), reads=[r_u2t, r_sc2], writes=[r_u2t])
        yield
        S.op("pool", lambda e: e.tensor_tensor(out=u2t[:], in0=u2t[:], in1=sh2[:], op=ALU.add), reads=[r_u2t, r_sh2], writes=[r_u2t])
        yield
        ub, r_ub = u2b[t % 3]
        S.op("act", lambda e: e.activation(out=ub[:], in_=u2t[:], func=AF.Copy), reads=[r_u2t], writes=[r_ub])
        yield
        S.dma("sp", u2_d[tok0:tok0 + 128, :], ub[:], reads=[r_ub], awrites=[r_u2])
        yield

    def stage2(t):
        u2t, r_u2t = u2t_r[t % 3]
        u2T, r_u2T = u2T_r[t % 3]
        lg, r_lg = lg_r[t % 3]; m8, r_m8 = m8_r[t % 3]; sml, r_sml = sml_r[t % 3]; ex, r_ex = ex_r[t % 3]; maskb, r_maskb = maskb_r[t % 3]
        for hf in range(2):
            pT, r_pT = (ptT[t % 2] if hf == 0 else psl[t % 2])
            for k4 in range(4):
                k = hf * 4 + k4
                S.op("pe", lambda e: e.transpose(out=pT[:, k4 * 128:(k4 + 1) * 128], in_=u2t[:, k * 128:(k + 1) * 128], identity=ident_f[:]),
                     reads=[r_u2t, r_identf], writes=[r_pT] if k4 == 0 else (), awrites=() if k4 == 0 else [r_pT])
                yield
            S.op("act", lambda e: e.activation(out=u2T[:, hf * 4:hf * 4 + 4, :].rearrange("p k t -> p (k t)"), in_=pT[:], func=AF.Copy), reads=[r_pT],
                 writes=[r_u2T] if hf == 0 else (), awrites=() if hf == 0 else [r_u2T])
            yield
        pl_, r_pl = psl[t % 2]
        for k in range(8):
            S.op("pe", lambda e: e.matmul(pl_[:, 0:NE], lhsT=u2T[:, k, :], rhs=wr[:, k, :], start=(k == 0), stop=(k == 7)),
                 reads=[r_u2T, r_wr], writes=[r_pl] if k == 0 else (), awrites=() if k == 0 else [r_pl])
            yield
        S.op("dve", lambda e: e.tensor_tensor(out=lg[:], in0=pl_[:, 0:NE], in1=br_bc[:], op=ALU.add), reads=[r_pl, r_br], writes=[r_lg])
        yield
        S.op("dve", lambda e: e.max(out=m8[:], in_=lg[:]), reads=[r_lg], writes=[r_m8])
        yield
        S.op("dve", lambda e: e.tensor_scalar(out=maskall[:, t, :], in0=lg[:], scalar1=m8[:, 3:4], scalar2=None, op0=ALU.is_ge), reads=[r_lg, r_m8], awrites=[r_maskall])
        yield
        S.op("dve", lambda e: e.tensor_scalar_mul(out=sml[:, 0:1], in0=m8[:, 0:1], scalar1=-1.0), reads=[r_m8], writes=[r_sml])
        yield
        S.op("act", lambda e: e.activation(out=ex[:], in_=lg[:], func=AF.Exp, bias=sml[:, 0:1]), reads=[r_lg, r_sml], writes=[r_ex])
        yield
        S.op("dve", lambda e: e.tensor_tensor(out=ex[:], in0=ex[:], in1=maskall[:, t, :], op=ALU.mult), reads=[r_ex, r_maskall], writes=[r_ex])
        yield
        S.op("dve", lambda e: e.reduce_sum(out=sml[:, 1:2], in_=ex[:], axis=AX.X), reads=[r_ex], awrites=[r_sml])
        yield
        S.op("dve", lambda e: e.reciprocal(out=sml[:, 2:3], in_=sml[:, 1:2]), reads=[r_sml], awrites=[r_sml])
        yield
        S.op("dve", lambda e: e.tensor_scalar(out=gall[:, t, :], in0=ex[:], scalar1=sml[:, 2:3], scalar2=None, op0=ALU.mult), reads=[r_ex, r_sml], awrites=[r_gall])
        yield
        S.op("dve", lambda e: e.tensor_copy(out=maskb[:], in_=maskall[:, t, :]), reads=[r_maskall], writes=[r_maskb])
        yield
        S.op("pe", lambda e: e.matmul(pl_[:, 64:64 + NE], lhsT=sutb[:], rhs=maskb[:], start=True, stop=True), reads=[r_sutb, r_maskb, r_lg], awrites=[r_pl])
        yield
        S.op("pe", lambda e: e.matmul(pl_[:, 128:128 + NE], lhsT=onesb[:], rhs=maskb[:], start=True, stop=True), reads=[r_onesb, r_maskb], awrites=[r_pl])
        yield
        S.op("dve", lambda e: e.tensor_tensor(out=posall[:, t, :], in0=pl_[:, 64:64 + NE], in1=rrun[:], op=ALU.add), reads=[r_pl, r_rrun], awrites=[r_posall])
        yield
        S.op("dve", lambda e: e.tensor_tensor(out=rrun[:], in0=pl_[:, 128:128 + NE], in1=rrun[:], op=ALU.add), reads=[r_pl, r_rrun], writes=[r_rrun])
        yield


    def tile_gen(t):
        if t + 2 < NT:
            loadD2(t + 2)
        yield from stage1(t)
        yield from stage2(t)

    for t0 in range(2):
        loadD2(t0)
    interleave((tile_gen(t) for t in range(NT)), 16)

    thr16, r_thr16 = C.sb("thr16", [128, NE, 16], F32)
    bstart, r_bstart = C.sb("bstart", [128, NBLK], F32)
    kp, r_kp = C.sb("kp", [128, 8], F32)
    pcol, r_pcol = C.sb("pcol", [128, 1], F32)
    sut32, r_sut32 = C.sb("sut32", [NE, NE], F32)
    S.dma("sp", thr16[:].rearrange("p e m -> p (e m)"), g_("thr16_d").partition_broadcast(128), reads=[r_in], writes=[r_thr16])
    S.dma("sp", bstart[:], g_("bstart_d").partition_broadcast(128), reads=[r_in], writes=[r_bstart])
    S.dma("sp", kp[:], g_("kp_d"), reads=[r_in], writes=[r_kp])
    S.dma("sp", pcol[:], g_("pcol_d"), reads=[r_in], writes=[r_pcol])
    S.dma("sp", sut32[:], g_("sut32_d"), reads=[r_in], writes=[r_sut32])
    big, r_big = C.sb("bigD", [128, NBLK * NE], F32)
    nbk, r_nbk = C.sb("nbk", [128, NE], F32)
    padT, r_padT = C.sb("padT", [NE, 128], F32)
    pstart, r_pstart = C.sb("pstart", [128, NE], F32)
    pend, r_pend = C.sb("pend", [128, NE], F32)
    bexp, r_bexp = C.sb("bexp", [128, NBLK], F32)
    wf, r_wf = C.sb("wf", [128, NBLK, 8], F32)
    S.op("dve", lambda e: e.tensor_tensor(out=big[:, 0:NE * 16].rearrange("p (e m) -> p e m", m=16), in0=rrun[:].unsqueeze(2).to_broadcast([128, NE, 16]), in1=thr16[:], op=ALU.is_gt),
         reads=[r_rrun, r_thr16], writes=[r_big])
    S.op("dve", lambda e: e.tensor_reduce(out=nbk[:], in_=big[:, 0:NE * 16].rearrange("p (e m) -> p e m", m=16), axis=AX.X, op=ALU.add), reads=[r_big], writes=[r_nbk])
    S.op("dve", lambda e: e.tensor_scalar_mul(out=nbk[:], in0=nbk[:], scalar1=512.0), reads=[r_nbk], writes=[r_nbk])
    pq, r_pq = psl[0]
    S.op("pe", lambda e: e.transpose(out=pq[0:NE, 0:128], in_=nbk[:], identity=ident_f[:]), reads=[r_nbk, r_identf], writes=[r_pq])
    S.op("act", lambda e: e.activation(out=padT[:], in_=pq[0:NE, 0:128], func=AF.Copy), reads=[r_pq], writes=[r_padT])
    S.op("pe", lambda e: e.matmul(pq[:, 256:256 + NE], lhsT=padT[:], rhs=sut32[:], start=True, stop=True), reads=[r_padT, r_sut32], awrites=[r_pq])
    S.op("act", lambda e: e.activation(out=pstart[:], in_=pq[:, 256:256 + NE], func=AF.Copy), reads=[r_pq], writes=[r_pstart])
    S.op("dve", lambda e: e.tensor_tensor(out=pend[:], in0=pstart[:], in1=nbk[:], op=ALU.add), reads=[r_pstart, r_nbk], writes=[r_pend])
    S.op("dve", lambda e: e.tensor_tensor(out=big[:].rearrange("p (i e) -> p i e", e=NE), in0=bstart[:].unsqueeze(2).to_broadcast([128, NBLK, NE]),
                                         in1=pend[:].unsqueeze(1).to_broadcast([128, NBLK, NE]), op=ALU.is_ge), reads=[r_bstart, r_pend], writes=[r_big])
    S.op("dve", lambda e: e.tensor_reduce(out=bexp[:], in_=big[:].rearrange("p (i e) -> p i e", e=NE), axis=AX.X, op=ALU.add), reads=[r_big], writes=[r_bexp])
    S.op("dve", lambda e: e.tensor_scalar_min(out=bexp[:], in0=bexp[:], scalar1=float(NE - 1)), reads=[r_bexp], writes=[r_bexp])
    S.op("dve", lambda e: e.tensor_copy(out=eidx[:], in_=bexp[:]), reads=[r_bexp], writes=[r_eidx])
    S.op("dve", lambda e: e.tensor_scalar(out=bidx[:], in0=bexp[:], scalar1=128.0, scalar2=pcol[:, 0:1], op0=ALU.mult, op1=ALU.add), reads=[r_bexp, r_pcol], writes=[r_bidx])
    S.op("dve", lambda e: e.tensor_scalar_mul(out=wf[:], in0=bexp[:].unsqueeze(2).to_broadcast([128, NBLK, 8]), scalar1=float(D)), reads=[r_bexp], writes=[r_wf])
    S.op("dve", lambda e: e.tensor_tensor(out=widx[:], in0=wf[:], in1=kp[:].unsqueeze(1).to_broadcast([128, NBLK, 8]), op=ALU.add), reads=[r_wf, r_kp], writes=[r_widx])
    sl_, r_sl = C.sb("slD", [128, NE], F32)
    eqt, r_eqt = C.sb("eqt", [128, NE], F32)
    m8b, r_m8b = C.sb("m8b", [128, 8], F32)
    S.op("dve", lambda e: e.tensor_scalar_add(out=pstart[:], in0=pstart[:], scalar1=1.0), reads=[r_pstart], writes=[r_pstart])
    for t0 in range(2):
        S.dma("sp", u2b[t0 % 3][0][:], u2_d[t0 * 128:t0 * 128 + 128, :], reads=[r_u2], writes=[u2b[t0 % 3][1]])
    for t in range(NT):
        tok0 = t * 128
        ub, r_ub = u2b[t % 3]
        if t + 2 < NT:
            S.dma("sp", u2b[(t + 2) % 3][0][:], u2_d[tok0 + 256:tok0 + 384, :], reads=[r_u2], writes=[u2b[(t + 2) % 3][1]])
        S.op("dve", lambda e: e.tensor_tensor(out=sl_[:], in0=posall[:, t, :], in1=pstart[:], op=ALU.add), reads=[r_posall, r_pstart], writes=[r_sl])
        S.op("dve", lambda e: e.tensor_tensor(out=sl_[:], in0=sl_[:], in1=maskall[:, t, :], op=ALU.mult), reads=[r_sl, r_maskall], writes=[r_sl])
        S.op("dve", lambda e: e.max(out=m8b[:], in_=sl_[:]), reads=[r_sl], writes=[r_m8b])
        for j in range(4):
            S.op("dve", lambda e: e.scalar_tensor_tensor(out=eqt[:], in0=sl_[:], scalar=m8b[:, j:j + 1], in1=gall[:, t, :], op0=ALU.is_equal, op1=ALU.mult, accum_out=g4[:, t, j:j + 1]),
                 reads=[r_sl, r_m8b, r_gall], writes=[r_eqt], awrites=[r_g4])
        S.op("dve", lambda e: e.tensor_scalar_add(out=idx4[:, t, :], in0=m8b[:, 0:4], scalar1=-1.0), reads=[r_m8b], awrites=[r_idx4])
        for j in range(4):
            S.op("pool", lambda e: e.indirect_dma_start(out=xs_d[:, :], out_offset=bass.IndirectOffsetOnAxis(ap=idx4[:, t, j:j + 1], axis=0), in_=ub[:], in_offset=None),
                 reads=[r_ub, r_idx4], awrites=[r_xsd], dma=True)
    C.pop()
    if "stopD" in dbg:
        C.pop()
        return

    C.push()
    w1b = C.ring("sb", "w1b", 2, [128, 8, 2 * D], BF16)
    w2b = C.ring("sb", "w2b", 2, [128, 8, D], BF16)
    b1t = C.ring("sb", "b1t", 2, [128, 16], F32)
    b2t = C.ring("sb", "b2t", 2, [128, D], F32)
    ones1f, r_ones1f = C.sb("ones1f", [1, 128], BF16)
    S.op("dve", lambda e: e.memset(ones1f[:], 1.0), writes=[r_ones1f])
    b2b = C.ring("sb", "b2b", 2, [1, D], BF16)
    xr = C.ring("sb", "xr", 8, [128, D], BF16)
    XTr = C.ring("sb", "XT", 2, [128, 8, 512], BF16)
    hg = C.ring("sb", "hg", 2, [128, 512], F32)
    hu = C.ring("sb", "hu", 2, [128, 512], F32)
    sgm = C.ring("sb", "sgm", 2, [128, 512], F32)
    actT, r_actT = C.sb("actT", [128, 8, 512], BF16)
    ysb = C.ring("sb", "ysb", 2, [128, D], BF16)
    pxt = C.ring("ps", "pxt", 2, [128, 512], F32)
    pg = C.ring("ps", "pg", 2, [128, 512], F32)
    pu = C.ring("ps", "pu", 2, [128, 512], F32)
    py = C.ring("ps", "py", 2, [128, 512], F32)

    def load_weights(i, slot):
        w1t, r_w1 = w1b[slot]
        w2t, r_w2 = w2b[slot]
        for k in range(8):
            S.op("pool", lambda e: e.indirect_dma_start(out=w1t[:, k, :], out_offset=None, in_=w1b_d[:, :], in_offset=bass.IndirectOffsetOnAxis(ap=widx[:, i, k:k + 1], axis=0)),
                 reads=[r_w1bd, r_widx], writes=[r_w1] if k == 0 else (), awrites=() if k == 0 else [r_w1], dma=True)
        for k in range(8):
            S.op("pool", lambda e: e.indirect_dma_start(out=w2t[:, k, :], out_offset=None, in_=w2b_d[:, :], in_offset=bass.IndirectOffsetOnAxis(ap=widx[:, i, k:k + 1], axis=0)),
                 reads=[r_w2bd, r_widx], writes=[r_w2] if k == 0 else (), awrites=() if k == 0 else [r_w2], dma=True)
        S.op("pool", lambda e: e.indirect_dma_start(out=b1t[slot][0][:], out_offset=None, in_=b1r_d[:, :], in_offset=bass.IndirectOffsetOnAxis(ap=bidx[:, i:i + 1], axis=0)),
             reads=[r_in, r_bidx], writes=[b1t[slot][1]], dma=True)
        S.op("pool", lambda e: e.indirect_dma_start(out=b2t[slot][0][:], out_offset=None, in_=b2_d[:, :], in_offset=bass.IndirectOffsetOnAxis(ap=eidx[:, i:i + 1], axis=0)),
             reads=[r_in, r_eidx], writes=[b2t[slot][1]], dma=True)

    def load_x(i):
        for s4 in range(4):
            x_t, r_x = xr[(i % 2) * 4 + s4]
            row0 = i * 512 + s4 * 128
            S.dma("sp", x_t[:], xs_d[row0:row0 + 128, :], reads=[r_xsd], writes=[r_x])

    nblk_run = NBLK if "nblk" not in L else L["nblk"]
    load_weights(0, 0)
    load_x(0)
    xstate = {"xc": 0}

    def xpose(i):
        XT, r_XT = XTr[i % 2]
        for s4 in range(4):
            x_t, r_x = xr[(i % 2) * 4 + s4]
            p_t, r_p = pxt[xstate["xc"] % 2]
            xstate["xc"] += 1
            p_b = p_t[:].bitcast(BF16)
            for k in range(8):
                S.op("pe", lambda e: e.transpose(out=p_b[:, k * 128:(k + 1) * 128], in_=x_t[:, k * 128:(k + 1) * 128], identity=ident_b[:]),
                     reads=[r_x, r_identb], writes=[r_p] if k == 0 else (), awrites=() if k == 0 else [r_p])
            S.op("act", lambda e: e.activation(out=XT[:, :, s4 * 128:(s4 + 1) * 128], in_=p_b.rearrange("p (k t) -> p k t", k=8), func=AF.Copy), reads=[r_p],
                 writes=[r_XT] if s4 == 0 else (), awrites=() if s4 == 0 else [r_XT])

    xpose(0)
    for i in range(nblk_run):
        slot = i % 2
        if i + 1 < nblk_run:
            load_weights(i + 1, (i + 1) % 2)
            load_x(i + 1)
        w1t, r_w1 = w1b[slot]
        w2t, r_w2 = w2b[slot]
        b1_t, r_b1 = b1t[slot]
        b2f_t, r_b2f = b2t[slot]
        b2_t, r_b2 = b2b[slot]
        XT, r_XT = XTr[i % 2]
        for fc in range(8):
            pg_t, r_pg = pg[fc % 2]
            pu_t, r_pu = pu[fc % 2]
            for k in range(8):
                S.op("pe", lambda e: e.matmul(pg_t[:], lhsT=w1t[:, k, fc * 128:(fc + 1) * 128], rhs=XT[:, k, :], start=(k == 0), stop=(k == 7)),
                     reads=[r_w1, r_XT], writes=[r_pg] if k == 0 else (), awrites=() if k == 0 else [r_pg])
            for k in range(8):
                S.op("pe", lambda e: e.matmul(pu_t[:], lhsT=w1t[:, k, D + fc * 128:D + (fc + 1) * 128], rhs=XT[:, k, :], start=(k == 0), stop=(k == 7)),
                     reads=[r_w1, r_XT], writes=[r_pu] if k == 0 else (), awrites=() if k == 0 else [r_pu])
            hg_t, r_hg = hg[fc % 2]
            hu_t, r_hu = hu[fc % 2]
            sg_t, r_sgm = sgm[fc % 2]
            S.op("act", lambda e: e.activation(out=hg_t[:], in_=pg_t[:], func=AF.Identity, bias=b1_t[:, fc:fc + 1]), reads=[r_pg, r_b1], writes=[r_hg])
            S.op("act", lambda e: e.activation(out=hu_t[:], in_=pu_t[:], func=AF.Identity, bias=b1_t[:, 8 + fc:9 + fc]), reads=[r_pu, r_b1], writes=[r_hu])
            S.op("dve", lambda e: e.tensor_scalar_min(out=hg_t[:], in0=hg_t[:], scalar1=7.0), reads=[r_hg], writes=[r_hg])
            S.op("act", lambda e: e.activation(out=sg_t[:], in_=hg_t[:], func=AF.Sigmoid, scale=1.702), reads=[r_hg], writes=[r_sgm])
            S.op("pool", lambda e: e.tensor_scalar(out=hu_t[:], in0=hu_t[:], scalar1=7.0, scalar2=-7.0, op0=ALU.min, op1=ALU.max), reads=[r_hu], writes=[r_hu])
            S.op("dve", lambda e: e.scalar_tensor_tensor(out=hu_t[:], in0=hu_t[:], scalar=1.0, in1=hg_t[:], op0=ALU.add, op1=ALU.mult), reads=[r_hu, r_hg], writes=[r_hu])
            S.op("dve", lambda e: e.tensor_tensor(out=actT[:, fc, :], in0=hu_t[:], in1=sg_t[:], op=ALU.mult), reads=[r_hu, r_sgm],
                 writes=[r_actT] if fc == 0 else (), awrites=() if fc == 0 else [r_actT])
        if i + 1 < nblk_run:
            xpose(i + 1)
        for s4 in range(4):
            y_t, r_y = ysb[s4 % 2]
            for hf in range(2):
                py_t, r_py = py[hf]
                for fc in range(8):
                    S.op("pe", lambda e: e.matmul(py_t[:], lhsT=actT[:, fc, s4 * 128:(s4 + 1) * 128], rhs=w2t[:, fc, hf * 512:(hf + 1) * 512], start=(fc == 0), stop=(fc == 7)),
                         reads=[r_actT, r_w2], writes=[r_py] if fc == 0 else (), awrites=() if fc == 0 else [r_py])
                S.op("dve", lambda e: e.tensor_tensor(out=y_t[:, hf * 512:(hf + 1) * 512], in0=py_t[:], in1=b2f_t[:, hf * 512:(hf + 1) * 512], op=ALU.add), reads=[r_py, r_b2f],
                     writes=[r_y] if hf == 0 else (), awrites=() if hf == 0 else [r_y])
            row0 = i * 512 + s4 * 128
            S.dma("act", ys_d[row0:row0 + 128, :], y_t[:], reads=[r_y], awrites=[r_ysd])
    C.pop()

    C.push()
    ln2g, r_ln2g = C.sb("ln2g", [128, D], F32)
    ln2b, r_ln2b = C.sb("ln2b", [128, D], F32)
    S.dma("sp", ln2g[:], g_("ln2g_d").partition_broadcast(128), reads=[r_in], writes=[r_ln2g])
    S.dma("sp", ln2b[:], g_("ln2b_d").partition_broadcast(128), reads=[r_in], writes=[r_ln2b])
    gate2r = C.ring("sb", "gate2r", 2, [128, D], F32)
    x1r = C.ring("sb", "x1r", 4, [128, D], F32)
    yg = C.ring("sb", "yg", 16, [128, D], BF16)
    accr = C.ring("sb", "accF", 3, [128, D], F32)
    ot = C.ring("sb", "otF", 3, [128, D], F32)
    stF = C.ring("sb", "stF", 3, [128, 2, 6], F32)
    mvF = C.ring("sb", "mvF", 3, [128, 4], F32)
    rsF = C.ring("sb", "rsF", 3, [128, 1], F32)

    def loadF(t):
        tok0 = t * 128
        slot = t % 4
        S.dma("sp", x1r[slot][0][:], x1_d[tok0:tok0 + 128, :], reads=[r_x1], writes=[x1r[slot][1]])
        for j in range(4):
            y_t, r_y = yg[slot * 4 + j]
            S.op("pool", lambda e: e.indirect_dma_start(out=y_t[:], out_offset=None, in_=ys_d[:, :], in_offset=bass.IndirectOffsetOnAxis(ap=idx4[:, t, j:j + 1], axis=0)),
                 reads=[r_ysd, r_idx4], writes=[r_y], dma=True)
        if t % TPS == 0:
            b_ = t // TPS
            S.dma("sp", gate2r[b_ % 2][0][:], mod_d[b_:b_ + 1, 5 * D:6 * D].partition_broadcast(128), reads=[r_mod], writes=[gate2r[b_ % 2][1]])

    def genF(t):
        if t + 3 < NT:
            loadF(t + 3)
        slot = t % 4
        tok0 = t * 128
        gate2, r_gate2 = gate2r[(t // TPS) % 2]
        x1_t, r_x1t = x1r[slot]
        acc, r_acc = accr[t % 3]
        st, r_st = stF[t % 3]; mv, r_mv = mvF[t % 3]; rs, r_rs = rsF[t % 3]
        o_t, r_o = ot[t % 3]
        for j in range(4):
            y_t, r_y = yg[slot * 4 + j]
            if j == 0:
                S.op("act", lambda e: e.activation(out=acc[:], in_=y_t[:], func=AF.Copy, scale=g4[:, t, 0:1]), reads=[r_y, r_g4], writes=[r_acc])
            else:
                S.op("dve", lambda e: e.scalar_tensor_tensor(out=acc[:], in0=y_t[:], scalar=g4[:, t, j:j + 1], in1=acc[:], op0=ALU.mult, op1=ALU.add), reads=[r_y, r_g4, r_acc], writes=[r_acc])
            yield
        S.op("pool", lambda e: e.tensor_tensor(out=acc[:], in0=acc[:], in1=gate2[:], op=ALU.mult), reads=[r_acc, r_gate2], writes=[r_acc])
        yield
        S.op("dve", lambda e: e.scalar_tensor_tensor(out=acc[:], in0=x1_t[:], scalar=float(ALPHA), in1=acc[:], op0=ALU.mult, op1=ALU.add), reads=[r_x1t, r_acc], writes=[r_acc])
        yield
        for j in range(2):
            S.op("dve", lambda e: e.bn_stats(out=st[:, j, :], in_=acc[:, j * 512:(j + 1) * 512]), reads=[r_acc], writes=[r_st] if j == 0 else (), awrites=() if j == 0 else [r_st])
            yield
        S.op("dve", lambda e: e.bn_aggr(out=mv[:, 0:2], in_=st[:].rearrange("p a b -> p (a b)")), reads=[r_st], writes=[r_mv])
        yield
        S.op("act", lambda e: e.activation(out=rs[:], in_=mv[:, 1:2], func=AF.Sqrt, bias=EPS), reads=[r_mv], writes=[r_rs])
        yield
        S.op("dve", lambda e: e.reciprocal(out=rs[:], in_=rs[:]), reads=[r_rs], writes=[r_rs])
        yield
        S.op("dve", lambda e: e.tensor_scalar(out=mv[:, 2:3], in0=mv[:, 0:1], scalar1=-1.0, scalar2=rs[:], op0=ALU.mult, op1=ALU.mult), reads=[r_mv, r_rs], awrites=[r_mv])
        yield
        S.op("act", lambda e: e.activation(out=o_t[:], in_=acc[:], func=AF.Identity, scale=rs[:], bias=mv[:, 2:3]), reads=[r_acc, r_mv, r_rs], writes=[r_o])
        yield
        S.op("dve", lambda e: e.tensor_tensor(out=o_t[:], in0=o_t[:], in1=ln2g[:], op=ALU.mult), reads=[r_o, r_ln2g], writes=[r_o])
        yield
        S.op("dve", lambda e: e.tensor_tensor(out=o_t[:], in0=o_t[:], in1=ln2b[:], op=ALU.add), reads=[r_o, r_ln2b], writes=[r_o])
        yield
        S.dma("sp", out_d[tok0:tok0 + 128, :], o_t[:], reads=[r_o], awrites=[r_out])
        yield

    for t0 in range(3):
        loadF(t0)
    interleave((genF(t) for t in range(NT)), 6)
    C.pop()
    C.pop()


def phase_C(nc, S, C, dbg, L):
    g_ = lambda n: L[n]
    r_in = g_("r_in"); ident_b = g_("ident_b"); r_identb = g_("r_identb"); ident_f = g_("ident_f"); r_identf = g_("r_identf")
    qT_d, r_qT = g_("qT_d"), g_("r_qT"); iqT_d, r_iqT = g_("iqT_d"), g_("r_iqT")
    kva_d, r_kva = g_("kva_d"), g_("r_kva"); kvT_d, r_kvT = g_("kvT_d"), g_("r_kvT"); ikT_d, r_ikT = g_("ikT_d"), g_("r_ikT")
    iw_d, r_iw = g_("iw_d"), g_("r_iw"); oaT_d, r_oaT = g_("oaT_d"), g_("r_oaT")
    C.push()
    w1_d, w2_d = g_("w1_d"), g_("w2_d")

    def cast_weights(i):
        e_ = i // 2
        if i % 2 == 0:
            S.dma("pool", g_("w1b_d")[e_ * D:(e_ + 1) * D, :], w1_d[e_ * D:(e_ + 1) * D, :], reads=[r_in], awrites=[g_("r_w1bd")])
        else:
            S.dma("pool", g_("w2b_d")[e_ * D:(e_ + 1) * D, :], w2_d[e_ * D:(e_ + 1) * D, :], reads=[r_in], awrites=[g_("r_w2bd")])
    tzf, r_tzf = C.sb("tzf", [128, 2, 8, 128], F32)
    tz, r_tz = C.sb("tzb", [128, 2, 8, 128], BF16)
    cfar, r_cfar = C.sb("cfar", [128, 8], F32)
    identN, r_identN = C.sb("identN", [128, 128], BF16)
    S.dma("sp", tzf[:], g_("tz_d"), reads=[r_in], writes=[r_tzf])
    S.dma("sp", cfar[:], g_("cfar_d").partition_broadcast(128), reads=[r_in], writes=[r_cfar])
    S.op("dve", lambda e: e.tensor_scalar_mul(out=identN[:], in0=ident_f[:], scalar1=-NEG), reads=[r_identf], writes=[r_identN])
    first = True
    for dl in range(2):
        for h in range(8):
            S.op("dve", lambda e: e.tensor_scalar(out=tz[:, dl, h, :], in0=tzf[:, dl, h, :], scalar1=cfar[:, h:h + 1], scalar2=None, op0=ALU.subtract),
                 reads=[r_tzf, r_cfar], writes=[r_tz] if first else (), awrites=() if first else [r_tz])
            first = False
    seqb = [(C.sb("kvT%d" % i, [128, SEQ], BF16), C.sb("ikT%d" % i, [128, SEQ], BF16), C.sb("kvaC%d" % i, [128, TPS, 136], BF16)) for i in range(2)]
    qt = C.ring("sb", "qt", 5, [128, 8, 128], BF16)
    iqt = C.ring("sb", "iqt", 4, [128, 4, 128], BF16)
    iwt = C.ring("sb", "iwC", 4, [128, 8], F32)
    scorer = C.ring("sb", "score", 3, [128, SEQ], F32)
    penr = C.ring("sb", "pen", 2, [128, SEQ], BF16)
    rl = C.ring("sb", "rl", 2, [128, 512], F32)
    m8, r_m8 = C.sb("m8", [128, 8], F32)
    expT = C.ring("sb", "expT", 2, [128, TPS * 128], BF16)
    posb = C.ring("sb", "posb", 2, [128, 8, 132], F32)
    rc, r_rc = C.sb("rcC", [128, 8], F32)
    oa, r_oa = C.sb("oa", [128, D], BF16)
    oaT = C.ring("sb", "oaT", 2, [128, 8, 128], BF16)
    pi = C.ring("ps", "pi", 2, [128, 512], F32)
    pl = C.ring("ps", "pl", 3, [128, 512], F32)
    po = C.ring("ps", "po", 2, [128, 512], F32)
    ptr = C.ring("ps", "ptrC", 1, [128, 512], F32)
    NTILE = NB * TPS

    def load_seq(b):
        (kvT, r_kvTs), (ikT, r_ikTs), (kva, r_kvas) = seqb[b % 2]
        s0 = b * SEQ
        S.dma("sp", kvT[:], kvT_d[:, s0:s0 + SEQ], reads=[r_kvT], writes=[r_kvTs])
        S.dma("sp", ikT[:], ikT_d[:, s0:s0 + SEQ], reads=[r_ikT], writes=[r_ikTs])
        S.dma("sp", kva[:], kva_d[s0:s0 + SEQ, :].rearrange("(k p) c -> p k c", p=128), reads=[r_kva], writes=[r_kvas])

    def load_tile(i):
        tok0 = i * 128
        S.dma("sp", qt[i % 5][0][:], qT_d[:, :, tok0:tok0 + 128].rearrange("h p t -> p h t"), reads=[r_qT], writes=[qt[i % 5][1]])
        S.dma("sp", iqt[i % 4][0][:], iqT_d[:, :, tok0:tok0 + 128].rearrange("h p t -> p h t"), reads=[r_iqT], writes=[iqt[i % 4][1]])
        S.dma("sp", iwt[i % 4][0][:], iw_d[tok0:tok0 + 128, :], reads=[r_iw], writes=[iwt[i % 4][1]])

    NIT = 24
    pow2, r_pow2 = C.sb("pow2", [128, NIT + 1], F32)
    stepsr = C.ring("sb", "steps", 3, [128, NIT + 1], F32)
    bisr = C.ring("sb", "bis", 3, [128, 8], F32)
    junkb, r_junkb = C.sb("junkb", [128, SEQ], BF16)
    junkd, r_junkd = C.sb("junkd", [128, SEQ], BF16)
    for j in range(NIT + 1):
        S.op("pool", lambda e: e.memset(pow2[:, j:j + 1], float(2.0 ** -(j + 1))), writes=[r_pow2] if j == 0 else (), awrites=() if j == 0 else [r_pow2])

    def prep_score_gen(i):
        b, t = divmod(i, TPS)
        (ikT, r_ikTs) = seqb[b % 2][1]
        iq_t, r_iq = iqt[i % 4]
        iw_t, r_iwt = iwt[i % 4]
        pen, r_pen = penr[i % 2]
        score, r_score = scorer[i % 3]
        steps, r_steps = stepsr[i % 3]
        bis, r_bis = bisr[i % 3]
        N = 128 * (t + 1)
        if t < 2:
            return
            yield
        for kg in range(0, N, 512):
            w = min(512, N - kg)
            for h in range(8):
                pr, hf = divmod(h, 2)
                p_t, r_p = pi[h % 2]
                S.op("pe", lambda e: e.matmul(p_t[:, 0:w], lhsT=iq_t[64 * hf:64 * hf + 64, pr, :], rhs=ikT[64 * hf:64 * hf + 64, kg:kg + w], start=True, stop=True),
                     reads=[r_iq, r_ikTs], writes=[r_p])
                r_t, r_r = rl[h % 2]
                if h == 0:
                    S.op("dve", lambda e: e.tensor_scalar(out=score[:, kg:kg + w], in0=p_t[:, 0:w], scalar1=0.0, scalar2=iw_t[:, 0:1], op0=ALU.max, op1=ALU.mult),
                         reads=[r_p, r_iwt], writes=[r_score] if kg == 0 else (), awrites=() if kg == 0 else [r_score])
                elif h % 2 == 1:
                    S.op("dve", lambda e: e.tensor_scalar(out=r_t[:, 0:w], in0=p_t[:, 0:w], scalar1=0.0, scalar2=iw_t[:, h:h + 1], op0=ALU.max, op1=ALU.mult),
                         reads=[r_p, r_iwt], writes=[r_r])
                    S.op("pool", lambda e: e.tensor_tensor(out=score[:, kg:kg + w], in0=score[:, kg:kg + w], in1=r_t[:, 0:w], op=ALU.add),
                         reads=[r_r, r_score], awrites=[r_score])
                else:
                    S.op("act", lambda e: e.activation(out=r_t[:, 0:w], in_=p_t[:, 0:w], func=AF.Relu), reads=[r_p], writes=[r_r])
                    S.op("dve", lambda e: e.scalar_tensor_tensor(out=score[:, kg:kg + w], in0=r_t[:, 0:w], scalar=iw_t[:, h:h + 1], in1=score[:, kg:kg + w], op0=ALU.mult, op1=ALU.add),
                         reads=[r_r, r_iwt, r_score], awrites=[r_score])
                yield
        S.op("dve", lambda e: e.tensor_reduce(out=bis[:, 0:1], in_=score[:, 0:N - 64], axis=AX.X, op=ALU.min), reads=[r_score], writes=[r_bis])
        S.op("dve", lambda e: e.memset(score[0:64, N - 64:N], -1e30), reads=[r_score], awrites=[r_score])
        S.op("dve", lambda e: e.max(out=m8[:], in_=score[:, 0:N]), reads=[r_score], writes=[r_m8])
        S.op("dve", lambda e: e.tensor_tensor(out=bis[:, 1:2], in0=m8[:, 0:1], in1=bis[:, 0:1], op=ALU.subtract), reads=[r_m8, r_bis], awrites=[r_bis])
        S.op("dve", lambda e: e.tensor_scalar(out=steps[:], in0=pow2[:], scalar1=bis[:, 1:2], scalar2=None, op0=ALU.mult), reads=[r_pow2, r_bis], writes=[r_steps])
        S.op("dve", lambda e: e.tensor_tensor(out=bis[:, 2:3], in0=bis[:, 0:1], in1=steps[:, 0:1], op=ALU.add), reads=[r_bis, r_steps], awrites=[r_bis])

    def prep_score(i):
        for _ in prep_score_gen(i):
            pass

    def prep_iter(i, j):
        b, t = divmod(i, TPS)
        if t < 2:
            return
        N = 128 * (t + 1)
        score, r_score = scorer[i % 3]
        steps, r_steps = stepsr[i % 3]
        bis, r_bis = bisr[i % 3]
        if j % 3 != 2:
            S.op("act", lambda e: e.activation(out=junkb[:, 0:N], in_=score[:, 0:N], func=AF.Sign, scale=-1.0, bias=bis[:, 2:3], accum_out=bis[:, 4:5]),
                 reads=[r_score, r_bis], writes=[r_junkb], awrites=[r_bis])
            S.op("dve", lambda e: e.tensor_scalar(out=bis[:, 3:4], in0=bis[:, 4:5], scalar1=float(N - 511), scalar2=steps[:, j:j + 1], op0=ALU.is_le, op1=ALU.mult),
                 reads=[r_bis, r_steps], awrites=[r_bis])
        else:
            S.op("dve", lambda e: e.tensor_scalar(out=junkd[:, 0:N], in0=score[:, 0:N], scalar1=bis[:, 2:3], scalar2=None, op0=ALU.is_ge, op1=ALU.add, accum_out=bis[:, 6:7]),
                 reads=[r_score, r_bis], writes=[r_junkd], awrites=[r_bis])
            S.op("dve", lambda e: e.tensor_scalar(out=bis[:, 3:4], in0=bis[:, 6:7], scalar1=255.5, scalar2=steps[:, j:j + 1], op0=ALU.is_ge, op1=ALU.mult),
                 reads=[r_bis, r_steps], awrites=[r_bis])
        S.op("dve", lambda e: e.scalar_tensor_tensor(out=bis[:, 2:3], in0=bis[:, 3:4], scalar=steps[:, j + 1:j + 2], in1=bis[:, 2:3], op0=ALU.subtract, op1=ALU.add),
             reads=[r_bis, r_steps], awrites=[r_bis])

    def prep_fin(i):
        b, t = divmod(i, TPS)
        N = 128 * (t + 1)
        pen, r_pen = penr[i % 2]
        if t < 2:
            S.op("dve", lambda e: e.memset(pen[:, 0:N], 0.0), writes=[r_pen])
            S.op("dve", lambda e: e.memset(pen[0:64, N - 64:N], -1.0), awrites=[r_pen])
            return
        score, r_score = scorer[i % 3]
        steps, r_steps = stepsr[i % 3]
        bis, r_bis = bisr[i % 3]
        S.op("dve", lambda e: e.tensor_tensor(out=bis[:, 5:6], in0=bis[:, 2:3], in1=steps[:, NIT:NIT + 1], op=ALU.subtract), reads=[r_bis, r_steps], awrites=[r_bis])
        S.op("dve", lambda e: e.tensor_scalar(out=pen[:, 0:N], in0=score[:, 0:N], scalar1=bis[:, 5:6], scalar2=1.0, op0=ALU.is_ge, op1=ALU.subtract),
             reads=[r_score, r_bis], writes=[r_pen])

    state = {"plc": 0, "hc": 0}

    def attend_head(i, h, part):
        b, t = divmod(i, TPS)
        (kvT, r_kvTs), _, (kva, r_kvas) = seqb[b % 2]
        q_t, r_q = qt[i % 5]
        pen, r_pen = penr[i % 2]
        ps_t, r_ps = posb[i % 2]
        nkb = t + 1
        e_t, r_e = expT[h % 2]
        if part == 0:
            for kg in range(0, nkb, 4):
                nb_ = min(4, nkb - kg)
                p_t, r_p = pl[state["plc"] % 3]
                state["plc"] += 1
                for ii in range(nb_):
                    kb = kg + ii
                    near = kb >= t - 1
                    cs_ = slice(ii * 128, (ii + 1) * 128)
                    S.op("pe", lambda e: e.matmul(p_t[:, cs_], lhsT=kvT[:, kb * 128:(kb + 1) * 128], rhs=q_t[:, h, :], start=True, stop=False),
                         reads=[r_kvTs, r_q], writes=[r_p] if ii == 0 else (), awrites=() if ii == 0 else [r_p])
                    S.op("pe", lambda e: e.matmul(p_t[:, cs_], lhsT=pen[:, kb * 128:(kb + 1) * 128], rhs=identN[:], start=False, stop=(not near)),
                         reads=[r_pen, r_identN], awrites=[r_p])
                    if near:
                        S.op("pe", lambda e: e.matmul(p_t[:, cs_], lhsT=ident_b[:], rhs=tz[:, t - kb, h, :], start=False, stop=True),
                             reads=[r_identb, r_tz], awrites=[r_p])
                S.op("act", lambda e: e.activation(out=e_t[:, kg * 128:(kg + nb_) * 128], in_=p_t[:, 0:nb_ * 128], func=AF.Exp), reads=[r_p],
                     writes=[r_e] if kg == 0 else (), awrites=() if kg == 0 else [r_e])
        else:
            o_t, r_o = po[h % 2]
            for kb in range(nkb):
                S.op("pe", lambda e: e.matmul(o_t[:, 0:129], lhsT=e_t[:, kb * 128:(kb + 1) * 128], rhs=kva[:, kb, 0:129], start=(kb == 0), stop=(kb == nkb - 1)),
                     reads=[r_e, r_kvas], writes=[r_o] if kb == 0 else (), awrites=() if kb == 0 else [r_o])
            S.op("act", lambda e: e.activation(out=ps_t[:, h, 0:129], in_=o_t[:, 0:129], func=AF.Copy), reads=[r_o],
                 writes=[r_ps] if h == 0 else (), awrites=() if h == 0 else [r_ps])

    def finalize(i):
        tok0 = i * 128
        ps_t, r_ps = posb[i % 2]
        S.op("dve", lambda e: e.reciprocal(out=rc[:], in_=ps_t[:, :, 128]), reads=[r_ps], writes=[r_rc])
        S.op("dve", lambda e: e.tensor_tensor(out=oa[:].rearrange("p (h c) -> p h c", h=8), in0=ps_t[:, :, 0:128], in1=rc[:].unsqueeze(2).to_broadcast([128, 8, 128]), op=ALU.mult),
             reads=[r_ps, r_rc], writes=[r_oa])
        pT, r_pT = ptr[0]
        pT_b = pT[:].bitcast(BF16)
        for k in range(8):
            S.op("pe", lambda e: e.transpose(out=pT_b[:, k * 128:(k + 1) * 128], in_=oa[:, k * 128:(k + 1) * 128], identity=ident_b[:]),
                 reads=[r_oa, r_identb], writes=[r_pT] if k == 0 else (), awrites=() if k == 0 else [r_pT])
        oT, r_oT = oaT[i % 2]
        S.op("act", lambda e: e.activation(out=oT[:].rearrange("p k t -> p (k t)"), in_=pT_b, func=AF.Copy), reads=[r_pT], writes=[r_oT])
        S.dma("sp", oaT_d[:, :, tok0:tok0 + 128].rearrange("c p t -> p c t"), oT[:], reads=[r_oT], awrites=[r_oaT])

    HALF = NIT // 2
    load_seq(0)
    for i0 in range(4):
        load_tile(i0)
    prep_score(0)
    for j in range(NIT):
        prep_iter(0, j)
    prep_fin(0)
    prep_score(1)
    for j in range(HALF):
        prep_iter(1, j)
    prep_score(2)
    for i in range(NTILE):
        b, t = divmod(i, TPS)
        if t == 0 and b + 1 < NB:
            load_seq(b + 1)
        if i + 4 < NTILE:
            load_tile(i + 4)
        cast_weights(i)
        n1 = i + 1 < NTILE
        n2 = i + 2 < NTILE
        gen = prep_score_gen(i + 3) if i + 3 < NTILE else iter(())
        npieces = 8 * ((128 * (((i + 3) % TPS) + 1) + 511) // 512) if (i + 3 < NTILE and (i + 3) % TPS >= 2) else 0
        ppb = (npieces + 7) // 8
        sched = []
        for k in range(HALF):
            if n1:
                sched.append((i + 1, HALF + k))
            if n2:
                sched.append((i + 2, k))
        per = (len(sched) + 7) // 8
        attend_head(i, 0, 0)
        for h in range(8):
            if h + 1 < 8:
                attend_head(i, h + 1, 0)
            attend_head(i, h, 1)
            its = sched[h * per:(h + 1) * per]
            for n_, (ti_, j) in enumerate(its):
                prep_iter(ti_, j)
                if n_ < ppb:
                    next(gen, None)
            for _ in range(max(0, ppb - len(its))):
                next(gen, None)
        for _ in gen:
            pass
        if n1:
            prep_fin(i + 1)
        if i >= 1:
            finalize(i - 1)
    finalize(NTILE - 1)
    C.pop()


def phase_B(nc, S, C, dbg, L):
    g_ = lambda n: L[n]
    r_in = g_("r_in"); ident_b = g_("ident_b"); r_identb = g_("r_identb")
    xbcT_d, r_xbcT = g_("xbcT_d"), g_("r_xbcT"); dt_d, r_dt = g_("dt_d"), g_("r_dt"); zs_d, r_zs = g_("zs_d"), g_("r_zs")
    obT_d, r_obT = g_("obT_d"), g_("r_obT")
    C.push()
    convw, r_convw = C.sb("convw", [128, 16, 4], F32)
    convb, r_convb = C.sb("convb", [128, 16], F32)
    dg, r_dg = C.sb("dg", [128, 16, 4, 128], BF16)
    identf2, r_identf2 = C.sb("identf2", [128, 128], F32)
    a_bc, r_abc = C.sb("a_bc", [128, 16], F32)
    dskip_bc, r_dskip = C.sb("dskip_bc", [128, 16], F32)
    normw_bc, r_normw = C.sb("normw_bc", [128, D], F32)
    triU, r_triU = C.sb("triU", [128, 128], F32)
    SLm, r_SL = C.sb("SLm", [128, 128], F32)
    onesf, r_onesf = C.sb("onesf", [128, 128], F32)
    negm4, r_negm4 = C.sb("negm4", [128, 512], BF16)
    S.dma("sp", convw[:], g_("convw_d"), reads=[r_in], writes=[r_convw])
    S.dma("sp", convb[:], g_("convb_d"), reads=[r_in], writes=[r_convb])
    S.dma("sp", identf2[:], g_("ident_d"), reads=[r_in], writes=[r_identf2])
    S.dma("sp", a_bc[:], g_("alog_d").partition_broadcast(128), reads=[r_in], writes=[r_abc])
    S.dma("sp", dskip_bc[:], g_("dskip_d").partition_broadcast(128), reads=[r_in], writes=[r_dskip])
    S.dma("sp", normw_bc[:], g_("normw_d").partition_broadcast(128), reads=[r_in], writes=[r_normw])
    S.dma("sp", triU[:], g_("triU_d"), reads=[r_in], writes=[r_triU])
    S.dma("sp", SLm[:], g_("SL_d"), reads=[r_in], writes=[r_SL])
    S.dma("pool", negm4[:], g_("negm4_d"), reads=[r_in], writes=[r_negm4])
    S.op("dve", lambda e: e.memset(onesf[:], 1.0), writes=[r_onesf])
    S.op("act", lambda e: e.activation(out=a_bc[:], in_=a_bc[:], func=AF.Exp), reads=[r_abc], writes=[r_abc])
    S.op("dve", lambda e: e.tensor_scalar_mul(out=a_bc[:], in0=a_bc[:], scalar1=-1.0), reads=[r_abc], writes=[r_abc])
    first = True
    for j in range(16):
        for k in range(4):
            S.op("dve", lambda e: e.tensor_scalar_mul(out=dg[:, j, k, :], in0=identf2[:], scalar1=convw[:, j, k:k + 1]),
                 reads=[r_identf2, r_convw], writes=[r_dg] if first else (), awrites=() if first else [r_dg])
            first = False

    bank = C.ring("ps", "bk", 8, [128, 512], F32)
    xh = C.ring("sb", "xh", 4, [128, 16, 131], BF16)
    dtl = C.ring("sb", "dtl", 4, [128, 16], F32)
    zl = C.ring("sb", "zl", 4, [128, D], BF16)
    xact_r = C.ring("sb", "xact", 2, [128, 16, 128], BF16)
    xs_r = C.ring("sb", "xs_tok", 2, [128, D], BF16)
    Bt_r = C.ring("sb", "B_tok", 2, [128, 512], BF16)
    adt_r = C.ring("sb", "adt", 2, [128, 16], F32)
    sm_r = C.ring("sb", "smB", 2, [128, 8, 16], F32)
    A_r = C.ring("sb", "Amat", 2, [128, 16, 128], F32)
    Lt_r = C.ring("sb", "Lt", 2, [128, 2, 512], F32)
    Mt_r = C.ring("sb", "Mt", 2, [128, 16, 128], BF16)
    xdt_r = C.ring("sb", "xdt", 2, [128, D], BF16)
    xdd_r = C.ring("sb", "xdd", 2, [128, D], BF16)
    prev_f, r_pf = C.sb("prev_f", [128, D], F32)
    prev_b, r_pb = C.sb("prev_b", [128, D], BF16)
    t1_r = C.ring("sb", "t1", 2, [128, D], F32)
    t2_r = C.ring("sb", "t2", 2, [128, D], F32)
    junk, r_junk = C.sb("junkB", [128, 256], F32)
    ob_r = C.ring("sb", "ob", 2, [128, D], BF16)
    obT = C.ring("sb", "obT", 2, [128, 8, 128], BF16)

    def bc3(ap2, n):
        return ap2.unsqueeze(2).to_broadcast([128, 16, n])

    def v3(t, n=64):
        return t.rearrange("p (h q) -> p h q", q=n)

    def load_chunk(b, t, slot):
        tok0 = b * SEQ + t * 128
        x_t, r_x = xh[slot]
        if t == 0:
            S.op("pool", lambda e: e.memset(x_t[:, :, 0:3], 0.0), writes=[r_x])
            S.dma("sp", x_t[:, :, 3:131], xbcT_d[:, :, tok0:tok0 + 128].rearrange("c p t -> p c t"), reads=[r_xbcT], awrites=[r_x])
        else:
            S.dma("sp", x_t[:, :, :], xbcT_d[:, :, tok0 - 3:tok0 + 128].rearrange("c p t -> p c t"), reads=[r_xbcT], writes=[r_x])
        S.dma("sp", dtl[slot][0][:], dt_d[tok0:tok0 + 128, :], reads=[r_dt], writes=[dtl[slot][1]])
        S.dma("sp", zl[slot][0][:], zs_d[tok0:tok0 + 128, :], reads=[r_zs], writes=[zl[slot][1]])

    nch = NB * TPS

    def chunk_gen(ci):
        b, t = divmod(ci, TPS)
        slot = ci % 4
        sl2 = ci % 2
        tok0 = b * SEQ + t * 128
        if ci + 2 < nch:
            load_chunk((ci + 2) // TPS, (ci + 2) % TPS, (ci + 2) % 4)
        x_t, r_x = xh[slot]
        d_t, r_d = dtl[slot]
        z_t, r_z = zl[slot]
        xact, r_xact = xact_r[sl2]; xs_tok, r_xs = xs_r[sl2]; B_tok, r_Bt = Bt_r[sl2]; adt, r_adt = adt_r[sl2]; sm, r_sm = sm_r[sl2]
        Amat, r_A = A_r[sl2]; Lt, r_Lt = Lt_r[sl2]; Mt, r_Mt = Mt_r[sl2]; xdt, r_xdt = xdt_r[sl2]; xdd, r_xdd = xdd_r[sl2]
        t1, r_t1 = t1_r[sl2]; t2, r_t2 = t2_r[sl2]; ob, r_ob = ob_r[sl2]
        for jg in range(4):
            pc, r_pc = bank[jg % 2]
            for jj in range(4):
                j = jg * 4 + jj
                for k in range(4):
                    S.op("pe", lambda e: e.matmul(pc[:, jj * 128:(jj + 1) * 128], lhsT=dg[:, j, k, :], rhs=x_t[:, j, k:k + 128], start=(k == 0), stop=(k == 3)),
                         reads=[r_dg, r_x], writes=[r_pc] if (jj == 0 and k == 0) else (), awrites=() if (jj == 0 and k == 0) else [r_pc])
                    yield
            for jj in range(4):
                j = jg * 4 + jj
                S.op("act", lambda e: e.activation(out=xact[:, j, :], in_=pc[:, jj * 128:(jj + 1) * 128], func=AF.Silu, bias=convb[:, j:j + 1]),
                     reads=[r_pc, r_convb], writes=[r_xact] if j == 0 else (), awrites=() if j == 0 else [r_xact])
                yield
        pxs, r_pxs = bank[2]
        pB, r_pB = bank[3]
        pxs_b = pxs[:].bitcast(BF16)
        pB_b = pB[:].bitcast(BF16)
        for k in range(8):
            S.op("pe", lambda e: e.transpose(out=pxs_b[:, k * 128:(k + 1) * 128], in_=xact[:, k, :], identity=ident_b[:]),
                 reads=[r_xact, r_identb], writes=[r_pxs] if k == 0 else (), awrites=() if k == 0 else [r_pxs])
            yield
        for k in range(4):
            S.op("pe", lambda e: e.transpose(out=pB_b[:, k * 128:(k + 1) * 128], in_=xact[:, 8 + k, :], identity=ident_b[:]),
                 reads=[r_xact, r_identb], writes=[r_pB] if k == 0 else (), awrites=() if k == 0 else [r_pB])
            yield
        S.op("act", lambda e: e.activation(out=xs_tok[:], in_=pxs_b, func=AF.Copy), reads=[r_pxs], writes=[r_xs])
        yield
        S.op("act", lambda e: e.activation(out=B_tok[:], in_=pB_b[:, 0:512], func=AF.Copy), reads=[r_pB], writes=[r_Bt])
        yield
        S.op("dve", lambda e: e.tensor_tensor(out=adt[:], in0=d_t[:], in1=a_bc[:], op=ALU.mult), reads=[r_d, r_abc], writes=[r_adt])
        yield
        S.op("pe", lambda e: e.matmul(pB[:, 256:272], lhsT=triU[:], rhs=adt[:], start=True, stop=True), reads=[r_triU, r_adt], awrites=[r_pB])
        yield
        S.op("pe", lambda e: e.matmul(pB[:, 272:288], lhsT=onesf[:], rhs=adt[:], start=True, stop=True), reads=[r_onesf, r_adt], awrites=[r_pB])
        yield
        S.op("act", lambda e: e.activation(out=sm[:, 0, :], in_=pB[:, 256:272], func=AF.Exp), reads=[r_pB], writes=[r_sm])
        yield
        S.op("act", lambda e: e.activation(out=sm[:, 1, :], in_=pB[:, 272:288], func=AF.Copy), reads=[r_pB], awrites=[r_sm])
        yield
        S.op("act", lambda e: e.activation(out=sm[:, 2, :], in_=pB[:, 272:288], func=AF.Exp), reads=[r_pB], awrites=[r_sm])
        yield
        S.op("dve", lambda e: e.tensor_tensor(out=sm[:, 3, :], in0=sm[:, 1, :], in1=pB[:, 256:272], op=ALU.subtract), reads=[r_sm, r_pB], awrites=[r_sm])
        yield
        S.op("act", lambda e: e.activation(out=sm[:, 4, :], in_=sm[:, 3, :], func=AF.Exp), reads=[r_sm], awrites=[r_sm])
        yield
        S.op("dve", lambda e: e.tensor_tensor(out=Amat[:], in0=triU[:].unsqueeze(1).to_broadcast([128, 16, 128]), in1=bc3(adt[:], 128), op=ALU.mult),
             reads=[r_triU, r_adt], writes=[r_A])
        yield
        pCB, r_pCB = bank[4]
        for g in range(4):
            S.op("pe", lambda e: e.matmul(pCB[:, g * 128:(g + 1) * 128], lhsT=xact[:, 8 + g, :], rhs=xact[:, 12 + g, :], start=True, stop=True),
                 reads=[r_xact], writes=[r_pCB] if g == 0 else (), awrites=() if g == 0 else [r_pCB])
            yield
        S.op("dve", lambda e: e.tensor_tensor(out=v3(xdt[:]), in0=v3(xs_tok[:]), in1=bc3(d_t[:], 64), op=ALU.mult), reads=[r_xs, r_d], writes=[r_xdt])
        yield
        S.op("pool", lambda e: e.tensor_tensor(out=v3(xdd[:]), in0=v3(xdt[:]), in1=bc3(sm[:, 4, :], 64), op=ALU.mult), reads=[r_xdt, r_sm], writes=[r_xdd])
        yield
        for g in range(4):
            pD, r_pD = bank[5 + g % 2]
            S.op("pe", lambda e: e.matmul(pD[:], lhsT=SLm[:], rhs=Amat[:, 4 * g:4 * g + 4, :], start=True, stop=False), reads=[r_SL, r_A], writes=[r_pD])
            yield
            S.op("pe", lambda e: e.matmul(pD[:], lhsT=ident_b[:], rhs=negm4[:], start=False, stop=True), reads=[r_identb, r_negm4], awrites=[r_pD])
            yield
            S.op("act", lambda e: e.activation(out=Lt[:, g % 2, :], in_=pD[:], func=AF.Exp), reads=[r_pD], writes=[r_Lt] if g % 2 == 0 else (), awrites=() if g % 2 == 0 else [r_Lt])
            yield
            S.op("dve", lambda e: e.tensor_tensor(out=Mt[:, 4 * g:4 * g + 4, :], in0=Lt[:, g % 2, :].rearrange("p (h l) -> p h l", h=4),
                                                 in1=pCB[:, g * 128:(g + 1) * 128].unsqueeze(1).to_broadcast([128, 4, 128]), op=ALU.mult),
                 reads=[r_Lt, r_pCB], writes=[r_Mt] if g == 0 else (), awrites=() if g == 0 else [r_Mt])
            yield
        if t == 0:
            S.op("pool", lambda e: e.memset(prev_f[:], 0.0), writes=[r_pf])
            yield
            S.op("pool", lambda e: e.memset(prev_b[:], 0.0), writes=[r_pb])
            yield
        for hh in range(2):
            pY, r_pY = bank[5]
            pO, r_pO = bank[6]
            pS, r_pS = bank[7]
            c0 = hh * 512
            for h8 in range(8):
                h = hh * 8 + h8
                S.op("pe", lambda e: e.matmul(pY[:, h8 * 64:(h8 + 1) * 64], lhsT=Mt[:, h, :], rhs=xdt[:, h * 64:(h + 1) * 64], start=True, stop=True),
                     reads=[r_Mt, r_xdt], writes=[r_pY] if h8 == 0 else (), awrites=() if h8 == 0 else [r_pY])
                yield
            for g2 in range(2):
                g = hh * 2 + g2
                S.op("pe", lambda e: e.matmul(pO[:, g2 * 256:(g2 + 1) * 256], lhsT=xact[:, 12 + g, :], rhs=prev_b[:, g * 256:(g + 1) * 256], start=True, stop=True),
                     reads=[r_xact, r_pb], writes=[r_pO] if g2 == 0 else (), awrites=() if g2 == 0 else [r_pO])
                yield
            for g2 in range(2):
                g = hh * 2 + g2
                S.op("pe", lambda e: e.matmul(pS[:, g2 * 256:(g2 + 1) * 256], lhsT=B_tok[:, g * 128:(g + 1) * 128], rhs=xdd[:, g * 256:(g + 1) * 256], start=True, stop=True),
                     reads=[r_Bt, r_xdd], writes=[r_pS] if g2 == 0 else (), awrites=() if g2 == 0 else [r_pS])
                yield
            hs = slice(hh * 8, hh * 8 + 8)

            def v8(ap):
                return ap.rearrange("p (h q) -> p h q", q=64)
            ex8 = sm[:, 0, hs].unsqueeze(2).to_broadcast([128, 8, 64])
            cd8 = sm[:, 2, hs].unsqueeze(2).to_broadcast([128, 8, 64])
            ds8 = dskip_bc[:, hs].unsqueeze(2).to_broadcast([128, 8, 64])
            S.op("dve", lambda e: e.tensor_tensor(out=v8(t1[:, c0:c0 + 512]), in0=v8(pO[:]), in1=ex8, op=ALU.mult), reads=[r_pO, r_sm], writes=[r_t1] if hh == 0 else (), awrites=() if hh == 0 else [r_t1])
            yield
            S.op("dve", lambda e: e.tensor_tensor(out=t1[:, c0:c0 + 512], in0=t1[:, c0:c0 + 512], in1=pY[:], op=ALU.add), reads=[r_t1, r_pY], awrites=[r_t1])
            yield
            S.op("pool", lambda e: e.tensor_tensor(out=v8(t2[:, c0:c0 + 512]), in0=v8(xs_tok[:, c0:c0 + 512]), in1=ds8, op=ALU.mult), reads=[r_xs, r_dskip], writes=[r_t2] if hh == 0 else (), awrites=() if hh == 0 else [r_t2])
            yield
            S.op("dve", lambda e: e.tensor_tensor(out=v8(prev_f[:, c0:c0 + 512]), in0=v8(prev_f[:, c0:c0 + 512]), in1=cd8, op=ALU.mult), reads=[r_pf, r_sm], awrites=[r_pf])
            yield
            S.op("dve", lambda e: e.tensor_tensor(out=prev_f[:, c0:c0 + 512], in0=prev_f[:, c0:c0 + 512], in1=pS[:], op=ALU.add), reads=[r_pf, r_pS], awrites=[r_pf])
            yield
            S.op("act", lambda e: e.activation(out=prev_b[:, c0:c0 + 512], in_=prev_f[:, c0:c0 + 512], func=AF.Copy), reads=[r_pf, r_pO], awrites=[r_pb])
            yield
        S.op("pool", lambda e: e.tensor_tensor(out=t1[:], in0=t1[:], in1=t2[:], op=ALU.add), reads=[r_t1, r_t2], writes=[r_t1])
        yield
        S.op("pool", lambda e: e.tensor_tensor(out=t1[:], in0=t1[:], in1=z_t[:], op=ALU.mult), reads=[r_t1, r_z], writes=[r_t1])
        yield
        for g in range(4):
            S.op("act", lambda e: e.activation(out=junk[:], in_=t1[:, g * 256:(g + 1) * 256], func=AF.Square, accum_out=sm[:, 5, g:g + 1]),
                 reads=[r_t1], writes=[r_junk], awrites=[r_sm])
            yield
        S.op("act", lambda e: e.activation(out=sm[:, 5, 4:8], in_=sm[:, 5, 0:4], func=AF.Sqrt, scale=1.0 / 256.0, bias=EPS), reads=[r_sm], awrites=[r_sm])
        yield
        S.op("dve", lambda e: e.reciprocal(out=sm[:, 5, 8:12], in_=sm[:, 5, 4:8]), reads=[r_sm], awrites=[r_sm])
        yield
        S.op("dve", lambda e: e.tensor_tensor(out=t1[:].rearrange("p (g q) -> p g q", g=4), in0=t1[:].rearrange("p (g q) -> p g q", g=4),
                                             in1=sm[:, 5, 8:12].unsqueeze(2).to_broadcast([128, 4, 256]), op=ALU.mult), reads=[r_t1, r_sm], writes=[r_t1])
        yield
        S.op("dve", lambda e: e.tensor_tensor(out=ob[:], in0=t1[:], in1=normw_bc[:], op=ALU.mult), reads=[r_t1, r_normw], writes=[r_ob])
        yield
        pT, r_pT = bank[4]
        pT_b = pT[:].bitcast(BF16)
        for k in range(8):
            S.op("pe", lambda e: e.transpose(out=pT_b[:, k * 128:(k + 1) * 128], in_=ob[:, k * 128:(k + 1) * 128], identity=ident_b[:]),
                 reads=[r_ob, r_identb], writes=[r_pT] if k == 0 else (), awrites=() if k == 0 else [r_pT])
            yield
        o_t, r_o = obT[sl2]
        S.op("act", lambda e: e.activation(out=o_t[:].rearrange("p k t -> p (k t)"), in_=pT_b, func=AF.Copy), reads=[r_pT], writes=[r_o])
        yield
        S.dma("sp", obT_d[:, :, tok0:tok0 + 128].rearrange("c p t -> p c t"), o_t[:], reads=[r_o], awrites=[r_obT])
        yield

    load_chunk(0, 0, 0)
    load_chunk(0, 1, 1)
    interleave((chunk_gen(ci) for ci in range(nch)), B_STAGGER)
    C.pop()


def _t5_bucket_np(rel):
    half, max_exact = 16, 8
    ret = (rel > 0).astype(np.int32) * half
    n = np.abs(rel)
    nf = np.maximum(n, 1).astype(np.float32)
    large = max_exact + (np.log(nf / np.float32(max_exact)) / np.float32(np.log(128.0 / 8.0)) * np.float32(half - max_exact)).astype(np.int32)
    large = np.minimum(large, half - 1)
    return ret + np.where(n < max_exact, n, large)


def _t5_blocks(rel_bias):
    k = np.arange(128)[:, None]
    q = np.arange(128)[None, :]
    out = np.zeros((128, 2, 8, 128), np.float32)
    for dl in range(2):
        bk = _t5_bucket_np((k - 128 * dl) - q)
        for h in range(8):
            out[:, dl, h, :] = rel_bias[bk, h]
    return out


def host_inputs(inputs, core):
    b0 = core * NB
    f = lambda a: np.ascontiguousarray(a, dtype=np.float32)
    c = inputs["c"][b0:b0 + NB]
    m = {
        "x": f(inputs["x"][b0:b0 + NB].reshape(NTOK, D)),
        "cT": f(c.reshape(NB, 8, 128).transpose(2, 1, 0)),
        "w_mod": f(inputs["w_mod"][0]),
        "b_mod": f(inputs["b_mod"][0].reshape(1, -1)),
        "w_in": f(inputs["w_in"][0]),
        "ident": np.eye(128, dtype=np.float32),
        "kv_norm_w": f(inputs["kv_norm_w"][0].reshape(1, -1)),
        "idx_k_norm_w": f(inputs["idx_k_norm_w"][0].reshape(1, -1)),
        "idx_k_norm_b": f(inputs["idx_k_norm_b"][0].reshape(1, -1)),
        "dt_bias": f(inputs["dt_bias"][0].reshape(1, -1)),
        "convw": f(inputs["conv_w"][0].reshape(4, 16, 128).transpose(2, 1, 0)),
        "convb": f(inputs["conv_b"][0].reshape(16, 128).T),
        "a_log": f(inputs["a_log"][0].reshape(1, -1)),
        "d_skip": f(inputs["d_skip"][0].reshape(1, -1)),
        "ssm_norm_w": f(inputs["ssm_norm_w"][0].reshape(1, -1)),
        "w_proj_a": f(inputs["w_proj_a"][0]), "w_proj_b": f(inputs["w_proj_b"][0]), "w_out": f(inputs["w_out"][0]),
        "ln1_g": f(inputs["ln1_g"][0].reshape(1, -1)), "ln1_b": f(inputs["ln1_b"][0].reshape(1, -1)),
        "ln2_g": f(inputs["ln2_g"][0].reshape(1, -1)), "ln2_b": f(inputs["ln2_b"][0].reshape(1, -1)),
        "w_router": f(inputs["w_router"][0]), "b_router": f(inputs["b_router"][0].reshape(1, -1)),
        "w1": f(inputs["w1"][0].reshape(NE * D, 2 * D)), "w2": f(inputs["w2"][0].reshape(NE * D, D)),
        "b1r": f(inputs["b1"][0].reshape(NE, 16, 128).transpose(0, 2, 1).reshape(NE * 128, 16)),
        "b2": f(inputs["b2"][0]),
        "sut": np.triu(np.ones((128, 128), np.float32), 1),
        "thr16": np.tile(512.0 * np.arange(16, dtype=np.float32), NE).reshape(1, -1),
        "bstart": (512.0 * np.arange(NBLK, dtype=np.float32)).reshape(1, -1),
        "kp": (np.arange(8, dtype=np.float32)[None, :] * 128 + np.arange(128, dtype=np.float32)[:, None]),
        "pcol": np.arange(128, dtype=np.float32).reshape(128, 1),
        "sut32": np.triu(np.ones((NE, NE), np.float32), 1),
        "tz": _t5_blocks(f(inputs["rel_bias"])),
        "cfar": f(inputs["rel_bias"][15:16, :]),
        "triU": np.triu(np.ones((128, 128), np.float32)),
        "SL": np.tril(np.ones((128, 128), np.float32), -1),
        "negm4": np.tile(np.tril(np.full((128, 128), NEG, np.float32), -1), (1, 4)),
    }
    return m


def kernel(**inputs):
    nc = build_program()
    in_maps = [host_inputs(inputs, c) for c in range(NCORES)]
    res = run_bass_kernel_spmd(nc, in_maps, core_ids=list(range(NCORES)))
    out = np.stack([np.asarray(r["out"]).reshape(NB, SEQ, D) for r in res.results], 0)
    return out.reshape(NCORES * NB, SEQ, D).astype(np.float32)
```
